# Optimizing a Trainium2 kernel written in Bass

```python
import math
import jax, jax.numpy as jnp
from jax import lax
import numpy as np

D_MODEL = 1024
BATCH = 8
SEQ = 4096
DEPTH = 1

CHUNK = 64
MEM_LEN = 256
CONV_WIDTH = D_MODEL // 2
CONV_KERNEL = 31
SSM_WIDTH = D_MODEL // 2
SSM_GROUP = 16
SSM_GROUPS = SSM_WIDTH // SSM_GROUP
SSM_STATE = 64
XATTN_HEADS = 4
XATTN_HEAD_DIM = 128
XATTN_WIDTH = XATTN_HEADS * XATTN_HEAD_DIM
N_BRANCH = 3
MOE_GROUPS = 4
EXPERTS_PER_GROUP = 8
N_EXPERTS = MOE_GROUPS * EXPERTS_PER_GROUP
TOP_K = 2
D_EXPERT = D_MODEL // 4
EPS = 1e-6
IN_COLS = 2 * CONV_WIDTH + SSM_WIDTH + XATTN_WIDTH + N_BRANCH * D_MODEL

kernel_name = "hybrid_conv_s5_memxattn_hmoe_block"


def rmsnorm(x, g):
    x32 = x.astype(jnp.float32)
    y = x32 * lax.rsqrt(jnp.mean(x32 * x32, axis=-1, keepdims=True) + EPS)
    return (y * g.astype(jnp.float32)).astype(x.dtype)


def layernorm(x, g, b):
    x32 = x.astype(jnp.float32)
    mu = jnp.mean(x32, axis=-1, keepdims=True)
    xc = x32 - mu
    var = jnp.mean(xc * xc, axis=-1, keepdims=True)
    y = xc * lax.rsqrt(var + EPS) * g.astype(jnp.float32) + b.astype(jnp.float32)
    return y.astype(x.dtype)


def conformer_conv(u2, w_dw, b_dw, ln_g, ln_b, w_pw):
    a, gate = jnp.split(u2, 2, axis=-1)
    v = a * jax.nn.sigmoid(gate)
    v = lax.conv_general_dilated(
        v, w_dw[:, None, :], window_strides=(1,),
        padding=[(CONV_KERNEL - 1, 0)],
        dimension_numbers=("NWC", "WIO", "NWC"),
        feature_group_count=CONV_WIDTH) + b_dw
    v = jax.nn.silu(layernorm(v, ln_g, ln_b))
    return v @ w_pw


def _ssm_combine(left, right):
    a_l, b_l = left
    a_r, b_r = right
    return a_l * a_r, a_r * b_l + b_r


def s5_ssm(u, lam_re, lam_im, log_dt, b_re, b_im, c_re, c_im, d, w_glu):
    bsz, seq, _ = u.shape
    ug = u.reshape(bsz, seq, SSM_GROUPS, SSM_GROUP).astype(jnp.float32)
    lam = lax.complex(lam_re.astype(jnp.float32), lam_im.astype(jnp.float32))
    dt = jnp.exp(log_dt.astype(jnp.float32))
    a_bar = jnp.exp(lam * dt[:, None])
    bmat = lax.complex(b_re.astype(jnp.float32), b_im.astype(jnp.float32))
    b_bar = ((a_bar - 1.0) / lam)[:, :, None] * bmat
    cmat = lax.complex(c_re.astype(jnp.float32), c_im.astype(jnp.float32))
    bu = jnp.einsum("bsgc,gpc->bsgp", ug, b_bar)
    a_elems = jnp.broadcast_to(a_bar[None, None], (1, seq, SSM_GROUPS, SSM_STATE))
    _, states = lax.associative_scan(_ssm_combine, (a_elems, bu), axis=1)
    y = jnp.real(jnp.einsum("bsgp,gcp->bsgc", states, cmat))
    y = y + d.astype(jnp.float32).reshape(SSM_GROUPS, SSM_GROUP) * ug
    y = y.reshape(bsz, seq, SSM_WIDTH).astype(u.dtype)
    za, zb = jnp.split(jax.nn.gelu(y) @ w_glu, 2, axis=-1)
    return za * jax.nn.sigmoid(zb)


def memory_cross_attention(q, mem_n, w_kv, w_o):
    bsz, seq, _ = q.shape
    qh = q.reshape(bsz, seq, XATTN_HEADS, XATTN_HEAD_DIM)
    k, v = jnp.split(mem_n @ w_kv, 2, axis=-1)
    kh = k.reshape(bsz, -1, XATTN_HEADS, XATTN_HEAD_DIM)
    vh = v.reshape(bsz, -1, XATTN_HEADS, XATTN_HEAD_DIM)
    s = jnp.einsum("bshd,bmhd->bhsm", qh, kh).astype(jnp.float32) * (XATTN_HEAD_DIM ** -0.5)
    p = jax.nn.softmax(s, axis=-1).astype(vh.dtype)
    o = jnp.einsum("bhsm,bmhd->bshd", p, vh).reshape(bsz, seq, XATTN_WIDTH)
    return o @ w_o


def hier_moe(h, w_rg, b_rg, w_re, b_re, w_gate, w_up, w_down):
    bsz, seq, dm = h.shape
    t = bsz * seq
    hf = h.reshape(t, dm)
    p_group = jax.nn.softmax((hf @ w_rg + b_rg).astype(jnp.float32), axis=-1)
    p_top, g_idx = lax.top_k(p_group, 1)
    logits_e = (hf @ w_re + b_re).astype(jnp.float32).reshape(t, MOE_GROUPS, EXPERTS_PER_GROUP)
    sel = jnp.take_along_axis(logits_e, g_idx[:, :, None], axis=1)[:, 0]
    p_in = jax.nn.softmax(sel, axis=-1)
    vals, e_idx = lax.top_k(p_in, TOP_K)
    weights = p_top * vals / jnp.sum(vals, axis=-1, keepdims=True)
    expert_id = (g_idx * EXPERTS_PER_GROUP + e_idx).reshape(-1)
    order = jnp.argsort(expert_id)
    tok = order // TOP_K
    xs = hf[tok]
    sizes = jnp.bincount(expert_id, length=N_EXPERTS).astype(jnp.int32)
    a = jax.nn.silu(lax.ragged_dot(xs, w_gate, sizes)) * lax.ragged_dot(xs, w_up, sizes)
    ys = lax.ragged_dot(a, w_down, sizes)
    ys = ys * weights.reshape(-1)[order][:, None].astype(ys.dtype)
    out = jax.ops.segment_sum(ys, tok, num_segments=t)
    return out.reshape(bsz, seq, dm)


def setup_inputs(seed: int = 0) -> dict:
    key = jax.random.key(seed)
    ks = jax.random.split(key, 32)
    f32 = jnp.float32
    nrm = lambda k, shape, s: jax.random.normal(k, shape, f32) * s
    L = DEPTH
    return {
        "x": nrm(ks[0], (BATCH, SEQ, D_MODEL), 1.0),
        "mem": nrm(ks[1], (BATCH, MEM_LEN, D_MODEL), 1.0),
        "g_mix": 1.0 + nrm(ks[2], (L, D_MODEL), 0.02),
        "w_in": nrm(ks[3], (L, D_MODEL, IN_COLS), D_MODEL ** -0.5),
        "conv_dw": nrm(ks[4], (L, CONV_KERNEL, CONV_WIDTH), CONV_KERNEL ** -0.5),
        "conv_dw_bias": nrm(ks[5], (L, CONV_WIDTH), 0.02),
        "conv_ln_g": 1.0 + nrm(ks[6], (L, CONV_WIDTH), 0.02),
        "conv_ln_b": nrm(ks[7], (L, CONV_WIDTH), 0.02),
        "w_conv_out": nrm(ks[8], (L, CONV_WIDTH, D_MODEL), CONV_WIDTH ** -0.5),
        "ssm_lambda_re": -0.5 + nrm(ks[9], (L, SSM_GROUPS, SSM_STATE), 0.01),
        "ssm_lambda_im": math.pi * jnp.arange(SSM_STATE, dtype=f32)[None, None, :]
                         + nrm(ks[10], (L, SSM_GROUPS, SSM_STATE), 0.01),
        "ssm_log_dt": jax.random.uniform(ks[11], (L, SSM_GROUPS), f32,
                                         math.log(1e-3), math.log(1e-1)),
        "ssm_b_re": nrm(ks[12], (L, SSM_GROUPS, SSM_STATE, SSM_GROUP), (2 * SSM_GROUP) ** -0.5),
        "ssm_b_im": nrm(ks[13], (L, SSM_GROUPS, SSM_STATE, SSM_GROUP), (2 * SSM_GROUP) ** -0.5),
        "ssm_c_re": nrm(ks[14], (L, SSM_GROUPS, SSM_GROUP, SSM_STATE), SSM_STATE ** -0.5),
        "ssm_c_im": nrm(ks[15], (L, SSM_GROUPS, SSM_GROUP, SSM_STATE), SSM_STATE ** -0.5),
        "ssm_d": nrm(ks[16], (L, SSM_WIDTH), 1.0),
        "w_ssm_glu": nrm(ks[17], (L, SSM_WIDTH, 2 * D_MODEL), SSM_WIDTH ** -0.5),
        "g_mem": 1.0 + nrm(ks[18], (L, D_MODEL), 0.02),
        "w_mem_kv": nrm(ks[19], (L, D_MODEL, 2 * XATTN_WIDTH), D_MODEL ** -0.5),
        "w_mem_out": nrm(ks[20], (L, XATTN_WIDTH, D_MODEL), XATTN_WIDTH ** -0.5),
        "w_out": nrm(ks[21], (L, D_MODEL, D_MODEL), D_MODEL ** -0.5),
        "g_ffn": 1.0 + nrm(ks[22], (L, D_MODEL), 0.02),
        "w_router_group": nrm(ks[23], (L, D_MODEL, MOE_GROUPS), D_MODEL ** -0.5),
        "b_router_group": nrm(ks[24], (L, MOE_GROUPS), 0.01),
        "w_router_expert": nrm(ks[25], (L, D_MODEL, N_EXPERTS), D_MODEL ** -0.5),
        "b_router_expert": nrm(ks[26], (L, N_EXPERTS), 0.01),
        "w_exp_gate": nrm(ks[27], (L, N_EXPERTS, D_MODEL, D_EXPERT), D_MODEL ** -0.5),
        "w_exp_up": nrm(ks[28], (L, N_EXPERTS, D_MODEL, D_EXPERT), D_MODEL ** -0.5),
        "w_exp_down": nrm(ks[29], (L, N_EXPERTS, D_EXPERT, D_MODEL), D_EXPERT ** -0.5),
        "g_final": 1.0 + nrm(ks[30], (D_MODEL,), 0.02),
    }


def reference(x, mem, g_mix, w_in, conv_dw, conv_dw_bias, conv_ln_g, conv_ln_b, w_conv_out,
              ssm_lambda_re, ssm_lambda_im, ssm_log_dt, ssm_b_re, ssm_b_im, ssm_c_re, ssm_c_im,
              ssm_d, w_ssm_glu, g_mem, w_mem_kv, w_mem_out, w_out, g_ffn,
              w_router_group, b_router_group, w_router_expert, b_router_expert,
              w_exp_gate, w_exp_up, w_exp_down, g_final):
    split_pts = [2 * CONV_WIDTH, 2 * CONV_WIDTH + SSM_WIDTH, 2 * CONV_WIDTH + SSM_WIDTH + XATTN_WIDTH]
    for l in range(DEPTH):
        h = rmsnorm(x, g_mix[l])
        proj = h @ w_in[l]
        conv_in, ssm_in, q, gates = jnp.split(proj, split_pts, axis=-1)
        y_conv = conformer_conv(conv_in, conv_dw[l], conv_dw_bias[l], conv_ln_g[l], conv_ln_b[l],
                                w_conv_out[l])
        y_ssm = s5_ssm(ssm_in, ssm_lambda_re[l], ssm_lambda_im[l], ssm_log_dt[l], ssm_b_re[l],
                       ssm_b_im[l], ssm_c_re[l], ssm_c_im[l], ssm_d[l], w_ssm_glu[l])
        mem_n = rmsnorm(mem, g_mem[l])
        y_mem = memory_cross_attention(q, mem_n, w_mem_kv[l], w_mem_out[l])
        g_a, g_b, g_c = jnp.split(jax.nn.sigmoid(gates), N_BRANCH, axis=-1)
        merged = g_a * y_conv + g_b * y_ssm + g_c * y_mem
        x = x + merged @ w_out[l]
        h2 = rmsnorm(x, g_ffn[l])
        x = x + hier_moe(h2, w_router_group[l], b_router_group[l], w_router_expert[l],
                         b_router_expert[l], w_exp_gate[l], w_exp_up[l], w_exp_down[l])
    return rmsnorm(x, g_final)
```

```python
import os
import numpy as np
import ml_dtypes
from contextlib import ExitStack
import concourse.bass as bass
import concourse.mybir as mybir
from concourse.bass_utils import run_bass_kernel_spmd

F32 = mybir.dt.float32
BF16 = mybir.dt.bfloat16
I32 = mybir.dt.int32
U32 = mybir.dt.uint32
AF = mybir.ActivationFunctionType
ALU = mybir.AluOpType
AX = mybir.AxisListType
GELU = AF.Gelu_apprx_tanh

D = 1024
SEQ = 4096
NCORES = 8
T = 512
NB = SEQ // T
NT = SEQ // 128
EPS = 1e-6
NEXP = 32
CAP = 384
NBLK = CAP // 128
ROWS = SEQ + 128


class Buf:
    __slots__ = ("name", "w", "r")

    def __init__(self, name):
        self.name = name
        self.w = {}
        self.r = {}


class Ctx:
    ENG = ("pe", "dve", "act", "pool", "sp")
    KROT = 4
    NDMA = 12

    def __init__(self, nc, es):
        self.nc = nc
        self.q = {e: [] for e in self.ENG}
        self.cnt = {e: 0 for e in self.ENG}
        self.seen = {e: {} for e in self.ENG}
        self.esem = {e: [es.enter_context(nc.semaphore(f"s_{e}{i}")) for i in range(self.KROT)]
                     for e in self.ENG}
        self.dsem = {e: [es.enter_context(nc.semaphore(f"d_{e}{i}")) for i in range(self.NDMA)]
                     for e in ("sp", "act", "pool")}
        self.dcnt = {e: [0] * self.NDMA for e in self.dsem}
        self.dnext = {e: 0 for e in self.dsem}
        self.nwait = 0

    def _wait(self, eng, tok):
        key, val = tok
        if key[0] == 'e' and key[1] == eng and eng == "pe":
            return
        if self.seen[eng].get(key, -1) >= val:
            return
        self.seen[eng][key] = val
        if key[0] == 'e':
            sem = self.esem[key[1]][val % self.KROT]
            v = val // self.KROT + 1
        else:
            sem = self.dsem[key[1]][key[2]]
            v = val
        self.q[eng].append(("w", sem, v))
        self.nwait += 1

    def op(self, eng, fn, reads=(), writes=(), full=(), dma=False):
        toks = []
        for b in reads:
            toks.extend(b.w.items())
        for b in tuple(writes) + tuple(full):
            toks.extend(b.w.items())
            toks.extend(b.r.items())
        for t in toks:
            self._wait(eng, t)
        if dma:
            i = self.dnext[eng]
            self.dnext[eng] = (i + 1) % self.NDMA
            key = ('d', eng, i)
            if self.dcnt[eng][i] > 0:
                self._wait(eng, (key, self.dcnt[eng][i]))
            self.dcnt[eng][i] += 16
            val = self.dcnt[eng][i]
            self.q[eng].append(("d", fn, self.dsem[eng][i]))
        else:
            key = ('e', eng)
            val = self.cnt[eng]
            self.cnt[eng] += 1
            self.q[eng].append(("i", fn, self.esem[eng][val % self.KROT]))
        for b in reads:
            b.r[key] = val
        for b in full:
            b.w = {key: val}
            b.r = {}
        for b in writes:
            b.w[key] = val
        return (key, val)

    def barrier(self):
        toks = []
        for e in self.ENG:
            if self.cnt[e] > 0:
                toks.append((('e', e), self.cnt[e] - 1))
        for e in self.dsem:
            for i in range(self.NDMA):
                if self.dcnt[e][i] > 0:
                    toks.append((('d', e, i), self.dcnt[e][i]))
        for e in self.ENG:
            for t in toks:
                self._wait(e, t)

    def emit(self, block):
        nc = self.nc

        def run(engname, engine):
            for item in self.q[engname]:
                if item[0] == "w":
                    engine.wait_ge(item[1], item[2])
                elif item[0] == "d":
                    item[1](engine).then_inc(item[2], 16)
                else:
                    item[1](engine).then_inc(item[2], 1)

        @block.tensor
        def _(e):
            run("pe", e)

        @block.vector
        def _(e):
            run("dve", e)

        @block.scalar
        def _(e):
            run("act", e)

        @block.gpsimd
        def _(e):
            run("pool", e)

        @block.sync
        def _(e):
            run("sp", e)


class TT:
    def __init__(self, t, name):
        self.t = t
        self.b = Buf(name)

    def __getitem__(self, k):
        return self.t[k]


class Alloc:
    cnt = [0]

    def __init__(self, nc, es=None):
        self.nc = nc
        self.es = es if es is not None else ExitStack()

    @property
    def n(self):
        return Alloc.cnt[0]

    @n.setter
    def n(self, v):
        Alloc.cnt[0] = v

    def close(self):
        self.es.close()

    def sb(self, shape, dt, name=None):
        self.n += 1
        name = name or f"sb{self.n}"
        t = self.es.enter_context(self.nc.sbuf_tensor(f"{name}_{self.n}", list(shape), dt))
        return TT(t, name)

    def ps(self, shape, dt, name=None):
        self.n += 1
        name = name or f"ps{self.n}"
        t = self.es.enter_context(self.nc.psum_tensor(f"{name}_{self.n}", list(shape), dt))
        return TT(t, name)


def build(stop_after="E", dbg=False):
    nc = bass.Bass("TRN2", target_bir_lowering=False)
    dram = {}

    def din(name, shape, dt=F32):
        dram[name] = nc.dram_tensor(name, list(shape), dt, kind="ExternalInput").ap()
        return dram[name]

    def dscr(name, shape, dt, kind="Internal"):
        dram[name] = nc.dram_tensor(name, list(shape), dt, kind=kind).ap()
        return dram[name]

    x_d = din("x", [SEQ, D])
    gmix_d = din("g_mix", [1, D])
    w_in_d = din("w_in", [D, 5120])
    ident_bf_d = din("ident_bf", [128, 128], BF16)
    ident_f_d = din("ident_f", [128, 128], F32)
    lamre_d = din("lamre_l", [128, 16])
    lamim_d = din("lamim_l", [128, 16])
    logdt_d = din("logdt_l", [128, 16])
    bre_d = din("bre_l", [128, 16, 16])
    bim_d = din("bim_l", [128, 16, 16])
    cre_d = din("cre_l", [128, 16, 16])
    cim_d = din("cim_l", [128, 16, 16])
    dl_d = din("d_l", [128, 32])
    psel_d = din("psel", [128, 8, 240], BF16)
    cmask_d = din("cmask", [128, 128])
    mem_d = din("mem", [256, D])
    gmem_d = din("g_mem", [1, D])
    gffn_d = din("g_ffn", [1, D])
    gfin_d = din("g_final", [1, D])
    wkv_d = din("w_mem_kv", [D, 1024])
    wmo_d = din("w_mem_out", [512, D])
    wco_d = din("w_conv_out", [512, D])
    wgl_d = din("w_ssm_glu", [512, 2048])
    wo_d = din("w_out", [D, D])
    wr_d = din("w_router", [D, 36])
    rbias_d = din("b_router", [1, 36])
    cdw_d = din("cdw_l", [128, 4, 31])
    cb_d = din("cb_l", [128, 4])
    lng_d = din("lng_l", [128, 4])
    lnb_d = din("lnb_l", [128, 4])
    tri_d = din("tri", [128, 128])
    ecap_d = din("ecap", [128, 32])
    tokid_d = din("tokid", [128, NT])
    lst_init_d = din("lst_init", [NEXP * CAP + 128, 4])
    trashp_d = din("trashp", [128, 1])
    weg_d = din("w_exp_gate", [NEXP, D, 256])
    weu_d = din("w_exp_up", [NEXP, D, 256])
    wed_d = din("w_exp_down", [NEXP, 256, D])
    dk = "ExternalOutput" if dbg else "Internal"
    lst_d = dscr("lst", [NEXP * CAP + 128, 4], F32, kind=dk)
    h2_scr = dscr("h2_scr", [ROWS, D], BF16, kind=dk)
    moe_scr = dscr("moe_scr", [2 * ROWS, D], F32, kind=dk)
    x2_scr = dscr("x2_scr", [SEQ, D], F32, kind=dk)
    ys_scr = dscr("ys_scr", [4, 128, SEQ], BF16, kind="ExternalOutput" if dbg else "Internal")
    out_d = dscr("out", [SEQ, D], F32, kind="ExternalOutput")
    hT_scr = dscr("hT_scr", [8, 128, SEQ], BF16, kind="ExternalOutput" if dbg else "Internal")
    u_dbg = dscr("u_dbg", [4, 128, SEQ], BF16, kind="ExternalOutput") if dbg else None

    with ExitStack() as es:
        cx = Ctx(nc, es)
        al = Alloc(nc, es)
        block = es.enter_context(nc.Block())

        ident_bf = al.sb([128, 128], BF16, "ident_bf")
        cx.op("sp", lambda e: e.dma_start(out=ident_bf[:], in_=ident_bf_d), full=[ident_bf.b], dma=True)
        ident_f = al.sb([128, 128], F32, "ident_f")
        cx.op("sp", lambda e: e.dma_start(out=ident_f[:], in_=ident_f_d), full=[ident_f.b], dma=True)

        psum = [al.ps([128, 512], F32, f"bank{i}") for i in range(6)]
        psb = [al.ps([128, 1024], BF16, f"bankb{i}") for i in range(2)]
        pctr = [0]

        def getps():
            p = psum[pctr[0] % len(psum)]
            pctr[0] += 1
            return p

        alAB = Alloc(nc)
        u_all = alAB.sb([128, 4, SEQ], BF16, "u_all")
        M_all = alAB.sb([128, 32, 128], BF16, "M_all")
        W2r = alAB.sb([128, 16, 2, 128], BF16, "W2r"); W2i = alAB.sb([128, 16, 2, 128], BF16, "W2i")
        C1r = alAB.sb([128, 16, 128], BF16, "C1r"); nC1i = alAB.sb([128, 16, 128], BF16, "nC1i")
        KAr = alAB.sb([128, 9, 16], F32, "KAr"); KAi = alAB.sb([128, 9, 16], F32, "KAi")
        KnAi = alAB.sb([128, 9, 16], F32, "KnAi")
        psel = alAB.sb([128, 8, 240], BF16, "psel")
        cx.op("sp", lambda e: e.dma_start(out=psel[:], in_=psel_d), full=[psel.b], dma=True)
        al_outer = al
        al = Alloc(nc)
        gmix = al.sb([128, D], F32, "gmix")
        cx.op("sp", lambda e: e.dma_start(out=gmix[:], in_=gmix_d.partition_broadcast(128)),
              full=[gmix.b], dma=True)

        stg = [al.sb([128, 8, 256], F32, f"stg{i}") for i in range(2)]
        sctr = [0]

        def load_cast(dst, dst_col0, src_d, c0, c1, kch):
            for cc in range(c0, c1, 256):
                w = min(256, c1 - cc)
                s = stg[sctr[0] % 2]
                sctr[0] += 1
                src = src_d[:, cc:cc + w].rearrange("(c p) n -> p c n", p=128)
                cx.op("sp", lambda e, s=s, src=src, w=w: e.dma_start(out=s[:, 0:kch, 0:w], in_=src),
                      full=[s.b], dma=True)
                o = dst_col0 + (cc - c0)
                cx.op("pool", lambda e, s=s, o=o, w=w: e.tensor_copy(out=dst[:, 0:kch, o:o + w],
                                                                     in_=s[:, 0:kch, 0:w]),
                      reads=[s.b], writes=[dst.b])

        w_ssm_in = al.sb([128, 8, 512], BF16, "w_ssm_in")
        load_cast(w_ssm_in, 0, w_in_d, 1024, 1536, 8)
        xt = [al.sb([128, D], F32, f"xt{i}") for i in range(2)]
        junk = al.sb([128, D], BF16, "junk")
        ss = [al.sb([128, 1], F32, f"ss{i}") for i in range(2)]
        rt = [al.sb([128, 1], F32, f"rt{i}") for i in range(2)]
        rstd = [al.sb([128, 1], F32, f"rstd{i}") for i in range(2)]
        hbf = [al.sb([128, D], BF16, f"hbf{i}") for i in range(2)]
        hTb = [al.sb([128, 8, T], BF16, f"hTb{i}") for i in range(2)]

        for i in range(NT):
            p = i % 2
            blk = i // 4
            hb = hTb[blk % 2]
            cx.op("sp", lambda e, p=p, i=i: e.dma_start(out=xt[p][:], in_=x_d[i * 128:(i + 1) * 128, :]),
                  full=[xt[p].b], dma=True)
            cx.op("act", lambda e, p=p: e.activation(out=junk[:], in_=xt[p][:], func=AF.Square,
                                                     accum_out=ss[p][:]),
                  reads=[xt[p].b], writes=[junk.b], full=[ss[p].b])
            cx.op("act", lambda e, p=p: e.activation(out=rt[p][:], in_=ss[p][:], func=AF.Sqrt,
                                                     scale=1.0 / D, bias=EPS),
                  reads=[ss[p].b], full=[rt[p].b])
            cx.op("dve", lambda e, p=p: e.reciprocal(out=rstd[p][:], in_=rt[p][:]),
                  reads=[rt[p].b], full=[rstd[p].b])
            cx.op("dve", lambda e, p=p: e.scalar_tensor_tensor(out=hbf[p][:], in0=xt[p][:],
                                                               scalar=rstd[p][:, 0:1], in1=gmix[:],
                                                               op0=ALU.mult, op1=ALU.mult),
                  reads=[xt[p].b, rstd[p].b, gmix.b], full=[hbf[p].b])
            pb = psb[i % 2]
            for c in range(8):
                cx.op("pe", lambda e, pb=pb, p=p, c=c: e.transpose(out=pb[:, c * 128:(c + 1) * 128],
                                                                   in_=hbf[p][:, c * 128:(c + 1) * 128],
                                                                   identity=ident_bf[:]),
                      reads=[hbf[p].b, ident_bf.b], writes=[pb.b])
            tt = i % 4
            cx.op("act", lambda e, pb=pb, hb=hb, tt=tt: e.copy(
                out=hb[:, :, tt * 128:(tt + 1) * 128],
                in_=pb[:].rearrange("p (c t) -> p c t", c=8)),
                reads=[pb.b], writes=[hb.b])
            if tt == 3:
                for f in range(4):
                    ps = getps()
                    for c in range(8):
                        cx.op("pe", lambda e, ps=ps, hb=hb, f=f, c=c: e.matmul(
                            ps[:], lhsT=w_ssm_in[:, c, f * 128:(f + 1) * 128], rhs=hb[:, c, :],
                            start=(c == 0), stop=(c == 7)),
                            reads=[w_ssm_in.b, hb.b], writes=[ps.b])
                    cx.op("dve", lambda e, ps=ps, f=f, blk=blk: e.tensor_copy(
                        out=u_all[:, f, blk * T:(blk + 1) * T], in_=ps[:]),
                        reads=[ps.b], writes=[u_all.b])
                cx.op("sp", lambda e, hb=hb, blk=blk: e.dma_start(
                    out=hT_scr[:, :, blk * T:(blk + 1) * T].rearrange("c p t -> p c t"), in_=hb[:]),
                    reads=[hb.b], dma=True)

        if dbg:
            cx.op("sp", lambda e: e.dma_start(out=u_dbg.rearrange("f p t -> p f t"), in_=u_all[:]),
                  reads=[u_all.b], dma=True)


        cx.barrier()
        al.close()
        al = Alloc(nc)
        TWO_PI = 2.0 * np.pi
        cmask = al.sb([128, 128], F32, "cmask")
        cx.op("sp", lambda e: e.dma_start(out=cmask[:], in_=cmask_d), full=[cmask.b], dma=True)
        dl = al.sb([128, 32], F32, "dl")
        cx.op("sp", lambda e: e.dma_start(out=dl[:], in_=dl_d), full=[dl.b], dma=True)
        SU = Buf("ssm_setup")

        def sload(shape, src, name):
            t = al.sb(shape, F32, name)
            cx.op("sp", lambda e: e.dma_start(out=t[:], in_=src), full=[t.b], dma=True)
            return t

        lamre = sload([128, 16], lamre_d, "lamre")
        lamim = sload([128, 16], lamim_d, "lamim")
        logdt = sload([128, 16], logdt_d, "logdt")
        Bre = sload([128, 16, 16], bre_d, "Bre")
        Bim = sload([128, 16, 16], bim_d, "Bim")
        Cre = sload([128, 16, 16], cre_d, "Cre")
        Cim = sload([128, 16, 16], cim_d, "Cim")
        ins_b = [lamre.b, lamim.b, logdt.b, Bre.b, Bim.b, Cre.b, Cim.b]

        def S(shape, name):
            return al.sb(shape, F32, name)

        def dv(fn):
            cx.op("dve", fn, reads=ins_b, writes=[SU])

        def ac(fn):
            cx.op("act", fn, reads=ins_b, writes=[SU])

        def tt_(out, a, b, op):
            dv(lambda e: e.tensor_tensor(out=out, in0=a, in1=b, op=op))

        sh16 = [128, 16]
        dt_ = S(sh16, "dt"); lrd = S(sh16, "lrd"); th = S(sh16, "th")
        ac(lambda e: e.activation(out=dt_[:], in_=logdt[:], func=AF.Exp))
        tt_(lrd[:], lamre[:], dt_[:], ALU.mult)
        tt_(th[:], lamim[:], dt_[:], ALU.mult)
        mag = S(sh16, "mag"); imag2 = S(sh16, "imag2")
        ac(lambda e: e.activation(out=mag[:], in_=lrd[:], func=AF.Exp))
        ac(lambda e: e.activation(out=imag2[:], in_=lrd[:], func=AF.Exp, scale=-2.0))
        kq_i = al.sb(sh16, I32, "kq_i"); kq = S(sh16, "kq"); red = S(sh16, "red"); msk = S(sh16, "msk")
        sinv = S(sh16, "sinv"); cosv = S(sh16, "cosv"); tmpa = S(sh16, "tmpa")

        def sin_of(outt, shift):
            dv(lambda e: e.tensor_scalar(out=tmpa[:], in0=th[:], scalar1=float(shift), scalar2=None,
                                         op0=ALU.add))
            dv(lambda e: e.tensor_scalar(out=kq[:], in0=tmpa[:], scalar1=float(1.0 / TWO_PI),
                                         scalar2=None, op0=ALU.mult))
            dv(lambda e: e.tensor_copy(out=kq_i[:], in_=kq[:]))
            dv(lambda e: e.tensor_copy(out=kq[:], in_=kq_i[:]))
            dv(lambda e: e.scalar_tensor_tensor(out=red[:], in0=kq[:], scalar=float(-TWO_PI),
                                                in1=tmpa[:], op0=ALU.mult, op1=ALU.add))
            dv(lambda e: e.tensor_single_scalar(out=msk[:], in_=red[:], scalar=float(np.pi), op=ALU.is_gt))
            dv(lambda e: e.scalar_tensor_tensor(out=red[:], in0=msk[:], scalar=float(-TWO_PI),
                                                in1=red[:], op0=ALU.mult, op1=ALU.add))
            dv(lambda e: e.tensor_single_scalar(out=msk[:], in_=red[:], scalar=float(-np.pi), op=ALU.is_lt))
            dv(lambda e: e.scalar_tensor_tensor(out=red[:], in0=msk[:], scalar=float(TWO_PI),
                                                in1=red[:], op0=ALU.mult, op1=ALU.add))
            ac(lambda e: e.activation(out=outt[:], in_=red[:], func=AF.Sin))

        sin_of(sinv, 0.0)
        sin_of(cosv, np.pi / 2)
        PWr = S([128, 9, 16], "PWr"); PWi = S([128, 9, 16], "PWi")
        IPr = S([128, 8, 16], "IPr"); IPi = S([128, 8, 16], "IPi")
        t1 = S([128, 16, 8, 16], "t1"); t2 = S([128, 16, 8, 16], "t2")

        def cmul(outr, outi, ar, ai, br, bi, shp, neg_i=False):
            a1 = t1[:].rearrange("p a b c -> p (a b c)")[:, 0:int(np.prod(shp[1:]))]
            a2 = t2[:].rearrange("p a b c -> p (a b c)")[:, 0:int(np.prod(shp[1:]))]
            if len(shp) == 3:
                a1 = a1.rearrange("p (a b) -> p a b", a=shp[1])
                a2 = a2.rearrange("p (a b) -> p a b", a=shp[1])
            tt_(a1, ar, br, ALU.mult)
            tt_(a2, ai, bi, ALU.mult)
            tt_(outr, a1, a2, ALU.subtract)
            tt_(a1, ar, bi, ALU.mult)
            tt_(a2, ai, br, ALU.mult)
            if neg_i:
                dv(lambda e: e.scalar_tensor_tensor(out=outi, in0=a1, scalar=-1.0, in1=a2,
                                                    op0=ALU.mult, op1=ALU.subtract))
            else:
                tt_(outi, a1, a2, ALU.add)

        dv(lambda e: e.memset(PWr[:, 0, :], 1.0))
        dv(lambda e: e.memset(PWi[:, 0, :], 0.0))
        dv(lambda e: e.memset(IPr[:, 0, :], 1.0))
        dv(lambda e: e.memset(IPi[:, 0, :], 0.0))
        tt_(PWr[:, 1, :], mag[:], cosv[:], ALU.mult)
        tt_(PWi[:, 1, :], mag[:], sinv[:], ALU.mult)
        tt_(IPr[:, 1, :], PWr[:, 1, :], imag2[:], ALU.mult)
        dv(lambda e: e.scalar_tensor_tensor(out=IPi[:, 1, :], in0=PWi[:, 1, :], scalar=-1.0, in1=imag2[:],
                                            op0=ALU.mult, op1=ALU.mult))
        for n in range(2, 9):
            cmul(PWr[:, n, :], PWi[:, n, :], PWr[:, n - 1, :], PWi[:, n - 1, :], PWr[:, 1, :], PWi[:, 1, :], sh16)
        for n in range(2, 8):
            cmul(IPr[:, n, :], IPi[:, n, :], IPr[:, n - 1, :], IPi[:, n - 1, :], IPr[:, 1, :], IPi[:, 1, :], sh16)
        dv(lambda e: e.tensor_copy(out=KAr[:, 0, :], in_=PWr[:, 8, :]))
        dv(lambda e: e.tensor_copy(out=KAi[:, 0, :], in_=PWi[:, 8, :]))
        for d_ in range(1, 9):
            cmul(KAr[:, d_, :], KAi[:, d_, :], KAr[:, d_ - 1, :], KAi[:, d_ - 1, :],
                 KAr[:, d_ - 1, :], KAi[:, d_ - 1, :], sh16)
        dv(lambda e: e.tensor_scalar(out=KnAi[:], in0=KAi[:], scalar1=-1.0, scalar2=None, op0=ALU.mult))
        am1 = S(sh16, "am1"); l2 = S(sh16, "l2"); il2 = S(sh16, "il2"); kr = S(sh16, "kr"); ki = S(sh16, "ki")
        dv(lambda e: e.tensor_scalar(out=am1[:], in0=PWr[:, 1, :], scalar1=-1.0, scalar2=None, op0=ALU.add))
        tt_(l2[:], lamre[:], lamre[:], ALU.mult)
        tt_(tmpa[:], lamim[:], lamim[:], ALU.mult)
        tt_(l2[:], l2[:], tmpa[:], ALU.add)
        dv(lambda e: e.reciprocal(out=il2[:], in_=l2[:]))
        tt_(kr[:], am1[:], lamre[:], ALU.mult)
        tt_(tmpa[:], PWi[:, 1, :], lamim[:], ALU.mult)
        tt_(kr[:], kr[:], tmpa[:], ALU.add)
        tt_(kr[:], kr[:], il2[:], ALU.mult)
        tt_(ki[:], PWi[:, 1, :], lamre[:], ALU.mult)
        tt_(tmpa[:], am1[:], lamim[:], ALU.mult)
        tt_(ki[:], ki[:], tmpa[:], ALU.subtract)
        tt_(ki[:], ki[:], il2[:], ALU.mult)
        sh3 = [128, 16, 16]

        def bc(a):
            return a.unsqueeze(2).to_broadcast(sh3)

        Bbr = S(sh3, "Bbr"); Bbi = S(sh3, "Bbi")
        cmul(Bbr[:], Bbi[:], bc(kr[:]), bc(ki[:]), Bre[:], Bim[:], sh3)
        Bhr = S([128, 16, 8, 16], "Bhr"); nBhi = S([128, 16, 8, 16], "nBhi"); Bhi = S([128, 16, 8, 16], "Bhi")
        Btr = S([128, 16, 8, 16], "Btr"); Bti = S([128, 16, 8, 16], "Bti")
        Chr = S([128, 16, 9, 16], "Chr"); Chi = S([128, 16, 9, 16], "Chi"); nChi = S([128, 16, 9, 16], "nChi")
        for k in range(8):
            cmul(Bhr[:, :, k, :], Bhi[:, :, k, :], bc(IPr[:, k, :]), bc(IPi[:, k, :]), Bbr[:], Bbi[:], sh3)
            cmul(Btr[:, :, k, :], Bti[:, :, k, :], bc(PWr[:, 7, :]), bc(PWi[:, 7, :]),
                 Bhr[:, :, k, :], Bhi[:, :, k, :], sh3)
        dv(lambda e: e.tensor_scalar(out=nBhi[:], in0=Bhi[:], scalar1=-1.0, scalar2=None, op0=ALU.mult))
        for j in range(9):
            cmul(Chr[:, :, j, :], Chi[:, :, j, :], bc(PWr[:, j, :]), bc(PWi[:, j, :]), Cre[:], Cim[:], sh3)
        dv(lambda e: e.tensor_scalar(out=nChi[:], in0=Chi[:], scalar1=-1.0, scalar2=None, op0=ALU.mult))
        dv(lambda e: e.tensor_copy(out=C1r[:].rearrange("p r (j c) -> p r j c", j=8), in_=Chr[:, :, 1:9, :]))
        dv(lambda e: e.tensor_copy(out=nC1i[:].rearrange("p r (j c) -> p r j c", j=8), in_=nChi[:, :, 1:9, :]))
        mtmp = S([128, 128], "mtmp")
        cx.op("pool", lambda e: e.memset(W2r[:], 0.0), reads=ins_b, writes=[SU])
        cx.op("pool", lambda e: e.memset(W2i[:], 0.0), reads=ins_b, writes=[SU])
        for r in range(16):
            for two in range(2):
                g = 2 * r + two
                rng = slice(two * 64, (two + 1) * 64)
                ps = getps()
                cx.op("pe", lambda e, ps=ps, r=r, rng=rng: e.matmul(
                    ps[:, 0:128], lhsT=Bhr[rng, r, :, :].rearrange("p k c -> p (k c)"),
                    rhs=Chr[rng, r, 0:8, :].rearrange("p j c -> p (j c)"), start=True, stop=False),
                    reads=[SU], writes=[ps.b])
                cx.op("pe", lambda e, ps=ps, r=r, rng=rng: e.matmul(
                    ps[:, 0:128], lhsT=nBhi[rng, r, :, :].rearrange("p k c -> p (k c)"),
                    rhs=Chi[rng, r, 0:8, :].rearrange("p j c -> p (j c)"), start=False, stop=True),
                    reads=[SU], writes=[ps.b])
                cx.op("dve", lambda e, ps=ps: e.tensor_tensor(out=mtmp[:], in0=ps[:, 0:128], in1=cmask[:],
                                                              op=ALU.mult),
                      reads=[ps.b, cmask.b], writes=[SU])
                cx.op("dve", lambda e, g=g: e.scalar_tensor_tensor(
                    out=M_all[:, g, :], in0=ident_f[:], scalar=dl[:, g:g + 1], in1=mtmp[:],
                    op0=ALU.mult, op1=ALU.add),
                    reads=[ident_f.b, dl.b], writes=[SU, M_all.b])
            for (Bt, W2) in ((Btr, W2r), (Bti, W2i)):
                ps = getps()
                cx.op("pe", lambda e, ps=ps, r=r, Bt=Bt: e.transpose(
                    out=ps[:, 0:128], in_=Bt[:, r, :, :].rearrange("p k c -> p (k c)"), identity=ident_f[:]),
                    reads=[SU, ident_f.b], writes=[ps.b])
                cx.op("dve", lambda e, ps=ps, r=r, W2=W2: e.tensor_copy(out=W2[:, r, 0, 0:64], in_=ps[:, 0:64]),
                      reads=[ps.b], writes=[SU, W2.b])
                cx.op("dve", lambda e, ps=ps, r=r, W2=W2: e.tensor_copy(out=W2[:, r, 1, 64:128], in_=ps[:, 64:128]),
                      reads=[ps.b], writes=[SU, W2.b])

        cx.barrier()
        al.close()
        al = Alloc(nc)
        NCH = SEQ // 8
        Vg = [al.sb([128, NCH], BF16, f"Vg{i}") for i in range(4)]
        Sre = [al.sb([128, NCH], F32, f"Sre{i}") for i in range(2)]
        Sim = [al.sb([128, NCH], F32, f"Sim{i}") for i in range(2)]
        Sbr = al.sb([128, NCH], BF16, "Sbr"); Sbi = al.sb([128, NCH], BF16, "Sbi")
        Gg = [al.sb([128, NCH], BF16, f"Gg{i}") for i in range(16)]
        ysf = [al.sb([128, SEQ], BF16, f"ysf{i}") for i in range(2)]
        cx.op("pool", lambda e: e.memset(Sbr[:, 0:1], 0.0), writes=[Sbr.b])
        cx.op("pool", lambda e: e.memset(Sbi[:, 0:1], 0.0), writes=[Sbi.b])
        for r in range(16):
            f = r // 4
            vg = [Vg[(2 * r) % 4], Vg[(2 * r + 1) % 4]]
            for two in range(2):
                g = 2 * r + two
                gl = g % 8
                ps = getps()
                for k in range(8):
                    cx.op("pe", lambda e, ps=ps, gl=gl, k=k, f=f: e.matmul(
                        ps[:], lhsT=psel[:, gl, (7 - k) * 16:(7 - k) * 16 + 128],
                        rhs=u_all[:, f, k:SEQ:8], start=(k == 0), stop=(k == 7)),
                        reads=[psel.b, u_all.b], writes=[ps.b])
                cx.op("act", lambda e, ps=ps, v=vg[two]: e.copy(out=v[:], in_=ps[:]),
                      reads=[ps.b], full=[vg[two].b])
            psr = getps(); psi = getps()
            for (pp, W2) in ((psr, W2r), (psi, W2i)):
                for two in range(2):
                    cx.op("pe", lambda e, pp=pp, W2=W2, two=two, r=r, v=vg[two]: e.matmul(
                        pp[:], lhsT=W2[:, r, two, :], rhs=v[:], start=(two == 0), stop=(two == 1)),
                        reads=[W2.b, vg[two].b], writes=[pp.b])
            cx.op("act", lambda e, psr=psr: e.copy(out=Sre[0][:], in_=psr[:]), reads=[psr.b], full=[Sre[0].b])
            cx.op("dve", lambda e, psi=psi: e.tensor_copy(out=Sim[0][:], in_=psi[:]), reads=[psi.b], full=[Sim[0].b])
            cur = 0
            for d_ in range(9):
                sh = 1 << d_
                s_r, s_i, d_r, d_i = Sre[cur], Sim[cur], Sre[1 - cur], Sim[1 - cur]
                n = NCH - sh
                cx.op("dve", lambda e, s_r=s_r, d_r=d_r, sh=sh, n=n, d_=d_, r=r: e.scalar_tensor_tensor(
                    out=d_r[:, sh:NCH], in0=s_r[:, 0:n], scalar=KAr[:, d_, r:r + 1], in1=s_r[:, sh:NCH],
                    op0=ALU.mult, op1=ALU.add), reads=[s_r.b, SU], writes=[d_r.b])
                cx.op("dve", lambda e, s_i=s_i, d_r=d_r, sh=sh, n=n, d_=d_, r=r: e.scalar_tensor_tensor(
                    out=d_r[:, sh:NCH], in0=s_i[:, 0:n], scalar=KnAi[:, d_, r:r + 1], in1=d_r[:, sh:NCH],
                    op0=ALU.mult, op1=ALU.add), reads=[s_i.b, SU], writes=[d_r.b])
                cx.op("dve", lambda e, s_i=s_i, d_i=d_i, sh=sh, n=n, d_=d_, r=r: e.scalar_tensor_tensor(
                    out=d_i[:, sh:NCH], in0=s_i[:, 0:n], scalar=KAr[:, d_, r:r + 1], in1=s_i[:, sh:NCH],
                    op0=ALU.mult, op1=ALU.add), reads=[s_i.b, SU], writes=[d_i.b])
                cx.op("dve", lambda e, s_r=s_r, d_i=d_i, sh=sh, n=n, d_=d_, r=r: e.scalar_tensor_tensor(
                    out=d_i[:, sh:NCH], in0=s_r[:, 0:n], scalar=KAi[:, d_, r:r + 1], in1=d_i[:, sh:NCH],
                    op0=ALU.mult, op1=ALU.add), reads=[s_r.b, SU], writes=[d_i.b])
                cx.op("pool", lambda e, s_r=s_r, d_r=d_r, sh=sh: e.tensor_copy(out=d_r[:, 0:sh], in_=s_r[:, 0:sh]),
                      reads=[s_r.b], writes=[d_r.b])
                cx.op("pool", lambda e, s_i=s_i, d_i=d_i, sh=sh: e.tensor_copy(out=d_i[:, 0:sh], in_=s_i[:, 0:sh]),
                      reads=[s_i.b], writes=[d_i.b])
                cur = 1 - cur
            fr, fi = Sre[cur], Sim[cur]
            cx.op("act", lambda e, fr=fr: e.copy(out=Sbr[:, 1:NCH], in_=fr[:, 0:NCH - 1]),
                  reads=[fr.b], writes=[Sbr.b])
            cx.op("act", lambda e, fi=fi: e.copy(out=Sbi[:, 1:NCH], in_=fi[:, 0:NCH - 1]),
                  reads=[fi.b], writes=[Sbi.b])
            for two in range(2):
                g = 2 * r + two
                rng = slice(two * 64, (two + 1) * 64)
                ps = getps()
                cx.op("pe", lambda e, ps=ps, g=g, v=vg[two]: e.matmul(
                    ps[:], lhsT=M_all[:, g, :], rhs=v[:], start=True, stop=False),
                    reads=[M_all.b, vg[two].b], writes=[ps.b])
                cx.op("pe", lambda e, ps=ps, r=r, rng=rng: e.matmul(
                    ps[:], lhsT=C1r[rng, r, :], rhs=Sbr[rng, :], start=False, stop=False),
                    reads=[SU, Sbr.b], writes=[ps.b])
                cx.op("pe", lambda e, ps=ps, r=r, rng=rng: e.matmul(
                    ps[:], lhsT=nC1i[rng, r, :], rhs=Sbi[rng, :], start=False, stop=True),
                    reads=[SU, Sbi.b], writes=[ps.b])
                gg = Gg[g % 16]
                cx.op("act", lambda e, ps=ps, gg=gg: e.activation(out=gg[:], in_=ps[:], func=GELU),
                      reads=[ps.b], full=[gg.b])
            if r % 4 == 3:
                yb = ysf[f % 2]
                for j in range(8):
                    ps = getps()
                    for gl in range(8):
                        gg = Gg[(8 * f + gl) % 16]
                        cx.op("pe", lambda e, ps=ps, j=j, gl=gl, gg=gg: e.matmul(
                            ps[:], lhsT=psel[:, j, (7 - gl) * 16:(7 - gl) * 16 + 128], rhs=gg[:],
                            start=(gl == 0), stop=(gl == 7)),
                            reads=[psel.b, gg.b], writes=[ps.b])
                    eng = "act" if j % 2 == 0 else "dve"
                    if eng == "act":
                        cx.op("act", lambda e, ps=ps, yb=yb, j=j: e.copy(out=yb[:, j:SEQ:8], in_=ps[:]),
                              reads=[ps.b], writes=[yb.b])
                    else:
                        cx.op("dve", lambda e, ps=ps, yb=yb, j=j: e.tensor_copy(out=yb[:, j:SEQ:8], in_=ps[:]),
                              reads=[ps.b], writes=[yb.b])
                cx.op("sp", lambda e, yb=yb, f=f: e.dma_start(out=ys_scr[f], in_=yb[:]),
                      reads=[yb.b], dma=True)

        cx.barrier()
        al.close()
        alAB.close()
        al = al_outer
        if stop_after in ("A", "B"):
            pass
        else:
            TC = 256
            NBC = SEQ // TC
            alC = Alloc(nc)
            wA = alC.sb([128, 8, 1536], BF16, "wA")
            wG = alC.sb([128, 8, 3072], BF16, "wG")
            wco = alC.sb([128, 4, 1024], BF16, "wco")
            wgl = alC.sb([128, 4, 2048], BF16, "wgl")
            wmo = alC.sb([128, 4, 1024], BF16, "wmo")
            wo = alC.sb([128, 8, 1024], BF16, "wo")
            Dg2 = [alC.sb([128, 31, 128], BF16, f"Dg{i}") for i in range(2)]
            kT = alC.sb([128, 4, 256], BF16, "kT")
            vtok = alC.sb([128, 2, 512], BF16, "vtok")
            gffn = alC.sb([128, D], F32, "gffn")
            wr = alC.sb([128, 8, 36], F32, "wr")
            rbias = alC.sb([128, 36], F32, "rbias")
            cdw = alC.sb([128, 4, 31], F32, "cdw")
            cb = alC.sb([128, 4], F32, "cb"); lng = alC.sb([128, 4], F32, "lng"); lnb = alC.sb([128, 4], F32, "lnb")
            onesm = alC.sb([128, 128], F32, "onesm")
            ones_bf = alC.sb([128, 128], BF16, "ones_bf")
            tri = alC.sb([128, 128], F32, "tri")
            ones_f = alC.sb([128, 128], F32, "ones_f")
            ecap = alC.sb([128, 32], F32, "ecap")
            tokid = alC.sb([128, NT], F32, "tokid")
            cum = alC.sb([128, 32], F32, "cum")
            trashp = alC.sb([128, 1], F32, "trashp")

            def ld(t, src):
                cx.op("sp", lambda e: e.dma_start(out=t[:], in_=src), full=[t.b], dma=True)

            ld(gffn, gffn_d.partition_broadcast(128))
            ld(wr, wr_d.rearrange("(c p) n -> p c n", p=128))
            ld(rbias, rbias_d.partition_broadcast(128))
            ld(cdw, cdw_d); ld(cb, cb_d); ld(lng, lng_d); ld(lnb, lnb_d)
            ld(tri, tri_d); ld(ecap, ecap_d); ld(tokid, tokid_d); ld(trashp, trashp_d)
            cx.op("pool", lambda e: e.memset(onesm[:], 1.0 / 512.0), full=[onesm.b])
            cx.op("pool", lambda e: e.memset(ones_bf[:], 1.0), full=[ones_bf.b])
            cx.op("pool", lambda e: e.memset(ones_f[:], 1.0), full=[ones_f.b])
            cx.op("pool", lambda e: e.memset(cum[:], 0.0), full=[cum.b])
            alS = Alloc(nc)
            zt = alS.sb([128, 1024], F32, "zt")
            cx.op("pool", lambda e: e.memset(zt[:], 0.0), full=[zt.b])
            lstB = Buf("lst"); h2B = Buf("h2scr"); moeB = Buf("moescr"); x2B = Buf("x2scr")
            cx.op("sp", lambda e: e.dma_start(out=lst_d, in_=lst_init_d), full=[lstB], dma=True)
            cx.op("sp", lambda e: e.dma_start(out=h2_scr[SEQ:ROWS, :], in_=zt[:, 0:512].bitcast(BF16)),
                  reads=[zt.b], writes=[h2B], dma=True)
            moe_flat = moe_scr.rearrange("(n p) d -> n p d", p=128)
            for n in range(0, 2 * ROWS // 128):
                cx.op("sp", lambda e, n=n: e.dma_start(out=moe_flat[n], in_=zt[:]),
                      reads=[zt.b], writes=[moeB], dma=True)

            stg2 = [alS.sb([128, 8, 256], F32, f"stgc{i}") for i in range(2)]
            s2 = [0]

            def load_cast2(dst, dst_col0, src_d, c0, c1, kch, engs=("pool", "act")):
                for cc in range(c0, c1, 256):
                    w = min(256, c1 - cc)
                    s = stg2[s2[0] % 2]
                    eng = engs[s2[0] % len(engs)]
                    s2[0] += 1
                    src = src_d[:, cc:cc + w].rearrange("(c p) n -> p c n", p=128)
                    cx.op("sp", lambda e, s=s, src=src, w=w: e.dma_start(out=s[:, 0:kch, 0:w], in_=src),
                          full=[s.b], dma=True)
                    o = dst_col0 + (cc - c0)
                    if eng == "act":
                        cx.op("act", lambda e, s=s, o=o, w=w: e.copy(out=dst[:, 0:kch, o:o + w], in_=s[:, 0:kch, 0:w]),
                              reads=[s.b], writes=[dst.b])
                    else:
                        cx.op(eng, lambda e, s=s, o=o, w=w: e.tensor_copy(out=dst[:, 0:kch, o:o + w],
                                                                          in_=s[:, 0:kch, 0:w]),
                              reads=[s.b], writes=[dst.b])

            load_cast2(wA, 0, w_in_d, 0, 1024, 8)
            load_cast2(wA, 1024, w_in_d, 1536, 2048, 8)
            load_cast2(wG, 0, w_in_d, 2048, 5120, 8)
            load_cast2(wco, 0, wco_d, 0, 1024, 4)
            load_cast2(wgl, 0, wgl_d, 0, 2048, 4)
            load_cast2(wmo, 0, wmo_d, 0, 1024, 4)
            load_cast2(wo, 0, wo_d, 0, 1024, 8)
            wkv = alS.sb([128, 8, 1024], BF16, "wkv")
            load_cast2(wkv, 0, wkv_d, 0, 1024, 8)
            gmem = alS.sb([128, D], F32, "gmem")
            ld(gmem, gmem_d.partition_broadcast(128))
            memT = alS.sb([128, 8, 256], BF16, "memT")
            mx = alS.sb([128, D], F32, "mx"); mjunk = alS.sb([128, D], BF16, "mjunk")
            mss = alS.sb([128, 1], F32, "mss"); mrt = alS.sb([128, 1], F32, "mrt"); mrs = alS.sb([128, 1], F32, "mrs")
            mh = alS.sb([128, D], BF16, "mh")
            for mt in range(2):
                cx.op("sp", lambda e, mt=mt: e.dma_start(out=mx[:], in_=mem_d[mt * 128:(mt + 1) * 128, :]),
                      full=[mx.b], dma=True)
                cx.op("act", lambda e: e.activation(out=mjunk[:], in_=mx[:], func=AF.Square, accum_out=mss[:]),
                      reads=[mx.b], full=[mjunk.b, mss.b])
                cx.op("act", lambda e: e.activation(out=mrt[:], in_=mss[:], func=AF.Sqrt, scale=1.0 / D, bias=EPS),
                      reads=[mss.b], full=[mrt.b])
                cx.op("dve", lambda e: e.reciprocal(out=mrs[:], in_=mrt[:]), reads=[mrt.b], full=[mrs.b])
                cx.op("dve", lambda e: e.scalar_tensor_tensor(out=mh[:], in0=mx[:], scalar=mrs[:, 0:1], in1=gmem[:],
                                                              op0=ALU.mult, op1=ALU.mult),
                      reads=[mx.b, mrs.b, gmem.b], full=[mh.b])
                pb = psb[mt % 2]
                for c in range(8):
                    cx.op("pe", lambda e, pb=pb, c=c: e.transpose(out=pb[:, c * 128:(c + 1) * 128],
                                                                  in_=mh[:, c * 128:(c + 1) * 128],
                                                                  identity=ident_bf[:]),
                          reads=[mh.b, ident_bf.b], writes=[pb.b])
                cx.op("act", lambda e, pb=pb, mt=mt: e.copy(out=memT[:, :, mt * 128:(mt + 1) * 128],
                                                            in_=pb[:].rearrange("p (c t) -> p c t", c=8)),
                      reads=[pb.b], writes=[memT.b])
            for hd in range(4):
                ps = getps()
                for c in range(8):
                    cx.op("pe", lambda e, ps=ps, c=c, hd=hd: e.matmul(
                        ps[:, 0:256], lhsT=wkv[:, c, hd * 128:(hd + 1) * 128], rhs=memT[:, c, :],
                        start=(c == 0), stop=(c == 7)), reads=[wkv.b, memT.b], writes=[ps.b])
                cx.op("dve", lambda e, ps=ps, hd=hd: e.tensor_copy(out=kT[:, hd, :], in_=ps[:, 0:256]),
                      reads=[ps.b], writes=[kT.b])
            for mc in range(2):
                ps = getps()
                for c in range(8):
                    cx.op("pe", lambda e, ps=ps, c=c, mc=mc: e.matmul(
                        ps[:], lhsT=memT[:, c, mc * 128:(mc + 1) * 128], rhs=wkv[:, c, 512:1024],
                        start=(c == 0), stop=(c == 7)), reads=[wkv.b, memT.b], writes=[ps.b])
                cx.op("dve", lambda e, ps=ps, mc=mc: e.tensor_copy(out=vtok[:, mc, :], in_=ps[:]),
                      reads=[ps.b], writes=[vtok.b])
            cx.barrier()
            alS.close()

            alW = Alloc(nc)
            hT = [alW.sb([128, 8, TC], BF16, "hTc0")] * 2
            ysb = [alW.sb([128, 4, TC], BF16, "ysb0")] * 2
            vbuf = alW.sb([128, 4, 30 + TC], BF16, "vbuf")
            sgt = [alW.sb([128, TC], F32, f"sgt{i}") for i in range(3)]
            cv = alW.sb([128, 4, TC], F32, "cv"); sq = alW.sb([128, 2, TC], F32, "sq")
            mean = alW.sb([128, TC], F32, "mean"); m2 = alW.sb([128, TC], F32, "m2")
            var = alW.sb([128, TC], F32, "var"); lnv = alW.sb([128, TC], F32, "lnv"); lrs = alW.sb([128, TC], F32, "lrs")
            xc = [alW.sb([128, TC], F32, f"xc{i}") for i in range(2)]
            cn = alW.sb([128, 4, TC], BF16, "cn")
            qb = alW.sb([128, 4, TC], BF16, "qb")
            Eb = [alW.sb([128, 2, TC], BF16, f"Eb{i}") for i in range(2)]
            rden = alW.sb([128, TC], F32, "rden")
            ob = alW.sb([128, 4, TC], BF16, "ob")
            macc = alW.sb([128, TC], F32, "macc"); mt1 = alW.sb([128, TC], F32, "mt1"); mt2 = alW.sb([128, TC], F32, "mt2")
            merged = alW.sb([128, 8, TC], BF16, "merged")
            xt2 = [alW.sb([128, D], F32, "xtc0")] * 2
            x2t = xt2
            h2f = alW.sb([128, D], F32, "h2f"); h2b = [alW.sb([128, D], BF16, "h2b0")] * 2
            junk2 = h2b[0]
            h2T = alW.sb([128, 8, 128], F32, "h2T")
            ss2 = alW.sb([128, 1], F32, "ss2"); rt2 = alW.sb([128, 1], F32, "rt2"); rs2 = alW.sb([128, 1], F32, "rs2")
            lg = alW.sb([128, 36], F32, "lg")
            RS = Buf("route_small")
            r_gmax = alW.sb([128, 1], F32, "r_gmax"); r_ngmax = alW.sb([128, 1], F32, "r_ngmax")
            r_ohg = alW.sb([128, 4], F32, "r_ohg"); r_eg = alW.sb([128, 4], F32, "r_eg")
            r_sumg = alW.sb([128, 1], F32, "r_sumg"); r_ptop = alW.sb([128, 1], F32, "r_ptop")
            r_selm = alW.sb([128, 4, 8], F32, "r_selm"); r_sel = alW.sb([128, 8], F32, "r_sel")
            r_sel2 = alW.sb([128, 8], F32, "r_sel2")
            r_m1 = alW.sb([128, 1], F32, "r_m1"); r_m2 = alW.sb([128, 1], F32, "r_m2")
            r_oh1 = alW.sb([128, 8], F32, "r_oh1"); r_oh2 = alW.sb([128, 8], F32, "r_oh2")
            r_dm = alW.sb([128, 1], F32, "r_dm"); r_w1 = alW.sb([128, 1], F32, "r_w1"); r_w2 = alW.sb([128, 1], F32, "r_w2")
            r_M1 = alW.sb([128, 4, 8], F32, "r_M1"); r_M2 = alW.sb([128, 4, 8], F32, "r_M2")
            r_Mc = [alW.sb([128, 32], F32, f"r_Mc{i}") for i in range(2)]
            r_sf = alW.sb([128, 32], F32, "r_sf"); r_ov = alW.sb([128, 32], F32, "r_ov"); r_t = alW.sb([128, 32], F32, "r_t")
            r_bk = alW.sb([128, 32], F32, "r_bk")
            r_s = alW.sb([128, 2], F32, "r_s"); r_o = alW.sb([128, 1], F32, "r_o"); r_d = alW.sb([128, 1], F32, "r_d")
            r_si = [[alW.sb([128, 1], I32, f"r_si{i}{k}") for k in range(2)] for i in range(2)]
            r_ent = [[alW.sb([128, 4], F32, f"r_ent{i}{k}") for k in range(2)] for i in range(2)]
            cx.op("pool", lambda e: e.memset(vbuf[:], 0.0), full=[vbuf.b])
            for i in range(2):
                for k in range(2):
                    cx.op("pool", lambda e, i=i, k=k: e.memset(r_ent[i][k][:], 0.0), full=[r_ent[i][k].b])

            breg = {}

            def mmgrp(ps_ap, ps_b, pairs, reads):
                n = len(pairs)
                for idx, (l, r_) in enumerate(pairs):
                    cx.op("pe", lambda e, l=l, r_=r_, idx=idx: e.matmul(ps_ap, lhsT=l, rhs=r_, start=(idx == 0),
                                                                         stop=(idx == n - 1)),
                          reads=reads, writes=[ps_b])

            KCUT = int(os.environ.get("KCUT", "9"))
            KNB = int(os.environ.get("KNB", str(NBC)))
            for bi in range(NBC if KCUT >= 2 else 0):
                if bi >= KNB:
                    break
                t0 = bi * TC
                h = hT[bi % 2]; yb = ysb[bi % 2]
                cx.op("sp", lambda e, h=h, t0=t0: e.dma_start(
                    out=h[:], in_=hT_scr[:, :, t0:t0 + TC].rearrange("c p t -> p c t")), full=[h.b], dma=True)
                cx.op("sp", lambda e, yb=yb, t0=t0: e.dma_start(
                    out=yb[:], in_=ys_scr[:, :, t0:t0 + TC].rearrange("f p t -> p f t")), full=[yb.b], dma=True)
                for f in range(4):
                    pa = getps(); pg = getps()
                    mmgrp(pa[:, 0:TC], pa.b, [(wA[:, c, f * 128:(f + 1) * 128], h[:, c, :]) for c in range(8)],
                          [wA.b, h.b])
                    mmgrp(pg[:, 0:TC], pg.b, [(wA[:, c, 512 + f * 128:512 + (f + 1) * 128], h[:, c, :]) for c in range(8)],
                          [wA.b, h.b])
                    s = sgt[f % 3]
                    cx.op("act", lambda e, pg=pg, s=s: e.activation(out=s[:], in_=pg[:, 0:TC], func=AF.Sigmoid),
                          reads=[pg.b], full=[s.b])
                    cx.op("dve", lambda e, pa=pa, s=s, f=f: e.tensor_tensor(out=vbuf[:, f, 30:30 + TC], in0=pa[:, 0:TC],
                                                                            in1=s[:], op=ALU.mult),
                          reads=[pa.b, s.b], writes=[vbuf.b])
                for f in range(4):
                    pc = getps()
                    Dg = Dg2[f % 2]
                    for k in range(31):
                        cx.op("pool", lambda e, Dg=Dg, f=f, k=k: e.tensor_scalar(
                            out=Dg[:, k, :], in0=ident_f[:], scalar1=cdw[:, f, k:k + 1], scalar2=1.0,
                            op0=ALU.mult, op1=ALU.mult), reads=[ident_f.b, cdw.b], writes=[Dg.b])
                    mmgrp(pc[:, 0:TC], pc.b, [(Dg[:, k, :], vbuf[:, f, k:k + TC]) for k in range(31)],
                          [Dg.b, vbuf.b])
                    cx.op("act", lambda e, pc=pc, f=f: e.activation(out=cv[:, f, :], in_=pc[:, 0:TC], func=AF.Identity,
                                                                    bias=cb[:, f:f + 1], scale=1.0),
                          reads=[pc.b, cb.b], writes=[cv.b])
                cx.op("pool", lambda e: e.tensor_copy(out=vbuf[:, :, 0:30], in_=vbuf[:, :, TC:TC + 30]),
                      reads=[vbuf.b], writes=[vbuf.b])
                pm = getps(); pq = getps()
                mmgrp(pm[:, 0:TC], pm.b, [(onesm[:], cv[:, f, :]) for f in range(4)], [onesm.b, cv.b])
                sqb = [Buf("sq0"), Buf("sq1")]
                for f in range(4):
                    cx.op("act", lambda e, f=f: e.activation(out=sq[:, f % 2, :], in_=cv[:, f, :], func=AF.Square),
                          reads=[cv.b], full=[sqb[f % 2]])
                    cx.op("pe", lambda e, pq=pq, f=f: e.matmul(pq[:, 0:TC], lhsT=onesm[:], rhs=sq[:, f % 2, :],
                                                               start=(f == 0), stop=(f == 3)),
                          reads=[onesm.b, sqb[f % 2]], writes=[pq.b])
                cx.op("act", lambda e, pm=pm: e.copy(out=mean[:], in_=pm[:, 0:TC]), reads=[pm.b], full=[mean.b])
                cx.op("dve", lambda e: e.tensor_tensor(out=m2[:], in0=mean[:], in1=mean[:], op=ALU.mult),
                      reads=[mean.b], full=[m2.b])
                cx.op("dve", lambda e, pq=pq: e.tensor_tensor(out=var[:], in0=pq[:, 0:TC], in1=m2[:], op=ALU.subtract),
                      reads=[pq.b, m2.b], full=[var.b])
                cx.op("dve", lambda e: e.tensor_scalar(out=var[:], in0=var[:], scalar1=float(EPS), scalar2=None,
                                                       op0=ALU.add), reads=[var.b], writes=[var.b])
                cx.op("act", lambda e: e.activation(out=lnv[:], in_=var[:], func=AF.Ln), reads=[var.b], full=[lnv.b])
                cx.op("act", lambda e: e.activation(out=lrs[:], in_=lnv[:], func=AF.Exp, scale=-0.5),
                      reads=[lnv.b], full=[lrs.b])
                for f in range(4):
                    x_ = xc[f % 2]
                    cx.op("dve", lambda e, x_=x_, f=f: e.tensor_tensor(out=x_[:], in0=cv[:, f, :], in1=mean[:],
                                                                       op=ALU.subtract),
                          reads=[cv.b, mean.b], full=[x_.b])
                    cx.op("dve", lambda e, x_=x_: e.tensor_tensor(out=x_[:], in0=x_[:], in1=lrs[:], op=ALU.mult),
                          reads=[lrs.b], writes=[x_.b])
                    cx.op("act", lambda e, x_=x_, f=f: e.activation(out=cn[:, f, :], in_=x_[:], func=AF.Silu,
                                                                    bias=lnb[:, f:f + 1], scale=lng[:, f:f + 1]),
                          reads=[x_.b, lnb.b, lng.b], writes=[cn.b])
                for hd in range(4):
                    pq_ = getps()
                    mmgrp(pq_[:, 0:TC], pq_.b, [(wA[:, c, 1024 + hd * 128:1024 + (hd + 1) * 128], h[:, c, :])
                                                for c in range(8)], [wA.b, h.b])
                    cx.op("act", lambda e, pq_=pq_, hd=hd: e.copy(out=qb[:, hd, :], in_=pq_[:, 0:TC]),
                          reads=[pq_.b], writes=[qb.b])
                for hd in range(4):
                    E = Eb[hd % 2]
                    for mc in range(2):
                        psc = getps()
                        mmgrp(psc[:, 0:TC], psc.b, [(kT[:, hd, mc * 128:(mc + 1) * 128], qb[:, hd, :])], [kT.b, qb.b])
                        cx.op("act", lambda e, psc=psc, E=E, mc=mc: e.activation(
                            out=E[:, mc, :], in_=psc[:, 0:TC], func=AF.Exp, scale=float(128 ** -0.5)),
                            reads=[psc.b], writes=[E.b])
                    po = getps(); pd = getps()
                    mmgrp(po[:, 0:TC], po.b, [(vtok[:, mc, hd * 128:(hd + 1) * 128], E[:, mc, :]) for mc in range(2)],
                          [vtok.b, E.b])
                    mmgrp(pd[:, 0:TC], pd.b, [(ones_bf[:], E[:, mc, :]) for mc in range(2)], [ones_bf.b, E.b])
                    cx.op("dve", lambda e, pd=pd: e.reciprocal(out=rden[:], in_=pd[:, 0:TC]), reads=[pd.b], full=[rden.b])
                    cx.op("dve", lambda e, po=po, hd=hd: e.tensor_tensor(out=ob[:, hd, :], in0=po[:, 0:TC], in1=rden[:],
                                                                         op=ALU.mult),
                          reads=[po.b, rden.b], writes=[ob.b])
                for j in range(8):
                    js = slice(j * 128, (j + 1) * 128)
                    pga = getps(); pyc = getps()
                    mmgrp(pga[:, 0:TC], pga.b, [(wG[:, c, j * 128:(j + 1) * 128], h[:, c, :]) for c in range(8)], [wG.b, h.b])
                    mmgrp(pyc[:, 0:TC], pyc.b, [(wco[:, f, js], cn[:, f, :]) for f in range(4)], [wco.b, cn.b])
                    s = sgt[0]
                    cx.op("act", lambda e, pga=pga, s=s: e.activation(out=s[:], in_=pga[:, 0:TC], func=AF.Sigmoid),
                          reads=[pga.b], full=[s.b])
                    cx.op("dve", lambda e, pyc=pyc, s=s: e.tensor_tensor(out=macc[:], in0=pyc[:, 0:TC], in1=s[:], op=ALU.mult),
                          reads=[pyc.b, s.b], full=[macc.b])
                    pgb = getps(); pza = getps(); pzb = getps()
                    mmgrp(pgb[:, 0:TC], pgb.b, [(wG[:, c, 1024 + j * 128:1024 + (j + 1) * 128], h[:, c, :]) for c in range(8)],
                          [wG.b, h.b])
                    mmgrp(pza[:, 0:TC], pza.b, [(wgl[:, f, js], yb[:, f, :]) for f in range(4)], [wgl.b, yb.b])
                    mmgrp(pzb[:, 0:TC], pzb.b, [(wgl[:, f, 1024 + j * 128:1024 + (j + 1) * 128], yb[:, f, :]) for f in range(4)],
                          [wgl.b, yb.b])
                    sb_ = sgt[1]; sz = sgt[2]
                    cx.op("act", lambda e, pgb=pgb, sb_=sb_: e.activation(out=sb_[:], in_=pgb[:, 0:TC], func=AF.Sigmoid),
                          reads=[pgb.b], full=[sb_.b])
                    cx.op("act", lambda e, pzb=pzb, sz=sz: e.activation(out=sz[:], in_=pzb[:, 0:TC], func=AF.Sigmoid),
                          reads=[pzb.b], full=[sz.b])
                    cx.op("dve", lambda e, pza=pza, sz=sz: e.tensor_tensor(out=mt1[:], in0=pza[:, 0:TC], in1=sz[:], op=ALU.mult),
                          reads=[pza.b, sz.b], full=[mt1.b])
                    cx.op("dve", lambda e, sb_=sb_: e.tensor_tensor(out=mt1[:], in0=mt1[:], in1=sb_[:], op=ALU.mult),
                          reads=[sb_.b], writes=[mt1.b])
                    cx.op("dve", lambda e: e.tensor_tensor(out=macc[:], in0=macc[:], in1=mt1[:], op=ALU.add),
                          reads=[mt1.b], writes=[macc.b])
                    pgc = getps(); pym = getps()
                    mmgrp(pgc[:, 0:TC], pgc.b, [(wG[:, c, 2048 + j * 128:2048 + (j + 1) * 128], h[:, c, :]) for c in range(8)],
                          [wG.b, h.b])
                    mmgrp(pym[:, 0:TC], pym.b, [(wmo[:, hd, js], ob[:, hd, :]) for hd in range(4)], [wmo.b, ob.b])
                    s = sgt[0]
                    cx.op("act", lambda e, pgc=pgc, s=s: e.activation(out=s[:], in_=pgc[:, 0:TC], func=AF.Sigmoid),
                          reads=[pgc.b], full=[s.b])
                    cx.op("dve", lambda e, pym=pym, s=s: e.tensor_tensor(out=mt2[:], in0=pym[:, 0:TC], in1=s[:], op=ALU.mult),
                          reads=[pym.b, s.b], full=[mt2.b])
                    cx.op("dve", lambda e, j=j: e.tensor_tensor(out=merged[:, j, :], in0=macc[:], in1=mt2[:], op=ALU.add),
                          reads=[macc.b, mt2.b], writes=[merged.b])
                for tt in range(TC // 128):
                    ti = bi * (TC // 128) + tt
                    pp = ti % 2
                    xt_ = xt2[pp]; x2 = x2t[pp]; hb2 = h2b[pp]
                    cx.op("sp", lambda e, xt_=xt_, ti=ti: e.dma_start(out=xt_[:], in_=x_d[ti * 128:(ti + 1) * 128, :]),
                          full=[xt_.b], dma=True)
                    for half in range(2):
                        po_ = getps()
                        mmgrp(po_[:], po_.b, [(merged[:, j, tt * 128:(tt + 1) * 128], wo[:, j, half * 512:(half + 1) * 512])
                                              for j in range(8)], [merged.b, wo.b])
                        cx.op("dve", lambda e, po_=po_, x2=x2, xt_=xt_, half=half: e.tensor_tensor(
                            out=x2[:, half * 512:(half + 1) * 512], in0=po_[:], in1=xt_[:, half * 512:(half + 1) * 512],
                            op=ALU.add), reads=[po_.b, xt_.b], writes=[x2.b])
                    cx.op("sp", lambda e, x2=x2, ti=ti: e.dma_start(out=x2_scr[ti * 128:(ti + 1) * 128, :], in_=x2[:]),
                          reads=[x2.b], writes=[x2B], dma=True)
                    cx.op("act", lambda e, x2=x2: e.activation(out=junk2[:], in_=x2[:], func=AF.Square, accum_out=ss2[:]),
                          reads=[x2.b], full=[junk2.b, ss2.b])
                    cx.op("act", lambda e: e.activation(out=rt2[:], in_=ss2[:], func=AF.Sqrt, scale=1.0 / D, bias=EPS),
                          reads=[ss2.b], full=[rt2.b])
                    cx.op("dve", lambda e: e.reciprocal(out=rs2[:], in_=rt2[:]), reads=[rt2.b], full=[rs2.b])
                    cx.op("dve", lambda e, x2=x2: e.scalar_tensor_tensor(out=h2f[:], in0=x2[:], scalar=rs2[:, 0:1],
                                                                         in1=gffn[:], op0=ALU.mult, op1=ALU.mult),
                          reads=[x2.b, rs2.b, gffn.b], full=[h2f.b])
                    cx.op("act", lambda e, hb2=hb2: e.copy(out=hb2[:], in_=h2f[:]), reads=[h2f.b], full=[hb2.b])
                    cx.op("sp", lambda e, hb2=hb2, ti=ti: e.dma_start(out=h2_scr[ti * 128:(ti + 1) * 128, :], in_=hb2[:]),
                          reads=[hb2.b], writes=[h2B], dma=True)
                    pra = getps(); prb = getps()
                    for c in range(8):
                        pr = pra if c < 4 else prb
                        cx.op("pe", lambda e, pr=pr, c=c: e.transpose(out=pr[:, (c % 4) * 128:(c % 4 + 1) * 128],
                                                                      in_=h2f[:, c * 128:(c + 1) * 128], identity=ident_f[:]),
                              reads=[h2f.b, ident_f.b], writes=[pr.b])
                    cx.op("act", lambda e, pra=pra: e.copy(out=h2T[:, 0:4, :], in_=pra[:].rearrange("p (c t) -> p c t", c=4)),
                          reads=[pra.b], writes=[h2T.b])
                    cx.op("dve", lambda e, prb=prb: e.tensor_copy(out=h2T[:, 4:8, :],
                                                                  in_=prb[:].rearrange("p (c t) -> p c t", c=4)),
                          reads=[prb.b], writes=[h2T.b])
                    plg = getps()
                    mmgrp(plg[:, 0:36], plg.b, [(h2T[:, c, :], wr[:, c, :]) for c in range(8)], [h2T.b, wr.b])

                    def rd(fn, extra_reads=(), extra_writes=()):
                        cx.op("dve", fn, reads=[RS] + list(extra_reads), writes=[RS] + list(extra_writes))

                    rd(lambda e, plg=plg: e.tensor_tensor(out=lg[:], in0=plg[:, 0:36], in1=rbias[:], op=ALU.add),
                       [plg.b, rbias.b])
                    rd(lambda e: e.reduce_max(out=r_gmax[:], in_=lg[:, 0:4], axis=AX.X))
                    rd(lambda e: e.tensor_scalar(out=r_ohg[:], in0=lg[:, 0:4], scalar1=r_gmax[:, 0:1], scalar2=None,
                                                 op0=ALU.is_equal))
                    rd(lambda e: e.tensor_scalar(out=r_ngmax[:], in0=r_gmax[:], scalar1=-1.0, scalar2=None, op0=ALU.mult))
                    cx.op("act", lambda e: e.activation(out=r_eg[:], in_=lg[:, 0:4], func=AF.Exp, bias=r_ngmax[:, 0:1],
                                                        scale=1.0, accum_out=r_sumg[:]), reads=[RS], writes=[RS])
                    rd(lambda e: e.reciprocal(out=r_ptop[:], in_=r_sumg[:]))
                    rd(lambda e: e.tensor_tensor(out=r_selm[:], in0=lg[:, 4:36].rearrange("p (g j) -> p g j", g=4),
                                                 in1=r_ohg[:].unsqueeze(2).to_broadcast([128, 4, 8]), op=ALU.mult))
                    rd(lambda e: e.tensor_reduce(out=r_sel[:], in_=r_selm[:].rearrange("p g j -> p j g"), axis=AX.X,
                                                 op=ALU.add))
                    rd(lambda e: e.reduce_max(out=r_m1[:], in_=r_sel[:], axis=AX.X))
                    rd(lambda e: e.tensor_scalar(out=r_oh1[:], in0=r_sel[:], scalar1=r_m1[:, 0:1], scalar2=None,
                                                 op0=ALU.is_equal))
                    rd(lambda e: e.scalar_tensor_tensor(out=r_sel2[:], in0=r_oh1[:], scalar=-1e30, in1=r_sel[:],
                                                        op0=ALU.mult, op1=ALU.add))
                    rd(lambda e: e.reduce_max(out=r_m2[:], in_=r_sel2[:], axis=AX.X))
                    rd(lambda e: e.tensor_scalar(out=r_oh2[:], in0=r_sel2[:], scalar1=r_m2[:, 0:1], scalar2=None,
                                                 op0=ALU.is_equal))
                    rd(lambda e: e.tensor_tensor(out=r_dm[:], in0=r_m1[:], in1=r_m2[:], op=ALU.subtract))
                    cx.op("act", lambda e: e.activation(out=r_w1[:], in_=r_dm[:], func=AF.Sigmoid), reads=[RS], writes=[RS])
                    rd(lambda e: e.tensor_tensor(out=r_w1[:], in0=r_w1[:], in1=r_ptop[:], op=ALU.mult))
                    rd(lambda e: e.tensor_tensor(out=r_w2[:], in0=r_ptop[:], in1=r_w1[:], op=ALU.subtract))
                    rd(lambda e: e.tensor_tensor(out=r_M1[:], in0=r_ohg[:].unsqueeze(2).to_broadcast([128, 4, 8]),
                                                 in1=r_oh1[:].unsqueeze(1).to_broadcast([128, 4, 8]), op=ALU.mult))
                    rd(lambda e: e.tensor_tensor(out=r_M2[:], in0=r_ohg[:].unsqueeze(2).to_broadcast([128, 4, 8]),
                                                 in1=r_oh2[:].unsqueeze(1).to_broadcast([128, 4, 8]), op=ALU.mult))
                    Mc = r_Mc[pp]
                    rd(lambda e, Mc=Mc: e.tensor_tensor(out=Mc[:], in0=r_M1[:].rearrange("p g j -> p (g j)"),
                                                        in1=r_M2[:].rearrange("p g j -> p (g j)"), op=ALU.add),
                       extra_writes=[Mc.b])
                    ppos = getps()
                    mmgrp(ppos[:, 0:32], ppos.b, [(tri[:], Mc[:]), (ones_f[:], cum[:])], [tri.b, ones_f.b, Mc.b, cum.b, RS])
                    rd(lambda e, ppos=ppos: e.tensor_single_scalar(out=r_bk[:], in_=ppos[:, 0:32], scalar=127.5, op=ALU.is_gt),
                       [ppos.b])
                    for thr in range(2, NBLK):
                        rd(lambda e, ppos=ppos, thr=thr: e.tensor_single_scalar(out=r_t[:], in_=ppos[:, 0:32],
                                                                                scalar=128.0 * thr - 0.5, op=ALU.is_gt), [ppos.b])
                        rd(lambda e: e.tensor_tensor(out=r_bk[:], in0=r_bk[:], in1=r_t[:], op=ALU.add))
                    rd(lambda e: e.scalar_tensor_tensor(out=r_bk[:], in0=r_bk[:], scalar=float(1 - 128 * NEXP * NBLK), in1=ecap[:],
                                                        op0=ALU.mult, op1=ALU.add), [ecap.b])
                    rd(lambda e, ppos=ppos: e.scalar_tensor_tensor(out=r_sf[:], in0=ppos[:, 0:32], scalar=float(NEXP * NBLK),
                                                                   in1=r_bk[:], op0=ALU.mult, op1=ALU.add), [ppos.b])
                    rd(lambda e, ppos=ppos: e.tensor_single_scalar(out=r_ov[:], in_=ppos[:, 0:32], scalar=float(CAP) - 0.5,
                                                                   op=ALU.is_gt), [ppos.b])
                    rd(lambda e, Mc=Mc: e.tensor_tensor(out=cum[:], in0=cum[:], in1=Mc[:], op=ALU.add), [Mc.b], [cum.b])
                    for k, (Mk, wk) in enumerate(((r_M1, r_w1), (r_M2, r_w2))):
                        si = r_si[pp][k]; ent = r_ent[pp][k]
                        rd(lambda e, Mk=Mk: e.tensor_tensor(out=r_t[:], in0=Mk[:].rearrange("p g j -> p (g j)"), in1=r_sf[:],
                                                            op=ALU.mult))
                        rd(lambda e, k=k: e.reduce_sum(out=r_s[:, k:k + 1], in_=r_t[:], axis=AX.X))
                        rd(lambda e, Mk=Mk: e.tensor_tensor(out=r_t[:], in0=Mk[:].rearrange("p g j -> p (g j)"), in1=r_ov[:],
                                                            op=ALU.mult))
                        rd(lambda e: e.reduce_sum(out=r_o[:], in_=r_t[:], axis=AX.X))
                        rd(lambda e, k=k: e.tensor_tensor(out=r_d[:], in0=trashp[:], in1=r_s[:, k:k + 1], op=ALU.subtract),
                           [trashp.b])
                        rd(lambda e, k=k: e.scalar_tensor_tensor(out=r_s[:, k:k + 1], in0=r_d[:], scalar=r_o[:, 0:1],
                                                                 in1=r_s[:, k:k + 1], op0=ALU.mult, op1=ALU.add))
                        rd(lambda e, k=k, si=si: e.tensor_copy(out=si[:], in_=r_s[:, k:k + 1]), extra_writes=[si.b])
                        rd(lambda e, ent=ent, ti=ti: e.tensor_copy(out=ent[:, 0:1], in_=tokid[:, ti:ti + 1]),
                           [tokid.b], [ent.b])
                        rd(lambda e, ent=ent, ti=ti, k=k: e.tensor_scalar(out=ent[:, 1:2], in0=tokid[:, ti:ti + 1],
                                                                          scalar1=float(k * ROWS), scalar2=None, op0=ALU.add),
                           [tokid.b], [ent.b])
                        rd(lambda e, ent=ent, wk=wk: e.tensor_copy(out=ent[:, 2:3], in_=wk[:]), (), [ent.b])
                        def scat(e, si=si, ent=ent):
                            return e.indirect_dma_start(
                                out=lst_d, out_offset=bass.IndirectOffsetOnAxis(ap=si[:, 0:1], axis=0),
                                in_=ent[:], in_offset=None)
                        if KCUT >= 3:
                            cx.op("pool", scat, reads=[si.b, ent.b], writes=[lstB], dma=True)
            cx.barrier()
            alW.close()
            alC.close()
        if stop_after in ("A", "B", "C"):
            pass
        else:
            alD = Alloc(nc)
            NEB = NEXP * NBLK
            lst_sb = alD.sb([128, NEB, 4], F32, "lst_sb")
            idx_i = alD.sb([128, NEB], I32, "idx_i")
            dst_i = alD.sb([128, NEB], I32, "dst_i")
            cx.op("sp", lambda e: e.dma_start(out=lst_sb[:], in_=lst_d[0:NEXP * CAP, :].rearrange("(s eb) w -> s eb w", s=128)),
                  reads=[lstB], full=[lst_sb.b], dma=True)
            cx.op("dve", lambda e: e.tensor_copy(out=idx_i[:], in_=lst_sb[:, :, 0]), reads=[lst_sb.b], full=[idx_i.b])
            cx.op("dve", lambda e: e.tensor_copy(out=dst_i[:], in_=lst_sb[:, :, 1]), reads=[lst_sb.b], full=[dst_i.b])
            sg_ = [alD.sb([128, 8, 256], F32, f"sg{i}") for i in range(2)]
            su_ = [alD.sb([128, 8, 256], F32, f"su{i}") for i in range(2)]
            sd_ = [alD.sb([128, 2, 1024], F32, f"sd{i}") for i in range(2)]
            Wg = [alD.sb([128, 8, 256], BF16, f"Wg{i}") for i in range(2)]
            Wu = [alD.sb([128, 8, 256], BF16, f"Wu{i}") for i in range(2)]
            Wd = [alD.sb([128, 2, 1024], BF16, f"Wd{i}") for i in range(2)]
            Gt = [alD.sb([128, D], BF16, f"Gt{i}") for i in range(3)]
            Xe = [alD.sb([128, 8, CAP], BF16, f"Xe{i}") for i in range(2)]
            sgl = [alD.sb([128, CAP], F32, f"sgl{i}") for i in range(2)]
            ae = [alD.sb([128, 2, CAP], BF16, f"ae{i}") for i in range(2)]
            Yt = [alD.sb([128, D], F32, f"Yt{i}") for i in range(3)]

            def load_w(e_):
                p = e_ % 2
                cx.op("sp", lambda e: e.dma_start(out=sg_[p][:], in_=weg_d[e_].rearrange("(c p) n -> p c n", p=128)),
                      full=[sg_[p].b], dma=True)
                cx.op("sp", lambda e: e.dma_start(out=su_[p][:], in_=weu_d[e_].rearrange("(c p) n -> p c n", p=128)),
                      full=[su_[p].b], dma=True)
                cx.op("sp", lambda e: e.dma_start(out=sd_[p][:], in_=wed_d[e_].rearrange("(c p) n -> p c n", p=128)),
                      full=[sd_[p].b], dma=True)
                cx.op("pool", lambda e: e.tensor_copy(out=Wg[p][:], in_=sg_[p][:]), reads=[sg_[p].b], full=[Wg[p].b])
                cx.op("pool", lambda e: e.tensor_copy(out=Wu[p][:], in_=su_[p][:]), reads=[su_[p].b], full=[Wu[p].b])
                cx.op("act", lambda e: e.copy(out=Wd[p][:], in_=sd_[p][:]), reads=[sd_[p].b], full=[Wd[p].b])

            gi = [0]
            load_w(0)
            KNE = int(os.environ.get("KNE", str(NEXP)))
            for e_ in range(KNE):
                p = e_ % 2
                if e_ + 1 < NEXP:
                    load_w(e_ + 1)
                X = Xe[p]
                for blk in range(NBLK):
                    eb = e_ * NBLK + blk
                    G = Gt[gi[0] % 3]
                    pbk = psb[gi[0] % 2]
                    gi[0] += 1
                    cx.op("pool", lambda e, G=G, eb=eb: e.indirect_dma_start(
                        out=G[:], out_offset=None, in_=h2_scr,
                        in_offset=bass.IndirectOffsetOnAxis(ap=idx_i[:, eb:eb + 1], axis=0)),
                        reads=[idx_i.b, h2B], full=[G.b], dma=True)
                    for c in range(8):
                        cx.op("pe", lambda e, pbk=pbk, G=G, c=c: e.transpose(
                            out=pbk[:, c * 128:(c + 1) * 128], in_=G[:, c * 128:(c + 1) * 128], identity=ident_bf[:]),
                            reads=[G.b, ident_bf.b], writes=[pbk.b])
                    if blk % 2 == 0:
                        cx.op("act", lambda e, pbk=pbk, X=X, blk=blk: e.copy(
                            out=X[:, :, blk * 128:(blk + 1) * 128], in_=pbk[:].rearrange("p (c t) -> p c t", c=8)),
                            reads=[pbk.b], writes=[X.b])
                    else:
                        cx.op("dve", lambda e, pbk=pbk, X=X, blk=blk: e.tensor_copy(
                            out=X[:, :, blk * 128:(blk + 1) * 128], in_=pbk[:].rearrange("p (c t) -> p c t", c=8)),
                            reads=[pbk.b], writes=[X.b])
                a_ = ae[p]
                for ft in range(2):
                    pg = getps(); pu = getps()
                    for c in range(8):
                        cx.op("pe", lambda e, pg=pg, c=c, ft=ft, X=X, p=p: e.matmul(
                            pg[:, 0:CAP], lhsT=Wg[p][:, c, ft * 128:(ft + 1) * 128], rhs=X[:, c, :],
                            start=(c == 0), stop=(c == 7)), reads=[Wg[p].b, X.b], writes=[pg.b])
                    for c in range(8):
                        cx.op("pe", lambda e, pu=pu, c=c, ft=ft, X=X, p=p: e.matmul(
                            pu[:, 0:CAP], lhsT=Wu[p][:, c, ft * 128:(ft + 1) * 128], rhs=X[:, c, :],
                            start=(c == 0), stop=(c == 7)), reads=[Wu[p].b, X.b], writes=[pu.b])
                    s = sgl[ft]
                    cx.op("act", lambda e, pg=pg, s=s: e.activation(out=s[:], in_=pg[:, 0:CAP], func=AF.Silu),
                          reads=[pg.b], full=[s.b])
                    cx.op("dve", lambda e, pu=pu, s=s, a_=a_, ft=ft: e.tensor_tensor(
                        out=a_[:, ft, :], in0=pu[:, 0:CAP], in1=s[:], op=ALU.mult),
                        reads=[pu.b, s.b], writes=[a_.b])
                for blk in range(NBLK):
                    eb = e_ * NBLK + blk
                    Y = Yt[eb % 3]
                    for half in range(2):
                        py = getps()
                        for ft in range(2):
                            cx.op("pe", lambda e, py=py, ft=ft, blk=blk, half=half, a_=a_, p=p: e.matmul(
                                py[:], lhsT=a_[:, ft, blk * 128:(blk + 1) * 128],
                                rhs=Wd[p][:, ft, half * 512:(half + 1) * 512], start=(ft == 0), stop=(ft == 1)),
                                reads=[a_.b, Wd[p].b], writes=[py.b])
                        if half == 0:
                            cx.op("dve", lambda e, py=py, Y=Y, eb=eb: e.tensor_scalar(
                                out=Y[:, 0:512], in0=py[:], scalar1=lst_sb[:, eb, 2:3], scalar2=None, op0=ALU.mult),
                                reads=[py.b, lst_sb.b], writes=[Y.b])
                        else:
                            cx.op("act", lambda e, py=py, Y=Y, eb=eb: e.activation(
                                out=Y[:, 512:1024], in_=py[:], func=AF.Copy, scale=lst_sb[:, eb, 2:3]),
                                reads=[py.b, lst_sb.b], writes=[Y.b])
                    cx.op("pool", lambda e, Y=Y, eb=eb: e.indirect_dma_start(
                        out=moe_scr, out_offset=bass.IndirectOffsetOnAxis(ap=dst_i[:, eb:eb + 1], axis=0),
                        in_=Y[:], in_offset=None), reads=[Y.b, dst_i.b], writes=[moeB], dma=True)
            cx.barrier()
            alD.close()

            alE = Alloc(nc)
            gfin = alE.sb([128, D], F32, "gfin")
            cx.op("sp", lambda e: e.dma_start(out=gfin[:], in_=gfin_d.partition_broadcast(128)), full=[gfin.b], dma=True)
            xa = [alE.sb([128, D], F32, f"xa{i}") for i in range(2)]
            m0 = [alE.sb([128, D], F32, f"m0{i}") for i in range(2)]
            m1 = [alE.sb([128, D], F32, f"m1{i}") for i in range(2)]
            ot = [alE.sb([128, D], F32, f"ot{i}") for i in range(2)]
            junk3 = alE.sb([128, D], BF16, "junk3")
            sse = [alE.sb([128, 1], F32, f"sse{i}") for i in range(2)]
            rte = [alE.sb([128, 1], F32, f"rte{i}") for i in range(2)]
            rse = [alE.sb([128, 1], F32, f"rse{i}") for i in range(2)]
            outB = Buf("out")
            for ti in range(NT):
                p = ti % 2
                rows = slice(ti * 128, (ti + 1) * 128)
                cx.op("sp", lambda e, p=p, rows=rows: e.dma_start(out=xa[p][:], in_=x2_scr[rows, :]),
                      reads=[x2B], full=[xa[p].b], dma=True)
                cx.op("sp", lambda e, p=p, rows=rows: e.dma_start(out=m0[p][:], in_=moe_scr[rows, :]),
                      reads=[moeB], full=[m0[p].b], dma=True)
                cx.op("sp", lambda e, p=p, ti=ti: e.dma_start(
                    out=m1[p][:], in_=moe_scr[ROWS + ti * 128:ROWS + (ti + 1) * 128, :]),
                    reads=[moeB], full=[m1[p].b], dma=True)
                cx.op("pool", lambda e, p=p: e.tensor_tensor(out=m0[p][:], in0=m0[p][:], in1=m1[p][:], op=ALU.add),
                      reads=[m1[p].b], writes=[m0[p].b])
                cx.op("dve", lambda e, p=p: e.tensor_tensor(out=xa[p][:], in0=xa[p][:], in1=m0[p][:], op=ALU.add),
                      reads=[m0[p].b], writes=[xa[p].b])
                cx.op("act", lambda e, p=p: e.activation(out=junk3[:], in_=xa[p][:], func=AF.Square, accum_out=sse[p][:]),
                      reads=[xa[p].b], full=[junk3.b, sse[p].b])
                cx.op("act", lambda e, p=p: e.activation(out=rte[p][:], in_=sse[p][:], func=AF.Sqrt, scale=1.0 / D, bias=EPS),
                      reads=[sse[p].b], full=[rte[p].b])
                cx.op("dve", lambda e, p=p: e.reciprocal(out=rse[p][:], in_=rte[p][:]), reads=[rte[p].b], full=[rse[p].b])
                cx.op("dve", lambda e, p=p: e.scalar_tensor_tensor(out=ot[p][:], in0=xa[p][:], scalar=rse[p][:, 0:1],
                                                                   in1=gfin[:], op0=ALU.mult, op1=ALU.mult),
                      reads=[xa[p].b, rse[p].b, gfin.b], full=[ot[p].b])
                cx.op("sp", lambda e, p=p, rows=rows: e.dma_start(out=out_d[rows, :], in_=ot[p][:]),
                      reads=[ot[p].b], writes=[outB], dma=True)
            cx.barrier()
            alE.close()
        cx.barrier()
        cx.emit(block)
        print("waits", cx.nwait, "instrs", {e: cx.cnt[e] for e in cx.ENG})
    return nc


def host_consts():
    c = {}
    c["ident_bf"] = np.eye(128, dtype=np.float32).astype(ml_dtypes.bfloat16)
    c["ident_f"] = np.eye(128, dtype=np.float32)
    psel = np.zeros((128, 8, 240), np.float32)
    for a in range(8):
        for i in range(16):
            psel[a * 16 + i, a, 7 * 16 + i] = 1.0
    c["psel"] = psel.astype(ml_dtypes.bfloat16)
    kk = np.arange(128) // 16
    c["cmask"] = (kk[None, :] >= kk[:, None]).astype(np.float32)
    c["tri"] = (np.arange(128)[:, None] < np.arange(128)[None, :]).astype(np.float32)
    c["ecap"] = np.ascontiguousarray(np.broadcast_to((np.arange(32) * NBLK).astype(np.float32)[None, :], (128, 32)))
    c["tokid"] = (np.arange(NT)[None, :] * 128 + np.arange(128)[:, None]).astype(np.float32)
    li = np.zeros((NEXP * CAP + 128, 4), np.float32)
    li[:, 0] = SEQ + ((np.arange(NEXP * CAP + 128) // (NEXP * NBLK)) % 128)
    li[:, 1] = li[:, 0]
    c["trashp"] = (NEXP * CAP + np.arange(128)).astype(np.float32).reshape(128, 1)
    c["lst_init"] = li
    return c


def pair_layout(a):
    rest = a.shape[2:]
    a = a.reshape((16, 2, 64) + rest)
    a = np.moveaxis(a, 0, 2)
    return np.ascontiguousarray(a.reshape((128, 16) + rest))


def make_inmap(inputs, b, consts=None):
    f = lambda a: np.ascontiguousarray(a, dtype=np.float32)
    m = {"x": f(inputs["x"][b]),
         "g_mix": f(inputs["g_mix"]),
         "w_in": f(inputs["w_in"][0])}
    m["lamre_l"] = pair_layout(f(inputs["ssm_lambda_re"][0]))
    m["lamim_l"] = pair_layout(f(inputs["ssm_lambda_im"][0]))
    m["logdt_l"] = pair_layout(np.broadcast_to(f(inputs["ssm_log_dt"][0])[:, None], (32, 64)))
    m["bre_l"] = pair_layout(f(inputs["ssm_b_re"][0]))
    m["bim_l"] = pair_layout(f(inputs["ssm_b_im"][0]))
    m["cre_l"] = pair_layout(f(inputs["ssm_c_re"][0]).transpose(0, 2, 1))
    m["cim_l"] = pair_layout(f(inputs["ssm_c_im"][0]).transpose(0, 2, 1))
    m["d_l"] = np.ascontiguousarray(np.tile(f(inputs["ssm_d"][0]).reshape(32, 16).T, (8, 1)))
    m["mem"] = f(inputs["mem"][b])
    for k_, n_ in (("g_mem", "g_mem"), ("g_ffn", "g_ffn")):
        m[n_] = f(inputs[k_])
    m["g_final"] = f(inputs["g_final"]).reshape(1, D)
    m["w_mem_kv"] = f(inputs["w_mem_kv"][0]); m["w_mem_out"] = f(inputs["w_mem_out"][0])
    m["w_conv_out"] = f(inputs["w_conv_out"][0]); m["w_ssm_glu"] = f(inputs["w_ssm_glu"][0])
    m["w_out"] = f(inputs["w_out"][0])
    m["w_router"] = np.ascontiguousarray(np.concatenate([f(inputs["w_router_group"][0]),
                                                         f(inputs["w_router_expert"][0])], axis=1))
    m["b_router"] = np.ascontiguousarray(np.concatenate([f(inputs["b_router_group"][0]),
                                                         f(inputs["b_router_expert"][0])])[None, :])
    m["cdw_l"] = np.ascontiguousarray(f(inputs["conv_dw"][0]).T.reshape(4, 128, 31).transpose(1, 0, 2))
    m["cb_l"] = np.ascontiguousarray(f(inputs["conv_dw_bias"][0]).reshape(4, 128).T)
    m["lng_l"] = np.ascontiguousarray(f(inputs["conv_ln_g"][0]).reshape(4, 128).T)
    m["lnb_l"] = np.ascontiguousarray(f(inputs["conv_ln_b"][0]).reshape(4, 128).T)
    m["w_exp_gate"] = f(inputs["w_exp_gate"][0]); m["w_exp_up"] = f(inputs["w_exp_up"][0])
    m["w_exp_down"] = f(inputs["w_exp_down"][0])
    m.update(consts if consts is not None else host_consts())
    return m


def kernel(**inputs):
    nc = build()
    consts = host_consts()
    in_maps = [make_inmap(inputs, b, consts) for b in range(NCORES)]
    res = run_bass_kernel_spmd(nc, in_maps, core_ids=list(range(NCORES)))
    return np.stack([r["out"] for r in res.results], axis=0)
```

```python
import os
import numpy as np
import ml_dtypes
from contextlib import ExitStack
import concourse.bass as bass
import concourse.mybir as mybir
from concourse.bass_utils import run_bass_kernel_spmd

F32 = mybir.dt.float32
BF16 = mybir.dt.bfloat16
I32 = mybir.dt.int32
U32 = mybir.dt.uint32
AF = mybir.ActivationFunctionType
ALU = mybir.AluOpType
AX = mybir.AxisListType
GELU = AF.Gelu_apprx_tanh

D = 1024
SEQ = 4096
NCORES = 8
T = 512
NB = SEQ // T
NT = SEQ // 128
EPS = 1e-6
NEXP = 32
CAP = 384
NBLK = CAP // 128
ROWS = SEQ + 128


class Buf:
    __slots__ = ("name", "w", "r")

    def __init__(self, name):
        self.name = name
        self.w = {}
        self.r = {}


class Ctx:
    ENG = ("pe", "dve", "act", "pool", "sp")
    KROT = 4
    NDMA = 12

    def __init__(self, nc, es):
        self.nc = nc
        self.q = {e: [] for e in self.ENG}
        self.cnt = {e: 0 for e in self.ENG}
        self.seen = {e: {} for e in self.ENG}
        self.esem = {e: [es.enter_context(nc.semaphore(f"s_{e}{i}")) for i in range(self.KROT)]
                     for e in self.ENG}
        self.dsem = {e: [es.enter_context(nc.semaphore(f"d_{e}{i}")) for i in range(self.NDMA)]
                     for e in ("sp", "act", "pool")}
        self.dcnt = {e: [0] * self.NDMA for e in self.dsem}
        self.dnext = {e: 0 for e in self.dsem}
        self.nwait = 0
        self.waited = {e: set() for e in self.ENG}

    def _wait(self, eng, tok):
        key, val = tok
        if key[0] == 'e' and key[1] == eng and eng == "pe":
            return
        if self.seen[eng].get(key, -1) >= val:
            return
        self.seen[eng][key] = val
        if key[0] == 'e':
            self.waited[key[1]].add(val)
        self.q[eng].append(("w", key, val))
        self.nwait += 1

    def op(self, eng, fn, reads=(), writes=(), full=(), dma=False):
        toks = []
        for b in reads:
            toks.extend(b.w.items())
        for b in tuple(writes) + tuple(full):
            toks.extend(b.w.items())
            toks.extend(b.r.items())
        for t in toks:
            self._wait(eng, t)
        if dma:
            i = self.dnext[eng]
            self.dnext[eng] = (i + 1) % self.NDMA
            key = ('d', eng, i)
            if self.dcnt[eng][i] > 0:
                self._wait(eng, (key, self.dcnt[eng][i]))
            self.dcnt[eng][i] += 16
            val = self.dcnt[eng][i]
            self.q[eng].append(("d", fn, self.dsem[eng][i]))
        else:
            key = ('e', eng)
            val = self.cnt[eng]
            self.cnt[eng] += 1
            self.q[eng].append(("i", fn, val))
        for b in reads:
            b.r[key] = val
        for b in full:
            b.w = {key: val}
            b.r = {}
        for b in writes:
            b.w[key] = val
        return (key, val)

    def barrier(self):
        toks = []
        for e in self.ENG:
            if self.cnt[e] > 0:
                toks.append((('e', e), self.cnt[e] - 1))
        for e in self.dsem:
            for i in range(self.NDMA):
                if self.dcnt[e][i] > 0:
                    toks.append((('d', e, i), self.dcnt[e][i]))
        for e in self.ENG:
            for t in toks:
                self._wait(e, t)

    def emit(self, block):
        nc = self.nc

        rank = {e: {v: i for i, v in enumerate(sorted(self.waited[e]))} for e in self.ENG}
        K_ = self.KROT

        def run(engname, engine):
            for item in self.q[engname]:
                if item[0] == "w":
                    key, val = item[1], item[2]
                    if key[0] == 'e':
                        r = rank[key[1]][val]
                        engine.wait_ge(self.esem[key[1]][r % K_], r // K_ + 1)
                    else:
                        engine.wait_ge(self.dsem[key[1]][key[2]], val)
                elif item[0] == "d":
                    item[1](engine).then_inc(item[2], 16)
                else:
                    ins = item[1](engine)
                    r = rank[engname].get(item[2])
                    if r is not None:
                        ins.then_inc(self.esem[engname][r % K_], 1)

        @block.tensor
        def _(e):
            run("pe", e)

        @block.vector
        def _(e):
            run("dve", e)

        @block.scalar
        def _(e):
            run("act", e)

        @block.gpsimd
        def _(e):
            run("pool", e)

        @block.sync
        def _(e):
            run("sp", e)


class TT:
    def __init__(self, t, name):
        self.t = t
        self.b = Buf(name)

    def __getitem__(self, k):
        return self.t[k]


class Alloc:
    cnt = [0]

    def __init__(self, nc, es=None):
        self.nc = nc
        self.es = es if es is not None else ExitStack()

    @property
    def n(self):
        return Alloc.cnt[0]

    @n.setter
    def n(self, v):
        Alloc.cnt[0] = v

    def close(self):
        self.es.close()

    def sb(self, shape, dt, name=None):
        self.n += 1
        name = name or f"sb{self.n}"
        t = self.es.enter_context(self.nc.sbuf_tensor(f"{name}_{self.n}", list(shape), dt))
        return TT(t, name)

    def ps(self, shape, dt, name=None):
        self.n += 1
        name = name or f"ps{self.n}"
        t = self.es.enter_context(self.nc.psum_tensor(f"{name}_{self.n}", list(shape), dt))
        return TT(t, name)


def build(stop_after="E", dbg=False):
    nc = bass.Bass("TRN2", target_bir_lowering=False)
    dram = {}

    def din(name, shape, dt=F32):
        dram[name] = nc.dram_tensor(name, list(shape), dt, kind="ExternalInput").ap()
        return dram[name]

    def dscr(name, shape, dt, kind="Internal"):
        dram[name] = nc.dram_tensor(name, list(shape), dt, kind=kind).ap()
        return dram[name]

    x_d = din("x", [SEQ, D])
    gmix_d = din("g_mix", [1, D])
    w_in_d = din("w_in", [D, 5120])
    ident_bf_d = din("ident_bf", [128, 128], BF16)
    ident_f_d = din("ident_f", [128, 128], F32)
    lamre_d = din("lamre_l", [128, 16])
    lamim_d = din("lamim_l", [128, 16])
    logdt_d = din("logdt_l", [128, 16])
    bre_d = din("bre_l", [128, 16, 16])
    bim_d = din("bim_l", [128, 16, 16])
    cre_d = din("cre_l", [128, 16, 16])
    cim_d = din("cim_l", [128, 16, 16])
    dl_d = din("d_l", [128, 32])
    psel_d = din("psel", [128, 8, 240], BF16)
    cmask_d = din("cmask", [128, 128])
    mem_d = din("mem", [256, D])
    gmem_d = din("g_mem", [1, D])
    gffn_d = din("g_ffn", [1, D])
    gfin_d = din("g_final", [1, D])
    wkv_d = din("w_mem_kv", [D, 1024])
    wmo_d = din("w_mem_out", [512, D])
    wco_d = din("w_conv_out", [512, D])
    wgl_d = din("w_ssm_glu", [512, 2048])
    wo_d = din("w_out", [D, D])
    wr_d = din("w_router", [D, 36])
    rbias_d = din("b_router", [1, 36])
    cdw_d = din("cdw_l", [128, 4, 31])
    cb_d = din("cb_l", [128, 4])
    lng_d = din("lng_l", [128, 4])
    lnb_d = din("lnb_l", [128, 4])
    tri_d = din("tri", [128, 128])
    ecap_d = din("ecap", [128, 32])
    tokid_d = din("tokid", [128, NT])
    lst_init_d = din("lst_init", [NEXP * CAP + 128, 4])
    trashp_d = din("trashp", [128, 1])
    weg_d = din("w_exp_gate", [NEXP, D, 256])
    weu_d = din("w_exp_up", [NEXP, D, 256])
    wed_d = din("w_exp_down", [NEXP, 256, D])
    dk = "ExternalOutput" if dbg else "Internal"
    lst_d = dscr("lst", [NEXP * CAP + 128, 4], F32, kind=dk)
    h2_scr = dscr("h2_scr", [ROWS, D], BF16, kind=dk)
    moe_scr = dscr("moe_scr", [2 * ROWS, D], F32, kind=dk)
    x2_scr = dscr("x2_scr", [SEQ, D], F32, kind=dk)
    ys_scr = dscr("ys_scr", [4, 128, SEQ], BF16, kind="ExternalOutput" if dbg else "Internal")
    out_d = dscr("out", [SEQ, D], F32, kind="ExternalOutput")
    hT_scr = dscr("hT_scr", [8, 128, SEQ], BF16, kind="ExternalOutput" if dbg else "Internal")
    u_dbg = dscr("u_dbg", [4, 128, SEQ], BF16, kind="ExternalOutput") if dbg else None

    with ExitStack() as es:
        cx = Ctx(nc, es)
        al = Alloc(nc, es)
        block = es.enter_context(nc.Block())

        ident_bf = al.sb([128, 128], BF16, "ident_bf")
        cx.op("sp", lambda e: e.dma_start(out=ident_bf[:], in_=ident_bf_d), full=[ident_bf.b], dma=True)
        ident_f = al.sb([128, 128], F32, "ident_f")
        cx.op("sp", lambda e: e.dma_start(out=ident_f[:], in_=ident_f_d), full=[ident_f.b], dma=True)

        psum = [al.ps([128, 512], F32, f"bank{i}") for i in range(6)]
        psb = [al.ps([128, 1024], BF16, f"bankb{i}") for i in range(2)]
        pctr = [0]

        def getps():
            p = psum[pctr[0] % len(psum)]
            pctr[0] += 1
            return p

        alAB = Alloc(nc)
        u_all = alAB.sb([128, 4, SEQ], BF16, "u_all")
        M_all = alAB.sb([128, 32, 128], BF16, "M_all")
        W2r = alAB.sb([128, 16, 2, 128], BF16, "W2r"); W2i = alAB.sb([128, 16, 2, 128], BF16, "W2i")
        C1r = alAB.sb([128, 16, 128], BF16, "C1r"); nC1i = alAB.sb([128, 16, 128], BF16, "nC1i")
        KAr = alAB.sb([128, 9, 16], F32, "KAr"); KAi = alAB.sb([128, 9, 16], F32, "KAi")
        KnAi = alAB.sb([128, 9, 16], F32, "KnAi")
        psel = alAB.sb([128, 8, 240], BF16, "psel")
        cx.op("sp", lambda e: e.dma_start(out=psel[:], in_=psel_d), full=[psel.b], dma=True)
        al_outer = al
        al = Alloc(nc)
        gmix = al.sb([128, D], F32, "gmix")
        cx.op("sp", lambda e: e.dma_start(out=gmix[:], in_=gmix_d.partition_broadcast(128)),
              full=[gmix.b], dma=True)

        stg = [al.sb([128, 8, 256], F32, f"stg{i}") for i in range(2)]
        sctr = [0]

        def load_cast(dst, dst_col0, src_d, c0, c1, kch):
            for cc in range(c0, c1, 256):
                w = min(256, c1 - cc)
                s = stg[sctr[0] % 2]
                sctr[0] += 1
                src = src_d[:, cc:cc + w].rearrange("(c p) n -> p c n", p=128)
                cx.op("sp", lambda e, s=s, src=src, w=w: e.dma_start(out=s[:, 0:kch, 0:w], in_=src),
                      full=[s.b], dma=True)
                o = dst_col0 + (cc - c0)
                cx.op("pool", lambda e, s=s, o=o, w=w: e.tensor_copy(out=dst[:, 0:kch, o:o + w],
                                                                     in_=s[:, 0:kch, 0:w]),
                      reads=[s.b], writes=[dst.b])

        w_ssm_in = al.sb([128, 8, 512], BF16, "w_ssm_in")
        load_cast(w_ssm_in, 0, w_in_d, 1024, 1536, 8)
        xt = [al.sb([128, D], F32, f"xt{i}") for i in range(2)]
        junk = al.sb([128, D], BF16, "junk")
        ss = [al.sb([128, 1], F32, f"ss{i}") for i in range(2)]
        rt = [al.sb([128, 1], F32, f"rt{i}") for i in range(2)]
        rstd = [al.sb([128, 1], F32, f"rstd{i}") for i in range(2)]
        hbf = [al.sb([128, D], BF16, f"hbf{i}") for i in range(2)]
        hTb = [al.sb([128, 8, T], BF16, f"hTb{i}") for i in range(2)]

        for i in range(NT):
            p = i % 2
            blk = i // 4
            hb = hTb[blk % 2]
            cx.op("sp", lambda e, p=p, i=i: e.dma_start(out=xt[p][:], in_=x_d[i * 128:(i + 1) * 128, :]),
                  full=[xt[p].b], dma=True)
            cx.op("act", lambda e, p=p: e.activation(out=junk[:], in_=xt[p][:], func=AF.Square,
                                                     accum_out=ss[p][:]),
                  reads=[xt[p].b], writes=[junk.b], full=[ss[p].b])
            cx.op("act", lambda e, p=p: e.activation(out=rt[p][:], in_=ss[p][:], func=AF.Sqrt,
                                                     scale=1.0 / D, bias=EPS),
                  reads=[ss[p].b], full=[rt[p].b])
            cx.op("dve", lambda e, p=p: e.reciprocal(out=rstd[p][:], in_=rt[p][:]),
                  reads=[rt[p].b], full=[rstd[p].b])
            cx.op("dve", lambda e, p=p: e.scalar_tensor_tensor(out=hbf[p][:], in0=xt[p][:],
                                                               scalar=rstd[p][:, 0:1], in1=gmix[:],
                                                               op0=ALU.mult, op1=ALU.mult),
                  reads=[xt[p].b, rstd[p].b, gmix.b], full=[hbf[p].b])
            pb = psb[i % 2]
            for c in range(8):
                cx.op("pe", lambda e, pb=pb, p=p, c=c: e.transpose(out=pb[:, c * 128:(c + 1) * 128],
                                                                   in_=hbf[p][:, c * 128:(c + 1) * 128],
                                                                   identity=ident_bf[:]),
                      reads=[hbf[p].b, ident_bf.b], writes=[pb.b])
            tt = i % 4
            cx.op("act", lambda e, pb=pb, hb=hb, tt=tt: e.copy(
                out=hb[:, :, tt * 128:(tt + 1) * 128],
                in_=pb[:].rearrange("p (c t) -> p c t", c=8)),
                reads=[pb.b], writes=[hb.b])
            if tt == 3:
                for f in range(4):
                    ps = getps()
                    for c in range(8):
                        cx.op("pe", lambda e, ps=ps, hb=hb, f=f, c=c: e.matmul(
                            ps[:], lhsT=w_ssm_in[:, c, f * 128:(f + 1) * 128], rhs=hb[:, c, :],
                            start=(c == 0), stop=(c == 7)),
                            reads=[w_ssm_in.b, hb.b], writes=[ps.b])
                    cx.op("dve", lambda e, ps=ps, f=f, blk=blk: e.tensor_copy(
                        out=u_all[:, f, blk * T:(blk + 1) * T], in_=ps[:]),
                        reads=[ps.b], writes=[u_all.b])
                cx.op("sp", lambda e, hb=hb, blk=blk: e.dma_start(
                    out=hT_scr[:, :, blk * T:(blk + 1) * T].rearrange("c p t -> p c t"), in_=hb[:]),
                    reads=[hb.b], dma=True)

        if dbg:
            cx.op("sp", lambda e: e.dma_start(out=u_dbg.rearrange("f p t -> p f t"), in_=u_all[:]),
                  reads=[u_all.b], dma=True)


        cx.barrier()
        al.close()
        al = Alloc(nc)
        TWO_PI = 2.0 * np.pi
        cmask = al.sb([128, 128], F32, "cmask")
        cx.op("sp", lambda e: e.dma_start(out=cmask[:], in_=cmask_d), full=[cmask.b], dma=True)
        dl = al.sb([128, 32], F32, "dl")
        cx.op("sp", lambda e: e.dma_start(out=dl[:], in_=dl_d), full=[dl.b], dma=True)
        SU = Buf("ssm_setup")

        def sload(shape, src, name):
            t = al.sb(shape, F32, name)
            cx.op("sp", lambda e: e.dma_start(out=t[:], in_=src), full=[t.b], dma=True)
            return t

        lamre = sload([128, 16], lamre_d, "lamre")
        lamim = sload([128, 16], lamim_d, "lamim")
        logdt = sload([128, 16], logdt_d, "logdt")
        Bre = sload([128, 16, 16], bre_d, "Bre")
        Bim = sload([128, 16, 16], bim_d, "Bim")
        Cre = sload([128, 16, 16], cre_d, "Cre")
        Cim = sload([128, 16, 16], cim_d, "Cim")
        ins_b = [lamre.b, lamim.b, logdt.b, Bre.b, Bim.b, Cre.b, Cim.b]

        def S(shape, name):
            return al.sb(shape, F32, name)

        def dv(fn):
            cx.op("dve", fn, reads=ins_b, writes=[SU])

        def ac(fn):
            cx.op("act", fn, reads=ins_b, writes=[SU])

        def tt_(out, a, b, op):
            dv(lambda e: e.tensor_tensor(out=out, in0=a, in1=b, op=op))

        sh16 = [128, 16]
        dt_ = S(sh16, "dt"); lrd = S(sh16, "lrd"); th = S(sh16, "th")
        ac(lambda e: e.activation(out=dt_[:], in_=logdt[:], func=AF.Exp))
        tt_(lrd[:], lamre[:], dt_[:], ALU.mult)
        tt_(th[:], lamim[:], dt_[:], ALU.mult)
        mag = S(sh16, "mag"); imag2 = S(sh16, "imag2")
        ac(lambda e: e.activation(out=mag[:], in_=lrd[:], func=AF.Exp))
        ac(lambda e: e.activation(out=imag2[:], in_=lrd[:], func=AF.Exp, scale=-2.0))
        kq_i = al.sb(sh16, I32, "kq_i"); kq = S(sh16, "kq"); red = S(sh16, "red"); msk = S(sh16, "msk")
        sinv = S(sh16, "sinv"); cosv = S(sh16, "cosv"); tmpa = S(sh16, "tmpa")

        def sin_of(outt, shift):
            dv(lambda e: e.tensor_scalar(out=tmpa[:], in0=th[:], scalar1=float(shift), scalar2=None,
                                         op0=ALU.add))
            dv(lambda e: e.tensor_scalar(out=kq[:], in0=tmpa[:], scalar1=float(1.0 / TWO_PI),
                                         scalar2=None, op0=ALU.mult))
            dv(lambda e: e.tensor_copy(out=kq_i[:], in_=kq[:]))
            dv(lambda e: e.tensor_copy(out=kq[:], in_=kq_i[:]))
            dv(lambda e: e.scalar_tensor_tensor(out=red[:], in0=kq[:], scalar=float(-TWO_PI),
                                                in1=tmpa[:], op0=ALU.mult, op1=ALU.add))
            dv(lambda e: e.tensor_single_scalar(out=msk[:], in_=red[:], scalar=float(np.pi), op=ALU.is_gt))
            dv(lambda e: e.scalar_tensor_tensor(out=red[:], in0=msk[:], scalar=float(-TWO_PI),
                                                in1=red[:], op0=ALU.mult, op1=ALU.add))
            dv(lambda e: e.tensor_single_scalar(out=msk[:], in_=red[:], scalar=float(-np.pi), op=ALU.is_lt))
            dv(lambda e: e.scalar_tensor_tensor(out=red[:], in0=msk[:], scalar=float(TWO_PI),
                                                in1=red[:], op0=ALU.mult, op1=ALU.add))
            ac(lambda e: e.activation(out=outt[:], in_=red[:], func=AF.Sin))

        sin_of(sinv, 0.0)
        sin_of(cosv, np.pi / 2)
        PWr = S([128, 9, 16], "PWr"); PWi = S([128, 9, 16], "PWi")
        IPr = S([128, 8, 16], "IPr"); IPi = S([128, 8, 16], "IPi")
        t1 = S([128, 16, 8, 16], "t1"); t2 = S([128, 16, 8, 16], "t2")

        def cmul(outr, outi, ar, ai, br, bi, shp, neg_i=False):
            a1 = t1[:].rearrange("p a b c -> p (a b c)")[:, 0:int(np.prod(shp[1:]))]
            a2 = t2[:].rearrange("p a b c -> p (a b c)")[:, 0:int(np.prod(shp[1:]))]
            if len(shp) == 3:
                a1 = a1.rearrange("p (a b) -> p a b", a=shp[1])
                a2 = a2.rearrange("p (a b) -> p a b", a=shp[1])
            tt_(a1, ar, br, ALU.mult)
            tt_(a2, ai, bi, ALU.mult)
            tt_(outr, a1, a2, ALU.subtract)
            tt_(a1, ar, bi, ALU.mult)
            tt_(a2, ai, br, ALU.mult)
            if neg_i:
                dv(lambda e: e.scalar_tensor_tensor(out=outi, in0=a1, scalar=-1.0, in1=a2,
                                                    op0=ALU.mult, op1=ALU.subtract))
            else:
                tt_(outi, a1, a2, ALU.add)

        dv(lambda e: e.memset(PWr[:, 0, :], 1.0))
        dv(lambda e: e.memset(PWi[:, 0, :], 0.0))
        dv(lambda e: e.memset(IPr[:, 0, :], 1.0))
        dv(lambda e: e.memset(IPi[:, 0, :], 0.0))
        tt_(PWr[:, 1, :], mag[:], cosv[:], ALU.mult)
        tt_(PWi[:, 1, :], mag[:], sinv[:], ALU.mult)
        tt_(IPr[:, 1, :], PWr[:, 1, :], imag2[:], ALU.mult)
        dv(lambda e: e.scalar_tensor_tensor(out=IPi[:, 1, :], in0=PWi[:, 1, :], scalar=-1.0, in1=imag2[:],
                                            op0=ALU.mult, op1=ALU.mult))
        for n in range(2, 9):
            cmul(PWr[:, n, :], PWi[:, n, :], PWr[:, n - 1, :], PWi[:, n - 1, :], PWr[:, 1, :], PWi[:, 1, :], sh16)
        for n in range(2, 8):
            cmul(IPr[:, n, :], IPi[:, n, :], IPr[:, n - 1, :], IPi[:, n - 1, :], IPr[:, 1, :], IPi[:, 1, :], sh16)
        dv(lambda e: e.tensor_copy(out=KAr[:, 0, :], in_=PWr[:, 8, :]))
        dv(lambda e: e.tensor_copy(out=KAi[:, 0, :], in_=PWi[:, 8, :]))
        for d_ in range(1, 9):
            cmul(KAr[:, d_, :], KAi[:, d_, :], KAr[:, d_ - 1, :], KAi[:, d_ - 1, :],
                 KAr[:, d_ - 1, :], KAi[:, d_ - 1, :], sh16)
        dv(lambda e: e.tensor_scalar(out=KnAi[:], in0=KAi[:], scalar1=-1.0, scalar2=None, op0=ALU.mult))
        am1 = S(sh16, "am1"); l2 = S(sh16, "l2"); il2 = S(sh16, "il2"); kr = S(sh16, "kr"); ki = S(sh16, "ki")
        dv(lambda e: e.tensor_scalar(out=am1[:], in0=PWr[:, 1, :], scalar1=-1.0, scalar2=None, op0=ALU.add))
        tt_(l2[:], lamre[:], lamre[:], ALU.mult)
        tt_(tmpa[:], lamim[:], lamim[:], ALU.mult)
        tt_(l2[:], l2[:], tmpa[:], ALU.add)
        dv(lambda e: e.reciprocal(out=il2[:], in_=l2[:]))
        tt_(kr[:], am1[:], lamre[:], ALU.mult)
        tt_(tmpa[:], PWi[:, 1, :], lamim[:], ALU.mult)
        tt_(kr[:], kr[:], tmpa[:], ALU.add)
        tt_(kr[:], kr[:], il2[:], ALU.mult)
        tt_(ki[:], PWi[:, 1, :], lamre[:], ALU.mult)
        tt_(tmpa[:], am1[:], lamim[:], ALU.mult)
        tt_(ki[:], ki[:], tmpa[:], ALU.subtract)
        tt_(ki[:], ki[:], il2[:], ALU.mult)
        sh3 = [128, 16, 16]

        def bc(a):
            return a.unsqueeze(2).to_broadcast(sh3)

        Bbr = S(sh3, "Bbr"); Bbi = S(sh3, "Bbi")
        cmul(Bbr[:], Bbi[:], bc(kr[:]), bc(ki[:]), Bre[:], Bim[:], sh3)
        Bhr = S([128, 16, 8, 16], "Bhr"); nBhi = S([128, 16, 8, 16], "nBhi"); Bhi = S([128, 16, 8, 16], "Bhi")
        Btr = S([128, 16, 8, 16], "Btr"); Bti = S([128, 16, 8, 16], "Bti")
        Chr = S([128, 16, 9, 16], "Chr"); Chi = S([128, 16, 9, 16], "Chi"); nChi = S([128, 16, 9, 16], "nChi")
        for k in range(8):
            cmul(Bhr[:, :, k, :], Bhi[:, :, k, :], bc(IPr[:, k, :]), bc(IPi[:, k, :]), Bbr[:], Bbi[:], sh3)
            cmul(Btr[:, :, k, :], Bti[:, :, k, :], bc(PWr[:, 7, :]), bc(PWi[:, 7, :]),
                 Bhr[:, :, k, :], Bhi[:, :, k, :], sh3)
        dv(lambda e: e.tensor_scalar(out=nBhi[:], in0=Bhi[:], scalar1=-1.0, scalar2=None, op0=ALU.mult))
        for j in range(9):
            cmul(Chr[:, :, j, :], Chi[:, :, j, :], bc(PWr[:, j, :]), bc(PWi[:, j, :]), Cre[:], Cim[:], sh3)
        dv(lambda e: e.tensor_scalar(out=nChi[:], in0=Chi[:], scalar1=-1.0, scalar2=None, op0=ALU.mult))
        dv(lambda e: e.tensor_copy(out=C1r[:].rearrange("p r (j c) -> p r j c", j=8), in_=Chr[:, :, 1:9, :]))
        dv(lambda e: e.tensor_copy(out=nC1i[:].rearrange("p r (j c) -> p r j c", j=8), in_=nChi[:, :, 1:9, :]))
        mtmp = S([128, 128], "mtmp")
        cx.op("pool", lambda e: e.memset(W2r[:], 0.0), reads=ins_b, writes=[SU])
        cx.op("pool", lambda e: e.memset(W2i[:], 0.0), reads=ins_b, writes=[SU])
        for r in range(16):
            for two in range(2):
                g = 2 * r + two
                rng = slice(two * 64, (two + 1) * 64)
                ps = getps()
                cx.op("pe", lambda e, ps=ps, r=r, rng=rng: e.matmul(
                    ps[:, 0:128], lhsT=Bhr[rng, r, :, :].rearrange("p k c -> p (k c)"),
                    rhs=Chr[rng, r, 0:8, :].rearrange("p j c -> p (j c)"), start=True, stop=False),
                    reads=[SU], writes=[ps.b])
                cx.op("pe", lambda e, ps=ps, r=r, rng=rng: e.matmul(
                    ps[:, 0:128], lhsT=nBhi[rng, r, :, :].rearrange("p k c -> p (k c)"),
                    rhs=Chi[rng, r, 0:8, :].rearrange("p j c -> p (j c)"), start=False, stop=True),
                    reads=[SU], writes=[ps.b])
                cx.op("dve", lambda e, ps=ps: e.tensor_tensor(out=mtmp[:], in0=ps[:, 0:128], in1=cmask[:],
                                                              op=ALU.mult),
                      reads=[ps.b, cmask.b], writes=[SU])
                cx.op("dve", lambda e, g=g: e.scalar_tensor_tensor(
                    out=M_all[:, g, :], in0=ident_f[:], scalar=dl[:, g:g + 1], in1=mtmp[:],
                    op0=ALU.mult, op1=ALU.add),
                    reads=[ident_f.b, dl.b], writes=[SU, M_all.b])
            for (Bt, W2) in ((Btr, W2r), (Bti, W2i)):
                ps = getps()
                cx.op("pe", lambda e, ps=ps, r=r, Bt=Bt: e.transpose(
                    out=ps[:, 0:128], in_=Bt[:, r, :, :].rearrange("p k c -> p (k c)"), identity=ident_f[:]),
                    reads=[SU, ident_f.b], writes=[ps.b])
                cx.op("dve", lambda e, ps=ps, r=r, W2=W2: e.tensor_copy(out=W2[:, r, 0, 0:64], in_=ps[:, 0:64]),
                      reads=[ps.b], writes=[SU, W2.b])
                cx.op("dve", lambda e, ps=ps, r=r, W2=W2: e.tensor_copy(out=W2[:, r, 1, 64:128], in_=ps[:, 64:128]),
                      reads=[ps.b], writes=[SU, W2.b])

        cx.barrier()
        al.close()
        al = Alloc(nc)
        NCH = SEQ // 8
        Vg = [al.sb([128, NCH], BF16, f"Vg{i}") for i in range(4)]
        Sre = [al.sb([128, NCH], F32, f"Sre{i}") for i in range(2)]
        Sim = [al.sb([128, NCH], F32, f"Sim{i}") for i in range(2)]
        Sbr = al.sb([128, NCH], BF16, "Sbr"); Sbi = al.sb([128, NCH], BF16, "Sbi")
        Gg = [al.sb([128, NCH], BF16, f"Gg{i}") for i in range(16)]
        ysf = [al.sb([128, SEQ], BF16, f"ysf{i}") for i in range(2)]
        cx.op("pool", lambda e: e.memset(Sbr[:, 0:1], 0.0), writes=[Sbr.b])
        cx.op("pool", lambda e: e.memset(Sbi[:, 0:1], 0.0), writes=[Sbi.b])
        for r in range(16):
            f = r // 4
            vg = [Vg[(2 * r) % 4], Vg[(2 * r + 1) % 4]]
            for two in range(2):
                g = 2 * r + two
                gl = g % 8
                ps = getps()
                for k in range(8):
                    cx.op("pe", lambda e, ps=ps, gl=gl, k=k, f=f: e.matmul(
                        ps[:], lhsT=psel[:, gl, (7 - k) * 16:(7 - k) * 16 + 128],
                        rhs=u_all[:, f, k:SEQ:8], start=(k == 0), stop=(k == 7)),
                        reads=[psel.b, u_all.b], writes=[ps.b])
                cx.op("act", lambda e, ps=ps, v=vg[two]: e.copy(out=v[:], in_=ps[:]),
                      reads=[ps.b], full=[vg[two].b])
            psr = getps(); psi = getps()
            for (pp, W2) in ((psr, W2r), (psi, W2i)):
                for two in range(2):
                    cx.op("pe", lambda e, pp=pp, W2=W2, two=two, r=r, v=vg[two]: e.matmul(
                        pp[:], lhsT=W2[:, r, two, :], rhs=v[:], start=(two == 0), stop=(two == 1)),
                        reads=[W2.b, vg[two].b], writes=[pp.b])
            cx.op("act", lambda e, psr=psr: e.copy(out=Sre[0][:], in_=psr[:]), reads=[psr.b], full=[Sre[0].b])
            cx.op("dve", lambda e, psi=psi: e.tensor_copy(out=Sim[0][:], in_=psi[:]), reads=[psi.b], full=[Sim[0].b])
            cur = 0
            for d_ in range(9):
                sh = 1 << d_
                s_r, s_i, d_r, d_i = Sre[cur], Sim[cur], Sre[1 - cur], Sim[1 - cur]
                n = NCH - sh
                cx.op("dve", lambda e, s_r=s_r, d_r=d_r, sh=sh, n=n, d_=d_, r=r: e.scalar_tensor_tensor(
                    out=d_r[:, sh:NCH], in0=s_r[:, 0:n], scalar=KAr[:, d_, r:r + 1], in1=s_r[:, sh:NCH],
                    op0=ALU.mult, op1=ALU.add), reads=[s_r.b, SU], writes=[d_r.b])
                cx.op("dve", lambda e, s_i=s_i, d_r=d_r, sh=sh, n=n, d_=d_, r=r: e.scalar_tensor_tensor(
                    out=d_r[:, sh:NCH], in0=s_i[:, 0:n], scalar=KnAi[:, d_, r:r + 1], in1=d_r[:, sh:NCH],
                    op0=ALU.mult, op1=ALU.add), reads=[s_i.b, SU], writes=[d_r.b])
                cx.op("dve", lambda e, s_i=s_i, d_i=d_i, sh=sh, n=n, d_=d_, r=r: e.scalar_tensor_tensor(
                    out=d_i[:, sh:NCH], in0=s_i[:, 0:n], scalar=KAr[:, d_, r:r + 1], in1=s_i[:, sh:NCH],
                    op0=ALU.mult, op1=ALU.add), reads=[s_i.b, SU], writes=[d_i.b])
                cx.op("dve", lambda e, s_r=s_r, d_i=d_i, sh=sh, n=n, d_=d_, r=r: e.scalar_tensor_tensor(
                    out=d_i[:, sh:NCH], in0=s_r[:, 0:n], scalar=KAi[:, d_, r:r + 1], in1=d_i[:, sh:NCH],
                    op0=ALU.mult, op1=ALU.add), reads=[s_r.b, SU], writes=[d_i.b])
                cx.op("pool", lambda e, s_r=s_r, d_r=d_r, sh=sh: e.tensor_copy(out=d_r[:, 0:sh], in_=s_r[:, 0:sh]),
                      reads=[s_r.b], writes=[d_r.b])
                cx.op("pool", lambda e, s_i=s_i, d_i=d_i, sh=sh: e.tensor_copy(out=d_i[:, 0:sh], in_=s_i[:, 0:sh]),
                      reads=[s_i.b], writes=[d_i.b])
                cur = 1 - cur
            fr, fi = Sre[cur], Sim[cur]
            cx.op("act", lambda e, fr=fr: e.copy(out=Sbr[:, 1:NCH], in_=fr[:, 0:NCH - 1]),
                  reads=[fr.b], writes=[Sbr.b])
            cx.op("act", lambda e, fi=fi: e.copy(out=Sbi[:, 1:NCH], in_=fi[:, 0:NCH - 1]),
                  reads=[fi.b], writes=[Sbi.b])
            for two in range(2):
                g = 2 * r + two
                rng = slice(two * 64, (two + 1) * 64)
                ps = getps()
                cx.op("pe", lambda e, ps=ps, g=g, v=vg[two]: e.matmul(
                    ps[:], lhsT=M_all[:, g, :], rhs=v[:], start=True, stop=False),
                    reads=[M_all.b, vg[two].b], writes=[ps.b])
                cx.op("pe", lambda e, ps=ps, r=r, rng=rng: e.matmul(
                    ps[:], lhsT=C1r[rng, r, :], rhs=Sbr[rng, :], start=False, stop=False),
                    reads=[SU, Sbr.b], writes=[ps.b])
                cx.op("pe", lambda e, ps=ps, r=r, rng=rng: e.matmul(
                    ps[:], lhsT=nC1i[rng, r, :], rhs=Sbi[rng, :], start=False, stop=True),
                    reads=[SU, Sbi.b], writes=[ps.b])
                gg = Gg[g % 16]
                cx.op("act", lambda e, ps=ps, gg=gg: e.activation(out=gg[:], in_=ps[:], func=GELU),
                      reads=[ps.b], full=[gg.b])
            if r % 4 == 3:
                yb = ysf[f % 2]
                for j in range(8):
                    ps = getps()
                    for gl in range(8):
                        gg = Gg[(8 * f + gl) % 16]
                        cx.op("pe", lambda e, ps=ps, j=j, gl=gl, gg=gg: e.matmul(
                            ps[:], lhsT=psel[:, j, (7 - gl) * 16:(7 - gl) * 16 + 128], rhs=gg[:],
                            start=(gl == 0), stop=(gl == 7)),
                            reads=[psel.b, gg.b], writes=[ps.b])
                    eng = "act" if j % 2 == 0 else "dve"
                    if eng == "act":
                        cx.op("act", lambda e, ps=ps, yb=yb, j=j: e.copy(out=yb[:, j:SEQ:8], in_=ps[:]),
                              reads=[ps.b], writes=[yb.b])
                    else:
                        cx.op("dve", lambda e, ps=ps, yb=yb, j=j: e.tensor_copy(out=yb[:, j:SEQ:8], in_=ps[:]),
                              reads=[ps.b], writes=[yb.b])
                cx.op("sp", lambda e, yb=yb, f=f: e.dma_start(out=ys_scr[f], in_=yb[:]),
                      reads=[yb.b], dma=True)

        cx.barrier()
        al.close()
        alAB.close()
        al = al_outer
        if stop_after in ("A", "B"):
            pass
        else:
            TC = 256
            NBC = SEQ // TC
            alC = Alloc(nc)
            wA = alC.sb([128, 8, 1536], BF16, "wA")
            wG = alC.sb([128, 8, 3072], BF16, "wG")
            wco = alC.sb([128, 4, 1024], BF16, "wco")
            wgl = alC.sb([128, 4, 2048], BF16, "wgl")
            wmo = alC.sb([128, 4, 1024], BF16, "wmo")
            wo = alC.sb([128, 8, 1024], BF16, "wo")
            Dg2 = [alC.sb([128, 31, 128], BF16, f"Dg{i}") for i in range(2)]
            kT = alC.sb([128, 4, 256], BF16, "kT")
            vtok = alC.sb([128, 2, 512], BF16, "vtok")
            gffn = alC.sb([128, D], F32, "gffn")
            wr = alC.sb([128, 8, 36], F32, "wr")
            rbias = alC.sb([128, 36], F32, "rbias")
            cdw = alC.sb([128, 4, 31], F32, "cdw")
            cb = alC.sb([128, 4], F32, "cb"); lng = alC.sb([128, 4], F32, "lng"); lnb = alC.sb([128, 4], F32, "lnb")
            onesm = alC.sb([128, 128], F32, "onesm")
            ones_bf = alC.sb([128, 128], BF16, "ones_bf")
            tri = alC.sb([128, 128], F32, "tri")
            ones_f = alC.sb([128, 128], F32, "ones_f")
            ecap = alC.sb([128, 32], F32, "ecap")
            tokid = alC.sb([128, NT], F32, "tokid")
            cum = alC.sb([128, 32], F32, "cum")
            lg_all = alC.sb([128, NT, 36], F32, "lg_all")
            trashp = alC.sb([128, 1], F32, "trashp")

            def ld(t, src):
                cx.op("sp", lambda e: e.dma_start(out=t[:], in_=src), full=[t.b], dma=True)

            ld(gffn, gffn_d.partition_broadcast(128))
            ld(wr, wr_d.rearrange("(c p) n -> p c n", p=128))
            ld(rbias, rbias_d.partition_broadcast(128))
            ld(cdw, cdw_d); ld(cb, cb_d); ld(lng, lng_d); ld(lnb, lnb_d)
            ld(tri, tri_d); ld(ecap, ecap_d); ld(tokid, tokid_d); ld(trashp, trashp_d)
            cx.op("pool", lambda e: e.memset(onesm[:], 1.0 / 512.0), full=[onesm.b])
            cx.op("pool", lambda e: e.memset(ones_bf[:], 1.0), full=[ones_bf.b])
            cx.op("pool", lambda e: e.memset(ones_f[:], 1.0), full=[ones_f.b])
            cx.op("pool", lambda e: e.memset(cum[:], 0.0), full=[cum.b])
            alS = Alloc(nc)
            zt = alS.sb([128, 1024], F32, "zt")
            cx.op("pool", lambda e: e.memset(zt[:], 0.0), full=[zt.b])
            lstB = Buf("lst"); h2B = Buf("h2scr"); moeB = Buf("moescr"); x2B = Buf("x2scr")
            cx.op("sp", lambda e: e.dma_start(out=lst_d, in_=lst_init_d), full=[lstB], dma=True)
            cx.op("sp", lambda e: e.dma_start(out=h2_scr[SEQ:ROWS, :], in_=zt[:, 0:512].bitcast(BF16)),
                  reads=[zt.b], writes=[h2B], dma=True)
            moe_flat = moe_scr.rearrange("(n p) d -> n p d", p=128)
            for n in range(0, 2 * ROWS // 128):
                cx.op("sp", lambda e, n=n: e.dma_start(out=moe_flat[n], in_=zt[:]),
                      reads=[zt.b], writes=[moeB], dma=True)

            stg2 = [alS.sb([128, 8, 256], F32, f"stgc{i}") for i in range(2)]
            s2 = [0]

            def load_cast2(dst, dst_col0, src_d, c0, c1, kch, engs=("pool", "act")):
                for cc in range(c0, c1, 256):
                    w = min(256, c1 - cc)
                    s = stg2[s2[0] % 2]
                    eng = engs[s2[0] % len(engs)]
                    s2[0] += 1
                    src = src_d[:, cc:cc + w].rearrange("(c p) n -> p c n", p=128)
                    cx.op("sp", lambda e, s=s, src=src, w=w: e.dma_start(out=s[:, 0:kch, 0:w], in_=src),
                          full=[s.b], dma=True)
                    o = dst_col0 + (cc - c0)
                    if eng == "act":
                        cx.op("act", lambda e, s=s, o=o, w=w: e.copy(out=dst[:, 0:kch, o:o + w], in_=s[:, 0:kch, 0:w]),
                              reads=[s.b], writes=[dst.b])
                    else:
                        cx.op(eng, lambda e, s=s, o=o, w=w: e.tensor_copy(out=dst[:, 0:kch, o:o + w],
                                                                          in_=s[:, 0:kch, 0:w]),
                              reads=[s.b], writes=[dst.b])

            load_cast2(wA, 0, w_in_d, 0, 1024, 8)
            load_cast2(wA, 1024, w_in_d, 1536, 2048, 8)
            load_cast2(wG, 0, w_in_d, 2048, 5120, 8)
            load_cast2(wco, 0, wco_d, 0, 1024, 4)
            load_cast2(wgl, 0, wgl_d, 0, 2048, 4)
            load_cast2(wmo, 0, wmo_d, 0, 1024, 4)
            load_cast2(wo, 0, wo_d, 0, 1024, 8)
            wkv = alS.sb([128, 8, 1024], BF16, "wkv")
            load_cast2(wkv, 0, wkv_d, 0, 1024, 8)
            gmem = alS.sb([128, D], F32, "gmem")
            ld(gmem, gmem_d.partition_broadcast(128))
            memT = alS.sb([128, 8, 256], BF16, "memT")
            mx = alS.sb([128, D], F32, "mx"); mjunk = alS.sb([128, D], BF16, "mjunk")
            mss = alS.sb([128, 1], F32, "mss"); mrt = alS.sb([128, 1], F32, "mrt"); mrs = alS.sb([128, 1], F32, "mrs")
            mh = alS.sb([128, D], BF16, "mh")
            for mt in range(2):
                cx.op("sp", lambda e, mt=mt: e.dma_start(out=mx[:], in_=mem_d[mt * 128:(mt + 1) * 128, :]),
                      full=[mx.b], dma=True)
                cx.op("act", lambda e: e.activation(out=mjunk[:], in_=mx[:], func=AF.Square, accum_out=mss[:]),
                      reads=[mx.b], full=[mjunk.b, mss.b])
                cx.op("act", lambda e: e.activation(out=mrt[:], in_=mss[:], func=AF.Sqrt, scale=1.0 / D, bias=EPS),
                      reads=[mss.b], full=[mrt.b])
                cx.op("dve", lambda e: e.reciprocal(out=mrs[:], in_=mrt[:]), reads=[mrt.b], full=[mrs.b])
                cx.op("dve", lambda e: e.scalar_tensor_tensor(out=mh[:], in0=mx[:], scalar=mrs[:, 0:1], in1=gmem[:],
                                                              op0=ALU.mult, op1=ALU.mult),
                      reads=[mx.b, mrs.b, gmem.b], full=[mh.b])
                pb = psb[mt % 2]
                for c in range(8):
                    cx.op("pe", lambda e, pb=pb, c=c: e.transpose(out=pb[:, c * 128:(c + 1) * 128],
                                                                  in_=mh[:, c * 128:(c + 1) * 128],
                                                                  identity=ident_bf[:]),
                          reads=[mh.b, ident_bf.b], writes=[pb.b])
                cx.op("act", lambda e, pb=pb, mt=mt: e.copy(out=memT[:, :, mt * 128:(mt + 1) * 128],
                                                            in_=pb[:].rearrange("p (c t) -> p c t", c=8)),
                      reads=[pb.b], writes=[memT.b])
            for hd in range(4):
                ps = getps()
                for c in range(8):
                    cx.op("pe", lambda e, ps=ps, c=c, hd=hd: e.matmul(
                        ps[:, 0:256], lhsT=wkv[:, c, hd * 128:(hd + 1) * 128], rhs=memT[:, c, :],
                        start=(c == 0), stop=(c == 7)), reads=[wkv.b, memT.b], writes=[ps.b])
                cx.op("dve", lambda e, ps=ps, hd=hd: e.tensor_copy(out=kT[:, hd, :], in_=ps[:, 0:256]),
                      reads=[ps.b], writes=[kT.b])
            for mc in range(2):
                ps = getps()
                for c in range(8):
                    cx.op("pe", lambda e, ps=ps, c=c, mc=mc: e.matmul(
                        ps[:], lhsT=memT[:, c, mc * 128:(mc + 1) * 128], rhs=wkv[:, c, 512:1024],
                        start=(c == 0), stop=(c == 7)), reads=[wkv.b, memT.b], writes=[ps.b])
                cx.op("dve", lambda e, ps=ps, mc=mc: e.tensor_copy(out=vtok[:, mc, :], in_=ps[:]),
                      reads=[ps.b], writes=[vtok.b])
            cx.barrier()
            alS.close()

            alW = Alloc(nc)
            hT = [alW.sb([128, 8, TC], BF16, "hTc0")] * 2
            ysb = [alW.sb([128, 4, TC], BF16, "ysb0")] * 2
            vbuf = alW.sb([128, 4, 30 + TC], BF16, "vbuf")
            sgt = [alW.sb([128, TC], F32, f"sgt{i}") for i in range(3)]
            cv = alW.sb([128, 4, TC], F32, "cv"); sq = alW.sb([128, 2, TC], F32, "sq")
            mean = alW.sb([128, TC], F32, "mean"); m2 = alW.sb([128, TC], F32, "m2")
            var = alW.sb([128, TC], F32, "var"); lnv = alW.sb([128, TC], F32, "lnv"); lrs = alW.sb([128, TC], F32, "lrs")
            xc = [alW.sb([128, TC], F32, f"xc{i}") for i in range(2)]
            cn = alW.sb([128, 4, TC], BF16, "cn")
            qb = alW.sb([128, 4, TC], BF16, "qb")
            Eb = [alW.sb([128, 2, TC], BF16, f"Eb{i}") for i in range(2)]
            rden = alW.sb([128, TC], F32, "rden")
            ob = alW.sb([128, 4, TC], BF16, "ob")
            macc = alW.sb([128, TC], F32, "macc"); mt1 = alW.sb([128, TC], F32, "mt1"); mt2 = alW.sb([128, TC], F32, "mt2")
            merged = alW.sb([128, 8, TC], BF16, "merged")
            xt2 = [alW.sb([128, D], F32, "xtc0")] * 2
            x2t = xt2
            h2f = alW.sb([128, D], F32, "h2f"); h2b = [alW.sb([128, D], BF16, "h2b0")] * 2
            junk2 = h2b[0]
            h2T = alW.sb([128, 8, 128], F32, "h2T")
            ss2 = alW.sb([128, 1], F32, "ss2"); rt2 = alW.sb([128, 1], F32, "rt2"); rs2 = alW.sb([128, 1], F32, "rs2")
            cx.op("pool", lambda e: e.memset(vbuf[:], 0.0), full=[vbuf.b])

            breg = {}

            def mmgrp(ps_ap, ps_b, pairs, reads):
                n = len(pairs)
                for idx, (l, r_) in enumerate(pairs):
                    cx.op("pe", lambda e, l=l, r_=r_, idx=idx: e.matmul(ps_ap, lhsT=l, rhs=r_, start=(idx == 0),
                                                                         stop=(idx == n - 1)),
                          reads=reads, writes=[ps_b])

            KCUT = int(os.environ.get("KCUT", "9"))
            KNB = int(os.environ.get("KNB", str(NBC)))
            for bi in range(NBC if KCUT >= 2 else 0):
                if bi >= KNB:
                    break
                t0 = bi * TC
                h = hT[bi % 2]; yb = ysb[bi % 2]
                cx.op("sp", lambda e, h=h, t0=t0: e.dma_start(
                    out=h[:], in_=hT_scr[:, :, t0:t0 + TC].rearrange("c p t -> p c t")), full=[h.b], dma=True)
                cx.op("sp", lambda e, yb=yb, t0=t0: e.dma_start(
                    out=yb[:], in_=ys_scr[:, :, t0:t0 + TC].rearrange("f p t -> p f t")), full=[yb.b], dma=True)
                for f in range(4):
                    pa = getps(); pg = getps()
                    mmgrp(pa[:, 0:TC], pa.b, [(wA[:, c, f * 128:(f + 1) * 128], h[:, c, :]) for c in range(8)],
                          [wA.b, h.b])
                    mmgrp(pg[:, 0:TC], pg.b, [(wA[:, c, 512 + f * 128:512 + (f + 1) * 128], h[:, c, :]) for c in range(8)],
                          [wA.b, h.b])
                    s = sgt[f % 3]
                    cx.op("act", lambda e, pg=pg, s=s: e.activation(out=s[:], in_=pg[:, 0:TC], func=AF.Sigmoid),
                          reads=[pg.b], full=[s.b])
                    cx.op("dve", lambda e, pa=pa, s=s, f=f: e.tensor_tensor(out=vbuf[:, f, 30:30 + TC], in0=pa[:, 0:TC],
                                                                            in1=s[:], op=ALU.mult),
                          reads=[pa.b, s.b], writes=[vbuf.b])
                for f in range(4):
                    pc = getps()
                    Dg = Dg2[f % 2]
                    for k in range(31):
                        cx.op("pool", lambda e, Dg=Dg, f=f, k=k: e.tensor_scalar(
                            out=Dg[:, k, :], in0=ident_f[:], scalar1=cdw[:, f, k:k + 1], scalar2=1.0,
                            op0=ALU.mult, op1=ALU.mult), reads=[ident_f.b, cdw.b], writes=[Dg.b])
                    mmgrp(pc[:, 0:TC], pc.b, [(Dg[:, k, :], vbuf[:, f, k:k + TC]) for k in range(31)],
                          [Dg.b, vbuf.b])
                    cx.op("act", lambda e, pc=pc, f=f: e.activation(out=cv[:, f, :], in_=pc[:, 0:TC], func=AF.Identity,
                                                                    bias=cb[:, f:f + 1], scale=1.0),
                          reads=[pc.b, cb.b], writes=[cv.b])
                cx.op("pool", lambda e: e.tensor_copy(out=vbuf[:, :, 0:30], in_=vbuf[:, :, TC:TC + 30]),
                      reads=[vbuf.b], writes=[vbuf.b])
                pm = getps(); pq = getps()
                mmgrp(pm[:, 0:TC], pm.b, [(onesm[:], cv[:, f, :]) for f in range(4)], [onesm.b, cv.b])
                sqb = [Buf("sq0"), Buf("sq1")]
                for f in range(4):
                    cx.op("act", lambda e, f=f: e.activation(out=sq[:, f % 2, :], in_=cv[:, f, :], func=AF.Square),
                          reads=[cv.b], full=[sqb[f % 2]])
                    cx.op("pe", lambda e, pq=pq, f=f: e.matmul(pq[:, 0:TC], lhsT=onesm[:], rhs=sq[:, f % 2, :],
                                                               start=(f == 0), stop=(f == 3)),
                          reads=[onesm.b, sqb[f % 2]], writes=[pq.b])
                cx.op("act", lambda e, pm=pm: e.copy(out=mean[:], in_=pm[:, 0:TC]), reads=[pm.b], full=[mean.b])
                cx.op("dve", lambda e: e.tensor_tensor(out=m2[:], in0=mean[:], in1=mean[:], op=ALU.mult),
                      reads=[mean.b], full=[m2.b])
                cx.op("dve", lambda e, pq=pq: e.tensor_tensor(out=var[:], in0=pq[:, 0:TC], in1=m2[:], op=ALU.subtract),
                      reads=[pq.b, m2.b], full=[var.b])
                cx.op("dve", lambda e: e.tensor_scalar(out=var[:], in0=var[:], scalar1=float(EPS), scalar2=None,
                                                       op0=ALU.add), reads=[var.b], writes=[var.b])
                cx.op("act", lambda e: e.activation(out=lnv[:], in_=var[:], func=AF.Ln), reads=[var.b], full=[lnv.b])
                cx.op("act", lambda e: e.activation(out=lrs[:], in_=lnv[:], func=AF.Exp, scale=-0.5),
                      reads=[lnv.b], full=[lrs.b])
                for f in range(4):
                    x_ = xc[f % 2]
                    cx.op("dve", lambda e, x_=x_, f=f: e.tensor_tensor(out=x_[:], in0=cv[:, f, :], in1=mean[:],
                                                                       op=ALU.subtract),
                          reads=[cv.b, mean.b], full=[x_.b])
                    cx.op("dve", lambda e, x_=x_: e.tensor_tensor(out=x_[:], in0=x_[:], in1=lrs[:], op=ALU.mult),
                          reads=[lrs.b], writes=[x_.b])
                    cx.op("act", lambda e, x_=x_, f=f: e.activation(out=cn[:, f, :], in_=x_[:], func=AF.Silu,
                                                                    bias=lnb[:, f:f + 1], scale=lng[:, f:f + 1]),
                          reads=[x_.b, lnb.b, lng.b], writes=[cn.b])
                for hd in range(4):
                    pq_ = getps()
                    mmgrp(pq_[:, 0:TC], pq_.b, [(wA[:, c, 1024 + hd * 128:1024 + (hd + 1) * 128], h[:, c, :])
                                                for c in range(8)], [wA.b, h.b])
                    cx.op("act", lambda e, pq_=pq_, hd=hd: e.copy(out=qb[:, hd, :], in_=pq_[:, 0:TC]),
                          reads=[pq_.b], writes=[qb.b])
                for hd in range(4):
                    E = Eb[hd % 2]
                    for mc in range(2):
                        psc = getps()
                        mmgrp(psc[:, 0:TC], psc.b, [(kT[:, hd, mc * 128:(mc + 1) * 128], qb[:, hd, :])], [kT.b, qb.b])
                        cx.op("act", lambda e, psc=psc, E=E, mc=mc: e.activation(
                            out=E[:, mc, :], in_=psc[:, 0:TC], func=AF.Exp, scale=float(128 ** -0.5)),
                            reads=[psc.b], writes=[E.b])
                    po = getps(); pd = getps()
                    mmgrp(po[:, 0:TC], po.b, [(vtok[:, mc, hd * 128:(hd + 1) * 128], E[:, mc, :]) for mc in range(2)],
                          [vtok.b, E.b])
                    mmgrp(pd[:, 0:TC], pd.b, [(ones_bf[:], E[:, mc, :]) for mc in range(2)], [ones_bf.b, E.b])
                    cx.op("dve", lambda e, pd=pd: e.reciprocal(out=rden[:], in_=pd[:, 0:TC]), reads=[pd.b], full=[rden.b])
                    cx.op("dve", lambda e, po=po, hd=hd: e.tensor_tensor(out=ob[:, hd, :], in0=po[:, 0:TC], in1=rden[:],
                                                                         op=ALU.mult),
                          reads=[po.b, rden.b], writes=[ob.b])
                for j in range(8):
                    js = slice(j * 128, (j + 1) * 128)
                    pga = getps(); pyc = getps()
                    mmgrp(pga[:, 0:TC], pga.b, [(wG[:, c, j * 128:(j + 1) * 128], h[:, c, :]) for c in range(8)], [wG.b, h.b])
                    mmgrp(pyc[:, 0:TC], pyc.b, [(wco[:, f, js], cn[:, f, :]) for f in range(4)], [wco.b, cn.b])
                    s = sgt[0]
                    cx.op("act", lambda e, pga=pga, s=s: e.activation(out=s[:], in_=pga[:, 0:TC], func=AF.Sigmoid),
                          reads=[pga.b], full=[s.b])
                    cx.op("dve", lambda e, pyc=pyc, s=s: e.tensor_tensor(out=macc[:], in0=pyc[:, 0:TC], in1=s[:], op=ALU.mult),
                          reads=[pyc.b, s.b], full=[macc.b])
                    pgb = getps(); pza = getps(); pzb = getps()
                    mmgrp(pgb[:, 0:TC], pgb.b, [(wG[:, c, 1024 + j * 128:1024 + (j + 1) * 128], h[:, c, :]) for c in range(8)],
                          [wG.b, h.b])
                    mmgrp(pza[:, 0:TC], pza.b, [(wgl[:, f, js], yb[:, f, :]) for f in range(4)], [wgl.b, yb.b])
                    mmgrp(pzb[:, 0:TC], pzb.b, [(wgl[:, f, 1024 + j * 128:1024 + (j + 1) * 128], yb[:, f, :]) for f in range(4)],
                          [wgl.b, yb.b])
                    sb_ = sgt[1]; sz = sgt[2]
                    cx.op("act", lambda e, pgb=pgb, sb_=sb_: e.activation(out=sb_[:], in_=pgb[:, 0:TC], func=AF.Sigmoid),
                          reads=[pgb.b], full=[sb_.b])
                    cx.op("act", lambda e, pzb=pzb, sz=sz: e.activation(out=sz[:], in_=pzb[:, 0:TC], func=AF.Sigmoid),
                          reads=[pzb.b], full=[sz.b])
                    cx.op("dve", lambda e, pza=pza, sz=sz: e.tensor_tensor(out=mt1[:], in0=pza[:, 0:TC], in1=sz[:], op=ALU.mult),
                          reads=[pza.b, sz.b], full=[mt1.b])
                    cx.op("dve", lambda e, sb_=sb_: e.tensor_tensor(out=mt1[:], in0=mt1[:], in1=sb_[:], op=ALU.mult),
                          reads=[sb_.b], writes=[mt1.b])
                    cx.op("dve", lambda e: e.tensor_tensor(out=macc[:], in0=macc[:], in1=mt1[:], op=ALU.add),
                          reads=[mt1.b], writes=[macc.b])
                    pgc = getps(); pym = getps()
                    mmgrp(pgc[:, 0:TC], pgc.b, [(wG[:, c, 2048 + j * 128:2048 + (j + 1) * 128], h[:, c, :]) for c in range(8)],
                          [wG.b, h.b])
                    mmgrp(pym[:, 0:TC], pym.b, [(wmo[:, hd, js], ob[:, hd, :]) for hd in range(4)], [wmo.b, ob.b])
                    s = sgt[0]
                    cx.op("act", lambda e, pgc=pgc, s=s: e.activation(out=s[:], in_=pgc[:, 0:TC], func=AF.Sigmoid),
                          reads=[pgc.b], full=[s.b])
                    cx.op("dve", lambda e, pym=pym, s=s: e.tensor_tensor(out=mt2[:], in0=pym[:, 0:TC], in1=s[:], op=ALU.mult),
                          reads=[pym.b, s.b], full=[mt2.b])
                    cx.op("dve", lambda e, j=j: e.tensor_tensor(out=merged[:, j, :], in0=macc[:], in1=mt2[:], op=ALU.add),
                          reads=[macc.b, mt2.b], writes=[merged.b])
                for tt in range(TC // 128):
                    ti = bi * (TC // 128) + tt
                    pp = ti % 2
                    xt_ = xt2[pp]; x2 = x2t[pp]; hb2 = h2b[pp]
                    cx.op("sp", lambda e, xt_=xt_, ti=ti: e.dma_start(out=xt_[:], in_=x_d[ti * 128:(ti + 1) * 128, :]),
                          full=[xt_.b], dma=True)
                    for half in range(2):
                        po_ = getps()
                        mmgrp(po_[:], po_.b, [(merged[:, j, tt * 128:(tt + 1) * 128], wo[:, j, half * 512:(half + 1) * 512])
                                              for j in range(8)], [merged.b, wo.b])
                        cx.op("dve", lambda e, po_=po_, x2=x2, xt_=xt_, half=half: e.tensor_tensor(
                            out=x2[:, half * 512:(half + 1) * 512], in0=po_[:], in1=xt_[:, half * 512:(half + 1) * 512],
                            op=ALU.add), reads=[po_.b, xt_.b], writes=[x2.b])
                    cx.op("sp", lambda e, x2=x2, ti=ti: e.dma_start(out=x2_scr[ti * 128:(ti + 1) * 128, :], in_=x2[:]),
                          reads=[x2.b], writes=[x2B], dma=True)
                    cx.op("act", lambda e, x2=x2: e.activation(out=junk2[:], in_=x2[:], func=AF.Square, accum_out=ss2[:]),
                          reads=[x2.b], full=[junk2.b, ss2.b])
                    cx.op("act", lambda e: e.activation(out=rt2[:], in_=ss2[:], func=AF.Sqrt, scale=1.0 / D, bias=EPS),
                          reads=[ss2.b], full=[rt2.b])
                    cx.op("dve", lambda e: e.reciprocal(out=rs2[:], in_=rt2[:]), reads=[rt2.b], full=[rs2.b])
                    cx.op("dve", lambda e, x2=x2: e.scalar_tensor_tensor(out=h2f[:], in0=x2[:], scalar=rs2[:, 0:1],
                                                                         in1=gffn[:], op0=ALU.mult, op1=ALU.mult),
                          reads=[x2.b, rs2.b, gffn.b], full=[h2f.b])
                    cx.op("act", lambda e, hb2=hb2: e.copy(out=hb2[:], in_=h2f[:]), reads=[h2f.b], full=[hb2.b])
                    cx.op("sp", lambda e, hb2=hb2, ti=ti: e.dma_start(out=h2_scr[ti * 128:(ti + 1) * 128, :], in_=hb2[:]),
                          reads=[hb2.b], writes=[h2B], dma=True)
                    pra = getps(); prb = getps()
                    for c in range(8):
                        pr = pra if c < 4 else prb
                        cx.op("pe", lambda e, pr=pr, c=c: e.transpose(out=pr[:, (c % 4) * 128:(c % 4 + 1) * 128],
                                                                      in_=h2f[:, c * 128:(c + 1) * 128], identity=ident_f[:]),
                              reads=[h2f.b, ident_f.b], writes=[pr.b])
                    cx.op("act", lambda e, pra=pra: e.copy(out=h2T[:, 0:4, :], in_=pra[:].rearrange("p (c t) -> p c t", c=4)),
                          reads=[pra.b], writes=[h2T.b])
                    cx.op("dve", lambda e, prb=prb: e.tensor_copy(out=h2T[:, 4:8, :],
                                                                  in_=prb[:].rearrange("p (c t) -> p c t", c=4)),
                          reads=[prb.b], writes=[h2T.b])
                    plg = getps()
                    mmgrp(plg[:, 0:36], plg.b, [(h2T[:, c, :], wr[:, c, :]) for c in range(8)], [h2T.b, wr.b])

                    cx.op("dve", lambda e, plg=plg, ti=ti: e.tensor_tensor(out=lg_all[:, ti, :], in0=plg[:, 0:36],
                                                                        in1=rbias[:], op=ALU.add),
                          reads=[plg.b, rbias.b], writes=[lg_all.b])
            cx.barrier()
            alW.close()
            alR = Alloc(nc)
            RS = Buf("route")

            def rd(fn, extra_reads=(), extra_writes=()):
                cx.op("dve", fn, reads=[RS, lg_all.b] + list(extra_reads), writes=[RS] + list(extra_writes))

            def R(shape, name, dt=F32):
                return alR.sb(shape, dt, name)

            NTT = NT
            NEB_ = NEXP * NBLK
            gmax = R([128, NTT], "gmax"); ohg = R([128, NTT, 4], "ohg"); eg = R([128, NTT, 4], "eg")
            sumg = R([128, NTT], "sumg"); ptop = R([128, NTT], "ptop")
            selm = R([128, NTT, 4, 8], "selm"); sel = R([128, NTT, 8], "sel"); sel2 = R([128, NTT, 8], "sel2")
            m1_ = R([128, NTT], "m1_"); m2_ = R([128, NTT], "m2_"); oh1 = R([128, NTT, 8], "oh1"); oh2 = R([128, NTT, 8], "oh2")
            dm = R([128, NTT], "dm"); w1 = R([128, NTT], "w1"); w2 = R([128, NTT], "w2")
            M1 = R([128, NTT, 4, 8], "M1"); M2 = R([128, NTT, 4, 8], "M2"); Mc = R([128, NTT, 32], "Mc")
            Cex = R([128, NTT, 32], "Cex"); pos = R([128, NTT, 32], "pos"); bk = R([128, NTT, 32], "bk")
            sf = R([128, NTT, 32], "sf"); ov = R([128, NTT, 32], "ov"); tq = R([128, NTT, 32], "tq")
            sk = [R([128, NTT], f"sk{k}") for k in range(2)]; okk = R([128, NTT], "okk"); dd = R([128, NTT], "dd")
            si = [R([128, NTT], f"si{k}", I32) for k in range(2)]
            ent = [R([128, NTT, 4], f"ent{k}") for k in range(2)]
            le4 = lg_all[:, :, 4:36].rearrange("p t (g j) -> p t g j", g=4)

            def bc3(a, n):
                return a.unsqueeze(2).to_broadcast([128, NTT, n])

            rd(lambda e: e.tensor_reduce(out=gmax[:], in_=lg_all[:, :, 0:4], axis=AX.X, op=ALU.max))
            rd(lambda e: e.tensor_tensor(out=ohg[:], in0=lg_all[:, :, 0:4], in1=bc3(gmax[:], 4), op=ALU.is_equal))
            rd(lambda e: e.tensor_tensor(out=eg[:], in0=lg_all[:, :, 0:4], in1=bc3(gmax[:], 4), op=ALU.subtract))
            cx.op("act", lambda e: e.activation(out=eg[:], in_=eg[:], func=AF.Exp), reads=[RS], writes=[RS])
            rd(lambda e: e.tensor_reduce(out=sumg[:], in_=eg[:], axis=AX.X, op=ALU.add))
            rd(lambda e: e.reciprocal(out=ptop[:], in_=sumg[:]))
            rd(lambda e: e.tensor_tensor(out=selm[:], in0=le4,
                                         in1=ohg[:].unsqueeze(3).to_broadcast([128, NTT, 4, 8]), op=ALU.mult))
            rd(lambda e: e.tensor_reduce(out=sel[:], in_=selm[:].rearrange("p t g j -> p t j g"), axis=AX.X, op=ALU.add))
            rd(lambda e: e.tensor_reduce(out=m1_[:], in_=sel[:], axis=AX.X, op=ALU.max))
            rd(lambda e: e.tensor_tensor(out=oh1[:], in0=sel[:], in1=bc3(m1_[:], 8), op=ALU.is_equal))
            rd(lambda e: e.scalar_tensor_tensor(out=sel2[:], in0=oh1[:], scalar=-1e30, in1=sel[:], op0=ALU.mult, op1=ALU.add))
            rd(lambda e: e.tensor_reduce(out=m2_[:], in_=sel2[:], axis=AX.X, op=ALU.max))
            rd(lambda e: e.tensor_tensor(out=oh2[:], in0=sel2[:], in1=bc3(m2_[:], 8), op=ALU.is_equal))
            rd(lambda e: e.tensor_tensor(out=dm[:], in0=m1_[:], in1=m2_[:], op=ALU.subtract))
            cx.op("act", lambda e: e.activation(out=w1[:], in_=dm[:], func=AF.Sigmoid), reads=[RS], writes=[RS])
            rd(lambda e: e.tensor_tensor(out=w1[:], in0=w1[:], in1=ptop[:], op=ALU.mult))
            rd(lambda e: e.tensor_tensor(out=w2[:], in0=ptop[:], in1=w1[:], op=ALU.subtract))
            rd(lambda e: e.tensor_tensor(out=M1[:], in0=ohg[:].unsqueeze(3).to_broadcast([128, NTT, 4, 8]),
                                         in1=oh1[:].unsqueeze(2).to_broadcast([128, NTT, 4, 8]), op=ALU.mult))
            rd(lambda e: e.tensor_tensor(out=M2[:], in0=ohg[:].unsqueeze(3).to_broadcast([128, NTT, 4, 8]),
                                         in1=oh2[:].unsqueeze(2).to_broadcast([128, NTT, 4, 8]), op=ALU.mult))
            rd(lambda e: e.tensor_tensor(out=Mc[:], in0=M1[:].rearrange("p t g j -> p t (g j)"),
                                         in1=M2[:].rearrange("p t g j -> p t (g j)"), op=ALU.add))
            rd(lambda e: e.memset(Cex[:, 0, :], 0.0))
            for i in range(1, NTT):
                rd(lambda e, i=i: e.tensor_tensor(out=Cex[:, i, :], in0=Cex[:, i - 1, :], in1=Mc[:, i - 1, :], op=ALU.add))
            pp = [getps(), getps()]
            for i in range(NTT):
                pb_ = pp[i // 16]
                o_ = pb_[:, (i % 16) * 32:(i % 16 + 1) * 32]
                cx.op("pe", lambda e, o_=o_, i=i: e.matmul(o_, lhsT=tri[:], rhs=Mc[:, i, :], start=True, stop=False),
                      reads=[tri.b, RS], writes=[pb_.b])
                cx.op("pe", lambda e, o_=o_, i=i: e.matmul(o_, lhsT=ones_f[:], rhs=Cex[:, i, :], start=False, stop=True),
                      reads=[ones_f.b, RS], writes=[pb_.b])
            for hh in range(2):
                rd(lambda e, hh=hh: e.tensor_copy(out=pos[:, hh * 16:(hh + 1) * 16, :],
                                                  in_=pp[hh][:].rearrange("p (t x) -> p t x", t=16)), [pp[hh].b])
            rd(lambda e: e.tensor_single_scalar(out=bk[:], in_=pos[:], scalar=127.5, op=ALU.is_gt))
            for thr in range(2, NBLK):
                rd(lambda e, thr=thr: e.tensor_single_scalar(out=tq[:], in_=pos[:], scalar=128.0 * thr - 0.5, op=ALU.is_gt))
                rd(lambda e: e.tensor_tensor(out=bk[:], in0=bk[:], in1=tq[:], op=ALU.add))
            rd(lambda e: e.scalar_tensor_tensor(out=bk[:], in0=bk[:], scalar=float(1 - 128 * NEB_),
                                                in1=ecap[:].unsqueeze(1).to_broadcast([128, NTT, 32]),
                                                op0=ALU.mult, op1=ALU.add), [ecap.b])
            rd(lambda e: e.scalar_tensor_tensor(out=sf[:], in0=pos[:], scalar=float(NEB_), in1=bk[:],
                                                op0=ALU.mult, op1=ALU.add))
            rd(lambda e: e.tensor_single_scalar(out=ov[:], in_=pos[:], scalar=float(CAP) - 0.5, op=ALU.is_gt))
            for k, (Mk, wk) in enumerate(((M1, w1), (M2, w2))):
                Mk32 = Mk[:].rearrange("p t g j -> p t (g j)")
                rd(lambda e, Mk32=Mk32: e.tensor_tensor(out=tq[:], in0=Mk32, in1=sf[:], op=ALU.mult))
                rd(lambda e, k=k: e.tensor_reduce(out=sk[k][:], in_=tq[:], axis=AX.X, op=ALU.add))
                rd(lambda e, Mk32=Mk32: e.tensor_tensor(out=tq[:], in0=Mk32, in1=ov[:], op=ALU.mult))
                rd(lambda e: e.tensor_reduce(out=okk[:], in_=tq[:], axis=AX.X, op=ALU.add))
                rd(lambda e, k=k: e.tensor_scalar(out=dd[:], in0=sk[k][:], scalar1=trashp[:, 0:1], scalar2=None,
                                                  op0=ALU.subtract), [trashp.b])
                rd(lambda e: e.tensor_tensor(out=dd[:], in0=dd[:], in1=okk[:], op=ALU.mult))
                rd(lambda e, k=k: e.tensor_tensor(out=sk[k][:], in0=sk[k][:], in1=dd[:], op=ALU.subtract))
                rd(lambda e, k=k: e.tensor_copy(out=si[k][:], in_=sk[k][:]), (), [si[k].b])
                rd(lambda e, k=k: e.memset(ent[k][:], 0.0), (), [ent[k].b])
                rd(lambda e, k=k: e.tensor_copy(out=ent[k][:, :, 0], in_=tokid[:]), [tokid.b], [ent[k].b])
                rd(lambda e, k=k: e.tensor_scalar(out=ent[k][:, :, 1], in0=tokid[:], scalar1=float(k * ROWS), scalar2=None,
                                                  op0=ALU.add), [tokid.b], [ent[k].b])
                rd(lambda e, k=k, wk=wk: e.tensor_copy(out=ent[k][:, :, 2], in_=wk[:]), (), [ent[k].b])
            for i in range(NTT):
                for k in range(2):
                    cx.op("pool", lambda e, i=i, k=k: e.indirect_dma_start(
                        out=lst_d, out_offset=bass.IndirectOffsetOnAxis(ap=si[k][:, i:i + 1], axis=0),
                        in_=ent[k][:, i, :], in_offset=None),
                        reads=[si[k].b, ent[k].b], writes=[lstB], dma=True)
            cx.barrier()
            alR.close()
            alC.close()
        if stop_after in ("A", "B", "C"):
            pass
        else:
            alD = Alloc(nc)
            NEB = NEXP * NBLK
            lst_sb = alD.sb([128, NEB, 4], F32, "lst_sb")
            idx_i = alD.sb([128, NEB], I32, "idx_i")
            dst_i = alD.sb([128, NEB], I32, "dst_i")
            cx.op("sp", lambda e: e.dma_start(out=lst_sb[:], in_=lst_d[0:NEXP * CAP, :].rearrange("(s eb) w -> s eb w", s=128)),
                  reads=[lstB], full=[lst_sb.b], dma=True)
            cx.op("dve", lambda e: e.tensor_copy(out=idx_i[:], in_=lst_sb[:, :, 0]), reads=[lst_sb.b], full=[idx_i.b])
            cx.op("dve", lambda e: e.tensor_copy(out=dst_i[:], in_=lst_sb[:, :, 1]), reads=[lst_sb.b], full=[dst_i.b])
            sg_ = [alD.sb([128, 8, 256], F32, f"sg{i}") for i in range(2)]
            su_ = [alD.sb([128, 8, 256], F32, f"su{i}") for i in range(2)]
            sd_ = [alD.sb([128, 2, 1024], F32, f"sd{i}") for i in range(2)]
            Wg = [alD.sb([128, 8, 256], BF16, f"Wg{i}") for i in range(2)]
            Wu = [alD.sb([128, 8, 256], BF16, f"Wu{i}") for i in range(2)]
            Wd = [alD.sb([128, 2, 1024], BF16, f"Wd{i}") for i in range(2)]
            Gt = [alD.sb([128, D], BF16, f"Gt{i}") for i in range(3)]
            Xe = [alD.sb([128, 8, CAP], BF16, f"Xe{i}") for i in range(2)]
            sgl = [alD.sb([128, CAP], F32, f"sgl{i}") for i in range(2)]
            ae = [alD.sb([128, 2, CAP], BF16, f"ae{i}") for i in range(2)]
            Yt = [alD.sb([128, D], F32, f"Yt{i}") for i in range(3)]

            def load_w(e_):
                p = e_ % 2
                cx.op("sp", lambda e: e.dma_start(out=sg_[p][:], in_=weg_d[e_].rearrange("(c p) n -> p c n", p=128)),
                      full=[sg_[p].b], dma=True)
                cx.op("sp", lambda e: e.dma_start(out=su_[p][:], in_=weu_d[e_].rearrange("(c p) n -> p c n", p=128)),
                      full=[su_[p].b], dma=True)
                cx.op("sp", lambda e: e.dma_start(out=sd_[p][:], in_=wed_d[e_].rearrange("(c p) n -> p c n", p=128)),
                      full=[sd_[p].b], dma=True)
                cx.op("pool", lambda e: e.tensor_copy(out=Wg[p][:], in_=sg_[p][:]), reads=[sg_[p].b], full=[Wg[p].b])
                cx.op("pool", lambda e: e.tensor_copy(out=Wu[p][:], in_=su_[p][:]), reads=[su_[p].b], full=[Wu[p].b])
                cx.op("act", lambda e: e.copy(out=Wd[p][:], in_=sd_[p][:]), reads=[sd_[p].b], full=[Wd[p].b])

            gi = [0]
            load_w(0)
            KNE = int(os.environ.get("KNE", str(NEXP)))
            for e_ in range(KNE):
                p = e_ % 2
                if e_ + 1 < NEXP:
                    load_w(e_ + 1)
                X = Xe[p]
                for blk in range(NBLK):
                    eb = e_ * NBLK + blk
                    G = Gt[gi[0] % 3]
                    pbk = psb[gi[0] % 2]
                    gi[0] += 1
                    cx.op("pool", lambda e, G=G, eb=eb: e.indirect_dma_start(
                        out=G[:], out_offset=None, in_=h2_scr,
                        in_offset=bass.IndirectOffsetOnAxis(ap=idx_i[:, eb:eb + 1], axis=0)),
                        reads=[idx_i.b, h2B], full=[G.b], dma=True)
                    for c in range(8):
                        cx.op("pe", lambda e, pbk=pbk, G=G, c=c: e.transpose(
                            out=pbk[:, c * 128:(c + 1) * 128], in_=G[:, c * 128:(c + 1) * 128], identity=ident_bf[:]),
                            reads=[G.b, ident_bf.b], writes=[pbk.b])
                    if blk % 2 == 0:
                        cx.op("act", lambda e, pbk=pbk, X=X, blk=blk: e.copy(
                            out=X[:, :, blk * 128:(blk + 1) * 128], in_=pbk[:].rearrange("p (c t) -> p c t", c=8)),
                            reads=[pbk.b], writes=[X.b])
                    else:
                        cx.op("dve", lambda e, pbk=pbk, X=X, blk=blk: e.tensor_copy(
                            out=X[:, :, blk * 128:(blk + 1) * 128], in_=pbk[:].rearrange("p (c t) -> p c t", c=8)),
                            reads=[pbk.b], writes=[X.b])
                a_ = ae[p]
                for ft in range(2):
                    pg = getps(); pu = getps()
                    for c in range(8):
                        cx.op("pe", lambda e, pg=pg, c=c, ft=ft, X=X, p=p: e.matmul(
                            pg[:, 0:CAP], lhsT=Wg[p][:, c, ft * 128:(ft + 1) * 128], rhs=X[:, c, :],
                            start=(c == 0), stop=(c == 7)), reads=[Wg[p].b, X.b], writes=[pg.b])
                    for c in range(8):
                        cx.op("pe", lambda e, pu=pu, c=c, ft=ft, X=X, p=p: e.matmul(
                            pu[:, 0:CAP], lhsT=Wu[p][:, c, ft * 128:(ft + 1) * 128], rhs=X[:, c, :],
                            start=(c == 0), stop=(c == 7)), reads=[Wu[p].b, X.b], writes=[pu.b])
                    s = sgl[ft]
                    cx.op("act", lambda e, pg=pg, s=s: e.activation(out=s[:], in_=pg[:, 0:CAP], func=AF.Silu),
                          reads=[pg.b], full=[s.b])
                    cx.op("dve", lambda e, pu=pu, s=s, a_=a_, ft=ft: e.tensor_tensor(
                        out=a_[:, ft, :], in0=pu[:, 0:CAP], in1=s[:], op=ALU.mult),
                        reads=[pu.b, s.b], writes=[a_.b])
                for blk in range(NBLK):
                    eb = e_ * NBLK + blk
                    Y = Yt[eb % 3]
                    for half in range(2):
                        py = getps()
                        for ft in range(2):
                            cx.op("pe", lambda e, py=py, ft=ft, blk=blk, half=half, a_=a_, p=p: e.matmul(
                                py[:], lhsT=a_[:, ft, blk * 128:(blk + 1) * 128],
                                rhs=Wd[p][:, ft, half * 512:(half + 1) * 512], start=(ft == 0), stop=(ft == 1)),
                                reads=[a_.b, Wd[p].b], writes=[py.b])
                        if half == 0:
                            cx.op("dve", lambda e, py=py, Y=Y, eb=eb: e.tensor_scalar(
                                out=Y[:, 0:512], in0=py[:], scalar1=lst_sb[:, eb, 2:3], scalar2=None, op0=ALU.mult),
                                reads=[py.b, lst_sb.b], writes=[Y.b])
                        else:
                            cx.op("act", lambda e, py=py, Y=Y, eb=eb: e.activation(
                                out=Y[:, 512:1024], in_=py[:], func=AF.Copy, scale=lst_sb[:, eb, 2:3]),
                                reads=[py.b, lst_sb.b], writes=[Y.b])
                    cx.op("pool", lambda e, Y=Y, eb=eb: e.indirect_dma_start(
                        out=moe_scr, out_offset=bass.IndirectOffsetOnAxis(ap=dst_i[:, eb:eb + 1], axis=0),
                        in_=Y[:], in_offset=None), reads=[Y.b, dst_i.b], writes=[moeB], dma=True)
            cx.barrier()
            alD.close()

            alE = Alloc(nc)
            gfin = alE.sb([128, D], F32, "gfin")
            cx.op("sp", lambda e: e.dma_start(out=gfin[:], in_=gfin_d.partition_broadcast(128)), full=[gfin.b], dma=True)
            xa = [alE.sb([128, D], F32, f"xa{i}") for i in range(2)]
            m0 = [alE.sb([128, D], F32, f"m0{i}") for i in range(2)]
            m1 = [alE.sb([128, D], F32, f"m1{i}") for i in range(2)]
            ot = [alE.sb([128, D], F32, f"ot{i}") for i in range(2)]
            junk3 = alE.sb([128, D], BF16, "junk3")
            sse = [alE.sb([128, 1], F32, f"sse{i}") for i in range(2)]
            rte = [alE.sb([128, 1], F32, f"rte{i}") for i in range(2)]
            rse = [alE.sb([128, 1], F32, f"rse{i}") for i in range(2)]
            outB = Buf("out")
            for ti in range(NT):
                p = ti % 2
                rows = slice(ti * 128, (ti + 1) * 128)
                cx.op("sp", lambda e, p=p, rows=rows: e.dma_start(out=xa[p][:], in_=x2_scr[rows, :]),
                      reads=[x2B], full=[xa[p].b], dma=True)
                cx.op("sp", lambda e, p=p, rows=rows: e.dma_start(out=m0[p][:], in_=moe_scr[rows, :]),
                      reads=[moeB], full=[m0[p].b], dma=True)
                cx.op("sp", lambda e, p=p, ti=ti: e.dma_start(
                    out=m1[p][:], in_=moe_scr[ROWS + ti * 128:ROWS + (ti + 1) * 128, :]),
                    reads=[moeB], full=[m1[p].b], dma=True)
                cx.op("pool", lambda e, p=p: e.tensor_tensor(out=m0[p][:], in0=m0[p][:], in1=m1[p][:], op=ALU.add),
                      reads=[m1[p].b], writes=[m0[p].b])
                cx.op("dve", lambda e, p=p: e.tensor_tensor(out=xa[p][:], in0=xa[p][:], in1=m0[p][:], op=ALU.add),
                      reads=[m0[p].b], writes=[xa[p].b])
                cx.op("act", lambda e, p=p: e.activation(out=junk3[:], in_=xa[p][:], func=AF.Square, accum_out=sse[p][:]),
                      reads=[xa[p].b], full=[junk3.b, sse[p].b])
                cx.op("act", lambda e, p=p: e.activation(out=rte[p][:], in_=sse[p][:], func=AF.Sqrt, scale=1.0 / D, bias=EPS),
                      reads=[sse[p].b], full=[rte[p].b])
                cx.op("dve", lambda e, p=p: e.reciprocal(out=rse[p][:], in_=rte[p][:]), reads=[rte[p].b], full=[rse[p].b])
                cx.op("dve", lambda e, p=p: e.scalar_tensor_tensor(out=ot[p][:], in0=xa[p][:], scalar=rse[p][:, 0:1],
                                                                   in1=gfin[:], op0=ALU.mult, op1=ALU.mult),
                      reads=[xa[p].b, rse[p].b, gfin.b], full=[ot[p].b])
                cx.op("sp", lambda e, p=p, rows=rows: e.dma_start(out=out_d[rows, :], in_=ot[p][:]),
                      reads=[ot[p].b], writes=[outB], dma=True)
            cx.barrier()
            alE.close()
        cx.barrier()
        cx.emit(block)
        print("waits", cx.nwait, "instrs", {e: cx.cnt[e] for e in cx.ENG}, "signals", {e: len(cx.waited[e]) for e in cx.ENG})
    return nc


def host_consts():
    c = {}
    c["ident_bf"] = np.eye(128, dtype=np.float32).astype(ml_dtypes.bfloat16)
    c["ident_f"] = np.eye(128, dtype=np.float32)
    psel = np.zeros((128, 8, 240), np.float32)
    for a in range(8):
        for i in range(16):
            psel[a * 16 + i, a, 7 * 16 + i] = 1.0
    c["psel"] = psel.astype(ml_dtypes.bfloat16)
    kk = np.arange(128) // 16
    c["cmask"] = (kk[None, :] >= kk[:, None]).astype(np.float32)
    c["tri"] = (np.arange(128)[:, None] < np.arange(128)[None, :]).astype(np.float32)
    c["ecap"] = np.ascontiguousarray(np.broadcast_to((np.arange(32) * NBLK).astype(np.float32)[None, :], (128, 32)))
    c["tokid"] = (np.arange(NT)[None, :] * 128 + np.arange(128)[:, None]).astype(np.float32)
    li = np.zeros((NEXP * CAP + 128, 4), np.float32)
    li[:, 0] = SEQ + ((np.arange(NEXP * CAP + 128) // (NEXP * NBLK)) % 128)
    li[:, 1] = li[:, 0]
    c["trashp"] = (NEXP * CAP + np.arange(128)).astype(np.float32).reshape(128, 1)
    c["lst_init"] = li
    return c


def pair_layout(a):
    rest = a.shape[2:]
    a = a.reshape((16, 2, 64) + rest)
    a = np.moveaxis(a, 0, 2)
    return np.ascontiguousarray(a.reshape((128, 16) + rest))


def make_inmap(inputs, b, consts=None):
    f = lambda a: np.ascontiguousarray(a, dtype=np.float32)
    m = {"x": f(inputs["x"][b]),
         "g_mix": f(inputs["g_mix"]),
         "w_in": f(inputs["w_in"][0])}
    m["lamre_l"] = pair_layout(f(inputs["ssm_lambda_re"][0]))
    m["lamim_l"] = pair_layout(f(inputs["ssm_lambda_im"][0]))
    m["logdt_l"] = pair_layout(np.broadcast_to(f(inputs["ssm_log_dt"][0])[:, None], (32, 64)))
    m["bre_l"] = pair_layout(f(inputs["ssm_b_re"][0]))
    m["bim_l"] = pair_layout(f(inputs["ssm_b_im"][0]))
    m["cre_l"] = pair_layout(f(inputs["ssm_c_re"][0]).transpose(0, 2, 1))
    m["cim_l"] = pair_layout(f(inputs["ssm_c_im"][0]).transpose(0, 2, 1))
    m["d_l"] = np.ascontiguousarray(np.tile(f(inputs["ssm_d"][0]).reshape(32, 16).T, (8, 1)))
    m["mem"] = f(inputs["mem"][b])
    for k_, n_ in (("g_mem", "g_mem"), ("g_ffn", "g_ffn")):
        m[n_] = f(inputs[k_])
    m["g_final"] = f(inputs["g_final"]).reshape(1, D)
    m["w_mem_kv"] = f(inputs["w_mem_kv"][0]); m["w_mem_out"] = f(inputs["w_mem_out"][0])
    m["w_conv_out"] = f(inputs["w_conv_out"][0]); m["w_ssm_glu"] = f(inputs["w_ssm_glu"][0])
    m["w_out"] = f(inputs["w_out"][0])
    m["w_router"] = np.ascontiguousarray(np.concatenate([f(inputs["w_router_group"][0]),
                                                         f(inputs["w_router_expert"][0])], axis=1))
    m["b_router"] = np.ascontiguousarray(np.concatenate([f(inputs["b_router_group"][0]),
                                                         f(inputs["b_router_expert"][0])])[None, :])
    m["cdw_l"] = np.ascontiguousarray(f(inputs["conv_dw"][0]).T.reshape(4, 128, 31).transpose(1, 0, 2))
    m["cb_l"] = np.ascontiguousarray(f(inputs["conv_dw_bias"][0]).reshape(4, 128).T)
    m["lng_l"] = np.ascontiguousarray(f(inputs["conv_ln_g"][0]).reshape(4, 128).T)
    m["lnb_l"] = np.ascontiguousarray(f(inputs["conv_ln_b"][0]).reshape(4, 128).T)
    m["w_exp_gate"] = f(inputs["w_exp_gate"][0]); m["w_exp_up"] = f(inputs["w_exp_up"][0])
    m["w_exp_down"] = f(inputs["w_exp_down"][0])
    m.update(consts if consts is not None else host_consts())
    return m


def kernel(**inputs):
    nc = build()
    consts = host_consts()
    in_maps = [make_inmap(inputs, b, consts) for b in range(NCORES)]
    res = run_bass_kernel_spmd(nc, in_maps, core_ids=list(range(NCORES)))
    return np.stack([r["out"] for r in res.results], axis=0)
```

```python
import os
import numpy as np
import ml_dtypes
from contextlib import ExitStack
import concourse.bass as bass
import concourse.mybir as mybir
from concourse.bass_utils import run_bass_kernel_spmd

F32 = mybir.dt.float32
BF16 = mybir.dt.bfloat16
I32 = mybir.dt.int32
U32 = mybir.dt.uint32
AF = mybir.ActivationFunctionType
ALU = mybir.AluOpType
AX = mybir.AxisListType
GELU = AF.Gelu_apprx_tanh

D = 1024
SEQ = 4096
NCORES = 8
T = 512
NB = SEQ // T
NT = SEQ // 128
EPS = 1e-6
NEXP = 32
CAP = 384
NBLK = CAP // 128
ROWS = SEQ + 128


class Buf:
    __slots__ = ("name", "w", "r")

    def __init__(self, name):
        self.name = name
        self.w = {}
        self.r = {}


class Ctx:
    ENG = ("pe", "dve", "act", "pool", "sp")
    KROT = 4
    NDMA = 12

    def __init__(self, nc, es):
        self.nc = nc
        self.q = {e: [] for e in self.ENG}
        self.cnt = {e: 0 for e in self.ENG}
        self.seen = {e: {} for e in self.ENG}
        self.esem = {e: [es.enter_context(nc.semaphore(f"s_{e}{i}")) for i in range(self.KROT)]
                     for e in self.ENG}
        self.dsem = {e: [es.enter_context(nc.semaphore(f"d_{e}{i}")) for i in range(self.NDMA)]
                     for e in ("sp", "act", "pool")}
        self.dcnt = {e: [0] * self.NDMA for e in self.dsem}
        self.dnext = {e: 0 for e in self.dsem}
        self.nwait = 0
        self.waited = {e: set() for e in self.ENG}

    def _wait(self, eng, tok):
        key, val = tok
        if key[0] == 'e' and key[1] == eng and eng == "pe":
            return
        if self.seen[eng].get(key, -1) >= val:
            return
        self.seen[eng][key] = val
        if key[0] == 'e':
            self.waited[key[1]].add(val)
        self.q[eng].append(("w", key, val))
        self.nwait += 1

    def op(self, eng, fn, reads=(), writes=(), full=(), dma=False):
        toks = []
        for b in reads:
            toks.extend(b.w.items())
        for b in tuple(writes) + tuple(full):
            toks.extend(b.w.items())
            toks.extend(b.r.items())
        for t in toks:
            self._wait(eng, t)
        if dma:
            i = self.dnext[eng]
            self.dnext[eng] = (i + 1) % self.NDMA
            key = ('d', eng, i)
            if self.dcnt[eng][i] > 0:
                self._wait(eng, (key, self.dcnt[eng][i]))
            self.dcnt[eng][i] += 16
            val = self.dcnt[eng][i]
            self.q[eng].append(("d", fn, self.dsem[eng][i]))
        else:
            key = ('e', eng)
            val = self.cnt[eng]
            self.cnt[eng] += 1
            self.q[eng].append(("i", fn, val))
        for b in reads:
            b.r[key] = val
        for b in full:
            b.w = {key: val}
            b.r = {}
        for b in writes:
            b.w[key] = val
        return (key, val)

    def barrier(self):
        toks = []
        for e in self.ENG:
            if self.cnt[e] > 0:
                toks.append((('e', e), self.cnt[e] - 1))
        for e in self.dsem:
            for i in range(self.NDMA):
                if self.dcnt[e][i] > 0:
                    toks.append((('d', e, i), self.dcnt[e][i]))
        for e in self.ENG:
            for t in toks:
                self._wait(e, t)

    def emit(self, block):
        nc = self.nc

        rank = {e: {v: i for i, v in enumerate(sorted(self.waited[e]))} for e in self.ENG}
        K_ = self.KROT

        def run(engname, engine):
            for item in self.q[engname]:
                if item[0] == "w":
                    key, val = item[1], item[2]
                    if key[0] == 'e':
                        r = rank[key[1]][val]
                        engine.wait_ge(self.esem[key[1]][r % K_], r // K_ + 1)
                    else:
                        engine.wait_ge(self.dsem[key[1]][key[2]], val)
                elif item[0] == "d":
                    item[1](engine).then_inc(item[2], 16)
                else:
                    ins = item[1](engine)
                    r = rank[engname].get(item[2])
                    if r is not None:
                        ins.then_inc(self.esem[engname][r % K_], 1)

        @block.tensor
        def _(e):
            run("pe", e)

        @block.vector
        def _(e):
            run("dve", e)

        @block.scalar
        def _(e):
            run("act", e)

        @block.gpsimd
        def _(e):
            run("pool", e)

        @block.sync
        def _(e):
            run("sp", e)


class TT:
    def __init__(self, t, name):
        self.t = t
        self.b = Buf(name)

    def __getitem__(self, k):
        return self.t[k]


class Alloc:
    cnt = [0]

    def __init__(self, nc, es=None):
        self.nc = nc
        self.es = es if es is not None else ExitStack()

    @property
    def n(self):
        return Alloc.cnt[0]

    @n.setter
    def n(self, v):
        Alloc.cnt[0] = v

    def close(self):
        self.es.close()

    def sb(self, shape, dt, name=None):
        self.n += 1
        name = name or f"sb{self.n}"
        t = self.es.enter_context(self.nc.sbuf_tensor(f"{name}_{self.n}", list(shape), dt))
        return TT(t, name)

    def ps(self, shape, dt, name=None):
        self.n += 1
        name = name or f"ps{self.n}"
        t = self.es.enter_context(self.nc.psum_tensor(f"{name}_{self.n}", list(shape), dt))
        return TT(t, name)


def build(stop_after="E", dbg=False):
    nc = bass.Bass("TRN2", target_bir_lowering=False)
    dram = {}

    def din(name, shape, dt=F32):
        dram[name] = nc.dram_tensor(name, list(shape), dt, kind="ExternalInput").ap()
        return dram[name]

    def dscr(name, shape, dt, kind="Internal"):
        dram[name] = nc.dram_tensor(name, list(shape), dt, kind=kind).ap()
        return dram[name]

    x_d = din("x", [SEQ, D])
    gmix_d = din("g_mix", [1, D])
    w_in_d = din("w_in", [D, 5120])
    ident_bf_d = din("ident_bf", [128, 128], BF16)
    ident_f_d = din("ident_f", [128, 128], F32)
    lamre_d = din("lamre_l", [128, 16])
    lamim_d = din("lamim_l", [128, 16])
    logdt_d = din("logdt_l", [128, 16])
    bre_d = din("bre_l", [128, 16, 16])
    bim_d = din("bim_l", [128, 16, 16])
    cre_d = din("cre_l", [128, 16, 16])
    cim_d = din("cim_l", [128, 16, 16])
    dl_d = din("d_l", [128, 32])
    psel_d = din("psel", [128, 8, 240], BF16)
    cmask_d = din("cmask", [128, 128])
    mem_d = din("mem", [256, D])
    gmem_d = din("g_mem", [1, D])
    gffn_d = din("g_ffn", [1, D])
    gfin_d = din("g_final", [1, D])
    wkv_d = din("w_mem_kv", [D, 1024])
    wmo_d = din("w_mem_out", [512, D])
    wco_d = din("w_conv_out", [512, D])
    wgl_d = din("w_ssm_glu", [512, 2048])
    wo_d = din("w_out", [D, D])
    wr_d = din("w_router", [D, 36])
    rbias_d = din("b_router", [1, 36])
    cdw_d = din("cdw_l", [128, 4, 31])
    cb_d = din("cb_l", [128, 4])
    lng_d = din("lng_l", [128, 4])
    lnb_d = din("lnb_l", [128, 4])
    tri_d = din("tri", [128, 128])
    ecap_d = din("ecap", [128, 32])
    tokid_d = din("tokid", [128, NT])
    lst_init_d = din("lst_init", [NEXP * CAP + 128, 4])
    trashp_d = din("trashp", [128, 1])
    weg_d = din("w_exp_gate", [NEXP, 128, 8, 256])
    weu_d = din("w_exp_up", [NEXP, 128, 8, 256])
    wed_d = din("w_exp_down", [NEXP, 128, 2, D])
    dk = "ExternalOutput" if dbg else "Internal"
    lst_d = dscr("lst", [NEXP * CAP + 128, 4], F32, kind=dk)
    h2_scr = dscr("h2_scr", [ROWS, D], BF16, kind=dk)
    moe_scr = dscr("moe_scr", [2 * ROWS, D], BF16, kind=dk)
    x2_scr = dscr("x2_scr", [SEQ, D], F32, kind=dk)
    ys_scr = dscr("ys_scr", [4, 128, SEQ], BF16, kind="ExternalOutput" if dbg else "Internal")
    out_d = dscr("out", [SEQ, D], F32, kind="ExternalOutput")
    hT_scr = dscr("hT_scr", [8, 128, SEQ], BF16, kind="ExternalOutput" if dbg else "Internal")
    u_dbg = dscr("u_dbg", [4, 128, SEQ], BF16, kind="ExternalOutput") if dbg else None

    with ExitStack() as es:
        cx = Ctx(nc, es)
        al = Alloc(nc, es)
        block = es.enter_context(nc.Block())

        ident_bf = al.sb([128, 128], BF16, "ident_bf")
        cx.op("sp", lambda e: e.dma_start(out=ident_bf[:], in_=ident_bf_d), full=[ident_bf.b], dma=True)
        ident_f = al.sb([128, 128], F32, "ident_f")
        cx.op("sp", lambda e: e.dma_start(out=ident_f[:], in_=ident_f_d), full=[ident_f.b], dma=True)

        psum = [al.ps([128, 512], F32, f"bank{i}") for i in range(6)]
        psb = [al.ps([128, 1024], BF16, f"bankb{i}") for i in range(2)]
        pctr = [0]

        def getps():
            p = psum[pctr[0] % len(psum)]
            pctr[0] += 1
            return p

        alAB = Alloc(nc)
        u_all = alAB.sb([128, 4, SEQ], BF16, "u_all")
        M_all = alAB.sb([128, 32, 128], BF16, "M_all")
        W2r = alAB.sb([128, 16, 2, 128], BF16, "W2r"); W2i = alAB.sb([128, 16, 2, 128], BF16, "W2i")
        C1r = alAB.sb([128, 16, 128], BF16, "C1r"); nC1i = alAB.sb([128, 16, 128], BF16, "nC1i")
        KAr = alAB.sb([128, 9, 16], F32, "KAr"); KAi = alAB.sb([128, 9, 16], F32, "KAi")
        KnAi = alAB.sb([128, 9, 16], F32, "KnAi")
        psel = alAB.sb([128, 8, 240], BF16, "psel")
        cx.op("sp", lambda e: e.dma_start(out=psel[:], in_=psel_d), full=[psel.b], dma=True)
        al_outer = al
        al = Alloc(nc)
        gmix = al.sb([128, D], F32, "gmix")
        cx.op("sp", lambda e: e.dma_start(out=gmix[:], in_=gmix_d.partition_broadcast(128)),
              full=[gmix.b], dma=True)

        stg = [al.sb([128, 8, 256], F32, f"stg{i}") for i in range(2)]
        sctr = [0]

        def load_cast(dst, dst_col0, src_d, c0, c1, kch):
            for cc in range(c0, c1, 256):
                w = min(256, c1 - cc)
                s = stg[sctr[0] % 2]
                sctr[0] += 1
                src = src_d[:, cc:cc + w].rearrange("(c p) n -> p c n", p=128)
                cx.op("sp", lambda e, s=s, src=src, w=w: e.dma_start(out=s[:, 0:kch, 0:w], in_=src),
                      full=[s.b], dma=True)
                o = dst_col0 + (cc - c0)
                cx.op("pool", lambda e, s=s, o=o, w=w: e.tensor_copy(out=dst[:, 0:kch, o:o + w],
                                                                     in_=s[:, 0:kch, 0:w]),
                      reads=[s.b], writes=[dst.b])

        w_ssm_in = al.sb([128, 8, 512], BF16, "w_ssm_in")
        load_cast(w_ssm_in, 0, w_in_d, 1024, 1536, 8)
        xt = [al.sb([128, D], F32, f"xt{i}") for i in range(2)]
        junk = al.sb([128, D], BF16, "junk")
        ss = [al.sb([128, 1], F32, f"ss{i}") for i in range(2)]
        rt = [al.sb([128, 1], F32, f"rt{i}") for i in range(2)]
        rstd = [al.sb([128, 1], F32, f"rstd{i}") for i in range(2)]
        hbf = [al.sb([128, D], BF16, f"hbf{i}") for i in range(2)]
        hTb = [al.sb([128, 8, T], BF16, f"hTb{i}") for i in range(2)]

        for i in range(NT):
            p = i % 2
            blk = i // 4
            hb = hTb[blk % 2]
            cx.op("sp", lambda e, p=p, i=i: e.dma_start(out=xt[p][:], in_=x_d[i * 128:(i + 1) * 128, :]),
                  full=[xt[p].b], dma=True)
            cx.op("act", lambda e, p=p: e.activation(out=junk[:], in_=xt[p][:], func=AF.Square,
                                                     accum_out=ss[p][:]),
                  reads=[xt[p].b], writes=[junk.b], full=[ss[p].b])
            cx.op("act", lambda e, p=p: e.activation(out=rt[p][:], in_=ss[p][:], func=AF.Sqrt,
                                                     scale=1.0 / D, bias=EPS),
                  reads=[ss[p].b], full=[rt[p].b])
            cx.op("dve", lambda e, p=p: e.reciprocal(out=rstd[p][:], in_=rt[p][:]),
                  reads=[rt[p].b], full=[rstd[p].b])
            cx.op("dve", lambda e, p=p: e.scalar_tensor_tensor(out=hbf[p][:], in0=xt[p][:],
                                                               scalar=rstd[p][:, 0:1], in1=gmix[:],
                                                               op0=ALU.mult, op1=ALU.mult),
                  reads=[xt[p].b, rstd[p].b, gmix.b], full=[hbf[p].b])
            pb = psb[i % 2]
            for c in range(8):
                cx.op("pe", lambda e, pb=pb, p=p, c=c: e.transpose(out=pb[:, c * 128:(c + 1) * 128],
                                                                   in_=hbf[p][:, c * 128:(c + 1) * 128],
                                                                   identity=ident_bf[:]),
                      reads=[hbf[p].b, ident_bf.b], writes=[pb.b])
            tt = i % 4
            cx.op("act", lambda e, pb=pb, hb=hb, tt=tt: e.copy(
                out=hb[:, :, tt * 128:(tt + 1) * 128],
                in_=pb[:].rearrange("p (c t) -> p c t", c=8)),
                reads=[pb.b], writes=[hb.b])
            if tt == 3:
                for f in range(4):
                    ps = getps()
                    for c in range(8):
                        cx.op("pe", lambda e, ps=ps, hb=hb, f=f, c=c: e.matmul(
                            ps[:], lhsT=w_ssm_in[:, c, f * 128:(f + 1) * 128], rhs=hb[:, c, :],
                            start=(c == 0), stop=(c == 7)),
                            reads=[w_ssm_in.b, hb.b], writes=[ps.b])
                    cx.op("dve", lambda e, ps=ps, f=f, blk=blk: e.tensor_copy(
                        out=u_all[:, f, blk * T:(blk + 1) * T], in_=ps[:]),
                        reads=[ps.b], writes=[u_all.b])
                cx.op("sp", lambda e, hb=hb, blk=blk: e.dma_start(
                    out=hT_scr[:, :, blk * T:(blk + 1) * T].rearrange("c p t -> p c t"), in_=hb[:]),
                    reads=[hb.b], dma=True)

        if dbg:
            cx.op("sp", lambda e: e.dma_start(out=u_dbg.rearrange("f p t -> p f t"), in_=u_all[:]),
                  reads=[u_all.b], dma=True)


        cx.barrier()
        al.close()
        al = Alloc(nc)
        TWO_PI = 2.0 * np.pi
        cmask = al.sb([128, 128], F32, "cmask")
        cx.op("sp", lambda e: e.dma_start(out=cmask[:], in_=cmask_d), full=[cmask.b], dma=True)
        dl = al.sb([128, 32], F32, "dl")
        cx.op("sp", lambda e: e.dma_start(out=dl[:], in_=dl_d), full=[dl.b], dma=True)
        SU = Buf("ssm_setup")

        def sload(shape, src, name):
            t = al.sb(shape, F32, name)
            cx.op("sp", lambda e: e.dma_start(out=t[:], in_=src), full=[t.b], dma=True)
            return t

        lamre = sload([128, 16], lamre_d, "lamre")
        lamim = sload([128, 16], lamim_d, "lamim")
        logdt = sload([128, 16], logdt_d, "logdt")
        Bre = sload([128, 16, 16], bre_d, "Bre")
        Bim = sload([128, 16, 16], bim_d, "Bim")
        Cre = sload([128, 16, 16], cre_d, "Cre")
        Cim = sload([128, 16, 16], cim_d, "Cim")
        ins_b = [lamre.b, lamim.b, logdt.b, Bre.b, Bim.b, Cre.b, Cim.b]

        def S(shape, name):
            return al.sb(shape, F32, name)

        def dv(fn):
            cx.op("dve", fn, reads=ins_b, writes=[SU])

        def ac(fn):
            cx.op("act", fn, reads=ins_b, writes=[SU])

        def tt_(out, a, b, op):
            dv(lambda e: e.tensor_tensor(out=out, in0=a, in1=b, op=op))

        sh16 = [128, 16]
        dt_ = S(sh16, "dt"); lrd = S(sh16, "lrd"); th = S(sh16, "th")
        ac(lambda e: e.activation(out=dt_[:], in_=logdt[:], func=AF.Exp))
        tt_(lrd[:], lamre[:], dt_[:], ALU.mult)
        tt_(th[:], lamim[:], dt_[:], ALU.mult)
        mag = S(sh16, "mag"); imag2 = S(sh16, "imag2")
        ac(lambda e: e.activation(out=mag[:], in_=lrd[:], func=AF.Exp))
        ac(lambda e: e.activation(out=imag2[:], in_=lrd[:], func=AF.Exp, scale=-2.0))
        kq_i = al.sb(sh16, I32, "kq_i"); kq = S(sh16, "kq"); red = S(sh16, "red"); msk = S(sh16, "msk")
        sinv = S(sh16, "sinv"); cosv = S(sh16, "cosv"); tmpa = S(sh16, "tmpa")

        def sin_of(outt, shift):
            dv(lambda e: e.tensor_scalar(out=tmpa[:], in0=th[:], scalar1=float(shift), scalar2=None,
                                         op0=ALU.add))
            dv(lambda e: e.tensor_scalar(out=kq[:], in0=tmpa[:], scalar1=float(1.0 / TWO_PI),
                                         scalar2=None, op0=ALU.mult))
            dv(lambda e: e.tensor_copy(out=kq_i[:], in_=kq[:]))
            dv(lambda e: e.tensor_copy(out=kq[:], in_=kq_i[:]))
            dv(lambda e: e.scalar_tensor_tensor(out=red[:], in0=kq[:], scalar=float(-TWO_PI),
                                                in1=tmpa[:], op0=ALU.mult, op1=ALU.add))
            dv(lambda e: e.tensor_single_scalar(out=msk[:], in_=red[:], scalar=float(np.pi), op=ALU.is_gt))
            dv(lambda e: e.scalar_tensor_tensor(out=red[:], in0=msk[:], scalar=float(-TWO_PI),
                                                in1=red[:], op0=ALU.mult, op1=ALU.add))
            dv(lambda e: e.tensor_single_scalar(out=msk[:], in_=red[:], scalar=float(-np.pi), op=ALU.is_lt))
            dv(lambda e: e.scalar_tensor_tensor(out=red[:], in0=msk[:], scalar=float(TWO_PI),
                                                in1=red[:], op0=ALU.mult, op1=ALU.add))
            ac(lambda e: e.activation(out=outt[:], in_=red[:], func=AF.Sin))

        sin_of(sinv, 0.0)
        sin_of(cosv, np.pi / 2)
        PWr = S([128, 9, 16], "PWr"); PWi = S([128, 9, 16], "PWi")
        IPr = S([128, 8, 16], "IPr"); IPi = S([128, 8, 16], "IPi")
        t1 = S([128, 16, 8, 16], "t1"); t2 = S([128, 16, 8, 16], "t2")

        def cmul(outr, outi, ar, ai, br, bi, shp, neg_i=False):
            a1 = t1[:].rearrange("p a b c -> p (a b c)")[:, 0:int(np.prod(shp[1:]))]
            a2 = t2[:].rearrange("p a b c -> p (a b c)")[:, 0:int(np.prod(shp[1:]))]
            if len(shp) == 3:
                a1 = a1.rearrange("p (a b) -> p a b", a=shp[1])
                a2 = a2.rearrange("p (a b) -> p a b", a=shp[1])
            tt_(a1, ar, br, ALU.mult)
            tt_(a2, ai, bi, ALU.mult)
            tt_(outr, a1, a2, ALU.subtract)
            tt_(a1, ar, bi, ALU.mult)
            tt_(a2, ai, br, ALU.mult)
            if neg_i:
                dv(lambda e: e.scalar_tensor_tensor(out=outi, in0=a1, scalar=-1.0, in1=a2,
                                                    op0=ALU.mult, op1=ALU.subtract))
            else:
                tt_(outi, a1, a2, ALU.add)

        dv(lambda e: e.memset(PWr[:, 0, :], 1.0))
        dv(lambda e: e.memset(PWi[:, 0, :], 0.0))
        dv(lambda e: e.memset(IPr[:, 0, :], 1.0))
        dv(lambda e: e.memset(IPi[:, 0, :], 0.0))
        tt_(PWr[:, 1, :], mag[:], cosv[:], ALU.mult)
        tt_(PWi[:, 1, :], mag[:], sinv[:], ALU.mult)
        tt_(IPr[:, 1, :], PWr[:, 1, :], imag2[:], ALU.mult)
        dv(lambda e: e.scalar_tensor_tensor(out=IPi[:, 1, :], in0=PWi[:, 1, :], scalar=-1.0, in1=imag2[:],
                                            op0=ALU.mult, op1=ALU.mult))
        for n in range(2, 9):
            cmul(PWr[:, n, :], PWi[:, n, :], PWr[:, n - 1, :], PWi[:, n - 1, :], PWr[:, 1, :], PWi[:, 1, :], sh16)
        for n in range(2, 8):
            cmul(IPr[:, n, :], IPi[:, n, :], IPr[:, n - 1, :], IPi[:, n - 1, :], IPr[:, 1, :], IPi[:, 1, :], sh16)
        dv(lambda e: e.tensor_copy(out=KAr[:, 0, :], in_=PWr[:, 8, :]))
        dv(lambda e: e.tensor_copy(out=KAi[:, 0, :], in_=PWi[:, 8, :]))
        for d_ in range(1, 9):
            cmul(KAr[:, d_, :], KAi[:, d_, :], KAr[:, d_ - 1, :], KAi[:, d_ - 1, :],
                 KAr[:, d_ - 1, :], KAi[:, d_ - 1, :], sh16)
        dv(lambda e: e.tensor_scalar(out=KnAi[:], in0=KAi[:], scalar1=-1.0, scalar2=None, op0=ALU.mult))
        am1 = S(sh16, "am1"); l2 = S(sh16, "l2"); il2 = S(sh16, "il2"); kr = S(sh16, "kr"); ki = S(sh16, "ki")
        dv(lambda e: e.tensor_scalar(out=am1[:], in0=PWr[:, 1, :], scalar1=-1.0, scalar2=None, op0=ALU.add))
        tt_(l2[:], lamre[:], lamre[:], ALU.mult)
        tt_(tmpa[:], lamim[:], lamim[:], ALU.mult)
        tt_(l2[:], l2[:], tmpa[:], ALU.add)
        dv(lambda e: e.reciprocal(out=il2[:], in_=l2[:]))
        tt_(kr[:], am1[:], lamre[:], ALU.mult)
        tt_(tmpa[:], PWi[:, 1, :], lamim[:], ALU.mult)
        tt_(kr[:], kr[:], tmpa[:], ALU.add)
        tt_(kr[:], kr[:], il2[:], ALU.mult)
        tt_(ki[:], PWi[:, 1, :], lamre[:], ALU.mult)
        tt_(tmpa[:], am1[:], lamim[:], ALU.mult)
        tt_(ki[:], ki[:], tmpa[:], ALU.subtract)
        tt_(ki[:], ki[:], il2[:], ALU.mult)
        sh3 = [128, 16, 16]

        def bc(a):
            return a.unsqueeze(2).to_broadcast(sh3)

        Bbr = S(sh3, "Bbr"); Bbi = S(sh3, "Bbi")
        cmul(Bbr[:], Bbi[:], bc(kr[:]), bc(ki[:]), Bre[:], Bim[:], sh3)
        Bhr = S([128, 16, 8, 16], "Bhr"); nBhi = S([128, 16, 8, 16], "nBhi"); Bhi = S([128, 16, 8, 16], "Bhi")
        Btr = S([128, 16, 8, 16], "Btr"); Bti = S([128, 16, 8, 16], "Bti")
        Chr = S([128, 16, 9, 16], "Chr"); Chi = S([128, 16, 9, 16], "Chi"); nChi = S([128, 16, 9, 16], "nChi")
        for k in range(8):
            cmul(Bhr[:, :, k, :], Bhi[:, :, k, :], bc(IPr[:, k, :]), bc(IPi[:, k, :]), Bbr[:], Bbi[:], sh3)
            cmul(Btr[:, :, k, :], Bti[:, :, k, :], bc(PWr[:, 7, :]), bc(PWi[:, 7, :]),
                 Bhr[:, :, k, :], Bhi[:, :, k, :], sh3)
        dv(lambda e: e.tensor_scalar(out=nBhi[:], in0=Bhi[:], scalar1=-1.0, scalar2=None, op0=ALU.mult))
        for j in range(9):
            cmul(Chr[:, :, j, :], Chi[:, :, j, :], bc(PWr[:, j, :]), bc(PWi[:, j, :]), Cre[:], Cim[:], sh3)
        dv(lambda e: e.tensor_scalar(out=nChi[:], in0=Chi[:], scalar1=-1.0, scalar2=None, op0=ALU.mult))
        dv(lambda e: e.tensor_copy(out=C1r[:].rearrange("p r (j c) -> p r j c", j=8), in_=Chr[:, :, 1:9, :]))
        dv(lambda e: e.tensor_copy(out=nC1i[:].rearrange("p r (j c) -> p r j c", j=8), in_=nChi[:, :, 1:9, :]))
        mtmp = S([128, 128], "mtmp")
        cx.op("pool", lambda e: e.memset(W2r[:], 0.0), reads=ins_b, writes=[SU])
        cx.op("pool", lambda e: e.memset(W2i[:], 0.0), reads=ins_b, writes=[SU])
        for r in range(16):
            for two in range(2):
                g = 2 * r + two
                rng = slice(two * 64, (two + 1) * 64)
                ps = getps()
                cx.op("pe", lambda e, ps=ps, r=r, rng=rng: e.matmul(
                    ps[:, 0:128], lhsT=Bhr[rng, r, :, :].rearrange("p k c -> p (k c)"),
                    rhs=Chr[rng, r, 0:8, :].rearrange("p j c -> p (j c)"), start=True, stop=False),
                    reads=[SU], writes=[ps.b])
                cx.op("pe", lambda e, ps=ps, r=r, rng=rng: e.matmul(
                    ps[:, 0:128], lhsT=nBhi[rng, r, :, :].rearrange("p k c -> p (k c)"),
                    rhs=Chi[rng, r, 0:8, :].rearrange("p j c -> p (j c)"), start=False, stop=True),
                    reads=[SU], writes=[ps.b])
                cx.op("dve", lambda e, ps=ps: e.tensor_tensor(out=mtmp[:], in0=ps[:, 0:128], in1=cmask[:],
                                                              op=ALU.mult),
                      reads=[ps.b, cmask.b], writes=[SU])
                cx.op("dve", lambda e, g=g: e.scalar_tensor_tensor(
                    out=M_all[:, g, :], in0=ident_f[:], scalar=dl[:, g:g + 1], in1=mtmp[:],
                    op0=ALU.mult, op1=ALU.add),
                    reads=[ident_f.b, dl.b], writes=[SU, M_all.b])
            for (Bt, W2) in ((Btr, W2r), (Bti, W2i)):
                ps = getps()
                cx.op("pe", lambda e, ps=ps, r=r, Bt=Bt: e.transpose(
                    out=ps[:, 0:128], in_=Bt[:, r, :, :].rearrange("p k c -> p (k c)"), identity=ident_f[:]),
                    reads=[SU, ident_f.b], writes=[ps.b])
                cx.op("dve", lambda e, ps=ps, r=r, W2=W2: e.tensor_copy(out=W2[:, r, 0, 0:64], in_=ps[:, 0:64]),
                      reads=[ps.b], writes=[SU, W2.b])
                cx.op("dve", lambda e, ps=ps, r=r, W2=W2: e.tensor_copy(out=W2[:, r, 1, 64:128], in_=ps[:, 64:128]),
                      reads=[ps.b], writes=[SU, W2.b])

        cx.barrier()
        al.close()
        al = Alloc(nc)
        NCH = SEQ // 8
        Vg = [al.sb([128, NCH], BF16, f"Vg{i}") for i in range(4)]
        Sre = [[al.sb([128, NCH], F32, f"Sre{s}{i}") for i in range(2)] for s in range(2)]
        Sim = [[al.sb([128, NCH], F32, f"Sim{s}{i}") for i in range(2)] for s in range(2)]
        Sbr = [al.sb([128, NCH], BF16, f"Sbr{s}") for s in range(2)]
        Sbi = [al.sb([128, NCH], BF16, f"Sbi{s}") for s in range(2)]
        Gg = [al.sb([128, NCH], BF16, f"Gg{i}") for i in range(16)]
        ysf = [al.sb([128, SEQ], BF16, f"ysf{i}") for i in range(2)]
        for s in range(2):
            cx.op("pool", lambda e, s=s: e.memset(Sbr[s][:, 0:1], 0.0), writes=[Sbr[s].b])
            cx.op("pool", lambda e, s=s: e.memset(Sbi[s][:, 0:1], 0.0), writes=[Sbi[s].b])

        def b_front(r):
            f = r // 4
            st = r % 2
            vg = [Vg[(2 * r) % 4], Vg[(2 * r + 1) % 4]]
            for two in range(2):
                g = 2 * r + two
                gl = g % 8
                ps = getps()
                for k in range(8):
                    cx.op("pe", lambda e, ps=ps, gl=gl, k=k, f=f: e.matmul(
                        ps[:], lhsT=psel[:, gl, (7 - k) * 16:(7 - k) * 16 + 128],
                        rhs=u_all[:, f, k:SEQ:8], start=(k == 0), stop=(k == 7)),
                        reads=[psel.b, u_all.b], writes=[ps.b])
                cx.op("act", lambda e, ps=ps, v=vg[two]: e.copy(out=v[:], in_=ps[:]),
                      reads=[ps.b], full=[vg[two].b])
            psr = getps(); psi = getps()
            for (pp, W2) in ((psr, W2r), (psi, W2i)):
                for two in range(2):
                    cx.op("pe", lambda e, pp=pp, W2=W2, two=two, r=r, v=vg[two]: e.matmul(
                        pp[:], lhsT=W2[:, r, two, :], rhs=v[:], start=(two == 0), stop=(two == 1)),
                        reads=[W2.b, vg[two].b], writes=[pp.b])
            cx.op("act", lambda e, psr=psr, st=st: e.copy(out=Sre[st][0][:], in_=psr[:]),
                  reads=[psr.b], full=[Sre[st][0].b])
            cx.op("act", lambda e, psi=psi, st=st: e.copy(out=Sim[st][0][:], in_=psi[:]),
                  reads=[psi.b], full=[Sim[st][0].b])

        def b_mid(r):
            st = r % 2
            cur = 0
            for d_ in range(9):
                sh = 1 << d_
                s_r, s_i, d_r, d_i = Sre[st][cur], Sim[st][cur], Sre[st][1 - cur], Sim[st][1 - cur]
                n = NCH - sh
                cx.op("dve", lambda e, s_r=s_r, d_r=d_r, sh=sh, n=n, d_=d_, r=r: e.scalar_tensor_tensor(
                    out=d_r[:, sh:NCH], in0=s_r[:, 0:n], scalar=KAr[:, d_, r:r + 1], in1=s_r[:, sh:NCH],
                    op0=ALU.mult, op1=ALU.add), reads=[s_r.b, SU], writes=[d_r.b])
                cx.op("dve", lambda e, s_i=s_i, d_r=d_r, sh=sh, n=n, d_=d_, r=r: e.scalar_tensor_tensor(
                    out=d_r[:, sh:NCH], in0=s_i[:, 0:n], scalar=KnAi[:, d_, r:r + 1], in1=d_r[:, sh:NCH],
                    op0=ALU.mult, op1=ALU.add), reads=[s_i.b, SU], writes=[d_r.b])
                cx.op("dve", lambda e, s_i=s_i, d_i=d_i, sh=sh, n=n, d_=d_, r=r: e.scalar_tensor_tensor(
                    out=d_i[:, sh:NCH], in0=s_i[:, 0:n], scalar=KAr[:, d_, r:r + 1], in1=s_i[:, sh:NCH],
                    op0=ALU.mult, op1=ALU.add), reads=[s_i.b, SU], writes=[d_i.b])
                cx.op("dve", lambda e, s_r=s_r, d_i=d_i, sh=sh, n=n, d_=d_, r=r: e.scalar_tensor_tensor(
                    out=d_i[:, sh:NCH], in0=s_r[:, 0:n], scalar=KAi[:, d_, r:r + 1], in1=d_i[:, sh:NCH],
                    op0=ALU.mult, op1=ALU.add), reads=[s_r.b, SU], writes=[d_i.b])
                cx.op("pool", lambda e, s_r=s_r, d_r=d_r, sh=sh: e.tensor_copy(out=d_r[:, 0:sh], in_=s_r[:, 0:sh]),
                      reads=[s_r.b], writes=[d_r.b])
                cx.op("pool", lambda e, s_i=s_i, d_i=d_i, sh=sh: e.tensor_copy(out=d_i[:, 0:sh], in_=s_i[:, 0:sh]),
                      reads=[s_i.b], writes=[d_i.b])
                cur = 1 - cur
            fr, fi = Sre[st][cur], Sim[st][cur]
            cx.op("pool", lambda e, fr=fr, st=st: e.tensor_copy(out=Sbr[st][:, 1:NCH], in_=fr[:, 0:NCH - 1]),
                  reads=[fr.b], writes=[Sbr[st].b])
            cx.op("pool", lambda e, fi=fi, st=st: e.tensor_copy(out=Sbi[st][:, 1:NCH], in_=fi[:, 0:NCH - 1]),
                  reads=[fi.b], writes=[Sbi[st].b])

        def b_back(r):
            f = r // 4
            st = r % 2
            vg = [Vg[(2 * r) % 4], Vg[(2 * r + 1) % 4]]
            for two in range(2):
                g = 2 * r + two
                rng = slice(two * 64, (two + 1) * 64)
                ps = getps()
                cx.op("pe", lambda e, ps=ps, g=g, v=vg[two]: e.matmul(
                    ps[:], lhsT=M_all[:, g, :], rhs=v[:], start=True, stop=False),
                    reads=[M_all.b, vg[two].b], writes=[ps.b])
                cx.op("pe", lambda e, ps=ps, r=r, rng=rng, st=st: e.matmul(
                    ps[:], lhsT=C1r[rng, r, :], rhs=Sbr[st][rng, :], start=False, stop=False),
                    reads=[SU, Sbr[st].b], writes=[ps.b])
                cx.op("pe", lambda e, ps=ps, r=r, rng=rng, st=st: e.matmul(
                    ps[:], lhsT=nC1i[rng, r, :], rhs=Sbi[st][rng, :], start=False, stop=True),
                    reads=[SU, Sbi[st].b], writes=[ps.b])
                gg = Gg[g % 16]
                cx.op("act", lambda e, ps=ps, gg=gg: e.activation(out=gg[:], in_=ps[:], func=GELU),
                      reads=[ps.b], full=[gg.b])
            if r % 4 == 3:
                yb = ysf[f % 2]
                for j in range(8):
                    ps = getps()
                    for gl in range(8):
                        gg = Gg[(8 * f + gl) % 16]
                        cx.op("pe", lambda e, ps=ps, j=j, gl=gl, gg=gg: e.matmul(
                            ps[:], lhsT=psel[:, j, (7 - gl) * 16:(7 - gl) * 16 + 128], rhs=gg[:],
                            start=(gl == 0), stop=(gl == 7)),
                            reads=[psel.b, gg.b], writes=[ps.b])
                    cx.op("act", lambda e, ps=ps, yb=yb, j=j: e.copy(out=yb[:, j:SEQ:8], in_=ps[:]),
                          reads=[ps.b], writes=[yb.b])
                cx.op("sp", lambda e, yb=yb, f=f: e.dma_start(out=ys_scr[f], in_=yb[:]),
                      reads=[yb.b], dma=True)

        b_front(0)
        for r in range(16):
            b_mid(r)
            if r + 1 < 16:
                b_front(r + 1)
            b_back(r)

        cx.barrier()
        al.close()
        alAB.close()
        al = al_outer
        if stop_after in ("A", "B"):
            pass
        else:
            TC = 256
            NBC = SEQ // TC
            alC = Alloc(nc)
            wA = alC.sb([128, 8, 1536], BF16, "wA")
            wG = alC.sb([128, 8, 3072], BF16, "wG")
            wco = alC.sb([128, 4, 1024], BF16, "wco")
            wgl = alC.sb([128, 4, 2048], BF16, "wgl")
            wmo = alC.sb([128, 4, 1024], BF16, "wmo")
            wo = alC.sb([128, 8, 1024], BF16, "wo")
            Dg2 = [alC.sb([128, 31, 128], BF16, f"Dg{i}") for i in range(2)]
            kT = alC.sb([128, 4, 256], BF16, "kT")
            vtok = alC.sb([128, 2, 512], BF16, "vtok")
            gffn = alC.sb([128, D], F32, "gffn")
            wr = alC.sb([128, 8, 36], F32, "wr")
            rbias = alC.sb([128, 36], F32, "rbias")
            cdw = alC.sb([128, 4, 31], F32, "cdw")
            cb = alC.sb([128, 4], F32, "cb"); lng = alC.sb([128, 4], F32, "lng"); lnb = alC.sb([128, 4], F32, "lnb")
            onesm = alC.sb([128, 128], F32, "onesm")
            ones_bf = alC.sb([128, 128], BF16, "ones_bf")
            tri = alC.sb([128, 128], F32, "tri")
            ones_f = alC.sb([128, 128], F32, "ones_f")
            ecap = alC.sb([128, 32], F32, "ecap")
            tokid = alC.sb([128, NT], F32, "tokid")
            cum = alC.sb([128, 32], F32, "cum")
            lg_all = alC.sb([128, NT, 36], F32, "lg_all")
            trashp = alC.sb([128, 1], F32, "trashp")

            def ld(t, src):
                cx.op("sp", lambda e: e.dma_start(out=t[:], in_=src), full=[t.b], dma=True)

            ld(gffn, gffn_d.partition_broadcast(128))
            ld(wr, wr_d.rearrange("(c p) n -> p c n", p=128))
            ld(rbias, rbias_d.partition_broadcast(128))
            ld(cdw, cdw_d); ld(cb, cb_d); ld(lng, lng_d); ld(lnb, lnb_d)
            ld(tri, tri_d); ld(ecap, ecap_d); ld(tokid, tokid_d); ld(trashp, trashp_d)
            cx.op("pool", lambda e: e.memset(onesm[:], 1.0 / 512.0), full=[onesm.b])
            cx.op("pool", lambda e: e.memset(ones_bf[:], 1.0), full=[ones_bf.b])
            cx.op("pool", lambda e: e.memset(ones_f[:], 1.0), full=[ones_f.b])
            cx.op("pool", lambda e: e.memset(cum[:], 0.0), full=[cum.b])
            alS = Alloc(nc)
            zt = alS.sb([128, 1024], F32, "zt")
            cx.op("pool", lambda e: e.memset(zt[:], 0.0), full=[zt.b])
            lstB = Buf("lst"); h2B = Buf("h2scr"); moeB = Buf("moescr"); x2B = Buf("x2scr")
            cx.op("sp", lambda e: e.dma_start(out=lst_d, in_=lst_init_d), full=[lstB], dma=True)
            cx.op("sp", lambda e: e.dma_start(out=h2_scr[SEQ:ROWS, :], in_=zt[:, 0:512].bitcast(BF16)),
                  reads=[zt.b], writes=[h2B], dma=True)
            moe_flat = moe_scr.rearrange("(n p) d -> n p d", p=128)
            for n in range(0, 2 * ROWS // 128):
                cx.op("sp", lambda e, n=n: e.dma_start(out=moe_flat[n], in_=zt[:, 0:512].bitcast(BF16)),
                      reads=[zt.b], writes=[moeB], dma=True)

            stg2 = [alS.sb([128, 8, 256], F32, f"stgc{i}") for i in range(2)]
            s2 = [0]

            def load_cast2(dst, dst_col0, src_d, c0, c1, kch, engs=("pool", "act")):
                for cc in range(c0, c1, 256):
                    w = min(256, c1 - cc)
                    s = stg2[s2[0] % 2]
                    eng = engs[s2[0] % len(engs)]
                    s2[0] += 1
                    src = src_d[:, cc:cc + w].rearrange("(c p) n -> p c n", p=128)
                    cx.op("sp", lambda e, s=s, src=src, w=w: e.dma_start(out=s[:, 0:kch, 0:w], in_=src),
                          full=[s.b], dma=True)
                    o = dst_col0 + (cc - c0)
                    if eng == "act":
                        cx.op("act", lambda e, s=s, o=o, w=w: e.copy(out=dst[:, 0:kch, o:o + w], in_=s[:, 0:kch, 0:w]),
                              reads=[s.b], writes=[dst.b])
                    else:
                        cx.op(eng, lambda e, s=s, o=o, w=w: e.tensor_copy(out=dst[:, 0:kch, o:o + w],
                                                                          in_=s[:, 0:kch, 0:w]),
                              reads=[s.b], writes=[dst.b])

            load_cast2(wA, 0, w_in_d, 0, 1024, 8)
            load_cast2(wA, 1024, w_in_d, 1536, 2048, 8)
            load_cast2(wG, 0, w_in_d, 2048, 5120, 8)
            load_cast2(wco, 0, wco_d, 0, 1024, 4)
            load_cast2(wgl, 0, wgl_d, 0, 2048, 4)
            load_cast2(wmo, 0, wmo_d, 0, 1024, 4)
            load_cast2(wo, 0, wo_d, 0, 1024, 8)
            wkv = alS.sb([128, 8, 1024], BF16, "wkv")
            load_cast2(wkv, 0, wkv_d, 0, 1024, 8)
            gmem = alS.sb([128, D], F32, "gmem")
            ld(gmem, gmem_d.partition_broadcast(128))
            memT = alS.sb([128, 8, 256], BF16, "memT")
            mx = alS.sb([128, D], F32, "mx"); mjunk = alS.sb([128, D], BF16, "mjunk")
            mss = alS.sb([128, 1], F32, "mss"); mrt = alS.sb([128, 1], F32, "mrt"); mrs = alS.sb([128, 1], F32, "mrs")
            mh = alS.sb([128, D], BF16, "mh")
            for mt in range(2):
                cx.op("sp", lambda e, mt=mt: e.dma_start(out=mx[:], in_=mem_d[mt * 128:(mt + 1) * 128, :]),
                      full=[mx.b], dma=True)
                cx.op("act", lambda e: e.activation(out=mjunk[:], in_=mx[:], func=AF.Square, accum_out=mss[:]),
                      reads=[mx.b], full=[mjunk.b, mss.b])
                cx.op("act", lambda e: e.activation(out=mrt[:], in_=mss[:], func=AF.Sqrt, scale=1.0 / D, bias=EPS),
                      reads=[mss.b], full=[mrt.b])
                cx.op("dve", lambda e: e.reciprocal(out=mrs[:], in_=mrt[:]), reads=[mrt.b], full=[mrs.b])
                cx.op("dve", lambda e: e.scalar_tensor_tensor(out=mh[:], in0=mx[:], scalar=mrs[:, 0:1], in1=gmem[:],
                                                              op0=ALU.mult, op1=ALU.mult),
                      reads=[mx.b, mrs.b, gmem.b], full=[mh.b])
                pb = psb[mt % 2]
                for c in range(8):
                    cx.op("pe", lambda e, pb=pb, c=c: e.transpose(out=pb[:, c * 128:(c + 1) * 128],
                                                                  in_=mh[:, c * 128:(c + 1) * 128],
                                                                  identity=ident_bf[:]),
                          reads=[mh.b, ident_bf.b], writes=[pb.b])
                cx.op("act", lambda e, pb=pb, mt=mt: e.copy(out=memT[:, :, mt * 128:(mt + 1) * 128],
                                                            in_=pb[:].rearrange("p (c t) -> p c t", c=8)),
                      reads=[pb.b], writes=[memT.b])
            for hd in range(4):
                ps = getps()
                for c in range(8):
                    cx.op("pe", lambda e, ps=ps, c=c, hd=hd: e.matmul(
                        ps[:, 0:256], lhsT=wkv[:, c, hd * 128:(hd + 1) * 128], rhs=memT[:, c, :],
                        start=(c == 0), stop=(c == 7)), reads=[wkv.b, memT.b], writes=[ps.b])
                cx.op("dve", lambda e, ps=ps, hd=hd: e.tensor_copy(out=kT[:, hd, :], in_=ps[:, 0:256]),
                      reads=[ps.b], writes=[kT.b])
            for mc in range(2):
                ps = getps()
                for c in range(8):
                    cx.op("pe", lambda e, ps=ps, c=c, mc=mc: e.matmul(
                        ps[:], lhsT=memT[:, c, mc * 128:(mc + 1) * 128], rhs=wkv[:, c, 512:1024],
                        start=(c == 0), stop=(c == 7)), reads=[wkv.b, memT.b], writes=[ps.b])
                cx.op("dve", lambda e, ps=ps, mc=mc: e.tensor_copy(out=vtok[:, mc, :], in_=ps[:]),
                      reads=[ps.b], writes=[vtok.b])
            cx.barrier()
            alS.close()

            alW = Alloc(nc)
            hT = [alW.sb([128, 8, TC], BF16, "hTc0")] * 2
            ysb = [alW.sb([128, 4, TC], BF16, "ysb0")] * 2
            vbuf = alW.sb([128, 4, 30 + TC], BF16, "vbuf")
            sgt = [alW.sb([128, TC], F32, f"sgt{i}") for i in range(3)]
            cv = alW.sb([128, 4, TC], F32, "cv"); sq = alW.sb([128, 2, TC], F32, "sq")
            mean = alW.sb([128, TC], F32, "mean"); m2 = alW.sb([128, TC], F32, "m2")
            var = alW.sb([128, TC], F32, "var"); lnv = alW.sb([128, TC], F32, "lnv"); lrs = alW.sb([128, TC], F32, "lrs")
            xc = [alW.sb([128, TC], F32, f"xc{i}") for i in range(2)]
            cn = alW.sb([128, 4, TC], BF16, "cn")
            qb = alW.sb([128, 4, TC], BF16, "qb")
            Eb = [alW.sb([128, 2, TC], BF16, f"Eb{i}") for i in range(2)]
            rden = alW.sb([128, TC], F32, "rden")
            ob = alW.sb([128, 4, TC], BF16, "ob")
            macc = alW.sb([128, TC], F32, "macc"); mt1 = alW.sb([128, TC], F32, "mt1"); mt2 = alW.sb([128, TC], F32, "mt2")
            merged = alW.sb([128, 8, TC], BF16, "merged")
            xt2 = [alW.sb([128, D], F32, "xtc0")] * 2
            x2t = xt2
            h2f = alW.sb([128, D], F32, "h2f"); h2b = [alW.sb([128, D], BF16, "h2b0")] * 2
            junk2 = h2b[0]
            h2T = alW.sb([128, 8, 128], F32, "h2T")
            ss2 = alW.sb([128, 1], F32, "ss2"); rt2 = alW.sb([128, 1], F32, "rt2"); rs2 = alW.sb([128, 1], F32, "rs2")
            cx.op("pool", lambda e: e.memset(vbuf[:], 0.0), full=[vbuf.b])

            breg = {}

            def mmgrp(ps_ap, ps_b, pairs, reads):
                n = len(pairs)
                for idx, (l, r_) in enumerate(pairs):
                    cx.op("pe", lambda e, l=l, r_=r_, idx=idx: e.matmul(ps_ap, lhsT=l, rhs=r_, start=(idx == 0),
                                                                         stop=(idx == n - 1)),
                          reads=reads, writes=[ps_b])

            KCUT = int(os.environ.get("KCUT", "9"))
            KNB = int(os.environ.get("KNB", str(NBC)))
            for bi in range(NBC if KCUT >= 2 else 0):
                if bi >= KNB:
                    break
                t0 = bi * TC
                h = hT[bi % 2]; yb = ysb[bi % 2]
                cx.op("sp", lambda e, h=h, t0=t0: e.dma_start(
                    out=h[:], in_=hT_scr[:, :, t0:t0 + TC].rearrange("c p t -> p c t")), full=[h.b], dma=True)
                cx.op("sp", lambda e, yb=yb, t0=t0: e.dma_start(
                    out=yb[:], in_=ys_scr[:, :, t0:t0 + TC].rearrange("f p t -> p f t")), full=[yb.b], dma=True)
                for f in range(4):
                    pa = getps(); pg = getps()
                    mmgrp(pa[:, 0:TC], pa.b, [(wA[:, c, f * 128:(f + 1) * 128], h[:, c, :]) for c in range(8)],
                          [wA.b, h.b])
                    mmgrp(pg[:, 0:TC], pg.b, [(wA[:, c, 512 + f * 128:512 + (f + 1) * 128], h[:, c, :]) for c in range(8)],
                          [wA.b, h.b])
                    s = sgt[f % 3]
                    cx.op("act", lambda e, pg=pg, s=s: e.activation(out=s[:], in_=pg[:, 0:TC], func=AF.Sigmoid),
                          reads=[pg.b], full=[s.b])
                    cx.op("dve", lambda e, pa=pa, s=s, f=f: e.tensor_tensor(out=vbuf[:, f, 30:30 + TC], in0=pa[:, 0:TC],
                                                                            in1=s[:], op=ALU.mult),
                          reads=[pa.b, s.b], writes=[vbuf.b])
                for f in range(4):
                    pc = getps()
                    Dg = Dg2[f % 2]
                    for k in range(31):
                        cx.op("pool", lambda e, Dg=Dg, f=f, k=k: e.tensor_scalar(
                            out=Dg[:, k, :], in0=ident_f[:], scalar1=cdw[:, f, k:k + 1], scalar2=1.0,
                            op0=ALU.mult, op1=ALU.mult), reads=[ident_f.b, cdw.b], writes=[Dg.b])
                    mmgrp(pc[:, 0:TC], pc.b, [(Dg[:, k, :], vbuf[:, f, k:k + TC]) for k in range(31)],
                          [Dg.b, vbuf.b])
                    cx.op("act", lambda e, pc=pc, f=f: e.activation(out=cv[:, f, :], in_=pc[:, 0:TC], func=AF.Identity,
                                                                    bias=cb[:, f:f + 1], scale=1.0),
                          reads=[pc.b, cb.b], writes=[cv.b])
                cx.op("pool", lambda e: e.tensor_copy(out=vbuf[:, :, 0:30], in_=vbuf[:, :, TC:TC + 30]),
                      reads=[vbuf.b], writes=[vbuf.b])
                pm = getps(); pq = getps()
                mmgrp(pm[:, 0:TC], pm.b, [(onesm[:], cv[:, f, :]) for f in range(4)], [onesm.b, cv.b])
                sqb = [Buf("sq0"), Buf("sq1")]
                for f in range(4):
                    cx.op("act", lambda e, f=f: e.activation(out=sq[:, f % 2, :], in_=cv[:, f, :], func=AF.Square),
                          reads=[cv.b], full=[sqb[f % 2]])
                    cx.op("pe", lambda e, pq=pq, f=f: e.matmul(pq[:, 0:TC], lhsT=onesm[:], rhs=sq[:, f % 2, :],
                                                               start=(f == 0), stop=(f == 3)),
                          reads=[onesm.b, sqb[f % 2]], writes=[pq.b])
                cx.op("act", lambda e, pm=pm: e.copy(out=mean[:], in_=pm[:, 0:TC]), reads=[pm.b], full=[mean.b])
                cx.op("dve", lambda e: e.tensor_tensor(out=m2[:], in0=mean[:], in1=mean[:], op=ALU.mult),
                      reads=[mean.b], full=[m2.b])
                cx.op("dve", lambda e, pq=pq: e.tensor_tensor(out=var[:], in0=pq[:, 0:TC], in1=m2[:], op=ALU.subtract),
                      reads=[pq.b, m2.b], full=[var.b])
                cx.op("dve", lambda e: e.tensor_scalar(out=var[:], in0=var[:], scalar1=float(EPS), scalar2=None,
                                                       op0=ALU.add), reads=[var.b], writes=[var.b])
                cx.op("act", lambda e: e.activation(out=lnv[:], in_=var[:], func=AF.Ln), reads=[var.b], full=[lnv.b])
                cx.op("act", lambda e: e.activation(out=lrs[:], in_=lnv[:], func=AF.Exp, scale=-0.5),
                      reads=[lnv.b], full=[lrs.b])
                for f in range(4):
                    x_ = xc[f % 2]
                    cx.op("dve", lambda e, x_=x_, f=f: e.tensor_tensor(out=x_[:], in0=cv[:, f, :], in1=mean[:],
                                                                       op=ALU.subtract),
                          reads=[cv.b, mean.b], full=[x_.b])
                    cx.op("dve", lambda e, x_=x_: e.tensor_tensor(out=x_[:], in0=x_[:], in1=lrs[:], op=ALU.mult),
                          reads=[lrs.b], writes=[x_.b])
                    cx.op("act", lambda e, x_=x_, f=f: e.activation(out=cn[:, f, :], in_=x_[:], func=AF.Silu,
                                                                    bias=lnb[:, f:f + 1], scale=lng[:, f:f + 1]),
                          reads=[x_.b, lnb.b, lng.b], writes=[cn.b])
                for hd in range(4):
                    pq_ = getps()
                    mmgrp(pq_[:, 0:TC], pq_.b, [(wA[:, c, 1024 + hd * 128:1024 + (hd + 1) * 128], h[:, c, :])
                                                for c in range(8)], [wA.b, h.b])
                    cx.op("act", lambda e, pq_=pq_, hd=hd: e.copy(out=qb[:, hd, :], in_=pq_[:, 0:TC]),
                          reads=[pq_.b], writes=[qb.b])
                for hd in range(4):
                    E = Eb[hd % 2]
                    for mc in range(2):
                        psc = getps()
                        mmgrp(psc[:, 0:TC], psc.b, [(kT[:, hd, mc * 128:(mc + 1) * 128], qb[:, hd, :])], [kT.b, qb.b])
                        cx.op("act", lambda e, psc=psc, E=E, mc=mc: e.activation(
                            out=E[:, mc, :], in_=psc[:, 0:TC], func=AF.Exp, scale=float(128 ** -0.5)),
                            reads=[psc.b], writes=[E.b])
                    po = getps(); pd = getps()
                    mmgrp(po[:, 0:TC], po.b, [(vtok[:, mc, hd * 128:(hd + 1) * 128], E[:, mc, :]) for mc in range(2)],
                          [vtok.b, E.b])
                    mmgrp(pd[:, 0:TC], pd.b, [(ones_bf[:], E[:, mc, :]) for mc in range(2)], [ones_bf.b, E.b])
                    cx.op("dve", lambda e, pd=pd: e.reciprocal(out=rden[:], in_=pd[:, 0:TC]), reads=[pd.b], full=[rden.b])
                    cx.op("dve", lambda e, po=po, hd=hd: e.tensor_tensor(out=ob[:, hd, :], in0=po[:, 0:TC], in1=rden[:],
                                                                         op=ALU.mult),
                          reads=[po.b, rden.b], writes=[ob.b])
                for j in range(8):
                    js = slice(j * 128, (j + 1) * 128)
                    pga = getps(); pyc = getps()
                    mmgrp(pga[:, 0:TC], pga.b, [(wG[:, c, j * 128:(j + 1) * 128], h[:, c, :]) for c in range(8)], [wG.b, h.b])
                    mmgrp(pyc[:, 0:TC], pyc.b, [(wco[:, f, js], cn[:, f, :]) for f in range(4)], [wco.b, cn.b])
                    s = sgt[0]
                    cx.op("act", lambda e, pga=pga, s=s: e.activation(out=s[:], in_=pga[:, 0:TC], func=AF.Sigmoid),
                          reads=[pga.b], full=[s.b])
                    cx.op("dve", lambda e, pyc=pyc, s=s: e.tensor_tensor(out=macc[:], in0=pyc[:, 0:TC], in1=s[:], op=ALU.mult),
                          reads=[pyc.b, s.b], full=[macc.b])
                    pgb = getps(); pza = getps(); pzb = getps()
                    mmgrp(pgb[:, 0:TC], pgb.b, [(wG[:, c, 1024 + j * 128:1024 + (j + 1) * 128], h[:, c, :]) for c in range(8)],
                          [wG.b, h.b])
                    mmgrp(pza[:, 0:TC], pza.b, [(wgl[:, f, js], yb[:, f, :]) for f in range(4)], [wgl.b, yb.b])
                    mmgrp(pzb[:, 0:TC], pzb.b, [(wgl[:, f, 1024 + j * 128:1024 + (j + 1) * 128], yb[:, f, :]) for f in range(4)],
                          [wgl.b, yb.b])
                    sb_ = sgt[1]; sz = sgt[2]
                    cx.op("act", lambda e, pgb=pgb, sb_=sb_: e.activation(out=sb_[:], in_=pgb[:, 0:TC], func=AF.Sigmoid),
                          reads=[pgb.b], full=[sb_.b])
                    cx.op("act", lambda e, pzb=pzb, sz=sz: e.activation(out=sz[:], in_=pzb[:, 0:TC], func=AF.Sigmoid),
                          reads=[pzb.b], full=[sz.b])
                    cx.op("dve", lambda e, pza=pza, sz=sz: e.tensor_tensor(out=mt1[:], in0=pza[:, 0:TC], in1=sz[:], op=ALU.mult),
                          reads=[pza.b, sz.b], full=[mt1.b])
                    cx.op("dve", lambda e, sb_=sb_: e.tensor_tensor(out=mt1[:], in0=mt1[:], in1=sb_[:], op=ALU.mult),
                          reads=[sb_.b], writes=[mt1.b])
                    cx.op("dve", lambda e: e.tensor_tensor(out=macc[:], in0=macc[:], in1=mt1[:], op=ALU.add),
                          reads=[mt1.b], writes=[macc.b])
                    pgc = getps(); pym = getps()
                    mmgrp(pgc[:, 0:TC], pgc.b, [(wG[:, c, 2048 + j * 128:2048 + (j + 1) * 128], h[:, c, :]) for c in range(8)],
                          [wG.b, h.b])
                    mmgrp(pym[:, 0:TC], pym.b, [(wmo[:, hd, js], ob[:, hd, :]) for hd in range(4)], [wmo.b, ob.b])
                    s = sgt[0]
                    cx.op("act", lambda e, pgc=pgc, s=s: e.activation(out=s[:], in_=pgc[:, 0:TC], func=AF.Sigmoid),
                          reads=[pgc.b], full=[s.b])
                    cx.op("dve", lambda e, pym=pym, s=s: e.tensor_tensor(out=mt2[:], in0=pym[:, 0:TC], in1=s[:], op=ALU.mult),
                          reads=[pym.b, s.b], full=[mt2.b])
                    cx.op("dve", lambda e, j=j: e.tensor_tensor(out=merged[:, j, :], in0=macc[:], in1=mt2[:], op=ALU.add),
                          reads=[macc.b, mt2.b], writes=[merged.b])
                for tt in range(TC // 128):
                    ti = bi * (TC // 128) + tt
                    pp = ti % 2
                    xt_ = xt2[pp]; x2 = x2t[pp]; hb2 = h2b[pp]
                    cx.op("sp", lambda e, xt_=xt_, ti=ti: e.dma_start(out=xt_[:], in_=x_d[ti * 128:(ti + 1) * 128, :]),
                          full=[xt_.b], dma=True)
                    for half in range(2):
                        po_ = getps()
                        mmgrp(po_[:], po_.b, [(merged[:, j, tt * 128:(tt + 1) * 128], wo[:, j, half * 512:(half + 1) * 512])
                                              for j in range(8)], [merged.b, wo.b])
                        cx.op("dve", lambda e, po_=po_, x2=x2, xt_=xt_, half=half: e.tensor_tensor(
                            out=x2[:, half * 512:(half + 1) * 512], in0=po_[:], in1=xt_[:, half * 512:(half + 1) * 512],
                            op=ALU.add), reads=[po_.b, xt_.b], writes=[x2.b])
                    cx.op("sp", lambda e, x2=x2, ti=ti: e.dma_start(out=x2_scr[ti * 128:(ti + 1) * 128, :], in_=x2[:]),
                          reads=[x2.b], writes=[x2B], dma=True)
                    cx.op("act", lambda e, x2=x2: e.activation(out=junk2[:], in_=x2[:], func=AF.Square, accum_out=ss2[:]),
                          reads=[x2.b], full=[junk2.b, ss2.b])
                    cx.op("act", lambda e: e.activation(out=rt2[:], in_=ss2[:], func=AF.Sqrt, scale=1.0 / D, bias=EPS),
                          reads=[ss2.b], full=[rt2.b])
                    cx.op("dve", lambda e: e.reciprocal(out=rs2[:], in_=rt2[:]), reads=[rt2.b], full=[rs2.b])
                    cx.op("dve", lambda e, x2=x2: e.scalar_tensor_tensor(out=h2f[:], in0=x2[:], scalar=rs2[:, 0:1],
                                                                         in1=gffn[:], op0=ALU.mult, op1=ALU.mult),
                          reads=[x2.b, rs2.b, gffn.b], full=[h2f.b])
                    cx.op("act", lambda e, hb2=hb2: e.copy(out=hb2[:], in_=h2f[:]), reads=[h2f.b], full=[hb2.b])
                    cx.op("sp", lambda e, hb2=hb2, ti=ti: e.dma_start(out=h2_scr[ti * 128:(ti + 1) * 128, :], in_=hb2[:]),
                          reads=[hb2.b], writes=[h2B], dma=True)
                    pra = getps(); prb = getps()
                    for c in range(8):
                        pr = pra if c < 4 else prb
                        cx.op("pe", lambda e, pr=pr, c=c: e.transpose(out=pr[:, (c % 4) * 128:(c % 4 + 1) * 128],
                                                                      in_=h2f[:, c * 128:(c + 1) * 128], identity=ident_f[:]),
                              reads=[h2f.b, ident_f.b], writes=[pr.b])
                    cx.op("act", lambda e, pra=pra: e.copy(out=h2T[:, 0:4, :], in_=pra[:].rearrange("p (c t) -> p c t", c=4)),
                          reads=[pra.b], writes=[h2T.b])
                    cx.op("dve", lambda e, prb=prb: e.tensor_copy(out=h2T[:, 4:8, :],
                                                                  in_=prb[:].rearrange("p (c t) -> p c t", c=4)),
                          reads=[prb.b], writes=[h2T.b])
                    plg = getps()
                    mmgrp(plg[:, 0:36], plg.b, [(h2T[:, c, :], wr[:, c, :]) for c in range(8)], [h2T.b, wr.b])

                    cx.op("dve", lambda e, plg=plg, ti=ti: e.tensor_tensor(out=lg_all[:, ti, :], in0=plg[:, 0:36],
                                                                        in1=rbias[:], op=ALU.add),
                          reads=[plg.b, rbias.b], writes=[lg_all.b])
            cx.barrier()
            alW.close()
            alR = Alloc(nc)
            RS = Buf("route")

            def rd(fn, extra_reads=(), extra_writes=()):
                cx.op("dve", fn, reads=[RS, lg_all.b] + list(extra_reads), writes=[RS] + list(extra_writes))

            def R(shape, name, dt=F32):
                return alR.sb(shape, dt, name)

            NTT = NT
            NEB_ = NEXP * NBLK
            gmax = R([128, NTT], "gmax"); ohg = R([128, NTT, 4], "ohg"); eg = R([128, NTT, 4], "eg")
            sumg = R([128, NTT], "sumg"); ptop = R([128, NTT], "ptop")
            selm = R([128, NTT, 4, 8], "selm"); sel = R([128, NTT, 8], "sel"); sel2 = R([128, NTT, 8], "sel2")
            m1_ = R([128, NTT], "m1_"); m2_ = R([128, NTT], "m2_"); oh1 = R([128, NTT, 8], "oh1"); oh2 = R([128, NTT, 8], "oh2")
            dm = R([128, NTT], "dm"); w1 = R([128, NTT], "w1"); w2 = R([128, NTT], "w2")
            M1 = R([128, NTT, 4, 8], "M1"); M2 = R([128, NTT, 4, 8], "M2"); Mc = R([128, NTT, 32], "Mc")
            Cex = R([128, NTT, 32], "Cex"); pos = R([128, NTT, 32], "pos"); bk = R([128, NTT, 32], "bk")
            sf = R([128, NTT, 32], "sf"); ov = R([128, NTT, 32], "ov"); tq = R([128, NTT, 32], "tq")
            sk = [R([128, NTT], f"sk{k}") for k in range(2)]; okk = R([128, NTT], "okk"); dd = R([128, NTT], "dd")
            si = [R([128, NTT], f"si{k}", I32) for k in range(2)]
            ent = [R([128, NTT, 4], f"ent{k}") for k in range(2)]
            le4 = lg_all[:, :, 4:36].rearrange("p t (g j) -> p t g j", g=4)

            def bc3(a, n):
                return a.unsqueeze(2).to_broadcast([128, NTT, n])

            rd(lambda e: e.tensor_reduce(out=gmax[:], in_=lg_all[:, :, 0:4], axis=AX.X, op=ALU.max))
            rd(lambda e: e.tensor_tensor(out=ohg[:], in0=lg_all[:, :, 0:4], in1=bc3(gmax[:], 4), op=ALU.is_equal))
            rd(lambda e: e.tensor_tensor(out=eg[:], in0=lg_all[:, :, 0:4], in1=bc3(gmax[:], 4), op=ALU.subtract))
            cx.op("act", lambda e: e.activation(out=eg[:], in_=eg[:], func=AF.Exp), reads=[RS], writes=[RS])
            rd(lambda e: e.tensor_reduce(out=sumg[:], in_=eg[:], axis=AX.X, op=ALU.add))
            rd(lambda e: e.reciprocal(out=ptop[:], in_=sumg[:]))
            rd(lambda e: e.tensor_tensor(out=selm[:], in0=le4,
                                         in1=ohg[:].unsqueeze(3).to_broadcast([128, NTT, 4, 8]), op=ALU.mult))
            rd(lambda e: e.tensor_reduce(out=sel[:], in_=selm[:].rearrange("p t g j -> p t j g"), axis=AX.X, op=ALU.add))
            rd(lambda e: e.tensor_reduce(out=m1_[:], in_=sel[:], axis=AX.X, op=ALU.max))
            rd(lambda e: e.tensor_tensor(out=oh1[:], in0=sel[:], in1=bc3(m1_[:], 8), op=ALU.is_equal))
            rd(lambda e: e.scalar_tensor_tensor(out=sel2[:], in0=oh1[:], scalar=-1e30, in1=sel[:], op0=ALU.mult, op1=ALU.add))
            rd(lambda e: e.tensor_reduce(out=m2_[:], in_=sel2[:], axis=AX.X, op=ALU.max))
            rd(lambda e: e.tensor_tensor(out=oh2[:], in0=sel2[:], in1=bc3(m2_[:], 8), op=ALU.is_equal))
            rd(lambda e: e.tensor_tensor(out=dm[:], in0=m1_[:], in1=m2_[:], op=ALU.subtract))
            cx.op("act", lambda e: e.activation(out=w1[:], in_=dm[:], func=AF.Sigmoid), reads=[RS], writes=[RS])
            rd(lambda e: e.tensor_tensor(out=w1[:], in0=w1[:], in1=ptop[:], op=ALU.mult))
            rd(lambda e: e.tensor_tensor(out=w2[:], in0=ptop[:], in1=w1[:], op=ALU.subtract))
            rd(lambda e: e.tensor_tensor(out=M1[:], in0=ohg[:].unsqueeze(3).to_broadcast([128, NTT, 4, 8]),
                                         in1=oh1[:].unsqueeze(2).to_broadcast([128, NTT, 4, 8]), op=ALU.mult))
            rd(lambda e: e.tensor_tensor(out=M2[:], in0=ohg[:].unsqueeze(3).to_broadcast([128, NTT, 4, 8]),
                                         in1=oh2[:].unsqueeze(2).to_broadcast([128, NTT, 4, 8]), op=ALU.mult))
            rd(lambda e: e.tensor_tensor(out=Mc[:], in0=M1[:].rearrange("p t g j -> p t (g j)"),
                                         in1=M2[:].rearrange("p t g j -> p t (g j)"), op=ALU.add))
            rd(lambda e: e.memset(Cex[:, 0, :], 0.0))
            for i in range(1, NTT):
                rd(lambda e, i=i: e.tensor_tensor(out=Cex[:, i, :], in0=Cex[:, i - 1, :], in1=Mc[:, i - 1, :], op=ALU.add))
            pp = [getps(), getps()]
            for i in range(NTT):
                pb_ = pp[i // 16]
                o_ = pb_[:, (i % 16) * 32:(i % 16 + 1) * 32]
                cx.op("pe", lambda e, o_=o_, i=i: e.matmul(o_, lhsT=tri[:], rhs=Mc[:, i, :], start=True, stop=False),
                      reads=[tri.b, RS], writes=[pb_.b])
                cx.op("pe", lambda e, o_=o_, i=i: e.matmul(o_, lhsT=ones_f[:], rhs=Cex[:, i, :], start=False, stop=True),
                      reads=[ones_f.b, RS], writes=[pb_.b])
            for hh in range(2):
                rd(lambda e, hh=hh: e.tensor_copy(out=pos[:, hh * 16:(hh + 1) * 16, :],
                                                  in_=pp[hh][:].rearrange("p (t x) -> p t x", t=16)), [pp[hh].b])
            rd(lambda e: e.tensor_single_scalar(out=bk[:], in_=pos[:], scalar=127.5, op=ALU.is_gt))
            for thr in range(2, NBLK):
                rd(lambda e, thr=thr: e.tensor_single_scalar(out=tq[:], in_=pos[:], scalar=128.0 * thr - 0.5, op=ALU.is_gt))
                rd(lambda e: e.tensor_tensor(out=bk[:], in0=bk[:], in1=tq[:], op=ALU.add))
            rd(lambda e: e.scalar_tensor_tensor(out=bk[:], in0=bk[:], scalar=float(1 - 128 * NEB_),
                                                in1=ecap[:].unsqueeze(1).to_broadcast([128, NTT, 32]),
                                                op0=ALU.mult, op1=ALU.add), [ecap.b])
            rd(lambda e: e.scalar_tensor_tensor(out=sf[:], in0=pos[:], scalar=float(NEB_), in1=bk[:],
                                                op0=ALU.mult, op1=ALU.add))
            rd(lambda e: e.tensor_single_scalar(out=ov[:], in_=pos[:], scalar=float(CAP) - 0.5, op=ALU.is_gt))
            for k, (Mk, wk) in enumerate(((M1, w1), (M2, w2))):
                Mk32 = Mk[:].rearrange("p t g j -> p t (g j)")
                rd(lambda e, Mk32=Mk32: e.tensor_tensor(out=tq[:], in0=Mk32, in1=sf[:], op=ALU.mult))
                rd(lambda e, k=k: e.tensor_reduce(out=sk[k][:], in_=tq[:], axis=AX.X, op=ALU.add))
                rd(lambda e, Mk32=Mk32: e.tensor_tensor(out=tq[:], in0=Mk32, in1=ov[:], op=ALU.mult))
                rd(lambda e: e.tensor_reduce(out=okk[:], in_=tq[:], axis=AX.X, op=ALU.add))
                rd(lambda e, k=k: e.tensor_scalar(out=dd[:], in0=sk[k][:], scalar1=trashp[:, 0:1], scalar2=None,
                                                  op0=ALU.subtract), [trashp.b])
                rd(lambda e: e.tensor_tensor(out=dd[:], in0=dd[:], in1=okk[:], op=ALU.mult))
                rd(lambda e, k=k: e.tensor_tensor(out=sk[k][:], in0=sk[k][:], in1=dd[:], op=ALU.subtract))
                rd(lambda e, k=k: e.tensor_copy(out=si[k][:], in_=sk[k][:]), (), [si[k].b])
                rd(lambda e, k=k: e.memset(ent[k][:], 0.0), (), [ent[k].b])
                rd(lambda e, k=k: e.tensor_copy(out=ent[k][:, :, 0], in_=tokid[:]), [tokid.b], [ent[k].b])
                rd(lambda e, k=k: e.tensor_scalar(out=ent[k][:, :, 1], in0=tokid[:], scalar1=float(k * ROWS), scalar2=None,
                                                  op0=ALU.add), [tokid.b], [ent[k].b])
                rd(lambda e, k=k, wk=wk: e.tensor_copy(out=ent[k][:, :, 2], in_=wk[:]), (), [ent[k].b])
            for i in range(NTT):
                for k in range(2):
                    cx.op("pool", lambda e, i=i, k=k: e.indirect_dma_start(
                        out=lst_d, out_offset=bass.IndirectOffsetOnAxis(ap=si[k][:, i:i + 1], axis=0),
                        in_=ent[k][:, i, :], in_offset=None),
                        reads=[si[k].b, ent[k].b], writes=[lstB], dma=True)
            cx.barrier()
            alR.close()
            alC.close()
        if stop_after in ("A", "B", "C"):
            pass
        else:
            alD = Alloc(nc)
            NEB = NEXP * NBLK
            lst_sb = alD.sb([128, NEB, 4], F32, "lst_sb")
            idx_i = alD.sb([128, NEB], I32, "idx_i")
            dst_i = alD.sb([128, NEB], I32, "dst_i")
            cx.op("sp", lambda e: e.dma_start(out=lst_sb[:], in_=lst_d[0:NEXP * CAP, :].rearrange("(s eb) w -> s eb w", s=128)),
                  reads=[lstB], full=[lst_sb.b], dma=True)
            cx.op("dve", lambda e: e.tensor_copy(out=idx_i[:], in_=lst_sb[:, :, 0]), reads=[lst_sb.b], full=[idx_i.b])
            cx.op("dve", lambda e: e.tensor_copy(out=dst_i[:], in_=lst_sb[:, :, 1]), reads=[lst_sb.b], full=[dst_i.b])
            sg_ = [alD.sb([128, 8, 256], F32, f"sg{i}") for i in range(2)]
            su_ = [alD.sb([128, 8, 256], F32, f"su{i}") for i in range(2)]
            sd_ = [alD.sb([128, 2, 1024], F32, f"sd{i}") for i in range(2)]
            Wg = [alD.sb([128, 8, 256], BF16, f"Wg{i}") for i in range(2)]
            Wu = [alD.sb([128, 8, 256], BF16, f"Wu{i}") for i in range(2)]
            Wd = [alD.sb([128, 2, 1024], BF16, f"Wd{i}") for i in range(2)]
            Gt = [alD.sb([128, D], BF16, f"Gt{i}") for i in range(3)]
            Xe = [alD.sb([128, 8, CAP], BF16, f"Xe{i}") for i in range(2)]
            sgl = [alD.sb([128, CAP], F32, f"sgl{i}") for i in range(2)]
            ae = [alD.sb([128, 2, CAP], BF16, f"ae{i}") for i in range(2)]
            Yt = [alD.sb([128, D], BF16, f"Yt{i}") for i in range(3)]

            Gt6 = Gt + [alD.sb([128, D], BF16, f"Gtx{i}") for i in range(3)]

            def load_w_dma(e_):
                p = e_ % 2
                cx.op("sp", lambda e: e.dma_start(out=sg_[p][:], in_=weg_d[e_]), full=[sg_[p].b], dma=True)
                cx.op("sp", lambda e: e.dma_start(out=su_[p][:], in_=weu_d[e_]), full=[su_[p].b], dma=True)
                cx.op("sp", lambda e: e.dma_start(out=sd_[p][:], in_=wed_d[e_]), full=[sd_[p].b], dma=True)

            def load_w_cast(e_):
                p = e_ % 2
                cx.op("dve", lambda e: e.tensor_copy(out=Wg[p][:], in_=sg_[p][:]), reads=[sg_[p].b], full=[Wg[p].b])
                cx.op("act", lambda e: e.copy(out=Wu[p][:], in_=su_[p][:]), reads=[su_[p].b], full=[Wu[p].b])
                cx.op("pool", lambda e: e.tensor_copy(out=Wd[p][:], in_=sd_[p][:]), reads=[sd_[p].b], full=[Wd[p].b])

            def gathers(e_):
                for blk in range(NBLK):
                    eb = e_ * NBLK + blk
                    G = Gt6[(e_ % 2) * 3 + blk]
                    cx.op("pool", lambda e, G=G, eb=eb: e.indirect_dma_start(
                        out=G[:], out_offset=None, in_=h2_scr,
                        in_offset=bass.IndirectOffsetOnAxis(ap=idx_i[:, eb:eb + 1], axis=0)),
                        reads=[idx_i.b, h2B], full=[G.b], dma=True)

            gi = [0]
            load_w_dma(0)
            gathers(0)
            load_w_cast(0)
            KNE = int(os.environ.get("KNE", str(NEXP)))
            for e_ in range(KNE):
                p = e_ % 2
                if e_ + 1 < NEXP:
                    load_w_dma(e_ + 1)
                    gathers(e_ + 1)
                X = Xe[p]
                for blk in range(NBLK):
                    G = Gt6[(e_ % 2) * 3 + blk]
                    pbk = psb[gi[0] % 2]
                    gi[0] += 1
                    for c in range(8):
                        cx.op("pe", lambda e, pbk=pbk, G=G, c=c: e.transpose(
                            out=pbk[:, c * 128:(c + 1) * 128], in_=G[:, c * 128:(c + 1) * 128], identity=ident_bf[:]),
                            reads=[G.b, ident_bf.b], writes=[pbk.b])
                    if blk % 2 == 0:
                        cx.op("act", lambda e, pbk=pbk, X=X, blk=blk: e.copy(
                            out=X[:, :, blk * 128:(blk + 1) * 128], in_=pbk[:].rearrange("p (c t) -> p c t", c=8)),
                            reads=[pbk.b], writes=[X.b])
                    else:
                        cx.op("dve", lambda e, pbk=pbk, X=X, blk=blk: e.tensor_copy(
                            out=X[:, :, blk * 128:(blk + 1) * 128], in_=pbk[:].rearrange("p (c t) -> p c t", c=8)),
                            reads=[pbk.b], writes=[X.b])
                a_ = ae[p]
                for ft in range(2):
                    pg = getps(); pu = getps()
                    for c in range(8):
                        cx.op("pe", lambda e, pg=pg, c=c, ft=ft, X=X, p=p: e.matmul(
                            pg[:, 0:CAP], lhsT=Wg[p][:, c, ft * 128:(ft + 1) * 128], rhs=X[:, c, :],
                            start=(c == 0), stop=(c == 7)), reads=[Wg[p].b, X.b], writes=[pg.b])
                    for c in range(8):
                        cx.op("pe", lambda e, pu=pu, c=c, ft=ft, X=X, p=p: e.matmul(
                            pu[:, 0:CAP], lhsT=Wu[p][:, c, ft * 128:(ft + 1) * 128], rhs=X[:, c, :],
                            start=(c == 0), stop=(c == 7)), reads=[Wu[p].b, X.b], writes=[pu.b])
                    s = sgl[ft]
                    cx.op("act", lambda e, pg=pg, s=s: e.activation(out=s[:], in_=pg[:, 0:CAP], func=AF.Silu),
                          reads=[pg.b], full=[s.b])
                    cx.op("dve", lambda e, pu=pu, s=s, a_=a_, ft=ft: e.tensor_tensor(
                        out=a_[:, ft, :], in0=pu[:, 0:CAP], in1=s[:], op=ALU.mult),
                        reads=[pu.b, s.b], writes=[a_.b])
                for blk in range(NBLK):
                    eb = e_ * NBLK + blk
                    Y = Yt[eb % 3]
                    for half in range(2):
                        py = getps()
                        for ft in range(2):
                            cx.op("pe", lambda e, py=py, ft=ft, blk=blk, half=half, a_=a_, p=p: e.matmul(
                                py[:], lhsT=a_[:, ft, blk * 128:(blk + 1) * 128],
                                rhs=Wd[p][:, ft, half * 512:(half + 1) * 512], start=(ft == 0), stop=(ft == 1)),
                                reads=[a_.b, Wd[p].b], writes=[py.b])
                        if half == 0:
                            cx.op("dve", lambda e, py=py, Y=Y, eb=eb: e.tensor_scalar(
                                out=Y[:, 0:512], in0=py[:], scalar1=lst_sb[:, eb, 2:3], scalar2=None, op0=ALU.mult),
                                reads=[py.b, lst_sb.b], writes=[Y.b])
                        else:
                            cx.op("act", lambda e, py=py, Y=Y, eb=eb: e.activation(
                                out=Y[:, 512:1024], in_=py[:], func=AF.Copy, scale=lst_sb[:, eb, 2:3]),
                                reads=[py.b, lst_sb.b], writes=[Y.b])
                    cx.op("pool", lambda e, Y=Y, eb=eb: e.indirect_dma_start(
                        out=moe_scr, out_offset=bass.IndirectOffsetOnAxis(ap=dst_i[:, eb:eb + 1], axis=0),
                        in_=Y[:], in_offset=None), reads=[Y.b, dst_i.b], writes=[moeB], dma=True)
                if e_ + 1 < NEXP:
                    load_w_cast(e_ + 1)
            cx.barrier()
            alD.close()

            alE = Alloc(nc)
            gfin = alE.sb([128, D], F32, "gfin")
            cx.op("sp", lambda e: e.dma_start(out=gfin[:], in_=gfin_d.partition_broadcast(128)), full=[gfin.b], dma=True)
            xa = [alE.sb([128, D], F32, f"xa{i}") for i in range(2)]
            m0 = [alE.sb([128, D], BF16, f"m0{i}") for i in range(2)]
            m1 = [alE.sb([128, D], BF16, f"m1{i}") for i in range(2)]
            ot = [alE.sb([128, D], F32, f"ot{i}") for i in range(2)]
            junk3 = alE.sb([128, D], BF16, "junk3")
            sse = [alE.sb([128, 1], F32, f"sse{i}") for i in range(2)]
            rte = [alE.sb([128, 1], F32, f"rte{i}") for i in range(2)]
            rse = [alE.sb([128, 1], F32, f"rse{i}") for i in range(2)]
            outB = Buf("out")
            for ti in range(NT):
                p = ti % 2
                rows = slice(ti * 128, (ti + 1) * 128)
                cx.op("sp", lambda e, p=p, rows=rows: e.dma_start(out=xa[p][:], in_=x2_scr[rows, :]),
                      reads=[x2B], full=[xa[p].b], dma=True)
                cx.op("sp", lambda e, p=p, rows=rows: e.dma_start(out=m0[p][:], in_=moe_scr[rows, :]),
                      reads=[moeB], full=[m0[p].b], dma=True)
                cx.op("sp", lambda e, p=p, ti=ti: e.dma_start(
                    out=m1[p][:], in_=moe_scr[ROWS + ti * 128:ROWS + (ti + 1) * 128, :]),
                    reads=[moeB], full=[m1[p].b], dma=True)
                cx.op("pool", lambda e, p=p: e.tensor_tensor(out=xa[p][:], in0=xa[p][:], in1=m0[p][:], op=ALU.add),
                      reads=[m0[p].b], writes=[xa[p].b])
                cx.op("dve", lambda e, p=p: e.tensor_tensor(out=xa[p][:], in0=xa[p][:], in1=m1[p][:], op=ALU.add),
                      reads=[m1[p].b], writes=[xa[p].b])
                cx.op("act", lambda e, p=p: e.activation(out=junk3[:], in_=xa[p][:], func=AF.Square, accum_out=sse[p][:]),
                      reads=[xa[p].b], full=[junk3.b, sse[p].b])
                cx.op("act", lambda e, p=p: e.activation(out=rte[p][:], in_=sse[p][:], func=AF.Sqrt, scale=1.0 / D, bias=EPS),
                      reads=[sse[p].b], full=[rte[p].b])
                cx.op("dve", lambda e, p=p: e.reciprocal(out=rse[p][:], in_=rte[p][:]), reads=[rte[p].b], full=[rse[p].b])
                cx.op("dve", lambda e, p=p: e.scalar_tensor_tensor(out=ot[p][:], in0=xa[p][:], scalar=rse[p][:, 0:1],
                                                                   in1=gfin[:], op0=ALU.mult, op1=ALU.mult),
                      reads=[xa[p].b, rse[p].b, gfin.b], full=[ot[p].b])
                cx.op("sp", lambda e, p=p, rows=rows: e.dma_start(out=out_d[rows, :], in_=ot[p][:]),
                      reads=[ot[p].b], writes=[outB], dma=True)
            cx.barrier()
            alE.close()
        cx.barrier()
        cx.emit(block)
        print("waits", cx.nwait, "instrs", {e: cx.cnt[e] for e in cx.ENG}, "signals", {e: len(cx.waited[e]) for e in cx.ENG})
    return nc


def host_consts():
    c = {}
    c["ident_bf"] = np.eye(128, dtype=np.float32).astype(ml_dtypes.bfloat16)
    c["ident_f"] = np.eye(128, dtype=np.float32)
    psel = np.zeros((128, 8, 240), np.float32)
    for a in range(8):
        for i in range(16):
            psel[a * 16 + i, a, 7 * 16 + i] = 1.0
    c["psel"] = psel.astype(ml_dtypes.bfloat16)
    kk = np.arange(128) // 16
    c["cmask"] = (kk[None, :] >= kk[:, None]).astype(np.float32)
    c["tri"] = (np.arange(128)[:, None] < np.arange(128)[None, :]).astype(np.float32)
    c["ecap"] = np.ascontiguousarray(np.broadcast_to((np.arange(32) * NBLK).astype(np.float32)[None, :], (128, 32)))
    c["tokid"] = (np.arange(NT)[None, :] * 128 + np.arange(128)[:, None]).astype(np.float32)
    li = np.zeros((NEXP * CAP + 128, 4), np.float32)
    li[:, 0] = SEQ + ((np.arange(NEXP * CAP + 128) // (NEXP * NBLK)) % 128)
    li[:, 1] = li[:, 0]
    c["trashp"] = (NEXP * CAP + np.arange(128)).astype(np.float32).reshape(128, 1)
    c["lst_init"] = li
    return c


def relayout_pc(w):
    E, K, N = w.shape
    return np.ascontiguousarray(w.reshape(E, K // 128, 128, N).transpose(0, 2, 1, 3))


def pair_layout(a):
    rest = a.shape[2:]
    a = a.reshape((16, 2, 64) + rest)
    a = np.moveaxis(a, 0, 2)
    return np.ascontiguousarray(a.reshape((128, 16) + rest))


def make_inmap(inputs, b, consts=None):
    f = lambda a: np.ascontiguousarray(a, dtype=np.float32)
    m = {"x": f(inputs["x"][b]),
         "g_mix": f(inputs["g_mix"]),
         "w_in": f(inputs["w_in"][0])}
    m["lamre_l"] = pair_layout(f(inputs["ssm_lambda_re"][0]))
    m["lamim_l"] = pair_layout(f(inputs["ssm_lambda_im"][0]))
    m["logdt_l"] = pair_layout(np.broadcast_to(f(inputs["ssm_log_dt"][0])[:, None], (32, 64)))
    m["bre_l"] = pair_layout(f(inputs["ssm_b_re"][0]))
    m["bim_l"] = pair_layout(f(inputs["ssm_b_im"][0]))
    m["cre_l"] = pair_layout(f(inputs["ssm_c_re"][0]).transpose(0, 2, 1))
    m["cim_l"] = pair_layout(f(inputs["ssm_c_im"][0]).transpose(0, 2, 1))
    m["d_l"] = np.ascontiguousarray(np.tile(f(inputs["ssm_d"][0]).reshape(32, 16).T, (8, 1)))
    m["mem"] = f(inputs["mem"][b])
    for k_, n_ in (("g_mem", "g_mem"), ("g_ffn", "g_ffn")):
        m[n_] = f(inputs[k_])
    m["g_final"] = f(inputs["g_final"]).reshape(1, D)
    m["w_mem_kv"] = f(inputs["w_mem_kv"][0]); m["w_mem_out"] = f(inputs["w_mem_out"][0])
    m["w_conv_out"] = f(inputs["w_conv_out"][0]); m["w_ssm_glu"] = f(inputs["w_ssm_glu"][0])
    m["w_out"] = f(inputs["w_out"][0])
    m["w_router"] = np.ascontiguousarray(np.concatenate([f(inputs["w_router_group"][0]),
                                                         f(inputs["w_router_expert"][0])], axis=1))
    m["b_router"] = np.ascontiguousarray(np.concatenate([f(inputs["b_router_group"][0]),
                                                         f(inputs["b_router_expert"][0])])[None, :])
    m["cdw_l"] = np.ascontiguousarray(f(inputs["conv_dw"][0]).T.reshape(4, 128, 31).transpose(1, 0, 2))
    m["cb_l"] = np.ascontiguousarray(f(inputs["conv_dw_bias"][0]).reshape(4, 128).T)
    m["lng_l"] = np.ascontiguousarray(f(inputs["conv_ln_g"][0]).reshape(4, 128).T)
    m["lnb_l"] = np.ascontiguousarray(f(inputs["conv_ln_b"][0]).reshape(4, 128).T)
    if consts is not None and "w_exp_gate" in consts:
        for k_ in ("w_exp_gate", "w_exp_up", "w_exp_down"):
            m[k_] = consts[k_]
    else:
        m["w_exp_gate"] = relayout_pc(f(inputs["w_exp_gate"][0]))
        m["w_exp_up"] = relayout_pc(f(inputs["w_exp_up"][0]))
        m["w_exp_down"] = relayout_pc(f(inputs["w_exp_down"][0]))
    m.update(consts if consts is not None else host_consts())
    return m


def kernel(**inputs):
    nc = build()
    consts = host_consts()
    f32 = lambda a: np.ascontiguousarray(a, dtype=np.float32)
    for k_ in ("w_exp_gate", "w_exp_up", "w_exp_down"):
        consts[k_] = relayout_pc(f32(inputs[k_][0]))
    in_maps = [make_inmap(inputs, b, consts) for b in range(NCORES)]
    res = run_bass_kernel_spmd(nc, in_maps, core_ids=list(range(NCORES)))
    return np.stack([r["out"] for r in res.results], axis=0)
```

```python
import os
import numpy as np
import ml_dtypes
from contextlib import ExitStack
import concourse.bass as bass
import concourse.mybir as mybir
from concourse.bass_utils import run_bass_kernel_spmd

F32 = mybir.dt.float32
BF16 = mybir.dt.bfloat16
I32 = mybir.dt.int32
U32 = mybir.dt.uint32
AF = mybir.ActivationFunctionType
ALU = mybir.AluOpType
AX = mybir.AxisListType
GELU = AF.Gelu_apprx_tanh

D = 1024
SEQ = 4096
NCORES = 8
T = 512
NB = SEQ // T
NT = SEQ // 128
EPS = 1e-6
NEXP = 32
CAP = 384
NBLK = CAP // 128
ROWS = SEQ + 128


class Buf:
    __slots__ = ("name", "w", "r")

    def __init__(self, name):
        self.name = name
        self.w = {}
        self.r = {}


class Ctx:
    ENG = ("pe", "dve", "act", "pool", "sp")
    KROT = 4
    NDMA = 12

    def __init__(self, nc, es):
        self.nc = nc
        self.q = {e: [] for e in self.ENG}
        self.cnt = {e: 0 for e in self.ENG}
        self.seen = {e: {} for e in self.ENG}
        self.esem = {e: [es.enter_context(nc.semaphore(f"s_{e}{i}")) for i in range(self.KROT)]
                     for e in self.ENG}
        self.dsem = {e: [es.enter_context(nc.semaphore(f"d_{e}{i}")) for i in range(self.NDMA)]
                     for e in ("sp", "act", "pool")}
        self.dcnt = {e: [0] * self.NDMA for e in self.dsem}
        self.dnext = {e: 0 for e in self.dsem}
        self.nwait = 0
        self.waited = {e: set() for e in self.ENG}

    def _wait(self, eng, tok):
        key, val = tok
        if key[0] == 'e' and key[1] == eng and eng == "pe":
            return
        if self.seen[eng].get(key, -1) >= val:
            return
        self.seen[eng][key] = val
        if key[0] == 'e':
            self.waited[key[1]].add(val)
        self.q[eng].append(("w", key, val))
        self.nwait += 1

    def op(self, eng, fn, reads=(), writes=(), full=(), dma=False):
        toks = []
        for b in reads:
            toks.extend(b.w.items())
        for b in tuple(writes) + tuple(full):
            toks.extend(b.w.items())
            toks.extend(b.r.items())
        for t in toks:
            self._wait(eng, t)
        if dma:
            i = self.dnext[eng]
            self.dnext[eng] = (i + 1) % self.NDMA
            key = ('d', eng, i)
            if self.dcnt[eng][i] > 0:
                self._wait(eng, (key, self.dcnt[eng][i]))
            self.dcnt[eng][i] += 16
            val = self.dcnt[eng][i]
            self.q[eng].append(("d", fn, self.dsem[eng][i]))
        else:
            key = ('e', eng)
            val = self.cnt[eng]
            self.cnt[eng] += 1
            self.q[eng].append(("i", fn, val))
        for b in reads:
            b.r[key] = val
        for b in full:
            b.w = {key: val}
            b.r = {}
        for b in writes:
            b.w[key] = val
        return (key, val)

    def barrier(self):
        toks = []
        for e in self.ENG:
            if self.cnt[e] > 0:
                toks.append((('e', e), self.cnt[e] - 1))
        for e in self.dsem:
            for i in range(self.NDMA):
                if self.dcnt[e][i] > 0:
                    toks.append((('d', e, i), self.dcnt[e][i]))
        for e in self.ENG:
            for t in toks:
                self._wait(e, t)

    def emit(self, block):
        nc = self.nc

        rank = {e: {v: i for i, v in enumerate(sorted(self.waited[e]))} for e in self.ENG}
        K_ = self.KROT

        def run(engname, engine):
            for item in self.q[engname]:
                if item[0] == "w":
                    key, val = item[1], item[2]
                    if key[0] == 'e':
                        r = rank[key[1]][val]
                        engine.wait_ge(self.esem[key[1]][r % K_], r // K_ + 1)
                    else:
                        engine.wait_ge(self.dsem[key[1]][key[2]], val)
                elif item[0] == "d":
                    item[1](engine).then_inc(item[2], 16)
                else:
                    ins = item[1](engine)
                    r = rank[engname].get(item[2])
                    if r is not None:
                        ins.then_inc(self.esem[engname][r % K_], 1)

        @block.tensor
        def _(e):
            run("pe", e)

        @block.vector
        def _(e):
            run("dve", e)

        @block.scalar
        def _(e):
            run("act", e)

        @block.gpsimd
        def _(e):
            run("pool", e)

        @block.sync
        def _(e):
            run("sp", e)


class TT:
    def __init__(self, t, name):
        self.t = t
        self.b = Buf(name)

    def __getitem__(self, k):
        return self.t[k]


class Alloc:
    cnt = [0]

    def __init__(self, nc, es=None):
        self.nc = nc
        self.es = es if es is not None else ExitStack()

    @property
    def n(self):
        return Alloc.cnt[0]

    @n.setter
    def n(self, v):
        Alloc.cnt[0] = v

    def close(self):
        self.es.close()

    def sb(self, shape, dt, name=None):
        self.n += 1
        name = name or f"sb{self.n}"
        t = self.es.enter_context(self.nc.sbuf_tensor(f"{name}_{self.n}", list(shape), dt))
        return TT(t, name)

    def ps(self, shape, dt, name=None):
        self.n += 1
        name = name or f"ps{self.n}"
        t = self.es.enter_context(self.nc.psum_tensor(f"{name}_{self.n}", list(shape), dt))
        return TT(t, name)


def build(stop_after="E", dbg=False):
    nc = bass.Bass("TRN2", target_bir_lowering=False)
    dram = {}

    def din(name, shape, dt=F32):
        dram[name] = nc.dram_tensor(name, list(shape), dt, kind="ExternalInput").ap()
        return dram[name]

    def dscr(name, shape, dt, kind="Internal"):
        dram[name] = nc.dram_tensor(name, list(shape), dt, kind=kind).ap()
        return dram[name]

    x_d = din("x", [SEQ, D])
    gmix_d = din("g_mix", [1, D])
    w_in_d = din("w_in", [D, 5120])
    ident_bf_d = din("ident_bf", [128, 128], BF16)
    ident_f_d = din("ident_f", [128, 128], F32)
    lamre_d = din("lamre_l", [128, 16])
    lamim_d = din("lamim_l", [128, 16])
    logdt_d = din("logdt_l", [128, 16])
    bre_d = din("bre_l", [128, 16, 16])
    bim_d = din("bim_l", [128, 16, 16])
    cre_d = din("cre_l", [128, 16, 16])
    cim_d = din("cim_l", [128, 16, 16])
    dl_d = din("d_l", [128, 32])
    psel_d = din("psel", [128, 8, 240], BF16)
    cmask_d = din("cmask", [128, 128])
    mem_d = din("mem", [256, D])
    gmem_d = din("g_mem", [1, D])
    gffn_d = din("g_ffn", [1, D])
    gfin_d = din("g_final", [1, D])
    wkv_d = din("w_mem_kv", [D, 1024])
    wmo_d = din("w_mem_out", [512, D])
    wco_d = din("w_conv_out", [512, D])
    wgl_d = din("w_ssm_glu", [512, 2048])
    wo_d = din("w_out", [D, D])
    wr_d = din("w_router", [D, 36])
    rbias_d = din("b_router", [1, 36])
    cdw_d = din("cdw_l", [128, 4, 31])
    cb_d = din("cb_l", [128, 4])
    lng_d = din("lng_l", [128, 4])
    lnb_d = din("lnb_l", [128, 4])
    tri_d = din("tri", [128, 128])
    ecap_d = din("ecap", [128, 32])
    tokid_d = din("tokid", [128, NT])
    lst_init_d = din("lst_init", [NEXP * CAP + 128, 4])
    trashp_d = din("trashp", [128, 1])
    weg_d = din("w_exp_gate", [NEXP, 128, 8, 256])
    weu_d = din("w_exp_up", [NEXP, 128, 8, 256])
    wed_d = din("w_exp_down", [NEXP, 128, 2, D])
    dk = "ExternalOutput" if dbg else "Internal"
    lst_d = dscr("lst", [NEXP * CAP + 128, 4], F32, kind=dk)
    h2_scr = dscr("h2_scr", [ROWS, D], BF16, kind=dk)
    moe_scr = dscr("moe_scr", [2 * ROWS, D], BF16, kind=dk)
    x2_scr = dscr("x2_scr", [SEQ, D], F32, kind=dk)
    ys_scr = dscr("ys_scr", [4, 128, SEQ], BF16, kind="ExternalOutput" if dbg else "Internal")
    out_d = dscr("out", [SEQ, D], F32, kind="ExternalOutput")
    hT_scr = dscr("hT_scr", [8, 128, SEQ], BF16, kind="ExternalOutput" if dbg else "Internal")
    u_dbg = dscr("u_dbg", [4, 128, SEQ], BF16, kind="ExternalOutput") if dbg else None

    with ExitStack() as es:
        cx = Ctx(nc, es)
        al = Alloc(nc, es)
        block = es.enter_context(nc.Block())

        ident_bf = al.sb([128, 128], BF16, "ident_bf")
        cx.op("sp", lambda e: e.dma_start(out=ident_bf[:], in_=ident_bf_d), full=[ident_bf.b], dma=True)
        ident_f = al.sb([128, 128], F32, "ident_f")
        cx.op("sp", lambda e: e.dma_start(out=ident_f[:], in_=ident_f_d), full=[ident_f.b], dma=True)

        psum = [al.ps([128, 512], F32, f"bank{i}") for i in range(6)]
        psb = [al.ps([128, 1024], BF16, f"bankb{i}") for i in range(2)]
        pctr = [0]

        def getps():
            p = psum[pctr[0] % len(psum)]
            pctr[0] += 1
            return p

        alAB = Alloc(nc)
        u_all = alAB.sb([128, 4, SEQ], BF16, "u_all")
        M_all = alAB.sb([128, 32, 128], BF16, "M_all")
        W2r = alAB.sb([128, 16, 2, 128], BF16, "W2r"); W2i = alAB.sb([128, 16, 2, 128], BF16, "W2i")
        C1r = alAB.sb([128, 16, 128], BF16, "C1r"); nC1i = alAB.sb([128, 16, 128], BF16, "nC1i")
        KAr = alAB.sb([128, 9, 16], F32, "KAr"); KAi = alAB.sb([128, 9, 16], F32, "KAi")
        KnAi = alAB.sb([128, 9, 16], F32, "KnAi")
        psel = alAB.sb([128, 8, 240], BF16, "psel")
        cx.op("sp", lambda e: e.dma_start(out=psel[:], in_=psel_d), full=[psel.b], dma=True)
        al_outer = al
        al = Alloc(nc)
        gmix = al.sb([128, D], F32, "gmix")
        cx.op("sp", lambda e: e.dma_start(out=gmix[:], in_=gmix_d.partition_broadcast(128)),
              full=[gmix.b], dma=True)

        stg = [al.sb([128, 8, 256], F32, f"stg{i}") for i in range(2)]
        sctr = [0]

        def load_cast(dst, dst_col0, src_d, c0, c1, kch):
            for cc in range(c0, c1, 256):
                w = min(256, c1 - cc)
                s = stg[sctr[0] % 2]
                sctr[0] += 1
                src = src_d[:, cc:cc + w].rearrange("(c p) n -> p c n", p=128)
                cx.op("sp", lambda e, s=s, src=src, w=w: e.dma_start(out=s[:, 0:kch, 0:w], in_=src),
                      full=[s.b], dma=True)
                o = dst_col0 + (cc - c0)
                cx.op("pool", lambda e, s=s, o=o, w=w: e.tensor_copy(out=dst[:, 0:kch, o:o + w],
                                                                     in_=s[:, 0:kch, 0:w]),
                      reads=[s.b], writes=[dst.b])

        w_ssm_in = al.sb([128, 8, 512], BF16, "w_ssm_in")
        load_cast(w_ssm_in, 0, w_in_d, 1024, 1536, 8)
        xt = [al.sb([128, D], F32, f"xt{i}") for i in range(2)]
        junk = al.sb([128, D], BF16, "junk")
        ss = [al.sb([128, 1], F32, f"ss{i}") for i in range(2)]
        rt = [al.sb([128, 1], F32, f"rt{i}") for i in range(2)]
        rstd = [al.sb([128, 1], F32, f"rstd{i}") for i in range(2)]
        hbf = [al.sb([128, D], BF16, f"hbf{i}") for i in range(2)]
        hTb = [al.sb([128, 8, T], BF16, f"hTb{i}") for i in range(2)]

        for i in range(NT):
            p = i % 2
            blk = i // 4
            hb = hTb[blk % 2]
            cx.op("sp", lambda e, p=p, i=i: e.dma_start(out=xt[p][:], in_=x_d[i * 128:(i + 1) * 128, :]),
                  full=[xt[p].b], dma=True)
            cx.op("act", lambda e, p=p: e.activation(out=junk[:], in_=xt[p][:], func=AF.Square,
                                                     accum_out=ss[p][:]),
                  reads=[xt[p].b], writes=[junk.b], full=[ss[p].b])
            cx.op("act", lambda e, p=p: e.activation(out=rt[p][:], in_=ss[p][:], func=AF.Sqrt,
                                                     scale=1.0 / D, bias=EPS),
                  reads=[ss[p].b], full=[rt[p].b])
            cx.op("dve", lambda e, p=p: e.reciprocal(out=rstd[p][:], in_=rt[p][:]),
                  reads=[rt[p].b], full=[rstd[p].b])
            cx.op("dve", lambda e, p=p: e.scalar_tensor_tensor(out=hbf[p][:], in0=xt[p][:],
                                                               scalar=rstd[p][:, 0:1], in1=gmix[:],
                                                               op0=ALU.mult, op1=ALU.mult),
                  reads=[xt[p].b, rstd[p].b, gmix.b], full=[hbf[p].b])
            pb = psb[i % 2]
            for c in range(8):
                cx.op("pe", lambda e, pb=pb, p=p, c=c: e.transpose(out=pb[:, c * 128:(c + 1) * 128],
                                                                   in_=hbf[p][:, c * 128:(c + 1) * 128],
                                                                   identity=ident_bf[:]),
                      reads=[hbf[p].b, ident_bf.b], writes=[pb.b])
            tt = i % 4
            cx.op("act", lambda e, pb=pb, hb=hb, tt=tt: e.copy(
                out=hb[:, :, tt * 128:(tt + 1) * 128],
                in_=pb[:].rearrange("p (c t) -> p c t", c=8)),
                reads=[pb.b], writes=[hb.b])
            if tt == 3:
                for f in range(4):
                    ps = getps()
                    for c in range(8):
                        cx.op("pe", lambda e, ps=ps, hb=hb, f=f, c=c: e.matmul(
                            ps[:], lhsT=w_ssm_in[:, c, f * 128:(f + 1) * 128], rhs=hb[:, c, :],
                            start=(c == 0), stop=(c == 7)),
                            reads=[w_ssm_in.b, hb.b], writes=[ps.b])
                    cx.op("dve", lambda e, ps=ps, f=f, blk=blk: e.tensor_copy(
                        out=u_all[:, f, blk * T:(blk + 1) * T], in_=ps[:]),
                        reads=[ps.b], writes=[u_all.b])
                cx.op("sp", lambda e, hb=hb, blk=blk: e.dma_start(
                    out=hT_scr[:, :, blk * T:(blk + 1) * T].rearrange("c p t -> p c t"), in_=hb[:]),
                    reads=[hb.b], dma=True)

        if dbg:
            cx.op("sp", lambda e: e.dma_start(out=u_dbg.rearrange("f p t -> p f t"), in_=u_all[:]),
                  reads=[u_all.b], dma=True)


        cx.barrier()
        al.close()
        al = Alloc(nc)
        TWO_PI = 2.0 * np.pi
        cmask = al.sb([128, 128], F32, "cmask")
        cx.op("sp", lambda e: e.dma_start(out=cmask[:], in_=cmask_d), full=[cmask.b], dma=True)
        dl = al.sb([128, 32], F32, "dl")
        cx.op("sp", lambda e: e.dma_start(out=dl[:], in_=dl_d), full=[dl.b], dma=True)
        SU = Buf("ssm_setup")

        def sload(shape, src, name):
            t = al.sb(shape, F32, name)
            cx.op("sp", lambda e: e.dma_start(out=t[:], in_=src), full=[t.b], dma=True)
            return t

        lamre = sload([128, 16], lamre_d, "lamre")
        lamim = sload([128, 16], lamim_d, "lamim")
        logdt = sload([128, 16], logdt_d, "logdt")
        Bre = sload([128, 16, 16], bre_d, "Bre")
        Bim = sload([128, 16, 16], bim_d, "Bim")
        Cre = sload([128, 16, 16], cre_d, "Cre")
        Cim = sload([128, 16, 16], cim_d, "Cim")
        ins_b = [lamre.b, lamim.b, logdt.b, Bre.b, Bim.b, Cre.b, Cim.b]

        def S(shape, name):
            return al.sb(shape, F32, name)

        def dv(fn):
            cx.op("dve", fn, reads=ins_b, writes=[SU])

        def ac(fn):
            cx.op("act", fn, reads=ins_b, writes=[SU])

        def tt_(out, a, b, op):
            dv(lambda e: e.tensor_tensor(out=out, in0=a, in1=b, op=op))

        sh16 = [128, 16]
        dt_ = S(sh16, "dt"); lrd = S(sh16, "lrd"); th = S(sh16, "th")
        ac(lambda e: e.activation(out=dt_[:], in_=logdt[:], func=AF.Exp))
        tt_(lrd[:], lamre[:], dt_[:], ALU.mult)
        tt_(th[:], lamim[:], dt_[:], ALU.mult)
        mag = S(sh16, "mag"); imag2 = S(sh16, "imag2")
        ac(lambda e: e.activation(out=mag[:], in_=lrd[:], func=AF.Exp))
        ac(lambda e: e.activation(out=imag2[:], in_=lrd[:], func=AF.Exp, scale=-2.0))
        kq_i = al.sb(sh16, I32, "kq_i"); kq = S(sh16, "kq"); red = S(sh16, "red"); msk = S(sh16, "msk")
        sinv = S(sh16, "sinv"); cosv = S(sh16, "cosv"); tmpa = S(sh16, "tmpa")

        def sin_of(outt, shift):
            dv(lambda e: e.tensor_scalar(out=tmpa[:], in0=th[:], scalar1=float(shift), scalar2=None,
                                         op0=ALU.add))
            dv(lambda e: e.tensor_scalar(out=kq[:], in0=tmpa[:], scalar1=float(1.0 / TWO_PI),
                                         scalar2=None, op0=ALU.mult))
            dv(lambda e: e.tensor_copy(out=kq_i[:], in_=kq[:]))
            dv(lambda e: e.tensor_copy(out=kq[:], in_=kq_i[:]))
            dv(lambda e: e.scalar_tensor_tensor(out=red[:], in0=kq[:], scalar=float(-TWO_PI),
                                                in1=tmpa[:], op0=ALU.mult, op1=ALU.add))
            dv(lambda e: e.tensor_single_scalar(out=msk[:], in_=red[:], scalar=float(np.pi), op=ALU.is_gt))
            dv(lambda e: e.scalar_tensor_tensor(out=red[:], in0=msk[:], scalar=float(-TWO_PI),
                                                in1=red[:], op0=ALU.mult, op1=ALU.add))
            dv(lambda e: e.tensor_single_scalar(out=msk[:], in_=red[:], scalar=float(-np.pi), op=ALU.is_lt))
            dv(lambda e: e.scalar_tensor_tensor(out=red[:], in0=msk[:], scalar=float(TWO_PI),
                                                in1=red[:], op0=ALU.mult, op1=ALU.add))
            ac(lambda e: e.activation(out=outt[:], in_=red[:], func=AF.Sin))

        sin_of(sinv, 0.0)
        sin_of(cosv, np.pi / 2)
        PWr = S([128, 9, 16], "PWr"); PWi = S([128, 9, 16], "PWi")
        IPr = S([128, 8, 16], "IPr"); IPi = S([128, 8, 16], "IPi")
        t1 = S([128, 16, 8, 16], "t1"); t2 = S([128, 16, 8, 16], "t2")

        def cmul(outr, outi, ar, ai, br, bi, shp, neg_i=False):
            a1 = t1[:].rearrange("p a b c -> p (a b c)")[:, 0:int(np.prod(shp[1:]))]
            a2 = t2[:].rearrange("p a b c -> p (a b c)")[:, 0:int(np.prod(shp[1:]))]
            if len(shp) == 3:
                a1 = a1.rearrange("p (a b) -> p a b", a=shp[1])
                a2 = a2.rearrange("p (a b) -> p a b", a=shp[1])
            tt_(a1, ar, br, ALU.mult)
            tt_(a2, ai, bi, ALU.mult)
            tt_(outr, a1, a2, ALU.subtract)
            tt_(a1, ar, bi, ALU.mult)
            tt_(a2, ai, br, ALU.mult)
            if neg_i:
                dv(lambda e: e.scalar_tensor_tensor(out=outi, in0=a1, scalar=-1.0, in1=a2,
                                                    op0=ALU.mult, op1=ALU.subtract))
            else:
                tt_(outi, a1, a2, ALU.add)

        dv(lambda e: e.memset(PWr[:, 0, :], 1.0))
        dv(lambda e: e.memset(PWi[:, 0, :], 0.0))
        dv(lambda e: e.memset(IPr[:, 0, :], 1.0))
        dv(lambda e: e.memset(IPi[:, 0, :], 0.0))
        tt_(PWr[:, 1, :], mag[:], cosv[:], ALU.mult)
        tt_(PWi[:, 1, :], mag[:], sinv[:], ALU.mult)
        tt_(IPr[:, 1, :], PWr[:, 1, :], imag2[:], ALU.mult)
        dv(lambda e: e.scalar_tensor_tensor(out=IPi[:, 1, :], in0=PWi[:, 1, :], scalar=-1.0, in1=imag2[:],
                                            op0=ALU.mult, op1=ALU.mult))
        for n in range(2, 9):
            cmul(PWr[:, n, :], PWi[:, n, :], PWr[:, n - 1, :], PWi[:, n - 1, :], PWr[:, 1, :], PWi[:, 1, :], sh16)
        for n in range(2, 8):
            cmul(IPr[:, n, :], IPi[:, n, :], IPr[:, n - 1, :], IPi[:, n - 1, :], IPr[:, 1, :], IPi[:, 1, :], sh16)
        dv(lambda e: e.tensor_copy(out=KAr[:, 0, :], in_=PWr[:, 8, :]))
        dv(lambda e: e.tensor_copy(out=KAi[:, 0, :], in_=PWi[:, 8, :]))
        for d_ in range(1, 9):
            cmul(KAr[:, d_, :], KAi[:, d_, :], KAr[:, d_ - 1, :], KAi[:, d_ - 1, :],
                 KAr[:, d_ - 1, :], KAi[:, d_ - 1, :], sh16)
        dv(lambda e: e.tensor_scalar(out=KnAi[:], in0=KAi[:], scalar1=-1.0, scalar2=None, op0=ALU.mult))
        am1 = S(sh16, "am1"); l2 = S(sh16, "l2"); il2 = S(sh16, "il2"); kr = S(sh16, "kr"); ki = S(sh16, "ki")
        dv(lambda e: e.tensor_scalar(out=am1[:], in0=PWr[:, 1, :], scalar1=-1.0, scalar2=None, op0=ALU.add))
        tt_(l2[:], lamre[:], lamre[:], ALU.mult)
        tt_(tmpa[:], lamim[:], lamim[:], ALU.mult)
        tt_(l2[:], l2[:], tmpa[:], ALU.add)
        dv(lambda e: e.reciprocal(out=il2[:], in_=l2[:]))
        tt_(kr[:], am1[:], lamre[:], ALU.mult)
        tt_(tmpa[:], PWi[:, 1, :], lamim[:], ALU.mult)
        tt_(kr[:], kr[:], tmpa[:], ALU.add)
        tt_(kr[:], kr[:], il2[:], ALU.mult)
        tt_(ki[:], PWi[:, 1, :], lamre[:], ALU.mult)
        tt_(tmpa[:], am1[:], lamim[:], ALU.mult)
        tt_(ki[:], ki[:], tmpa[:], ALU.subtract)
        tt_(ki[:], ki[:], il2[:], ALU.mult)
        sh3 = [128, 16, 16]

        def bc(a):
            return a.unsqueeze(2).to_broadcast(sh3)

        Bbr = S(sh3, "Bbr"); Bbi = S(sh3, "Bbi")
        cmul(Bbr[:], Bbi[:], bc(kr[:]), bc(ki[:]), Bre[:], Bim[:], sh3)
        Bhr = S([128, 16, 8, 16], "Bhr"); nBhi = S([128, 16, 8, 16], "nBhi"); Bhi = S([128, 16, 8, 16], "Bhi")
        Btr = S([128, 16, 8, 16], "Btr"); Bti = S([128, 16, 8, 16], "Bti")
        Chr = S([128, 16, 9, 16], "Chr"); Chi = S([128, 16, 9, 16], "Chi"); nChi = S([128, 16, 9, 16], "nChi")
        for k in range(8):
            cmul(Bhr[:, :, k, :], Bhi[:, :, k, :], bc(IPr[:, k, :]), bc(IPi[:, k, :]), Bbr[:], Bbi[:], sh3)
            cmul(Btr[:, :, k, :], Bti[:, :, k, :], bc(PWr[:, 7, :]), bc(PWi[:, 7, :]),
                 Bhr[:, :, k, :], Bhi[:, :, k, :], sh3)
        dv(lambda e: e.tensor_scalar(out=nBhi[:], in0=Bhi[:], scalar1=-1.0, scalar2=None, op0=ALU.mult))
        for j in range(9):
            cmul(Chr[:, :, j, :], Chi[:, :, j, :], bc(PWr[:, j, :]), bc(PWi[:, j, :]), Cre[:], Cim[:], sh3)
        dv(lambda e: e.tensor_scalar(out=nChi[:], in0=Chi[:], scalar1=-1.0, scalar2=None, op0=ALU.mult))
        dv(lambda e: e.tensor_copy(out=C1r[:].rearrange("p r (j c) -> p r j c", j=8), in_=Chr[:, :, 1:9, :]))
        dv(lambda e: e.tensor_copy(out=nC1i[:].rearrange("p r (j c) -> p r j c", j=8), in_=nChi[:, :, 1:9, :]))
        mtmp = S([128, 128], "mtmp")
        cx.op("pool", lambda e: e.memset(W2r[:], 0.0), reads=ins_b, writes=[SU])
        cx.op("pool", lambda e: e.memset(W2i[:], 0.0), reads=ins_b, writes=[SU])
        for r in range(16):
            for two in range(2):
                g = 2 * r + two
                rng = slice(two * 64, (two + 1) * 64)
                ps = getps()
                cx.op("pe", lambda e, ps=ps, r=r, rng=rng: e.matmul(
                    ps[:, 0:128], lhsT=Bhr[rng, r, :, :].rearrange("p k c -> p (k c)"),
                    rhs=Chr[rng, r, 0:8, :].rearrange("p j c -> p (j c)"), start=True, stop=False),
                    reads=[SU], writes=[ps.b])
                cx.op("pe", lambda e, ps=ps, r=r, rng=rng: e.matmul(
                    ps[:, 0:128], lhsT=nBhi[rng, r, :, :].rearrange("p k c -> p (k c)"),
                    rhs=Chi[rng, r, 0:8, :].rearrange("p j c -> p (j c)"), start=False, stop=True),
                    reads=[SU], writes=[ps.b])
                cx.op("dve", lambda e, ps=ps: e.tensor_tensor(out=mtmp[:], in0=ps[:, 0:128], in1=cmask[:],
                                                              op=ALU.mult),
                      reads=[ps.b, cmask.b], writes=[SU])
                cx.op("dve", lambda e, g=g: e.scalar_tensor_tensor(
                    out=M_all[:, g, :], in0=ident_f[:], scalar=dl[:, g:g + 1], in1=mtmp[:],
                    op0=ALU.mult, op1=ALU.add),
                    reads=[ident_f.b, dl.b], writes=[SU, M_all.b])
            for (Bt, W2) in ((Btr, W2r), (Bti, W2i)):
                ps = getps()
                cx.op("pe", lambda e, ps=ps, r=r, Bt=Bt: e.transpose(
                    out=ps[:, 0:128], in_=Bt[:, r, :, :].rearrange("p k c -> p (k c)"), identity=ident_f[:]),
                    reads=[SU, ident_f.b], writes=[ps.b])
                cx.op("dve", lambda e, ps=ps, r=r, W2=W2: e.tensor_copy(out=W2[:, r, 0, 0:64], in_=ps[:, 0:64]),
                      reads=[ps.b], writes=[SU, W2.b])
                cx.op("dve", lambda e, ps=ps, r=r, W2=W2: e.tensor_copy(out=W2[:, r, 1, 64:128], in_=ps[:, 64:128]),
                      reads=[ps.b], writes=[SU, W2.b])

        cx.barrier()
        al.close()
        al = Alloc(nc)
        NCH = SEQ // 8
        Vg = [al.sb([128, NCH], BF16, f"Vg{i}") for i in range(4)]
        Sre = [[al.sb([128, NCH], F32, f"Sre{s}{i}") for i in range(2)] for s in range(2)]
        Sim = [[al.sb([128, NCH], F32, f"Sim{s}{i}") for i in range(2)] for s in range(2)]
        Sbr = [al.sb([128, NCH], BF16, f"Sbr{s}") for s in range(2)]
        Sbi = [al.sb([128, NCH], BF16, f"Sbi{s}") for s in range(2)]
        Gg = [al.sb([128, NCH], BF16, f"Gg{i}") for i in range(16)]
        ysf = [al.sb([128, SEQ], BF16, f"ysf{i}") for i in range(2)]
        for s in range(2):
            cx.op("pool", lambda e, s=s: e.memset(Sbr[s][:, 0:1], 0.0), writes=[Sbr[s].b])
            cx.op("pool", lambda e, s=s: e.memset(Sbi[s][:, 0:1], 0.0), writes=[Sbi[s].b])

        def b_front(r):
            f = r // 4
            st = r % 2
            vg = [Vg[(2 * r) % 4], Vg[(2 * r + 1) % 4]]
            for two in range(2):
                g = 2 * r + two
                gl = g % 8
                ps = getps()
                for k in range(8):
                    cx.op("pe", lambda e, ps=ps, gl=gl, k=k, f=f: e.matmul(
                        ps[:], lhsT=psel[:, gl, (7 - k) * 16:(7 - k) * 16 + 128],
                        rhs=u_all[:, f, k:SEQ:8], start=(k == 0), stop=(k == 7)),
                        reads=[psel.b, u_all.b], writes=[ps.b])
                cx.op("act", lambda e, ps=ps, v=vg[two]: e.copy(out=v[:], in_=ps[:]),
                      reads=[ps.b], full=[vg[two].b])
            psr = getps(); psi = getps()
            for (pp, W2) in ((psr, W2r), (psi, W2i)):
                for two in range(2):
                    cx.op("pe", lambda e, pp=pp, W2=W2, two=two, r=r, v=vg[two]: e.matmul(
                        pp[:], lhsT=W2[:, r, two, :], rhs=v[:], start=(two == 0), stop=(two == 1)),
                        reads=[W2.b, vg[two].b], writes=[pp.b])
            cx.op("act", lambda e, psr=psr, st=st: e.copy(out=Sre[st][0][:], in_=psr[:]),
                  reads=[psr.b], full=[Sre[st][0].b])
            cx.op("act", lambda e, psi=psi, st=st: e.copy(out=Sim[st][0][:], in_=psi[:]),
                  reads=[psi.b], full=[Sim[st][0].b])

        def b_mid(r):
            st = r % 2
            cur = 0
            for d_ in range(9):
                sh = 1 << d_
                s_r, s_i, d_r, d_i = Sre[st][cur], Sim[st][cur], Sre[st][1 - cur], Sim[st][1 - cur]
                n = NCH - sh
                cx.op("dve", lambda e, s_r=s_r, d_r=d_r, sh=sh, n=n, d_=d_, r=r: e.scalar_tensor_tensor(
                    out=d_r[:, sh:NCH], in0=s_r[:, 0:n], scalar=KAr[:, d_, r:r + 1], in1=s_r[:, sh:NCH],
                    op0=ALU.mult, op1=ALU.add), reads=[s_r.b, SU], writes=[d_r.b])
                cx.op("dve", lambda e, s_i=s_i, d_r=d_r, sh=sh, n=n, d_=d_, r=r: e.scalar_tensor_tensor(
                    out=d_r[:, sh:NCH], in0=s_i[:, 0:n], scalar=KnAi[:, d_, r:r + 1], in1=d_r[:, sh:NCH],
                    op0=ALU.mult, op1=ALU.add), reads=[s_i.b, SU], writes=[d_r.b])
                cx.op("dve", lambda e, s_i=s_i, d_i=d_i, sh=sh, n=n, d_=d_, r=r: e.scalar_tensor_tensor(
                    out=d_i[:, sh:NCH], in0=s_i[:, 0:n], scalar=KAr[:, d_, r:r + 1], in1=s_i[:, sh:NCH],
                    op0=ALU.mult, op1=ALU.add), reads=[s_i.b, SU], writes=[d_i.b])
                cx.op("dve", lambda e, s_r=s_r, d_i=d_i, sh=sh, n=n, d_=d_, r=r: e.scalar_tensor_tensor(
                    out=d_i[:, sh:NCH], in0=s_r[:, 0:n], scalar=KAi[:, d_, r:r + 1], in1=d_i[:, sh:NCH],
                    op0=ALU.mult, op1=ALU.add), reads=[s_r.b, SU], writes=[d_i.b])
                cx.op("pool", lambda e, s_r=s_r, d_r=d_r, sh=sh: e.tensor_copy(out=d_r[:, 0:sh], in_=s_r[:, 0:sh]),
                      reads=[s_r.b], writes=[d_r.b])
                cx.op("pool", lambda e, s_i=s_i, d_i=d_i, sh=sh: e.tensor_copy(out=d_i[:, 0:sh], in_=s_i[:, 0:sh]),
                      reads=[s_i.b], writes=[d_i.b])
                cur = 1 - cur
            fr, fi = Sre[st][cur], Sim[st][cur]
            cx.op("pool", lambda e, fr=fr, st=st: e.tensor_copy(out=Sbr[st][:, 1:NCH], in_=fr[:, 0:NCH - 1]),
                  reads=[fr.b], writes=[Sbr[st].b])
            cx.op("pool", lambda e, fi=fi, st=st: e.tensor_copy(out=Sbi[st][:, 1:NCH], in_=fi[:, 0:NCH - 1]),
                  reads=[fi.b], writes=[Sbi[st].b])

        def b_back(r):
            f = r // 4
            st = r % 2
            vg = [Vg[(2 * r) % 4], Vg[(2 * r + 1) % 4]]
            for two in range(2):
                g = 2 * r + two
                rng = slice(two * 64, (two + 1) * 64)
                ps = getps()
                cx.op("pe", lambda e, ps=ps, g=g, v=vg[two]: e.matmul(
                    ps[:], lhsT=M_all[:, g, :], rhs=v[:], start=True, stop=False),
                    reads=[M_all.b, vg[two].b], writes=[ps.b])
                cx.op("pe", lambda e, ps=ps, r=r, rng=rng, st=st: e.matmul(
                    ps[:], lhsT=C1r[rng, r, :], rhs=Sbr[st][rng, :], start=False, stop=False),
                    reads=[SU, Sbr[st].b], writes=[ps.b])
                cx.op("pe", lambda e, ps=ps, r=r, rng=rng, st=st: e.matmul(
                    ps[:], lhsT=nC1i[rng, r, :], rhs=Sbi[st][rng, :], start=False, stop=True),
                    reads=[SU, Sbi[st].b], writes=[ps.b])
                gg = Gg[g % 16]
                cx.op("act", lambda e, ps=ps, gg=gg: e.activation(out=gg[:], in_=ps[:], func=GELU),
                      reads=[ps.b], full=[gg.b])
            if r % 4 == 3:
                yb = ysf[f % 2]
                for j in range(8):
                    ps = getps()
                    for gl in range(8):
                        gg = Gg[(8 * f + gl) % 16]
                        cx.op("pe", lambda e, ps=ps, j=j, gl=gl, gg=gg: e.matmul(
                            ps[:], lhsT=psel[:, j, (7 - gl) * 16:(7 - gl) * 16 + 128], rhs=gg[:],
                            start=(gl == 0), stop=(gl == 7)),
                            reads=[psel.b, gg.b], writes=[ps.b])
                    cx.op("act", lambda e, ps=ps, yb=yb, j=j: e.copy(out=yb[:, j:SEQ:8], in_=ps[:]),
                          reads=[ps.b], writes=[yb.b])
                cx.op("sp", lambda e, yb=yb, f=f: e.dma_start(out=ys_scr[f], in_=yb[:]),
                      reads=[yb.b], dma=True)

        b_front(0)
        for r in range(16):
            b_mid(r)
            if r + 1 < 16:
                b_front(r + 1)
            b_back(r)

        cx.barrier()
        al.close()
        alAB.close()
        al = al_outer
        if stop_after in ("A", "B"):
            pass
        else:
            TC = 256
            NBC = SEQ // TC
            alC = Alloc(nc)
            wA = alC.sb([128, 8, 1536], BF16, "wA")
            wG = alC.sb([128, 8, 3072], BF16, "wG")
            wco = alC.sb([128, 4, 1024], BF16, "wco")
            wgl = alC.sb([128, 4, 2048], BF16, "wgl")
            wmo = alC.sb([128, 4, 1024], BF16, "wmo")
            wo = alC.sb([128, 8, 1024], BF16, "wo")
            Dg2 = [alC.sb([128, 31, 128], BF16, f"Dg{i}") for i in range(2)]
            kT = alC.sb([128, 4, 256], BF16, "kT")
            vtok = alC.sb([128, 2, 512], BF16, "vtok")
            gffn = alC.sb([128, D], F32, "gffn")
            wr = alC.sb([128, 8, 36], F32, "wr")
            rbias = alC.sb([128, 36], F32, "rbias")
            cdw = alC.sb([128, 4, 31], F32, "cdw")
            cb = alC.sb([128, 4], F32, "cb"); lng = alC.sb([128, 4], F32, "lng"); lnb = alC.sb([128, 4], F32, "lnb")
            onesm = alC.sb([128, 128], F32, "onesm")
            ones_bf = alC.sb([128, 128], BF16, "ones_bf")
            tri = alC.sb([128, 128], F32, "tri")
            ones_f = alC.sb([128, 128], F32, "ones_f")
            ecap = alC.sb([128, 32], F32, "ecap")
            tokid = alC.sb([128, NT], F32, "tokid")
            cum = alC.sb([128, 32], F32, "cum")
            lg_all = alC.sb([128, NT, 36], F32, "lg_all")
            trashp = alC.sb([128, 1], F32, "trashp")

            def ld(t, src):
                cx.op("sp", lambda e: e.dma_start(out=t[:], in_=src), full=[t.b], dma=True)

            ld(gffn, gffn_d.partition_broadcast(128))
            ld(wr, wr_d.rearrange("(c p) n -> p c n", p=128))
            ld(rbias, rbias_d.partition_broadcast(128))
            ld(cdw, cdw_d); ld(cb, cb_d); ld(lng, lng_d); ld(lnb, lnb_d)
            ld(tri, tri_d); ld(ecap, ecap_d); ld(tokid, tokid_d); ld(trashp, trashp_d)
            cx.op("pool", lambda e: e.memset(onesm[:], 1.0 / 512.0), full=[onesm.b])
            cx.op("pool", lambda e: e.memset(ones_bf[:], 1.0), full=[ones_bf.b])
            cx.op("pool", lambda e: e.memset(ones_f[:], 1.0), full=[ones_f.b])
            cx.op("pool", lambda e: e.memset(cum[:], 0.0), full=[cum.b])
            alS = Alloc(nc)
            zt = alS.sb([128, 1024], F32, "zt")
            cx.op("pool", lambda e: e.memset(zt[:], 0.0), full=[zt.b])
            lstB = Buf("lst"); h2B = Buf("h2scr"); moeB = Buf("moescr"); x2B = Buf("x2scr")
            cx.op("sp", lambda e: e.dma_start(out=lst_d, in_=lst_init_d), full=[lstB], dma=True)
            cx.op("sp", lambda e: e.dma_start(out=h2_scr[SEQ:ROWS, :], in_=zt[:, 0:512].bitcast(BF16)),
                  reads=[zt.b], writes=[h2B], dma=True)
            moe_flat = moe_scr.rearrange("(n p) d -> n p d", p=128)
            for n in range(0, 2 * ROWS // 128):
                cx.op("sp", lambda e, n=n: e.dma_start(out=moe_flat[n], in_=zt[:, 0:512].bitcast(BF16)),
                      reads=[zt.b], writes=[moeB], dma=True)

            stg2 = [alS.sb([128, 8, 256], F32, f"stgc{i}") for i in range(2)]
            s2 = [0]

            def load_cast2(dst, dst_col0, src_d, c0, c1, kch, engs=("pool", "act")):
                for cc in range(c0, c1, 256):
                    w = min(256, c1 - cc)
                    s = stg2[s2[0] % 2]
                    eng = engs[s2[0] % len(engs)]
                    s2[0] += 1
                    src = src_d[:, cc:cc + w].rearrange("(c p) n -> p c n", p=128)
                    cx.op("sp", lambda e, s=s, src=src, w=w: e.dma_start(out=s[:, 0:kch, 0:w], in_=src),
                          full=[s.b], dma=True)
                    o = dst_col0 + (cc - c0)
                    if eng == "act":
                        cx.op("act", lambda e, s=s, o=o, w=w: e.copy(out=dst[:, 0:kch, o:o + w], in_=s[:, 0:kch, 0:w]),
                              reads=[s.b], writes=[dst.b])
                    else:
                        cx.op(eng, lambda e, s=s, o=o, w=w: e.tensor_copy(out=dst[:, 0:kch, o:o + w],
                                                                          in_=s[:, 0:kch, 0:w]),
                              reads=[s.b], writes=[dst.b])

            load_cast2(wA, 0, w_in_d, 0, 1024, 8)
            load_cast2(wA, 1024, w_in_d, 1536, 2048, 8)
            load_cast2(wG, 0, w_in_d, 2048, 5120, 8)
            load_cast2(wco, 0, wco_d, 0, 1024, 4)
            load_cast2(wgl, 0, wgl_d, 0, 2048, 4)
            load_cast2(wmo, 0, wmo_d, 0, 1024, 4)
            load_cast2(wo, 0, wo_d, 0, 1024, 8)
            wkv = alS.sb([128, 8, 1024], BF16, "wkv")
            load_cast2(wkv, 0, wkv_d, 0, 1024, 8)
            gmem = alS.sb([128, D], F32, "gmem")
            ld(gmem, gmem_d.partition_broadcast(128))
            memT = alS.sb([128, 8, 256], BF16, "memT")
            mx = alS.sb([128, D], F32, "mx"); mjunk = alS.sb([128, D], BF16, "mjunk")
            mss = alS.sb([128, 1], F32, "mss"); mrt = alS.sb([128, 1], F32, "mrt"); mrs = alS.sb([128, 1], F32, "mrs")
            mh = alS.sb([128, D], BF16, "mh")
            for mt in range(2):
                cx.op("sp", lambda e, mt=mt: e.dma_start(out=mx[:], in_=mem_d[mt * 128:(mt + 1) * 128, :]),
                      full=[mx.b], dma=True)
                cx.op("act", lambda e: e.activation(out=mjunk[:], in_=mx[:], func=AF.Square, accum_out=mss[:]),
                      reads=[mx.b], full=[mjunk.b, mss.b])
                cx.op("act", lambda e: e.activation(out=mrt[:], in_=mss[:], func=AF.Sqrt, scale=1.0 / D, bias=EPS),
                      reads=[mss.b], full=[mrt.b])
                cx.op("dve", lambda e: e.reciprocal(out=mrs[:], in_=mrt[:]), reads=[mrt.b], full=[mrs.b])
                cx.op("dve", lambda e: e.scalar_tensor_tensor(out=mh[:], in0=mx[:], scalar=mrs[:, 0:1], in1=gmem[:],
                                                              op0=ALU.mult, op1=ALU.mult),
                      reads=[mx.b, mrs.b, gmem.b], full=[mh.b])
                pb = psb[mt % 2]
                for c in range(8):
                    cx.op("pe", lambda e, pb=pb, c=c: e.transpose(out=pb[:, c * 128:(c + 1) * 128],
                                                                  in_=mh[:, c * 128:(c + 1) * 128],
                                                                  identity=ident_bf[:]),
                          reads=[mh.b, ident_bf.b], writes=[pb.b])
                cx.op("act", lambda e, pb=pb, mt=mt: e.copy(out=memT[:, :, mt * 128:(mt + 1) * 128],
                                                            in_=pb[:].rearrange("p (c t) -> p c t", c=8)),
                      reads=[pb.b], writes=[memT.b])
            for hd in range(4):
                ps = getps()
                for c in range(8):
                    cx.op("pe", lambda e, ps=ps, c=c, hd=hd: e.matmul(
                        ps[:, 0:256], lhsT=wkv[:, c, hd * 128:(hd + 1) * 128], rhs=memT[:, c, :],
                        start=(c == 0), stop=(c == 7)), reads=[wkv.b, memT.b], writes=[ps.b])
                cx.op("dve", lambda e, ps=ps, hd=hd: e.tensor_copy(out=kT[:, hd, :], in_=ps[:, 0:256]),
                      reads=[ps.b], writes=[kT.b])
            for mc in range(2):
                ps = getps()
                for c in range(8):
                    cx.op("pe", lambda e, ps=ps, c=c, mc=mc: e.matmul(
                        ps[:], lhsT=memT[:, c, mc * 128:(mc + 1) * 128], rhs=wkv[:, c, 512:1024],
                        start=(c == 0), stop=(c == 7)), reads=[wkv.b, memT.b], writes=[ps.b])
                cx.op("dve", lambda e, ps=ps, mc=mc: e.tensor_copy(out=vtok[:, mc, :], in_=ps[:]),
                      reads=[ps.b], writes=[vtok.b])
            cx.barrier()
            alS.close()

            alW = Alloc(nc)
            hT = [alW.sb([128, 8, TC], BF16, f"hTc{i}") for i in range(2)]
            ysb = [alW.sb([128, 4, TC], BF16, "ysb0")] * 2
            vbuf = alW.sb([128, 4, 30 + TC], BF16, "vbuf")
            sgt = [alW.sb([128, TC], F32, f"sgt{i}") for i in range(3)]
            cv = alW.sb([128, 4, TC], F32, "cv")
            sq = [sgt[1], sgt[2]]
            mean = alW.sb([128, TC], F32, "mean")
            var = alW.sb([128, TC], F32, "var"); lrs = alW.sb([128, TC], F32, "lrs")
            m2 = var; lnv = lrs
            cn = alW.sb([128, 4, TC], BF16, "cn")
            qb = alW.sb([128, 4, TC], BF16, "qb")
            Eb = [alW.sb([128, 2, TC], BF16, "Eb0")] * 2
            ob = alW.sb([128, 4, TC], BF16, "ob")
            macc = alW.sb([128, TC], F32, "macc"); mt1 = alW.sb([128, TC], F32, "mt1"); mt2 = alW.sb([128, TC], F32, "mt2")
            rden = macc
            xc = [mt1, mt2]
            merged = alW.sb([128, 8, TC], BF16, "merged")
            xt2 = [alW.sb([128, D], F32, f"xtc{i}") for i in range(2)]
            x2t = xt2
            h2f = alW.sb([128, D], F32, "h2f"); h2b = [alW.sb([128, D], BF16, "h2b0")] * 2
            junk2 = h2b[0]
            h2T = alW.sb([128, 8, 128], F32, "h2T")
            ss2 = alW.sb([128, 1], F32, "ss2"); rt2 = alW.sb([128, 1], F32, "rt2"); rs2 = alW.sb([128, 1], F32, "rs2")
            cx.op("pool", lambda e: e.memset(vbuf[:], 0.0), full=[vbuf.b])

            breg = {}

            def mmgrp(ps_ap, ps_b, pairs, reads):
                n = len(pairs)
                for idx, (l, r_) in enumerate(pairs):
                    cx.op("pe", lambda e, l=l, r_=r_, idx=idx: e.matmul(ps_ap, lhsT=l, rhs=r_, start=(idx == 0),
                                                                         stop=(idx == n - 1)),
                          reads=reads, writes=[ps_b])

            KCUT = int(os.environ.get("KCUT", "9"))
            KNB = int(os.environ.get("KNB", str(NBC)))
            def c_load_h(bi):
                t0 = bi * TC
                h = hT[bi % 2]
                cx.op("sp", lambda e, h=h, t0=t0: e.dma_start(
                    out=h[:], in_=hT_scr[:, :, t0:t0 + TC].rearrange("c p t -> p c t")), full=[h.b], dma=True)

            def c_load_y(bi):
                t0 = bi * TC
                yb = ysb[bi % 2]
                cx.op("sp", lambda e, yb=yb, t0=t0: e.dma_start(
                    out=yb[:], in_=ys_scr[:, :, t0:t0 + TC].rearrange("f p t -> p f t")), full=[yb.b], dma=True)

            def c_s2(bi):
                t0 = bi * TC
                h = hT[bi % 2]; yb = ysb[bi % 2]
                for f in range(4):
                    pa = getps(); pg = getps()
                    mmgrp(pa[:, 0:TC], pa.b, [(wA[:, c, f * 128:(f + 1) * 128], h[:, c, :]) for c in range(8)],
                          [wA.b, h.b])
                    mmgrp(pg[:, 0:TC], pg.b, [(wA[:, c, 512 + f * 128:512 + (f + 1) * 128], h[:, c, :]) for c in range(8)],
                          [wA.b, h.b])
                    s = sgt[f % 3]
                    cx.op("act", lambda e, pg=pg, s=s: e.activation(out=s[:], in_=pg[:, 0:TC], func=AF.Sigmoid),
                          reads=[pg.b], full=[s.b])
                    cx.op("dve", lambda e, pa=pa, s=s, f=f: e.tensor_tensor(out=vbuf[:, f, 30:30 + TC], in0=pa[:, 0:TC],
                                                                            in1=s[:], op=ALU.mult),
                          reads=[pa.b, s.b], writes=[vbuf.b])

            def c_mid(bi):
                t0 = bi * TC
                h = hT[bi % 2]; yb = ysb[bi % 2]
                for f in range(4):
                    pc = getps()
                    Dg = Dg2[f % 2]
                    cx.op("pool", lambda e, Dg=Dg, f=f: e.tensor_tensor(
                        out=Dg[:], in0=ident_f[:].unsqueeze(1).to_broadcast([128, 31, 128]),
                        in1=cdw[:, f, :].unsqueeze(2).to_broadcast([128, 31, 128]), op=ALU.mult),
                        reads=[ident_f.b, cdw.b], full=[Dg.b])
                    mmgrp(pc[:, 0:TC], pc.b, [(Dg[:, k, :], vbuf[:, f, k:k + TC]) for k in range(31)],
                          [Dg.b, vbuf.b])
                    cx.op("act", lambda e, pc=pc, f=f: e.activation(out=cv[:, f, :], in_=pc[:, 0:TC], func=AF.Identity,
                                                                    bias=cb[:, f:f + 1], scale=1.0),
                          reads=[pc.b, cb.b], writes=[cv.b])
                cx.op("pool", lambda e: e.tensor_copy(out=vbuf[:, :, 0:30], in_=vbuf[:, :, TC:TC + 30]),
                      reads=[vbuf.b], writes=[vbuf.b])
                pm = getps(); pq = getps()
                mmgrp(pm[:, 0:TC], pm.b, [(onesm[:], cv[:, f, :]) for f in range(4)], [onesm.b, cv.b])
                for f in range(4):
                    cx.op("act", lambda e, f=f: e.activation(out=sq[f % 2][:], in_=cv[:, f, :], func=AF.Square),
                          reads=[cv.b], full=[sq[f % 2].b])
                    cx.op("pe", lambda e, pq=pq, f=f: e.matmul(pq[:, 0:TC], lhsT=onesm[:], rhs=sq[f % 2][:],
                                                               start=(f == 0), stop=(f == 3)),
                          reads=[onesm.b, sq[f % 2].b], writes=[pq.b])
                cx.op("act", lambda e, pm=pm: e.copy(out=mean[:], in_=pm[:, 0:TC]), reads=[pm.b], full=[mean.b])
                cx.op("dve", lambda e: e.tensor_tensor(out=var[:], in0=mean[:], in1=mean[:], op=ALU.mult),
                      reads=[mean.b], full=[var.b])
                cx.op("dve", lambda e, pq=pq: e.tensor_tensor(out=var[:], in0=pq[:, 0:TC], in1=var[:], op=ALU.subtract),
                      reads=[pq.b], writes=[var.b])
                cx.op("dve", lambda e: e.tensor_scalar(out=var[:], in0=var[:], scalar1=float(EPS), scalar2=None,
                                                       op0=ALU.add), reads=[var.b], writes=[var.b])
                cx.op("act", lambda e: e.activation(out=lrs[:], in_=var[:], func=AF.Ln), reads=[var.b], full=[lrs.b])
                cx.op("act", lambda e: e.activation(out=lrs[:], in_=lrs[:], func=AF.Exp, scale=-0.5),
                      reads=[], writes=[lrs.b])
                for f in range(4):
                    x_ = xc[f % 2]
                    cx.op("dve", lambda e, x_=x_, f=f: e.tensor_tensor(out=x_[:], in0=cv[:, f, :], in1=mean[:],
                                                                       op=ALU.subtract),
                          reads=[cv.b, mean.b], full=[x_.b])
                    cx.op("dve", lambda e, x_=x_: e.tensor_tensor(out=x_[:], in0=x_[:], in1=lrs[:], op=ALU.mult),
                          reads=[lrs.b], writes=[x_.b])
                    cx.op("act", lambda e, x_=x_, f=f: e.activation(out=cn[:, f, :], in_=x_[:], func=AF.Silu,
                                                                    bias=lnb[:, f:f + 1], scale=lng[:, f:f + 1]),
                          reads=[x_.b, lnb.b, lng.b], writes=[cn.b])
                for hd in range(4):
                    pq_ = getps()
                    mmgrp(pq_[:, 0:TC], pq_.b, [(wA[:, c, 1024 + hd * 128:1024 + (hd + 1) * 128], h[:, c, :])
                                                for c in range(8)], [wA.b, h.b])
                    cx.op("act", lambda e, pq_=pq_, hd=hd: e.copy(out=qb[:, hd, :], in_=pq_[:, 0:TC]),
                          reads=[pq_.b], writes=[qb.b])
                for hd in range(4):
                    E = Eb[hd % 2]
                    for mc in range(2):
                        psc = getps()
                        mmgrp(psc[:, 0:TC], psc.b, [(kT[:, hd, mc * 128:(mc + 1) * 128], qb[:, hd, :])], [kT.b, qb.b])
                        cx.op("act", lambda e, psc=psc, E=E, mc=mc: e.activation(
                            out=E[:, mc, :], in_=psc[:, 0:TC], func=AF.Exp, scale=float(128 ** -0.5)),
                            reads=[psc.b], writes=[E.b])
                    po = getps(); pd = getps()
                    mmgrp(po[:, 0:TC], po.b, [(vtok[:, mc, hd * 128:(hd + 1) * 128], E[:, mc, :]) for mc in range(2)],
                          [vtok.b, E.b])
                    mmgrp(pd[:, 0:TC], pd.b, [(ones_bf[:], E[:, mc, :]) for mc in range(2)], [ones_bf.b, E.b])
                    cx.op("dve", lambda e, pd=pd: e.reciprocal(out=rden[:], in_=pd[:, 0:TC]), reads=[pd.b], full=[rden.b])
                    cx.op("dve", lambda e, po=po, hd=hd: e.tensor_tensor(out=ob[:, hd, :], in0=po[:, 0:TC], in1=rden[:],
                                                                         op=ALU.mult),
                          reads=[po.b, rden.b], writes=[ob.b])
                for j in range(8):
                    js = slice(j * 128, (j + 1) * 128)
                    pga = getps(); pyc = getps()
                    mmgrp(pga[:, 0:TC], pga.b, [(wG[:, c, j * 128:(j + 1) * 128], h[:, c, :]) for c in range(8)], [wG.b, h.b])
                    mmgrp(pyc[:, 0:TC], pyc.b, [(wco[:, f, js], cn[:, f, :]) for f in range(4)], [wco.b, cn.b])
                    s = sgt[0]
                    cx.op("act", lambda e, pga=pga, s=s: e.activation(out=s[:], in_=pga[:, 0:TC], func=AF.Sigmoid),
                          reads=[pga.b], full=[s.b])
                    cx.op("dve", lambda e, pyc=pyc, s=s: e.tensor_tensor(out=macc[:], in0=pyc[:, 0:TC], in1=s[:], op=ALU.mult),
                          reads=[pyc.b, s.b], full=[macc.b])
                    pgb = getps(); pza = getps(); pzb = getps()
                    mmgrp(pgb[:, 0:TC], pgb.b, [(wG[:, c, 1024 + j * 128:1024 + (j + 1) * 128], h[:, c, :]) for c in range(8)],
                          [wG.b, h.b])
                    mmgrp(pza[:, 0:TC], pza.b, [(wgl[:, f, js], yb[:, f, :]) for f in range(4)], [wgl.b, yb.b])
                    mmgrp(pzb[:, 0:TC], pzb.b, [(wgl[:, f, 1024 + j * 128:1024 + (j + 1) * 128], yb[:, f, :]) for f in range(4)],
                          [wgl.b, yb.b])
                    sb_ = sgt[1]; sz = sgt[2]
                    cx.op("act", lambda e, pgb=pgb, sb_=sb_: e.activation(out=sb_[:], in_=pgb[:, 0:TC], func=AF.Sigmoid),
                          reads=[pgb.b], full=[sb_.b])
                    cx.op("act", lambda e, pzb=pzb, sz=sz: e.activation(out=sz[:], in_=pzb[:, 0:TC], func=AF.Sigmoid),
                          reads=[pzb.b], full=[sz.b])
                    cx.op("dve", lambda e, pza=pza, sz=sz: e.tensor_tensor(out=mt1[:], in0=pza[:, 0:TC], in1=sz[:], op=ALU.mult),
                          reads=[pza.b, sz.b], full=[mt1.b])
                    cx.op("dve", lambda e, sb_=sb_: e.tensor_tensor(out=mt1[:], in0=mt1[:], in1=sb_[:], op=ALU.mult),
                          reads=[sb_.b], writes=[mt1.b])
                    cx.op("dve", lambda e: e.tensor_tensor(out=macc[:], in0=macc[:], in1=mt1[:], op=ALU.add),
                          reads=[mt1.b], writes=[macc.b])
                    pgc = getps(); pym = getps()
                    mmgrp(pgc[:, 0:TC], pgc.b, [(wG[:, c, 2048 + j * 128:2048 + (j + 1) * 128], h[:, c, :]) for c in range(8)],
                          [wG.b, h.b])
                    mmgrp(pym[:, 0:TC], pym.b, [(wmo[:, hd, js], ob[:, hd, :]) for hd in range(4)], [wmo.b, ob.b])
                    s = sgt[0]
                    cx.op("act", lambda e, pgc=pgc, s=s: e.activation(out=s[:], in_=pgc[:, 0:TC], func=AF.Sigmoid),
                          reads=[pgc.b], full=[s.b])
                    cx.op("dve", lambda e, pym=pym, s=s: e.tensor_tensor(out=mt2[:], in0=pym[:, 0:TC], in1=s[:], op=ALU.mult),
                          reads=[pym.b, s.b], full=[mt2.b])
                    cx.op("dve", lambda e, j=j: e.tensor_tensor(out=merged[:, j, :], in0=macc[:], in1=mt2[:], op=ALU.add),
                          reads=[macc.b, mt2.b], writes=[merged.b])

            def c_tail(bi):
                ntt = TC // 128
                tis = [bi * ntt + tt for tt in range(ntt)]
                for tt, ti in enumerate(tis):
                    xt_ = xt2[ti % 2]
                    cx.op("sp", lambda e, xt_=xt_, ti=ti: e.dma_start(out=xt_[:], in_=x_d[ti * 128:(ti + 1) * 128, :]),
                          full=[xt_.b], dma=True)
                for tt, ti in enumerate(tis):
                    xt_ = xt2[ti % 2]; x2 = xt_
                    for half in range(2):
                        po_ = getps()
                        mmgrp(po_[:], po_.b, [(merged[:, j, tt * 128:(tt + 1) * 128], wo[:, j, half * 512:(half + 1) * 512])
                                              for j in range(8)], [merged.b, wo.b])
                        cx.op("dve", lambda e, po_=po_, x2=x2, xt_=xt_, half=half: e.tensor_tensor(
                            out=x2[:, half * 512:(half + 1) * 512], in0=po_[:], in1=xt_[:, half * 512:(half + 1) * 512],
                            op=ALU.add), reads=[po_.b, xt_.b], writes=[x2.b])
                    cx.op("sp", lambda e, x2=x2, ti=ti: e.dma_start(out=x2_scr[ti * 128:(ti + 1) * 128, :], in_=x2[:]),
                          reads=[x2.b], writes=[x2B], dma=True)
                for tt, ti in enumerate(tis):
                    x2 = xt2[ti % 2]; hb2 = h2b[0]
                    cx.op("act", lambda e, x2=x2: e.activation(out=junk2[:], in_=x2[:], func=AF.Square, accum_out=ss2[:]),
                          reads=[x2.b], full=[junk2.b, ss2.b])
                    cx.op("act", lambda e: e.activation(out=rt2[:], in_=ss2[:], func=AF.Sqrt, scale=1.0 / D, bias=EPS),
                          reads=[ss2.b], full=[rt2.b])
                    cx.op("dve", lambda e: e.reciprocal(out=rs2[:], in_=rt2[:]), reads=[rt2.b], full=[rs2.b])
                    cx.op("dve", lambda e, x2=x2: e.scalar_tensor_tensor(out=h2f[:], in0=x2[:], scalar=rs2[:, 0:1],
                                                                         in1=gffn[:], op0=ALU.mult, op1=ALU.mult),
                          reads=[x2.b, rs2.b, gffn.b], full=[h2f.b])
                    cx.op("act", lambda e, hb2=hb2: e.copy(out=hb2[:], in_=h2f[:]), reads=[h2f.b], full=[hb2.b])
                    cx.op("sp", lambda e, hb2=hb2, ti=ti: e.dma_start(out=h2_scr[ti * 128:(ti + 1) * 128, :], in_=hb2[:]),
                          reads=[hb2.b], writes=[h2B], dma=True)
                    pra = getps(); prb = getps()
                    for c in range(8):
                        pr = pra if c < 4 else prb
                        cx.op("pe", lambda e, pr=pr, c=c: e.transpose(out=pr[:, (c % 4) * 128:(c % 4 + 1) * 128],
                                                                      in_=h2f[:, c * 128:(c + 1) * 128], identity=ident_f[:]),
                              reads=[h2f.b, ident_f.b], writes=[pr.b])
                    cx.op("act", lambda e, pra=pra: e.copy(out=h2T[:, 0:4, :], in_=pra[:].rearrange("p (c t) -> p c t", c=4)),
                          reads=[pra.b], writes=[h2T.b])
                    cx.op("dve", lambda e, prb=prb: e.tensor_copy(out=h2T[:, 4:8, :],
                                                                  in_=prb[:].rearrange("p (c t) -> p c t", c=4)),
                          reads=[prb.b], writes=[h2T.b])
                    plg = getps()
                    mmgrp(plg[:, 0:36], plg.b, [(h2T[:, c, :], wr[:, c, :]) for c in range(8)], [h2T.b, wr.b])
                    cx.op("dve", lambda e, plg=plg, ti=ti: e.tensor_tensor(out=lg_all[:, ti, :], in0=plg[:, 0:36],
                                                                        in1=rbias[:], op=ALU.add),
                          reads=[plg.b, rbias.b], writes=[lg_all.b])

            NBR = min(NBC, KNB) if KCUT >= 2 else 0
            if NBR > 0:
                c_load_h(0); c_load_y(0); c_s2(0)
            for bi in range(NBR):
                if bi + 1 < NBR:
                    c_load_h(bi + 1)
                c_mid(bi)
                if bi + 1 < NBR:
                    c_load_y(bi + 1)
                    c_s2(bi + 1)
                c_tail(bi)
            cx.barrier()
            alW.close()
            alR = Alloc(nc)
            RS = Buf("route")

            def rd(fn, extra_reads=(), extra_writes=()):
                cx.op("dve", fn, reads=[RS, lg_all.b] + list(extra_reads), writes=[RS] + list(extra_writes))

            def R(shape, name, dt=F32):
                return alR.sb(shape, dt, name)

            NTT = NT
            NEB_ = NEXP * NBLK
            gmax = R([128, NTT], "gmax"); ohg = R([128, NTT, 4], "ohg"); eg = R([128, NTT, 4], "eg")
            sumg = R([128, NTT], "sumg"); ptop = R([128, NTT], "ptop")
            selm = R([128, NTT, 4, 8], "selm"); sel = R([128, NTT, 8], "sel"); sel2 = R([128, NTT, 8], "sel2")
            m1_ = R([128, NTT], "m1_"); m2_ = R([128, NTT], "m2_"); oh1 = R([128, NTT, 8], "oh1"); oh2 = R([128, NTT, 8], "oh2")
            dm = R([128, NTT], "dm"); w1 = R([128, NTT], "w1"); w2 = R([128, NTT], "w2")
            M1 = R([128, NTT, 4, 8], "M1"); M2 = R([128, NTT, 4, 8], "M2"); Mc = R([128, NTT, 32], "Mc")
            Cex = R([128, NTT, 32], "Cex"); pos = R([128, NTT, 32], "pos"); bk = R([128, NTT, 32], "bk")
            sf = R([128, NTT, 32], "sf"); ov = R([128, NTT, 32], "ov"); tq = R([128, NTT, 32], "tq")
            sk = [R([128, NTT], f"sk{k}") for k in range(2)]; okk = R([128, NTT], "okk"); dd = R([128, NTT], "dd")
            si = [R([128, NTT], f"si{k}", I32) for k in range(2)]
            ent = [R([128, NTT, 4], f"ent{k}") for k in range(2)]
            le4 = lg_all[:, :, 4:36].rearrange("p t (g j) -> p t g j", g=4)

            def bc3(a, n):
                return a.unsqueeze(2).to_broadcast([128, NTT, n])

            rd(lambda e: e.tensor_reduce(out=gmax[:], in_=lg_all[:, :, 0:4], axis=AX.X, op=ALU.max))
            rd(lambda e: e.tensor_tensor(out=ohg[:], in0=lg_all[:, :, 0:4], in1=bc3(gmax[:], 4), op=ALU.is_equal))
            rd(lambda e: e.tensor_tensor(out=eg[:], in0=lg_all[:, :, 0:4], in1=bc3(gmax[:], 4), op=ALU.subtract))
            cx.op("act", lambda e: e.activation(out=eg[:], in_=eg[:], func=AF.Exp), reads=[RS], writes=[RS])
            rd(lambda e: e.tensor_reduce(out=sumg[:], in_=eg[:], axis=AX.X, op=ALU.add))
            rd(lambda e: e.reciprocal(out=ptop[:], in_=sumg[:]))
            rd(lambda e: e.tensor_tensor(out=selm[:], in0=le4,
                                         in1=ohg[:].unsqueeze(3).to_broadcast([128, NTT, 4, 8]), op=ALU.mult))
            rd(lambda e: e.tensor_reduce(out=sel[:], in_=selm[:].rearrange("p t g j -> p t j g"), axis=AX.X, op=ALU.add))
            rd(lambda e: e.tensor_reduce(out=m1_[:], in_=sel[:], axis=AX.X, op=ALU.max))
            rd(lambda e: e.tensor_tensor(out=oh1[:], in0=sel[:], in1=bc3(m1_[:], 8), op=ALU.is_equal))
            rd(lambda e: e.scalar_tensor_tensor(out=sel2[:], in0=oh1[:], scalar=-1e30, in1=sel[:], op0=ALU.mult, op1=ALU.add))
            rd(lambda e: e.tensor_reduce(out=m2_[:], in_=sel2[:], axis=AX.X, op=ALU.max))
            rd(lambda e: e.tensor_tensor(out=oh2[:], in0=sel2[:], in1=bc3(m2_[:], 8), op=ALU.is_equal))
            rd(lambda e: e.tensor_tensor(out=dm[:], in0=m1_[:], in1=m2_[:], op=ALU.subtract))
            cx.op("act", lambda e: e.activation(out=w1[:], in_=dm[:], func=AF.Sigmoid), reads=[RS], writes=[RS])
            rd(lambda e: e.tensor_tensor(out=w1[:], in0=w1[:], in1=ptop[:], op=ALU.mult))
            rd(lambda e: e.tensor_tensor(out=w2[:], in0=ptop[:], in1=w1[:], op=ALU.subtract))
            rd(lambda e: e.tensor_tensor(out=M1[:], in0=ohg[:].unsqueeze(3).to_broadcast([128, NTT, 4, 8]),
                                         in1=oh1[:].unsqueeze(2).to_broadcast([128, NTT, 4, 8]), op=ALU.mult))
            rd(lambda e: e.tensor_tensor(out=M2[:], in0=ohg[:].unsqueeze(3).to_broadcast([128, NTT, 4, 8]),
                                         in1=oh2[:].unsqueeze(2).to_broadcast([128, NTT, 4, 8]), op=ALU.mult))
            rd(lambda e: e.tensor_tensor(out=Mc[:], in0=M1[:].rearrange("p t g j -> p t (g j)"),
                                         in1=M2[:].rearrange("p t g j -> p t (g j)"), op=ALU.add))
            rd(lambda e: e.memset(Cex[:, 0, :], 0.0))
            for i in range(1, NTT):
                rd(lambda e, i=i: e.tensor_tensor(out=Cex[:, i, :], in0=Cex[:, i - 1, :], in1=Mc[:, i - 1, :], op=ALU.add))
            pp = [getps(), getps()]
            for i in range(NTT):
                pb_ = pp[i // 16]
                o_ = pb_[:, (i % 16) * 32:(i % 16 + 1) * 32]
                cx.op("pe", lambda e, o_=o_, i=i: e.matmul(o_, lhsT=tri[:], rhs=Mc[:, i, :], start=True, stop=False),
                      reads=[tri.b, RS], writes=[pb_.b])
                cx.op("pe", lambda e, o_=o_, i=i: e.matmul(o_, lhsT=ones_f[:], rhs=Cex[:, i, :], start=False, stop=True),
                      reads=[ones_f.b, RS], writes=[pb_.b])
            for hh in range(2):
                rd(lambda e, hh=hh: e.tensor_copy(out=pos[:, hh * 16:(hh + 1) * 16, :],
                                                  in_=pp[hh][:].rearrange("p (t x) -> p t x", t=16)), [pp[hh].b])
            rd(lambda e: e.tensor_single_scalar(out=bk[:], in_=pos[:], scalar=127.5, op=ALU.is_gt))
            for thr in range(2, NBLK):
                rd(lambda e, thr=thr: e.tensor_single_scalar(out=tq[:], in_=pos[:], scalar=128.0 * thr - 0.5, op=ALU.is_gt))
                rd(lambda e: e.tensor_tensor(out=bk[:], in0=bk[:], in1=tq[:], op=ALU.add))
            rd(lambda e: e.scalar_tensor_tensor(out=bk[:], in0=bk[:], scalar=float(1 - 128 * NEB_),
                                                in1=ecap[:].unsqueeze(1).to_broadcast([128, NTT, 32]),
                                                op0=ALU.mult, op1=ALU.add), [ecap.b])
            rd(lambda e: e.scalar_tensor_tensor(out=sf[:], in0=pos[:], scalar=float(NEB_), in1=bk[:],
                                                op0=ALU.mult, op1=ALU.add))
            rd(lambda e: e.tensor_single_scalar(out=ov[:], in_=pos[:], scalar=float(CAP) - 0.5, op=ALU.is_gt))
            for k, (Mk, wk) in enumerate(((M1, w1), (M2, w2))):
                Mk32 = Mk[:].rearrange("p t g j -> p t (g j)")
                rd(lambda e, Mk32=Mk32: e.tensor_tensor(out=tq[:], in0=Mk32, in1=sf[:], op=ALU.mult))
                rd(lambda e, k=k: e.tensor_reduce(out=sk[k][:], in_=tq[:], axis=AX.X, op=ALU.add))
                rd(lambda e, Mk32=Mk32: e.tensor_tensor(out=tq[:], in0=Mk32, in1=ov[:], op=ALU.mult))
                rd(lambda e: e.tensor_reduce(out=okk[:], in_=tq[:], axis=AX.X, op=ALU.add))
                rd(lambda e, k=k: e.tensor_scalar(out=dd[:], in0=sk[k][:], scalar1=trashp[:, 0:1], scalar2=None,
                                                  op0=ALU.subtract), [trashp.b])
                rd(lambda e: e.tensor_tensor(out=dd[:], in0=dd[:], in1=okk[:], op=ALU.mult))
                rd(lambda e, k=k: e.tensor_tensor(out=sk[k][:], in0=sk[k][:], in1=dd[:], op=ALU.subtract))
                rd(lambda e, k=k: e.tensor_copy(out=si[k][:], in_=sk[k][:]), (), [si[k].b])
                rd(lambda e, k=k: e.memset(ent[k][:], 0.0), (), [ent[k].b])
                rd(lambda e, k=k: e.tensor_copy(out=ent[k][:, :, 0], in_=tokid[:]), [tokid.b], [ent[k].b])
                rd(lambda e, k=k: e.tensor_scalar(out=ent[k][:, :, 1], in0=tokid[:], scalar1=float(k * ROWS), scalar2=None,
                                                  op0=ALU.add), [tokid.b], [ent[k].b])
                rd(lambda e, k=k, wk=wk: e.tensor_copy(out=ent[k][:, :, 2], in_=wk[:]), (), [ent[k].b])
            for i in range(NTT):
                for k in range(2):
                    cx.op("pool", lambda e, i=i, k=k: e.indirect_dma_start(
                        out=lst_d, out_offset=bass.IndirectOffsetOnAxis(ap=si[k][:, i:i + 1], axis=0),
                        in_=ent[k][:, i, :], in_offset=None),
                        reads=[si[k].b, ent[k].b], writes=[lstB], dma=True)
            cx.barrier()
            alR.close()
            alC.close()
        if stop_after in ("A", "B", "C"):
            pass
        else:
            alD = Alloc(nc)
            NEB = NEXP * NBLK
            lst_sb = alD.sb([128, NEB, 4], F32, "lst_sb")
            idx_i = alD.sb([128, NEB], I32, "idx_i")
            dst_i = alD.sb([128, NEB], I32, "dst_i")
            cx.op("sp", lambda e: e.dma_start(out=lst_sb[:], in_=lst_d[0:NEXP * CAP, :].rearrange("(s eb) w -> s eb w", s=128)),
                  reads=[lstB], full=[lst_sb.b], dma=True)
            cx.op("dve", lambda e: e.tensor_copy(out=idx_i[:], in_=lst_sb[:, :, 0]), reads=[lst_sb.b], full=[idx_i.b])
            cx.op("dve", lambda e: e.tensor_copy(out=dst_i[:], in_=lst_sb[:, :, 1]), reads=[lst_sb.b], full=[dst_i.b])
            sg_ = [alD.sb([128, 8, 256], F32, f"sg{i}") for i in range(2)]
            su_ = [alD.sb([128, 8, 256], F32, f"su{i}") for i in range(2)]
            sd_ = [alD.sb([128, 2, 1024], F32, f"sd{i}") for i in range(2)]
            Wg = [alD.sb([128, 8, 256], BF16, f"Wg{i}") for i in range(2)]
            Wu = [alD.sb([128, 8, 256], BF16, f"Wu{i}") for i in range(2)]
            Wd = [alD.sb([128, 2, 1024], BF16, f"Wd{i}") for i in range(2)]
            Gt = [alD.sb([128, D], BF16, f"Gt{i}") for i in range(3)]
            Xe = [alD.sb([128, 8, CAP], BF16, f"Xe{i}") for i in range(2)]
            sgl = [alD.sb([128, CAP], F32, f"sgl{i}") for i in range(2)]
            ae = [alD.sb([128, 2, CAP], BF16, f"ae{i}") for i in range(2)]
            Yt = [alD.sb([128, D], BF16, f"Yt{i}") for i in range(3)]

            Gt6 = Gt + [alD.sb([128, D], BF16, f"Gtx{i}") for i in range(3)]

            def load_w_dma(e_):
                p = e_ % 2
                cx.op("sp", lambda e: e.dma_start(out=sg_[p][:], in_=weg_d[e_]), full=[sg_[p].b], dma=True)
                cx.op("sp", lambda e: e.dma_start(out=su_[p][:], in_=weu_d[e_]), full=[su_[p].b], dma=True)
                cx.op("sp", lambda e: e.dma_start(out=sd_[p][:], in_=wed_d[e_]), full=[sd_[p].b], dma=True)

            def load_w_cast(e_):
                p = e_ % 2
                cx.op("dve", lambda e: e.tensor_copy(out=Wg[p][:], in_=sg_[p][:]), reads=[sg_[p].b], full=[Wg[p].b])
                cx.op("act", lambda e: e.copy(out=Wu[p][:], in_=su_[p][:]), reads=[su_[p].b], full=[Wu[p].b])
                cx.op("pool", lambda e: e.tensor_copy(out=Wd[p][:], in_=sd_[p][:]), reads=[sd_[p].b], full=[Wd[p].b])

            def gathers(e_):
                for blk in range(NBLK):
                    eb = e_ * NBLK + blk
                    G = Gt6[(e_ % 2) * 3 + blk]
                    cx.op("pool", lambda e, G=G, eb=eb: e.indirect_dma_start(
                        out=G[:], out_offset=None, in_=h2_scr,
                        in_offset=bass.IndirectOffsetOnAxis(ap=idx_i[:, eb:eb + 1], axis=0)),
                        reads=[idx_i.b, h2B], full=[G.b], dma=True)

            gi = [0]
            load_w_dma(0)
            gathers(0)
            load_w_cast(0)
            KNE = int(os.environ.get("KNE", str(NEXP)))
            for e_ in range(KNE):
                p = e_ % 2
                if e_ + 1 < NEXP:
                    load_w_dma(e_ + 1)
                    gathers(e_ + 1)
                X = Xe[p]
                for blk in range(NBLK):
                    G = Gt6[(e_ % 2) * 3 + blk]
                    pbk = psb[gi[0] % 2]
                    gi[0] += 1
                    for c in range(8):
                        cx.op("pe", lambda e, pbk=pbk, G=G, c=c: e.transpose(
                            out=pbk[:, c * 128:(c + 1) * 128], in_=G[:, c * 128:(c + 1) * 128], identity=ident_bf[:]),
                            reads=[G.b, ident_bf.b], writes=[pbk.b])
                    if blk % 2 == 0:
                        cx.op("act", lambda e, pbk=pbk, X=X, blk=blk: e.copy(
                            out=X[:, :, blk * 128:(blk + 1) * 128], in_=pbk[:].rearrange("p (c t) -> p c t", c=8)),
                            reads=[pbk.b], writes=[X.b])
                    else:
                        cx.op("dve", lambda e, pbk=pbk, X=X, blk=blk: e.tensor_copy(
                            out=X[:, :, blk * 128:(blk + 1) * 128], in_=pbk[:].rearrange("p (c t) -> p c t", c=8)),
                            reads=[pbk.b], writes=[X.b])
                a_ = ae[p]
                for ft in range(2):
                    pg = getps(); pu = getps()
                    for c in range(8):
                        cx.op("pe", lambda e, pg=pg, c=c, ft=ft, X=X, p=p: e.matmul(
                            pg[:, 0:CAP], lhsT=Wg[p][:, c, ft * 128:(ft + 1) * 128], rhs=X[:, c, :],
                            start=(c == 0), stop=(c == 7)), reads=[Wg[p].b, X.b], writes=[pg.b])
                    for c in range(8):
                        cx.op("pe", lambda e, pu=pu, c=c, ft=ft, X=X, p=p: e.matmul(
                            pu[:, 0:CAP], lhsT=Wu[p][:, c, ft * 128:(ft + 1) * 128], rhs=X[:, c, :],
                            start=(c == 0), stop=(c == 7)), reads=[Wu[p].b, X.b], writes=[pu.b])
                    s = sgl[ft]
                    cx.op("act", lambda e, pg=pg, s=s: e.activation(out=s[:], in_=pg[:, 0:CAP], func=AF.Silu),
                          reads=[pg.b], full=[s.b])
                    cx.op("dve", lambda e, pu=pu, s=s, a_=a_, ft=ft: e.tensor_tensor(
                        out=a_[:, ft, :], in0=pu[:, 0:CAP], in1=s[:], op=ALU.mult),
                        reads=[pu.b, s.b], writes=[a_.b])
                for blk in range(NBLK):
                    eb = e_ * NBLK + blk
                    Y = Yt[eb % 3]
                    for half in range(2):
                        py = getps()
                        for ft in range(2):
                            cx.op("pe", lambda e, py=py, ft=ft, blk=blk, half=half, a_=a_, p=p: e.matmul(
                                py[:], lhsT=a_[:, ft, blk * 128:(blk + 1) * 128],
                                rhs=Wd[p][:, ft, half * 512:(half + 1) * 512], start=(ft == 0), stop=(ft == 1)),
                                reads=[a_.b, Wd[p].b], writes=[py.b])
                        if half == 0:
                            cx.op("dve", lambda e, py=py, Y=Y, eb=eb: e.tensor_scalar(
                                out=Y[:, 0:512], in0=py[:], scalar1=lst_sb[:, eb, 2:3], scalar2=None, op0=ALU.mult),
                                reads=[py.b, lst_sb.b], writes=[Y.b])
                        else:
                            cx.op("act", lambda e, py=py, Y=Y, eb=eb: e.activation(
                                out=Y[:, 512:1024], in_=py[:], func=AF.Copy, scale=lst_sb[:, eb, 2:3]),
                                reads=[py.b, lst_sb.b], writes=[Y.b])
                    cx.op("pool", lambda e, Y=Y, eb=eb: e.indirect_dma_start(
                        out=moe_scr, out_offset=bass.IndirectOffsetOnAxis(ap=dst_i[:, eb:eb + 1], axis=0),
                        in_=Y[:], in_offset=None), reads=[Y.b, dst_i.b], writes=[moeB], dma=True)
                if e_ + 1 < NEXP:
                    load_w_cast(e_ + 1)
            cx.barrier()
            alD.close()

            alE = Alloc(nc)
            gfin = alE.sb([128, D], F32, "gfin")
            cx.op("sp", lambda e: e.dma_start(out=gfin[:], in_=gfin_d.partition_broadcast(128)), full=[gfin.b], dma=True)
            NE_ = 4
            xa = [alE.sb([128, D], F32, f"xa{i}") for i in range(NE_)]
            m0 = [alE.sb([128, D], BF16, f"m0{i}") for i in range(NE_)]
            m1 = [alE.sb([128, D], BF16, f"m1{i}") for i in range(NE_)]
            ot = [alE.sb([128, D], F32, f"ot{i}") for i in range(NE_)]
            junk3 = alE.sb([128, D], BF16, "junk3")
            sse = [alE.sb([128, 1], F32, f"sse{i}") for i in range(NE_)]
            rte = [alE.sb([128, 1], F32, f"rte{i}") for i in range(NE_)]
            rse = [alE.sb([128, 1], F32, f"rse{i}") for i in range(NE_)]
            outB = Buf("out")

            def e_load(ti):
                p = ti % NE_
                rows = slice(ti * 128, (ti + 1) * 128)
                cx.op("sp", lambda e, p=p, rows=rows: e.dma_start(out=xa[p][:], in_=x2_scr[rows, :]),
                      reads=[x2B], full=[xa[p].b], dma=True)
                cx.op("sp", lambda e, p=p, rows=rows: e.dma_start(out=m0[p][:], in_=moe_scr[rows, :]),
                      reads=[moeB], full=[m0[p].b], dma=True)
                cx.op("sp", lambda e, p=p, ti=ti: e.dma_start(
                    out=m1[p][:], in_=moe_scr[ROWS + ti * 128:ROWS + (ti + 1) * 128, :]),
                    reads=[moeB], full=[m1[p].b], dma=True)

            for ti in range(min(NE_ - 1, NT)):
                e_load(ti)
            for ti in range(NT):
                p = ti % NE_
                rows = slice(ti * 128, (ti + 1) * 128)
                if ti + NE_ - 1 < NT:
                    e_load(ti + NE_ - 1)
                cx.op("pool", lambda e, p=p: e.tensor_tensor(out=xa[p][:], in0=xa[p][:], in1=m0[p][:], op=ALU.add),
                      reads=[m0[p].b], writes=[xa[p].b])
                cx.op("dve", lambda e, p=p: e.tensor_tensor(out=xa[p][:], in0=xa[p][:], in1=m1[p][:], op=ALU.add),
                      reads=[m1[p].b], writes=[xa[p].b])
                cx.op("act", lambda e, p=p: e.activation(out=junk3[:], in_=xa[p][:], func=AF.Square, accum_out=sse[p][:]),
                      reads=[xa[p].b], full=[junk3.b, sse[p].b])
                cx.op("act", lambda e, p=p: e.activation(out=rte[p][:], in_=sse[p][:], func=AF.Sqrt, scale=1.0 / D, bias=EPS),
                      reads=[sse[p].b], full=[rte[p].b])
                cx.op("dve", lambda e, p=p: e.reciprocal(out=rse[p][:], in_=rte[p][:]), reads=[rte[p].b], full=[rse[p].b])
                cx.op("dve", lambda e, p=p: e.scalar_tensor_tensor(out=ot[p][:], in0=xa[p][:], scalar=rse[p][:, 0:1],
                                                                   in1=gfin[:], op0=ALU.mult, op1=ALU.mult),
                      reads=[xa[p].b, rse[p].b, gfin.b], full=[ot[p].b])
                cx.op("sp", lambda e, p=p, rows=rows: e.dma_start(out=out_d[rows, :], in_=ot[p][:]),
                      reads=[ot[p].b], writes=[outB], dma=True)
            cx.barrier()
            alE.close()
        cx.barrier()
        cx.emit(block)
        print("waits", cx.nwait, "instrs", {e: cx.cnt[e] for e in cx.ENG}, "signals", {e: len(cx.waited[e]) for e in cx.ENG})
    return nc


def host_consts():
    c = {}
    c["ident_bf"] = np.eye(128, dtype=np.float32).astype(ml_dtypes.bfloat16)
    c["ident_f"] = np.eye(128, dtype=np.float32)
    psel = np.zeros((128, 8, 240), np.float32)
    for a in range(8):
        for i in range(16):
            psel[a * 16 + i, a, 7 * 16 + i] = 1.0
    c["psel"] = psel.astype(ml_dtypes.bfloat16)
    kk = np.arange(128) // 16
    c["cmask"] = (kk[None, :] >= kk[:, None]).astype(np.float32)
    c["tri"] = (np.arange(128)[:, None] < np.arange(128)[None, :]).astype(np.float32)
    c["ecap"] = np.ascontiguousarray(np.broadcast_to((np.arange(32) * NBLK).astype(np.float32)[None, :], (128, 32)))
    c["tokid"] = (np.arange(NT)[None, :] * 128 + np.arange(128)[:, None]).astype(np.float32)
    li = np.zeros((NEXP * CAP + 128, 4), np.float32)
    li[:, 0] = SEQ + ((np.arange(NEXP * CAP + 128) // (NEXP * NBLK)) % 128)
    li[:, 1] = li[:, 0]
    c["trashp"] = (NEXP * CAP + np.arange(128)).astype(np.float32).reshape(128, 1)
    c["lst_init"] = li
    return c


def relayout_pc(w):
    E, K, N = w.shape
    return np.ascontiguousarray(w.reshape(E, K // 128, 128, N).transpose(0, 2, 1, 3))


def pair_layout(a):
    rest = a.shape[2:]
    a = a.reshape((16, 2, 64) + rest)
    a = np.moveaxis(a, 0, 2)
    return np.ascontiguousarray(a.reshape((128, 16) + rest))


def make_inmap(inputs, b, consts=None):
    f = lambda a: np.ascontiguousarray(a, dtype=np.float32)
    m = {"x": f(inputs["x"][b]),
         "g_mix": f(inputs["g_mix"]),
         "w_in": f(inputs["w_in"][0])}
    m["lamre_l"] = pair_layout(f(inputs["ssm_lambda_re"][0]))
    m["lamim_l"] = pair_layout(f(inputs["ssm_lambda_im"][0]))
    m["logdt_l"] = pair_layout(np.broadcast_to(f(inputs["ssm_log_dt"][0])[:, None], (32, 64)))
    m["bre_l"] = pair_layout(f(inputs["ssm_b_re"][0]))
    m["bim_l"] = pair_layout(f(inputs["ssm_b_im"][0]))
    m["cre_l"] = pair_layout(f(inputs["ssm_c_re"][0]).transpose(0, 2, 1))
    m["cim_l"] = pair_layout(f(inputs["ssm_c_im"][0]).transpose(0, 2, 1))
    m["d_l"] = np.ascontiguousarray(np.tile(f(inputs["ssm_d"][0]).reshape(32, 16).T, (8, 1)))
    m["mem"] = f(inputs["mem"][b])
    for k_, n_ in (("g_mem", "g_mem"), ("g_ffn", "g_ffn")):
        m[n_] = f(inputs[k_])
    m["g_final"] = f(inputs["g_final"]).reshape(1, D)
    m["w_mem_kv"] = f(inputs["w_mem_kv"][0]); m["w_mem_out"] = f(inputs["w_mem_out"][0])
    m["w_conv_out"] = f(inputs["w_conv_out"][0]); m["w_ssm_glu"] = f(inputs["w_ssm_glu"][0])
    m["w_out"] = f(inputs["w_out"][0])
    m["w_router"] = np.ascontiguousarray(np.concatenate([f(inputs["w_router_group"][0]),
                                                         f(inputs["w_router_expert"][0])], axis=1))
    m["b_router"] = np.ascontiguousarray(np.concatenate([f(inputs["b_router_group"][0]),
                                                         f(inputs["b_router_expert"][0])])[None, :])
    m["cdw_l"] = np.ascontiguousarray(f(inputs["conv_dw"][0]).T.reshape(4, 128, 31).transpose(1, 0, 2))
    m["cb_l"] = np.ascontiguousarray(f(inputs["conv_dw_bias"][0]).reshape(4, 128).T)
    m["lng_l"] = np.ascontiguousarray(f(inputs["conv_ln_g"][0]).reshape(4, 128).T)
    m["lnb_l"] = np.ascontiguousarray(f(inputs["conv_ln_b"][0]).reshape(4, 128).T)
    if consts is not None and "w_exp_gate" in consts:
        for k_ in ("w_exp_gate", "w_exp_up", "w_exp_down"):
            m[k_] = consts[k_]
    else:
        m["w_exp_gate"] = relayout_pc(f(inputs["w_exp_gate"][0]))
        m["w_exp_up"] = relayout_pc(f(inputs["w_exp_up"][0]))
        m["w_exp_down"] = relayout_pc(f(inputs["w_exp_down"][0]))
    m.update(consts if consts is not None else host_consts())
    return m


def kernel(**inputs):
    nc = build()
    consts = host_consts()
    f32 = lambda a: np.ascontiguousarray(a, dtype=np.float32)
    for k_ in ("w_exp_gate", "w_exp_up", "w_exp_down"):
        consts[k_] = relayout_pc(f32(inputs[k_][0]))
    in_maps = [make_inmap(inputs, b, consts) for b in range(NCORES)]
    res = run_bass_kernel_spmd(nc, in_maps, core_ids=list(range(NCORES)))
    return np.stack([r["out"] for r in res.results], axis=0)
```

```python
import os
import numpy as np
import ml_dtypes
from contextlib import ExitStack
import concourse.bass as bass
import concourse.mybir as mybir
from concourse.bass_utils import run_bass_kernel_spmd

F32 = mybir.dt.float32
BF16 = mybir.dt.bfloat16
I32 = mybir.dt.int32
U32 = mybir.dt.uint32
AF = mybir.ActivationFunctionType
ALU = mybir.AluOpType
AX = mybir.AxisListType
GELU = AF.Gelu_apprx_tanh

D = 1024
SEQ = 4096
NCORES = 8
T = 512
NB = SEQ // T
NT = SEQ // 128
EPS = 1e-6
NEXP = 32
CAP = 384
NBLK = CAP // 128
ROWS = SEQ + 128


class Buf:
    __slots__ = ("name", "w", "r")

    def __init__(self, name):
        self.name = name
        self.w = {}
        self.r = {}


class Ctx:
    ENG = ("pe", "dve", "act", "pool", "sp")
    KROT = 4
    NDMA = 12

    def __init__(self, nc, es):
        self.nc = nc
        self.q = {e: [] for e in self.ENG}
        self.cnt = {e: 0 for e in self.ENG}
        self.seen = {e: {} for e in self.ENG}
        self.esem = {e: [es.enter_context(nc.semaphore(f"s_{e}{i}")) for i in range(self.KROT)]
                     for e in self.ENG}
        self.dsem = {e: [es.enter_context(nc.semaphore(f"d_{e}{i}")) for i in range(self.NDMA)]
                     for e in ("sp", "act", "pool")}
        self.dcnt = {e: [0] * self.NDMA for e in self.dsem}
        self.dnext = {e: 0 for e in self.dsem}
        self.nwait = 0
        self.waited = {e: set() for e in self.ENG}

    def _wait(self, eng, tok):
        key, val = tok
        if key[0] == 'e' and key[1] == eng and eng == "pe":
            return
        if self.seen[eng].get(key, -1) >= val:
            return
        self.seen[eng][key] = val
        if key[0] == 'e':
            self.waited[key[1]].add(val)
        self.q[eng].append(("w", key, val))
        self.nwait += 1

    def op(self, eng, fn, reads=(), writes=(), full=(), dma=False):
        toks = []
        for b in reads:
            toks.extend(b.w.items())
        for b in tuple(writes) + tuple(full):
            toks.extend(b.w.items())
            toks.extend(b.r.items())
        for t in toks:
            self._wait(eng, t)
        if dma:
            i = self.dnext[eng]
            self.dnext[eng] = (i + 1) % self.NDMA
            key = ('d', eng, i)
            if self.dcnt[eng][i] > 0:
                self._wait(eng, (key, self.dcnt[eng][i]))
            self.dcnt[eng][i] += 16
            val = self.dcnt[eng][i]
            self.q[eng].append(("d", fn, self.dsem[eng][i]))
        else:
            key = ('e', eng)
            val = self.cnt[eng]
            self.cnt[eng] += 1
            self.q[eng].append(("i", fn, val))
        for b in reads:
            b.r[key] = val
        for b in full:
            b.w = {key: val}
            b.r = {}
        for b in writes:
            b.w[key] = val
        return (key, val)

    def barrier(self):
        toks = []
        for e in self.ENG:
            if self.cnt[e] > 0:
                toks.append((('e', e), self.cnt[e] - 1))
        for e in self.dsem:
            for i in range(self.NDMA):
                if self.dcnt[e][i] > 0:
                    toks.append((('d', e, i), self.dcnt[e][i]))
        for e in self.ENG:
            for t in toks:
                self._wait(e, t)

    def emit(self, block):
        nc = self.nc

        rank = {e: {v: i for i, v in enumerate(sorted(self.waited[e]))} for e in self.ENG}
        K_ = self.KROT

        def run(engname, engine):
            for item in self.q[engname]:
                if item[0] == "w":
                    key, val = item[1], item[2]
                    if key[0] == 'e':
                        r = rank[key[1]][val]
                        engine.wait_ge(self.esem[key[1]][r % K_], r // K_ + 1)
                    else:
                        engine.wait_ge(self.dsem[key[1]][key[2]], val)
                elif item[0] == "d":
                    item[1](engine).then_inc(item[2], 16)
                else:
                    ins = item[1](engine)
                    r = rank[engname].get(item[2])
                    if r is not None:
                        ins.then_inc(self.esem[engname][r % K_], 1)

        @block.tensor
        def _(e):
            run("pe", e)

        @block.vector
        def _(e):
            run("dve", e)

        @block.scalar
        def _(e):
            run("act", e)

        @block.gpsimd
        def _(e):
            run("pool", e)

        @block.sync
        def _(e):
            run("sp", e)


class TT:
    def __init__(self, t, name):
        self.t = t
        self.b = Buf(name)

    def __getitem__(self, k):
        return self.t[k]


class Alloc:
    cnt = [0]

    def __init__(self, nc, es=None):
        self.nc = nc
        self.es = es if es is not None else ExitStack()

    @property
    def n(self):
        return Alloc.cnt[0]

    @n.setter
    def n(self, v):
        Alloc.cnt[0] = v

    def close(self):
        self.es.close()

    def sb(self, shape, dt, name=None):
        self.n += 1
        name = name or f"sb{self.n}"
        t = self.es.enter_context(self.nc.sbuf_tensor(f"{name}_{self.n}", list(shape), dt))
        return TT(t, name)

    def ps(self, shape, dt, name=None):
        self.n += 1
        name = name or f"ps{self.n}"
        t = self.es.enter_context(self.nc.psum_tensor(f"{name}_{self.n}", list(shape), dt))
        return TT(t, name)


def build(stop_after="E", dbg=False):
    nc = bass.Bass("TRN2", target_bir_lowering=False)
    dram = {}

    def din(name, shape, dt=F32):
        dram[name] = nc.dram_tensor(name, list(shape), dt, kind="ExternalInput").ap()
        return dram[name]

    def dscr(name, shape, dt, kind="Internal"):
        dram[name] = nc.dram_tensor(name, list(shape), dt, kind=kind).ap()
        return dram[name]

    x_d = din("x", [SEQ, D])
    gmix_d = din("g_mix", [1, D])
    w_in_d = din("w_in", [D, 5120])
    ident_bf_d = din("ident_bf", [128, 128], BF16)
    ident_f_d = din("ident_f", [128, 128], F32)
    lamre_d = din("lamre_l", [128, 16])
    lamim_d = din("lamim_l", [128, 16])
    logdt_d = din("logdt_l", [128, 16])
    bre_d = din("bre_l", [128, 16, 16])
    bim_d = din("bim_l", [128, 16, 16])
    cre_d = din("cre_l", [128, 16, 16])
    cim_d = din("cim_l", [128, 16, 16])
    dl_d = din("d_l", [128, 32])
    psel_d = din("psel", [128, 8, 240], BF16)
    cmask_d = din("cmask", [128, 128])
    mem_d = din("mem", [256, D])
    gmem_d = din("g_mem", [1, D])
    gffn_d = din("g_ffn", [1, D])
    gfin_d = din("g_final", [1, D])
    wkv_d = din("w_mem_kv", [D, 1024])
    wmo_d = din("w_mem_out", [512, D])
    wco_d = din("w_conv_out", [512, D])
    wgl_d = din("w_ssm_glu", [512, 2048])
    wo_d = din("w_out", [D, D])
    wr_d = din("w_router", [D, 36])
    rbias_d = din("b_router", [1, 36])
    cdw_d = din("cdw_l", [128, 4, 31])
    cb_d = din("cb_l", [128, 4])
    lng_d = din("lng_l", [128, 4])
    lnb_d = din("lnb_l", [128, 4])
    tri_d = din("tri", [128, 128])
    ecap_d = din("ecap", [128, 32])
    tokid_d = din("tokid", [128, NT])
    lst_init_d = din("lst_init", [NEXP * CAP + 128, 4])
    trashp_d = din("trashp", [128, 1])
    weg_d = din("w_exp_gate", [NEXP, 128, 8, 256])
    weu_d = din("w_exp_up", [NEXP, 128, 8, 256])
    wed_d = din("w_exp_down", [NEXP, 128, 2, D])
    dk = "ExternalOutput" if dbg else "Internal"
    lst_d = dscr("lst", [NEXP * CAP + 128, 4], F32, kind=dk)
    h2_scr = dscr("h2_scr", [ROWS, D], BF16, kind=dk)
    moe_scr = dscr("moe_scr", [2 * ROWS, D], BF16, kind=dk)
    x2_scr = dscr("x2_scr", [SEQ, D], F32, kind=dk)
    ys_scr = dscr("ys_scr", [4, 128, SEQ], BF16, kind="ExternalOutput" if dbg else "Internal")
    out_d = dscr("out", [SEQ, D], F32, kind="ExternalOutput")
    hT_scr = dscr("hT_scr", [8, 128, SEQ], BF16, kind="ExternalOutput" if dbg else "Internal")
    u_dbg = dscr("u_dbg", [4, 128, SEQ], BF16, kind="ExternalOutput") if dbg else None

    with ExitStack() as es:
        cx = Ctx(nc, es)
        al = Alloc(nc, es)
        block = es.enter_context(nc.Block())

        ident_bf = al.sb([128, 128], BF16, "ident_bf")
        cx.op("sp", lambda e: e.dma_start(out=ident_bf[:], in_=ident_bf_d), full=[ident_bf.b], dma=True)
        ident_f = al.sb([128, 128], F32, "ident_f")
        cx.op("sp", lambda e: e.dma_start(out=ident_f[:], in_=ident_f_d), full=[ident_f.b], dma=True)

        psum = [al.ps([128, 512], F32, f"bank{i}") for i in range(6)]
        psb = [al.ps([128, 1024], BF16, f"bankb{i}") for i in range(2)]
        pctr = [0]

        def getps():
            p = psum[pctr[0] % len(psum)]
            pctr[0] += 1
            return p

        alAB = Alloc(nc)
        u_all = alAB.sb([128, 4, SEQ], BF16, "u_all")
        M_all = alAB.sb([128, 32, 128], BF16, "M_all")
        W2r = alAB.sb([128, 16, 2, 128], BF16, "W2r"); W2i = alAB.sb([128, 16, 2, 128], BF16, "W2i")
        C1r = alAB.sb([128, 16, 128], BF16, "C1r"); nC1i = alAB.sb([128, 16, 128], BF16, "nC1i")
        KAr = alAB.sb([128, 9, 16], F32, "KAr"); KAi = alAB.sb([128, 9, 16], F32, "KAi")
        KnAi = alAB.sb([128, 9, 16], F32, "KnAi")
        psel = alAB.sb([128, 8, 240], BF16, "psel")
        cx.op("sp", lambda e: e.dma_start(out=psel[:], in_=psel_d), full=[psel.b], dma=True)
        al_outer = al
        al = Alloc(nc)
        gmix = al.sb([128, D], F32, "gmix")
        cx.op("sp", lambda e: e.dma_start(out=gmix[:], in_=gmix_d.partition_broadcast(128)),
              full=[gmix.b], dma=True)

        stg = [al.sb([128, 8, 256], F32, f"stg{i}") for i in range(2)]
        sctr = [0]

        def load_cast(dst, dst_col0, src_d, c0, c1, kch):
            for cc in range(c0, c1, 256):
                w = min(256, c1 - cc)
                s = stg[sctr[0] % 2]
                sctr[0] += 1
                src = src_d[:, cc:cc + w].rearrange("(c p) n -> p c n", p=128)
                cx.op("sp", lambda e, s=s, src=src, w=w: e.dma_start(out=s[:, 0:kch, 0:w], in_=src),
                      full=[s.b], dma=True)
                o = dst_col0 + (cc - c0)
                cx.op("pool", lambda e, s=s, o=o, w=w: e.tensor_copy(out=dst[:, 0:kch, o:o + w],
                                                                     in_=s[:, 0:kch, 0:w]),
                      reads=[s.b], writes=[dst.b])

        w_ssm_in = al.sb([128, 8, 512], BF16, "w_ssm_in")
        load_cast(w_ssm_in, 0, w_in_d, 1024, 1536, 8)
        xt = [al.sb([128, D], F32, f"xt{i}") for i in range(2)]
        junk = al.sb([128, D], BF16, "junk")
        ss = [al.sb([128, 1], F32, f"ss{i}") for i in range(2)]
        rt = [al.sb([128, 1], F32, f"rt{i}") for i in range(2)]
        rstd = [al.sb([128, 1], F32, f"rstd{i}") for i in range(2)]
        hbf = [al.sb([128, D], BF16, f"hbf{i}") for i in range(2)]
        hTb = [al.sb([128, 8, T], BF16, f"hTb{i}") for i in range(2)]

        for i in range(NT):
            p = i % 2
            blk = i // 4
            hb = hTb[blk % 2]
            cx.op("sp", lambda e, p=p, i=i: e.dma_start(out=xt[p][:], in_=x_d[i * 128:(i + 1) * 128, :]),
                  full=[xt[p].b], dma=True)
            cx.op("act", lambda e, p=p: e.activation(out=junk[:], in_=xt[p][:], func=AF.Square,
                                                     accum_out=ss[p][:]),
                  reads=[xt[p].b], writes=[junk.b], full=[ss[p].b])
            cx.op("act", lambda e, p=p: e.activation(out=rt[p][:], in_=ss[p][:], func=AF.Sqrt,
                                                     scale=1.0 / D, bias=EPS),
                  reads=[ss[p].b], full=[rt[p].b])
            cx.op("dve", lambda e, p=p: e.reciprocal(out=rstd[p][:], in_=rt[p][:]),
                  reads=[rt[p].b], full=[rstd[p].b])
            cx.op("dve", lambda e, p=p: e.scalar_tensor_tensor(out=hbf[p][:], in0=xt[p][:],
                                                               scalar=rstd[p][:, 0:1], in1=gmix[:],
                                                               op0=ALU.mult, op1=ALU.mult),
                  reads=[xt[p].b, rstd[p].b, gmix.b], full=[hbf[p].b])
            pb = psb[i % 2]
            for c in range(8):
                cx.op("pe", lambda e, pb=pb, p=p, c=c: e.transpose(out=pb[:, c * 128:(c + 1) * 128],
                                                                   in_=hbf[p][:, c * 128:(c + 1) * 128],
                                                                   identity=ident_bf[:]),
                      reads=[hbf[p].b, ident_bf.b], writes=[pb.b])
            tt = i % 4
            cx.op("act", lambda e, pb=pb, hb=hb, tt=tt: e.copy(
                out=hb[:, :, tt * 128:(tt + 1) * 128],
                in_=pb[:].rearrange("p (c t) -> p c t", c=8)),
                reads=[pb.b], writes=[hb.b])
            if tt == 3:
                for f in range(4):
                    ps = getps()
                    for c in range(8):
                        cx.op("pe", lambda e, ps=ps, hb=hb, f=f, c=c: e.matmul(
                            ps[:], lhsT=w_ssm_in[:, c, f * 128:(f + 1) * 128], rhs=hb[:, c, :],
                            start=(c == 0), stop=(c == 7)),
                            reads=[w_ssm_in.b, hb.b], writes=[ps.b])
                    cx.op("dve", lambda e, ps=ps, f=f, blk=blk: e.tensor_copy(
                        out=u_all[:, f, blk * T:(blk + 1) * T], in_=ps[:]),
                        reads=[ps.b], writes=[u_all.b])
                cx.op("sp", lambda e, hb=hb, blk=blk: e.dma_start(
                    out=hT_scr[:, :, blk * T:(blk + 1) * T].rearrange("c p t -> p c t"), in_=hb[:]),
                    reads=[hb.b], dma=True)

        if dbg:
            cx.op("sp", lambda e: e.dma_start(out=u_dbg.rearrange("f p t -> p f t"), in_=u_all[:]),
                  reads=[u_all.b], dma=True)


        cx.barrier()
        al.close()
        al = Alloc(nc)
        TWO_PI = 2.0 * np.pi
        cmask = al.sb([128, 128], F32, "cmask")
        cx.op("sp", lambda e: e.dma_start(out=cmask[:], in_=cmask_d), full=[cmask.b], dma=True)
        dl = al.sb([128, 32], F32, "dl")
        cx.op("sp", lambda e: e.dma_start(out=dl[:], in_=dl_d), full=[dl.b], dma=True)
        SU = Buf("ssm_setup")

        def sload(shape, src, name):
            t = al.sb(shape, F32, name)
            cx.op("sp", lambda e: e.dma_start(out=t[:], in_=src), full=[t.b], dma=True)
            return t

        lamre = sload([128, 16], lamre_d, "lamre")
        lamim = sload([128, 16], lamim_d, "lamim")
        logdt = sload([128, 16], logdt_d, "logdt")
        Bre = sload([128, 16, 16], bre_d, "Bre")
        Bim = sload([128, 16, 16], bim_d, "Bim")
        Cre = sload([128, 16, 16], cre_d, "Cre")
        Cim = sload([128, 16, 16], cim_d, "Cim")
        ins_b = [lamre.b, lamim.b, logdt.b, Bre.b, Bim.b, Cre.b, Cim.b]

        def S(shape, name):
            return al.sb(shape, F32, name)

        def dv(fn):
            cx.op("dve", fn, reads=ins_b, writes=[SU])

        def ac(fn):
            cx.op("act", fn, reads=ins_b, writes=[SU])

        def tt_(out, a, b, op):
            dv(lambda e: e.tensor_tensor(out=out, in0=a, in1=b, op=op))

        sh16 = [128, 16]
        dt_ = S(sh16, "dt"); lrd = S(sh16, "lrd"); th = S(sh16, "th")
        ac(lambda e: e.activation(out=dt_[:], in_=logdt[:], func=AF.Exp))
        tt_(lrd[:], lamre[:], dt_[:], ALU.mult)
        tt_(th[:], lamim[:], dt_[:], ALU.mult)
        mag = S(sh16, "mag"); imag2 = S(sh16, "imag2")
        ac(lambda e: e.activation(out=mag[:], in_=lrd[:], func=AF.Exp))
        ac(lambda e: e.activation(out=imag2[:], in_=lrd[:], func=AF.Exp, scale=-2.0))
        kq_i = al.sb(sh16, I32, "kq_i"); kq = S(sh16, "kq"); red = S(sh16, "red"); msk = S(sh16, "msk")
        sinv = S(sh16, "sinv"); cosv = S(sh16, "cosv"); tmpa = S(sh16, "tmpa")

        def sin_of(outt, shift):
            dv(lambda e: e.tensor_scalar(out=tmpa[:], in0=th[:], scalar1=float(shift), scalar2=None,
                                         op0=ALU.add))
            dv(lambda e: e.tensor_scalar(out=kq[:], in0=tmpa[:], scalar1=float(1.0 / TWO_PI),
                                         scalar2=None, op0=ALU.mult))
            dv(lambda e: e.tensor_copy(out=kq_i[:], in_=kq[:]))
            dv(lambda e: e.tensor_copy(out=kq[:], in_=kq_i[:]))
            dv(lambda e: e.scalar_tensor_tensor(out=red[:], in0=kq[:], scalar=float(-TWO_PI),
                                                in1=tmpa[:], op0=ALU.mult, op1=ALU.add))
            dv(lambda e: e.tensor_single_scalar(out=msk[:], in_=red[:], scalar=float(np.pi), op=ALU.is_gt))
            dv(lambda e: e.scalar_tensor_tensor(out=red[:], in0=msk[:], scalar=float(-TWO_PI),
                                                in1=red[:], op0=ALU.mult, op1=ALU.add))
            dv(lambda e: e.tensor_single_scalar(out=msk[:], in_=red[:], scalar=float(-np.pi), op=ALU.is_lt))
            dv(lambda e: e.scalar_tensor_tensor(out=red[:], in0=msk[:], scalar=float(TWO_PI),
                                                in1=red[:], op0=ALU.mult, op1=ALU.add))
            ac(lambda e: e.activation(out=outt[:], in_=red[:], func=AF.Sin))

        sin_of(sinv, 0.0)
        sin_of(cosv, np.pi / 2)
        PWr = S([128, 9, 16], "PWr"); PWi = S([128, 9, 16], "PWi")
        IPr = S([128, 8, 16], "IPr"); IPi = S([128, 8, 16], "IPi")
        t1 = S([128, 16, 8, 16], "t1"); t2 = S([128, 16, 8, 16], "t2")

        def cmul(outr, outi, ar, ai, br, bi, shp, neg_i=False):
            a1 = t1[:].rearrange("p a b c -> p (a b c)")[:, 0:int(np.prod(shp[1:]))]
            a2 = t2[:].rearrange("p a b c -> p (a b c)")[:, 0:int(np.prod(shp[1:]))]
            if len(shp) == 3:
                a1 = a1.rearrange("p (a b) -> p a b", a=shp[1])
                a2 = a2.rearrange("p (a b) -> p a b", a=shp[1])
            tt_(a1, ar, br, ALU.mult)
            tt_(a2, ai, bi, ALU.mult)
            tt_(outr, a1, a2, ALU.subtract)
            tt_(a1, ar, bi, ALU.mult)
            tt_(a2, ai, br, ALU.mult)
            if neg_i:
                dv(lambda e: e.scalar_tensor_tensor(out=outi, in0=a1, scalar=-1.0, in1=a2,
                                                    op0=ALU.mult, op1=ALU.subtract))
            else:
                tt_(outi, a1, a2, ALU.add)

        dv(lambda e: e.memset(PWr[:, 0, :], 1.0))
        dv(lambda e: e.memset(PWi[:, 0, :], 0.0))
        dv(lambda e: e.memset(IPr[:, 0, :], 1.0))
        dv(lambda e: e.memset(IPi[:, 0, :], 0.0))
        tt_(PWr[:, 1, :], mag[:], cosv[:], ALU.mult)
        tt_(PWi[:, 1, :], mag[:], sinv[:], ALU.mult)
        tt_(IPr[:, 1, :], PWr[:, 1, :], imag2[:], ALU.mult)
        dv(lambda e: e.scalar_tensor_tensor(out=IPi[:, 1, :], in0=PWi[:, 1, :], scalar=-1.0, in1=imag2[:],
                                            op0=ALU.mult, op1=ALU.mult))
        for n in range(2, 9):
            cmul(PWr[:, n, :], PWi[:, n, :], PWr[:, n - 1, :], PWi[:, n - 1, :], PWr[:, 1, :], PWi[:, 1, :], sh16)
        for n in range(2, 8):
            cmul(IPr[:, n, :], IPi[:, n, :], IPr[:, n - 1, :], IPi[:, n - 1, :], IPr[:, 1, :], IPi[:, 1, :], sh16)
        dv(lambda e: e.tensor_copy(out=KAr[:, 0, :], in_=PWr[:, 8, :]))
        dv(lambda e: e.tensor_copy(out=KAi[:, 0, :], in_=PWi[:, 8, :]))
        for d_ in range(1, 9):
            cmul(KAr[:, d_, :], KAi[:, d_, :], KAr[:, d_ - 1, :], KAi[:, d_ - 1, :],
                 KAr[:, d_ - 1, :], KAi[:, d_ - 1, :], sh16)
        dv(lambda e: e.tensor_scalar(out=KnAi[:], in0=KAi[:], scalar1=-1.0, scalar2=None, op0=ALU.mult))
        am1 = S(sh16, "am1"); l2 = S(sh16, "l2"); il2 = S(sh16, "il2"); kr = S(sh16, "kr"); ki = S(sh16, "ki")
        dv(lambda e: e.tensor_scalar(out=am1[:], in0=PWr[:, 1, :], scalar1=-1.0, scalar2=None, op0=ALU.add))
        tt_(l2[:], lamre[:], lamre[:], ALU.mult)
        tt_(tmpa[:], lamim[:], lamim[:], ALU.mult)
        tt_(l2[:], l2[:], tmpa[:], ALU.add)
        dv(lambda e: e.reciprocal(out=il2[:], in_=l2[:]))
        tt_(kr[:], am1[:], lamre[:], ALU.mult)
        tt_(tmpa[:], PWi[:, 1, :], lamim[:], ALU.mult)
        tt_(kr[:], kr[:], tmpa[:], ALU.add)
        tt_(kr[:], kr[:], il2[:], ALU.mult)
        tt_(ki[:], PWi[:, 1, :], lamre[:], ALU.mult)
        tt_(tmpa[:], am1[:], lamim[:], ALU.mult)
        tt_(ki[:], ki[:], tmpa[:], ALU.subtract)
        tt_(ki[:], ki[:], il2[:], ALU.mult)
        sh3 = [128, 16, 16]

        def bc(a):
            return a.unsqueeze(2).to_broadcast(sh3)

        Bbr = S(sh3, "Bbr"); Bbi = S(sh3, "Bbi")
        cmul(Bbr[:], Bbi[:], bc(kr[:]), bc(ki[:]), Bre[:], Bim[:], sh3)
        Bhr = S([128, 16, 8, 16], "Bhr"); nBhi = S([128, 16, 8, 16], "nBhi"); Bhi = S([128, 16, 8, 16], "Bhi")
        Btr = S([128, 16, 8, 16], "Btr"); Bti = S([128, 16, 8, 16], "Bti")
        Chr = S([128, 16, 9, 16], "Chr"); Chi = S([128, 16, 9, 16], "Chi"); nChi = S([128, 16, 9, 16], "nChi")
        for k in range(8):
            cmul(Bhr[:, :, k, :], Bhi[:, :, k, :], bc(IPr[:, k, :]), bc(IPi[:, k, :]), Bbr[:], Bbi[:], sh3)
            cmul(Btr[:, :, k, :], Bti[:, :, k, :], bc(PWr[:, 7, :]), bc(PWi[:, 7, :]),
                 Bhr[:, :, k, :], Bhi[:, :, k, :], sh3)
        dv(lambda e: e.tensor_scalar(out=nBhi[:], in0=Bhi[:], scalar1=-1.0, scalar2=None, op0=ALU.mult))
        for j in range(9):
            cmul(Chr[:, :, j, :], Chi[:, :, j, :], bc(PWr[:, j, :]), bc(PWi[:, j, :]), Cre[:], Cim[:], sh3)
        dv(lambda e: e.tensor_scalar(out=nChi[:], in0=Chi[:], scalar1=-1.0, scalar2=None, op0=ALU.mult))
        dv(lambda e: e.tensor_copy(out=C1r[:].rearrange("p r (j c) -> p r j c", j=8), in_=Chr[:, :, 1:9, :]))
        dv(lambda e: e.tensor_copy(out=nC1i[:].rearrange("p r (j c) -> p r j c", j=8), in_=nChi[:, :, 1:9, :]))
        mtmp = S([128, 128], "mtmp")
        cx.op("pool", lambda e: e.memset(W2r[:], 0.0), reads=ins_b, writes=[SU])
        cx.op("pool", lambda e: e.memset(W2i[:], 0.0), reads=ins_b, writes=[SU])
        for r in range(16):
            for two in range(2):
                g = 2 * r + two
                rng = slice(two * 64, (two + 1) * 64)
                ps = getps()
                cx.op("pe", lambda e, ps=ps, r=r, rng=rng: e.matmul(
                    ps[:, 0:128], lhsT=Bhr[rng, r, :, :].rearrange("p k c -> p (k c)"),
                    rhs=Chr[rng, r, 0:8, :].rearrange("p j c -> p (j c)"), start=True, stop=False),
                    reads=[SU], writes=[ps.b])
                cx.op("pe", lambda e, ps=ps, r=r, rng=rng: e.matmul(
                    ps[:, 0:128], lhsT=nBhi[rng, r, :, :].rearrange("p k c -> p (k c)"),
                    rhs=Chi[rng, r, 0:8, :].rearrange("p j c -> p (j c)"), start=False, stop=True),
                    reads=[SU], writes=[ps.b])
                cx.op("dve", lambda e, ps=ps: e.tensor_tensor(out=mtmp[:], in0=ps[:, 0:128], in1=cmask[:],
                                                              op=ALU.mult),
                      reads=[ps.b, cmask.b], writes=[SU])
                cx.op("dve", lambda e, g=g: e.scalar_tensor_tensor(
                    out=M_all[:, g, :], in0=ident_f[:], scalar=dl[:, g:g + 1], in1=mtmp[:],
                    op0=ALU.mult, op1=ALU.add),
                    reads=[ident_f.b, dl.b], writes=[SU, M_all.b])
            for (Bt, W2) in ((Btr, W2r), (Bti, W2i)):
                ps = getps()
                cx.op("pe", lambda e, ps=ps, r=r, Bt=Bt: e.transpose(
                    out=ps[:, 0:128], in_=Bt[:, r, :, :].rearrange("p k c -> p (k c)"), identity=ident_f[:]),
                    reads=[SU, ident_f.b], writes=[ps.b])
                cx.op("dve", lambda e, ps=ps, r=r, W2=W2: e.tensor_copy(out=W2[:, r, 0, 0:64], in_=ps[:, 0:64]),
                      reads=[ps.b], writes=[SU, W2.b])
                cx.op("dve", lambda e, ps=ps, r=r, W2=W2: e.tensor_copy(out=W2[:, r, 1, 64:128], in_=ps[:, 64:128]),
                      reads=[ps.b], writes=[SU, W2.b])

        cx.barrier()
        al.close()
        al = Alloc(nc)
        NCH = SEQ // 8
        Vg = [al.sb([128, NCH], BF16, f"Vg{i}") for i in range(4)]
        Sre = [[al.sb([128, NCH], F32, f"Sre{s}{i}") for i in range(2)] for s in range(2)]
        Sim = [[al.sb([128, NCH], F32, f"Sim{s}{i}") for i in range(2)] for s in range(2)]
        Sbr = [al.sb([128, NCH], BF16, f"Sbr{s}") for s in range(2)]
        Sbi = [al.sb([128, NCH], BF16, f"Sbi{s}") for s in range(2)]
        Gg = [al.sb([128, NCH], BF16, f"Gg{i}") for i in range(16)]
        ysf = [al.sb([128, SEQ], BF16, f"ysf{i}") for i in range(2)]
        for s in range(2):
            cx.op("pool", lambda e, s=s: e.memset(Sbr[s][:, 0:1], 0.0), writes=[Sbr[s].b])
            cx.op("pool", lambda e, s=s: e.memset(Sbi[s][:, 0:1], 0.0), writes=[Sbi[s].b])

        def b_front(r):
            f = r // 4
            st = r % 2
            vg = [Vg[(2 * r) % 4], Vg[(2 * r + 1) % 4]]
            for two in range(2):
                g = 2 * r + two
                gl = g % 8
                ps = getps()
                for k in range(8):
                    cx.op("pe", lambda e, ps=ps, gl=gl, k=k, f=f: e.matmul(
                        ps[:], lhsT=psel[:, gl, (7 - k) * 16:(7 - k) * 16 + 128],
                        rhs=u_all[:, f, k:SEQ:8], start=(k == 0), stop=(k == 7)),
                        reads=[psel.b, u_all.b], writes=[ps.b])
                cx.op("act", lambda e, ps=ps, v=vg[two]: e.copy(out=v[:], in_=ps[:]),
                      reads=[ps.b], full=[vg[two].b])
            psr = getps(); psi = getps()
            for (pp, W2) in ((psr, W2r), (psi, W2i)):
                for two in range(2):
                    cx.op("pe", lambda e, pp=pp, W2=W2, two=two, r=r, v=vg[two]: e.matmul(
                        pp[:], lhsT=W2[:, r, two, :], rhs=v[:], start=(two == 0), stop=(two == 1)),
                        reads=[W2.b, vg[two].b], writes=[pp.b])
            cx.op("act", lambda e, psr=psr, st=st: e.copy(out=Sre[st][0][:], in_=psr[:]),
                  reads=[psr.b], full=[Sre[st][0].b])
            cx.op("act", lambda e, psi=psi, st=st: e.copy(out=Sim[st][0][:], in_=psi[:]),
                  reads=[psi.b], full=[Sim[st][0].b])

        def b_mid(r):
            st = r % 2
            cur = 0
            for d_ in range(9):
                sh = 1 << d_
                s_r, s_i, d_r, d_i = Sre[st][cur], Sim[st][cur], Sre[st][1 - cur], Sim[st][1 - cur]
                n = NCH - sh
                cx.op("dve", lambda e, s_r=s_r, d_r=d_r, sh=sh, n=n, d_=d_, r=r: e.scalar_tensor_tensor(
                    out=d_r[:, sh:NCH], in0=s_r[:, 0:n], scalar=KAr[:, d_, r:r + 1], in1=s_r[:, sh:NCH],
                    op0=ALU.mult, op1=ALU.add), reads=[s_r.b, SU], writes=[d_r.b])
                cx.op("dve", lambda e, s_i=s_i, d_r=d_r, sh=sh, n=n, d_=d_, r=r: e.scalar_tensor_tensor(
                    out=d_r[:, sh:NCH], in0=s_i[:, 0:n], scalar=KnAi[:, d_, r:r + 1], in1=d_r[:, sh:NCH],
                    op0=ALU.mult, op1=ALU.add), reads=[s_i.b, SU], writes=[d_r.b])
                cx.op("dve", lambda e, s_i=s_i, d_i=d_i, sh=sh, n=n, d_=d_, r=r: e.scalar_tensor_tensor(
                    out=d_i[:, sh:NCH], in0=s_i[:, 0:n], scalar=KAr[:, d_, r:r + 1], in1=s_i[:, sh:NCH],
                    op0=ALU.mult, op1=ALU.add), reads=[s_i.b, SU], writes=[d_i.b])
                cx.op("dve", lambda e, s_r=s_r, d_i=d_i, sh=sh, n=n, d_=d_, r=r: e.scalar_tensor_tensor(
                    out=d_i[:, sh:NCH], in0=s_r[:, 0:n], scalar=KAi[:, d_, r:r + 1], in1=d_i[:, sh:NCH],
                    op0=ALU.mult, op1=ALU.add), reads=[s_r.b, SU], writes=[d_i.b])
                cx.op("pool", lambda e, s_r=s_r, d_r=d_r, sh=sh: e.tensor_copy(out=d_r[:, 0:sh], in_=s_r[:, 0:sh]),
                      reads=[s_r.b], writes=[d_r.b])
                cx.op("pool", lambda e, s_i=s_i, d_i=d_i, sh=sh: e.tensor_copy(out=d_i[:, 0:sh], in_=s_i[:, 0:sh]),
                      reads=[s_i.b], writes=[d_i.b])
                cur = 1 - cur
            fr, fi = Sre[st][cur], Sim[st][cur]
            cx.op("pool", lambda e, fr=fr, st=st: e.tensor_copy(out=Sbr[st][:, 1:NCH], in_=fr[:, 0:NCH - 1]),
                  reads=[fr.b], writes=[Sbr[st].b])
            cx.op("pool", lambda e, fi=fi, st=st: e.tensor_copy(out=Sbi[st][:, 1:NCH], in_=fi[:, 0:NCH - 1]),
                  reads=[fi.b], writes=[Sbi[st].b])

        def b_back(r):
            f = r // 4
            st = r % 2
            vg = [Vg[(2 * r) % 4], Vg[(2 * r + 1) % 4]]
            for two in range(2):
                g = 2 * r + two
                rng = slice(two * 64, (two + 1) * 64)
                ps = getps()
                cx.op("pe", lambda e, ps=ps, g=g, v=vg[two]: e.matmul(
                    ps[:], lhsT=M_all[:, g, :], rhs=v[:], start=True, stop=False),
                    reads=[M_all.b, vg[two].b], writes=[ps.b])
                cx.op("pe", lambda e, ps=ps, r=r, rng=rng, st=st: e.matmul(
                    ps[:], lhsT=C1r[rng, r, :], rhs=Sbr[st][rng, :], start=False, stop=False),
                    reads=[SU, Sbr[st].b], writes=[ps.b])
                cx.op("pe", lambda e, ps=ps, r=r, rng=rng, st=st: e.matmul(
                    ps[:], lhsT=nC1i[rng, r, :], rhs=Sbi[st][rng, :], start=False, stop=True),
                    reads=[SU, Sbi[st].b], writes=[ps.b])
                gg = Gg[g % 16]
                cx.op("act", lambda e, ps=ps, gg=gg: e.activation(out=gg[:], in_=ps[:], func=GELU),
                      reads=[ps.b], full=[gg.b])
            if r % 4 == 3:
                yb = ysf[f % 2]
                for j in range(8):
                    ps = getps()
                    for gl in range(8):
                        gg = Gg[(8 * f + gl) % 16]
                        cx.op("pe", lambda e, ps=ps, j=j, gl=gl, gg=gg: e.matmul(
                            ps[:], lhsT=psel[:, j, (7 - gl) * 16:(7 - gl) * 16 + 128], rhs=gg[:],
                            start=(gl == 0), stop=(gl == 7)),
                            reads=[psel.b, gg.b], writes=[ps.b])
                    cx.op("act", lambda e, ps=ps, yb=yb, j=j: e.copy(out=yb[:, j:SEQ:8], in_=ps[:]),
                          reads=[ps.b], writes=[yb.b])
                cx.op("sp", lambda e, yb=yb, f=f: e.dma_start(out=ys_scr[f], in_=yb[:]),
                      reads=[yb.b], dma=True)

        b_front(0)
        for r in range(16):
            b_mid(r)
            if r + 1 < 16:
                b_front(r + 1)
            b_back(r)

        cx.barrier()
        al.close()
        alAB.close()
        al = al_outer
        if stop_after in ("A", "B"):
            pass
        else:
            TC = 256
            NBC = SEQ // TC
            alC = Alloc(nc)
            wA = alC.sb([128, 8, 1536], BF16, "wA")
            wG = alC.sb([128, 8, 3072], BF16, "wG")
            wco = alC.sb([128, 4, 1024], BF16, "wco")
            wgl = alC.sb([128, 4, 2048], BF16, "wgl")
            wmo = alC.sb([128, 4, 1024], BF16, "wmo")
            wo = alC.sb([128, 8, 1024], BF16, "wo")
            Dg2 = [alC.sb([128, 31, 128], BF16, f"Dg{i}") for i in range(2)]
            kT = alC.sb([128, 4, 256], BF16, "kT")
            vtok = alC.sb([128, 2, 512], BF16, "vtok")
            gffn = alC.sb([128, D], F32, "gffn")
            wr = alC.sb([128, 8, 36], F32, "wr")
            rbias = alC.sb([128, 36], F32, "rbias")
            cdw = alC.sb([128, 4, 31], F32, "cdw")
            cb = alC.sb([128, 4], F32, "cb"); lng = alC.sb([128, 4], F32, "lng"); lnb = alC.sb([128, 4], F32, "lnb")
            onesm = alC.sb([128, 128], F32, "onesm")
            ones_bf = alC.sb([128, 128], BF16, "ones_bf")
            ecap = alC.sb([128, 32], F32, "ecap")
            tokid = alC.sb([128, NT], F32, "tokid")
            cum = alC.sb([128, 32], F32, "cum")
            lg_all = alC.sb([128, NT, 36], F32, "lg_all")
            trashp = alC.sb([128, 1], F32, "trashp")

            def ld(t, src):
                cx.op("sp", lambda e: e.dma_start(out=t[:], in_=src), full=[t.b], dma=True)

            ld(gffn, gffn_d.partition_broadcast(128))
            ld(wr, wr_d.rearrange("(c p) n -> p c n", p=128))
            ld(rbias, rbias_d.partition_broadcast(128))
            ld(cdw, cdw_d); ld(cb, cb_d); ld(lng, lng_d); ld(lnb, lnb_d)
            ld(ecap, ecap_d); ld(tokid, tokid_d); ld(trashp, trashp_d)
            cx.op("pool", lambda e: e.memset(onesm[:], 1.0 / 512.0), full=[onesm.b])
            cx.op("pool", lambda e: e.memset(ones_bf[:], 1.0), full=[ones_bf.b])
            cx.op("pool", lambda e: e.memset(cum[:], 0.0), full=[cum.b])
            alS = Alloc(nc)
            zt = alS.sb([128, 1024], F32, "zt")
            cx.op("pool", lambda e: e.memset(zt[:], 0.0), full=[zt.b])
            lstB = Buf("lst"); h2B = Buf("h2scr"); moeB = Buf("moescr"); x2B = Buf("x2scr")
            cx.op("sp", lambda e: e.dma_start(out=lst_d, in_=lst_init_d), full=[lstB], dma=True)
            cx.op("sp", lambda e: e.dma_start(out=h2_scr[SEQ:ROWS, :], in_=zt[:, 0:512].bitcast(BF16)),
                  reads=[zt.b], writes=[h2B], dma=True)
            moe_flat = moe_scr.rearrange("(n p) d -> n p d", p=128)
            for n in range(0, 2 * ROWS // 128):
                cx.op("sp", lambda e, n=n: e.dma_start(out=moe_flat[n], in_=zt[:, 0:512].bitcast(BF16)),
                      reads=[zt.b], writes=[moeB], dma=True)

            stg2 = [alS.sb([128, 8, 256], F32, f"stgc{i}") for i in range(2)]
            s2 = [0]

            def load_cast2(dst, dst_col0, src_d, c0, c1, kch, engs=("pool", "act")):
                for cc in range(c0, c1, 256):
                    w = min(256, c1 - cc)
                    s = stg2[s2[0] % 2]
                    eng = engs[s2[0] % len(engs)]
                    s2[0] += 1
                    src = src_d[:, cc:cc + w].rearrange("(c p) n -> p c n", p=128)
                    cx.op("sp", lambda e, s=s, src=src, w=w: e.dma_start(out=s[:, 0:kch, 0:w], in_=src),
                          full=[s.b], dma=True)
                    o = dst_col0 + (cc - c0)
                    if eng == "act":
                        cx.op("act", lambda e, s=s, o=o, w=w: e.copy(out=dst[:, 0:kch, o:o + w], in_=s[:, 0:kch, 0:w]),
                              reads=[s.b], writes=[dst.b])
                    else:
                        cx.op(eng, lambda e, s=s, o=o, w=w: e.tensor_copy(out=dst[:, 0:kch, o:o + w],
                                                                          in_=s[:, 0:kch, 0:w]),
                              reads=[s.b], writes=[dst.b])

            load_cast2(wA, 0, w_in_d, 0, 1024, 8)
            load_cast2(wA, 1024, w_in_d, 1536, 2048, 8)
            load_cast2(wG, 0, w_in_d, 2048, 5120, 8)
            load_cast2(wco, 0, wco_d, 0, 1024, 4)
            load_cast2(wgl, 0, wgl_d, 0, 2048, 4)
            load_cast2(wmo, 0, wmo_d, 0, 1024, 4)
            load_cast2(wo, 0, wo_d, 0, 1024, 8)
            wkv = alS.sb([128, 8, 1024], BF16, "wkv")
            load_cast2(wkv, 0, wkv_d, 0, 1024, 8)
            gmem = alS.sb([128, D], F32, "gmem")
            ld(gmem, gmem_d.partition_broadcast(128))
            memT = alS.sb([128, 8, 256], BF16, "memT")
            mx = alS.sb([128, D], F32, "mx"); mjunk = alS.sb([128, D], BF16, "mjunk")
            mss = alS.sb([128, 1], F32, "mss"); mrt = alS.sb([128, 1], F32, "mrt"); mrs = alS.sb([128, 1], F32, "mrs")
            mh = alS.sb([128, D], BF16, "mh")
            for mt in range(2):
                cx.op("sp", lambda e, mt=mt: e.dma_start(out=mx[:], in_=mem_d[mt * 128:(mt + 1) * 128, :]),
                      full=[mx.b], dma=True)
                cx.op("act", lambda e: e.activation(out=mjunk[:], in_=mx[:], func=AF.Square, accum_out=mss[:]),
                      reads=[mx.b], full=[mjunk.b, mss.b])
                cx.op("act", lambda e: e.activation(out=mrt[:], in_=mss[:], func=AF.Sqrt, scale=1.0 / D, bias=EPS),
                      reads=[mss.b], full=[mrt.b])
                cx.op("dve", lambda e: e.reciprocal(out=mrs[:], in_=mrt[:]), reads=[mrt.b], full=[mrs.b])
                cx.op("dve", lambda e: e.scalar_tensor_tensor(out=mh[:], in0=mx[:], scalar=mrs[:, 0:1], in1=gmem[:],
                                                              op0=ALU.mult, op1=ALU.mult),
                      reads=[mx.b, mrs.b, gmem.b], full=[mh.b])
                pb = psb[mt % 2]
                for c in range(8):
                    cx.op("pe", lambda e, pb=pb, c=c: e.transpose(out=pb[:, c * 128:(c + 1) * 128],
                                                                  in_=mh[:, c * 128:(c + 1) * 128],
                                                                  identity=ident_bf[:]),
                          reads=[mh.b, ident_bf.b], writes=[pb.b])
                cx.op("act", lambda e, pb=pb, mt=mt: e.copy(out=memT[:, :, mt * 128:(mt + 1) * 128],
                                                            in_=pb[:].rearrange("p (c t) -> p c t", c=8)),
                      reads=[pb.b], writes=[memT.b])
            for hd in range(4):
                ps = getps()
                for c in range(8):
                    cx.op("pe", lambda e, ps=ps, c=c, hd=hd: e.matmul(
                        ps[:, 0:256], lhsT=wkv[:, c, hd * 128:(hd + 1) * 128], rhs=memT[:, c, :],
                        start=(c == 0), stop=(c == 7)), reads=[wkv.b, memT.b], writes=[ps.b])
                cx.op("dve", lambda e, ps=ps, hd=hd: e.tensor_copy(out=kT[:, hd, :], in_=ps[:, 0:256]),
                      reads=[ps.b], writes=[kT.b])
            for mc in range(2):
                ps = getps()
                for c in range(8):
                    cx.op("pe", lambda e, ps=ps, c=c, mc=mc: e.matmul(
                        ps[:], lhsT=memT[:, c, mc * 128:(mc + 1) * 128], rhs=wkv[:, c, 512:1024],
                        start=(c == 0), stop=(c == 7)), reads=[wkv.b, memT.b], writes=[ps.b])
                cx.op("dve", lambda e, ps=ps, mc=mc: e.tensor_copy(out=vtok[:, mc, :], in_=ps[:]),
                      reads=[ps.b], writes=[vtok.b])
            cx.barrier()
            alS.close()

            alW = Alloc(nc)
            hT = [alW.sb([128, 8, TC], BF16, f"hTc{i}") for i in range(2)]
            ysb = [alW.sb([128, 4, TC], BF16, "ysb0")] * 2
            vbuf = alW.sb([128, 4, 30 + TC], BF16, "vbuf")
            sgt = [alW.sb([128, TC], F32, f"sgt{i}") for i in range(3)]
            cv = alW.sb([128, 4, TC], F32, "cv")
            sq = [sgt[1], sgt[2]]
            mean = alW.sb([128, TC], F32, "mean")
            var = alW.sb([128, TC], F32, "var"); lrs = alW.sb([128, TC], F32, "lrs")
            m2 = var; lnv = lrs
            cn = alW.sb([128, 4, TC], BF16, "cn")
            qb = alW.sb([128, 4, TC], BF16, "qb")
            Eb = [alW.sb([128, 2, TC], BF16, f"Eb{i}") for i in range(2)]
            ob = alW.sb([128, 4, TC], BF16, "ob")
            macc = alW.sb([128, TC], F32, "macc"); mt1 = alW.sb([128, TC], F32, "mt1"); mt2 = alW.sb([128, TC], F32, "mt2")
            rden = macc
            xc = [mt1, mt2]
            sqf = [sgt[1], sgt[2], mt1, mt2]
            merged = alW.sb([128, 8, TC], BF16, "merged")
            xt2 = [alW.sb([128, D], F32, f"xtc{i}") for i in range(2)]
            x2t = xt2
            h2f = alW.sb([128, D], F32, "h2f"); h2b = [alW.sb([128, D], BF16, "h2b0")] * 2
            junk2 = h2b[0]
            h2T = alW.sb([128, 8, 128], F32, "h2T")
            ss2 = alW.sb([128, 1], F32, "ss2"); rt2 = alW.sb([128, 1], F32, "rt2"); rs2 = alW.sb([128, 1], F32, "rs2")
            cx.op("pool", lambda e: e.memset(vbuf[:], 0.0), full=[vbuf.b])

            breg = {}

            def mmgrp(ps_ap, ps_b, pairs, reads):
                n = len(pairs)
                for idx, (l, r_) in enumerate(pairs):
                    cx.op("pe", lambda e, l=l, r_=r_, idx=idx: e.matmul(ps_ap, lhsT=l, rhs=r_, start=(idx == 0),
                                                                         stop=(idx == n - 1)),
                          reads=reads, writes=[ps_b])

            KCUT = int(os.environ.get("KCUT", "9"))
            KNB = int(os.environ.get("KNB", str(NBC)))
            def c_load_h(bi):
                t0 = bi * TC
                h = hT[bi % 2]
                cx.op("sp", lambda e, h=h, t0=t0: e.dma_start(
                    out=h[:], in_=hT_scr[:, :, t0:t0 + TC].rearrange("c p t -> p c t")), full=[h.b], dma=True)

            def c_load_y(bi):
                t0 = bi * TC
                yb = ysb[bi % 2]
                cx.op("sp", lambda e, yb=yb, t0=t0: e.dma_start(
                    out=yb[:], in_=ys_scr[:, :, t0:t0 + TC].rearrange("f p t -> p f t")), full=[yb.b], dma=True)

            def c_s2(bi):
                t0 = bi * TC
                h = hT[bi % 2]; yb = ysb[bi % 2]
                for f in range(4):
                    pa = getps(); pg = getps()
                    mmgrp(pa[:, 0:TC], pa.b, [(wA[:, c, f * 128:(f + 1) * 128], h[:, c, :]) for c in range(8)],
                          [wA.b, h.b])
                    mmgrp(pg[:, 0:TC], pg.b, [(wA[:, c, 512 + f * 128:512 + (f + 1) * 128], h[:, c, :]) for c in range(8)],
                          [wA.b, h.b])
                    s = sgt[f % 3]
                    cx.op("act", lambda e, pg=pg, s=s: e.activation(out=s[:], in_=pg[:, 0:TC], func=AF.Sigmoid),
                          reads=[pg.b], full=[s.b])
                    cx.op("dve", lambda e, pa=pa, s=s, f=f: e.tensor_tensor(out=vbuf[:, f, 30:30 + TC], in0=pa[:, 0:TC],
                                                                            in1=s[:], op=ALU.mult),
                          reads=[pa.b, s.b], writes=[vbuf.b])

            def c_mid(bi):
                t0 = bi * TC
                h = hT[bi % 2]; yb = ysb[bi % 2]
                for f in range(4):
                    pc = getps()
                    Dg = Dg2[f % 2]
                    cx.op("pool", lambda e, Dg=Dg, f=f: e.tensor_tensor(
                        out=Dg[:], in0=ident_f[:].unsqueeze(1).to_broadcast([128, 31, 128]),
                        in1=cdw[:, f, :].unsqueeze(2).to_broadcast([128, 31, 128]), op=ALU.mult),
                        reads=[ident_f.b, cdw.b], full=[Dg.b])
                    mmgrp(pc[:, 0:TC], pc.b, [(Dg[:, k, :], vbuf[:, f, k:k + TC]) for k in range(31)],
                          [Dg.b, vbuf.b])
                    cx.op("act", lambda e, pc=pc, f=f: e.activation(out=cv[:, f, :], in_=pc[:, 0:TC], func=AF.Identity,
                                                                    bias=cb[:, f:f + 1], scale=1.0),
                          reads=[pc.b, cb.b], writes=[cv.b])
                cx.op("pool", lambda e: e.tensor_copy(out=vbuf[:, :, 0:30], in_=vbuf[:, :, TC:TC + 30]),
                      reads=[vbuf.b], writes=[vbuf.b])
                for hd in range(4):
                    pq_ = getps()
                    mmgrp(pq_[:, 0:TC], pq_.b, [(wA[:, c, 1024 + hd * 128:1024 + (hd + 1) * 128], h[:, c, :])
                                                for c in range(8)], [wA.b, h.b])
                    cx.op("dve", lambda e, pq_=pq_, hd=hd: e.tensor_copy(out=qb[:, hd, :], in_=pq_[:, 0:TC]),
                          reads=[pq_.b], writes=[qb.b])
                for f in range(4):
                    cx.op("act", lambda e, f=f: e.activation(out=sq[f % 2][:] if False else sqf[f][:], in_=cv[:, f, :],
                                                             func=AF.Square),
                          reads=[cv.b], full=[sqf[f].b])

                def att_scores(hd):
                    E = Eb[hd % 2]
                    for mc in range(2):
                        psc = getps()
                        mmgrp(psc[:, 0:TC], psc.b, [(kT[:, hd, mc * 128:(mc + 1) * 128], qb[:, hd, :])], [kT.b, qb.b])
                        cx.op("act", lambda e, psc=psc, E=E, mc=mc: e.activation(
                            out=E[:, mc, :], in_=psc[:, 0:TC], func=AF.Exp, scale=float(128 ** -0.5)),
                            reads=[psc.b], writes=[E.b])

                def att_out(hd):
                    E = Eb[hd % 2]
                    po = getps(); pd = getps()
                    mmgrp(po[:, 0:TC], po.b, [(vtok[:, mc, hd * 128:(hd + 1) * 128], E[:, mc, :]) for mc in range(2)],
                          [vtok.b, E.b])
                    mmgrp(pd[:, 0:TC], pd.b, [(ones_bf[:], E[:, mc, :]) for mc in range(2)], [ones_bf.b, E.b])
                    cx.op("dve", lambda e, pd=pd: e.reciprocal(out=rden[:], in_=pd[:, 0:TC]), reads=[pd.b], full=[rden.b])
                    cx.op("dve", lambda e, po=po, hd=hd: e.tensor_tensor(out=ob[:, hd, :], in0=po[:, 0:TC], in1=rden[:],
                                                                         op=ALU.mult),
                          reads=[po.b, rden.b], writes=[ob.b])

                att_scores(0)
                att_scores(1)
                pm = getps(); pq = getps()
                mmgrp(pm[:, 0:TC], pm.b, [(onesm[:], cv[:, f, :]) for f in range(4)], [onesm.b, cv.b])
                mmgrp(pq[:, 0:TC], pq.b, [(onesm[:], sqf[f][:]) for f in range(4)], [onesm.b] + [sqf[f].b for f in range(4)])
                cx.op("act", lambda e, pm=pm: e.copy(out=mean[:], in_=pm[:, 0:TC]), reads=[pm.b], full=[mean.b])
                cx.op("dve", lambda e: e.tensor_tensor(out=var[:], in0=mean[:], in1=mean[:], op=ALU.mult),
                      reads=[mean.b], full=[var.b])
                cx.op("dve", lambda e, pq=pq: e.tensor_tensor(out=var[:], in0=pq[:, 0:TC], in1=var[:], op=ALU.subtract),
                      reads=[pq.b], writes=[var.b])
                cx.op("dve", lambda e: e.tensor_scalar(out=var[:], in0=var[:], scalar1=float(EPS), scalar2=None,
                                                       op0=ALU.add), reads=[var.b], writes=[var.b])
                cx.op("act", lambda e: e.activation(out=lrs[:], in_=var[:], func=AF.Ln), reads=[var.b], full=[lrs.b])
                cx.op("act", lambda e: e.activation(out=lrs[:], in_=lrs[:], func=AF.Exp, scale=-0.5),
                      reads=[], writes=[lrs.b])
                cx.op("dve", lambda e: e.tensor_tensor(out=cv[:], in0=cv[:],
                                                       in1=mean[:].unsqueeze(1).to_broadcast([128, 4, TC]),
                                                       op=ALU.subtract), reads=[mean.b], writes=[cv.b])
                cx.op("dve", lambda e: e.tensor_tensor(out=cv[:], in0=cv[:],
                                                       in1=lrs[:].unsqueeze(1).to_broadcast([128, 4, TC]),
                                                       op=ALU.mult), reads=[lrs.b], writes=[cv.b])
                att_out(0)
                att_scores(2)
                att_out(1)
                att_scores(3)
                att_out(2)
                att_out(3)
                for f in range(4):
                    cx.op("act", lambda e, f=f: e.activation(out=cn[:, f, :], in_=cv[:, f, :], func=AF.Silu,
                                                             bias=lnb[:, f:f + 1], scale=lng[:, f:f + 1]),
                          reads=[cv.b, lnb.b, lng.b], writes=[cn.b])
                for j in range(8):
                    js = slice(j * 128, (j + 1) * 128)
                    pga = getps(); pyc = getps()
                    mmgrp(pga[:, 0:TC], pga.b, [(wG[:, c, j * 128:(j + 1) * 128], h[:, c, :]) for c in range(8)], [wG.b, h.b])
                    mmgrp(pyc[:, 0:TC], pyc.b, [(wco[:, f, js], cn[:, f, :]) for f in range(4)], [wco.b, cn.b])
                    s = sgt[0]
                    cx.op("act", lambda e, pga=pga, s=s: e.activation(out=s[:], in_=pga[:, 0:TC], func=AF.Sigmoid),
                          reads=[pga.b], full=[s.b])
                    cx.op("dve", lambda e, pyc=pyc, s=s: e.tensor_tensor(out=macc[:], in0=pyc[:, 0:TC], in1=s[:], op=ALU.mult),
                          reads=[pyc.b, s.b], full=[macc.b])
                    pgb = getps(); pza = getps(); pzb = getps()
                    mmgrp(pgb[:, 0:TC], pgb.b, [(wG[:, c, 1024 + j * 128:1024 + (j + 1) * 128], h[:, c, :]) for c in range(8)],
                          [wG.b, h.b])
                    mmgrp(pza[:, 0:TC], pza.b, [(wgl[:, f, js], yb[:, f, :]) for f in range(4)], [wgl.b, yb.b])
                    mmgrp(pzb[:, 0:TC], pzb.b, [(wgl[:, f, 1024 + j * 128:1024 + (j + 1) * 128], yb[:, f, :]) for f in range(4)],
                          [wgl.b, yb.b])
                    sb_ = sgt[1]; sz = sgt[2]
                    cx.op("act", lambda e, pgb=pgb, sb_=sb_: e.activation(out=sb_[:], in_=pgb[:, 0:TC], func=AF.Sigmoid),
                          reads=[pgb.b], full=[sb_.b])
                    cx.op("act", lambda e, pzb=pzb, sz=sz: e.activation(out=sz[:], in_=pzb[:, 0:TC], func=AF.Sigmoid),
                          reads=[pzb.b], full=[sz.b])
                    cx.op("dve", lambda e, pza=pza, sz=sz: e.tensor_tensor(out=mt1[:], in0=pza[:, 0:TC], in1=sz[:], op=ALU.mult),
                          reads=[pza.b, sz.b], full=[mt1.b])
                    cx.op("dve", lambda e, sb_=sb_: e.tensor_tensor(out=mt1[:], in0=mt1[:], in1=sb_[:], op=ALU.mult),
                          reads=[sb_.b], writes=[mt1.b])
                    cx.op("dve", lambda e: e.tensor_tensor(out=macc[:], in0=macc[:], in1=mt1[:], op=ALU.add),
                          reads=[mt1.b], writes=[macc.b])
                    pgc = getps(); pym = getps()
                    mmgrp(pgc[:, 0:TC], pgc.b, [(wG[:, c, 2048 + j * 128:2048 + (j + 1) * 128], h[:, c, :]) for c in range(8)],
                          [wG.b, h.b])
                    mmgrp(pym[:, 0:TC], pym.b, [(wmo[:, hd, js], ob[:, hd, :]) for hd in range(4)], [wmo.b, ob.b])
                    s = sgt[0]
                    cx.op("act", lambda e, pgc=pgc, s=s: e.activation(out=s[:], in_=pgc[:, 0:TC], func=AF.Sigmoid),
                          reads=[pgc.b], full=[s.b])
                    cx.op("dve", lambda e, pym=pym, s=s: e.tensor_tensor(out=mt2[:], in0=pym[:, 0:TC], in1=s[:], op=ALU.mult),
                          reads=[pym.b, s.b], full=[mt2.b])
                    cx.op("dve", lambda e, j=j: e.tensor_tensor(out=merged[:, j, :], in0=macc[:], in1=mt2[:], op=ALU.add),
                          reads=[macc.b, mt2.b], writes=[merged.b])

            def c_tail(bi):
                ntt = TC // 128
                tis = [bi * ntt + tt for tt in range(ntt)]
                for tt, ti in enumerate(tis):
                    xt_ = xt2[ti % 2]
                    cx.op("sp", lambda e, xt_=xt_, ti=ti: e.dma_start(out=xt_[:], in_=x_d[ti * 128:(ti + 1) * 128, :]),
                          full=[xt_.b], dma=True)
                for tt, ti in enumerate(tis):
                    xt_ = xt2[ti % 2]; x2 = xt_
                    for half in range(2):
                        po_ = getps()
                        mmgrp(po_[:], po_.b, [(merged[:, j, tt * 128:(tt + 1) * 128], wo[:, j, half * 512:(half + 1) * 512])
                                              for j in range(8)], [merged.b, wo.b])
                        cx.op("dve", lambda e, po_=po_, x2=x2, xt_=xt_, half=half: e.tensor_tensor(
                            out=x2[:, half * 512:(half + 1) * 512], in0=po_[:], in1=xt_[:, half * 512:(half + 1) * 512],
                            op=ALU.add), reads=[po_.b, xt_.b], writes=[x2.b])
                    cx.op("sp", lambda e, x2=x2, ti=ti: e.dma_start(out=x2_scr[ti * 128:(ti + 1) * 128, :], in_=x2[:]),
                          reads=[x2.b], writes=[x2B], dma=True)
                for tt, ti in enumerate(tis):
                    x2 = xt2[ti % 2]; hb2 = h2b[0]
                    cx.op("act", lambda e, x2=x2: e.activation(out=junk2[:], in_=x2[:], func=AF.Square, accum_out=ss2[:]),
                          reads=[x2.b], full=[junk2.b, ss2.b])
                    cx.op("act", lambda e: e.activation(out=rt2[:], in_=ss2[:], func=AF.Sqrt, scale=1.0 / D, bias=EPS),
                          reads=[ss2.b], full=[rt2.b])
                    cx.op("dve", lambda e: e.reciprocal(out=rs2[:], in_=rt2[:]), reads=[rt2.b], full=[rs2.b])
                    cx.op("dve", lambda e, x2=x2: e.scalar_tensor_tensor(out=h2f[:], in0=x2[:], scalar=rs2[:, 0:1],
                                                                         in1=gffn[:], op0=ALU.mult, op1=ALU.mult),
                          reads=[x2.b, rs2.b, gffn.b], full=[h2f.b])
                    cx.op("act", lambda e, hb2=hb2: e.copy(out=hb2[:], in_=h2f[:]), reads=[h2f.b], full=[hb2.b])
                    cx.op("sp", lambda e, hb2=hb2, ti=ti: e.dma_start(out=h2_scr[ti * 128:(ti + 1) * 128, :], in_=hb2[:]),
                          reads=[hb2.b], writes=[h2B], dma=True)
                    pra = getps(); prb = getps()
                    for c in range(8):
                        pr = pra if c < 4 else prb
                        cx.op("pe", lambda e, pr=pr, c=c: e.transpose(out=pr[:, (c % 4) * 128:(c % 4 + 1) * 128],
                                                                      in_=h2f[:, c * 128:(c + 1) * 128], identity=ident_f[:]),
                              reads=[h2f.b, ident_f.b], writes=[pr.b])
                    cx.op("act", lambda e, pra=pra: e.copy(out=h2T[:, 0:4, :], in_=pra[:].rearrange("p (c t) -> p c t", c=4)),
                          reads=[pra.b], writes=[h2T.b])
                    cx.op("dve", lambda e, prb=prb: e.tensor_copy(out=h2T[:, 4:8, :],
                                                                  in_=prb[:].rearrange("p (c t) -> p c t", c=4)),
                          reads=[prb.b], writes=[h2T.b])
                    plg = getps()
                    mmgrp(plg[:, 0:36], plg.b, [(h2T[:, c, :], wr[:, c, :]) for c in range(8)], [h2T.b, wr.b])
                    cx.op("dve", lambda e, plg=plg, ti=ti: e.tensor_tensor(out=lg_all[:, ti, :], in0=plg[:, 0:36],
                                                                        in1=rbias[:], op=ALU.add),
                          reads=[plg.b, rbias.b], writes=[lg_all.b])

            NBR = min(NBC, KNB) if KCUT >= 2 else 0
            if NBR > 0:
                c_load_h(0); c_load_y(0); c_s2(0)
            for bi in range(NBR):
                if bi + 1 < NBR:
                    c_load_h(bi + 1)
                c_mid(bi)
                if bi + 1 < NBR:
                    c_load_y(bi + 1)
                    c_s2(bi + 1)
                c_tail(bi)
            cx.barrier()
            alW.close()
            alR = Alloc(nc)
            RS = Buf("route")
            tri = alR.sb([128, 128], F32, "tri")
            ones_f = alR.sb([128, 128], F32, "ones_f")
            cx.op("sp", lambda e: e.dma_start(out=tri[:], in_=tri_d), full=[tri.b], dma=True)
            cx.op("pool", lambda e: e.memset(ones_f[:], 1.0), full=[ones_f.b])

            def rd(fn, extra_reads=(), extra_writes=()):
                cx.op("dve", fn, reads=[RS, lg_all.b] + list(extra_reads), writes=[RS] + list(extra_writes))

            def R(shape, name, dt=F32):
                return alR.sb(shape, dt, name)

            NTT = NT
            NEB_ = NEXP * NBLK
            gmax = R([128, NTT], "gmax"); ohg = R([128, NTT, 4], "ohg"); eg = R([128, NTT, 4], "eg")
            sumg = R([128, NTT], "sumg"); ptop = R([128, NTT], "ptop")
            selm = R([128, NTT, 4, 8], "selm"); sel = R([128, NTT, 8], "sel"); sel2 = R([128, NTT, 8], "sel2")
            m1_ = R([128, NTT], "m1_"); m2_ = R([128, NTT], "m2_"); oh1 = R([128, NTT, 8], "oh1"); oh2 = R([128, NTT, 8], "oh2")
            dm = R([128, NTT], "dm"); w1 = R([128, NTT], "w1"); w2 = R([128, NTT], "w2")
            M1 = R([128, NTT, 4, 8], "M1"); M2 = R([128, NTT, 4, 8], "M2"); Mc = R([128, NTT, 32], "Mc")
            Cex = R([128, NTT, 32], "Cex"); pos = R([128, NTT, 32], "pos"); bk = R([128, NTT, 32], "bk")
            sf = R([128, NTT, 32], "sf"); ov = R([128, NTT, 32], "ov"); tq = R([128, NTT, 32], "tq")
            sk = [R([128, NTT], f"sk{k}") for k in range(2)]; okk = R([128, NTT], "okk"); dd = R([128, NTT], "dd")
            si = [R([128, NTT], f"si{k}", I32) for k in range(2)]
            ent = [R([128, NTT, 4], f"ent{k}") for k in range(2)]
            le4 = lg_all[:, :, 4:36].rearrange("p t (g j) -> p t g j", g=4)

            def bc3(a, n):
                return a.unsqueeze(2).to_broadcast([128, NTT, n])

            rd(lambda e: e.tensor_reduce(out=gmax[:], in_=lg_all[:, :, 0:4], axis=AX.X, op=ALU.max))
            rd(lambda e: e.tensor_tensor(out=ohg[:], in0=lg_all[:, :, 0:4], in1=bc3(gmax[:], 4), op=ALU.is_equal))
            rd(lambda e: e.tensor_tensor(out=eg[:], in0=lg_all[:, :, 0:4], in1=bc3(gmax[:], 4), op=ALU.subtract))
            cx.op("act", lambda e: e.activation(out=eg[:], in_=eg[:], func=AF.Exp), reads=[RS], writes=[RS])
            rd(lambda e: e.tensor_reduce(out=sumg[:], in_=eg[:], axis=AX.X, op=ALU.add))
            rd(lambda e: e.reciprocal(out=ptop[:], in_=sumg[:]))
            rd(lambda e: e.tensor_tensor(out=selm[:], in0=le4,
                                         in1=ohg[:].unsqueeze(3).to_broadcast([128, NTT, 4, 8]), op=ALU.mult))
            rd(lambda e: e.tensor_reduce(out=sel[:], in_=selm[:].rearrange("p t g j -> p t j g"), axis=AX.X, op=ALU.add))
            rd(lambda e: e.tensor_reduce(out=m1_[:], in_=sel[:], axis=AX.X, op=ALU.max))
            rd(lambda e: e.tensor_tensor(out=oh1[:], in0=sel[:], in1=bc3(m1_[:], 8), op=ALU.is_equal))
            rd(lambda e: e.scalar_tensor_tensor(out=sel2[:], in0=oh1[:], scalar=-1e30, in1=sel[:], op0=ALU.mult, op1=ALU.add))
            rd(lambda e: e.tensor_reduce(out=m2_[:], in_=sel2[:], axis=AX.X, op=ALU.max))
            rd(lambda e: e.tensor_tensor(out=oh2[:], in0=sel2[:], in1=bc3(m2_[:], 8), op=ALU.is_equal))
            rd(lambda e: e.tensor_tensor(out=dm[:], in0=m1_[:], in1=m2_[:], op=ALU.subtract))
            cx.op("act", lambda e: e.activation(out=w1[:], in_=dm[:], func=AF.Sigmoid), reads=[RS], writes=[RS])
            rd(lambda e: e.tensor_tensor(out=w1[:], in0=w1[:], in1=ptop[:], op=ALU.mult))
            rd(lambda e: e.tensor_tensor(out=w2[:], in0=ptop[:], in1=w1[:], op=ALU.subtract))
            rd(lambda e: e.tensor_tensor(out=M1[:], in0=ohg[:].unsqueeze(3).to_broadcast([128, NTT, 4, 8]),
                                         in1=oh1[:].unsqueeze(2).to_broadcast([128, NTT, 4, 8]), op=ALU.mult))
            rd(lambda e: e.tensor_tensor(out=M2[:], in0=ohg[:].unsqueeze(3).to_broadcast([128, NTT, 4, 8]),
                                         in1=oh2[:].unsqueeze(2).to_broadcast([128, NTT, 4, 8]), op=ALU.mult))
            rd(lambda e: e.tensor_tensor(out=Mc[:], in0=M1[:].rearrange("p t g j -> p t (g j)"),
                                         in1=M2[:].rearrange("p t g j -> p t (g j)"), op=ALU.add))
            rd(lambda e: e.memset(Cex[:, 0, :], 0.0))
            for i in range(1, NTT):
                rd(lambda e, i=i: e.tensor_tensor(out=Cex[:, i, :], in0=Cex[:, i - 1, :], in1=Mc[:, i - 1, :], op=ALU.add))
            pp = [getps(), getps()]
            for i in range(NTT):
                pb_ = pp[i // 16]
                o_ = pb_[:, (i % 16) * 32:(i % 16 + 1) * 32]
                cx.op("pe", lambda e, o_=o_, i=i: e.matmul(o_, lhsT=tri[:], rhs=Mc[:, i, :], start=True, stop=False),
                      reads=[tri.b, RS], writes=[pb_.b])
                cx.op("pe", lambda e, o_=o_, i=i: e.matmul(o_, lhsT=ones_f[:], rhs=Cex[:, i, :], start=False, stop=True),
                      reads=[ones_f.b, RS], writes=[pb_.b])
            for hh in range(2):
                rd(lambda e, hh=hh: e.tensor_copy(out=pos[:, hh * 16:(hh + 1) * 16, :],
                                                  in_=pp[hh][:].rearrange("p (t x) -> p t x", t=16)), [pp[hh].b])
            rd(lambda e: e.tensor_single_scalar(out=bk[:], in_=pos[:], scalar=127.5, op=ALU.is_gt))
            for thr in range(2, NBLK):
                rd(lambda e, thr=thr: e.tensor_single_scalar(out=tq[:], in_=pos[:], scalar=128.0 * thr - 0.5, op=ALU.is_gt))
                rd(lambda e: e.tensor_tensor(out=bk[:], in0=bk[:], in1=tq[:], op=ALU.add))
            rd(lambda e: e.scalar_tensor_tensor(out=bk[:], in0=bk[:], scalar=float(1 - 128 * NEB_),
                                                in1=ecap[:].unsqueeze(1).to_broadcast([128, NTT, 32]),
                                                op0=ALU.mult, op1=ALU.add), [ecap.b])
            rd(lambda e: e.scalar_tensor_tensor(out=sf[:], in0=pos[:], scalar=float(NEB_), in1=bk[:],
                                                op0=ALU.mult, op1=ALU.add))
            rd(lambda e: e.tensor_single_scalar(out=ov[:], in_=pos[:], scalar=float(CAP) - 0.5, op=ALU.is_gt))
            for k, (Mk, wk) in enumerate(((M1, w1), (M2, w2))):
                Mk32 = Mk[:].rearrange("p t g j -> p t (g j)")
                rd(lambda e, Mk32=Mk32: e.tensor_tensor(out=tq[:], in0=Mk32, in1=sf[:], op=ALU.mult))
                rd(lambda e, k=k: e.tensor_reduce(out=sk[k][:], in_=tq[:], axis=AX.X, op=ALU.add))
                rd(lambda e, Mk32=Mk32: e.tensor_tensor(out=tq[:], in0=Mk32, in1=ov[:], op=ALU.mult))
                rd(lambda e: e.tensor_reduce(out=okk[:], in_=tq[:], axis=AX.X, op=ALU.add))
                rd(lambda e, k=k: e.tensor_scalar(out=dd[:], in0=sk[k][:], scalar1=trashp[:, 0:1], scalar2=None,
                                                  op0=ALU.subtract), [trashp.b])
                rd(lambda e: e.tensor_tensor(out=dd[:], in0=dd[:], in1=okk[:], op=ALU.mult))
                rd(lambda e, k=k: e.tensor_tensor(out=sk[k][:], in0=sk[k][:], in1=dd[:], op=ALU.subtract))
                rd(lambda e, k=k: e.tensor_copy(out=si[k][:], in_=sk[k][:]), (), [si[k].b])
                rd(lambda e, k=k: e.memset(ent[k][:], 0.0), (), [ent[k].b])
                rd(lambda e, k=k: e.tensor_copy(out=ent[k][:, :, 0], in_=tokid[:]), [tokid.b], [ent[k].b])
                rd(lambda e, k=k: e.tensor_scalar(out=ent[k][:, :, 1], in0=tokid[:], scalar1=float(k * ROWS), scalar2=None,
                                                  op0=ALU.add), [tokid.b], [ent[k].b])
                rd(lambda e, k=k, wk=wk: e.tensor_copy(out=ent[k][:, :, 2], in_=wk[:]), (), [ent[k].b])
            for i in range(NTT):
                for k in range(2):
                    cx.op("pool", lambda e, i=i, k=k: e.indirect_dma_start(
                        out=lst_d, out_offset=bass.IndirectOffsetOnAxis(ap=si[k][:, i:i + 1], axis=0),
                        in_=ent[k][:, i, :], in_offset=None),
                        reads=[si[k].b, ent[k].b], writes=[lstB], dma=True)
            cx.barrier()
            alR.close()
            alC.close()
        if stop_after in ("A", "B", "C"):
            pass
        else:
            alD = Alloc(nc)
            NEB = NEXP * NBLK
            lst_sb = alD.sb([128, NEB, 4], F32, "lst_sb")
            idx_i = alD.sb([128, NEB], I32, "idx_i")
            dst_i = alD.sb([128, NEB], I32, "dst_i")
            cx.op("sp", lambda e: e.dma_start(out=lst_sb[:], in_=lst_d[0:NEXP * CAP, :].rearrange("(s eb) w -> s eb w", s=128)),
                  reads=[lstB], full=[lst_sb.b], dma=True)
            cx.op("dve", lambda e: e.tensor_copy(out=idx_i[:], in_=lst_sb[:, :, 0]), reads=[lst_sb.b], full=[idx_i.b])
            cx.op("dve", lambda e: e.tensor_copy(out=dst_i[:], in_=lst_sb[:, :, 1]), reads=[lst_sb.b], full=[dst_i.b])
            sg_ = [alD.sb([128, 8, 256], F32, f"sg{i}") for i in range(2)]
            su_ = [alD.sb([128, 8, 256], F32, f"su{i}") for i in range(2)]
            sd_ = [alD.sb([128, 2, 1024], F32, f"sd{i}") for i in range(2)]
            Wg = [alD.sb([128, 8, 256], BF16, f"Wg{i}") for i in range(2)]
            Wu = [alD.sb([128, 8, 256], BF16, f"Wu{i}") for i in range(2)]
            Wd = [alD.sb([128, 2, 1024], BF16, f"Wd{i}") for i in range(2)]
            Gt = [alD.sb([128, D], BF16, f"Gt{i}") for i in range(3)]
            Xe = [alD.sb([128, 8, CAP], BF16, f"Xe{i}") for i in range(2)]
            sgl = [alD.sb([128, CAP], F32, f"sgl{i}") for i in range(2)]
            ae = [alD.sb([128, 2, CAP], BF16, f"ae{i}") for i in range(2)]
            Yt = [alD.sb([128, D], BF16, f"Yt{i}") for i in range(3)]

            Gt6 = Gt + [alD.sb([128, D], BF16, f"Gtx{i}") for i in range(3)]

            def load_w_dma(e_):
                p = e_ % 2
                cx.op("sp", lambda e: e.dma_start(out=sg_[p][:], in_=weg_d[e_]), full=[sg_[p].b], dma=True)
                cx.op("sp", lambda e: e.dma_start(out=su_[p][:], in_=weu_d[e_]), full=[su_[p].b], dma=True)
                cx.op("sp", lambda e: e.dma_start(out=sd_[p][:], in_=wed_d[e_]), full=[sd_[p].b], dma=True)

            def load_w_cast(e_):
                p = e_ % 2
                cx.op("dve", lambda e: e.tensor_copy(out=Wg[p][:], in_=sg_[p][:]), reads=[sg_[p].b], full=[Wg[p].b])
                cx.op("act", lambda e: e.copy(out=Wu[p][:], in_=su_[p][:]), reads=[su_[p].b], full=[Wu[p].b])
                cx.op("pool", lambda e: e.tensor_copy(out=Wd[p][:], in_=sd_[p][:]), reads=[sd_[p].b], full=[Wd[p].b])

            def gathers(e_):
                for blk in range(NBLK):
                    eb = e_ * NBLK + blk
                    G = Gt6[(e_ % 2) * 3 + blk]
                    cx.op("pool", lambda e, G=G, eb=eb: e.indirect_dma_start(
                        out=G[:], out_offset=None, in_=h2_scr,
                        in_offset=bass.IndirectOffsetOnAxis(ap=idx_i[:, eb:eb + 1], axis=0)),
                        reads=[idx_i.b, h2B], full=[G.b], dma=True)

            gi = [0]
            load_w_dma(0)
            gathers(0)
            load_w_cast(0)
            KNE = int(os.environ.get("KNE", str(NEXP)))
            for e_ in range(KNE):
                p = e_ % 2
                if e_ + 1 < NEXP:
                    load_w_dma(e_ + 1)
                    gathers(e_ + 1)
                X = Xe[p]
                for blk in range(NBLK):
                    G = Gt6[(e_ % 2) * 3 + blk]
                    pbk = psb[gi[0] % 2]
                    gi[0] += 1
                    for c in range(8):
                        cx.op("pe", lambda e, pbk=pbk, G=G, c=c: e.transpose(
                            out=pbk[:, c * 128:(c + 1) * 128], in_=G[:, c * 128:(c + 1) * 128], identity=ident_bf[:]),
                            reads=[G.b, ident_bf.b], writes=[pbk.b])
                    if blk % 2 == 0:
                        cx.op("act", lambda e, pbk=pbk, X=X, blk=blk: e.copy(
                            out=X[:, :, blk * 128:(blk + 1) * 128], in_=pbk[:].rearrange("p (c t) -> p c t", c=8)),
                            reads=[pbk.b], writes=[X.b])
                    else:
                        cx.op("dve", lambda e, pbk=pbk, X=X, blk=blk: e.tensor_copy(
                            out=X[:, :, blk * 128:(blk + 1) * 128], in_=pbk[:].rearrange("p (c t) -> p c t", c=8)),
                            reads=[pbk.b], writes=[X.b])
                a_ = ae[p]
                for ft in range(2):
                    pg = getps(); pu = getps()
                    for c in range(8):
                        cx.op("pe", lambda e, pg=pg, c=c, ft=ft, X=X, p=p: e.matmul(
                            pg[:, 0:CAP], lhsT=Wg[p][:, c, ft * 128:(ft + 1) * 128], rhs=X[:, c, :],
                            start=(c == 0), stop=(c == 7)), reads=[Wg[p].b, X.b], writes=[pg.b])
                    for c in range(8):
                        cx.op("pe", lambda e, pu=pu, c=c, ft=ft, X=X, p=p: e.matmul(
                            pu[:, 0:CAP], lhsT=Wu[p][:, c, ft * 128:(ft + 1) * 128], rhs=X[:, c, :],
                            start=(c == 0), stop=(c == 7)), reads=[Wu[p].b, X.b], writes=[pu.b])
                    s = sgl[ft]
                    cx.op("act", lambda e, pg=pg, s=s: e.activation(out=s[:], in_=pg[:, 0:CAP], func=AF.Silu),
                          reads=[pg.b], full=[s.b])
                    cx.op("dve", lambda e, pu=pu, s=s, a_=a_, ft=ft: e.tensor_tensor(
                        out=a_[:, ft, :], in0=pu[:, 0:CAP], in1=s[:], op=ALU.mult),
                        reads=[pu.b, s.b], writes=[a_.b])
                for blk in range(NBLK):
                    eb = e_ * NBLK + blk
                    Y = Yt[eb % 3]
                    for half in range(2):
                        py = getps()
                        for ft in range(2):
                            cx.op("pe", lambda e, py=py, ft=ft, blk=blk, half=half, a_=a_, p=p: e.matmul(
                                py[:], lhsT=a_[:, ft, blk * 128:(blk + 1) * 128],
                                rhs=Wd[p][:, ft, half * 512:(half + 1) * 512], start=(ft == 0), stop=(ft == 1)),
                                reads=[a_.b, Wd[p].b], writes=[py.b])
                        if half == 0:
                            cx.op("dve", lambda e, py=py, Y=Y, eb=eb: e.tensor_scalar(
                                out=Y[:, 0:512], in0=py[:], scalar1=lst_sb[:, eb, 2:3], scalar2=None, op0=ALU.mult),
                                reads=[py.b, lst_sb.b], writes=[Y.b])
                        else:
                            cx.op("act", lambda e, py=py, Y=Y, eb=eb: e.activation(
                                out=Y[:, 512:1024], in_=py[:], func=AF.Copy, scale=lst_sb[:, eb, 2:3]),
                                reads=[py.b, lst_sb.b], writes=[Y.b])
                    cx.op("pool", lambda e, Y=Y, eb=eb: e.indirect_dma_start(
                        out=moe_scr, out_offset=bass.IndirectOffsetOnAxis(ap=dst_i[:, eb:eb + 1], axis=0),
                        in_=Y[:], in_offset=None), reads=[Y.b, dst_i.b], writes=[moeB], dma=True)
                if e_ + 1 < NEXP:
                    load_w_cast(e_ + 1)
            cx.barrier()
            alD.close()

            alE = Alloc(nc)
            gfin = alE.sb([128, D], F32, "gfin")
            cx.op("sp", lambda e: e.dma_start(out=gfin[:], in_=gfin_d.partition_broadcast(128)), full=[gfin.b], dma=True)
            NE_ = 4
            xa = [alE.sb([128, D], F32, f"xa{i}") for i in range(NE_)]
            m0 = [alE.sb([128, D], BF16, f"m0{i}") for i in range(NE_)]
            m1 = [alE.sb([128, D], BF16, f"m1{i}") for i in range(NE_)]
            ot = [alE.sb([128, D], F32, f"ot{i}") for i in range(NE_)]
            junk3 = alE.sb([128, D], BF16, "junk3")
            sse = [alE.sb([128, 1], F32, f"sse{i}") for i in range(NE_)]
            rte = [alE.sb([128, 1], F32, f"rte{i}") for i in range(NE_)]
            rse = [alE.sb([128, 1], F32, f"rse{i}") for i in range(NE_)]
            outB = Buf("out")

            def e_load(ti):
                p = ti % NE_
                rows = slice(ti * 128, (ti + 1) * 128)
                cx.op("sp", lambda e, p=p, rows=rows: e.dma_start(out=xa[p][:], in_=x2_scr[rows, :]),
                      reads=[x2B], full=[xa[p].b], dma=True)
                cx.op("sp", lambda e, p=p, rows=rows: e.dma_start(out=m0[p][:], in_=moe_scr[rows, :]),
                      reads=[moeB], full=[m0[p].b], dma=True)
                cx.op("sp", lambda e, p=p, ti=ti: e.dma_start(
                    out=m1[p][:], in_=moe_scr[ROWS + ti * 128:ROWS + (ti + 1) * 128, :]),
                    reads=[moeB], full=[m1[p].b], dma=True)

            for ti in range(min(NE_ - 1, NT)):
                e_load(ti)
            for ti in range(NT):
                p = ti % NE_
                rows = slice(ti * 128, (ti + 1) * 128)
                if ti + NE_ - 1 < NT:
                    e_load(ti + NE_ - 1)
                cx.op("pool", lambda e, p=p: e.tensor_tensor(out=xa[p][:], in0=xa[p][:], in1=m0[p][:], op=ALU.add),
                      reads=[m0[p].b], writes=[xa[p].b])
                cx.op("dve", lambda e, p=p: e.tensor_tensor(out=xa[p][:], in0=xa[p][:], in1=m1[p][:], op=ALU.add),
                      reads=[m1[p].b], writes=[xa[p].b])
                cx.op("act", lambda e, p=p: e.activation(out=junk3[:], in_=xa[p][:], func=AF.Square, accum_out=sse[p][:]),
                      reads=[xa[p].b], full=[junk3.b, sse[p].b])
                cx.op("act", lambda e, p=p: e.activation(out=rte[p][:], in_=sse[p][:], func=AF.Sqrt, scale=1.0 / D, bias=EPS),
                      reads=[sse[p].b], full=[rte[p].b])
                cx.op("dve", lambda e, p=p: e.reciprocal(out=rse[p][:], in_=rte[p][:]), reads=[rte[p].b], full=[rse[p].b])
                cx.op("dve", lambda e, p=p: e.scalar_tensor_tensor(out=ot[p][:], in0=xa[p][:], scalar=rse[p][:, 0:1],
                                                                   in1=gfin[:], op0=ALU.mult, op1=ALU.mult),
                      reads=[xa[p].b, rse[p].b, gfin.b], full=[ot[p].b])
                cx.op("sp", lambda e, p=p, rows=rows: e.dma_start(out=out_d[rows, :], in_=ot[p][:]),
                      reads=[ot[p].b], writes=[outB], dma=True)
            cx.barrier()
            alE.close()
        cx.barrier()
        cx.emit(block)
        print("waits", cx.nwait, "instrs", {e: cx.cnt[e] for e in cx.ENG}, "signals", {e: len(cx.waited[e]) for e in cx.ENG})
    return nc


def host_consts():
    c = {}
    c["ident_bf"] = np.eye(128, dtype=np.float32).astype(ml_dtypes.bfloat16)
    c["ident_f"] = np.eye(128, dtype=np.float32)
    psel = np.zeros((128, 8, 240), np.float32)
    for a in range(8):
        for i in range(16):
            psel[a * 16 + i, a, 7 * 16 + i] = 1.0
    c["psel"] = psel.astype(ml_dtypes.bfloat16)
    kk = np.arange(128) // 16
    c["cmask"] = (kk[None, :] >= kk[:, None]).astype(np.float32)
    c["tri"] = (np.arange(128)[:, None] < np.arange(128)[None, :]).astype(np.float32)
    c["ecap"] = np.ascontiguousarray(np.broadcast_to((np.arange(32) * NBLK).astype(np.float32)[None, :], (128, 32)))
    c["tokid"] = (np.arange(NT)[None, :] * 128 + np.arange(128)[:, None]).astype(np.float32)
    li = np.zeros((NEXP * CAP + 128, 4), np.float32)
    li[:, 0] = SEQ + ((np.arange(NEXP * CAP + 128) // (NEXP * NBLK)) % 128)
    li[:, 1] = li[:, 0]
    c["trashp"] = (NEXP * CAP + np.arange(128)).astype(np.float32).reshape(128, 1)
    c["lst_init"] = li
    return c


def relayout_pc(w):
    E, K, N = w.shape
    return np.ascontiguousarray(w.reshape(E, K // 128, 128, N).transpose(0, 2, 1, 3))


def pair_layout(a):
    rest = a.shape[2:]
    a = a.reshape((16, 2, 64) + rest)
    a = np.moveaxis(a, 0, 2)
    return np.ascontiguousarray(a.reshape((128, 16) + rest))


def make_inmap(inputs, b, consts=None):
    f = lambda a: np.ascontiguousarray(a, dtype=np.float32)
    m = {"x": f(inputs["x"][b]),
         "g_mix": f(inputs["g_mix"]),
         "w_in": f(inputs["w_in"][0])}
    m["lamre_l"] = pair_layout(f(inputs["ssm_lambda_re"][0]))
    m["lamim_l"] = pair_layout(f(inputs["ssm_lambda_im"][0]))
    m["logdt_l"] = pair_layout(np.broadcast_to(f(inputs["ssm_log_dt"][0])[:, None], (32, 64)))
    m["bre_l"] = pair_layout(f(inputs["ssm_b_re"][0]))
    m["bim_l"] = pair_layout(f(inputs["ssm_b_im"][0]))
    m["cre_l"] = pair_layout(f(inputs["ssm_c_re"][0]).transpose(0, 2, 1))
    m["cim_l"] = pair_layout(f(inputs["ssm_c_im"][0]).transpose(0, 2, 1))
    m["d_l"] = np.ascontiguousarray(np.tile(f(inputs["ssm_d"][0]).reshape(32, 16).T, (8, 1)))
    m["mem"] = f(inputs["mem"][b])
    for k_, n_ in (("g_mem", "g_mem"), ("g_ffn", "g_ffn")):
        m[n_] = f(inputs[k_])
    m["g_final"] = f(inputs["g_final"]).reshape(1, D)
    m["w_mem_kv"] = f(inputs["w_mem_kv"][0]); m["w_mem_out"] = f(inputs["w_mem_out"][0])
    m["w_conv_out"] = f(inputs["w_conv_out"][0]); m["w_ssm_glu"] = f(inputs["w_ssm_glu"][0])
    m["w_out"] = f(inputs["w_out"][0])
    m["w_router"] = np.ascontiguousarray(np.concatenate([f(inputs["w_router_group"][0]),
                                                         f(inputs["w_router_expert"][0])], axis=1))
    m["b_router"] = np.ascontiguousarray(np.concatenate([f(inputs["b_router_group"][0]),
                                                         f(inputs["b_router_expert"][0])])[None, :])
    m["cdw_l"] = np.ascontiguousarray(f(inputs["conv_dw"][0]).T.reshape(4, 128, 31).transpose(1, 0, 2))
    m["cb_l"] = np.ascontiguousarray(f(inputs["conv_dw_bias"][0]).reshape(4, 128).T)
    m["lng_l"] = np.ascontiguousarray(f(inputs["conv_ln_g"][0]).reshape(4, 128).T)
    m["lnb_l"] = np.ascontiguousarray(f(inputs["conv_ln_b"][0]).reshape(4, 128).T)
    if consts is not None and "w_exp_gate" in consts:
        for k_ in ("w_exp_gate", "w_exp_up", "w_exp_down"):
            m[k_] = consts[k_]
    else:
        m["w_exp_gate"] = relayout_pc(f(inputs["w_exp_gate"][0]))
        m["w_exp_up"] = relayout_pc(f(inputs["w_exp_up"][0]))
        m["w_exp_down"] = relayout_pc(f(inputs["w_exp_down"][0]))
    m.update(consts if consts is not None else host_consts())
    return m


def kernel(**inputs):
    nc = build()
    consts = host_consts()
    f32 = lambda a: np.ascontiguousarray(a, dtype=np.float32)
    for k_ in ("w_exp_gate", "w_exp_up", "w_exp_down"):
        consts[k_] = relayout_pc(f32(inputs[k_][0]))
    in_maps = [make_inmap(inputs, b, consts) for b in range(NCORES)]
    res = run_bass_kernel_spmd(nc, in_maps, core_ids=list(range(NCORES)))
    return np.stack([r["out"] for r in res.results], axis=0)
```

```python
import os
import numpy as np
import ml_dtypes
from contextlib import ExitStack
import concourse.bass as bass
import concourse.mybir as mybir
from concourse.bass_utils import run_bass_kernel_spmd

F32 = mybir.dt.float32
BF16 = mybir.dt.bfloat16
I32 = mybir.dt.int32
U32 = mybir.dt.uint32
AF = mybir.ActivationFunctionType
ALU = mybir.AluOpType
AX = mybir.AxisListType
GELU = AF.Gelu_apprx_tanh

D = 1024
SEQ = 4096
NCORES = 8
T = 512
NB = SEQ // T
NT = SEQ // 128
EPS = 1e-6
NEXP = 32
CAP = 384
NBLK = CAP // 128
ROWS = SEQ + 128


class Buf:
    __slots__ = ("name", "w", "r")

    def __init__(self, name):
        self.name = name
        self.w = {}
        self.r = {}


class Ctx:
    ENG = ("pe", "dve", "act", "pool", "sp")
    KROT = 4
    NDMA = 12

    def __init__(self, nc, es):
        self.nc = nc
        self.q = {e: [] for e in self.ENG}
        self.cnt = {e: 0 for e in self.ENG}
        self.seen = {e: {} for e in self.ENG}
        self.esem = {e: [es.enter_context(nc.semaphore(f"s_{e}{i}")) for i in range(self.KROT)]
                     for e in self.ENG}
        self.dsem = {e: [es.enter_context(nc.semaphore(f"d_{e}{i}")) for i in range(self.NDMA)]
                     for e in ("sp", "act", "pool")}
        self.dcnt = {e: [0] * self.NDMA for e in self.dsem}
        self.dnext = {e: 0 for e in self.dsem}
        self.nwait = 0
        self.waited = {e: set() for e in self.ENG}

    def _wait(self, eng, tok):
        key, val = tok
        if key[0] == 'e' and key[1] == eng and eng == "pe":
            return
        if self.seen[eng].get(key, -1) >= val:
            return
        self.seen[eng][key] = val
        if key[0] == 'e':
            self.waited[key[1]].add(val)
        self.q[eng].append(("w", key, val))
        self.nwait += 1

    def op(self, eng, fn, reads=(), writes=(), full=(), dma=False):
        toks = []
        for b in reads:
            toks.extend(b.w.items())
        for b in tuple(writes) + tuple(full):
            toks.extend(b.w.items())
            toks.extend(b.r.items())
        for t in toks:
            self._wait(eng, t)
        if dma:
            i = self.dnext[eng]
            self.dnext[eng] = (i + 1) % self.NDMA
            key = ('d', eng, i)
            if self.dcnt[eng][i] > 0:
                self._wait(eng, (key, self.dcnt[eng][i]))
            self.dcnt[eng][i] += 16
            val = self.dcnt[eng][i]
            self.q[eng].append(("d", fn, self.dsem[eng][i]))
        else:
            key = ('e', eng)
            val = self.cnt[eng]
            self.cnt[eng] += 1
            self.q[eng].append(("i", fn, val))
        for b in reads:
            b.r[key] = val
        for b in full:
            b.w = {key: val}
            b.r = {}
        for b in writes:
            b.w[key] = val
        return (key, val)

    def barrier(self, skip_pool_dma=False):
        toks = []
        for e in self.ENG:
            if self.cnt[e] > 0:
                toks.append((('e', e), self.cnt[e] - 1))
        for e in self.dsem:
            if skip_pool_dma and e == "pool":
                continue
            for i in range(self.NDMA):
                if self.dcnt[e][i] > 0:
                    toks.append((('d', e, i), self.dcnt[e][i]))
        for e in self.ENG:
            for t in toks:
                self._wait(e, t)

    def emit(self, block):
        nc = self.nc

        rank = {e: {v: i for i, v in enumerate(sorted(self.waited[e]))} for e in self.ENG}
        K_ = self.KROT

        def run(engname, engine):
            for item in self.q[engname]:
                if item[0] == "w":
                    key, val = item[1], item[2]
                    if key[0] == 'e':
                        r = rank[key[1]][val]
                        engine.wait_ge(self.esem[key[1]][r % K_], r // K_ + 1)
                    else:
                        engine.wait_ge(self.dsem[key[1]][key[2]], val)
                elif item[0] == "d":
                    item[1](engine).then_inc(item[2], 16)
                else:
                    ins = item[1](engine)
                    r = rank[engname].get(item[2])
                    if r is not None:
                        ins.then_inc(self.esem[engname][r % K_], 1)

        @block.tensor
        def _(e):
            run("pe", e)

        @block.vector
        def _(e):
            run("dve", e)

        @block.scalar
        def _(e):
            run("act", e)

        @block.gpsimd
        def _(e):
            run("pool", e)

        @block.sync
        def _(e):
            run("sp", e)


class TT:
    def __init__(self, t, name):
        self.t = t
        self.b = Buf(name)

    def __getitem__(self, k):
        return self.t[k]


class Alloc:
    cnt = [0]

    def __init__(self, nc, es=None):
        self.nc = nc
        self.es = es if es is not None else ExitStack()

    @property
    def n(self):
        return Alloc.cnt[0]

    @n.setter
    def n(self, v):
        Alloc.cnt[0] = v

    def close(self):
        self.es.close()

    def sb(self, shape, dt, name=None):
        self.n += 1
        name = name or f"sb{self.n}"
        t = self.es.enter_context(self.nc.sbuf_tensor(f"{name}_{self.n}", list(shape), dt))
        return TT(t, name)

    def ps(self, shape, dt, name=None):
        self.n += 1
        name = name or f"ps{self.n}"
        t = self.es.enter_context(self.nc.psum_tensor(f"{name}_{self.n}", list(shape), dt))
        return TT(t, name)


def build(stop_after="E", dbg=False):
    nc = bass.Bass("TRN2", target_bir_lowering=False)
    dram = {}

    def din(name, shape, dt=F32):
        dram[name] = nc.dram_tensor(name, list(shape), dt, kind="ExternalInput").ap()
        return dram[name]

    def dscr(name, shape, dt, kind="Internal"):
        dram[name] = nc.dram_tensor(name, list(shape), dt, kind=kind).ap()
        return dram[name]

    x_d = din("x", [SEQ, D])
    gmix_d = din("g_mix", [1, D])
    w_in_d = din("w_in", [D, 5120])
    ident_bf_d = din("ident_bf", [128, 128], BF16)
    ident_f_d = din("ident_f", [128, 128], F32)
    lamre_d = din("lamre_l", [128, 16])
    lamim_d = din("lamim_l", [128, 16])
    logdt_d = din("logdt_l", [128, 16])
    bre_d = din("bre_l", [128, 16, 16])
    bim_d = din("bim_l", [128, 16, 16])
    cre_d = din("cre_l", [128, 16, 16])
    cim_d = din("cim_l", [128, 16, 16])
    dl_d = din("d_l", [128, 32])
    psel_d = din("psel", [128, 8, 240], BF16)
    cmask_d = din("cmask", [128, 128])
    mem_d = din("mem", [256, D])
    gmem_d = din("g_mem", [1, D])
    gffn_d = din("g_ffn", [1, D])
    gfin_d = din("g_final", [1, D])
    wkv_d = din("w_mem_kv", [D, 1024])
    wmo_d = din("w_mem_out", [512, D])
    wco_d = din("w_conv_out", [512, D])
    wgl_d = din("w_ssm_glu", [512, 2048])
    wo_d = din("w_out", [D, D])
    wr_d = din("w_router", [D, 36])
    rbias_d = din("b_router", [1, 36])
    cdw_d = din("cdw_l", [128, 4, 31])
    cb_d = din("cb_l", [128, 4])
    lng_d = din("lng_l", [128, 4])
    lnb_d = din("lnb_l", [128, 4])
    tri_d = din("tri", [128, 128])
    ecap_d = din("ecap", [128, 32])
    tokid_d = din("tokid", [128, NT])
    lst_init_d = din("lst_init", [NEXP * CAP + 128, 4])
    trashp_d = din("trashp", [128, 1])
    weg_d = din("w_exp_gate", [NEXP, 128, 8, 256])
    weu_d = din("w_exp_up", [NEXP, 128, 8, 256])
    wed_d = din("w_exp_down", [NEXP, 128, 2, D])
    dk = "ExternalOutput" if dbg else "Internal"
    lst_d = dscr("lst", [NEXP * CAP + 128, 4], F32, kind=dk)
    h2_scr = dscr("h2_scr", [ROWS, D], BF16, kind=dk)
    moe_scr = dscr("moe_scr", [2 * ROWS, D], BF16, kind=dk)
    x2_scr = dscr("x2_scr", [SEQ, D], F32, kind=dk)
    ys_scr = dscr("ys_scr", [4, 128, SEQ], BF16, kind="ExternalOutput" if dbg else "Internal")
    out_d = dscr("out", [SEQ, D], F32, kind="ExternalOutput")
    hT_scr = dscr("hT_scr", [8, 128, SEQ], BF16, kind="ExternalOutput" if dbg else "Internal")
    u_dbg = dscr("u_dbg", [4, 128, SEQ], BF16, kind="ExternalOutput") if dbg else None

    with ExitStack() as es:
        cx = Ctx(nc, es)
        al = Alloc(nc, es)
        block = es.enter_context(nc.Block())

        ident_bf = al.sb([128, 128], BF16, "ident_bf")
        cx.op("sp", lambda e: e.dma_start(out=ident_bf[:], in_=ident_bf_d), full=[ident_bf.b], dma=True)
        ident_f = al.sb([128, 128], F32, "ident_f")
        cx.op("sp", lambda e: e.dma_start(out=ident_f[:], in_=ident_f_d), full=[ident_f.b], dma=True)

        psum = [al.ps([128, 512], F32, f"bank{i}") for i in range(6)]
        psb = [al.ps([128, 1024], BF16, f"bankb{i}") for i in range(2)]
        pctr = [0]

        def getps():
            p = psum[pctr[0] % len(psum)]
            pctr[0] += 1
            return p

        alAB = Alloc(nc)
        u_all = alAB.sb([128, 4, SEQ], BF16, "u_all")
        M_all = alAB.sb([128, 32, 128], BF16, "M_all")
        W2r = alAB.sb([128, 16, 2, 128], BF16, "W2r"); W2i = alAB.sb([128, 16, 2, 128], BF16, "W2i")
        C1r = alAB.sb([128, 16, 128], BF16, "C1r"); nC1i = alAB.sb([128, 16, 128], BF16, "nC1i")
        KAr = alAB.sb([128, 9, 16], F32, "KAr"); KAi = alAB.sb([128, 9, 16], F32, "KAi")
        KnAi = alAB.sb([128, 9, 16], F32, "KnAi")
        psel = alAB.sb([128, 8, 240], BF16, "psel")
        zt = alAB.sb([128, 1024], F32, "zt")
        cx.op("sp", lambda e: e.dma_start(out=psel[:], in_=psel_d), full=[psel.b], dma=True)
        al_outer = al
        al = Alloc(nc)
        gmix = al.sb([128, D], F32, "gmix")
        cx.op("sp", lambda e: e.dma_start(out=gmix[:], in_=gmix_d.partition_broadcast(128)),
              full=[gmix.b], dma=True)

        stg = [al.sb([128, 8, 256], F32, f"stg{i}") for i in range(2)]
        sctr = [0]

        def load_cast(dst, dst_col0, src_d, c0, c1, kch):
            for cc in range(c0, c1, 256):
                w = min(256, c1 - cc)
                s = stg[sctr[0] % 2]
                sctr[0] += 1
                src = src_d[:, cc:cc + w].rearrange("(c p) n -> p c n", p=128)
                cx.op("sp", lambda e, s=s, src=src, w=w: e.dma_start(out=s[:, 0:kch, 0:w], in_=src),
                      full=[s.b], dma=True)
                o = dst_col0 + (cc - c0)
                cx.op("pool", lambda e, s=s, o=o, w=w: e.tensor_copy(out=dst[:, 0:kch, o:o + w],
                                                                     in_=s[:, 0:kch, 0:w]),
                      reads=[s.b], writes=[dst.b])

        w_ssm_in = al.sb([128, 8, 512], BF16, "w_ssm_in")
        load_cast(w_ssm_in, 0, w_in_d, 1024, 1536, 8)
        NA_ = 4
        xt = [al.sb([128, D], F32, f"xt{i}") for i in range(NA_)]
        junk = al.sb([128, D], BF16, "junk")
        ss = [al.sb([128, 1], F32, f"ss{i}") for i in range(NA_)]
        rt = [al.sb([128, 1], F32, f"rt{i}") for i in range(NA_)]
        rstd = [al.sb([128, 1], F32, f"rstd{i}") for i in range(NA_)]
        hbf = [al.sb([128, D], BF16, f"hbf{i}") for i in range(NA_)]
        hTb = [al.sb([128, 8, T], BF16, f"hTb{i}") for i in range(2)]
        cx.op("pool", lambda e: e.memset(zt[:], 0.0), full=[zt.b])
        lstB = Buf("lst"); h2B = Buf("h2scr"); moeB = Buf("moescr"); x2B = Buf("x2scr")
        cx.op("pool", lambda e: e.dma_start(out=lst_d, in_=lst_init_d), full=[lstB], dma=True)
        cx.op("pool", lambda e: e.dma_start(out=h2_scr[SEQ:ROWS, :], in_=zt[:, 0:512].bitcast(BF16)),
              reads=[zt.b], writes=[h2B], dma=True)
        moe_flat = moe_scr.rearrange("(n p) d -> n p d", p=128)
        for n in range(0, 2 * ROWS // 128):
            cx.op("pool", lambda e, n=n: e.dma_start(out=moe_flat[n], in_=zt[:, 0:512].bitcast(BF16)),
                  reads=[zt.b], writes=[moeB], dma=True)

        def a_front(i):
            p = i % NA_
            cx.op("sp", lambda e, p=p, i=i: e.dma_start(out=xt[p][:], in_=x_d[i * 128:(i + 1) * 128, :]),
                  full=[xt[p].b], dma=True)
            cx.op("act", lambda e, p=p: e.activation(out=junk[:], in_=xt[p][:], func=AF.Square,
                                                     accum_out=ss[p][:]),
                  reads=[xt[p].b], writes=[junk.b], full=[ss[p].b])
            cx.op("act", lambda e, p=p: e.activation(out=rt[p][:], in_=ss[p][:], func=AF.Sqrt,
                                                     scale=1.0 / D, bias=EPS),
                  reads=[ss[p].b], full=[rt[p].b])
            cx.op("dve", lambda e, p=p: e.reciprocal(out=rstd[p][:], in_=rt[p][:]),
                  reads=[rt[p].b], full=[rstd[p].b])
            cx.op("dve", lambda e, p=p: e.scalar_tensor_tensor(out=hbf[p][:], in0=xt[p][:],
                                                               scalar=rstd[p][:, 0:1], in1=gmix[:],
                                                               op0=ALU.mult, op1=ALU.mult),
                  reads=[xt[p].b, rstd[p].b, gmix.b], full=[hbf[p].b])

        def a_back(i):
            p = i % NA_
            blk = i // 4
            hb = hTb[blk % 2]
            pb = psb[i % 2]
            for c in range(8):
                cx.op("pe", lambda e, pb=pb, p=p, c=c: e.transpose(out=pb[:, c * 128:(c + 1) * 128],
                                                                   in_=hbf[p][:, c * 128:(c + 1) * 128],
                                                                   identity=ident_bf[:]),
                      reads=[hbf[p].b, ident_bf.b], writes=[pb.b])
            tt = i % 4
            cx.op("act", lambda e, pb=pb, hb=hb, tt=tt: e.copy(
                out=hb[:, :, tt * 128:(tt + 1) * 128],
                in_=pb[:].rearrange("p (c t) -> p c t", c=8)),
                reads=[pb.b], writes=[hb.b])
            if tt == 3:
                for f in range(4):
                    ps = getps()
                    for c in range(8):
                        cx.op("pe", lambda e, ps=ps, hb=hb, f=f, c=c: e.matmul(
                            ps[:], lhsT=w_ssm_in[:, c, f * 128:(f + 1) * 128], rhs=hb[:, c, :],
                            start=(c == 0), stop=(c == 7)),
                            reads=[w_ssm_in.b, hb.b], writes=[ps.b])
                    cx.op("dve", lambda e, ps=ps, f=f, blk=blk: e.tensor_copy(
                        out=u_all[:, f, blk * T:(blk + 1) * T], in_=ps[:]),
                        reads=[ps.b], writes=[u_all.b])
                cx.op("act", lambda e, hb=hb, blk=blk: e.dma_start(
                    out=hT_scr[:, :, blk * T:(blk + 1) * T].rearrange("c p t -> p c t"), in_=hb[:]),
                    reads=[hb.b], dma=True)

        a_front(0); a_front(1)
        for i in range(NT):
            if i + 2 < NT:
                a_front(i + 2)
            a_back(i)

        if dbg:
            cx.op("sp", lambda e: e.dma_start(out=u_dbg.rearrange("f p t -> p f t"), in_=u_all[:]),
                  reads=[u_all.b], dma=True)


        cx.barrier(skip_pool_dma=True)
        al.close()
        al = Alloc(nc)
        TWO_PI = 2.0 * np.pi
        cmask = al.sb([128, 128], F32, "cmask")
        cx.op("sp", lambda e: e.dma_start(out=cmask[:], in_=cmask_d), full=[cmask.b], dma=True)
        dl = al.sb([128, 32], F32, "dl")
        cx.op("sp", lambda e: e.dma_start(out=dl[:], in_=dl_d), full=[dl.b], dma=True)
        SU = Buf("ssm_setup")

        def sload(shape, src, name):
            t = al.sb(shape, F32, name)
            cx.op("sp", lambda e: e.dma_start(out=t[:], in_=src), full=[t.b], dma=True)
            return t

        lamre = sload([128, 16], lamre_d, "lamre")
        lamim = sload([128, 16], lamim_d, "lamim")
        logdt = sload([128, 16], logdt_d, "logdt")
        Bre = sload([128, 16, 16], bre_d, "Bre")
        Bim = sload([128, 16, 16], bim_d, "Bim")
        Cre = sload([128, 16, 16], cre_d, "Cre")
        Cim = sload([128, 16, 16], cim_d, "Cim")
        ins_b = [lamre.b, lamim.b, logdt.b, Bre.b, Bim.b, Cre.b, Cim.b]

        def S(shape, name):
            return al.sb(shape, F32, name)

        def dv(fn):
            cx.op("dve", fn, reads=ins_b, writes=[SU])

        def ac(fn):
            cx.op("act", fn, reads=ins_b, writes=[SU])

        def tt_(out, a, b, op):
            dv(lambda e: e.tensor_tensor(out=out, in0=a, in1=b, op=op))

        sh16 = [128, 16]
        dt_ = S(sh16, "dt"); lrd = S(sh16, "lrd"); th = S(sh16, "th")
        ac(lambda e: e.activation(out=dt_[:], in_=logdt[:], func=AF.Exp))
        tt_(lrd[:], lamre[:], dt_[:], ALU.mult)
        tt_(th[:], lamim[:], dt_[:], ALU.mult)
        mag = S(sh16, "mag"); imag2 = S(sh16, "imag2")
        ac(lambda e: e.activation(out=mag[:], in_=lrd[:], func=AF.Exp))
        ac(lambda e: e.activation(out=imag2[:], in_=lrd[:], func=AF.Exp, scale=-2.0))
        kq_i = al.sb(sh16, I32, "kq_i"); kq = S(sh16, "kq"); red = S(sh16, "red"); msk = S(sh16, "msk")
        sinv = S(sh16, "sinv"); cosv = S(sh16, "cosv"); tmpa = S(sh16, "tmpa")

        def sin_of(outt, shift):
            dv(lambda e: e.tensor_scalar(out=tmpa[:], in0=th[:], scalar1=float(shift), scalar2=None,
                                         op0=ALU.add))
            dv(lambda e: e.tensor_scalar(out=kq[:], in0=tmpa[:], scalar1=float(1.0 / TWO_PI),
                                         scalar2=None, op0=ALU.mult))
            dv(lambda e: e.tensor_copy(out=kq_i[:], in_=kq[:]))
            dv(lambda e: e.tensor_copy(out=kq[:], in_=kq_i[:]))
            dv(lambda e: e.scalar_tensor_tensor(out=red[:], in0=kq[:], scalar=float(-TWO_PI),
                                                in1=tmpa[:], op0=ALU.mult, op1=ALU.add))
            dv(lambda e: e.tensor_single_scalar(out=msk[:], in_=red[:], scalar=float(np.pi), op=ALU.is_gt))
            dv(lambda e: e.scalar_tensor_tensor(out=red[:], in0=msk[:], scalar=float(-TWO_PI),
                                                in1=red[:], op0=ALU.mult, op1=ALU.add))
            dv(lambda e: e.tensor_single_scalar(out=msk[:], in_=red[:], scalar=float(-np.pi), op=ALU.is_lt))
            dv(lambda e: e.scalar_tensor_tensor(out=red[:], in0=msk[:], scalar=float(TWO_PI),
                                                in1=red[:], op0=ALU.mult, op1=ALU.add))
            ac(lambda e: e.activation(out=outt[:], in_=red[:], func=AF.Sin))

        sin_of(sinv, 0.0)
        sin_of(cosv, np.pi / 2)
        PWr = S([128, 9, 16], "PWr"); PWi = S([128, 9, 16], "PWi")
        IPr = S([128, 8, 16], "IPr"); IPi = S([128, 8, 16], "IPi")
        t1 = S([128, 16, 8, 16], "t1"); t2 = S([128, 16, 8, 16], "t2")

        def cmul(outr, outi, ar, ai, br, bi, shp, neg_i=False):
            a1 = t1[:].rearrange("p a b c -> p (a b c)")[:, 0:int(np.prod(shp[1:]))]
            a2 = t2[:].rearrange("p a b c -> p (a b c)")[:, 0:int(np.prod(shp[1:]))]
            if len(shp) == 3:
                a1 = a1.rearrange("p (a b) -> p a b", a=shp[1])
                a2 = a2.rearrange("p (a b) -> p a b", a=shp[1])
            if len(shp) == 4:
                a1 = t1[:, :, 0:shp[2], :]
                a2 = t2[:, :, 0:shp[2], :]
            tt_(a1, ar, br, ALU.mult)
            tt_(a2, ai, bi, ALU.mult)
            tt_(outr, a1, a2, ALU.subtract)
            tt_(a1, ar, bi, ALU.mult)
            tt_(a2, ai, br, ALU.mult)
            if neg_i:
                dv(lambda e: e.scalar_tensor_tensor(out=outi, in0=a1, scalar=-1.0, in1=a2,
                                                    op0=ALU.mult, op1=ALU.subtract))
            else:
                tt_(outi, a1, a2, ALU.add)

        dv(lambda e: e.memset(PWr[:, 0, :], 1.0))
        dv(lambda e: e.memset(PWi[:, 0, :], 0.0))
        dv(lambda e: e.memset(IPr[:, 0, :], 1.0))
        dv(lambda e: e.memset(IPi[:, 0, :], 0.0))
        tt_(PWr[:, 1, :], mag[:], cosv[:], ALU.mult)
        tt_(PWi[:, 1, :], mag[:], sinv[:], ALU.mult)
        tt_(IPr[:, 1, :], PWr[:, 1, :], imag2[:], ALU.mult)
        dv(lambda e: e.scalar_tensor_tensor(out=IPi[:, 1, :], in0=PWi[:, 1, :], scalar=-1.0, in1=imag2[:],
                                            op0=ALU.mult, op1=ALU.mult))
        for n in range(2, 9):
            cmul(PWr[:, n, :], PWi[:, n, :], PWr[:, n - 1, :], PWi[:, n - 1, :], PWr[:, 1, :], PWi[:, 1, :], sh16)
        for n in range(2, 8):
            cmul(IPr[:, n, :], IPi[:, n, :], IPr[:, n - 1, :], IPi[:, n - 1, :], IPr[:, 1, :], IPi[:, 1, :], sh16)
        dv(lambda e: e.tensor_copy(out=KAr[:, 0, :], in_=PWr[:, 8, :]))
        dv(lambda e: e.tensor_copy(out=KAi[:, 0, :], in_=PWi[:, 8, :]))
        for d_ in range(1, 9):
            cmul(KAr[:, d_, :], KAi[:, d_, :], KAr[:, d_ - 1, :], KAi[:, d_ - 1, :],
                 KAr[:, d_ - 1, :], KAi[:, d_ - 1, :], sh16)
        dv(lambda e: e.tensor_scalar(out=KnAi[:], in0=KAi[:], scalar1=-1.0, scalar2=None, op0=ALU.mult))
        am1 = S(sh16, "am1"); l2 = S(sh16, "l2"); il2 = S(sh16, "il2"); kr = S(sh16, "kr"); ki = S(sh16, "ki")
        dv(lambda e: e.tensor_scalar(out=am1[:], in0=PWr[:, 1, :], scalar1=-1.0, scalar2=None, op0=ALU.add))
        tt_(l2[:], lamre[:], lamre[:], ALU.mult)
        tt_(tmpa[:], lamim[:], lamim[:], ALU.mult)
        tt_(l2[:], l2[:], tmpa[:], ALU.add)
        dv(lambda e: e.reciprocal(out=il2[:], in_=l2[:]))
        tt_(kr[:], am1[:], lamre[:], ALU.mult)
        tt_(tmpa[:], PWi[:, 1, :], lamim[:], ALU.mult)
        tt_(kr[:], kr[:], tmpa[:], ALU.add)
        tt_(kr[:], kr[:], il2[:], ALU.mult)
        tt_(ki[:], PWi[:, 1, :], lamre[:], ALU.mult)
        tt_(tmpa[:], am1[:], lamim[:], ALU.mult)
        tt_(ki[:], ki[:], tmpa[:], ALU.subtract)
        tt_(ki[:], ki[:], il2[:], ALU.mult)
        sh3 = [128, 16, 16]

        def bc(a):
            return a.unsqueeze(2).to_broadcast(sh3)

        Bbr = S(sh3, "Bbr"); Bbi = S(sh3, "Bbi")
        cmul(Bbr[:], Bbi[:], bc(kr[:]), bc(ki[:]), Bre[:], Bim[:], sh3)
        Bhr = S([128, 16, 8, 16], "Bhr"); nBhi = S([128, 16, 8, 16], "nBhi"); Bhi = S([128, 16, 8, 16], "Bhi")
        Btr = S([128, 16, 8, 16], "Btr"); Bti = S([128, 16, 8, 16], "Bti")
        Chr = S([128, 16, 9, 16], "Chr"); Chi = S([128, 16, 9, 16], "Chi"); nChi = S([128, 16, 9, 16], "nChi")
        sh4 = [128, 16, 8, 16]

        def bk(a):
            return a.rearrange("p k r -> p r k").unsqueeze(3).to_broadcast(sh4)

        def bmid(a):
            return a.unsqueeze(2).to_broadcast(sh4)

        def b2(a):
            return a.unsqueeze(2).unsqueeze(3).to_broadcast(sh4)

        cmul(Bhr[:], Bhi[:], bk(IPr[:]), bk(IPi[:]), bmid(Bbr[:]), bmid(Bbi[:]), sh4)
        cmul(Btr[:], Bti[:], b2(PWr[:, 7, :]), b2(PWi[:, 7, :]), Bhr[:], Bhi[:], sh4)
        dv(lambda e: e.tensor_scalar(out=nBhi[:], in0=Bhi[:], scalar1=-1.0, scalar2=None, op0=ALU.mult))
        cmul(Chr[:, :, 0:8, :], Chi[:, :, 0:8, :], bk(PWr[:, 0:8, :]), bk(PWi[:, 0:8, :]), bmid(Cre[:]), bmid(Cim[:]), sh4)
        cmul(Chr[:, :, 8, :], Chi[:, :, 8, :], bc(PWr[:, 8, :]), bc(PWi[:, 8, :]), Cre[:], Cim[:], sh3)
        dv(lambda e: e.tensor_scalar(out=nChi[:], in0=Chi[:], scalar1=-1.0, scalar2=None, op0=ALU.mult))
        dv(lambda e: e.tensor_copy(out=C1r[:].rearrange("p r (j c) -> p r j c", j=8), in_=Chr[:, :, 1:9, :]))
        dv(lambda e: e.tensor_copy(out=nC1i[:].rearrange("p r (j c) -> p r j c", j=8), in_=nChi[:, :, 1:9, :]))
        mtmp = S([128, 128], "mtmp")
        cx.op("pool", lambda e: e.memset(W2r[:], 0.0), reads=ins_b, writes=[SU])
        cx.op("pool", lambda e: e.memset(W2i[:], 0.0), reads=ins_b, writes=[SU])
        for r in range(16):
            for two in range(2):
                g = 2 * r + two
                rng = slice(two * 64, (two + 1) * 64)
                ps = getps()
                cx.op("pe", lambda e, ps=ps, r=r, rng=rng: e.matmul(
                    ps[:, 0:128], lhsT=Bhr[rng, r, :, :].rearrange("p k c -> p (k c)"),
                    rhs=Chr[rng, r, 0:8, :].rearrange("p j c -> p (j c)"), start=True, stop=False),
                    reads=[SU], writes=[ps.b])
                cx.op("pe", lambda e, ps=ps, r=r, rng=rng: e.matmul(
                    ps[:, 0:128], lhsT=nBhi[rng, r, :, :].rearrange("p k c -> p (k c)"),
                    rhs=Chi[rng, r, 0:8, :].rearrange("p j c -> p (j c)"), start=False, stop=True),
                    reads=[SU], writes=[ps.b])
                cx.op("dve", lambda e, ps=ps: e.tensor_tensor(out=mtmp[:], in0=ps[:, 0:128], in1=cmask[:],
                                                              op=ALU.mult),
                      reads=[ps.b, cmask.b], writes=[SU])
                cx.op("dve", lambda e, g=g: e.scalar_tensor_tensor(
                    out=M_all[:, g, :], in0=ident_f[:], scalar=dl[:, g:g + 1], in1=mtmp[:],
                    op0=ALU.mult, op1=ALU.add),
                    reads=[ident_f.b, dl.b], writes=[SU, M_all.b])
            for (Bt, W2) in ((Btr, W2r), (Bti, W2i)):
                ps = getps()
                cx.op("pe", lambda e, ps=ps, r=r, Bt=Bt: e.transpose(
                    out=ps[:, 0:128], in_=Bt[:, r, :, :].rearrange("p k c -> p (k c)"), identity=ident_f[:]),
                    reads=[SU, ident_f.b], writes=[ps.b])
                cx.op("dve", lambda e, ps=ps, r=r, W2=W2: e.tensor_copy(out=W2[:, r, 0, 0:64], in_=ps[:, 0:64]),
                      reads=[ps.b], writes=[SU, W2.b])
                cx.op("dve", lambda e, ps=ps, r=r, W2=W2: e.tensor_copy(out=W2[:, r, 1, 64:128], in_=ps[:, 64:128]),
                      reads=[ps.b], writes=[SU, W2.b])

        cx.barrier(skip_pool_dma=True)
        al.close()
        al = Alloc(nc)
        NCH = SEQ // 8
        Vg = [al.sb([128, NCH], BF16, f"Vg{i}") for i in range(4)]
        Sre = [[al.sb([128, NCH], F32, f"Sre{s}{i}") for i in range(2)] for s in range(2)]
        Sim = [[al.sb([128, NCH], F32, f"Sim{s}{i}") for i in range(2)] for s in range(2)]
        Sbr = [al.sb([128, NCH], BF16, f"Sbr{s}") for s in range(2)]
        Sbi = [al.sb([128, NCH], BF16, f"Sbi{s}") for s in range(2)]
        Gg = [al.sb([128, NCH], BF16, f"Gg{i}") for i in range(16)]
        ysf = [al.sb([128, SEQ], BF16, f"ysf{i}") for i in range(2)]
        for s in range(2):
            cx.op("pool", lambda e, s=s: e.memset(Sbr[s][:, 0:1], 0.0), writes=[Sbr[s].b])
            cx.op("pool", lambda e, s=s: e.memset(Sbi[s][:, 0:1], 0.0), writes=[Sbi[s].b])

        def b_front(r):
            f = r // 4
            st = r % 2
            vg = [Vg[(2 * r) % 4], Vg[(2 * r + 1) % 4]]
            for two in range(2):
                g = 2 * r + two
                gl = g % 8
                ps = getps()
                for k in range(8):
                    cx.op("pe", lambda e, ps=ps, gl=gl, k=k, f=f: e.matmul(
                        ps[:], lhsT=psel[:, gl, (7 - k) * 16:(7 - k) * 16 + 128],
                        rhs=u_all[:, f, k:SEQ:8], start=(k == 0), stop=(k == 7)),
                        reads=[psel.b, u_all.b], writes=[ps.b])
                cx.op("act", lambda e, ps=ps, v=vg[two]: e.copy(out=v[:], in_=ps[:]),
                      reads=[ps.b], full=[vg[two].b])
            psr = getps(); psi = getps()
            for (pp, W2) in ((psr, W2r), (psi, W2i)):
                for two in range(2):
                    cx.op("pe", lambda e, pp=pp, W2=W2, two=two, r=r, v=vg[two]: e.matmul(
                        pp[:], lhsT=W2[:, r, two, :], rhs=v[:], start=(two == 0), stop=(two == 1)),
                        reads=[W2.b, vg[two].b], writes=[pp.b])
            cx.op("act", lambda e, psr=psr, st=st: e.copy(out=Sre[st][0][:], in_=psr[:]),
                  reads=[psr.b], full=[Sre[st][0].b])
            cx.op("act", lambda e, psi=psi, st=st: e.copy(out=Sim[st][0][:], in_=psi[:]),
                  reads=[psi.b], full=[Sim[st][0].b])

        def b_mid(r):
            st = r % 2
            cur = 0
            for d_ in range(9):
                sh = 1 << d_
                s_r, s_i, d_r, d_i = Sre[st][cur], Sim[st][cur], Sre[st][1 - cur], Sim[st][1 - cur]
                n = NCH - sh
                cx.op("dve", lambda e, s_r=s_r, d_r=d_r, sh=sh, n=n, d_=d_, r=r: e.scalar_tensor_tensor(
                    out=d_r[:, sh:NCH], in0=s_r[:, 0:n], scalar=KAr[:, d_, r:r + 1], in1=s_r[:, sh:NCH],
                    op0=ALU.mult, op1=ALU.add), reads=[s_r.b, SU], writes=[d_r.b])
                cx.op("dve", lambda e, s_i=s_i, d_r=d_r, sh=sh, n=n, d_=d_, r=r: e.scalar_tensor_tensor(
                    out=d_r[:, sh:NCH], in0=s_i[:, 0:n], scalar=KnAi[:, d_, r:r + 1], in1=d_r[:, sh:NCH],
                    op0=ALU.mult, op1=ALU.add), reads=[s_i.b, SU], writes=[d_r.b])
                cx.op("dve", lambda e, s_i=s_i, d_i=d_i, sh=sh, n=n, d_=d_, r=r: e.scalar_tensor_tensor(
                    out=d_i[:, sh:NCH], in0=s_i[:, 0:n], scalar=KAr[:, d_, r:r + 1], in1=s_i[:, sh:NCH],
                    op0=ALU.mult, op1=ALU.add), reads=[s_i.b, SU], writes=[d_i.b])
                cx.op("dve", lambda e, s_r=s_r, d_i=d_i, sh=sh, n=n, d_=d_, r=r: e.scalar_tensor_tensor(
                    out=d_i[:, sh:NCH], in0=s_r[:, 0:n], scalar=KAi[:, d_, r:r + 1], in1=d_i[:, sh:NCH],
                    op0=ALU.mult, op1=ALU.add), reads=[s_r.b, SU], writes=[d_i.b])
                cx.op("pool", lambda e, s_r=s_r, d_r=d_r, sh=sh: e.tensor_copy(out=d_r[:, 0:sh], in_=s_r[:, 0:sh]),
                      reads=[s_r.b], writes=[d_r.b])
                cx.op("pool", lambda e, s_i=s_i, d_i=d_i, sh=sh: e.tensor_copy(out=d_i[:, 0:sh], in_=s_i[:, 0:sh]),
                      reads=[s_i.b], writes=[d_i.b])
                cur = 1 - cur
            fr, fi = Sre[st][cur], Sim[st][cur]
            cx.op("pool", lambda e, fr=fr, st=st: e.tensor_copy(out=Sbr[st][:, 1:NCH], in_=fr[:, 0:NCH - 1]),
                  reads=[fr.b], writes=[Sbr[st].b])
            cx.op("pool", lambda e, fi=fi, st=st: e.tensor_copy(out=Sbi[st][:, 1:NCH], in_=fi[:, 0:NCH - 1]),
                  reads=[fi.b], writes=[Sbi[st].b])

        def b_back(r):
            f = r // 4
            st = r % 2
            vg = [Vg[(2 * r) % 4], Vg[(2 * r + 1) % 4]]
            for two in range(2):
                g = 2 * r + two
                rng = slice(two * 64, (two + 1) * 64)
                ps = getps()
                cx.op("pe", lambda e, ps=ps, g=g, v=vg[two]: e.matmul(
                    ps[:], lhsT=M_all[:, g, :], rhs=v[:], start=True, stop=False),
                    reads=[M_all.b, vg[two].b], writes=[ps.b])
                cx.op("pe", lambda e, ps=ps, r=r, rng=rng, st=st: e.matmul(
                    ps[:], lhsT=C1r[rng, r, :], rhs=Sbr[st][rng, :], start=False, stop=False),
                    reads=[SU, Sbr[st].b], writes=[ps.b])
                cx.op("pe", lambda e, ps=ps, r=r, rng=rng, st=st: e.matmul(
                    ps[:], lhsT=nC1i[rng, r, :], rhs=Sbi[st][rng, :], start=False, stop=True),
                    reads=[SU, Sbi[st].b], writes=[ps.b])
                gg = Gg[g % 16]
                cx.op("act", lambda e, ps=ps, gg=gg: e.activation(out=gg[:], in_=ps[:], func=GELU),
                      reads=[ps.b], full=[gg.b])
            if r % 4 == 3:
                yb = ysf[f % 2]
                for j in range(8):
                    ps = getps()
                    for gl in range(8):
                        gg = Gg[(8 * f + gl) % 16]
                        cx.op("pe", lambda e, ps=ps, j=j, gl=gl, gg=gg: e.matmul(
                            ps[:], lhsT=psel[:, j, (7 - gl) * 16:(7 - gl) * 16 + 128], rhs=gg[:],
                            start=(gl == 0), stop=(gl == 7)),
                            reads=[psel.b, gg.b], writes=[ps.b])
                    cx.op("act", lambda e, ps=ps, yb=yb, j=j: e.copy(out=yb[:, j:SEQ:8], in_=ps[:]),
                          reads=[ps.b], writes=[yb.b])
                cx.op("sp", lambda e, yb=yb, f=f: e.dma_start(out=ys_scr[f], in_=yb[:]),
                      reads=[yb.b], dma=True)

        b_front(0)
        for r in range(16):
            b_mid(r)
            if r + 1 < 16:
                b_front(r + 1)
            b_back(r)

        cx.barrier()
        al.close()
        alAB.close()
        al = al_outer
        if stop_after in ("A", "B"):
            pass
        else:
            TC = 256
            NBC = SEQ // TC
            alC = Alloc(nc)
            wA = alC.sb([128, 8, 1536], BF16, "wA")
            wG = alC.sb([128, 8, 3072], BF16, "wG")
            wco = alC.sb([128, 4, 1024], BF16, "wco")
            wgl = alC.sb([128, 4, 2048], BF16, "wgl")
            wmo = alC.sb([128, 4, 1024], BF16, "wmo")
            wo = alC.sb([128, 8, 1024], BF16, "wo")
            Dg2 = [alC.sb([128, 31, 128], BF16, f"Dg{i}") for i in range(2)]
            kT = alC.sb([128, 4, 256], BF16, "kT")
            vtok = alC.sb([128, 2, 512], BF16, "vtok")
            gffn = alC.sb([128, D], F32, "gffn")
            wr = alC.sb([128, 8, 36], F32, "wr")
            rbias = alC.sb([128, 36], F32, "rbias")
            cdw = alC.sb([128, 4, 31], F32, "cdw")
            cb = alC.sb([128, 4], F32, "cb"); lng = alC.sb([128, 4], F32, "lng"); lnb = alC.sb([128, 4], F32, "lnb")
            onesm = alC.sb([128, 128], F32, "onesm")
            ones_bf = alC.sb([128, 128], BF16, "ones_bf")
            ecap = alC.sb([128, 32], F32, "ecap")
            tokid = alC.sb([128, NT], F32, "tokid")
            cum = alC.sb([128, 32], F32, "cum")
            lg_all = alC.sb([128, NT, 36], F32, "lg_all")
            trashp = alC.sb([128, 1], F32, "trashp")

            def ld(t, src):
                cx.op("sp", lambda e: e.dma_start(out=t[:], in_=src), full=[t.b], dma=True)

            ld(gffn, gffn_d.partition_broadcast(128))
            ld(wr, wr_d.rearrange("(c p) n -> p c n", p=128))
            ld(rbias, rbias_d.partition_broadcast(128))
            ld(cdw, cdw_d); ld(cb, cb_d); ld(lng, lng_d); ld(lnb, lnb_d)
            ld(ecap, ecap_d); ld(tokid, tokid_d); ld(trashp, trashp_d)
            cx.op("pool", lambda e: e.memset(onesm[:], 1.0 / 512.0), full=[onesm.b])
            cx.op("pool", lambda e: e.memset(ones_bf[:], 1.0), full=[ones_bf.b])
            cx.op("pool", lambda e: e.memset(cum[:], 0.0), full=[cum.b])
            alS = Alloc(nc)
            stg2 = [alS.sb([128, 8, 256], F32, f"stgc{i}") for i in range(2)]
            s2 = [0]

            def load_cast2(dst, dst_col0, src_d, c0, c1, kch, engs=("pool", "act")):
                for cc in range(c0, c1, 256):
                    w = min(256, c1 - cc)
                    s = stg2[s2[0] % 2]
                    eng = engs[s2[0] % len(engs)]
                    s2[0] += 1
                    src = src_d[:, cc:cc + w].rearrange("(c p) n -> p c n", p=128)
                    cx.op("sp", lambda e, s=s, src=src, w=w: e.dma_start(out=s[:, 0:kch, 0:w], in_=src),
                          full=[s.b], dma=True)
                    o = dst_col0 + (cc - c0)
                    if eng == "act":
                        cx.op("act", lambda e, s=s, o=o, w=w: e.copy(out=dst[:, 0:kch, o:o + w], in_=s[:, 0:kch, 0:w]),
                              reads=[s.b], writes=[dst.b])
                    else:
                        cx.op(eng, lambda e, s=s, o=o, w=w: e.tensor_copy(out=dst[:, 0:kch, o:o + w],
                                                                          in_=s[:, 0:kch, 0:w]),
                              reads=[s.b], writes=[dst.b])

            load_cast2(wA, 0, w_in_d, 0, 1024, 8)
            load_cast2(wA, 1024, w_in_d, 1536, 2048, 8)
            load_cast2(wG, 0, w_in_d, 2048, 5120, 8)
            load_cast2(wco, 0, wco_d, 0, 1024, 4)
            load_cast2(wgl, 0, wgl_d, 0, 2048, 4)
            load_cast2(wmo, 0, wmo_d, 0, 1024, 4)
            load_cast2(wo, 0, wo_d, 0, 1024, 8)
            wkv = alS.sb([128, 8, 1024], BF16, "wkv")
            load_cast2(wkv, 0, wkv_d, 0, 1024, 8)
            gmem = alS.sb([128, D], F32, "gmem")
            ld(gmem, gmem_d.partition_broadcast(128))
            memT = alS.sb([128, 8, 256], BF16, "memT")
            mx = alS.sb([128, D], F32, "mx"); mjunk = alS.sb([128, D], BF16, "mjunk")
            mss = alS.sb([128, 1], F32, "mss"); mrt = alS.sb([128, 1], F32, "mrt"); mrs = alS.sb([128, 1], F32, "mrs")
            mh = alS.sb([128, D], BF16, "mh")
            for mt in range(2):
                cx.op("sp", lambda e, mt=mt: e.dma_start(out=mx[:], in_=mem_d[mt * 128:(mt + 1) * 128, :]),
                      full=[mx.b], dma=True)
                cx.op("act", lambda e: e.activation(out=mjunk[:], in_=mx[:], func=AF.Square, accum_out=mss[:]),
                      reads=[mx.b], full=[mjunk.b, mss.b])
                cx.op("act", lambda e: e.activation(out=mrt[:], in_=mss[:], func=AF.Sqrt, scale=1.0 / D, bias=EPS),
                      reads=[mss.b], full=[mrt.b])
                cx.op("dve", lambda e: e.reciprocal(out=mrs[:], in_=mrt[:]), reads=[mrt.b], full=[mrs.b])
                cx.op("dve", lambda e: e.scalar_tensor_tensor(out=mh[:], in0=mx[:], scalar=mrs[:, 0:1], in1=gmem[:],
                                                              op0=ALU.mult, op1=ALU.mult),
                      reads=[mx.b, mrs.b, gmem.b], full=[mh.b])
                pb = psb[mt % 2]
                for c in range(8):
                    cx.op("pe", lambda e, pb=pb, c=c: e.transpose(out=pb[:, c * 128:(c + 1) * 128],
                                                                  in_=mh[:, c * 128:(c + 1) * 128],
                                                                  identity=ident_bf[:]),
                          reads=[mh.b, ident_bf.b], writes=[pb.b])
                cx.op("act", lambda e, pb=pb, mt=mt: e.copy(out=memT[:, :, mt * 128:(mt + 1) * 128],
                                                            in_=pb[:].rearrange("p (c t) -> p c t", c=8)),
                      reads=[pb.b], writes=[memT.b])
            for hd in range(4):
                ps = getps()
                for c in range(8):
                    cx.op("pe", lambda e, ps=ps, c=c, hd=hd: e.matmul(
                        ps[:, 0:256], lhsT=wkv[:, c, hd * 128:(hd + 1) * 128], rhs=memT[:, c, :],
                        start=(c == 0), stop=(c == 7)), reads=[wkv.b, memT.b], writes=[ps.b])
                cx.op("dve", lambda e, ps=ps, hd=hd: e.tensor_copy(out=kT[:, hd, :], in_=ps[:, 0:256]),
                      reads=[ps.b], writes=[kT.b])
            for mc in range(2):
                ps = getps()
                for c in range(8):
                    cx.op("pe", lambda e, ps=ps, c=c, mc=mc: e.matmul(
                        ps[:], lhsT=memT[:, c, mc * 128:(mc + 1) * 128], rhs=wkv[:, c, 512:1024],
                        start=(c == 0), stop=(c == 7)), reads=[wkv.b, memT.b], writes=[ps.b])
                cx.op("dve", lambda e, ps=ps, mc=mc: e.tensor_copy(out=vtok[:, mc, :], in_=ps[:]),
                      reads=[ps.b], writes=[vtok.b])
            cx.barrier()
            alS.close()

            alW = Alloc(nc)
            hT = [alW.sb([128, 8, TC], BF16, f"hTc{i}") for i in range(2)]
            ysb = [alW.sb([128, 4, TC], BF16, "ysb0")] * 2
            vbuf = alW.sb([128, 4, 30 + TC], BF16, "vbuf")
            sgt = [alW.sb([128, TC], F32, f"sgt{i}") for i in range(3)]
            cv = alW.sb([128, 4, TC], F32, "cv")
            sq = [sgt[1], sgt[2]]
            mean = alW.sb([128, TC], F32, "mean")
            var = alW.sb([128, TC], F32, "var"); lrs = alW.sb([128, TC], F32, "lrs")
            m2 = var; lnv = lrs
            cn = alW.sb([128, 4, TC], BF16, "cn")
            qb = alW.sb([128, 4, TC], BF16, "qb")
            Eb = [alW.sb([128, 2, TC], BF16, f"Eb{i}") for i in range(2)]
            ob = alW.sb([128, 4, TC], BF16, "ob")
            macc = alW.sb([128, TC], F32, "macc"); mt1 = alW.sb([128, TC], F32, "mt1"); mt2 = alW.sb([128, TC], F32, "mt2")
            rden = macc
            xc = [mt1, mt2]
            sqf = [sgt[1], sgt[2], mt1, mt2]
            merged = alW.sb([128, 8, TC], BF16, "merged")
            xt2 = [alW.sb([128, D], F32, f"xtc{i}") for i in range(2)]
            x2t = xt2
            h2f = alW.sb([128, D], F32, "h2f"); h2b = [alW.sb([128, D], BF16, "h2b0")] * 2
            junk2 = h2b[0]
            h2T = alW.sb([128, 8, 128], F32, "h2T")
            ss2 = alW.sb([128, 1], F32, "ss2"); rt2 = alW.sb([128, 1], F32, "rt2"); rs2 = alW.sb([128, 1], F32, "rs2")
            cx.op("pool", lambda e: e.memset(vbuf[:], 0.0), full=[vbuf.b])

            breg = {}

            def mmgrp(ps_ap, ps_b, pairs, reads):
                n = len(pairs)
                for idx, (l, r_) in enumerate(pairs):
                    cx.op("pe", lambda e, l=l, r_=r_, idx=idx: e.matmul(ps_ap, lhsT=l, rhs=r_, start=(idx == 0),
                                                                         stop=(idx == n - 1)),
                          reads=reads, writes=[ps_b])

            KCUT = int(os.environ.get("KCUT", "9"))
            KNB = int(os.environ.get("KNB", str(NBC)))
            def c_load_h(bi):
                t0 = bi * TC
                h = hT[bi % 2]
                cx.op("sp", lambda e, h=h, t0=t0: e.dma_start(
                    out=h[:], in_=hT_scr[:, :, t0:t0 + TC].rearrange("c p t -> p c t")), full=[h.b], dma=True)

            def c_load_y(bi):
                t0 = bi * TC
                yb = ysb[bi % 2]
                cx.op("sp", lambda e, yb=yb, t0=t0: e.dma_start(
                    out=yb[:], in_=ys_scr[:, :, t0:t0 + TC].rearrange("f p t -> p f t")), full=[yb.b], dma=True)

            def c_s2(bi):
                t0 = bi * TC
                h = hT[bi % 2]; yb = ysb[bi % 2]
                for f in range(4):
                    pa = getps(); pg = getps()
                    mmgrp(pa[:, 0:TC], pa.b, [(wA[:, c, f * 128:(f + 1) * 128], h[:, c, :]) for c in range(8)],
                          [wA.b, h.b])
                    mmgrp(pg[:, 0:TC], pg.b, [(wA[:, c, 512 + f * 128:512 + (f + 1) * 128], h[:, c, :]) for c in range(8)],
                          [wA.b, h.b])
                    s = sgt[f % 3]
                    cx.op("act", lambda e, pg=pg, s=s: e.activation(out=s[:], in_=pg[:, 0:TC], func=AF.Sigmoid),
                          reads=[pg.b], full=[s.b])
                    cx.op("dve", lambda e, pa=pa, s=s, f=f: e.tensor_tensor(out=vbuf[:, f, 30:30 + TC], in0=pa[:, 0:TC],
                                                                            in1=s[:], op=ALU.mult),
                          reads=[pa.b, s.b], writes=[vbuf.b])

            def c_mid(bi):
                t0 = bi * TC
                h = hT[bi % 2]; yb = ysb[bi % 2]
                for f in range(4):
                    pc = getps()
                    Dg = Dg2[f % 2]
                    cx.op("pool", lambda e, Dg=Dg, f=f: e.tensor_tensor(
                        out=Dg[:], in0=ident_f[:].unsqueeze(1).to_broadcast([128, 31, 128]),
                        in1=cdw[:, f, :].unsqueeze(2).to_broadcast([128, 31, 128]), op=ALU.mult),
                        reads=[ident_f.b, cdw.b], full=[Dg.b])
                    mmgrp(pc[:, 0:TC], pc.b, [(Dg[:, k, :], vbuf[:, f, k:k + TC]) for k in range(31)],
                          [Dg.b, vbuf.b])
                    cx.op("act", lambda e, pc=pc, f=f: e.activation(out=cv[:, f, :], in_=pc[:, 0:TC], func=AF.Identity,
                                                                    bias=cb[:, f:f + 1], scale=1.0),
                          reads=[pc.b, cb.b], writes=[cv.b])
                cx.op("pool", lambda e: e.tensor_copy(out=vbuf[:, :, 0:30], in_=vbuf[:, :, TC:TC + 30]),
                      reads=[vbuf.b], writes=[vbuf.b])
                for hd in range(4):
                    pq_ = getps()
                    mmgrp(pq_[:, 0:TC], pq_.b, [(wA[:, c, 1024 + hd * 128:1024 + (hd + 1) * 128], h[:, c, :])
                                                for c in range(8)], [wA.b, h.b])
                    cx.op("dve", lambda e, pq_=pq_, hd=hd: e.tensor_copy(out=qb[:, hd, :], in_=pq_[:, 0:TC]),
                          reads=[pq_.b], writes=[qb.b])
                for f in range(4):
                    cx.op("act", lambda e, f=f: e.activation(out=sq[f % 2][:] if False else sqf[f][:], in_=cv[:, f, :],
                                                             func=AF.Square),
                          reads=[cv.b], full=[sqf[f].b])

                def att_scores(hd):
                    E = Eb[hd % 2]
                    for mc in range(2):
                        psc = getps()
                        mmgrp(psc[:, 0:TC], psc.b, [(kT[:, hd, mc * 128:(mc + 1) * 128], qb[:, hd, :])], [kT.b, qb.b])
                        cx.op("act", lambda e, psc=psc, E=E, mc=mc: e.activation(
                            out=E[:, mc, :], in_=psc[:, 0:TC], func=AF.Exp, scale=float(128 ** -0.5)),
                            reads=[psc.b], writes=[E.b])

                def att_out(hd):
                    E = Eb[hd % 2]
                    po = getps(); pd = getps()
                    mmgrp(po[:, 0:TC], po.b, [(vtok[:, mc, hd * 128:(hd + 1) * 128], E[:, mc, :]) for mc in range(2)],
                          [vtok.b, E.b])
                    mmgrp(pd[:, 0:TC], pd.b, [(ones_bf[:], E[:, mc, :]) for mc in range(2)], [ones_bf.b, E.b])
                    cx.op("dve", lambda e, pd=pd: e.reciprocal(out=rden[:], in_=pd[:, 0:TC]), reads=[pd.b], full=[rden.b])
                    cx.op("dve", lambda e, po=po, hd=hd: e.tensor_tensor(out=ob[:, hd, :], in0=po[:, 0:TC], in1=rden[:],
                                                                         op=ALU.mult),
                          reads=[po.b, rden.b], writes=[ob.b])

                att_scores(0)
                att_scores(1)
                pm = getps(); pq = getps()
                mmgrp(pm[:, 0:TC], pm.b, [(onesm[:], cv[:, f, :]) for f in range(4)], [onesm.b, cv.b])
                mmgrp(pq[:, 0:TC], pq.b, [(onesm[:], sqf[f][:]) for f in range(4)], [onesm.b] + [sqf[f].b for f in range(4)])
                cx.op("act", lambda e, pm=pm: e.copy(out=mean[:], in_=pm[:, 0:TC]), reads=[pm.b], full=[mean.b])
                cx.op("dve", lambda e: e.tensor_tensor(out=var[:], in0=mean[:], in1=mean[:], op=ALU.mult),
                      reads=[mean.b], full=[var.b])
                cx.op("dve", lambda e, pq=pq: e.tensor_tensor(out=var[:], in0=pq[:, 0:TC], in1=var[:], op=ALU.subtract),
                      reads=[pq.b], writes=[var.b])
                cx.op("dve", lambda e: e.tensor_scalar(out=var[:], in0=var[:], scalar1=float(EPS), scalar2=None,
                                                       op0=ALU.add), reads=[var.b], writes=[var.b])
                cx.op("act", lambda e: e.activation(out=lrs[:], in_=var[:], func=AF.Ln), reads=[var.b], full=[lrs.b])
                cx.op("act", lambda e: e.activation(out=lrs[:], in_=lrs[:], func=AF.Exp, scale=-0.5),
                      reads=[], writes=[lrs.b])
                cx.op("dve", lambda e: e.tensor_tensor(out=cv[:], in0=cv[:],
                                                       in1=mean[:].unsqueeze(1).to_broadcast([128, 4, TC]),
                                                       op=ALU.subtract), reads=[mean.b], writes=[cv.b])
                cx.op("dve", lambda e: e.tensor_tensor(out=cv[:], in0=cv[:],
                                                       in1=lrs[:].unsqueeze(1).to_broadcast([128, 4, TC]),
                                                       op=ALU.mult), reads=[lrs.b], writes=[cv.b])
                att_out(0)
                att_scores(2)
                att_out(1)
                att_scores(3)
                att_out(2)
                att_out(3)
                for f in range(4):
                    cx.op("act", lambda e, f=f: e.activation(out=cn[:, f, :], in_=cv[:, f, :], func=AF.Silu,
                                                             bias=lnb[:, f:f + 1], scale=lng[:, f:f + 1]),
                          reads=[cv.b, lnb.b, lng.b], writes=[cn.b])
                for j in range(8):
                    js = slice(j * 128, (j + 1) * 128)
                    pga = getps(); pyc = getps()
                    mmgrp(pga[:, 0:TC], pga.b, [(wG[:, c, j * 128:(j + 1) * 128], h[:, c, :]) for c in range(8)], [wG.b, h.b])
                    mmgrp(pyc[:, 0:TC], pyc.b, [(wco[:, f, js], cn[:, f, :]) for f in range(4)], [wco.b, cn.b])
                    s = sgt[0]
                    cx.op("act", lambda e, pga=pga, s=s: e.activation(out=s[:], in_=pga[:, 0:TC], func=AF.Sigmoid),
                          reads=[pga.b], full=[s.b])
                    cx.op("dve", lambda e, pyc=pyc, s=s: e.tensor_tensor(out=macc[:], in0=pyc[:, 0:TC], in1=s[:], op=ALU.mult),
                          reads=[pyc.b, s.b], full=[macc.b])
                    pgb = getps(); pza = getps(); pzb = getps()
                    mmgrp(pgb[:, 0:TC], pgb.b, [(wG[:, c, 1024 + j * 128:1024 + (j + 1) * 128], h[:, c, :]) for c in range(8)],
                          [wG.b, h.b])
                    mmgrp(pza[:, 0:TC], pza.b, [(wgl[:, f, js], yb[:, f, :]) for f in range(4)], [wgl.b, yb.b])
                    mmgrp(pzb[:, 0:TC], pzb.b, [(wgl[:, f, 1024 + j * 128:1024 + (j + 1) * 128], yb[:, f, :]) for f in range(4)],
                          [wgl.b, yb.b])
                    sb_ = sgt[1]; sz = sgt[2]
                    cx.op("act", lambda e, pgb=pgb, sb_=sb_: e.activation(out=sb_[:], in_=pgb[:, 0:TC], func=AF.Sigmoid),
                          reads=[pgb.b], full=[sb_.b])
                    cx.op("act", lambda e, pzb=pzb, sz=sz: e.activation(out=sz[:], in_=pzb[:, 0:TC], func=AF.Sigmoid),
                          reads=[pzb.b], full=[sz.b])
                    cx.op("dve", lambda e, pza=pza, sz=sz: e.tensor_tensor(out=mt1[:], in0=pza[:, 0:TC], in1=sz[:], op=ALU.mult),
                          reads=[pza.b, sz.b], full=[mt1.b])
                    cx.op("dve", lambda e, sb_=sb_: e.tensor_tensor(out=mt1[:], in0=mt1[:], in1=sb_[:], op=ALU.mult),
                          reads=[sb_.b], writes=[mt1.b])
                    cx.op("dve", lambda e: e.tensor_tensor(out=macc[:], in0=macc[:], in1=mt1[:], op=ALU.add),
                          reads=[mt1.b], writes=[macc.b])
                    pgc = getps(); pym = getps()
                    mmgrp(pgc[:, 0:TC], pgc.b, [(wG[:, c, 2048 + j * 128:2048 + (j + 1) * 128], h[:, c, :]) for c in range(8)],
                          [wG.b, h.b])
                    mmgrp(pym[:, 0:TC], pym.b, [(wmo[:, hd, js], ob[:, hd, :]) for hd in range(4)], [wmo.b, ob.b])
                    s = sgt[0]
                    cx.op("act", lambda e, pgc=pgc, s=s: e.activation(out=s[:], in_=pgc[:, 0:TC], func=AF.Sigmoid),
                          reads=[pgc.b], full=[s.b])
                    cx.op("dve", lambda e, pym=pym, s=s: e.tensor_tensor(out=mt2[:], in0=pym[:, 0:TC], in1=s[:], op=ALU.mult),
                          reads=[pym.b, s.b], full=[mt2.b])
                    cx.op("dve", lambda e, j=j: e.tensor_tensor(out=merged[:, j, :], in0=macc[:], in1=mt2[:], op=ALU.add),
                          reads=[macc.b, mt2.b], writes=[merged.b])

            def c_tail(bi):
                ntt = TC // 128
                tis = [bi * ntt + tt for tt in range(ntt)]
                for tt, ti in enumerate(tis):
                    xt_ = xt2[ti % 2]
                    cx.op("sp", lambda e, xt_=xt_, ti=ti: e.dma_start(out=xt_[:], in_=x_d[ti * 128:(ti + 1) * 128, :]),
                          full=[xt_.b], dma=True)
                for tt, ti in enumerate(tis):
                    xt_ = xt2[ti % 2]; x2 = xt_
                    for half in range(2):
                        po_ = getps()
                        mmgrp(po_[:], po_.b, [(merged[:, j, tt * 128:(tt + 1) * 128], wo[:, j, half * 512:(half + 1) * 512])
                                              for j in range(8)], [merged.b, wo.b])
                        cx.op("dve", lambda e, po_=po_, x2=x2, xt_=xt_, half=half: e.tensor_tensor(
                            out=x2[:, half * 512:(half + 1) * 512], in0=po_[:], in1=xt_[:, half * 512:(half + 1) * 512],
                            op=ALU.add), reads=[po_.b, xt_.b], writes=[x2.b])
                    cx.op("sp", lambda e, x2=x2, ti=ti: e.dma_start(out=x2_scr[ti * 128:(ti + 1) * 128, :], in_=x2[:]),
                          reads=[x2.b], writes=[x2B], dma=True)
                for tt, ti in enumerate(tis):
                    x2 = xt2[ti % 2]; hb2 = h2b[0]
                    cx.op("act", lambda e, x2=x2: e.activation(out=junk2[:], in_=x2[:], func=AF.Square, accum_out=ss2[:]),
                          reads=[x2.b], full=[junk2.b, ss2.b])
                    cx.op("act", lambda e: e.activation(out=rt2[:], in_=ss2[:], func=AF.Sqrt, scale=1.0 / D, bias=EPS),
                          reads=[ss2.b], full=[rt2.b])
                    cx.op("dve", lambda e: e.reciprocal(out=rs2[:], in_=rt2[:]), reads=[rt2.b], full=[rs2.b])
                    cx.op("dve", lambda e, x2=x2: e.scalar_tensor_tensor(out=h2f[:], in0=x2[:], scalar=rs2[:, 0:1],
                                                                         in1=gffn[:], op0=ALU.mult, op1=ALU.mult),
                          reads=[x2.b, rs2.b, gffn.b], full=[h2f.b])
                    cx.op("act", lambda e, hb2=hb2: e.copy(out=hb2[:], in_=h2f[:]), reads=[h2f.b], full=[hb2.b])
                    cx.op("sp", lambda e, hb2=hb2, ti=ti: e.dma_start(out=h2_scr[ti * 128:(ti + 1) * 128, :], in_=hb2[:]),
                          reads=[hb2.b], writes=[h2B], dma=True)
                    pra = getps(); prb = getps()
                    for c in range(8):
                        pr = pra if c < 4 else prb
                        cx.op("pe", lambda e, pr=pr, c=c: e.transpose(out=pr[:, (c % 4) * 128:(c % 4 + 1) * 128],
                                                                      in_=h2f[:, c * 128:(c + 1) * 128], identity=ident_f[:]),
                              reads=[h2f.b, ident_f.b], writes=[pr.b])
                    cx.op("act", lambda e, pra=pra: e.copy(out=h2T[:, 0:4, :], in_=pra[:].rearrange("p (c t) -> p c t", c=4)),
                          reads=[pra.b], writes=[h2T.b])
                    cx.op("dve", lambda e, prb=prb: e.tensor_copy(out=h2T[:, 4:8, :],
                                                                  in_=prb[:].rearrange("p (c t) -> p c t", c=4)),
                          reads=[prb.b], writes=[h2T.b])
                    plg = getps()
                    mmgrp(plg[:, 0:36], plg.b, [(h2T[:, c, :], wr[:, c, :]) for c in range(8)], [h2T.b, wr.b])
                    cx.op("dve", lambda e, plg=plg, ti=ti: e.tensor_tensor(out=lg_all[:, ti, :], in0=plg[:, 0:36],
                                                                        in1=rbias[:], op=ALU.add),
                          reads=[plg.b, rbias.b], writes=[lg_all.b])

            NBR = min(NBC, KNB) if KCUT >= 2 else 0
            if NBR > 0:
                c_load_h(0); c_load_y(0); c_s2(0)
            for bi in range(NBR):
                if bi + 1 < NBR:
                    c_load_h(bi + 1)
                c_mid(bi)
                if bi + 1 < NBR:
                    c_load_y(bi + 1)
                    c_s2(bi + 1)
                c_tail(bi)
            cx.barrier()
            alW.close()
            alR = Alloc(nc)
            RS = Buf("route")
            tri = alR.sb([128, 128], F32, "tri")
            ones_f = alR.sb([128, 128], F32, "ones_f")
            cx.op("sp", lambda e: e.dma_start(out=tri[:], in_=tri_d), full=[tri.b], dma=True)
            cx.op("pool", lambda e: e.memset(ones_f[:], 1.0), full=[ones_f.b])

            def rd(fn, extra_reads=(), extra_writes=()):
                cx.op("dve", fn, reads=[RS, lg_all.b] + list(extra_reads), writes=[RS] + list(extra_writes))

            def R(shape, name, dt=F32):
                return alR.sb(shape, dt, name)

            NTT = NT
            NEB_ = NEXP * NBLK
            gmax = R([128, NTT], "gmax"); ohg = R([128, NTT, 4], "ohg"); eg = R([128, NTT, 4], "eg")
            sumg = R([128, NTT], "sumg"); ptop = R([128, NTT], "ptop")
            selm = R([128, NTT, 4, 8], "selm"); sel = R([128, NTT, 8], "sel"); sel2 = R([128, NTT, 8], "sel2")
            m1_ = R([128, NTT], "m1_"); m2_ = R([128, NTT], "m2_"); oh1 = R([128, NTT, 8], "oh1"); oh2 = R([128, NTT, 8], "oh2")
            dm = R([128, NTT], "dm"); w1 = R([128, NTT], "w1"); w2 = R([128, NTT], "w2")
            M1 = R([128, NTT, 4, 8], "M1"); M2 = R([128, NTT, 4, 8], "M2"); Mc = R([128, NTT, 32], "Mc")
            Cex = R([128, NTT, 32], "Cex"); pos = R([128, NTT, 32], "pos"); bk = R([128, NTT, 32], "bk")
            sf = R([128, NTT, 32], "sf"); ov = R([128, NTT, 32], "ov"); tq = R([128, NTT, 32], "tq")
            sk = [R([128, NTT], f"sk{k}") for k in range(2)]; okk = R([128, NTT], "okk"); dd = R([128, NTT], "dd")
            si = [R([128, NTT], f"si{k}", I32) for k in range(2)]
            ent = [R([128, NTT, 4], f"ent{k}") for k in range(2)]
            le4 = lg_all[:, :, 4:36].rearrange("p t (g j) -> p t g j", g=4)

            def bc3(a, n):
                return a.unsqueeze(2).to_broadcast([128, NTT, n])

            rd(lambda e: e.tensor_reduce(out=gmax[:], in_=lg_all[:, :, 0:4], axis=AX.X, op=ALU.max))
            rd(lambda e: e.tensor_tensor(out=ohg[:], in0=lg_all[:, :, 0:4], in1=bc3(gmax[:], 4), op=ALU.is_equal))
            rd(lambda e: e.tensor_tensor(out=eg[:], in0=lg_all[:, :, 0:4], in1=bc3(gmax[:], 4), op=ALU.subtract))
            cx.op("act", lambda e: e.activation(out=eg[:], in_=eg[:], func=AF.Exp), reads=[RS], writes=[RS])
            rd(lambda e: e.tensor_reduce(out=sumg[:], in_=eg[:], axis=AX.X, op=ALU.add))
            rd(lambda e: e.reciprocal(out=ptop[:], in_=sumg[:]))
            rd(lambda e: e.tensor_tensor(out=selm[:], in0=le4,
                                         in1=ohg[:].unsqueeze(3).to_broadcast([128, NTT, 4, 8]), op=ALU.mult))
            rd(lambda e: e.tensor_reduce(out=sel[:], in_=selm[:].rearrange("p t g j -> p t j g"), axis=AX.X, op=ALU.add))
            rd(lambda e: e.tensor_reduce(out=m1_[:], in_=sel[:], axis=AX.X, op=ALU.max))
            rd(lambda e: e.tensor_tensor(out=oh1[:], in0=sel[:], in1=bc3(m1_[:], 8), op=ALU.is_equal))
            rd(lambda e: e.scalar_tensor_tensor(out=sel2[:], in0=oh1[:], scalar=-1e30, in1=sel[:], op0=ALU.mult, op1=ALU.add))
            rd(lambda e: e.tensor_reduce(out=m2_[:], in_=sel2[:], axis=AX.X, op=ALU.max))
            rd(lambda e: e.tensor_tensor(out=oh2[:], in0=sel2[:], in1=bc3(m2_[:], 8), op=ALU.is_equal))
            rd(lambda e: e.tensor_tensor(out=dm[:], in0=m1_[:], in1=m2_[:], op=ALU.subtract))
            cx.op("act", lambda e: e.activation(out=w1[:], in_=dm[:], func=AF.Sigmoid), reads=[RS], writes=[RS])
            rd(lambda e: e.tensor_tensor(out=w1[:], in0=w1[:], in1=ptop[:], op=ALU.mult))
            rd(lambda e: e.tensor_tensor(out=w2[:], in0=ptop[:], in1=w1[:], op=ALU.subtract))
            rd(lambda e: e.tensor_tensor(out=M1[:], in0=ohg[:].unsqueeze(3).to_broadcast([128, NTT, 4, 8]),
                                         in1=oh1[:].unsqueeze(2).to_broadcast([128, NTT, 4, 8]), op=ALU.mult))
            rd(lambda e: e.tensor_tensor(out=M2[:], in0=ohg[:].unsqueeze(3).to_broadcast([128, NTT, 4, 8]),
                                         in1=oh2[:].unsqueeze(2).to_broadcast([128, NTT, 4, 8]), op=ALU.mult))
            rd(lambda e: e.tensor_tensor(out=Mc[:], in0=M1[:].rearrange("p t g j -> p t (g j)"),
                                         in1=M2[:].rearrange("p t g j -> p t (g j)"), op=ALU.add))
            rd(lambda e: e.memset(Cex[:, 0, :], 0.0))
            for i in range(1, NTT):
                rd(lambda e, i=i: e.tensor_tensor(out=Cex[:, i, :], in0=Cex[:, i - 1, :], in1=Mc[:, i - 1, :], op=ALU.add))
            pp = [getps(), getps()]
            for i in range(NTT):
                pb_ = pp[i // 16]
                o_ = pb_[:, (i % 16) * 32:(i % 16 + 1) * 32]
                cx.op("pe", lambda e, o_=o_, i=i: e.matmul(o_, lhsT=tri[:], rhs=Mc[:, i, :], start=True, stop=False),
                      reads=[tri.b, RS], writes=[pb_.b])
                cx.op("pe", lambda e, o_=o_, i=i: e.matmul(o_, lhsT=ones_f[:], rhs=Cex[:, i, :], start=False, stop=True),
                      reads=[ones_f.b, RS], writes=[pb_.b])
            for hh in range(2):
                rd(lambda e, hh=hh: e.tensor_copy(out=pos[:, hh * 16:(hh + 1) * 16, :],
                                                  in_=pp[hh][:].rearrange("p (t x) -> p t x", t=16)), [pp[hh].b])
            rd(lambda e: e.tensor_single_scalar(out=bk[:], in_=pos[:], scalar=127.5, op=ALU.is_gt))
            for thr in range(2, NBLK):
                rd(lambda e, thr=thr: e.tensor_single_scalar(out=tq[:], in_=pos[:], scalar=128.0 * thr - 0.5, op=ALU.is_gt))
                rd(lambda e: e.tensor_tensor(out=bk[:], in0=bk[:], in1=tq[:], op=ALU.add))
            rd(lambda e: e.scalar_tensor_tensor(out=bk[:], in0=bk[:], scalar=float(1 - 128 * NEB_),
                                                in1=ecap[:].unsqueeze(1).to_broadcast([128, NTT, 32]),
                                                op0=ALU.mult, op1=ALU.add), [ecap.b])
            rd(lambda e: e.scalar_tensor_tensor(out=sf[:], in0=pos[:], scalar=float(NEB_), in1=bk[:],
                                                op0=ALU.mult, op1=ALU.add))
            rd(lambda e: e.tensor_single_scalar(out=ov[:], in_=pos[:], scalar=float(CAP) - 0.5, op=ALU.is_gt))
            for k, (Mk, wk) in enumerate(((M1, w1), (M2, w2))):
                Mk32 = Mk[:].rearrange("p t g j -> p t (g j)")
                rd(lambda e, Mk32=Mk32: e.tensor_tensor(out=tq[:], in0=Mk32, in1=sf[:], op=ALU.mult))
                rd(lambda e, k=k: e.tensor_reduce(out=sk[k][:], in_=tq[:], axis=AX.X, op=ALU.add))
                rd(lambda e, Mk32=Mk32: e.tensor_tensor(out=tq[:], in0=Mk32, in1=ov[:], op=ALU.mult))
                rd(lambda e: e.tensor_reduce(out=okk[:], in_=tq[:], axis=AX.X, op=ALU.add))
                rd(lambda e, k=k: e.tensor_scalar(out=dd[:], in0=sk[k][:], scalar1=trashp[:, 0:1], scalar2=None,
                                                  op0=ALU.subtract), [trashp.b])
                rd(lambda e: e.tensor_tensor(out=dd[:], in0=dd[:], in1=okk[:], op=ALU.mult))
                rd(lambda e, k=k: e.tensor_tensor(out=sk[k][:], in0=sk[k][:], in1=dd[:], op=ALU.subtract))
                rd(lambda e, k=k: e.tensor_copy(out=si[k][:], in_=sk[k][:]), (), [si[k].b])
                rd(lambda e, k=k: e.memset(ent[k][:], 0.0), (), [ent[k].b])
                rd(lambda e, k=k: e.tensor_copy(out=ent[k][:, :, 0], in_=tokid[:]), [tokid.b], [ent[k].b])
                rd(lambda e, k=k: e.tensor_scalar(out=ent[k][:, :, 1], in0=tokid[:], scalar1=float(k * ROWS), scalar2=None,
                                                  op0=ALU.add), [tokid.b], [ent[k].b])
                rd(lambda e, k=k, wk=wk: e.tensor_copy(out=ent[k][:, :, 2], in_=wk[:]), (), [ent[k].b])
            for i in range(NTT):
                for k in range(2):
                    cx.op("pool", lambda e, i=i, k=k: e.indirect_dma_start(
                        out=lst_d, out_offset=bass.IndirectOffsetOnAxis(ap=si[k][:, i:i + 1], axis=0),
                        in_=ent[k][:, i, :], in_offset=None),
                        reads=[si[k].b, ent[k].b], writes=[lstB], dma=True)
            cx.barrier()
            alR.close()
            alC.close()
        if stop_after in ("A", "B", "C"):
            pass
        else:
            alD = Alloc(nc)
            NEB = NEXP * NBLK
            lst_sb = alD.sb([128, NEB, 4], F32, "lst_sb")
            idx_i = alD.sb([128, NEB], I32, "idx_i")
            dst_i = alD.sb([128, NEB], I32, "dst_i")
            cx.op("sp", lambda e: e.dma_start(out=lst_sb[:], in_=lst_d[0:NEXP * CAP, :].rearrange("(s eb) w -> s eb w", s=128)),
                  reads=[lstB], full=[lst_sb.b], dma=True)
            cx.op("dve", lambda e: e.tensor_copy(out=idx_i[:], in_=lst_sb[:, :, 0]), reads=[lst_sb.b], full=[idx_i.b])
            cx.op("dve", lambda e: e.tensor_copy(out=dst_i[:], in_=lst_sb[:, :, 1]), reads=[lst_sb.b], full=[dst_i.b])
            sg_ = [alD.sb([128, 8, 256], F32, f"sg{i}") for i in range(2)]
            su_ = [alD.sb([128, 8, 256], F32, f"su{i}") for i in range(2)]
            sd_ = [alD.sb([128, 2, 1024], F32, f"sd{i}") for i in range(2)]
            Wg = [alD.sb([128, 8, 256], BF16, f"Wg{i}") for i in range(2)]
            Wu = [alD.sb([128, 8, 256], BF16, f"Wu{i}") for i in range(2)]
            Wd = [alD.sb([128, 2, 1024], BF16, f"Wd{i}") for i in range(2)]
            Gt = [alD.sb([128, D], BF16, f"Gt{i}") for i in range(3)]
            Xe = [alD.sb([128, 8, CAP], BF16, f"Xe{i}") for i in range(2)]
            sgl = [alD.sb([128, CAP], F32, f"sgl{i}") for i in range(2)]
            ae = [alD.sb([128, 2, CAP], BF16, f"ae{i}") for i in range(2)]
            Yt = [alD.sb([128, D], BF16, f"Yt{i}") for i in range(3)]

            Gt6 = Gt + [alD.sb([128, D], BF16, f"Gtx{i}") for i in range(3)]

            def load_w_dma(e_):
                p = e_ % 2
                cx.op("sp", lambda e: e.dma_start(out=sg_[p][:], in_=weg_d[e_]), full=[sg_[p].b], dma=True)
                cx.op("sp", lambda e: e.dma_start(out=su_[p][:], in_=weu_d[e_]), full=[su_[p].b], dma=True)
                cx.op("sp", lambda e: e.dma_start(out=sd_[p][:], in_=wed_d[e_]), full=[sd_[p].b], dma=True)

            def load_w_cast(e_):
                p = e_ % 2
                cx.op("dve", lambda e: e.tensor_copy(out=Wg[p][:], in_=sg_[p][:]), reads=[sg_[p].b], full=[Wg[p].b])
                cx.op("act", lambda e: e.copy(out=Wu[p][:], in_=su_[p][:]), reads=[su_[p].b], full=[Wu[p].b])
                cx.op("pool", lambda e: e.tensor_copy(out=Wd[p][:], in_=sd_[p][:]), reads=[sd_[p].b], full=[Wd[p].b])

            def gathers(e_):
                for blk in range(NBLK):
                    eb = e_ * NBLK + blk
                    G = Gt6[(e_ % 2) * 3 + blk]
                    cx.op("pool", lambda e, G=G, eb=eb: e.indirect_dma_start(
                        out=G[:], out_offset=None, in_=h2_scr,
                        in_offset=bass.IndirectOffsetOnAxis(ap=idx_i[:, eb:eb + 1], axis=0)),
                        reads=[idx_i.b, h2B], full=[G.b], dma=True)

            gi = [0]
            load_w_dma(0)
            gathers(0)
            load_w_cast(0)
            KNE = int(os.environ.get("KNE", str(NEXP)))
            for e_ in range(KNE):
                p = e_ % 2
                if e_ + 1 < NEXP:
                    load_w_dma(e_ + 1)
                    gathers(e_ + 1)
                X = Xe[p]
                for blk in range(NBLK):
                    G = Gt6[(e_ % 2) * 3 + blk]
                    pbk = psb[gi[0] % 2]
                    gi[0] += 1
                    for c in range(8):
                        cx.op("pe", lambda e, pbk=pbk, G=G, c=c: e.transpose(
                            out=pbk[:, c * 128:(c + 1) * 128], in_=G[:, c * 128:(c + 1) * 128], identity=ident_bf[:]),
                            reads=[G.b, ident_bf.b], writes=[pbk.b])
                    if blk % 2 == 0:
                        cx.op("act", lambda e, pbk=pbk, X=X, blk=blk: e.copy(
                            out=X[:, :, blk * 128:(blk + 1) * 128], in_=pbk[:].rearrange("p (c t) -> p c t", c=8)),
                            reads=[pbk.b], writes=[X.b])
                    else:
                        cx.op("dve", lambda e, pbk=pbk, X=X, blk=blk: e.tensor_copy(
                            out=X[:, :, blk * 128:(blk + 1) * 128], in_=pbk[:].rearrange("p (c t) -> p c t", c=8)),
                            reads=[pbk.b], writes=[X.b])
                a_ = ae[p]
                for ft in range(2):
                    pg = getps(); pu = getps()
                    for c in range(8):
                        cx.op("pe", lambda e, pg=pg, c=c, ft=ft, X=X, p=p: e.matmul(
                            pg[:, 0:CAP], lhsT=Wg[p][:, c, ft * 128:(ft + 1) * 128], rhs=X[:, c, :],
                            start=(c == 0), stop=(c == 7)), reads=[Wg[p].b, X.b], writes=[pg.b])
                    for c in range(8):
                        cx.op("pe", lambda e, pu=pu, c=c, ft=ft, X=X, p=p: e.matmul(
                            pu[:, 0:CAP], lhsT=Wu[p][:, c, ft * 128:(ft + 1) * 128], rhs=X[:, c, :],
                            start=(c == 0), stop=(c == 7)), reads=[Wu[p].b, X.b], writes=[pu.b])
                    s = sgl[ft]
                    cx.op("act", lambda e, pg=pg, s=s: e.activation(out=s[:], in_=pg[:, 0:CAP], func=AF.Silu),
                          reads=[pg.b], full=[s.b])
                    cx.op("dve", lambda e, pu=pu, s=s, a_=a_, ft=ft: e.tensor_tensor(
                        out=a_[:, ft, :], in0=pu[:, 0:CAP], in1=s[:], op=ALU.mult),
                        reads=[pu.b, s.b], writes=[a_.b])
                for blk in range(NBLK):
                    eb = e_ * NBLK + blk
                    Y = Yt[eb % 3]
                    for half in range(2):
                        py = getps()
                        for ft in range(2):
                            cx.op("pe", lambda e, py=py, ft=ft, blk=blk, half=half, a_=a_, p=p: e.matmul(
                                py[:], lhsT=a_[:, ft, blk * 128:(blk + 1) * 128],
                                rhs=Wd[p][:, ft, half * 512:(half + 1) * 512], start=(ft == 0), stop=(ft == 1)),
                                reads=[a_.b, Wd[p].b], writes=[py.b])
                        if half == 0:
                            cx.op("dve", lambda e, py=py, Y=Y, eb=eb: e.tensor_scalar(
                                out=Y[:, 0:512], in0=py[:], scalar1=lst_sb[:, eb, 2:3], scalar2=None, op0=ALU.mult),
                                reads=[py.b, lst_sb.b], writes=[Y.b])
                        else:
                            cx.op("act", lambda e, py=py, Y=Y, eb=eb: e.activation(
                                out=Y[:, 512:1024], in_=py[:], func=AF.Copy, scale=lst_sb[:, eb, 2:3]),
                                reads=[py.b, lst_sb.b], writes=[Y.b])
                    cx.op("pool", lambda e, Y=Y, eb=eb: e.indirect_dma_start(
                        out=moe_scr, out_offset=bass.IndirectOffsetOnAxis(ap=dst_i[:, eb:eb + 1], axis=0),
                        in_=Y[:], in_offset=None), reads=[Y.b, dst_i.b], writes=[moeB], dma=True)
                if e_ + 1 < NEXP:
                    load_w_cast(e_ + 1)
            cx.barrier()
            alD.close()

            alE = Alloc(nc)
            gfin = alE.sb([128, D], F32, "gfin")
            cx.op("sp", lambda e: e.dma_start(out=gfin[:], in_=gfin_d.partition_broadcast(128)), full=[gfin.b], dma=True)
            NE_ = 4
            xa = [alE.sb([128, D], F32, f"xa{i}") for i in range(NE_)]
            m0 = [alE.sb([128, D], BF16, f"m0{i}") for i in range(NE_)]
            m1 = [alE.sb([128, D], BF16, f"m1{i}") for i in range(NE_)]
            ot = [alE.sb([128, D], F32, f"ot{i}") for i in range(NE_)]
            junk3 = alE.sb([128, D], BF16, "junk3")
            sse = [alE.sb([128, 1], F32, f"sse{i}") for i in range(NE_)]
            rte = [alE.sb([128, 1], F32, f"rte{i}") for i in range(NE_)]
            rse = [alE.sb([128, 1], F32, f"rse{i}") for i in range(NE_)]
            outB = Buf("out")

            def e_load(ti):
                p = ti % NE_
                rows = slice(ti * 128, (ti + 1) * 128)
                cx.op("sp", lambda e, p=p, rows=rows: e.dma_start(out=xa[p][:], in_=x2_scr[rows, :]),
                      reads=[x2B], full=[xa[p].b], dma=True)
                cx.op("sp", lambda e, p=p, rows=rows: e.dma_start(out=m0[p][:], in_=moe_scr[rows, :]),
                      reads=[moeB], full=[m0[p].b], dma=True)
                cx.op("sp", lambda e, p=p, ti=ti: e.dma_start(
                    out=m1[p][:], in_=moe_scr[ROWS + ti * 128:ROWS + (ti + 1) * 128, :]),
                    reads=[moeB], full=[m1[p].b], dma=True)

            for ti in range(min(NE_ - 1, NT)):
                e_load(ti)
            for ti in range(NT):
                p = ti % NE_
                rows = slice(ti * 128, (ti + 1) * 128)
                if ti + NE_ - 1 < NT:
                    e_load(ti + NE_ - 1)
                cx.op("pool", lambda e, p=p: e.tensor_tensor(out=xa[p][:], in0=xa[p][:], in1=m0[p][:], op=ALU.add),
                      reads=[m0[p].b], writes=[xa[p].b])
                cx.op("dve", lambda e, p=p: e.tensor_tensor(out=xa[p][:], in0=xa[p][:], in1=m1[p][:], op=ALU.add),
                      reads=[m1[p].b], writes=[xa[p].b])
                cx.op("act", lambda e, p=p: e.activation(out=junk3[:], in_=xa[p][:], func=AF.Square, accum_out=sse[p][:]),
                      reads=[xa[p].b], full=[junk3.b, sse[p].b])
                cx.op("act", lambda e, p=p: e.activation(out=rte[p][:], in_=sse[p][:], func=AF.Sqrt, scale=1.0 / D, bias=EPS),
                      reads=[sse[p].b], full=[rte[p].b])
                cx.op("dve", lambda e, p=p: e.reciprocal(out=rse[p][:], in_=rte[p][:]), reads=[rte[p].b], full=[rse[p].b])
                cx.op("dve", lambda e, p=p: e.scalar_tensor_tensor(out=ot[p][:], in0=xa[p][:], scalar=rse[p][:, 0:1],
                                                                   in1=gfin[:], op0=ALU.mult, op1=ALU.mult),
                      reads=[xa[p].b, rse[p].b, gfin.b], full=[ot[p].b])
                cx.op("sp", lambda e, p=p, rows=rows: e.dma_start(out=out_d[rows, :], in_=ot[p][:]),
                      reads=[ot[p].b], writes=[outB], dma=True)
            cx.barrier()
            alE.close()
        cx.barrier()
        cx.emit(block)
        print("waits", cx.nwait, "instrs", {e: cx.cnt[e] for e in cx.ENG}, "signals", {e: len(cx.waited[e]) for e in cx.ENG})
    return nc


def host_consts():
    c = {}
    c["ident_bf"] = np.eye(128, dtype=np.float32).astype(ml_dtypes.bfloat16)
    c["ident_f"] = np.eye(128, dtype=np.float32)
    psel = np.zeros((128, 8, 240), np.float32)
    for a in range(8):
        for i in range(16):
            psel[a * 16 + i, a, 7 * 16 + i] = 1.0
    c["psel"] = psel.astype(ml_dtypes.bfloat16)
    kk = np.arange(128) // 16
    c["cmask"] = (kk[None, :] >= kk[:, None]).astype(np.float32)
    c["tri"] = (np.arange(128)[:, None] < np.arange(128)[None, :]).astype(np.float32)
    c["ecap"] = np.ascontiguousarray(np.broadcast_to((np.arange(32) * NBLK).astype(np.float32)[None, :], (128, 32)))
    c["tokid"] = (np.arange(NT)[None, :] * 128 + np.arange(128)[:, None]).astype(np.float32)
    li = np.zeros((NEXP * CAP + 128, 4), np.float32)
    li[:, 0] = SEQ + ((np.arange(NEXP * CAP + 128) // (NEXP * NBLK)) % 128)
    li[:, 1] = li[:, 0]
    c["trashp"] = (NEXP * CAP + np.arange(128)).astype(np.float32).reshape(128, 1)
    c["lst_init"] = li
    return c


def relayout_pc(w):
    E, K, N = w.shape
    return np.ascontiguousarray(w.reshape(E, K // 128, 128, N).transpose(0, 2, 1, 3))


def pair_layout(a):
    rest = a.shape[2:]
    a = a.reshape((16, 2, 64) + rest)
    a = np.moveaxis(a, 0, 2)
    return np.ascontiguousarray(a.reshape((128, 16) + rest))


def make_inmap(inputs, b, consts=None):
    f = lambda a: np.ascontiguousarray(a, dtype=np.float32)
    m = {"x": f(inputs["x"][b]),
         "g_mix": f(inputs["g_mix"]),
         "w_in": f(inputs["w_in"][0])}
    m["lamre_l"] = pair_layout(f(inputs["ssm_lambda_re"][0]))
    m["lamim_l"] = pair_layout(f(inputs["ssm_lambda_im"][0]))
    m["logdt_l"] = pair_layout(np.broadcast_to(f(inputs["ssm_log_dt"][0])[:, None], (32, 64)))
    m["bre_l"] = pair_layout(f(inputs["ssm_b_re"][0]))
    m["bim_l"] = pair_layout(f(inputs["ssm_b_im"][0]))
    m["cre_l"] = pair_layout(f(inputs["ssm_c_re"][0]).transpose(0, 2, 1))
    m["cim_l"] = pair_layout(f(inputs["ssm_c_im"][0]).transpose(0, 2, 1))
    m["d_l"] = np.ascontiguousarray(np.tile(f(inputs["ssm_d"][0]).reshape(32, 16).T, (8, 1)))
    m["mem"] = f(inputs["mem"][b])
    for k_, n_ in (("g_mem", "g_mem"), ("g_ffn", "g_ffn")):
        m[n_] = f(inputs[k_])
    m["g_final"] = f(inputs["g_final"]).reshape(1, D)
    m["w_mem_kv"] = f(inputs["w_mem_kv"][0]); m["w_mem_out"] = f(inputs["w_mem_out"][0])
    m["w_conv_out"] = f(inputs["w_conv_out"][0]); m["w_ssm_glu"] = f(inputs["w_ssm_glu"][0])
    m["w_out"] = f(inputs["w_out"][0])
    m["w_router"] = np.ascontiguousarray(np.concatenate([f(inputs["w_router_group"][0]),
                                                         f(inputs["w_router_expert"][0])], axis=1))
    m["b_router"] = np.ascontiguousarray(np.concatenate([f(inputs["b_router_group"][0]),
                                                         f(inputs["b_router_expert"][0])])[None, :])
    m["cdw_l"] = np.ascontiguousarray(f(inputs["conv_dw"][0]).T.reshape(4, 128, 31).transpose(1, 0, 2))
    m["cb_l"] = np.ascontiguousarray(f(inputs["conv_dw_bias"][0]).reshape(4, 128).T)
    m["lng_l"] = np.ascontiguousarray(f(inputs["conv_ln_g"][0]).reshape(4, 128).T)
    m["lnb_l"] = np.ascontiguousarray(f(inputs["conv_ln_b"][0]).reshape(4, 128).T)
    if consts is not None and "w_exp_gate" in consts:
        for k_ in ("w_exp_gate", "w_exp_up", "w_exp_down"):
            m[k_] = consts[k_]
    else:
        m["w_exp_gate"] = relayout_pc(f(inputs["w_exp_gate"][0]))
        m["w_exp_up"] = relayout_pc(f(inputs["w_exp_up"][0]))
        m["w_exp_down"] = relayout_pc(f(inputs["w_exp_down"][0]))
    m.update(consts if consts is not None else host_consts())
    return m


def kernel(**inputs):
    nc = build()
    consts = host_consts()
    f32 = lambda a: np.ascontiguousarray(a, dtype=np.float32)
    for k_ in ("w_exp_gate", "w_exp_up", "w_exp_down"):
        consts[k_] = relayout_pc(f32(inputs[k_][0]))
    in_maps = [make_inmap(inputs, b, consts) for b in range(NCORES)]
    res = run_bass_kernel_spmd(nc, in_maps, core_ids=list(range(NCORES)))
    return np.stack([r["out"] for r in res.results], axis=0)
```

```python
import os
import numpy as np
import ml_dtypes
from contextlib import ExitStack
import concourse.bass as bass
import concourse.mybir as mybir
from concourse.bass_utils import run_bass_kernel_spmd

F32 = mybir.dt.float32
BF16 = mybir.dt.bfloat16
I32 = mybir.dt.int32
U32 = mybir.dt.uint32
AF = mybir.ActivationFunctionType
ALU = mybir.AluOpType
AX = mybir.AxisListType
GELU = AF.Gelu_apprx_tanh

D = 1024
SEQ = 4096
NCORES = 8
T = 512
NB = SEQ // T
NT = SEQ // 128
EPS = 1e-6
NEXP = 32
CAP = 384
NBLK = CAP // 128
ROWS = SEQ + 128


class Buf:
    __slots__ = ("name", "w", "r")

    def __init__(self, name):
        self.name = name
        self.w = {}
        self.r = {}


class Ctx:
    ENG = ("pe", "dve", "act", "pool", "sp")
    KROT = 4
    NDMA = 12

    def __init__(self, nc, es):
        self.nc = nc
        self.q = {e: [] for e in self.ENG}
        self.cnt = {e: 0 for e in self.ENG}
        self.seen = {e: {} for e in self.ENG}
        self.esem = {e: [es.enter_context(nc.semaphore(f"s_{e}{i}")) for i in range(self.KROT)]
                     for e in self.ENG}
        self.dsem = {e: [es.enter_context(nc.semaphore(f"d_{e}{i}")) for i in range(self.NDMA)]
                     for e in ("sp", "act", "pool")}
        self.dcnt = {e: [0] * self.NDMA for e in self.dsem}
        self.dnext = {e: 0 for e in self.dsem}
        self.nwait = 0
        self.waited = {e: set() for e in self.ENG}

    def _wait(self, eng, tok):
        key, val = tok
        if key[0] == 'e' and key[1] == eng and eng == "pe":
            return
        if self.seen[eng].get(key, -1) >= val:
            return
        self.seen[eng][key] = val
        if key[0] == 'e':
            self.waited[key[1]].add(val)
        self.q[eng].append(("w", key, val))
        self.nwait += 1

    def op(self, eng, fn, reads=(), writes=(), full=(), dma=False):
        toks = []
        for b in reads:
            toks.extend(b.w.items())
        for b in tuple(writes) + tuple(full):
            toks.extend(b.w.items())
            toks.extend(b.r.items())
        for t in toks:
            self._wait(eng, t)
        if dma:
            i = self.dnext[eng]
            self.dnext[eng] = (i + 1) % self.NDMA
            key = ('d', eng, i)
            if self.dcnt[eng][i] > 0:
                self._wait(eng, (key, self.dcnt[eng][i]))
            self.dcnt[eng][i] += 16
            val = self.dcnt[eng][i]
            self.q[eng].append(("d", fn, self.dsem[eng][i]))
        else:
            key = ('e', eng)
            val = self.cnt[eng]
            self.cnt[eng] += 1
            self.q[eng].append(("i", fn, val))
        for b in reads:
            b.r[key] = val
        for b in full:
            b.w = {key: val}
            b.r = {}
        for b in writes:
            b.w[key] = val
        return (key, val)

    def barrier(self, skip_pool_dma=False):
        toks = []
        for e in self.ENG:
            if skip_pool_dma and e == "pool":
                continue
            if self.cnt[e] > 0:
                toks.append((('e', e), self.cnt[e] - 1))
        for e in self.dsem:
            if skip_pool_dma and e == "pool":
                continue
            for i in range(self.NDMA):
                if self.dcnt[e][i] > 0:
                    toks.append((('d', e, i), self.dcnt[e][i]))
        for e in self.ENG:
            for t in toks:
                self._wait(e, t)

    def emit(self, block):
        nc = self.nc

        rank = {e: {v: i for i, v in enumerate(sorted(self.waited[e]))} for e in self.ENG}
        K_ = self.KROT

        def run(engname, engine):
            for item in self.q[engname]:
                if item[0] == "w":
                    key, val = item[1], item[2]
                    if key[0] == 'e':
                        r = rank[key[1]][val]
                        engine.wait_ge(self.esem[key[1]][r % K_], r // K_ + 1)
                    else:
                        engine.wait_ge(self.dsem[key[1]][key[2]], val)
                elif item[0] == "d":
                    item[1](engine).then_inc(item[2], 16)
                else:
                    ins = item[1](engine)
                    r = rank[engname].get(item[2])
                    if r is not None:
                        ins.then_inc(self.esem[engname][r % K_], 1)

        @block.tensor
        def _(e):
            run("pe", e)

        @block.vector
        def _(e):
            run("dve", e)

        @block.scalar
        def _(e):
            run("act", e)

        @block.gpsimd
        def _(e):
            run("pool", e)

        @block.sync
        def _(e):
            run("sp", e)


class TT:
    def __init__(self, t, name):
        self.t = t
        self.b = Buf(name)

    def __getitem__(self, k):
        return self.t[k]


class Alloc:
    cnt = [0]

    def __init__(self, nc, es=None):
        self.nc = nc
        self.es = es if es is not None else ExitStack()

    @property
    def n(self):
        return Alloc.cnt[0]

    @n.setter
    def n(self, v):
        Alloc.cnt[0] = v

    def close(self):
        self.es.close()

    def sb(self, shape, dt, name=None):
        self.n += 1
        name = name or f"sb{self.n}"
        t = self.es.enter_context(self.nc.sbuf_tensor(f"{name}_{self.n}", list(shape), dt))
        return TT(t, name)

    def ps(self, shape, dt, name=None):
        self.n += 1
        name = name or f"ps{self.n}"
        t = self.es.enter_context(self.nc.psum_tensor(f"{name}_{self.n}", list(shape), dt))
        return TT(t, name)


def build(stop_after="E", dbg=False):
    nc = bass.Bass("TRN2", target_bir_lowering=False)
    dram = {}

    def din(name, shape, dt=F32):
        dram[name] = nc.dram_tensor(name, list(shape), dt, kind="ExternalInput").ap()
        return dram[name]

    def dscr(name, shape, dt, kind="Internal"):
        dram[name] = nc.dram_tensor(name, list(shape), dt, kind=kind).ap()
        return dram[name]

    x_d = din("x", [SEQ, D])
    gmix_d = din("g_mix", [1, D])
    w_in_d = din("w_in", [D, 5120])
    ident_bf_d = din("ident_bf", [128, 128], BF16)
    ident_f_d = din("ident_f", [128, 128], F32)
    lamre_d = din("lamre_l", [128, 16])
    lamim_d = din("lamim_l", [128, 16])
    logdt_d = din("logdt_l", [128, 16])
    bre_d = din("bre_l", [128, 16, 16])
    bim_d = din("bim_l", [128, 16, 16])
    cre_d = din("cre_l", [128, 16, 16])
    cim_d = din("cim_l", [128, 16, 16])
    dl_d = din("d_l", [128, 32])
    psel_d = din("psel", [128, 8, 240], BF16)
    cmask_d = din("cmask", [128, 128])
    mem_d = din("mem", [256, D])
    gmem_d = din("g_mem", [1, D])
    gffn_d = din("g_ffn", [1, D])
    gfin_d = din("g_final", [1, D])
    wkv_d = din("w_mem_kv", [D, 1024])
    wmo_d = din("w_mem_out", [512, D])
    wco_d = din("w_conv_out", [512, D])
    wgl_d = din("w_ssm_glu", [512, 2048])
    wo_d = din("w_out", [D, D])
    wr_d = din("w_router", [D, 36])
    rbias_d = din("b_router", [1, 36])
    cdw_d = din("cdw_l", [128, 4, 31])
    cb_d = din("cb_l", [128, 4])
    lng_d = din("lng_l", [128, 4])
    lnb_d = din("lnb_l", [128, 4])
    tri_d = din("tri", [128, 128])
    ecap_d = din("ecap", [128, 32])
    tokid_d = din("tokid", [128, NT])
    lst_init_d = din("lst_init", [NEXP * CAP + 128, 4])
    trashp_d = din("trashp", [128, 1])
    weg_d = din("w_exp_gate", [NEXP, 128, 8, 256])
    weu_d = din("w_exp_up", [NEXP, 128, 8, 256])
    wed_d = din("w_exp_down", [NEXP, 128, 2, D])
    dk = "ExternalOutput" if dbg else "Internal"
    wbf_scr = dscr("wbf_scr", [NEXP, 3, 128, 2048], BF16)
    lst_d = dscr("lst", [NEXP * CAP + 128, 4], F32, kind=dk)
    h2_scr = dscr("h2_scr", [ROWS, D], BF16, kind=dk)
    moe_scr = dscr("moe_scr", [2 * ROWS, D], BF16, kind=dk)
    x2_scr = dscr("x2_scr", [SEQ, D], F32, kind=dk)
    ys_scr = dscr("ys_scr", [4, 128, SEQ], BF16, kind="ExternalOutput" if dbg else "Internal")
    out_d = dscr("out", [SEQ, D], F32, kind="ExternalOutput")
    hT_scr = dscr("hT_scr", [8, 128, SEQ], BF16, kind="ExternalOutput" if dbg else "Internal")
    u_dbg = dscr("u_dbg", [4, 128, SEQ], BF16, kind="ExternalOutput") if dbg else None

    with ExitStack() as es:
        cx = Ctx(nc, es)
        al = Alloc(nc, es)
        block = es.enter_context(nc.Block())

        ident_bf = al.sb([128, 128], BF16, "ident_bf")
        cx.op("sp", lambda e: e.dma_start(out=ident_bf[:], in_=ident_bf_d), full=[ident_bf.b], dma=True)
        ident_f = al.sb([128, 128], F32, "ident_f")
        cx.op("sp", lambda e: e.dma_start(out=ident_f[:], in_=ident_f_d), full=[ident_f.b], dma=True)

        psum = [al.ps([128, 512], F32, f"bank{i}") for i in range(6)]
        psb = [al.ps([128, 1024], BF16, f"bankb{i}") for i in range(2)]
        pctr = [0]

        def getps():
            p = psum[pctr[0] % len(psum)]
            pctr[0] += 1
            return p

        alAB = Alloc(nc)
        u_all = alAB.sb([128, 4, SEQ], BF16, "u_all")
        M_all = alAB.sb([128, 32, 128], BF16, "M_all")
        W2r = alAB.sb([128, 16, 2, 128], BF16, "W2r"); W2i = alAB.sb([128, 16, 2, 128], BF16, "W2i")
        C1r = alAB.sb([128, 16, 128], BF16, "C1r"); nC1i = alAB.sb([128, 16, 128], BF16, "nC1i")
        KAr = alAB.sb([128, 9, 16], F32, "KAr"); KAi = alAB.sb([128, 9, 16], F32, "KAi")
        KnAi = alAB.sb([128, 9, 16], F32, "KnAi")
        psel = alAB.sb([128, 8, 240], BF16, "psel")
        zt = alAB.sb([128, 1024], F32, "zt")
        NPB = 3
        pst = [alAB.sb([128, 2048], F32, f"pst{i}") for i in range(NPB)]
        pbf = [alAB.sb([128, 2048], BF16, f"pbf{i}") for i in range(NPB)]
        wbfB = Buf("wbf")
        pc_next = [0]

        def precast(n, mode):
            for _ in range(n):
                ci = pc_next[0]
                if ci >= NEXP * 3:
                    return
                pc_next[0] += 1
                e_, m_ = ci // 3, ci % 3
                srcw = (weg_d, weu_d, wed_d)[m_][e_].rearrange("p c n -> p (c n)")
                s_ = pst[ci % NPB]; b_ = pbf[ci % NPB]
                dst = wbf_scr[e_, m_]
                if mode == "pool":
                    cx.op("pool", lambda e, s_=s_, srcw=srcw: e.dma_start(out=s_[:], in_=srcw), full=[s_.b], dma=True)
                    cx.op("pool", lambda e, s_=s_, b_=b_: e.tensor_copy(out=b_[:], in_=s_[:]), reads=[s_.b], full=[b_.b])
                    cx.op("pool", lambda e, b_=b_, dst=dst: e.dma_start(out=dst, in_=b_[:]), reads=[b_.b], writes=[wbfB],
                          dma=True)
                else:
                    cx.op("sp", lambda e, s_=s_, srcw=srcw: e.dma_start(out=s_[:], in_=srcw), full=[s_.b], dma=True)
                    cx.op("act", lambda e, s_=s_, b_=b_: e.copy(out=b_[:], in_=s_[:]), reads=[s_.b], full=[b_.b])
                    cx.op("act", lambda e, b_=b_, dst=dst: e.dma_start(out=dst, in_=b_[:]), reads=[b_.b], writes=[wbfB],
                          dma=True)
        cx.op("sp", lambda e: e.dma_start(out=psel[:], in_=psel_d), full=[psel.b], dma=True)
        al_outer = al
        al = Alloc(nc)
        gmix = al.sb([128, D], F32, "gmix")
        cx.op("sp", lambda e: e.dma_start(out=gmix[:], in_=gmix_d.partition_broadcast(128)),
              full=[gmix.b], dma=True)

        stg = [al.sb([128, 8, 256], F32, f"stg{i}") for i in range(2)]
        sctr = [0]

        def load_cast(dst, dst_col0, src_d, c0, c1, kch):
            for cc in range(c0, c1, 256):
                w = min(256, c1 - cc)
                s = stg[sctr[0] % 2]
                sctr[0] += 1
                src = src_d[:, cc:cc + w].rearrange("(c p) n -> p c n", p=128)
                cx.op("sp", lambda e, s=s, src=src, w=w: e.dma_start(out=s[:, 0:kch, 0:w], in_=src),
                      full=[s.b], dma=True)
                o = dst_col0 + (cc - c0)
                cx.op("pool", lambda e, s=s, o=o, w=w: e.tensor_copy(out=dst[:, 0:kch, o:o + w],
                                                                     in_=s[:, 0:kch, 0:w]),
                      reads=[s.b], writes=[dst.b])

        w_ssm_in = al.sb([128, 8, 512], BF16, "w_ssm_in")
        load_cast(w_ssm_in, 0, w_in_d, 1024, 1536, 8)
        NA_ = 4
        xt = [al.sb([128, D], F32, f"xt{i}") for i in range(NA_)]
        junk = al.sb([128, D], BF16, "junk")
        ss = [al.sb([128, 1], F32, f"ss{i}") for i in range(NA_)]
        rt = [al.sb([128, 1], F32, f"rt{i}") for i in range(NA_)]
        rstd = [al.sb([128, 1], F32, f"rstd{i}") for i in range(NA_)]
        hbf = [al.sb([128, D], BF16, f"hbf{i}") for i in range(NA_)]
        hTb = [al.sb([128, 8, T], BF16, f"hTb{i}") for i in range(2)]
        cx.op("pool", lambda e: e.memset(zt[:], 0.0), full=[zt.b])
        lstB = Buf("lst"); h2B = Buf("h2scr"); moeB = Buf("moescr"); x2B = Buf("x2scr")
        cx.op("pool", lambda e: e.dma_start(out=lst_d, in_=lst_init_d), full=[lstB], dma=True)
        cx.op("pool", lambda e: e.dma_start(out=h2_scr[SEQ:ROWS, :], in_=zt[:, 0:512].bitcast(BF16)),
              reads=[zt.b], writes=[h2B], dma=True)
        moe_flat = moe_scr.rearrange("(n p) d -> n p d", p=128)
        for n in range(0, 2 * ROWS // 128):
            cx.op("pool", lambda e, n=n: e.dma_start(out=moe_flat[n], in_=zt[:, 0:512].bitcast(BF16)),
                  reads=[zt.b], writes=[moeB], dma=True)

        def a_front(i):
            p = i % NA_
            cx.op("sp", lambda e, p=p, i=i: e.dma_start(out=xt[p][:], in_=x_d[i * 128:(i + 1) * 128, :]),
                  full=[xt[p].b], dma=True)
            cx.op("act", lambda e, p=p: e.activation(out=junk[:], in_=xt[p][:], func=AF.Square,
                                                     accum_out=ss[p][:]),
                  reads=[xt[p].b], writes=[junk.b], full=[ss[p].b])
            cx.op("act", lambda e, p=p: e.activation(out=rt[p][:], in_=ss[p][:], func=AF.Sqrt,
                                                     scale=1.0 / D, bias=EPS),
                  reads=[ss[p].b], full=[rt[p].b])
            cx.op("dve", lambda e, p=p: e.reciprocal(out=rstd[p][:], in_=rt[p][:]),
                  reads=[rt[p].b], full=[rstd[p].b])
            cx.op("dve", lambda e, p=p: e.scalar_tensor_tensor(out=hbf[p][:], in0=xt[p][:],
                                                               scalar=rstd[p][:, 0:1], in1=gmix[:],
                                                               op0=ALU.mult, op1=ALU.mult),
                  reads=[xt[p].b, rstd[p].b, gmix.b], full=[hbf[p].b])

        def a_back(i):
            p = i % NA_
            blk = i // 4
            hb = hTb[blk % 2]
            pb = psb[i % 2]
            for c in range(8):
                cx.op("pe", lambda e, pb=pb, p=p, c=c: e.transpose(out=pb[:, c * 128:(c + 1) * 128],
                                                                   in_=hbf[p][:, c * 128:(c + 1) * 128],
                                                                   identity=ident_bf[:]),
                      reads=[hbf[p].b, ident_bf.b], writes=[pb.b])
            tt = i % 4
            cx.op("act", lambda e, pb=pb, hb=hb, tt=tt: e.copy(
                out=hb[:, :, tt * 128:(tt + 1) * 128],
                in_=pb[:].rearrange("p (c t) -> p c t", c=8)),
                reads=[pb.b], writes=[hb.b])
            if tt == 3:
                for f in range(4):
                    ps = getps()
                    for c in range(8):
                        cx.op("pe", lambda e, ps=ps, hb=hb, f=f, c=c: e.matmul(
                            ps[:], lhsT=w_ssm_in[:, c, f * 128:(f + 1) * 128], rhs=hb[:, c, :],
                            start=(c == 0), stop=(c == 7)),
                            reads=[w_ssm_in.b, hb.b], writes=[ps.b])
                    cx.op("dve", lambda e, ps=ps, f=f, blk=blk: e.tensor_copy(
                        out=u_all[:, f, blk * T:(blk + 1) * T], in_=ps[:]),
                        reads=[ps.b], writes=[u_all.b])
                cx.op("act", lambda e, hb=hb, blk=blk: e.dma_start(
                    out=hT_scr[:, :, blk * T:(blk + 1) * T].rearrange("c p t -> p c t"), in_=hb[:]),
                    reads=[hb.b], dma=True)

        a_front(0); a_front(1)
        for i in range(NT):
            if i + 2 < NT:
                a_front(i + 2)
            a_back(i)
            if i % 3 == 2:
                precast(1, "pool")

        if dbg:
            cx.op("sp", lambda e: e.dma_start(out=u_dbg.rearrange("f p t -> p f t"), in_=u_all[:]),
                  reads=[u_all.b], dma=True)


        cx.barrier(skip_pool_dma=True)
        al.close()
        precast(8, "pool")
        al = Alloc(nc)
        TWO_PI = 2.0 * np.pi
        cmask = al.sb([128, 128], F32, "cmask")
        cx.op("sp", lambda e: e.dma_start(out=cmask[:], in_=cmask_d), full=[cmask.b], dma=True)
        dl = al.sb([128, 32], F32, "dl")
        cx.op("sp", lambda e: e.dma_start(out=dl[:], in_=dl_d), full=[dl.b], dma=True)
        SU = Buf("ssm_setup")

        def sload(shape, src, name):
            t = al.sb(shape, F32, name)
            cx.op("sp", lambda e: e.dma_start(out=t[:], in_=src), full=[t.b], dma=True)
            return t

        lamre = sload([128, 16], lamre_d, "lamre")
        lamim = sload([128, 16], lamim_d, "lamim")
        logdt = sload([128, 16], logdt_d, "logdt")
        Bre = sload([128, 16, 16], bre_d, "Bre")
        Bim = sload([128, 16, 16], bim_d, "Bim")
        Cre = sload([128, 16, 16], cre_d, "Cre")
        Cim = sload([128, 16, 16], cim_d, "Cim")
        ins_b = [lamre.b, lamim.b, logdt.b, Bre.b, Bim.b, Cre.b, Cim.b]

        def S(shape, name):
            return al.sb(shape, F32, name)

        def dv(fn):
            cx.op("dve", fn, reads=ins_b, writes=[SU])

        def ac(fn):
            cx.op("act", fn, reads=ins_b, writes=[SU])

        def tt_(out, a, b, op):
            dv(lambda e: e.tensor_tensor(out=out, in0=a, in1=b, op=op))

        sh16 = [128, 16]
        dt_ = S(sh16, "dt"); lrd = S(sh16, "lrd"); th = S(sh16, "th")
        ac(lambda e: e.activation(out=dt_[:], in_=logdt[:], func=AF.Exp))
        tt_(lrd[:], lamre[:], dt_[:], ALU.mult)
        tt_(th[:], lamim[:], dt_[:], ALU.mult)
        mag = S(sh16, "mag"); imag2 = S(sh16, "imag2")
        ac(lambda e: e.activation(out=mag[:], in_=lrd[:], func=AF.Exp))
        ac(lambda e: e.activation(out=imag2[:], in_=lrd[:], func=AF.Exp, scale=-2.0))
        kq_i = al.sb(sh16, I32, "kq_i"); kq = S(sh16, "kq"); red = S(sh16, "red"); msk = S(sh16, "msk")
        sinv = S(sh16, "sinv"); cosv = S(sh16, "cosv"); tmpa = S(sh16, "tmpa")

        def sin_of(outt, shift):
            dv(lambda e: e.tensor_scalar(out=tmpa[:], in0=th[:], scalar1=float(shift), scalar2=None,
                                         op0=ALU.add))
            dv(lambda e: e.tensor_scalar(out=kq[:], in0=tmpa[:], scalar1=float(1.0 / TWO_PI),
                                         scalar2=None, op0=ALU.mult))
            dv(lambda e: e.tensor_copy(out=kq_i[:], in_=kq[:]))
            dv(lambda e: e.tensor_copy(out=kq[:], in_=kq_i[:]))
            dv(lambda e: e.scalar_tensor_tensor(out=red[:], in0=kq[:], scalar=float(-TWO_PI),
                                                in1=tmpa[:], op0=ALU.mult, op1=ALU.add))
            dv(lambda e: e.tensor_single_scalar(out=msk[:], in_=red[:], scalar=float(np.pi), op=ALU.is_gt))
            dv(lambda e: e.scalar_tensor_tensor(out=red[:], in0=msk[:], scalar=float(-TWO_PI),
                                                in1=red[:], op0=ALU.mult, op1=ALU.add))
            dv(lambda e: e.tensor_single_scalar(out=msk[:], in_=red[:], scalar=float(-np.pi), op=ALU.is_lt))
            dv(lambda e: e.scalar_tensor_tensor(out=red[:], in0=msk[:], scalar=float(TWO_PI),
                                                in1=red[:], op0=ALU.mult, op1=ALU.add))
            ac(lambda e: e.activation(out=outt[:], in_=red[:], func=AF.Sin))

        sin_of(sinv, 0.0)
        sin_of(cosv, np.pi / 2)
        PWr = S([128, 9, 16], "PWr"); PWi = S([128, 9, 16], "PWi")
        IPr = S([128, 8, 16], "IPr"); IPi = S([128, 8, 16], "IPi")
        t1 = S([128, 16, 8, 16], "t1"); t2 = S([128, 16, 8, 16], "t2")

        def cmul(outr, outi, ar, ai, br, bi, shp, neg_i=False):
            a1 = t1[:].rearrange("p a b c -> p (a b c)")[:, 0:int(np.prod(shp[1:]))]
            a2 = t2[:].rearrange("p a b c -> p (a b c)")[:, 0:int(np.prod(shp[1:]))]
            if len(shp) == 3:
                a1 = a1.rearrange("p (a b) -> p a b", a=shp[1])
                a2 = a2.rearrange("p (a b) -> p a b", a=shp[1])
            if len(shp) == 4:
                a1 = t1[:, :, 0:shp[2], :]
                a2 = t2[:, :, 0:shp[2], :]
            tt_(a1, ar, br, ALU.mult)
            tt_(a2, ai, bi, ALU.mult)
            tt_(outr, a1, a2, ALU.subtract)
            tt_(a1, ar, bi, ALU.mult)
            tt_(a2, ai, br, ALU.mult)
            if neg_i:
                dv(lambda e: e.scalar_tensor_tensor(out=outi, in0=a1, scalar=-1.0, in1=a2,
                                                    op0=ALU.mult, op1=ALU.subtract))
            else:
                tt_(outi, a1, a2, ALU.add)

        dv(lambda e: e.memset(PWr[:, 0, :], 1.0))
        dv(lambda e: e.memset(PWi[:, 0, :], 0.0))
        dv(lambda e: e.memset(IPr[:, 0, :], 1.0))
        dv(lambda e: e.memset(IPi[:, 0, :], 0.0))
        tt_(PWr[:, 1, :], mag[:], cosv[:], ALU.mult)
        tt_(PWi[:, 1, :], mag[:], sinv[:], ALU.mult)
        tt_(IPr[:, 1, :], PWr[:, 1, :], imag2[:], ALU.mult)
        dv(lambda e: e.scalar_tensor_tensor(out=IPi[:, 1, :], in0=PWi[:, 1, :], scalar=-1.0, in1=imag2[:],
                                            op0=ALU.mult, op1=ALU.mult))
        for n in range(2, 9):
            cmul(PWr[:, n, :], PWi[:, n, :], PWr[:, n - 1, :], PWi[:, n - 1, :], PWr[:, 1, :], PWi[:, 1, :], sh16)
        for n in range(2, 8):
            cmul(IPr[:, n, :], IPi[:, n, :], IPr[:, n - 1, :], IPi[:, n - 1, :], IPr[:, 1, :], IPi[:, 1, :], sh16)
        dv(lambda e: e.tensor_copy(out=KAr[:, 0, :], in_=PWr[:, 8, :]))
        dv(lambda e: e.tensor_copy(out=KAi[:, 0, :], in_=PWi[:, 8, :]))
        for d_ in range(1, 9):
            cmul(KAr[:, d_, :], KAi[:, d_, :], KAr[:, d_ - 1, :], KAi[:, d_ - 1, :],
                 KAr[:, d_ - 1, :], KAi[:, d_ - 1, :], sh16)
        dv(lambda e: e.tensor_scalar(out=KnAi[:], in0=KAi[:], scalar1=-1.0, scalar2=None, op0=ALU.mult))
        am1 = S(sh16, "am1"); l2 = S(sh16, "l2"); il2 = S(sh16, "il2"); kr = S(sh16, "kr"); ki = S(sh16, "ki")
        dv(lambda e: e.tensor_scalar(out=am1[:], in0=PWr[:, 1, :], scalar1=-1.0, scalar2=None, op0=ALU.add))
        tt_(l2[:], lamre[:], lamre[:], ALU.mult)
        tt_(tmpa[:], lamim[:], lamim[:], ALU.mult)
        tt_(l2[:], l2[:], tmpa[:], ALU.add)
        dv(lambda e: e.reciprocal(out=il2[:], in_=l2[:]))
        tt_(kr[:], am1[:], lamre[:], ALU.mult)
        tt_(tmpa[:], PWi[:, 1, :], lamim[:], ALU.mult)
        tt_(kr[:], kr[:], tmpa[:], ALU.add)
        tt_(kr[:], kr[:], il2[:], ALU.mult)
        tt_(ki[:], PWi[:, 1, :], lamre[:], ALU.mult)
        tt_(tmpa[:], am1[:], lamim[:], ALU.mult)
        tt_(ki[:], ki[:], tmpa[:], ALU.subtract)
        tt_(ki[:], ki[:], il2[:], ALU.mult)
        sh3 = [128, 16, 16]

        def bc(a):
            return a.unsqueeze(2).to_broadcast(sh3)

        Bbr = S(sh3, "Bbr"); Bbi = S(sh3, "Bbi")
        cmul(Bbr[:], Bbi[:], bc(kr[:]), bc(ki[:]), Bre[:], Bim[:], sh3)
        Bhr = S([128, 16, 8, 16], "Bhr"); nBhi = S([128, 16, 8, 16], "nBhi"); Bhi = S([128, 16, 8, 16], "Bhi")
        Btr = S([128, 16, 8, 16], "Btr"); Bti = S([128, 16, 8, 16], "Bti")
        Chr = S([128, 16, 9, 16], "Chr"); Chi = S([128, 16, 9, 16], "Chi"); nChi = S([128, 16, 9, 16], "nChi")
        sh4 = [128, 16, 8, 16]

        def bk(a):
            return a.rearrange("p k r -> p r k").unsqueeze(3).to_broadcast(sh4)

        def bmid(a):
            return a.unsqueeze(2).to_broadcast(sh4)

        def b2(a):
            return a.unsqueeze(2).unsqueeze(3).to_broadcast(sh4)

        cmul(Bhr[:], Bhi[:], bk(IPr[:]), bk(IPi[:]), bmid(Bbr[:]), bmid(Bbi[:]), sh4)
        cmul(Btr[:], Bti[:], b2(PWr[:, 7, :]), b2(PWi[:, 7, :]), Bhr[:], Bhi[:], sh4)
        dv(lambda e: e.tensor_scalar(out=nBhi[:], in0=Bhi[:], scalar1=-1.0, scalar2=None, op0=ALU.mult))
        cmul(Chr[:, :, 0:8, :], Chi[:, :, 0:8, :], bk(PWr[:, 0:8, :]), bk(PWi[:, 0:8, :]), bmid(Cre[:]), bmid(Cim[:]), sh4)
        cmul(Chr[:, :, 8, :], Chi[:, :, 8, :], bc(PWr[:, 8, :]), bc(PWi[:, 8, :]), Cre[:], Cim[:], sh3)
        dv(lambda e: e.tensor_scalar(out=nChi[:], in0=Chi[:], scalar1=-1.0, scalar2=None, op0=ALU.mult))
        dv(lambda e: e.tensor_copy(out=C1r[:].rearrange("p r (j c) -> p r j c", j=8), in_=Chr[:, :, 1:9, :]))
        dv(lambda e: e.tensor_copy(out=nC1i[:].rearrange("p r (j c) -> p r j c", j=8), in_=nChi[:, :, 1:9, :]))
        mtmp = S([128, 128], "mtmp")
        cx.op("dve", lambda e: e.memset(W2r[:], 0.0), reads=ins_b, writes=[SU])
        cx.op("dve", lambda e: e.memset(W2i[:], 0.0), reads=ins_b, writes=[SU])
        for r in range(16):
            for two in range(2):
                g = 2 * r + two
                rng = slice(two * 64, (two + 1) * 64)
                ps = getps()
                cx.op("pe", lambda e, ps=ps, r=r, rng=rng: e.matmul(
                    ps[:, 0:128], lhsT=Bhr[rng, r, :, :].rearrange("p k c -> p (k c)"),
                    rhs=Chr[rng, r, 0:8, :].rearrange("p j c -> p (j c)"), start=True, stop=False),
                    reads=[SU], writes=[ps.b])
                cx.op("pe", lambda e, ps=ps, r=r, rng=rng: e.matmul(
                    ps[:, 0:128], lhsT=nBhi[rng, r, :, :].rearrange("p k c -> p (k c)"),
                    rhs=Chi[rng, r, 0:8, :].rearrange("p j c -> p (j c)"), start=False, stop=True),
                    reads=[SU], writes=[ps.b])
                cx.op("dve", lambda e, ps=ps: e.tensor_tensor(out=mtmp[:], in0=ps[:, 0:128], in1=cmask[:],
                                                              op=ALU.mult),
                      reads=[ps.b, cmask.b], writes=[SU])
                cx.op("dve", lambda e, g=g: e.scalar_tensor_tensor(
                    out=M_all[:, g, :], in0=ident_f[:], scalar=dl[:, g:g + 1], in1=mtmp[:],
                    op0=ALU.mult, op1=ALU.add),
                    reads=[ident_f.b, dl.b], writes=[SU, M_all.b])
            for (Bt, W2) in ((Btr, W2r), (Bti, W2i)):
                ps = getps()
                cx.op("pe", lambda e, ps=ps, r=r, Bt=Bt: e.transpose(
                    out=ps[:, 0:128], in_=Bt[:, r, :, :].rearrange("p k c -> p (k c)"), identity=ident_f[:]),
                    reads=[SU, ident_f.b], writes=[ps.b])
                cx.op("dve", lambda e, ps=ps, r=r, W2=W2: e.tensor_copy(out=W2[:, r, 0, 0:64], in_=ps[:, 0:64]),
                      reads=[ps.b], writes=[SU, W2.b])
                cx.op("dve", lambda e, ps=ps, r=r, W2=W2: e.tensor_copy(out=W2[:, r, 1, 64:128], in_=ps[:, 64:128]),
                      reads=[ps.b], writes=[SU, W2.b])

        cx.barrier(skip_pool_dma=True)
        al.close()
        al = Alloc(nc)
        NCH = SEQ // 8
        Vg = [al.sb([128, NCH], BF16, f"Vg{i}") for i in range(4)]
        Sre = [[al.sb([128, NCH], F32, f"Sre{s}{i}") for i in range(2)] for s in range(2)]
        Sim = [[al.sb([128, NCH], F32, f"Sim{s}{i}") for i in range(2)] for s in range(2)]
        Sbr = [al.sb([128, NCH], BF16, f"Sbr{s}") for s in range(2)]
        Sbi = [al.sb([128, NCH], BF16, f"Sbi{s}") for s in range(2)]
        Gg = [al.sb([128, NCH], BF16, f"Gg{i}") for i in range(16)]
        ysf = [al.sb([128, SEQ], BF16, f"ysf{i}") for i in range(2)]
        for s in range(2):
            cx.op("pool", lambda e, s=s: e.memset(Sbr[s][:, 0:1], 0.0), writes=[Sbr[s].b])
            cx.op("pool", lambda e, s=s: e.memset(Sbi[s][:, 0:1], 0.0), writes=[Sbi[s].b])

        def b_front(r):
            f = r // 4
            st = r % 2
            vg = [Vg[(2 * r) % 4], Vg[(2 * r + 1) % 4]]
            for two in range(2):
                g = 2 * r + two
                gl = g % 8
                ps = getps()
                for k in range(8):
                    cx.op("pe", lambda e, ps=ps, gl=gl, k=k, f=f: e.matmul(
                        ps[:], lhsT=psel[:, gl, (7 - k) * 16:(7 - k) * 16 + 128],
                        rhs=u_all[:, f, k:SEQ:8], start=(k == 0), stop=(k == 7)),
                        reads=[psel.b, u_all.b], writes=[ps.b])
                cx.op("act", lambda e, ps=ps, v=vg[two]: e.copy(out=v[:], in_=ps[:]),
                      reads=[ps.b], full=[vg[two].b])
            psr = getps(); psi = getps()
            for (pp, W2) in ((psr, W2r), (psi, W2i)):
                for two in range(2):
                    cx.op("pe", lambda e, pp=pp, W2=W2, two=two, r=r, v=vg[two]: e.matmul(
                        pp[:], lhsT=W2[:, r, two, :], rhs=v[:], start=(two == 0), stop=(two == 1)),
                        reads=[W2.b, vg[two].b], writes=[pp.b])
            cx.op("act", lambda e, psr=psr, st=st: e.copy(out=Sre[st][0][:], in_=psr[:]),
                  reads=[psr.b], full=[Sre[st][0].b])
            cx.op("act", lambda e, psi=psi, st=st: e.copy(out=Sim[st][0][:], in_=psi[:]),
                  reads=[psi.b], full=[Sim[st][0].b])

        def b_mid(r):
            st = r % 2
            cur = 0
            for d_ in range(9):
                sh = 1 << d_
                s_r, s_i, d_r, d_i = Sre[st][cur], Sim[st][cur], Sre[st][1 - cur], Sim[st][1 - cur]
                n = NCH - sh
                cx.op("dve", lambda e, s_r=s_r, d_r=d_r, sh=sh, n=n, d_=d_, r=r: e.scalar_tensor_tensor(
                    out=d_r[:, sh:NCH], in0=s_r[:, 0:n], scalar=KAr[:, d_, r:r + 1], in1=s_r[:, sh:NCH],
                    op0=ALU.mult, op1=ALU.add), reads=[s_r.b, SU], writes=[d_r.b])
                cx.op("dve", lambda e, s_i=s_i, d_r=d_r, sh=sh, n=n, d_=d_, r=r: e.scalar_tensor_tensor(
                    out=d_r[:, sh:NCH], in0=s_i[:, 0:n], scalar=KnAi[:, d_, r:r + 1], in1=d_r[:, sh:NCH],
                    op0=ALU.mult, op1=ALU.add), reads=[s_i.b, SU], writes=[d_r.b])
                cx.op("dve", lambda e, s_i=s_i, d_i=d_i, sh=sh, n=n, d_=d_, r=r: e.scalar_tensor_tensor(
                    out=d_i[:, sh:NCH], in0=s_i[:, 0:n], scalar=KAr[:, d_, r:r + 1], in1=s_i[:, sh:NCH],
                    op0=ALU.mult, op1=ALU.add), reads=[s_i.b, SU], writes=[d_i.b])
                cx.op("dve", lambda e, s_r=s_r, d_i=d_i, sh=sh, n=n, d_=d_, r=r: e.scalar_tensor_tensor(
                    out=d_i[:, sh:NCH], in0=s_r[:, 0:n], scalar=KAi[:, d_, r:r + 1], in1=d_i[:, sh:NCH],
                    op0=ALU.mult, op1=ALU.add), reads=[s_r.b, SU], writes=[d_i.b])
                cx.op("pool", lambda e, s_r=s_r, d_r=d_r, sh=sh: e.tensor_copy(out=d_r[:, 0:sh], in_=s_r[:, 0:sh]),
                      reads=[s_r.b], writes=[d_r.b])
                cx.op("pool", lambda e, s_i=s_i, d_i=d_i, sh=sh: e.tensor_copy(out=d_i[:, 0:sh], in_=s_i[:, 0:sh]),
                      reads=[s_i.b], writes=[d_i.b])
                cur = 1 - cur
            fr, fi = Sre[st][cur], Sim[st][cur]
            cx.op("pool", lambda e, fr=fr, st=st: e.tensor_copy(out=Sbr[st][:, 1:NCH], in_=fr[:, 0:NCH - 1]),
                  reads=[fr.b], writes=[Sbr[st].b])
            cx.op("pool", lambda e, fi=fi, st=st: e.tensor_copy(out=Sbi[st][:, 1:NCH], in_=fi[:, 0:NCH - 1]),
                  reads=[fi.b], writes=[Sbi[st].b])

        def b_back(r):
            f = r // 4
            st = r % 2
            vg = [Vg[(2 * r) % 4], Vg[(2 * r + 1) % 4]]
            for two in range(2):
                g = 2 * r + two
                rng = slice(two * 64, (two + 1) * 64)
                ps = getps()
                cx.op("pe", lambda e, ps=ps, g=g, v=vg[two]: e.matmul(
                    ps[:], lhsT=M_all[:, g, :], rhs=v[:], start=True, stop=False),
                    reads=[M_all.b, vg[two].b], writes=[ps.b])
                cx.op("pe", lambda e, ps=ps, r=r, rng=rng, st=st: e.matmul(
                    ps[:], lhsT=C1r[rng, r, :], rhs=Sbr[st][rng, :], start=False, stop=False),
                    reads=[SU, Sbr[st].b], writes=[ps.b])
                cx.op("pe", lambda e, ps=ps, r=r, rng=rng, st=st: e.matmul(
                    ps[:], lhsT=nC1i[rng, r, :], rhs=Sbi[st][rng, :], start=False, stop=True),
                    reads=[SU, Sbi[st].b], writes=[ps.b])
                gg = Gg[g % 16]
                cx.op("act", lambda e, ps=ps, gg=gg: e.activation(out=gg[:], in_=ps[:], func=GELU),
                      reads=[ps.b], full=[gg.b])
            if r % 4 == 3:
                yb = ysf[f % 2]
                for j in range(8):
                    ps = getps()
                    for gl in range(8):
                        gg = Gg[(8 * f + gl) % 16]
                        cx.op("pe", lambda e, ps=ps, j=j, gl=gl, gg=gg: e.matmul(
                            ps[:], lhsT=psel[:, j, (7 - gl) * 16:(7 - gl) * 16 + 128], rhs=gg[:],
                            start=(gl == 0), stop=(gl == 7)),
                            reads=[psel.b, gg.b], writes=[ps.b])
                    cx.op("act", lambda e, ps=ps, yb=yb, j=j: e.copy(out=yb[:, j:SEQ:8], in_=ps[:]),
                          reads=[ps.b], writes=[yb.b])
                cx.op("sp", lambda e, yb=yb, f=f: e.dma_start(out=ys_scr[f], in_=yb[:]),
                      reads=[yb.b], dma=True)

        b_front(0)
        for r in range(16):
            precast(2, "act")
            b_mid(r)
            precast(2, "act")
            if r + 1 < 16:
                b_front(r + 1)
            precast(1, "act")
            b_back(r)
        precast(NEXP * 3, "act")

        cx.barrier()
        al.close()
        alAB.close()
        al = al_outer
        if stop_after in ("A", "B"):
            pass
        else:
            TC = 256
            NBC = SEQ // TC
            alC = Alloc(nc)
            wA = alC.sb([128, 8, 1536], BF16, "wA")
            wG = alC.sb([128, 8, 3072], BF16, "wG")
            wco = alC.sb([128, 4, 1024], BF16, "wco")
            wgl = alC.sb([128, 4, 2048], BF16, "wgl")
            wmo = alC.sb([128, 4, 1024], BF16, "wmo")
            wo = alC.sb([128, 8, 1024], BF16, "wo")
            Dg2 = [alC.sb([128, 31, 128], BF16, f"Dg{i}") for i in range(2)]
            kT = alC.sb([128, 4, 256], BF16, "kT")
            vtok = alC.sb([128, 2, 512], BF16, "vtok")
            gffn = alC.sb([128, D], F32, "gffn")
            wr = alC.sb([128, 8, 36], F32, "wr")
            rbias = alC.sb([128, 36], F32, "rbias")
            cdw = alC.sb([128, 4, 31], F32, "cdw")
            cb = alC.sb([128, 4], F32, "cb"); lng = alC.sb([128, 4], F32, "lng"); lnb = alC.sb([128, 4], F32, "lnb")
            onesm = alC.sb([128, 128], F32, "onesm")
            ones_bf = alC.sb([128, 128], BF16, "ones_bf")
            ecap = alC.sb([128, 32], F32, "ecap")
            tokid = alC.sb([128, NT], F32, "tokid")
            cum = alC.sb([128, 32], F32, "cum")
            lg_all = alC.sb([128, NT, 36], F32, "lg_all")
            trashp = alC.sb([128, 1], F32, "trashp")

            def ld(t, src):
                cx.op("sp", lambda e: e.dma_start(out=t[:], in_=src), full=[t.b], dma=True)

            ld(gffn, gffn_d.partition_broadcast(128))
            ld(wr, wr_d.rearrange("(c p) n -> p c n", p=128))
            ld(rbias, rbias_d.partition_broadcast(128))
            ld(cdw, cdw_d); ld(cb, cb_d); ld(lng, lng_d); ld(lnb, lnb_d)
            ld(ecap, ecap_d); ld(tokid, tokid_d); ld(trashp, trashp_d)
            cx.op("pool", lambda e: e.memset(onesm[:], 1.0 / 512.0), full=[onesm.b])
            cx.op("pool", lambda e: e.memset(ones_bf[:], 1.0), full=[ones_bf.b])
            cx.op("pool", lambda e: e.memset(cum[:], 0.0), full=[cum.b])
            alS = Alloc(nc)
            stg2 = [alS.sb([128, 8, 256], F32, f"stgc{i}") for i in range(2)]
            s2 = [0]

            def load_cast2(dst, dst_col0, src_d, c0, c1, kch, engs=("pool", "act")):
                for cc in range(c0, c1, 256):
                    w = min(256, c1 - cc)
                    s = stg2[s2[0] % 2]
                    eng = engs[s2[0] % len(engs)]
                    s2[0] += 1
                    src = src_d[:, cc:cc + w].rearrange("(c p) n -> p c n", p=128)
                    cx.op("sp", lambda e, s=s, src=src, w=w: e.dma_start(out=s[:, 0:kch, 0:w], in_=src),
                          full=[s.b], dma=True)
                    o = dst_col0 + (cc - c0)
                    if eng == "act":
                        cx.op("act", lambda e, s=s, o=o, w=w: e.copy(out=dst[:, 0:kch, o:o + w], in_=s[:, 0:kch, 0:w]),
                              reads=[s.b], writes=[dst.b])
                    else:
                        cx.op(eng, lambda e, s=s, o=o, w=w: e.tensor_copy(out=dst[:, 0:kch, o:o + w],
                                                                          in_=s[:, 0:kch, 0:w]),
                              reads=[s.b], writes=[dst.b])

            load_cast2(wA, 0, w_in_d, 0, 1024, 8)
            load_cast2(wA, 1024, w_in_d, 1536, 2048, 8)
            load_cast2(wG, 0, w_in_d, 2048, 5120, 8)
            load_cast2(wco, 0, wco_d, 0, 1024, 4)
            load_cast2(wgl, 0, wgl_d, 0, 2048, 4)
            load_cast2(wmo, 0, wmo_d, 0, 1024, 4)
            load_cast2(wo, 0, wo_d, 0, 1024, 8)
            wkv = alS.sb([128, 8, 1024], BF16, "wkv")
            load_cast2(wkv, 0, wkv_d, 0, 1024, 8)
            gmem = alS.sb([128, D], F32, "gmem")
            ld(gmem, gmem_d.partition_broadcast(128))
            memT = alS.sb([128, 8, 256], BF16, "memT")
            mx = alS.sb([128, D], F32, "mx"); mjunk = alS.sb([128, D], BF16, "mjunk")
            mss = alS.sb([128, 1], F32, "mss"); mrt = alS.sb([128, 1], F32, "mrt"); mrs = alS.sb([128, 1], F32, "mrs")
            mh = alS.sb([128, D], BF16, "mh")
            for mt in range(2):
                cx.op("sp", lambda e, mt=mt: e.dma_start(out=mx[:], in_=mem_d[mt * 128:(mt + 1) * 128, :]),
                      full=[mx.b], dma=True)
                cx.op("act", lambda e: e.activation(out=mjunk[:], in_=mx[:], func=AF.Square, accum_out=mss[:]),
                      reads=[mx.b], full=[mjunk.b, mss.b])
                cx.op("act", lambda e: e.activation(out=mrt[:], in_=mss[:], func=AF.Sqrt, scale=1.0 / D, bias=EPS),
                      reads=[mss.b], full=[mrt.b])
                cx.op("dve", lambda e: e.reciprocal(out=mrs[:], in_=mrt[:]), reads=[mrt.b], full=[mrs.b])
                cx.op("dve", lambda e: e.scalar_tensor_tensor(out=mh[:], in0=mx[:], scalar=mrs[:, 0:1], in1=gmem[:],
                                                              op0=ALU.mult, op1=ALU.mult),
                      reads=[mx.b, mrs.b, gmem.b], full=[mh.b])
                pb = psb[mt % 2]
                for c in range(8):
                    cx.op("pe", lambda e, pb=pb, c=c: e.transpose(out=pb[:, c * 128:(c + 1) * 128],
                                                                  in_=mh[:, c * 128:(c + 1) * 128],
                                                                  identity=ident_bf[:]),
                          reads=[mh.b, ident_bf.b], writes=[pb.b])
                cx.op("act", lambda e, pb=pb, mt=mt: e.copy(out=memT[:, :, mt * 128:(mt + 1) * 128],
                                                            in_=pb[:].rearrange("p (c t) -> p c t", c=8)),
                      reads=[pb.b], writes=[memT.b])
            for hd in range(4):
                ps = getps()
                for c in range(8):
                    cx.op("pe", lambda e, ps=ps, c=c, hd=hd: e.matmul(
                        ps[:, 0:256], lhsT=wkv[:, c, hd * 128:(hd + 1) * 128], rhs=memT[:, c, :],
                        start=(c == 0), stop=(c == 7)), reads=[wkv.b, memT.b], writes=[ps.b])
                cx.op("dve", lambda e, ps=ps, hd=hd: e.tensor_copy(out=kT[:, hd, :], in_=ps[:, 0:256]),
                      reads=[ps.b], writes=[kT.b])
            for mc in range(2):
                ps = getps()
                for c in range(8):
                    cx.op("pe", lambda e, ps=ps, c=c, mc=mc: e.matmul(
                        ps[:], lhsT=memT[:, c, mc * 128:(mc + 1) * 128], rhs=wkv[:, c, 512:1024],
                        start=(c == 0), stop=(c == 7)), reads=[wkv.b, memT.b], writes=[ps.b])
                cx.op("dve", lambda e, ps=ps, mc=mc: e.tensor_copy(out=vtok[:, mc, :], in_=ps[:]),
                      reads=[ps.b], writes=[vtok.b])
            cx.barrier()
            alS.close()

            alW = Alloc(nc)
            hT = [alW.sb([128, 8, TC], BF16, f"hTc{i}") for i in range(2)]
            ysb = [alW.sb([128, 4, TC], BF16, "ysb0")] * 2
            vbuf = alW.sb([128, 4, 30 + TC], BF16, "vbuf")
            sgt = [alW.sb([128, TC], F32, f"sgt{i}") for i in range(3)]
            cv = alW.sb([128, 4, TC], F32, "cv")
            sq = [sgt[1], sgt[2]]
            mean = alW.sb([128, TC], F32, "mean")
            var = alW.sb([128, TC], F32, "var"); lrs = alW.sb([128, TC], F32, "lrs")
            m2 = var; lnv = lrs
            cn = alW.sb([128, 4, TC], BF16, "cn")
            qb = alW.sb([128, 4, TC], BF16, "qb")
            Eb = [alW.sb([128, 2, TC], BF16, f"Eb{i}") for i in range(2)]
            ob = alW.sb([128, 4, TC], BF16, "ob")
            macc = alW.sb([128, TC], F32, "macc"); mt1 = alW.sb([128, TC], F32, "mt1"); mt2 = alW.sb([128, TC], F32, "mt2")
            rden = macc
            xc = [mt1, mt2]
            sqf = [sgt[1], sgt[2], mt1, mt2]
            merged = alW.sb([128, 8, TC], BF16, "merged")
            xt2 = [alW.sb([128, D], F32, f"xtc{i}") for i in range(2)]
            x2t = xt2
            h2f = alW.sb([128, D], F32, "h2f"); h2b = [alW.sb([128, D], BF16, "h2b0")] * 2
            junk2 = h2b[0]
            h2T = alW.sb([128, 8, 128], F32, "h2T")
            ss2 = alW.sb([128, 1], F32, "ss2"); rt2 = alW.sb([128, 1], F32, "rt2"); rs2 = alW.sb([128, 1], F32, "rs2")
            cx.op("pool", lambda e: e.memset(vbuf[:], 0.0), full=[vbuf.b])

            breg = {}

            def mmgrp(ps_ap, ps_b, pairs, reads):
                n = len(pairs)
                for idx, (l, r_) in enumerate(pairs):
                    cx.op("pe", lambda e, l=l, r_=r_, idx=idx: e.matmul(ps_ap, lhsT=l, rhs=r_, start=(idx == 0),
                                                                         stop=(idx == n - 1)),
                          reads=reads, writes=[ps_b])

            KCUT = int(os.environ.get("KCUT", "9"))
            KNB = int(os.environ.get("KNB", str(NBC)))
            def c_load_h(bi):
                t0 = bi * TC
                h = hT[bi % 2]
                cx.op("sp", lambda e, h=h, t0=t0: e.dma_start(
                    out=h[:], in_=hT_scr[:, :, t0:t0 + TC].rearrange("c p t -> p c t")), full=[h.b], dma=True)

            def c_load_y(bi):
                t0 = bi * TC
                yb = ysb[bi % 2]
                cx.op("sp", lambda e, yb=yb, t0=t0: e.dma_start(
                    out=yb[:], in_=ys_scr[:, :, t0:t0 + TC].rearrange("f p t -> p f t")), full=[yb.b], dma=True)

            def c_s2(bi):
                t0 = bi * TC
                h = hT[bi % 2]; yb = ysb[bi % 2]
                for f in range(4):
                    pa = getps(); pg = getps()
                    mmgrp(pa[:, 0:TC], pa.b, [(wA[:, c, f * 128:(f + 1) * 128], h[:, c, :]) for c in range(8)],
                          [wA.b, h.b])
                    mmgrp(pg[:, 0:TC], pg.b, [(wA[:, c, 512 + f * 128:512 + (f + 1) * 128], h[:, c, :]) for c in range(8)],
                          [wA.b, h.b])
                    s = sgt[f % 3]
                    cx.op("act", lambda e, pg=pg, s=s: e.activation(out=s[:], in_=pg[:, 0:TC], func=AF.Sigmoid),
                          reads=[pg.b], full=[s.b])
                    cx.op("dve", lambda e, pa=pa, s=s, f=f: e.tensor_tensor(out=vbuf[:, f, 30:30 + TC], in0=pa[:, 0:TC],
                                                                            in1=s[:], op=ALU.mult),
                          reads=[pa.b, s.b], writes=[vbuf.b])

            def c_mid(bi):
                t0 = bi * TC
                h = hT[bi % 2]; yb = ysb[bi % 2]
                for f in range(4):
                    pc = getps()
                    Dg = Dg2[f % 2]
                    cx.op("pool", lambda e, Dg=Dg, f=f: e.tensor_tensor(
                        out=Dg[:], in0=ident_f[:].unsqueeze(1).to_broadcast([128, 31, 128]),
                        in1=cdw[:, f, :].unsqueeze(2).to_broadcast([128, 31, 128]), op=ALU.mult),
                        reads=[ident_f.b, cdw.b], full=[Dg.b])
                    mmgrp(pc[:, 0:TC], pc.b, [(Dg[:, k, :], vbuf[:, f, k:k + TC]) for k in range(31)],
                          [Dg.b, vbuf.b])
                    cx.op("act", lambda e, pc=pc, f=f: e.activation(out=cv[:, f, :], in_=pc[:, 0:TC], func=AF.Identity,
                                                                    bias=cb[:, f:f + 1], scale=1.0),
                          reads=[pc.b, cb.b], writes=[cv.b])
                cx.op("pool", lambda e: e.tensor_copy(out=vbuf[:, :, 0:30], in_=vbuf[:, :, TC:TC + 30]),
                      reads=[vbuf.b], writes=[vbuf.b])
                for hd in range(4):
                    pq_ = getps()
                    mmgrp(pq_[:, 0:TC], pq_.b, [(wA[:, c, 1024 + hd * 128:1024 + (hd + 1) * 128], h[:, c, :])
                                                for c in range(8)], [wA.b, h.b])
                    cx.op("dve", lambda e, pq_=pq_, hd=hd: e.tensor_copy(out=qb[:, hd, :], in_=pq_[:, 0:TC]),
                          reads=[pq_.b], writes=[qb.b])
                for f in range(4):
                    cx.op("act", lambda e, f=f: e.activation(out=sq[f % 2][:] if False else sqf[f][:], in_=cv[:, f, :],
                                                             func=AF.Square),
                          reads=[cv.b], full=[sqf[f].b])

                def att_scores(hd):
                    E = Eb[hd % 2]
                    for mc in range(2):
                        psc = getps()
                        mmgrp(psc[:, 0:TC], psc.b, [(kT[:, hd, mc * 128:(mc + 1) * 128], qb[:, hd, :])], [kT.b, qb.b])
                        cx.op("act", lambda e, psc=psc, E=E, mc=mc: e.activation(
                            out=E[:, mc, :], in_=psc[:, 0:TC], func=AF.Exp, scale=float(128 ** -0.5)),
                            reads=[psc.b], writes=[E.b])

                def att_out(hd):
                    E = Eb[hd % 2]
                    po = getps(); pd = getps()
                    mmgrp(po[:, 0:TC], po.b, [(vtok[:, mc, hd * 128:(hd + 1) * 128], E[:, mc, :]) for mc in range(2)],
                          [vtok.b, E.b])
                    mmgrp(pd[:, 0:TC], pd.b, [(ones_bf[:], E[:, mc, :]) for mc in range(2)], [ones_bf.b, E.b])
                    cx.op("dve", lambda e, pd=pd: e.reciprocal(out=rden[:], in_=pd[:, 0:TC]), reads=[pd.b], full=[rden.b])
                    cx.op("dve", lambda e, po=po, hd=hd: e.tensor_tensor(out=ob[:, hd, :], in0=po[:, 0:TC], in1=rden[:],
                                                                         op=ALU.mult),
                          reads=[po.b, rden.b], writes=[ob.b])

                att_scores(0)
                att_scores(1)
                pm = getps(); pq = getps()
                mmgrp(pm[:, 0:TC], pm.b, [(onesm[:], cv[:, f, :]) for f in range(4)], [onesm.b, cv.b])
                mmgrp(pq[:, 0:TC], pq.b, [(onesm[:], sqf[f][:]) for f in range(4)], [onesm.b] + [sqf[f].b for f in range(4)])
                cx.op("act", lambda e, pm=pm: e.copy(out=mean[:], in_=pm[:, 0:TC]), reads=[pm.b], full=[mean.b])
                cx.op("dve", lambda e: e.tensor_tensor(out=var[:], in0=mean[:], in1=mean[:], op=ALU.mult),
                      reads=[mean.b], full=[var.b])
                cx.op("dve", lambda e, pq=pq: e.tensor_tensor(out=var[:], in0=pq[:, 0:TC], in1=var[:], op=ALU.subtract),
                      reads=[pq.b], writes=[var.b])
                cx.op("dve", lambda e: e.tensor_scalar(out=var[:], in0=var[:], scalar1=float(EPS), scalar2=None,
                                                       op0=ALU.add), reads=[var.b], writes=[var.b])
                cx.op("act", lambda e: e.activation(out=lrs[:], in_=var[:], func=AF.Ln), reads=[var.b], full=[lrs.b])
                cx.op("act", lambda e: e.activation(out=lrs[:], in_=lrs[:], func=AF.Exp, scale=-0.5),
                      reads=[], writes=[lrs.b])
                cx.op("dve", lambda e: e.tensor_tensor(out=cv[:], in0=cv[:],
                                                       in1=mean[:].unsqueeze(1).to_broadcast([128, 4, TC]),
                                                       op=ALU.subtract), reads=[mean.b], writes=[cv.b])
                cx.op("dve", lambda e: e.tensor_tensor(out=cv[:], in0=cv[:],
                                                       in1=lrs[:].unsqueeze(1).to_broadcast([128, 4, TC]),
                                                       op=ALU.mult), reads=[lrs.b], writes=[cv.b])
                att_out(0)
                att_scores(2)
                att_out(1)
                att_scores(3)
                att_out(2)
                att_out(3)
                for f in range(4):
                    cx.op("act", lambda e, f=f: e.activation(out=cn[:, f, :], in_=cv[:, f, :], func=AF.Silu,
                                                             bias=lnb[:, f:f + 1], scale=lng[:, f:f + 1]),
                          reads=[cv.b, lnb.b, lng.b], writes=[cn.b])
                for j in range(8):
                    js = slice(j * 128, (j + 1) * 128)
                    pga = getps(); pyc = getps()
                    mmgrp(pga[:, 0:TC], pga.b, [(wG[:, c, j * 128:(j + 1) * 128], h[:, c, :]) for c in range(8)], [wG.b, h.b])
                    mmgrp(pyc[:, 0:TC], pyc.b, [(wco[:, f, js], cn[:, f, :]) for f in range(4)], [wco.b, cn.b])
                    s = sgt[0]
                    cx.op("act", lambda e, pga=pga, s=s: e.activation(out=s[:], in_=pga[:, 0:TC], func=AF.Sigmoid),
                          reads=[pga.b], full=[s.b])
                    cx.op("dve", lambda e, pyc=pyc, s=s: e.tensor_tensor(out=macc[:], in0=pyc[:, 0:TC], in1=s[:], op=ALU.mult),
                          reads=[pyc.b, s.b], full=[macc.b])
                    pgb = getps(); pza = getps(); pzb = getps()
                    mmgrp(pgb[:, 0:TC], pgb.b, [(wG[:, c, 1024 + j * 128:1024 + (j + 1) * 128], h[:, c, :]) for c in range(8)],
                          [wG.b, h.b])
                    mmgrp(pza[:, 0:TC], pza.b, [(wgl[:, f, js], yb[:, f, :]) for f in range(4)], [wgl.b, yb.b])
                    mmgrp(pzb[:, 0:TC], pzb.b, [(wgl[:, f, 1024 + j * 128:1024 + (j + 1) * 128], yb[:, f, :]) for f in range(4)],
                          [wgl.b, yb.b])
                    sb_ = sgt[1]; sz = sgt[2]
                    cx.op("act", lambda e, pgb=pgb, sb_=sb_: e.activation(out=sb_[:], in_=pgb[:, 0:TC], func=AF.Sigmoid),
                          reads=[pgb.b], full=[sb_.b])
                    cx.op("act", lambda e, pzb=pzb, sz=sz: e.activation(out=sz[:], in_=pzb[:, 0:TC], func=AF.Sigmoid),
                          reads=[pzb.b], full=[sz.b])
                    cx.op("dve", lambda e, pza=pza, sz=sz: e.tensor_tensor(out=mt1[:], in0=pza[:, 0:TC], in1=sz[:], op=ALU.mult),
                          reads=[pza.b, sz.b], full=[mt1.b])
                    cx.op("dve", lambda e, sb_=sb_: e.tensor_tensor(out=mt1[:], in0=mt1[:], in1=sb_[:], op=ALU.mult),
                          reads=[sb_.b], writes=[mt1.b])
                    cx.op("dve", lambda e: e.tensor_tensor(out=macc[:], in0=macc[:], in1=mt1[:], op=ALU.add),
                          reads=[mt1.b], writes=[macc.b])
                    pgc = getps(); pym = getps()
                    mmgrp(pgc[:, 0:TC], pgc.b, [(wG[:, c, 2048 + j * 128:2048 + (j + 1) * 128], h[:, c, :]) for c in range(8)],
                          [wG.b, h.b])
                    mmgrp(pym[:, 0:TC], pym.b, [(wmo[:, hd, js], ob[:, hd, :]) for hd in range(4)], [wmo.b, ob.b])
                    s = sgt[0]
                    cx.op("act", lambda e, pgc=pgc, s=s: e.activation(out=s[:], in_=pgc[:, 0:TC], func=AF.Sigmoid),
                          reads=[pgc.b], full=[s.b])
                    cx.op("dve", lambda e, pym=pym, s=s: e.tensor_tensor(out=mt2[:], in0=pym[:, 0:TC], in1=s[:], op=ALU.mult),
                          reads=[pym.b, s.b], full=[mt2.b])
                    cx.op("dve", lambda e, j=j: e.tensor_tensor(out=merged[:, j, :], in0=macc[:], in1=mt2[:], op=ALU.add),
                          reads=[macc.b, mt2.b], writes=[merged.b])

            def c_tail(bi):
                ntt = TC // 128
                tis = [bi * ntt + tt for tt in range(ntt)]
                for tt, ti in enumerate(tis):
                    xt_ = xt2[ti % 2]
                    cx.op("sp", lambda e, xt_=xt_, ti=ti: e.dma_start(out=xt_[:], in_=x_d[ti * 128:(ti + 1) * 128, :]),
                          full=[xt_.b], dma=True)
                for tt, ti in enumerate(tis):
                    xt_ = xt2[ti % 2]; x2 = xt_
                    for half in range(2):
                        po_ = getps()
                        mmgrp(po_[:], po_.b, [(merged[:, j, tt * 128:(tt + 1) * 128], wo[:, j, half * 512:(half + 1) * 512])
                                              for j in range(8)], [merged.b, wo.b])
                        cx.op("dve", lambda e, po_=po_, x2=x2, xt_=xt_, half=half: e.tensor_tensor(
                            out=x2[:, half * 512:(half + 1) * 512], in0=po_[:], in1=xt_[:, half * 512:(half + 1) * 512],
                            op=ALU.add), reads=[po_.b, xt_.b], writes=[x2.b])
                    cx.op("sp", lambda e, x2=x2, ti=ti: e.dma_start(out=x2_scr[ti * 128:(ti + 1) * 128, :], in_=x2[:]),
                          reads=[x2.b], writes=[x2B], dma=True)
                for tt, ti in enumerate(tis):
                    x2 = xt2[ti % 2]; hb2 = h2b[0]
                    cx.op("act", lambda e, x2=x2: e.activation(out=junk2[:], in_=x2[:], func=AF.Square, accum_out=ss2[:]),
                          reads=[x2.b], full=[junk2.b, ss2.b])
                    cx.op("act", lambda e: e.activation(out=rt2[:], in_=ss2[:], func=AF.Sqrt, scale=1.0 / D, bias=EPS),
                          reads=[ss2.b], full=[rt2.b])
                    cx.op("dve", lambda e: e.reciprocal(out=rs2[:], in_=rt2[:]), reads=[rt2.b], full=[rs2.b])
                    cx.op("dve", lambda e, x2=x2: e.scalar_tensor_tensor(out=h2f[:], in0=x2[:], scalar=rs2[:, 0:1],
                                                                         in1=gffn[:], op0=ALU.mult, op1=ALU.mult),
                          reads=[x2.b, rs2.b, gffn.b], full=[h2f.b])
                    cx.op("act", lambda e, hb2=hb2: e.copy(out=hb2[:], in_=h2f[:]), reads=[h2f.b], full=[hb2.b])
                    cx.op("sp", lambda e, hb2=hb2, ti=ti: e.dma_start(out=h2_scr[ti * 128:(ti + 1) * 128, :], in_=hb2[:]),
                          reads=[hb2.b], writes=[h2B], dma=True)
                    pra = getps(); prb = getps()
                    for c in range(8):
                        pr = pra if c < 4 else prb
                        cx.op("pe", lambda e, pr=pr, c=c: e.transpose(out=pr[:, (c % 4) * 128:(c % 4 + 1) * 128],
                                                                      in_=h2f[:, c * 128:(c + 1) * 128], identity=ident_f[:]),
                              reads=[h2f.b, ident_f.b], writes=[pr.b])
                    cx.op("act", lambda e, pra=pra: e.copy(out=h2T[:, 0:4, :], in_=pra[:].rearrange("p (c t) -> p c t", c=4)),
                          reads=[pra.b], writes=[h2T.b])
                    cx.op("dve", lambda e, prb=prb: e.tensor_copy(out=h2T[:, 4:8, :],
                                                                  in_=prb[:].rearrange("p (c t) -> p c t", c=4)),
                          reads=[prb.b], writes=[h2T.b])
                    plg = getps()
                    mmgrp(plg[:, 0:36], plg.b, [(h2T[:, c, :], wr[:, c, :]) for c in range(8)], [h2T.b, wr.b])
                    cx.op("dve", lambda e, plg=plg, ti=ti: e.tensor_tensor(out=lg_all[:, ti, :], in0=plg[:, 0:36],
                                                                        in1=rbias[:], op=ALU.add),
                          reads=[plg.b, rbias.b], writes=[lg_all.b])

            NBR = min(NBC, KNB) if KCUT >= 2 else 0
            if NBR > 0:
                c_load_h(0); c_load_y(0); c_s2(0)
            for bi in range(NBR):
                if bi + 1 < NBR:
                    c_load_h(bi + 1)
                c_mid(bi)
                if bi + 1 < NBR:
                    c_load_y(bi + 1)
                    c_s2(bi + 1)
                c_tail(bi)
            cx.barrier()
            alW.close()
            alR = Alloc(nc)
            RS = Buf("route")
            tri = alR.sb([128, 128], F32, "tri")
            ones_f = alR.sb([128, 128], F32, "ones_f")
            cx.op("sp", lambda e: e.dma_start(out=tri[:], in_=tri_d), full=[tri.b], dma=True)
            cx.op("pool", lambda e: e.memset(ones_f[:], 1.0), full=[ones_f.b])

            def rd(fn, extra_reads=(), extra_writes=()):
                cx.op("dve", fn, reads=[RS, lg_all.b] + list(extra_reads), writes=[RS] + list(extra_writes))

            def R(shape, name, dt=F32):
                return alR.sb(shape, dt, name)

            NTT = NT
            NEB_ = NEXP * NBLK
            gmax = R([128, NTT], "gmax"); ohg = R([128, NTT, 4], "ohg"); eg = R([128, NTT, 4], "eg")
            sumg = R([128, NTT], "sumg"); ptop = R([128, NTT], "ptop")
            selm = R([128, NTT, 4, 8], "selm"); sel = R([128, NTT, 8], "sel"); sel2 = R([128, NTT, 8], "sel2")
            m1_ = R([128, NTT], "m1_"); m2_ = R([128, NTT], "m2_"); oh1 = R([128, NTT, 8], "oh1"); oh2 = R([128, NTT, 8], "oh2")
            dm = R([128, NTT], "dm"); w1 = R([128, NTT], "w1"); w2 = R([128, NTT], "w2")
            M1 = R([128, NTT, 4, 8], "M1"); M2 = R([128, NTT, 4, 8], "M2"); Mc = R([128, NTT, 32], "Mc")
            Cex = R([128, NTT, 32], "Cex"); pos = R([128, NTT, 32], "pos"); bk = R([128, NTT, 32], "bk")
            sf = R([128, NTT, 32], "sf"); ov = R([128, NTT, 32], "ov"); tq = R([128, NTT, 32], "tq")
            sk = [R([128, NTT], f"sk{k}") for k in range(2)]; okk = R([128, NTT], "okk"); dd = R([128, NTT], "dd")
            si = [R([128, NTT], f"si{k}", I32) for k in range(2)]
            ent = [R([128, NTT, 4], f"ent{k}") for k in range(2)]
            le4 = lg_all[:, :, 4:36].rearrange("p t (g j) -> p t g j", g=4)

            def bc3(a, n):
                return a.unsqueeze(2).to_broadcast([128, NTT, n])

            rd(lambda e: e.tensor_reduce(out=gmax[:], in_=lg_all[:, :, 0:4], axis=AX.X, op=ALU.max))
            rd(lambda e: e.tensor_tensor(out=ohg[:], in0=lg_all[:, :, 0:4], in1=bc3(gmax[:], 4), op=ALU.is_equal))
            rd(lambda e: e.tensor_tensor(out=eg[:], in0=lg_all[:, :, 0:4], in1=bc3(gmax[:], 4), op=ALU.subtract))
            cx.op("act", lambda e: e.activation(out=eg[:], in_=eg[:], func=AF.Exp), reads=[RS], writes=[RS])
            rd(lambda e: e.tensor_reduce(out=sumg[:], in_=eg[:], axis=AX.X, op=ALU.add))
            rd(lambda e: e.reciprocal(out=ptop[:], in_=sumg[:]))
            rd(lambda e: e.tensor_tensor(out=selm[:], in0=le4,
                                         in1=ohg[:].unsqueeze(3).to_broadcast([128, NTT, 4, 8]), op=ALU.mult))
            rd(lambda e: e.tensor_reduce(out=sel[:], in_=selm[:].rearrange("p t g j -> p t j g"), axis=AX.X, op=ALU.add))
            rd(lambda e: e.tensor_reduce(out=m1_[:], in_=sel[:], axis=AX.X, op=ALU.max))
            rd(lambda e: e.tensor_tensor(out=oh1[:], in0=sel[:], in1=bc3(m1_[:], 8), op=ALU.is_equal))
            rd(lambda e: e.scalar_tensor_tensor(out=sel2[:], in0=oh1[:], scalar=-1e30, in1=sel[:], op0=ALU.mult, op1=ALU.add))
            rd(lambda e: e.tensor_reduce(out=m2_[:], in_=sel2[:], axis=AX.X, op=ALU.max))
            rd(lambda e: e.tensor_tensor(out=oh2[:], in0=sel2[:], in1=bc3(m2_[:], 8), op=ALU.is_equal))
            rd(lambda e: e.tensor_tensor(out=dm[:], in0=m1_[:], in1=m2_[:], op=ALU.subtract))
            cx.op("act", lambda e: e.activation(out=w1[:], in_=dm[:], func=AF.Sigmoid), reads=[RS], writes=[RS])
            rd(lambda e: e.tensor_tensor(out=w1[:], in0=w1[:], in1=ptop[:], op=ALU.mult))
            rd(lambda e: e.tensor_tensor(out=w2[:], in0=ptop[:], in1=w1[:], op=ALU.subtract))
            rd(lambda e: e.tensor_tensor(out=M1[:], in0=ohg[:].unsqueeze(3).to_broadcast([128, NTT, 4, 8]),
                                         in1=oh1[:].unsqueeze(2).to_broadcast([128, NTT, 4, 8]), op=ALU.mult))
            rd(lambda e: e.tensor_tensor(out=M2[:], in0=ohg[:].unsqueeze(3).to_broadcast([128, NTT, 4, 8]),
                                         in1=oh2[:].unsqueeze(2).to_broadcast([128, NTT, 4, 8]), op=ALU.mult))
            rd(lambda e: e.tensor_tensor(out=Mc[:], in0=M1[:].rearrange("p t g j -> p t (g j)"),
                                         in1=M2[:].rearrange("p t g j -> p t (g j)"), op=ALU.add))
            rd(lambda e: e.memset(Cex[:, 0, :], 0.0))
            for i in range(1, NTT):
                rd(lambda e, i=i: e.tensor_tensor(out=Cex[:, i, :], in0=Cex[:, i - 1, :], in1=Mc[:, i - 1, :], op=ALU.add))
            pp = [getps(), getps()]
            for i in range(NTT):
                pb_ = pp[i // 16]
                o_ = pb_[:, (i % 16) * 32:(i % 16 + 1) * 32]
                cx.op("pe", lambda e, o_=o_, i=i: e.matmul(o_, lhsT=tri[:], rhs=Mc[:, i, :], start=True, stop=False),
                      reads=[tri.b, RS], writes=[pb_.b])
                cx.op("pe", lambda e, o_=o_, i=i: e.matmul(o_, lhsT=ones_f[:], rhs=Cex[:, i, :], start=False, stop=True),
                      reads=[ones_f.b, RS], writes=[pb_.b])
            for hh in range(2):
                rd(lambda e, hh=hh: e.tensor_copy(out=pos[:, hh * 16:(hh + 1) * 16, :],
                                                  in_=pp[hh][:].rearrange("p (t x) -> p t x", t=16)), [pp[hh].b])
            rd(lambda e: e.tensor_single_scalar(out=bk[:], in_=pos[:], scalar=127.5, op=ALU.is_gt))
            for thr in range(2, NBLK):
                rd(lambda e, thr=thr: e.tensor_single_scalar(out=tq[:], in_=pos[:], scalar=128.0 * thr - 0.5, op=ALU.is_gt))
                rd(lambda e: e.tensor_tensor(out=bk[:], in0=bk[:], in1=tq[:], op=ALU.add))
            rd(lambda e: e.scalar_tensor_tensor(out=bk[:], in0=bk[:], scalar=float(1 - 128 * NEB_),
                                                in1=ecap[:].unsqueeze(1).to_broadcast([128, NTT, 32]),
                                                op0=ALU.mult, op1=ALU.add), [ecap.b])
            rd(lambda e: e.scalar_tensor_tensor(out=sf[:], in0=pos[:], scalar=float(NEB_), in1=bk[:],
                                                op0=ALU.mult, op1=ALU.add))
            rd(lambda e: e.tensor_single_scalar(out=ov[:], in_=pos[:], scalar=float(CAP) - 0.5, op=ALU.is_gt))
            for k, (Mk, wk) in enumerate(((M1, w1), (M2, w2))):
                Mk32 = Mk[:].rearrange("p t g j -> p t (g j)")
                rd(lambda e, Mk32=Mk32: e.tensor_tensor(out=tq[:], in0=Mk32, in1=sf[:], op=ALU.mult))
                rd(lambda e, k=k: e.tensor_reduce(out=sk[k][:], in_=tq[:], axis=AX.X, op=ALU.add))
                rd(lambda e, Mk32=Mk32: e.tensor_tensor(out=tq[:], in0=Mk32, in1=ov[:], op=ALU.mult))
                rd(lambda e: e.tensor_reduce(out=okk[:], in_=tq[:], axis=AX.X, op=ALU.add))
                rd(lambda e, k=k: e.tensor_scalar(out=dd[:], in0=sk[k][:], scalar1=trashp[:, 0:1], scalar2=None,
                                                  op0=ALU.subtract), [trashp.b])
                rd(lambda e: e.tensor_tensor(out=dd[:], in0=dd[:], in1=okk[:], op=ALU.mult))
                rd(lambda e, k=k: e.tensor_tensor(out=sk[k][:], in0=sk[k][:], in1=dd[:], op=ALU.subtract))
                rd(lambda e, k=k: e.tensor_copy(out=si[k][:], in_=sk[k][:]), (), [si[k].b])
                rd(lambda e, k=k: e.memset(ent[k][:], 0.0), (), [ent[k].b])
                rd(lambda e, k=k: e.tensor_copy(out=ent[k][:, :, 0], in_=tokid[:]), [tokid.b], [ent[k].b])
                rd(lambda e, k=k: e.tensor_scalar(out=ent[k][:, :, 1], in0=tokid[:], scalar1=float(k * ROWS), scalar2=None,
                                                  op0=ALU.add), [tokid.b], [ent[k].b])
                rd(lambda e, k=k, wk=wk: e.tensor_copy(out=ent[k][:, :, 2], in_=wk[:]), (), [ent[k].b])
            for i in range(NTT):
                for k in range(2):
                    cx.op("pool", lambda e, i=i, k=k: e.indirect_dma_start(
                        out=lst_d, out_offset=bass.IndirectOffsetOnAxis(ap=si[k][:, i:i + 1], axis=0),
                        in_=ent[k][:, i, :], in_offset=None),
                        reads=[si[k].b, ent[k].b], writes=[lstB], dma=True)
            cx.barrier()
            alR.close()
            alC.close()
        if stop_after in ("A", "B", "C"):
            pass
        else:
            alD = Alloc(nc)
            NEB = NEXP * NBLK
            lst_sb = alD.sb([128, NEB, 4], F32, "lst_sb")
            idx_i = alD.sb([128, NEB], I32, "idx_i")
            dst_i = alD.sb([128, NEB], I32, "dst_i")
            cx.op("sp", lambda e: e.dma_start(out=lst_sb[:], in_=lst_d[0:NEXP * CAP, :].rearrange("(s eb) w -> s eb w", s=128)),
                  reads=[lstB], full=[lst_sb.b], dma=True)
            cx.op("dve", lambda e: e.tensor_copy(out=idx_i[:], in_=lst_sb[:, :, 0]), reads=[lst_sb.b], full=[idx_i.b])
            cx.op("dve", lambda e: e.tensor_copy(out=dst_i[:], in_=lst_sb[:, :, 1]), reads=[lst_sb.b], full=[dst_i.b])
            NWB = 3
            Wg = [alD.sb([128, 8, 256], BF16, f"Wg{i}") for i in range(NWB)]
            Wu = [alD.sb([128, 8, 256], BF16, f"Wu{i}") for i in range(NWB)]
            Wd = [alD.sb([128, 2, 1024], BF16, f"Wd{i}") for i in range(NWB)]
            Gt = [alD.sb([128, D], BF16, f"Gt{i}") for i in range(3)]
            Xe = [alD.sb([128, 8, CAP], BF16, f"Xe{i}") for i in range(2)]
            sgl = [alD.sb([128, CAP], F32, f"sgl{i}") for i in range(2)]
            ae = [alD.sb([128, 2, CAP], BF16, f"ae{i}") for i in range(2)]
            Yt = [alD.sb([128, D], BF16, f"Yt{i}") for i in range(3)]

            Gt6 = Gt + [alD.sb([128, D], BF16, f"Gtx{i}") for i in range(3)]

            def load_w_dma(e_):
                p = e_ % NWB
                cx.op("sp", lambda e: e.dma_start(out=Wg[p][:].rearrange("p c n -> p (c n)"), in_=wbf_scr[e_, 0]),
                      reads=[wbfB], full=[Wg[p].b], dma=True)
                cx.op("sp", lambda e: e.dma_start(out=Wu[p][:].rearrange("p c n -> p (c n)"), in_=wbf_scr[e_, 1]),
                      reads=[wbfB], full=[Wu[p].b], dma=True)
                cx.op("sp", lambda e: e.dma_start(out=Wd[p][:].rearrange("p c n -> p (c n)"), in_=wbf_scr[e_, 2]),
                      reads=[wbfB], full=[Wd[p].b], dma=True)

            def load_w_cast(e_):
                pass

            def gathers(e_):
                for blk in range(NBLK):
                    eb = e_ * NBLK + blk
                    G = Gt6[(e_ % 2) * 3 + blk]
                    cx.op("pool", lambda e, G=G, eb=eb: e.indirect_dma_start(
                        out=G[:], out_offset=None, in_=h2_scr,
                        in_offset=bass.IndirectOffsetOnAxis(ap=idx_i[:, eb:eb + 1], axis=0)),
                        reads=[idx_i.b, h2B], full=[G.b], dma=True)

            gi = [0]
            load_w_dma(0)
            load_w_dma(1)
            gathers(0)
            KNE = int(os.environ.get("KNE", str(NEXP)))
            for e_ in range(KNE):
                p = e_ % NWB
                if e_ + 2 < NEXP:
                    load_w_dma(e_ + 2)
                if e_ + 1 < NEXP:
                    gathers(e_ + 1)
                X = Xe[e_ % 2]
                for blk in range(NBLK):
                    G = Gt6[(e_ % 2) * 3 + blk]
                    pbk = psb[gi[0] % 2]
                    gi[0] += 1
                    for c in range(8):
                        cx.op("pe", lambda e, pbk=pbk, G=G, c=c: e.transpose(
                            out=pbk[:, c * 128:(c + 1) * 128], in_=G[:, c * 128:(c + 1) * 128], identity=ident_bf[:]),
                            reads=[G.b, ident_bf.b], writes=[pbk.b])
                    if blk % 2 == 0:
                        cx.op("act", lambda e, pbk=pbk, X=X, blk=blk: e.copy(
                            out=X[:, :, blk * 128:(blk + 1) * 128], in_=pbk[:].rearrange("p (c t) -> p c t", c=8)),
                            reads=[pbk.b], writes=[X.b])
                    else:
                        cx.op("dve", lambda e, pbk=pbk, X=X, blk=blk: e.tensor_copy(
                            out=X[:, :, blk * 128:(blk + 1) * 128], in_=pbk[:].rearrange("p (c t) -> p c t", c=8)),
                            reads=[pbk.b], writes=[X.b])
                a_ = ae[e_ % 2]
                for ft in range(2):
                    pg = getps(); pu = getps()
                    for c in range(8):
                        cx.op("pe", lambda e, pg=pg, c=c, ft=ft, X=X, p=p: e.matmul(
                            pg[:, 0:CAP], lhsT=Wg[p][:, c, ft * 128:(ft + 1) * 128], rhs=X[:, c, :],
                            start=(c == 0), stop=(c == 7)), reads=[Wg[p].b, X.b], writes=[pg.b])
                    for c in range(8):
                        cx.op("pe", lambda e, pu=pu, c=c, ft=ft, X=X, p=p: e.matmul(
                            pu[:, 0:CAP], lhsT=Wu[p][:, c, ft * 128:(ft + 1) * 128], rhs=X[:, c, :],
                            start=(c == 0), stop=(c == 7)), reads=[Wu[p].b, X.b], writes=[pu.b])
                    s = sgl[ft]
                    cx.op("act", lambda e, pg=pg, s=s: e.activation(out=s[:], in_=pg[:, 0:CAP], func=AF.Silu),
                          reads=[pg.b], full=[s.b])
                    cx.op("dve", lambda e, pu=pu, s=s, a_=a_, ft=ft: e.tensor_tensor(
                        out=a_[:, ft, :], in0=pu[:, 0:CAP], in1=s[:], op=ALU.mult),
                        reads=[pu.b, s.b], writes=[a_.b])
                for blk in range(NBLK):
                    eb = e_ * NBLK + blk
                    Y = Yt[eb % 3]
                    for half in range(2):
                        py = getps()
                        for ft in range(2):
                            cx.op("pe", lambda e, py=py, ft=ft, blk=blk, half=half, a_=a_, p=p: e.matmul(
                                py[:], lhsT=a_[:, ft, blk * 128:(blk + 1) * 128],
                                rhs=Wd[p][:, ft, half * 512:(half + 1) * 512], start=(ft == 0), stop=(ft == 1)),
                                reads=[a_.b, Wd[p].b], writes=[py.b])
                        if half == 0:
                            cx.op("dve", lambda e, py=py, Y=Y, eb=eb: e.tensor_scalar(
                                out=Y[:, 0:512], in0=py[:], scalar1=lst_sb[:, eb, 2:3], scalar2=None, op0=ALU.mult),
                                reads=[py.b, lst_sb.b], writes=[Y.b])
                        else:
                            cx.op("act", lambda e, py=py, Y=Y, eb=eb: e.activation(
                                out=Y[:, 512:1024], in_=py[:], func=AF.Copy, scale=lst_sb[:, eb, 2:3]),
                                reads=[py.b, lst_sb.b], writes=[Y.b])
                    cx.op("pool", lambda e, Y=Y, eb=eb: e.indirect_dma_start(
                        out=moe_scr, out_offset=bass.IndirectOffsetOnAxis(ap=dst_i[:, eb:eb + 1], axis=0),
                        in_=Y[:], in_offset=None), reads=[Y.b, dst_i.b], writes=[moeB], dma=True)
                if e_ + 1 < NEXP:
                    load_w_cast(e_ + 1)
            cx.barrier()
            alD.close()

            alE = Alloc(nc)
            gfin = alE.sb([128, D], F32, "gfin")
            cx.op("sp", lambda e: e.dma_start(out=gfin[:], in_=gfin_d.partition_broadcast(128)), full=[gfin.b], dma=True)
            NE_ = 4
            xa = [alE.sb([128, D], F32, f"xa{i}") for i in range(NE_)]
            m0 = [alE.sb([128, D], BF16, f"m0{i}") for i in range(NE_)]
            m1 = [alE.sb([128, D], BF16, f"m1{i}") for i in range(NE_)]
            ot = [alE.sb([128, D], F32, f"ot{i}") for i in range(NE_)]
            junk3 = alE.sb([128, D], BF16, "junk3")
            sse = [alE.sb([128, 1], F32, f"sse{i}") for i in range(NE_)]
            rte = [alE.sb([128, 1], F32, f"rte{i}") for i in range(NE_)]
            rse = [alE.sb([128, 1], F32, f"rse{i}") for i in range(NE_)]
            outB = Buf("out")

            def e_load(ti):
                p = ti % NE_
                rows = slice(ti * 128, (ti + 1) * 128)
                cx.op("sp", lambda e, p=p, rows=rows: e.dma_start(out=xa[p][:], in_=x2_scr[rows, :]),
                      reads=[x2B], full=[xa[p].b], dma=True)
                cx.op("sp", lambda e, p=p, rows=rows: e.dma_start(out=m0[p][:], in_=moe_scr[rows, :]),
                      reads=[moeB], full=[m0[p].b], dma=True)
                cx.op("sp", lambda e, p=p, ti=ti: e.dma_start(
                    out=m1[p][:], in_=moe_scr[ROWS + ti * 128:ROWS + (ti + 1) * 128, :]),
                    reads=[moeB], full=[m1[p].b], dma=True)

            for ti in range(min(NE_ - 1, NT)):
                e_load(ti)
            for ti in range(NT):
                p = ti % NE_
                rows = slice(ti * 128, (ti + 1) * 128)
                if ti + NE_ - 1 < NT:
                    e_load(ti + NE_ - 1)
                cx.op("pool", lambda e, p=p: e.tensor_tensor(out=xa[p][:], in0=xa[p][:], in1=m0[p][:], op=ALU.add),
                      reads=[m0[p].b], writes=[xa[p].b])
                cx.op("dve", lambda e, p=p: e.tensor_tensor(out=xa[p][:], in0=xa[p][:], in1=m1[p][:], op=ALU.add),
                      reads=[m1[p].b], writes=[xa[p].b])
                cx.op("act", lambda e, p=p: e.activation(out=junk3[:], in_=xa[p][:], func=AF.Square, accum_out=sse[p][:]),
                      reads=[xa[p].b], full=[junk3.b, sse[p].b])
                cx.op("act", lambda e, p=p: e.activation(out=rte[p][:], in_=sse[p][:], func=AF.Sqrt, scale=1.0 / D, bias=EPS),
                      reads=[sse[p].b], full=[rte[p].b])
                cx.op("dve", lambda e, p=p: e.reciprocal(out=rse[p][:], in_=rte[p][:]), reads=[rte[p].b], full=[rse[p].b])
                cx.op("dve", lambda e, p=p: e.scalar_tensor_tensor(out=ot[p][:], in0=xa[p][:], scalar=rse[p][:, 0:1],
                                                                   in1=gfin[:], op0=ALU.mult, op1=ALU.mult),
                      reads=[xa[p].b, rse[p].b, gfin.b], full=[ot[p].b])
                cx.op("sp", lambda e, p=p, rows=rows: e.dma_start(out=out_d[rows, :], in_=ot[p][:]),
                      reads=[ot[p].b], writes=[outB], dma=True)
            cx.barrier()
            alE.close()
        cx.barrier()
        cx.emit(block)
        print("waits", cx.nwait, "instrs", {e: cx.cnt[e] for e in cx.ENG}, "signals", {e: len(cx.waited[e]) for e in cx.ENG})
    return nc


def host_consts():
    c = {}
    c["ident_bf"] = np.eye(128, dtype=np.float32).astype(ml_dtypes.bfloat16)
    c["ident_f"] = np.eye(128, dtype=np.float32)
    psel = np.zeros((128, 8, 240), np.float32)
    for a in range(8):
        for i in range(16):
            psel[a * 16 + i, a, 7 * 16 + i] = 1.0
    c["psel"] = psel.astype(ml_dtypes.bfloat16)
    kk = np.arange(128) // 16
    c["cmask"] = (kk[None, :] >= kk[:, None]).astype(np.float32)
    c["tri"] = (np.arange(128)[:, None] < np.arange(128)[None, :]).astype(np.float32)
    c["ecap"] = np.ascontiguousarray(np.broadcast_to((np.arange(32) * NBLK).astype(np.float32)[None, :], (128, 32)))
    c["tokid"] = (np.arange(NT)[None, :] * 128 + np.arange(128)[:, None]).astype(np.float32)
    li = np.zeros((NEXP * CAP + 128, 4), np.float32)
    li[:, 0] = SEQ + ((np.arange(NEXP * CAP + 128) // (NEXP * NBLK)) % 128)
    li[:, 1] = li[:, 0]
    c["trashp"] = (NEXP * CAP + np.arange(128)).astype(np.float32).reshape(128, 1)
    c["lst_init"] = li
    return c


def relayout_pc(w):
    E, K, N = w.shape
    return np.ascontiguousarray(w.reshape(E, K // 128, 128, N).transpose(0, 2, 1, 3))


def pair_layout(a):
    rest = a.shape[2:]
    a = a.reshape((16, 2, 64) + rest)
    a = np.moveaxis(a, 0, 2)
    return np.ascontiguousarray(a.reshape((128, 16) + rest))


def make_inmap(inputs, b, consts=None):
    f = lambda a: np.ascontiguousarray(a, dtype=np.float32)
    m = {"x": f(inputs["x"][b]),
         "g_mix": f(inputs["g_mix"]),
         "w_in": f(inputs["w_in"][0])}
    m["lamre_l"] = pair_layout(f(inputs["ssm_lambda_re"][0]))
    m["lamim_l"] = pair_layout(f(inputs["ssm_lambda_im"][0]))
    m["logdt_l"] = pair_layout(np.broadcast_to(f(inputs["ssm_log_dt"][0])[:, None], (32, 64)))
    m["bre_l"] = pair_layout(f(inputs["ssm_b_re"][0]))
    m["bim_l"] = pair_layout(f(inputs["ssm_b_im"][0]))
    m["cre_l"] = pair_layout(f(inputs["ssm_c_re"][0]).transpose(0, 2, 1))
    m["cim_l"] = pair_layout(f(inputs["ssm_c_im"][0]).transpose(0, 2, 1))
    m["d_l"] = np.ascontiguousarray(np.tile(f(inputs["ssm_d"][0]).reshape(32, 16).T, (8, 1)))
    m["mem"] = f(inputs["mem"][b])
    for k_, n_ in (("g_mem", "g_mem"), ("g_ffn", "g_ffn")):
        m[n_] = f(inputs[k_])
    m["g_final"] = f(inputs["g_final"]).reshape(1, D)
    m["w_mem_kv"] = f(inputs["w_mem_kv"][0]); m["w_mem_out"] = f(inputs["w_mem_out"][0])
    m["w_conv_out"] = f(inputs["w_conv_out"][0]); m["w_ssm_glu"] = f(inputs["w_ssm_glu"][0])
    m["w_out"] = f(inputs["w_out"][0])
    m["w_router"] = np.ascontiguousarray(np.concatenate([f(inputs["w_router_group"][0]),
                                                         f(inputs["w_router_expert"][0])], axis=1))
    m["b_router"] = np.ascontiguousarray(np.concatenate([f(inputs["b_router_group"][0]),
                                                         f(inputs["b_router_expert"][0])])[None, :])
    m["cdw_l"] = np.ascontiguousarray(f(inputs["conv_dw"][0]).T.reshape(4, 128, 31).transpose(1, 0, 2))
    m["cb_l"] = np.ascontiguousarray(f(inputs["conv_dw_bias"][0]).reshape(4, 128).T)
    m["lng_l"] = np.ascontiguousarray(f(inputs["conv_ln_g"][0]).reshape(4, 128).T)
    m["lnb_l"] = np.ascontiguousarray(f(inputs["conv_ln_b"][0]).reshape(4, 128).T)
    if consts is not None and "w_exp_gate" in consts:
        for k_ in ("w_exp_gate", "w_exp_up", "w_exp_down"):
            m[k_] = consts[k_]
    else:
        m["w_exp_gate"] = relayout_pc(f(inputs["w_exp_gate"][0]))
        m["w_exp_up"] = relayout_pc(f(inputs["w_exp_up"][0]))
        m["w_exp_down"] = relayout_pc(f(inputs["w_exp_down"][0]))
    m.update(consts if consts is not None else host_consts())
    return m


def kernel(**inputs):
    nc = build()
    consts = host_consts()
    f32 = lambda a: np.ascontiguousarray(a, dtype=np.float32)
    for k_ in ("w_exp_gate", "w_exp_up", "w_exp_down"):
        consts[k_] = relayout_pc(f32(inputs[k_][0]))
    in_maps = [make_inmap(inputs, b, consts) for b in range(NCORES)]
    res = run_bass_kernel_spmd(nc, in_maps, core_ids=list(range(NCORES)))
    return np.stack([r["out"] for r in res.results], axis=0)
```

```python
import os
import numpy as np
import ml_dtypes
from contextlib import ExitStack
import concourse.bass as bass
import concourse.mybir as mybir
from concourse.bass_utils import run_bass_kernel_spmd

F32 = mybir.dt.float32
BF16 = mybir.dt.bfloat16
I32 = mybir.dt.int32
U32 = mybir.dt.uint32
AF = mybir.ActivationFunctionType
ALU = mybir.AluOpType
AX = mybir.AxisListType
GELU = AF.Gelu_apprx_tanh

D = 1024
SEQ = 4096
NCORES = 8
T = 512
NB = SEQ // T
NT = SEQ // 128
EPS = 1e-6
NEXP = 32
CAP = 384
NBLK = CAP // 128
ROWS = SEQ + 128


class Buf:
    __slots__ = ("name", "w", "r")

    def __init__(self, name):
        self.name = name
        self.w = {}
        self.r = {}


class Ctx:
    ENG = ("pe", "dve", "act", "pool", "sp")
    KROT = 4
    NDMA = 12

    def __init__(self, nc, es):
        self.nc = nc
        self.q = {e: [] for e in self.ENG}
        self.cnt = {e: 0 for e in self.ENG}
        self.seen = {e: {} for e in self.ENG}
        self.esem = {e: [es.enter_context(nc.semaphore(f"s_{e}{i}")) for i in range(self.KROT)]
                     for e in self.ENG}
        self.dsem = {e: [es.enter_context(nc.semaphore(f"d_{e}{i}")) for i in range(self.NDMA)]
                     for e in ("sp", "act", "pool")}
        self.dcnt = {e: [0] * self.NDMA for e in self.dsem}
        self.dnext = {e: 0 for e in self.dsem}
        self.nwait = 0
        self.waited = {e: set() for e in self.ENG}

    def _wait(self, eng, tok):
        key, val = tok
        if key[0] == 'e' and key[1] == eng and eng == "pe":
            return
        if self.seen[eng].get(key, -1) >= val:
            return
        self.seen[eng][key] = val
        if key[0] == 'e':
            self.waited[key[1]].add(val)
        self.q[eng].append(("w", key, val))
        self.nwait += 1

    def op(self, eng, fn, reads=(), writes=(), full=(), dma=False):
        toks = []
        for b in reads:
            toks.extend(b.w.items())
        for b in tuple(writes) + tuple(full):
            toks.extend(b.w.items())
            toks.extend(b.r.items())
        for t in toks:
            self._wait(eng, t)
        if dma:
            i = self.dnext[eng]
            self.dnext[eng] = (i + 1) % self.NDMA
            key = ('d', eng, i)
            if self.dcnt[eng][i] > 0:
                self._wait(eng, (key, self.dcnt[eng][i]))
            self.dcnt[eng][i] += 16
            val = self.dcnt[eng][i]
            self.q[eng].append(("d", fn, self.dsem[eng][i]))
        else:
            key = ('e', eng)
            val = self.cnt[eng]
            self.cnt[eng] += 1
            self.q[eng].append(("i", fn, val))
        for b in reads:
            b.r[key] = val
        for b in full:
            b.w = {key: val}
            b.r = {}
        for b in writes:
            b.w[key] = val
        return (key, val)

    def barrier(self, skip_pool_dma=False):
        toks = []
        for e in self.ENG:
            if skip_pool_dma and e == "pool":
                continue
            if self.cnt[e] > 0:
                toks.append((('e', e), self.cnt[e] - 1))
        for e in self.dsem:
            if skip_pool_dma and e == "pool":
                continue
            for i in range(self.NDMA):
                if self.dcnt[e][i] > 0:
                    toks.append((('d', e, i), self.dcnt[e][i]))
        for e in self.ENG:
            for t in toks:
                self._wait(e, t)

    def emit(self, block):
        nc = self.nc

        rank = {e: {v: i for i, v in enumerate(sorted(self.waited[e]))} for e in self.ENG}
        K_ = self.KROT

        def run(engname, engine):
            for item in self.q[engname]:
                if item[0] == "w":
                    key, val = item[1], item[2]
                    if key[0] == 'e':
                        r = rank[key[1]][val]
                        engine.wait_ge(self.esem[key[1]][r % K_], r // K_ + 1)
                    else:
                        engine.wait_ge(self.dsem[key[1]][key[2]], val)
                elif item[0] == "d":
                    item[1](engine).then_inc(item[2], 16)
                else:
                    ins = item[1](engine)
                    r = rank[engname].get(item[2])
                    if r is not None:
                        ins.then_inc(self.esem[engname][r % K_], 1)

        @block.tensor
        def _(e):
            run("pe", e)

        @block.vector
        def _(e):
            run("dve", e)

        @block.scalar
        def _(e):
            run("act", e)

        @block.gpsimd
        def _(e):
            run("pool", e)

        @block.sync
        def _(e):
            run("sp", e)


class TT:
    def __init__(self, t, name):
        self.t = t
        self.b = Buf(name)

    def __getitem__(self, k):
        return self.t[k]


class Alloc:
    cnt = [0]

    def __init__(self, nc, es=None):
        self.nc = nc
        self.es = es if es is not None else ExitStack()

    @property
    def n(self):
        return Alloc.cnt[0]

    @n.setter
    def n(self, v):
        Alloc.cnt[0] = v

    def close(self):
        self.es.close()

    def sb(self, shape, dt, name=None):
        self.n += 1
        name = name or f"sb{self.n}"
        t = self.es.enter_context(self.nc.sbuf_tensor(f"{name}_{self.n}", list(shape), dt))
        return TT(t, name)

    def ps(self, shape, dt, name=None):
        self.n += 1
        name = name or f"ps{self.n}"
        t = self.es.enter_context(self.nc.psum_tensor(f"{name}_{self.n}", list(shape), dt))
        return TT(t, name)


def build(stop_after="E", dbg=False):
    nc = bass.Bass("TRN2", target_bir_lowering=False)
    dram = {}

    def din(name, shape, dt=F32):
        dram[name] = nc.dram_tensor(name, list(shape), dt, kind="ExternalInput").ap()
        return dram[name]

    def dscr(name, shape, dt, kind="Internal"):
        dram[name] = nc.dram_tensor(name, list(shape), dt, kind=kind).ap()
        return dram[name]

    x_d = din("x", [SEQ, D])
    gmix_d = din("g_mix", [1, D])
    w_in_d = din("w_in", [D, 5120])
    ident_bf_d = din("ident_bf", [128, 128], BF16)
    ident_f_d = din("ident_f", [128, 128], F32)
    lamre_d = din("lamre_l", [128, 16])
    lamim_d = din("lamim_l", [128, 16])
    logdt_d = din("logdt_l", [128, 16])
    bre_d = din("bre_l", [128, 16, 16])
    bim_d = din("bim_l", [128, 16, 16])
    cre_d = din("cre_l", [128, 16, 16])
    cim_d = din("cim_l", [128, 16, 16])
    dl_d = din("d_l", [128, 32])
    psel_d = din("psel", [128, 8, 240], BF16)
    cmask_d = din("cmask", [128, 128])
    mem_d = din("mem", [256, D])
    gmem_d = din("g_mem", [1, D])
    gffn_d = din("g_ffn", [1, D])
    gfin_d = din("g_final", [1, D])
    wkv_d = din("w_mem_kv", [D, 1024])
    wmo_d = din("w_mem_out", [512, D])
    wco_d = din("w_conv_out", [512, D])
    wgl_d = din("w_ssm_glu", [512, 2048])
    wo_d = din("w_out", [D, D])
    wr_d = din("w_router", [D, 36])
    rbias_d = din("b_router", [1, 36])
    cdw_d = din("cdw_l", [128, 4, 31])
    cb_d = din("cb_l", [128, 4])
    lng_d = din("lng_l", [128, 4])
    lnb_d = din("lnb_l", [128, 4])
    tri_d = din("tri", [128, 128])
    ecap_d = din("ecap", [128, 32])
    tokid_d = din("tokid", [128, NT])
    lst_init_d = din("lst_init", [NEXP * CAP + 128, 4])
    trashp_d = din("trashp", [128, 1])
    weg_d = din("w_exp_gate", [NEXP, 128, 8, 256])
    weu_d = din("w_exp_up", [NEXP, 128, 8, 256])
    wed_d = din("w_exp_down", [NEXP, 128, 2, D])
    dk = "ExternalOutput" if dbg else "Internal"
    wbf_scr = dscr("wbf_scr", [NEXP, 3, 128, 2048], BF16)
    lst_d = dscr("lst", [NEXP * CAP + 128, 4], F32, kind=dk)
    h2_scr = dscr("h2_scr", [ROWS, D], BF16, kind=dk)
    moe_scr = dscr("moe_scr", [2 * ROWS, D], BF16, kind=dk)
    x2_scr = dscr("x2_scr", [SEQ, D], F32, kind=dk)
    ys_scr = dscr("ys_scr", [4, 128, SEQ], BF16, kind="ExternalOutput" if dbg else "Internal")
    out_d = dscr("out", [SEQ, D], F32, kind="ExternalOutput")
    hT_scr = dscr("hT_scr", [8, 128, SEQ], BF16, kind="ExternalOutput" if dbg else "Internal")
    u_dbg = dscr("u_dbg", [4, 128, SEQ], BF16, kind="ExternalOutput") if dbg else None

    with ExitStack() as es:
        cx = Ctx(nc, es)
        al = Alloc(nc, es)
        block = es.enter_context(nc.Block())

        ident_bf = al.sb([128, 128], BF16, "ident_bf")
        cx.op("sp", lambda e: e.dma_start(out=ident_bf[:], in_=ident_bf_d), full=[ident_bf.b], dma=True)
        ident_f = al.sb([128, 128], F32, "ident_f")
        cx.op("sp", lambda e: e.dma_start(out=ident_f[:], in_=ident_f_d), full=[ident_f.b], dma=True)

        psum = [al.ps([128, 512], F32, f"bank{i}") for i in range(6)]
        psb = [al.ps([128, 1024], BF16, f"bankb{i}") for i in range(2)]
        pctr = [0]

        def getps():
            p = psum[pctr[0] % len(psum)]
            pctr[0] += 1
            return p

        alAB = Alloc(nc)
        u_all = alAB.sb([128, 4, SEQ], BF16, "u_all")
        M_all = alAB.sb([128, 32, 128], BF16, "M_all")
        W2r = alAB.sb([128, 16, 2, 128], BF16, "W2r"); W2i = alAB.sb([128, 16, 2, 128], BF16, "W2i")
        C1r = alAB.sb([128, 16, 128], BF16, "C1r"); nC1i = alAB.sb([128, 16, 128], BF16, "nC1i")
        KAr = alAB.sb([128, 9, 16], F32, "KAr"); KAi = alAB.sb([128, 9, 16], F32, "KAi")
        KnAi = alAB.sb([128, 9, 16], F32, "KnAi")
        psel = alAB.sb([128, 8, 240], BF16, "psel")
        zt = alAB.sb([128, 1024], F32, "zt")
        NPB = 3
        pst = [alAB.sb([128, 2048], F32, f"pst{i}") for i in range(NPB)]
        pbf = [alAB.sb([128, 2048], BF16, f"pbf{i}") for i in range(NPB)]
        wbfB = Buf("wbf")
        pc_next = [0]

        def precast(n, mode):
            for _ in range(n):
                ci = pc_next[0]
                if ci >= NEXP * 3:
                    return
                pc_next[0] += 1
                e_, m_ = ci // 3, ci % 3
                srcw = (weg_d, weu_d, wed_d)[m_][e_].rearrange("p c n -> p (c n)")
                s_ = pst[ci % NPB]; b_ = pbf[ci % NPB]
                dst = wbf_scr[e_, m_]
                if mode == "pool":
                    cx.op("pool", lambda e, s_=s_, srcw=srcw: e.dma_start(out=s_[:], in_=srcw), full=[s_.b], dma=True)
                    cx.op("pool", lambda e, s_=s_, b_=b_: e.tensor_copy(out=b_[:], in_=s_[:]), reads=[s_.b], full=[b_.b])
                    cx.op("pool", lambda e, b_=b_, dst=dst: e.dma_start(out=dst, in_=b_[:]), reads=[b_.b], dma=True)
                else:
                    cx.op("sp", lambda e, s_=s_, srcw=srcw: e.dma_start(out=s_[:], in_=srcw), full=[s_.b], dma=True)
                    cx.op("act", lambda e, s_=s_, b_=b_: e.copy(out=b_[:], in_=s_[:]), reads=[s_.b], full=[b_.b])
                    cx.op("act", lambda e, b_=b_, dst=dst: e.dma_start(out=dst, in_=b_[:]), reads=[b_.b], dma=True)
        cx.op("sp", lambda e: e.dma_start(out=psel[:], in_=psel_d), full=[psel.b], dma=True)
        al_outer = al
        al = Alloc(nc)
        gmix = al.sb([128, D], F32, "gmix")
        cx.op("sp", lambda e: e.dma_start(out=gmix[:], in_=gmix_d.partition_broadcast(128)),
              full=[gmix.b], dma=True)

        stg = [al.sb([128, 8, 256], F32, f"stg{i}") for i in range(2)]
        sctr = [0]

        def load_cast(dst, dst_col0, src_d, c0, c1, kch):
            for cc in range(c0, c1, 256):
                w = min(256, c1 - cc)
                s = stg[sctr[0] % 2]
                sctr[0] += 1
                src = src_d[:, cc:cc + w].rearrange("(c p) n -> p c n", p=128)
                cx.op("sp", lambda e, s=s, src=src, w=w: e.dma_start(out=s[:, 0:kch, 0:w], in_=src),
                      full=[s.b], dma=True)
                o = dst_col0 + (cc - c0)
                cx.op("pool", lambda e, s=s, o=o, w=w: e.tensor_copy(out=dst[:, 0:kch, o:o + w],
                                                                     in_=s[:, 0:kch, 0:w]),
                      reads=[s.b], writes=[dst.b])

        w_ssm_in = al.sb([128, 8, 512], BF16, "w_ssm_in")
        load_cast(w_ssm_in, 0, w_in_d, 1024, 1536, 8)
        NA_ = 4
        xt = [al.sb([128, D], F32, f"xt{i}") for i in range(NA_)]
        junk = al.sb([128, D], BF16, "junk")
        ss = [al.sb([128, 1], F32, f"ss{i}") for i in range(NA_)]
        rt = [al.sb([128, 1], F32, f"rt{i}") for i in range(NA_)]
        rstd = [al.sb([128, 1], F32, f"rstd{i}") for i in range(NA_)]
        hbf = [al.sb([128, D], BF16, f"hbf{i}") for i in range(NA_)]
        hTb = [al.sb([128, 8, T], BF16, f"hTb{i}") for i in range(2)]
        cx.op("pool", lambda e: e.memset(zt[:], 0.0), full=[zt.b])
        lstB = Buf("lst"); h2B = Buf("h2scr"); moeB = Buf("moescr"); x2B = Buf("x2scr")
        cx.op("pool", lambda e: e.dma_start(out=lst_d, in_=lst_init_d), full=[lstB], dma=True)
        cx.op("pool", lambda e: e.dma_start(out=h2_scr[SEQ:ROWS, :], in_=zt[:, 0:512].bitcast(BF16)),
              reads=[zt.b], writes=[h2B], dma=True)
        moe_flat = moe_scr.rearrange("(n p) d -> n p d", p=128)
        for n in range(0, 2 * ROWS // 128):
            tok = cx.op("pool", lambda e, n=n: e.dma_start(out=moe_flat[n], in_=zt[:, 0:512].bitcast(BF16)),
                        reads=[zt.b], dma=True)
            moeB.w[tok[0]] = tok[1]

        def a_front(i):
            p = i % NA_
            cx.op("sp", lambda e, p=p, i=i: e.dma_start(out=xt[p][:], in_=x_d[i * 128:(i + 1) * 128, :]),
                  full=[xt[p].b], dma=True)
            cx.op("act", lambda e, p=p: e.activation(out=junk[:], in_=xt[p][:], func=AF.Square,
                                                     accum_out=ss[p][:]),
                  reads=[xt[p].b], writes=[junk.b], full=[ss[p].b])
            cx.op("act", lambda e, p=p: e.activation(out=rt[p][:], in_=ss[p][:], func=AF.Sqrt,
                                                     scale=1.0 / D, bias=EPS),
                  reads=[ss[p].b], full=[rt[p].b])
            cx.op("dve", lambda e, p=p: e.reciprocal(out=rstd[p][:], in_=rt[p][:]),
                  reads=[rt[p].b], full=[rstd[p].b])
            cx.op("dve", lambda e, p=p: e.scalar_tensor_tensor(out=hbf[p][:], in0=xt[p][:],
                                                               scalar=rstd[p][:, 0:1], in1=gmix[:],
                                                               op0=ALU.mult, op1=ALU.mult),
                  reads=[xt[p].b, rstd[p].b, gmix.b], full=[hbf[p].b])

        def a_back(i):
            p = i % NA_
            blk = i // 4
            hb = hTb[blk % 2]
            pb = psb[i % 2]
            for c in range(8):
                cx.op("pe", lambda e, pb=pb, p=p, c=c: e.transpose(out=pb[:, c * 128:(c + 1) * 128],
                                                                   in_=hbf[p][:, c * 128:(c + 1) * 128],
                                                                   identity=ident_bf[:]),
                      reads=[hbf[p].b, ident_bf.b], writes=[pb.b])
            tt = i % 4
            cx.op("act", lambda e, pb=pb, hb=hb, tt=tt: e.copy(
                out=hb[:, :, tt * 128:(tt + 1) * 128],
                in_=pb[:].rearrange("p (c t) -> p c t", c=8)),
                reads=[pb.b], writes=[hb.b])
            if tt == 3:
                for f in range(4):
                    ps = getps()
                    for c in range(8):
                        cx.op("pe", lambda e, ps=ps, hb=hb, f=f, c=c: e.matmul(
                            ps[:], lhsT=w_ssm_in[:, c, f * 128:(f + 1) * 128], rhs=hb[:, c, :],
                            start=(c == 0), stop=(c == 7)),
                            reads=[w_ssm_in.b, hb.b], writes=[ps.b])
                    cx.op("dve", lambda e, ps=ps, f=f, blk=blk: e.tensor_copy(
                        out=u_all[:, f, blk * T:(blk + 1) * T], in_=ps[:]),
                        reads=[ps.b], writes=[u_all.b])
                cx.op("act", lambda e, hb=hb, blk=blk: e.dma_start(
                    out=hT_scr[:, :, blk * T:(blk + 1) * T].rearrange("c p t -> p c t"), in_=hb[:]),
                    reads=[hb.b], dma=True)

        a_front(0); a_front(1)
        for i in range(NT):
            if i + 2 < NT:
                a_front(i + 2)
            a_back(i)
            if i % 3 == 2:
                precast(1, "pool")

        if dbg:
            cx.op("sp", lambda e: e.dma_start(out=u_dbg.rearrange("f p t -> p f t"), in_=u_all[:]),
                  reads=[u_all.b], dma=True)


        cx.barrier(skip_pool_dma=True)
        al.close()
        precast(8, "pool")
        al = Alloc(nc)
        TWO_PI = 2.0 * np.pi
        cmask = al.sb([128, 128], F32, "cmask")
        cx.op("sp", lambda e: e.dma_start(out=cmask[:], in_=cmask_d), full=[cmask.b], dma=True)
        dl = al.sb([128, 32], F32, "dl")
        cx.op("sp", lambda e: e.dma_start(out=dl[:], in_=dl_d), full=[dl.b], dma=True)
        SU = Buf("ssm_setup")

        def sload(shape, src, name):
            t = al.sb(shape, F32, name)
            cx.op("sp", lambda e: e.dma_start(out=t[:], in_=src), full=[t.b], dma=True)
            return t

        lamre = sload([128, 16], lamre_d, "lamre")
        lamim = sload([128, 16], lamim_d, "lamim")
        logdt = sload([128, 16], logdt_d, "logdt")
        Bre = sload([128, 16, 16], bre_d, "Bre")
        Bim = sload([128, 16, 16], bim_d, "Bim")
        Cre = sload([128, 16, 16], cre_d, "Cre")
        Cim = sload([128, 16, 16], cim_d, "Cim")
        ins_b = [lamre.b, lamim.b, logdt.b, Bre.b, Bim.b, Cre.b, Cim.b]

        def S(shape, name):
            return al.sb(shape, F32, name)

        def dv(fn):
            cx.op("dve", fn, reads=ins_b, writes=[SU])

        def ac(fn):
            cx.op("act", fn, reads=ins_b, writes=[SU])

        def tt_(out, a, b, op):
            dv(lambda e: e.tensor_tensor(out=out, in0=a, in1=b, op=op))

        sh16 = [128, 16]
        dt_ = S(sh16, "dt"); lrd = S(sh16, "lrd"); th = S(sh16, "th")
        ac(lambda e: e.activation(out=dt_[:], in_=logdt[:], func=AF.Exp))
        tt_(lrd[:], lamre[:], dt_[:], ALU.mult)
        tt_(th[:], lamim[:], dt_[:], ALU.mult)
        mag = S(sh16, "mag"); imag2 = S(sh16, "imag2")
        ac(lambda e: e.activation(out=mag[:], in_=lrd[:], func=AF.Exp))
        ac(lambda e: e.activation(out=imag2[:], in_=lrd[:], func=AF.Exp, scale=-2.0))
        kq_i = al.sb(sh16, I32, "kq_i"); kq = S(sh16, "kq"); red = S(sh16, "red"); msk = S(sh16, "msk")
        sinv = S(sh16, "sinv"); cosv = S(sh16, "cosv"); tmpa = S(sh16, "tmpa")

        def sin_of(outt, shift):
            dv(lambda e: e.tensor_scalar(out=tmpa[:], in0=th[:], scalar1=float(shift), scalar2=None,
                                         op0=ALU.add))
            dv(lambda e: e.tensor_scalar(out=kq[:], in0=tmpa[:], scalar1=float(1.0 / TWO_PI),
                                         scalar2=None, op0=ALU.mult))
            dv(lambda e: e.tensor_copy(out=kq_i[:], in_=kq[:]))
            dv(lambda e: e.tensor_copy(out=kq[:], in_=kq_i[:]))
            dv(lambda e: e.scalar_tensor_tensor(out=red[:], in0=kq[:], scalar=float(-TWO_PI),
                                                in1=tmpa[:], op0=ALU.mult, op1=ALU.add))
            dv(lambda e: e.tensor_single_scalar(out=msk[:], in_=red[:], scalar=float(np.pi), op=ALU.is_gt))
            dv(lambda e: e.scalar_tensor_tensor(out=red[:], in0=msk[:], scalar=float(-TWO_PI),
                                                in1=red[:], op0=ALU.mult, op1=ALU.add))
            dv(lambda e: e.tensor_single_scalar(out=msk[:], in_=red[:], scalar=float(-np.pi), op=ALU.is_lt))
            dv(lambda e: e.scalar_tensor_tensor(out=red[:], in0=msk[:], scalar=float(TWO_PI),
                                                in1=red[:], op0=ALU.mult, op1=ALU.add))
            ac(lambda e: e.activation(out=outt[:], in_=red[:], func=AF.Sin))

        sin_of(sinv, 0.0)
        sin_of(cosv, np.pi / 2)
        PWr = S([128, 9, 16], "PWr"); PWi = S([128, 9, 16], "PWi")
        IPr = S([128, 8, 16], "IPr"); IPi = S([128, 8, 16], "IPi")
        t1 = S([128, 16, 8, 16], "t1"); t2 = S([128, 16, 8, 16], "t2")

        def cmul(outr, outi, ar, ai, br, bi, shp, neg_i=False):
            a1 = t1[:].rearrange("p a b c -> p (a b c)")[:, 0:int(np.prod(shp[1:]))]
            a2 = t2[:].rearrange("p a b c -> p (a b c)")[:, 0:int(np.prod(shp[1:]))]
            if len(shp) == 3:
                a1 = a1.rearrange("p (a b) -> p a b", a=shp[1])
                a2 = a2.rearrange("p (a b) -> p a b", a=shp[1])
            if len(shp) == 4:
                a1 = t1[:, :, 0:shp[2], :]
                a2 = t2[:, :, 0:shp[2], :]
            tt_(a1, ar, br, ALU.mult)
            tt_(a2, ai, bi, ALU.mult)
            tt_(outr, a1, a2, ALU.subtract)
            tt_(a1, ar, bi, ALU.mult)
            tt_(a2, ai, br, ALU.mult)
            if neg_i:
                dv(lambda e: e.scalar_tensor_tensor(out=outi, in0=a1, scalar=-1.0, in1=a2,
                                                    op0=ALU.mult, op1=ALU.subtract))
            else:
                tt_(outi, a1, a2, ALU.add)

        dv(lambda e: e.memset(PWr[:, 0, :], 1.0))
        dv(lambda e: e.memset(PWi[:, 0, :], 0.0))
        dv(lambda e: e.memset(IPr[:, 0, :], 1.0))
        dv(lambda e: e.memset(IPi[:, 0, :], 0.0))
        tt_(PWr[:, 1, :], mag[:], cosv[:], ALU.mult)
        tt_(PWi[:, 1, :], mag[:], sinv[:], ALU.mult)
        tt_(IPr[:, 1, :], PWr[:, 1, :], imag2[:], ALU.mult)
        dv(lambda e: e.scalar_tensor_tensor(out=IPi[:, 1, :], in0=PWi[:, 1, :], scalar=-1.0, in1=imag2[:],
                                            op0=ALU.mult, op1=ALU.mult))
        for n in range(2, 9):
            cmul(PWr[:, n, :], PWi[:, n, :], PWr[:, n - 1, :], PWi[:, n - 1, :], PWr[:, 1, :], PWi[:, 1, :], sh16)
        for n in range(2, 8):
            cmul(IPr[:, n, :], IPi[:, n, :], IPr[:, n - 1, :], IPi[:, n - 1, :], IPr[:, 1, :], IPi[:, 1, :], sh16)
        dv(lambda e: e.tensor_copy(out=KAr[:, 0, :], in_=PWr[:, 8, :]))
        dv(lambda e: e.tensor_copy(out=KAi[:, 0, :], in_=PWi[:, 8, :]))
        for d_ in range(1, 9):
            cmul(KAr[:, d_, :], KAi[:, d_, :], KAr[:, d_ - 1, :], KAi[:, d_ - 1, :],
                 KAr[:, d_ - 1, :], KAi[:, d_ - 1, :], sh16)
        dv(lambda e: e.tensor_scalar(out=KnAi[:], in0=KAi[:], scalar1=-1.0, scalar2=None, op0=ALU.mult))
        am1 = S(sh16, "am1"); l2 = S(sh16, "l2"); il2 = S(sh16, "il2"); kr = S(sh16, "kr"); ki = S(sh16, "ki")
        dv(lambda e: e.tensor_scalar(out=am1[:], in0=PWr[:, 1, :], scalar1=-1.0, scalar2=None, op0=ALU.add))
        tt_(l2[:], lamre[:], lamre[:], ALU.mult)
        tt_(tmpa[:], lamim[:], lamim[:], ALU.mult)
        tt_(l2[:], l2[:], tmpa[:], ALU.add)
        dv(lambda e: e.reciprocal(out=il2[:], in_=l2[:]))
        tt_(kr[:], am1[:], lamre[:], ALU.mult)
        tt_(tmpa[:], PWi[:, 1, :], lamim[:], ALU.mult)
        tt_(kr[:], kr[:], tmpa[:], ALU.add)
        tt_(kr[:], kr[:], il2[:], ALU.mult)
        tt_(ki[:], PWi[:, 1, :], lamre[:], ALU.mult)
        tt_(tmpa[:], am1[:], lamim[:], ALU.mult)
        tt_(ki[:], ki[:], tmpa[:], ALU.subtract)
        tt_(ki[:], ki[:], il2[:], ALU.mult)
        sh3 = [128, 16, 16]

        def bc(a):
            return a.unsqueeze(2).to_broadcast(sh3)

        Bbr = S(sh3, "Bbr"); Bbi = S(sh3, "Bbi")
        cmul(Bbr[:], Bbi[:], bc(kr[:]), bc(ki[:]), Bre[:], Bim[:], sh3)
        Bhr = S([128, 16, 8, 16], "Bhr"); nBhi = S([128, 16, 8, 16], "nBhi"); Bhi = S([128, 16, 8, 16], "Bhi")
        Btr = S([128, 16, 8, 16], "Btr"); Bti = S([128, 16, 8, 16], "Bti")
        Chr = S([128, 16, 9, 16], "Chr"); Chi = S([128, 16, 9, 16], "Chi"); nChi = S([128, 16, 9, 16], "nChi")
        sh4 = [128, 16, 8, 16]

        def bk(a):
            return a.rearrange("p k r -> p r k").unsqueeze(3).to_broadcast(sh4)

        def bmid(a):
            return a.unsqueeze(2).to_broadcast(sh4)

        def b2(a):
            return a.unsqueeze(2).unsqueeze(3).to_broadcast(sh4)

        cmul(Bhr[:], Bhi[:], bk(IPr[:]), bk(IPi[:]), bmid(Bbr[:]), bmid(Bbi[:]), sh4)
        cmul(Btr[:], Bti[:], b2(PWr[:, 7, :]), b2(PWi[:, 7, :]), Bhr[:], Bhi[:], sh4)
        dv(lambda e: e.tensor_scalar(out=nBhi[:], in0=Bhi[:], scalar1=-1.0, scalar2=None, op0=ALU.mult))
        cmul(Chr[:, :, 0:8, :], Chi[:, :, 0:8, :], bk(PWr[:, 0:8, :]), bk(PWi[:, 0:8, :]), bmid(Cre[:]), bmid(Cim[:]), sh4)
        cmul(Chr[:, :, 8, :], Chi[:, :, 8, :], bc(PWr[:, 8, :]), bc(PWi[:, 8, :]), Cre[:], Cim[:], sh3)
        dv(lambda e: e.tensor_scalar(out=nChi[:], in0=Chi[:], scalar1=-1.0, scalar2=None, op0=ALU.mult))
        dv(lambda e: e.tensor_copy(out=C1r[:].rearrange("p r (j c) -> p r j c", j=8), in_=Chr[:, :, 1:9, :]))
        dv(lambda e: e.tensor_copy(out=nC1i[:].rearrange("p r (j c) -> p r j c", j=8), in_=nChi[:, :, 1:9, :]))
        mtmp = S([128, 128], "mtmp")
        cx.op("dve", lambda e: e.memset(W2r[:], 0.0), reads=ins_b, writes=[SU])
        cx.op("dve", lambda e: e.memset(W2i[:], 0.0), reads=ins_b, writes=[SU])
        for r in range(16):
            for two in range(2):
                g = 2 * r + two
                rng = slice(two * 64, (two + 1) * 64)
                ps = getps()
                cx.op("pe", lambda e, ps=ps, r=r, rng=rng: e.matmul(
                    ps[:, 0:128], lhsT=Bhr[rng, r, :, :].rearrange("p k c -> p (k c)"),
                    rhs=Chr[rng, r, 0:8, :].rearrange("p j c -> p (j c)"), start=True, stop=False),
                    reads=[SU], writes=[ps.b])
                cx.op("pe", lambda e, ps=ps, r=r, rng=rng: e.matmul(
                    ps[:, 0:128], lhsT=nBhi[rng, r, :, :].rearrange("p k c -> p (k c)"),
                    rhs=Chi[rng, r, 0:8, :].rearrange("p j c -> p (j c)"), start=False, stop=True),
                    reads=[SU], writes=[ps.b])
                cx.op("dve", lambda e, ps=ps: e.tensor_tensor(out=mtmp[:], in0=ps[:, 0:128], in1=cmask[:],
                                                              op=ALU.mult),
                      reads=[ps.b, cmask.b], writes=[SU])
                cx.op("dve", lambda e, g=g: e.scalar_tensor_tensor(
                    out=M_all[:, g, :], in0=ident_f[:], scalar=dl[:, g:g + 1], in1=mtmp[:],
                    op0=ALU.mult, op1=ALU.add),
                    reads=[ident_f.b, dl.b], writes=[SU, M_all.b])
            for (Bt, W2) in ((Btr, W2r), (Bti, W2i)):
                ps = getps()
                cx.op("pe", lambda e, ps=ps, r=r, Bt=Bt: e.transpose(
                    out=ps[:, 0:128], in_=Bt[:, r, :, :].rearrange("p k c -> p (k c)"), identity=ident_f[:]),
                    reads=[SU, ident_f.b], writes=[ps.b])
                cx.op("dve", lambda e, ps=ps, r=r, W2=W2: e.tensor_copy(out=W2[:, r, 0, 0:64], in_=ps[:, 0:64]),
                      reads=[ps.b], writes=[SU, W2.b])
                cx.op("dve", lambda e, ps=ps, r=r, W2=W2: e.tensor_copy(out=W2[:, r, 1, 64:128], in_=ps[:, 64:128]),
                      reads=[ps.b], writes=[SU, W2.b])

        cx.barrier(skip_pool_dma=True)
        al.close()
        al = Alloc(nc)
        NCH = SEQ // 8
        Vg = [al.sb([128, NCH], BF16, f"Vg{i}") for i in range(4)]
        Sre = [[al.sb([128, NCH], F32, f"Sre{s}{i}") for i in range(2)] for s in range(2)]
        Sim = [[al.sb([128, NCH], F32, f"Sim{s}{i}") for i in range(2)] for s in range(2)]
        Sbr = [al.sb([128, NCH], BF16, f"Sbr{s}") for s in range(2)]
        Sbi = [al.sb([128, NCH], BF16, f"Sbi{s}") for s in range(2)]
        Gg = [al.sb([128, NCH], BF16, f"Gg{i}") for i in range(16)]
        ysf = [al.sb([128, SEQ], BF16, f"ysf{i}") for i in range(2)]
        for s in range(2):
            cx.op("pool", lambda e, s=s: e.memset(Sbr[s][:, 0:1], 0.0), writes=[Sbr[s].b])
            cx.op("pool", lambda e, s=s: e.memset(Sbi[s][:, 0:1], 0.0), writes=[Sbi[s].b])

        def b_front(r):
            f = r // 4
            st = r % 2
            vg = [Vg[(2 * r) % 4], Vg[(2 * r + 1) % 4]]
            for two in range(2):
                g = 2 * r + two
                gl = g % 8
                ps = getps()
                for k in range(8):
                    cx.op("pe", lambda e, ps=ps, gl=gl, k=k, f=f: e.matmul(
                        ps[:], lhsT=psel[:, gl, (7 - k) * 16:(7 - k) * 16 + 128],
                        rhs=u_all[:, f, k:SEQ:8], start=(k == 0), stop=(k == 7)),
                        reads=[psel.b, u_all.b], writes=[ps.b])
                cx.op("act", lambda e, ps=ps, v=vg[two]: e.copy(out=v[:], in_=ps[:]),
                      reads=[ps.b], full=[vg[two].b])
            psr = getps(); psi = getps()
            for (pp, W2) in ((psr, W2r), (psi, W2i)):
                for two in range(2):
                    cx.op("pe", lambda e, pp=pp, W2=W2, two=two, r=r, v=vg[two]: e.matmul(
                        pp[:], lhsT=W2[:, r, two, :], rhs=v[:], start=(two == 0), stop=(two == 1)),
                        reads=[W2.b, vg[two].b], writes=[pp.b])
            cx.op("act", lambda e, psr=psr, st=st: e.copy(out=Sre[st][0][:], in_=psr[:]),
                  reads=[psr.b], full=[Sre[st][0].b])
            cx.op("act", lambda e, psi=psi, st=st: e.copy(out=Sim[st][0][:], in_=psi[:]),
                  reads=[psi.b], full=[Sim[st][0].b])

        def b_mid(r):
            st = r % 2
            cur = 0
            for d_ in range(9):
                sh = 1 << d_
                s_r, s_i, d_r, d_i = Sre[st][cur], Sim[st][cur], Sre[st][1 - cur], Sim[st][1 - cur]
                n = NCH - sh
                cx.op("dve", lambda e, s_r=s_r, d_r=d_r, sh=sh, n=n, d_=d_, r=r: e.scalar_tensor_tensor(
                    out=d_r[:, sh:NCH], in0=s_r[:, 0:n], scalar=KAr[:, d_, r:r + 1], in1=s_r[:, sh:NCH],
                    op0=ALU.mult, op1=ALU.add), reads=[s_r.b, SU], writes=[d_r.b])
                cx.op("dve", lambda e, s_i=s_i, d_r=d_r, sh=sh, n=n, d_=d_, r=r: e.scalar_tensor_tensor(
                    out=d_r[:, sh:NCH], in0=s_i[:, 0:n], scalar=KnAi[:, d_, r:r + 1], in1=d_r[:, sh:NCH],
                    op0=ALU.mult, op1=ALU.add), reads=[s_i.b, SU], writes=[d_r.b])
                cx.op("dve", lambda e, s_i=s_i, d_i=d_i, sh=sh, n=n, d_=d_, r=r: e.scalar_tensor_tensor(
                    out=d_i[:, sh:NCH], in0=s_i[:, 0:n], scalar=KAr[:, d_, r:r + 1], in1=s_i[:, sh:NCH],
                    op0=ALU.mult, op1=ALU.add), reads=[s_i.b, SU], writes=[d_i.b])
                cx.op("dve", lambda e, s_r=s_r, d_i=d_i, sh=sh, n=n, d_=d_, r=r: e.scalar_tensor_tensor(
                    out=d_i[:, sh:NCH], in0=s_r[:, 0:n], scalar=KAi[:, d_, r:r + 1], in1=d_i[:, sh:NCH],
                    op0=ALU.mult, op1=ALU.add), reads=[s_r.b, SU], writes=[d_i.b])
                cx.op("pool", lambda e, s_r=s_r, d_r=d_r, sh=sh: e.tensor_copy(out=d_r[:, 0:sh], in_=s_r[:, 0:sh]),
                      reads=[s_r.b], writes=[d_r.b])
                cx.op("pool", lambda e, s_i=s_i, d_i=d_i, sh=sh: e.tensor_copy(out=d_i[:, 0:sh], in_=s_i[:, 0:sh]),
                      reads=[s_i.b], writes=[d_i.b])
                cur = 1 - cur
            fr, fi = Sre[st][cur], Sim[st][cur]
            cx.op("pool", lambda e, fr=fr, st=st: e.tensor_copy(out=Sbr[st][:, 1:NCH], in_=fr[:, 0:NCH - 1]),
                  reads=[fr.b], writes=[Sbr[st].b])
            cx.op("pool", lambda e, fi=fi, st=st: e.tensor_copy(out=Sbi[st][:, 1:NCH], in_=fi[:, 0:NCH - 1]),
                  reads=[fi.b], writes=[Sbi[st].b])

        def b_back(r):
            f = r // 4
            st = r % 2
            vg = [Vg[(2 * r) % 4], Vg[(2 * r + 1) % 4]]
            for two in range(2):
                g = 2 * r + two
                rng = slice(two * 64, (two + 1) * 64)
                ps = getps()
                cx.op("pe", lambda e, ps=ps, g=g, v=vg[two]: e.matmul(
                    ps[:], lhsT=M_all[:, g, :], rhs=v[:], start=True, stop=False),
                    reads=[M_all.b, vg[two].b], writes=[ps.b])
                cx.op("pe", lambda e, ps=ps, r=r, rng=rng, st=st: e.matmul(
                    ps[:], lhsT=C1r[rng, r, :], rhs=Sbr[st][rng, :], start=False, stop=False),
                    reads=[SU, Sbr[st].b], writes=[ps.b])
                cx.op("pe", lambda e, ps=ps, r=r, rng=rng, st=st: e.matmul(
                    ps[:], lhsT=nC1i[rng, r, :], rhs=Sbi[st][rng, :], start=False, stop=True),
                    reads=[SU, Sbi[st].b], writes=[ps.b])
                gg = Gg[g % 16]
                cx.op("act", lambda e, ps=ps, gg=gg: e.activation(out=gg[:], in_=ps[:], func=GELU),
                      reads=[ps.b], full=[gg.b])
            if r % 4 == 3:
                yb = ysf[f % 2]
                for j in range(8):
                    ps = getps()
                    for gl in range(8):
                        gg = Gg[(8 * f + gl) % 16]
                        cx.op("pe", lambda e, ps=ps, j=j, gl=gl, gg=gg: e.matmul(
                            ps[:], lhsT=psel[:, j, (7 - gl) * 16:(7 - gl) * 16 + 128], rhs=gg[:],
                            start=(gl == 0), stop=(gl == 7)),
                            reads=[psel.b, gg.b], writes=[ps.b])
                    cx.op("act", lambda e, ps=ps, yb=yb, j=j: e.copy(out=yb[:, j:SEQ:8], in_=ps[:]),
                          reads=[ps.b], writes=[yb.b])
                cx.op("sp", lambda e, yb=yb, f=f: e.dma_start(out=ys_scr[f], in_=yb[:]),
                      reads=[yb.b], dma=True)

        b_front(0)
        for r in range(16):
            precast(2, "act")
            b_mid(r)
            precast(2, "act")
            if r + 1 < 16:
                b_front(r + 1)
            precast(1, "act")
            b_back(r)
        precast(NEXP * 3, "act")

        cx.barrier()
        al.close()
        alAB.close()
        al = al_outer
        if stop_after in ("A", "B"):
            pass
        else:
            TC = 256
            NBC = SEQ // TC
            alC = Alloc(nc)
            wA = alC.sb([128, 8, 1536], BF16, "wA")
            wG = alC.sb([128, 8, 3072], BF16, "wG")
            wco = alC.sb([128, 4, 1024], BF16, "wco")
            wgl = alC.sb([128, 4, 2048], BF16, "wgl")
            wmo = alC.sb([128, 4, 1024], BF16, "wmo")
            wo = alC.sb([128, 8, 1024], BF16, "wo")
            Dg2 = [alC.sb([128, 31, 128], BF16, f"Dg{i}") for i in range(2)]
            kT = alC.sb([128, 4, 256], BF16, "kT")
            vtok = alC.sb([128, 2, 512], BF16, "vtok")
            gffn = alC.sb([128, D], F32, "gffn")
            wr = alC.sb([128, 8, 36], F32, "wr")
            rbias = alC.sb([128, 36], F32, "rbias")
            cdw = alC.sb([128, 4, 31], F32, "cdw")
            cb = alC.sb([128, 4], F32, "cb"); lng = alC.sb([128, 4], F32, "lng"); lnb = alC.sb([128, 4], F32, "lnb")
            onesm = alC.sb([128, 128], F32, "onesm")
            ones_bf = alC.sb([128, 128], BF16, "ones_bf")
            ecap = alC.sb([128, 32], F32, "ecap")
            tokid = alC.sb([128, NT], F32, "tokid")
            cum = alC.sb([128, 32], F32, "cum")
            lg_all = alC.sb([128, NT, 36], F32, "lg_all")
            trashp = alC.sb([128, 1], F32, "trashp")

            def ld(t, src):
                cx.op("sp", lambda e: e.dma_start(out=t[:], in_=src), full=[t.b], dma=True)

            ld(gffn, gffn_d.partition_broadcast(128))
            ld(wr, wr_d.rearrange("(c p) n -> p c n", p=128))
            ld(rbias, rbias_d.partition_broadcast(128))
            ld(cdw, cdw_d); ld(cb, cb_d); ld(lng, lng_d); ld(lnb, lnb_d)
            ld(ecap, ecap_d); ld(tokid, tokid_d); ld(trashp, trashp_d)
            cx.op("pool", lambda e: e.memset(onesm[:], 1.0 / 512.0), full=[onesm.b])
            cx.op("pool", lambda e: e.memset(ones_bf[:], 1.0), full=[ones_bf.b])
            cx.op("pool", lambda e: e.memset(cum[:], 0.0), full=[cum.b])
            alS = Alloc(nc)
            stg2 = [alS.sb([128, 8, 256], F32, f"stgc{i}") for i in range(2)]
            s2 = [0]

            def load_cast2(dst, dst_col0, src_d, c0, c1, kch, engs=("pool", "act")):
                for cc in range(c0, c1, 256):
                    w = min(256, c1 - cc)
                    s = stg2[s2[0] % 2]
                    eng = engs[s2[0] % len(engs)]
                    s2[0] += 1
                    src = src_d[:, cc:cc + w].rearrange("(c p) n -> p c n", p=128)
                    cx.op("sp", lambda e, s=s, src=src, w=w: e.dma_start(out=s[:, 0:kch, 0:w], in_=src),
                          full=[s.b], dma=True)
                    o = dst_col0 + (cc - c0)
                    if eng == "act":
                        cx.op("act", lambda e, s=s, o=o, w=w: e.copy(out=dst[:, 0:kch, o:o + w], in_=s[:, 0:kch, 0:w]),
                              reads=[s.b], writes=[dst.b])
                    else:
                        cx.op(eng, lambda e, s=s, o=o, w=w: e.tensor_copy(out=dst[:, 0:kch, o:o + w],
                                                                          in_=s[:, 0:kch, 0:w]),
                              reads=[s.b], writes=[dst.b])

            load_cast2(wA, 0, w_in_d, 0, 1024, 8)
            load_cast2(wA, 1024, w_in_d, 1536, 2048, 8)
            load_cast2(wG, 0, w_in_d, 2048, 5120, 8)
            load_cast2(wco, 0, wco_d, 0, 1024, 4)
            load_cast2(wgl, 0, wgl_d, 0, 2048, 4)
            load_cast2(wmo, 0, wmo_d, 0, 1024, 4)
            load_cast2(wo, 0, wo_d, 0, 1024, 8)
            wkv = alS.sb([128, 8, 1024], BF16, "wkv")
            load_cast2(wkv, 0, wkv_d, 0, 1024, 8)
            gmem = alS.sb([128, D], F32, "gmem")
            ld(gmem, gmem_d.partition_broadcast(128))
            memT = alS.sb([128, 8, 256], BF16, "memT")
            mx = alS.sb([128, D], F32, "mx"); mjunk = alS.sb([128, D], BF16, "mjunk")
            mss = alS.sb([128, 1], F32, "mss"); mrt = alS.sb([128, 1], F32, "mrt"); mrs = alS.sb([128, 1], F32, "mrs")
            mh = alS.sb([128, D], BF16, "mh")
            for mt in range(2):
                cx.op("sp", lambda e, mt=mt: e.dma_start(out=mx[:], in_=mem_d[mt * 128:(mt + 1) * 128, :]),
                      full=[mx.b], dma=True)
                cx.op("act", lambda e: e.activation(out=mjunk[:], in_=mx[:], func=AF.Square, accum_out=mss[:]),
                      reads=[mx.b], full=[mjunk.b, mss.b])
                cx.op("act", lambda e: e.activation(out=mrt[:], in_=mss[:], func=AF.Sqrt, scale=1.0 / D, bias=EPS),
                      reads=[mss.b], full=[mrt.b])
                cx.op("dve", lambda e: e.reciprocal(out=mrs[:], in_=mrt[:]), reads=[mrt.b], full=[mrs.b])
                cx.op("dve", lambda e: e.scalar_tensor_tensor(out=mh[:], in0=mx[:], scalar=mrs[:, 0:1], in1=gmem[:],
                                                              op0=ALU.mult, op1=ALU.mult),
                      reads=[mx.b, mrs.b, gmem.b], full=[mh.b])
                pb = psb[mt % 2]
                for c in range(8):
                    cx.op("pe", lambda e, pb=pb, c=c: e.transpose(out=pb[:, c * 128:(c + 1) * 128],
                                                                  in_=mh[:, c * 128:(c + 1) * 128],
                                                                  identity=ident_bf[:]),
                          reads=[mh.b, ident_bf.b], writes=[pb.b])
                cx.op("act", lambda e, pb=pb, mt=mt: e.copy(out=memT[:, :, mt * 128:(mt + 1) * 128],
                                                            in_=pb[:].rearrange("p (c t) -> p c t", c=8)),
                      reads=[pb.b], writes=[memT.b])
            for hd in range(4):
                ps = getps()
                for c in range(8):
                    cx.op("pe", lambda e, ps=ps, c=c, hd=hd: e.matmul(
                        ps[:, 0:256], lhsT=wkv[:, c, hd * 128:(hd + 1) * 128], rhs=memT[:, c, :],
                        start=(c == 0), stop=(c == 7)), reads=[wkv.b, memT.b], writes=[ps.b])
                cx.op("dve", lambda e, ps=ps, hd=hd: e.tensor_copy(out=kT[:, hd, :], in_=ps[:, 0:256]),
                      reads=[ps.b], writes=[kT.b])
            for mc in range(2):
                ps = getps()
                for c in range(8):
                    cx.op("pe", lambda e, ps=ps, c=c, mc=mc: e.matmul(
                        ps[:], lhsT=memT[:, c, mc * 128:(mc + 1) * 128], rhs=wkv[:, c, 512:1024],
                        start=(c == 0), stop=(c == 7)), reads=[wkv.b, memT.b], writes=[ps.b])
                cx.op("dve", lambda e, ps=ps, mc=mc: e.tensor_copy(out=vtok[:, mc, :], in_=ps[:]),
                      reads=[ps.b], writes=[vtok.b])
            cx.barrier()
            alS.close()

            alW = Alloc(nc)
            hT = [alW.sb([128, 8, TC], BF16, f"hTc{i}") for i in range(2)]
            ysb = [alW.sb([128, 4, TC], BF16, "ysb0")] * 2
            vbuf = alW.sb([128, 4, 30 + TC], BF16, "vbuf")
            sgt = [alW.sb([128, TC], F32, f"sgt{i}") for i in range(3)]
            cv = alW.sb([128, 4, TC], F32, "cv")
            sq = [sgt[1], sgt[2]]
            mean = alW.sb([128, TC], F32, "mean")
            var = alW.sb([128, TC], F32, "var"); lrs = alW.sb([128, TC], F32, "lrs")
            m2 = var; lnv = lrs
            cn = alW.sb([128, 4, TC], BF16, "cn")
            qb = alW.sb([128, 4, TC], BF16, "qb")
            Eb = [alW.sb([128, 2, TC], BF16, f"Eb{i}") for i in range(2)]
            ob = alW.sb([128, 4, TC], BF16, "ob")
            macc = alW.sb([128, TC], F32, "macc"); mt1 = alW.sb([128, TC], F32, "mt1"); mt2 = alW.sb([128, TC], F32, "mt2")
            rden = macc
            xc = [mt1, mt2]
            sqf = [sgt[1], sgt[2], mt1, mt2]
            merged = alW.sb([128, 8, TC], BF16, "merged")
            xt2 = [alW.sb([128, D], F32, f"xtc{i}") for i in range(2)]
            x2t = xt2
            h2f = alW.sb([128, D], F32, "h2f"); h2b = [alW.sb([128, D], BF16, "h2b0")] * 2
            junk2 = h2b[0]
            h2T = alW.sb([128, 8, 128], F32, "h2T")
            ss2 = alW.sb([128, 1], F32, "ss2"); rt2 = alW.sb([128, 1], F32, "rt2"); rs2 = alW.sb([128, 1], F32, "rs2")
            cx.op("pool", lambda e: e.memset(vbuf[:], 0.0), full=[vbuf.b])

            breg = {}

            def mmgrp(ps_ap, ps_b, pairs, reads):
                n = len(pairs)
                for idx, (l, r_) in enumerate(pairs):
                    cx.op("pe", lambda e, l=l, r_=r_, idx=idx: e.matmul(ps_ap, lhsT=l, rhs=r_, start=(idx == 0),
                                                                         stop=(idx == n - 1)),
                          reads=reads, writes=[ps_b])

            KCUT = int(os.environ.get("KCUT", "9"))
            KNB = int(os.environ.get("KNB", str(NBC)))
            def c_load_h(bi):
                t0 = bi * TC
                h = hT[bi % 2]
                cx.op("sp", lambda e, h=h, t0=t0: e.dma_start(
                    out=h[:], in_=hT_scr[:, :, t0:t0 + TC].rearrange("c p t -> p c t")), full=[h.b], dma=True)

            def c_load_y(bi):
                t0 = bi * TC
                yb = ysb[bi % 2]
                cx.op("sp", lambda e, yb=yb, t0=t0: e.dma_start(
                    out=yb[:], in_=ys_scr[:, :, t0:t0 + TC].rearrange("f p t -> p f t")), full=[yb.b], dma=True)

            def c_s2(bi):
                t0 = bi * TC
                h = hT[bi % 2]; yb = ysb[bi % 2]
                for f in range(4):
                    pa = getps(); pg = getps()
                    mmgrp(pa[:, 0:TC], pa.b, [(wA[:, c, f * 128:(f + 1) * 128], h[:, c, :]) for c in range(8)],
                          [wA.b, h.b])
                    mmgrp(pg[:, 0:TC], pg.b, [(wA[:, c, 512 + f * 128:512 + (f + 1) * 128], h[:, c, :]) for c in range(8)],
                          [wA.b, h.b])
                    s = sgt[f % 3]
                    cx.op("act", lambda e, pg=pg, s=s: e.activation(out=s[:], in_=pg[:, 0:TC], func=AF.Sigmoid),
                          reads=[pg.b], full=[s.b])
                    cx.op("dve", lambda e, pa=pa, s=s, f=f: e.tensor_tensor(out=vbuf[:, f, 30:30 + TC], in0=pa[:, 0:TC],
                                                                            in1=s[:], op=ALU.mult),
                          reads=[pa.b, s.b], writes=[vbuf.b])

            def c_mid(bi):
                t0 = bi * TC
                h = hT[bi % 2]; yb = ysb[bi % 2]
                for f in range(4):
                    pc = getps()
                    Dg = Dg2[f % 2]
                    cx.op("pool", lambda e, Dg=Dg, f=f: e.tensor_tensor(
                        out=Dg[:], in0=ident_f[:].unsqueeze(1).to_broadcast([128, 31, 128]),
                        in1=cdw[:, f, :].unsqueeze(2).to_broadcast([128, 31, 128]), op=ALU.mult),
                        reads=[ident_f.b, cdw.b], full=[Dg.b])
                    mmgrp(pc[:, 0:TC], pc.b, [(Dg[:, k, :], vbuf[:, f, k:k + TC]) for k in range(31)],
                          [Dg.b, vbuf.b])
                    cx.op("act", lambda e, pc=pc, f=f: e.activation(out=cv[:, f, :], in_=pc[:, 0:TC], func=AF.Identity,
                                                                    bias=cb[:, f:f + 1], scale=1.0),
                          reads=[pc.b, cb.b], writes=[cv.b])
                cx.op("pool", lambda e: e.tensor_copy(out=vbuf[:, :, 0:30], in_=vbuf[:, :, TC:TC + 30]),
                      reads=[vbuf.b], writes=[vbuf.b])
                for hd in range(4):
                    pq_ = getps()
                    mmgrp(pq_[:, 0:TC], pq_.b, [(wA[:, c, 1024 + hd * 128:1024 + (hd + 1) * 128], h[:, c, :])
                                                for c in range(8)], [wA.b, h.b])
                    cx.op("dve", lambda e, pq_=pq_, hd=hd: e.tensor_copy(out=qb[:, hd, :], in_=pq_[:, 0:TC]),
                          reads=[pq_.b], writes=[qb.b])
                for f in range(4):
                    cx.op("act", lambda e, f=f: e.activation(out=sq[f % 2][:] if False else sqf[f][:], in_=cv[:, f, :],
                                                             func=AF.Square),
                          reads=[cv.b], full=[sqf[f].b])

                def att_scores(hd):
                    E = Eb[hd % 2]
                    for mc in range(2):
                        psc = getps()
                        mmgrp(psc[:, 0:TC], psc.b, [(kT[:, hd, mc * 128:(mc + 1) * 128], qb[:, hd, :])], [kT.b, qb.b])
                        cx.op("act", lambda e, psc=psc, E=E, mc=mc: e.activation(
                            out=E[:, mc, :], in_=psc[:, 0:TC], func=AF.Exp, scale=float(128 ** -0.5)),
                            reads=[psc.b], writes=[E.b])

                def att_out(hd):
                    E = Eb[hd % 2]
                    po = getps(); pd = getps()
                    mmgrp(po[:, 0:TC], po.b, [(vtok[:, mc, hd * 128:(hd + 1) * 128], E[:, mc, :]) for mc in range(2)],
                          [vtok.b, E.b])
                    mmgrp(pd[:, 0:TC], pd.b, [(ones_bf[:], E[:, mc, :]) for mc in range(2)], [ones_bf.b, E.b])
                    cx.op("dve", lambda e, pd=pd: e.reciprocal(out=rden[:], in_=pd[:, 0:TC]), reads=[pd.b], full=[rden.b])
                    cx.op("dve", lambda e, po=po, hd=hd: e.tensor_tensor(out=ob[:, hd, :], in0=po[:, 0:TC], in1=rden[:],
                                                                         op=ALU.mult),
                          reads=[po.b, rden.b], writes=[ob.b])

                att_scores(0)
                att_scores(1)
                pm = getps(); pq = getps()
                mmgrp(pm[:, 0:TC], pm.b, [(onesm[:], cv[:, f, :]) for f in range(4)], [onesm.b, cv.b])
                mmgrp(pq[:, 0:TC], pq.b, [(onesm[:], sqf[f][:]) for f in range(4)], [onesm.b] + [sqf[f].b for f in range(4)])
                cx.op("act", lambda e, pm=pm: e.copy(out=mean[:], in_=pm[:, 0:TC]), reads=[pm.b], full=[mean.b])
                cx.op("dve", lambda e: e.tensor_tensor(out=var[:], in0=mean[:], in1=mean[:], op=ALU.mult),
                      reads=[mean.b], full=[var.b])
                cx.op("dve", lambda e, pq=pq: e.tensor_tensor(out=var[:], in0=pq[:, 0:TC], in1=var[:], op=ALU.subtract),
                      reads=[pq.b], writes=[var.b])
                cx.op("dve", lambda e: e.tensor_scalar(out=var[:], in0=var[:], scalar1=float(EPS), scalar2=None,
                                                       op0=ALU.add), reads=[var.b], writes=[var.b])
                cx.op("act", lambda e: e.activation(out=lrs[:], in_=var[:], func=AF.Ln), reads=[var.b], full=[lrs.b])
                cx.op("act", lambda e: e.activation(out=lrs[:], in_=lrs[:], func=AF.Exp, scale=-0.5),
                      reads=[], writes=[lrs.b])
                cx.op("dve", lambda e: e.tensor_tensor(out=cv[:], in0=cv[:],
                                                       in1=mean[:].unsqueeze(1).to_broadcast([128, 4, TC]),
                                                       op=ALU.subtract), reads=[mean.b], writes=[cv.b])
                cx.op("dve", lambda e: e.tensor_tensor(out=cv[:], in0=cv[:],
                                                       in1=lrs[:].unsqueeze(1).to_broadcast([128, 4, TC]),
                                                       op=ALU.mult), reads=[lrs.b], writes=[cv.b])
                att_out(0)
                att_scores(2)
                att_out(1)
                att_scores(3)
                att_out(2)
                att_out(3)
                for f in range(4):
                    cx.op("act", lambda e, f=f: e.activation(out=cn[:, f, :], in_=cv[:, f, :], func=AF.Silu,
                                                             bias=lnb[:, f:f + 1], scale=lng[:, f:f + 1]),
                          reads=[cv.b, lnb.b, lng.b], writes=[cn.b])
                for j in range(8):
                    js = slice(j * 128, (j + 1) * 128)
                    pga = getps(); pyc = getps()
                    mmgrp(pga[:, 0:TC], pga.b, [(wG[:, c, j * 128:(j + 1) * 128], h[:, c, :]) for c in range(8)], [wG.b, h.b])
                    mmgrp(pyc[:, 0:TC], pyc.b, [(wco[:, f, js], cn[:, f, :]) for f in range(4)], [wco.b, cn.b])
                    s = sgt[0]
                    cx.op("act", lambda e, pga=pga, s=s: e.activation(out=s[:], in_=pga[:, 0:TC], func=AF.Sigmoid),
                          reads=[pga.b], full=[s.b])
                    cx.op("dve", lambda e, pyc=pyc, s=s: e.tensor_tensor(out=macc[:], in0=pyc[:, 0:TC], in1=s[:], op=ALU.mult),
                          reads=[pyc.b, s.b], full=[macc.b])
                    pgb = getps(); pza = getps(); pzb = getps()
                    mmgrp(pgb[:, 0:TC], pgb.b, [(wG[:, c, 1024 + j * 128:1024 + (j + 1) * 128], h[:, c, :]) for c in range(8)],
                          [wG.b, h.b])
                    mmgrp(pza[:, 0:TC], pza.b, [(wgl[:, f, js], yb[:, f, :]) for f in range(4)], [wgl.b, yb.b])
                    mmgrp(pzb[:, 0:TC], pzb.b, [(wgl[:, f, 1024 + j * 128:1024 + (j + 1) * 128], yb[:, f, :]) for f in range(4)],
                          [wgl.b, yb.b])
                    sb_ = sgt[1]; sz = sgt[2]
                    cx.op("act", lambda e, pgb=pgb, sb_=sb_: e.activation(out=sb_[:], in_=pgb[:, 0:TC], func=AF.Sigmoid),
                          reads=[pgb.b], full=[sb_.b])
                    cx.op("act", lambda e, pzb=pzb, sz=sz: e.activation(out=sz[:], in_=pzb[:, 0:TC], func=AF.Sigmoid),
                          reads=[pzb.b], full=[sz.b])
                    cx.op("dve", lambda e, pza=pza, sz=sz: e.tensor_tensor(out=mt1[:], in0=pza[:, 0:TC], in1=sz[:], op=ALU.mult),
                          reads=[pza.b, sz.b], full=[mt1.b])
                    cx.op("dve", lambda e, sb_=sb_: e.tensor_tensor(out=mt1[:], in0=mt1[:], in1=sb_[:], op=ALU.mult),
                          reads=[sb_.b], writes=[mt1.b])
                    cx.op("dve", lambda e: e.tensor_tensor(out=macc[:], in0=macc[:], in1=mt1[:], op=ALU.add),
                          reads=[mt1.b], writes=[macc.b])
                    pgc = getps(); pym = getps()
                    mmgrp(pgc[:, 0:TC], pgc.b, [(wG[:, c, 2048 + j * 128:2048 + (j + 1) * 128], h[:, c, :]) for c in range(8)],
                          [wG.b, h.b])
                    mmgrp(pym[:, 0:TC], pym.b, [(wmo[:, hd, js], ob[:, hd, :]) for hd in range(4)], [wmo.b, ob.b])
                    s = sgt[0]
                    cx.op("act", lambda e, pgc=pgc, s=s: e.activation(out=s[:], in_=pgc[:, 0:TC], func=AF.Sigmoid),
                          reads=[pgc.b], full=[s.b])
                    cx.op("dve", lambda e, pym=pym, s=s: e.tensor_tensor(out=mt2[:], in0=pym[:, 0:TC], in1=s[:], op=ALU.mult),
                          reads=[pym.b, s.b], full=[mt2.b])
                    cx.op("dve", lambda e, j=j: e.tensor_tensor(out=merged[:, j, :], in0=macc[:], in1=mt2[:], op=ALU.add),
                          reads=[macc.b, mt2.b], writes=[merged.b])

            def c_tail(bi):
                ntt = TC // 128
                tis = [bi * ntt + tt for tt in range(ntt)]
                for tt, ti in enumerate(tis):
                    xt_ = xt2[ti % 2]
                    cx.op("sp", lambda e, xt_=xt_, ti=ti: e.dma_start(out=xt_[:], in_=x_d[ti * 128:(ti + 1) * 128, :]),
                          full=[xt_.b], dma=True)
                for tt, ti in enumerate(tis):
                    xt_ = xt2[ti % 2]; x2 = xt_
                    for half in range(2):
                        po_ = getps()
                        mmgrp(po_[:], po_.b, [(merged[:, j, tt * 128:(tt + 1) * 128], wo[:, j, half * 512:(half + 1) * 512])
                                              for j in range(8)], [merged.b, wo.b])
                        cx.op("dve", lambda e, po_=po_, x2=x2, xt_=xt_, half=half: e.tensor_tensor(
                            out=x2[:, half * 512:(half + 1) * 512], in0=po_[:], in1=xt_[:, half * 512:(half + 1) * 512],
                            op=ALU.add), reads=[po_.b, xt_.b], writes=[x2.b])
                    cx.op("sp", lambda e, x2=x2, ti=ti: e.dma_start(out=x2_scr[ti * 128:(ti + 1) * 128, :], in_=x2[:]),
                          reads=[x2.b], dma=True)
                for tt, ti in enumerate(tis):
                    x2 = xt2[ti % 2]; hb2 = h2b[0]
                    cx.op("act", lambda e, x2=x2: e.activation(out=junk2[:], in_=x2[:], func=AF.Square, accum_out=ss2[:]),
                          reads=[x2.b], full=[junk2.b, ss2.b])
                    cx.op("act", lambda e: e.activation(out=rt2[:], in_=ss2[:], func=AF.Sqrt, scale=1.0 / D, bias=EPS),
                          reads=[ss2.b], full=[rt2.b])
                    cx.op("dve", lambda e: e.reciprocal(out=rs2[:], in_=rt2[:]), reads=[rt2.b], full=[rs2.b])
                    cx.op("dve", lambda e, x2=x2: e.scalar_tensor_tensor(out=h2f[:], in0=x2[:], scalar=rs2[:, 0:1],
                                                                         in1=gffn[:], op0=ALU.mult, op1=ALU.mult),
                          reads=[x2.b, rs2.b, gffn.b], full=[h2f.b])
                    cx.op("act", lambda e, hb2=hb2: e.copy(out=hb2[:], in_=h2f[:]), reads=[h2f.b], full=[hb2.b])
                    cx.op("sp", lambda e, hb2=hb2, ti=ti: e.dma_start(out=h2_scr[ti * 128:(ti + 1) * 128, :], in_=hb2[:]),
                          reads=[hb2.b], dma=True)
                    pra = getps(); prb = getps()
                    for c in range(8):
                        pr = pra if c < 4 else prb
                        cx.op("pe", lambda e, pr=pr, c=c: e.transpose(out=pr[:, (c % 4) * 128:(c % 4 + 1) * 128],
                                                                      in_=h2f[:, c * 128:(c + 1) * 128], identity=ident_f[:]),
                              reads=[h2f.b, ident_f.b], writes=[pr.b])
                    cx.op("act", lambda e, pra=pra: e.copy(out=h2T[:, 0:4, :], in_=pra[:].rearrange("p (c t) -> p c t", c=4)),
                          reads=[pra.b], writes=[h2T.b])
                    cx.op("dve", lambda e, prb=prb: e.tensor_copy(out=h2T[:, 4:8, :],
                                                                  in_=prb[:].rearrange("p (c t) -> p c t", c=4)),
                          reads=[prb.b], writes=[h2T.b])
                    plg = getps()
                    mmgrp(plg[:, 0:36], plg.b, [(h2T[:, c, :], wr[:, c, :]) for c in range(8)], [h2T.b, wr.b])
                    cx.op("dve", lambda e, plg=plg, ti=ti: e.tensor_tensor(out=lg_all[:, ti, :], in0=plg[:, 0:36],
                                                                        in1=rbias[:], op=ALU.add),
                          reads=[plg.b, rbias.b], writes=[lg_all.b])

            NBR = min(NBC, KNB) if KCUT >= 2 else 0
            if NBR > 0:
                c_load_h(0); c_load_y(0); c_s2(0)
            for bi in range(NBR):
                if bi + 1 < NBR:
                    c_load_h(bi + 1)
                c_mid(bi)
                if bi + 1 < NBR:
                    c_load_y(bi + 1)
                    c_s2(bi + 1)
                c_tail(bi)
            cx.barrier()
            alW.close()
            alR = Alloc(nc)
            RS = Buf("route")
            tri = alR.sb([128, 128], F32, "tri")
            ones_f = alR.sb([128, 128], F32, "ones_f")
            cx.op("sp", lambda e: e.dma_start(out=tri[:], in_=tri_d), full=[tri.b], dma=True)
            cx.op("pool", lambda e: e.memset(ones_f[:], 1.0), full=[ones_f.b])

            def rd(fn, extra_reads=(), extra_writes=()):
                cx.op("dve", fn, reads=[RS, lg_all.b] + list(extra_reads), writes=[RS] + list(extra_writes))

            def R(shape, name, dt=F32):
                return alR.sb(shape, dt, name)

            NTT = NT
            NEB_ = NEXP * NBLK
            gmax = R([128, NTT], "gmax"); ohg = R([128, NTT, 4], "ohg"); eg = R([128, NTT, 4], "eg")
            sumg = R([128, NTT], "sumg"); ptop = R([128, NTT], "ptop")
            selm = R([128, NTT, 4, 8], "selm"); sel = R([128, NTT, 8], "sel"); sel2 = R([128, NTT, 8], "sel2")
            m1_ = R([128, NTT], "m1_"); m2_ = R([128, NTT], "m2_"); oh1 = R([128, NTT, 8], "oh1"); oh2 = R([128, NTT, 8], "oh2")
            dm = R([128, NTT], "dm"); w1 = R([128, NTT], "w1"); w2 = R([128, NTT], "w2")
            M1 = R([128, NTT, 4, 8], "M1"); M2 = R([128, NTT, 4, 8], "M2"); Mc = R([128, NTT, 32], "Mc")
            Cex = R([128, NTT, 32], "Cex"); pos = R([128, NTT, 32], "pos"); bk = R([128, NTT, 32], "bk")
            sf = R([128, NTT, 32], "sf"); ov = R([128, NTT, 32], "ov"); tq = R([128, NTT, 32], "tq")
            sk = [R([128, NTT], f"sk{k}") for k in range(2)]; okk = R([128, NTT], "okk"); dd = R([128, NTT], "dd")
            si = [R([128, NTT], f"si{k}", I32) for k in range(2)]
            ent = [R([128, NTT, 4], f"ent{k}") for k in range(2)]
            le4 = lg_all[:, :, 4:36].rearrange("p t (g j) -> p t g j", g=4)

            def bc3(a, n):
                return a.unsqueeze(2).to_broadcast([128, NTT, n])

            rd(lambda e: e.tensor_reduce(out=gmax[:], in_=lg_all[:, :, 0:4], axis=AX.X, op=ALU.max))
            rd(lambda e: e.tensor_tensor(out=ohg[:], in0=lg_all[:, :, 0:4], in1=bc3(gmax[:], 4), op=ALU.is_equal))
            rd(lambda e: e.tensor_tensor(out=eg[:], in0=lg_all[:, :, 0:4], in1=bc3(gmax[:], 4), op=ALU.subtract))
            cx.op("act", lambda e: e.activation(out=eg[:], in_=eg[:], func=AF.Exp), reads=[RS], writes=[RS])
            rd(lambda e: e.tensor_reduce(out=sumg[:], in_=eg[:], axis=AX.X, op=ALU.add))
            rd(lambda e: e.reciprocal(out=ptop[:], in_=sumg[:]))
            rd(lambda e: e.tensor_tensor(out=selm[:], in0=le4,
                                         in1=ohg[:].unsqueeze(3).to_broadcast([128, NTT, 4, 8]), op=ALU.mult))
            rd(lambda e: e.tensor_reduce(out=sel[:], in_=selm[:].rearrange("p t g j -> p t j g"), axis=AX.X, op=ALU.add))
            rd(lambda e: e.tensor_reduce(out=m1_[:], in_=sel[:], axis=AX.X, op=ALU.max))
            rd(lambda e: e.tensor_tensor(out=oh1[:], in0=sel[:], in1=bc3(m1_[:], 8), op=ALU.is_equal))
            rd(lambda e: e.scalar_tensor_tensor(out=sel2[:], in0=oh1[:], scalar=-1e30, in1=sel[:], op0=ALU.mult, op1=ALU.add))
            rd(lambda e: e.tensor_reduce(out=m2_[:], in_=sel2[:], axis=AX.X, op=ALU.max))
            rd(lambda e: e.tensor_tensor(out=oh2[:], in0=sel2[:], in1=bc3(m2_[:], 8), op=ALU.is_equal))
            rd(lambda e: e.tensor_tensor(out=dm[:], in0=m1_[:], in1=m2_[:], op=ALU.subtract))
            cx.op("act", lambda e: e.activation(out=w1[:], in_=dm[:], func=AF.Sigmoid), reads=[RS], writes=[RS])
            rd(lambda e: e.tensor_tensor(out=w1[:], in0=w1[:], in1=ptop[:], op=ALU.mult))
            rd(lambda e: e.tensor_tensor(out=w2[:], in0=ptop[:], in1=w1[:], op=ALU.subtract))
            rd(lambda e: e.tensor_tensor(out=M1[:], in0=ohg[:].unsqueeze(3).to_broadcast([128, NTT, 4, 8]),
                                         in1=oh1[:].unsqueeze(2).to_broadcast([128, NTT, 4, 8]), op=ALU.mult))
            rd(lambda e: e.tensor_tensor(out=M2[:], in0=ohg[:].unsqueeze(3).to_broadcast([128, NTT, 4, 8]),
                                         in1=oh2[:].unsqueeze(2).to_broadcast([128, NTT, 4, 8]), op=ALU.mult))
            rd(lambda e: e.tensor_tensor(out=Mc[:], in0=M1[:].rearrange("p t g j -> p t (g j)"),
                                         in1=M2[:].rearrange("p t g j -> p t (g j)"), op=ALU.add))
            rd(lambda e: e.memset(Cex[:, 0, :], 0.0))
            for i in range(1, NTT):
                rd(lambda e, i=i: e.tensor_tensor(out=Cex[:, i, :], in0=Cex[:, i - 1, :], in1=Mc[:, i - 1, :], op=ALU.add))
            pp = [getps(), getps()]
            for i in range(NTT):
                pb_ = pp[i // 16]
                o_ = pb_[:, (i % 16) * 32:(i % 16 + 1) * 32]
                cx.op("pe", lambda e, o_=o_, i=i: e.matmul(o_, lhsT=tri[:], rhs=Mc[:, i, :], start=True, stop=False),
                      reads=[tri.b, RS], writes=[pb_.b])
                cx.op("pe", lambda e, o_=o_, i=i: e.matmul(o_, lhsT=ones_f[:], rhs=Cex[:, i, :], start=False, stop=True),
                      reads=[ones_f.b, RS], writes=[pb_.b])
            for hh in range(2):
                rd(lambda e, hh=hh: e.tensor_copy(out=pos[:, hh * 16:(hh + 1) * 16, :],
                                                  in_=pp[hh][:].rearrange("p (t x) -> p t x", t=16)), [pp[hh].b])
            rd(lambda e: e.tensor_single_scalar(out=bk[:], in_=pos[:], scalar=127.5, op=ALU.is_gt))
            for thr in range(2, NBLK):
                rd(lambda e, thr=thr: e.tensor_single_scalar(out=tq[:], in_=pos[:], scalar=128.0 * thr - 0.5, op=ALU.is_gt))
                rd(lambda e: e.tensor_tensor(out=bk[:], in0=bk[:], in1=tq[:], op=ALU.add))
            rd(lambda e: e.scalar_tensor_tensor(out=bk[:], in0=bk[:], scalar=float(1 - 128 * NEB_),
                                                in1=ecap[:].unsqueeze(1).to_broadcast([128, NTT, 32]),
                                                op0=ALU.mult, op1=ALU.add), [ecap.b])
            rd(lambda e: e.scalar_tensor_tensor(out=sf[:], in0=pos[:], scalar=float(NEB_), in1=bk[:],
                                                op0=ALU.mult, op1=ALU.add))
            rd(lambda e: e.tensor_single_scalar(out=ov[:], in_=pos[:], scalar=float(CAP) - 0.5, op=ALU.is_gt))
            for k, (Mk, wk) in enumerate(((M1, w1), (M2, w2))):
                Mk32 = Mk[:].rearrange("p t g j -> p t (g j)")
                rd(lambda e, Mk32=Mk32: e.tensor_tensor(out=tq[:], in0=Mk32, in1=sf[:], op=ALU.mult))
                rd(lambda e, k=k: e.tensor_reduce(out=sk[k][:], in_=tq[:], axis=AX.X, op=ALU.add))
                rd(lambda e, Mk32=Mk32: e.tensor_tensor(out=tq[:], in0=Mk32, in1=ov[:], op=ALU.mult))
                rd(lambda e: e.tensor_reduce(out=okk[:], in_=tq[:], axis=AX.X, op=ALU.add))
                rd(lambda e, k=k: e.tensor_scalar(out=dd[:], in0=sk[k][:], scalar1=trashp[:, 0:1], scalar2=None,
                                                  op0=ALU.subtract), [trashp.b])
                rd(lambda e: e.tensor_tensor(out=dd[:], in0=dd[:], in1=okk[:], op=ALU.mult))
                rd(lambda e, k=k: e.tensor_tensor(out=sk[k][:], in0=sk[k][:], in1=dd[:], op=ALU.subtract))
                rd(lambda e, k=k: e.tensor_copy(out=si[k][:], in_=sk[k][:]), (), [si[k].b])
                rd(lambda e, k=k: e.memset(ent[k][:], 0.0), (), [ent[k].b])
                rd(lambda e, k=k: e.tensor_copy(out=ent[k][:, :, 0], in_=tokid[:]), [tokid.b], [ent[k].b])
                rd(lambda e, k=k: e.tensor_scalar(out=ent[k][:, :, 1], in0=tokid[:], scalar1=float(k * ROWS), scalar2=None,
                                                  op0=ALU.add), [tokid.b], [ent[k].b])
                rd(lambda e, k=k, wk=wk: e.tensor_copy(out=ent[k][:, :, 2], in_=wk[:]), (), [ent[k].b])
            for i in range(NTT):
                for k in range(2):
                    cx.op("pool", lambda e, i=i, k=k: e.indirect_dma_start(
                        out=lst_d, out_offset=bass.IndirectOffsetOnAxis(ap=si[k][:, i:i + 1], axis=0),
                        in_=ent[k][:, i, :], in_offset=None),
                        reads=[si[k].b, ent[k].b, lstB], dma=True)
            cx.barrier()
            alR.close()
            alC.close()
        if stop_after in ("A", "B", "C"):
            pass
        else:
            alD = Alloc(nc)
            NEB = NEXP * NBLK
            lst_sb = alD.sb([128, NEB, 4], F32, "lst_sb")
            idx_i = alD.sb([128, NEB], I32, "idx_i")
            dst_i = alD.sb([128, NEB], I32, "dst_i")
            cx.op("sp", lambda e: e.dma_start(out=lst_sb[:], in_=lst_d[0:NEXP * CAP, :].rearrange("(s eb) w -> s eb w", s=128)),
                  reads=[lstB], full=[lst_sb.b], dma=True)
            cx.op("dve", lambda e: e.tensor_copy(out=idx_i[:], in_=lst_sb[:, :, 0]), reads=[lst_sb.b], full=[idx_i.b])
            cx.op("dve", lambda e: e.tensor_copy(out=dst_i[:], in_=lst_sb[:, :, 1]), reads=[lst_sb.b], full=[dst_i.b])
            NWB = 3
            Wg = [alD.sb([128, 8, 256], BF16, f"Wg{i}") for i in range(NWB)]
            Wu = [alD.sb([128, 8, 256], BF16, f"Wu{i}") for i in range(NWB)]
            Wd = [alD.sb([128, 2, 1024], BF16, f"Wd{i}") for i in range(NWB)]
            Gt = [alD.sb([128, D], BF16, f"Gt{i}") for i in range(3)]
            Xe = [alD.sb([128, 8, CAP], BF16, f"Xe{i}") for i in range(2)]
            sgl = [alD.sb([128, CAP], F32, f"sgl{i}") for i in range(2)]
            ae = [alD.sb([128, 2, CAP], BF16, f"ae{i}") for i in range(2)]
            Yt = [alD.sb([128, D], BF16, f"Yt{i}") for i in range(3)]

            Gt6 = Gt + [alD.sb([128, D], BF16, f"Gtx{i}") for i in range(3)]

            def load_w_dma(e_):
                p = e_ % NWB
                cx.op("sp", lambda e: e.dma_start(out=Wg[p][:].rearrange("p c n -> p (c n)"), in_=wbf_scr[e_, 0]),
                      full=[Wg[p].b], dma=True)
                cx.op("sp", lambda e: e.dma_start(out=Wu[p][:].rearrange("p c n -> p (c n)"), in_=wbf_scr[e_, 1]),
                      full=[Wu[p].b], dma=True)
                cx.op("sp", lambda e: e.dma_start(out=Wd[p][:].rearrange("p c n -> p (c n)"), in_=wbf_scr[e_, 2]),
                      full=[Wd[p].b], dma=True)

            def load_w_cast(e_):
                pass

            def gathers(e_):
                for blk in range(NBLK):
                    eb = e_ * NBLK + blk
                    G = Gt6[(e_ % 2) * 3 + blk]
                    cx.op("pool", lambda e, G=G, eb=eb: e.indirect_dma_start(
                        out=G[:], out_offset=None, in_=h2_scr,
                        in_offset=bass.IndirectOffsetOnAxis(ap=idx_i[:, eb:eb + 1], axis=0)),
                        reads=[idx_i.b, h2B], full=[G.b], dma=True)

            gi = [0]
            load_w_dma(0)
            load_w_dma(1)
            gathers(0)
            KNE = int(os.environ.get("KNE", str(NEXP)))
            for e_ in range(KNE):
                p = e_ % NWB
                if e_ + 2 < NEXP:
                    load_w_dma(e_ + 2)
                if e_ + 1 < NEXP:
                    gathers(e_ + 1)
                X = Xe[e_ % 2]
                for blk in range(NBLK):
                    G = Gt6[(e_ % 2) * 3 + blk]
                    pbk = psb[gi[0] % 2]
                    gi[0] += 1
                    for c in range(8):
                        cx.op("pe", lambda e, pbk=pbk, G=G, c=c: e.transpose(
                            out=pbk[:, c * 128:(c + 1) * 128], in_=G[:, c * 128:(c + 1) * 128], identity=ident_bf[:]),
                            reads=[G.b, ident_bf.b], writes=[pbk.b])
                    if blk % 2 == 0:
                        cx.op("act", lambda e, pbk=pbk, X=X, blk=blk: e.copy(
                            out=X[:, :, blk * 128:(blk + 1) * 128], in_=pbk[:].rearrange("p (c t) -> p c t", c=8)),
                            reads=[pbk.b], writes=[X.b])
                    else:
                        cx.op("dve", lambda e, pbk=pbk, X=X, blk=blk: e.tensor_copy(
                            out=X[:, :, blk * 128:(blk + 1) * 128], in_=pbk[:].rearrange("p (c t) -> p c t", c=8)),
                            reads=[pbk.b], writes=[X.b])
                a_ = ae[e_ % 2]
                for ft in range(2):
                    pg = getps(); pu = getps()
                    for c in range(8):
                        cx.op("pe", lambda e, pg=pg, c=c, ft=ft, X=X, p=p: e.matmul(
                            pg[:, 0:CAP], lhsT=Wg[p][:, c, ft * 128:(ft + 1) * 128], rhs=X[:, c, :],
                            start=(c == 0), stop=(c == 7)), reads=[Wg[p].b, X.b], writes=[pg.b])
                    for c in range(8):
                        cx.op("pe", lambda e, pu=pu, c=c, ft=ft, X=X, p=p: e.matmul(
                            pu[:, 0:CAP], lhsT=Wu[p][:, c, ft * 128:(ft + 1) * 128], rhs=X[:, c, :],
                            start=(c == 0), stop=(c == 7)), reads=[Wu[p].b, X.b], writes=[pu.b])
                    s = sgl[ft]
                    cx.op("act", lambda e, pg=pg, s=s: e.activation(out=s[:], in_=pg[:, 0:CAP], func=AF.Silu),
                          reads=[pg.b], full=[s.b])
                    cx.op("dve", lambda e, pu=pu, s=s, a_=a_, ft=ft: e.tensor_tensor(
                        out=a_[:, ft, :], in0=pu[:, 0:CAP], in1=s[:], op=ALU.mult),
                        reads=[pu.b, s.b], writes=[a_.b])
                for blk in range(NBLK):
                    eb = e_ * NBLK + blk
                    Y = Yt[eb % 3]
                    for half in range(2):
                        py = getps()
                        for ft in range(2):
                            cx.op("pe", lambda e, py=py, ft=ft, blk=blk, half=half, a_=a_, p=p: e.matmul(
                                py[:], lhsT=a_[:, ft, blk * 128:(blk + 1) * 128],
                                rhs=Wd[p][:, ft, half * 512:(half + 1) * 512], start=(ft == 0), stop=(ft == 1)),
                                reads=[a_.b, Wd[p].b], writes=[py.b])
                        if half == 0:
                            cx.op("dve", lambda e, py=py, Y=Y, eb=eb: e.tensor_scalar(
                                out=Y[:, 0:512], in0=py[:], scalar1=lst_sb[:, eb, 2:3], scalar2=None, op0=ALU.mult),
                                reads=[py.b, lst_sb.b], writes=[Y.b])
                        else:
                            cx.op("act", lambda e, py=py, Y=Y, eb=eb: e.activation(
                                out=Y[:, 512:1024], in_=py[:], func=AF.Copy, scale=lst_sb[:, eb, 2:3]),
                                reads=[py.b, lst_sb.b], writes=[Y.b])
                    cx.op("pool", lambda e, Y=Y, eb=eb: e.indirect_dma_start(
                        out=moe_scr, out_offset=bass.IndirectOffsetOnAxis(ap=dst_i[:, eb:eb + 1], axis=0),
                        in_=Y[:], in_offset=None), reads=[Y.b, dst_i.b, moeB], dma=True)
                if e_ + 1 < NEXP:
                    load_w_cast(e_ + 1)
            cx.barrier()
            alD.close()

            alE = Alloc(nc)
            gfin = alE.sb([128, D], F32, "gfin")
            cx.op("sp", lambda e: e.dma_start(out=gfin[:], in_=gfin_d.partition_broadcast(128)), full=[gfin.b], dma=True)
            NE_ = 4
            xa = [alE.sb([128, D], F32, f"xa{i}") for i in range(NE_)]
            m0 = [alE.sb([128, D], BF16, f"m0{i}") for i in range(NE_)]
            m1 = [alE.sb([128, D], BF16, f"m1{i}") for i in range(NE_)]
            ot = [alE.sb([128, D], F32, f"ot{i}") for i in range(NE_)]
            junk3 = alE.sb([128, D], BF16, "junk3")
            sse = [alE.sb([128, 1], F32, f"sse{i}") for i in range(NE_)]
            rte = [alE.sb([128, 1], F32, f"rte{i}") for i in range(NE_)]
            rse = [alE.sb([128, 1], F32, f"rse{i}") for i in range(NE_)]
            outB = Buf("out")

            def e_load(ti):
                p = ti % NE_
                rows = slice(ti * 128, (ti + 1) * 128)
                cx.op("sp", lambda e, p=p, rows=rows: e.dma_start(out=xa[p][:], in_=x2_scr[rows, :]),
                      full=[xa[p].b], dma=True)
                cx.op("sp", lambda e, p=p, rows=rows: e.dma_start(out=m0[p][:], in_=moe_scr[rows, :]),
                      full=[m0[p].b], dma=True)
                cx.op("sp", lambda e, p=p, ti=ti: e.dma_start(
                    out=m1[p][:], in_=moe_scr[ROWS + ti * 128:ROWS + (ti + 1) * 128, :]),
                    full=[m1[p].b], dma=True)

            for ti in range(min(NE_ - 1, NT)):
                e_load(ti)
            for ti in range(NT):
                p = ti % NE_
                rows = slice(ti * 128, (ti + 1) * 128)
                if ti + NE_ - 1 < NT:
                    e_load(ti + NE_ - 1)
                cx.op("pool", lambda e, p=p: e.tensor_tensor(out=xa[p][:], in0=xa[p][:], in1=m0[p][:], op=ALU.add),
                      reads=[m0[p].b], writes=[xa[p].b])
                cx.op("dve", lambda e, p=p: e.tensor_tensor(out=xa[p][:], in0=xa[p][:], in1=m1[p][:], op=ALU.add),
                      reads=[m1[p].b], writes=[xa[p].b])
                cx.op("act", lambda e, p=p: e.activation(out=junk3[:], in_=xa[p][:], func=AF.Square, accum_out=sse[p][:]),
                      reads=[xa[p].b], full=[junk3.b, sse[p].b])
                cx.op("act", lambda e, p=p: e.activation(out=rte[p][:], in_=sse[p][:], func=AF.Sqrt, scale=1.0 / D, bias=EPS),
                      reads=[sse[p].b], full=[rte[p].b])
                cx.op("dve", lambda e, p=p: e.reciprocal(out=rse[p][:], in_=rte[p][:]), reads=[rte[p].b], full=[rse[p].b])
                cx.op("dve", lambda e, p=p: e.scalar_tensor_tensor(out=ot[p][:], in0=xa[p][:], scalar=rse[p][:, 0:1],
                                                                   in1=gfin[:], op0=ALU.mult, op1=ALU.mult),
                      reads=[xa[p].b, rse[p].b, gfin.b], full=[ot[p].b])
                cx.op("sp", lambda e, p=p, rows=rows: e.dma_start(out=out_d[rows, :], in_=ot[p][:]),
                      reads=[ot[p].b], writes=[outB], dma=True)
            cx.barrier()
            alE.close()
        cx.barrier()
        cx.emit(block)
        print("waits", cx.nwait, "instrs", {e: cx.cnt[e] for e in cx.ENG}, "signals", {e: len(cx.waited[e]) for e in cx.ENG})
    return nc


def host_consts():
    c = {}
    c["ident_bf"] = np.eye(128, dtype=np.float32).astype(ml_dtypes.bfloat16)
    c["ident_f"] = np.eye(128, dtype=np.float32)
    psel = np.zeros((128, 8, 240), np.float32)
    for a in range(8):
        for i in range(16):
            psel[a * 16 + i, a, 7 * 16 + i] = 1.0
    c["psel"] = psel.astype(ml_dtypes.bfloat16)
    kk = np.arange(128) // 16
    c["cmask"] = (kk[None, :] >= kk[:, None]).astype(np.float32)
    c["tri"] = (np.arange(128)[:, None] < np.arange(128)[None, :]).astype(np.float32)
    c["ecap"] = np.ascontiguousarray(np.broadcast_to((np.arange(32) * NBLK).astype(np.float32)[None, :], (128, 32)))
    c["tokid"] = (np.arange(NT)[None, :] * 128 + np.arange(128)[:, None]).astype(np.float32)
    li = np.zeros((NEXP * CAP + 128, 4), np.float32)
    li[:, 0] = SEQ + ((np.arange(NEXP * CAP + 128) // (NEXP * NBLK)) % 128)
    li[:, 1] = li[:, 0]
    c["trashp"] = (NEXP * CAP + np.arange(128)).astype(np.float32).reshape(128, 1)
    c["lst_init"] = li
    return c


def relayout_pc(w):
    E, K, N = w.shape
    return np.ascontiguousarray(w.reshape(E, K // 128, 128, N).transpose(0, 2, 1, 3))


def pair_layout(a):
    rest = a.shape[2:]
    a = a.reshape((16, 2, 64) + rest)
    a = np.moveaxis(a, 0, 2)
    return np.ascontiguousarray(a.reshape((128, 16) + rest))


def make_inmap(inputs, b, consts=None):
    f = lambda a: np.ascontiguousarray(a, dtype=np.float32)
    m = {"x": f(inputs["x"][b]),
         "g_mix": f(inputs["g_mix"]),
         "w_in": f(inputs["w_in"][0])}
    m["lamre_l"] = pair_layout(f(inputs["ssm_lambda_re"][0]))
    m["lamim_l"] = pair_layout(f(inputs["ssm_lambda_im"][0]))
    m["logdt_l"] = pair_layout(np.broadcast_to(f(inputs["ssm_log_dt"][0])[:, None], (32, 64)))
    m["bre_l"] = pair_layout(f(inputs["ssm_b_re"][0]))
    m["bim_l"] = pair_layout(f(inputs["ssm_b_im"][0]))
    m["cre_l"] = pair_layout(f(inputs["ssm_c_re"][0]).transpose(0, 2, 1))
    m["cim_l"] = pair_layout(f(inputs["ssm_c_im"][0]).transpose(0, 2, 1))
    m["d_l"] = np.ascontiguousarray(np.tile(f(inputs["ssm_d"][0]).reshape(32, 16).T, (8, 1)))
    m["mem"] = f(inputs["mem"][b])
    for k_, n_ in (("g_mem", "g_mem"), ("g_ffn", "g_ffn")):
        m[n_] = f(inputs[k_])
    m["g_final"] = f(inputs["g_final"]).reshape(1, D)
    m["w_mem_kv"] = f(inputs["w_mem_kv"][0]); m["w_mem_out"] = f(inputs["w_mem_out"][0])
    m["w_conv_out"] = f(inputs["w_conv_out"][0]); m["w_ssm_glu"] = f(inputs["w_ssm_glu"][0])
    m["w_out"] = f(inputs["w_out"][0])
    m["w_router"] = np.ascontiguousarray(np.concatenate([f(inputs["w_router_group"][0]),
                                                         f(inputs["w_router_expert"][0])], axis=1))
    m["b_router"] = np.ascontiguousarray(np.concatenate([f(inputs["b_router_group"][0]),
                                                         f(inputs["b_router_expert"][0])])[None, :])
    m["cdw_l"] = np.ascontiguousarray(f(inputs["conv_dw"][0]).T.reshape(4, 128, 31).transpose(1, 0, 2))
    m["cb_l"] = np.ascontiguousarray(f(inputs["conv_dw_bias"][0]).reshape(4, 128).T)
    m["lng_l"] = np.ascontiguousarray(f(inputs["conv_ln_g"][0]).reshape(4, 128).T)
    m["lnb_l"] = np.ascontiguousarray(f(inputs["conv_ln_b"][0]).reshape(4, 128).T)
    if consts is not None and "w_exp_gate" in consts:
        for k_ in ("w_exp_gate", "w_exp_up", "w_exp_down"):
            m[k_] = consts[k_]
    else:
        m["w_exp_gate"] = relayout_pc(f(inputs["w_exp_gate"][0]))
        m["w_exp_up"] = relayout_pc(f(inputs["w_exp_up"][0]))
        m["w_exp_down"] = relayout_pc(f(inputs["w_exp_down"][0]))
    m.update(consts if consts is not None else host_consts())
    return m


def kernel(**inputs):
    nc = build()
    consts = host_consts()
    f32 = lambda a: np.ascontiguousarray(a, dtype=np.float32)
    for k_ in ("w_exp_gate", "w_exp_up", "w_exp_down"):
        consts[k_] = relayout_pc(f32(inputs[k_][0]))
    in_maps = [make_inmap(inputs, b, consts) for b in range(NCORES)]
    res = run_bass_kernel_spmd(nc, in_maps, core_ids=list(range(NCORES)))
    return np.stack([r["out"] for r in res.results], axis=0)
```

```python
import os
import numpy as np
import ml_dtypes
from contextlib import ExitStack
import concourse.bass as bass
import concourse.mybir as mybir
from concourse.bass_utils import run_bass_kernel_spmd

F32 = mybir.dt.float32
BF16 = mybir.dt.bfloat16
I32 = mybir.dt.int32
U32 = mybir.dt.uint32
AF = mybir.ActivationFunctionType
ALU = mybir.AluOpType
AX = mybir.AxisListType
GELU = AF.Gelu_apprx_tanh

D = 1024
SEQ = 4096
NCORES = 8
T = 512
NB = SEQ // T
NT = SEQ // 128
EPS = 1e-6
NEXP = 32
CAP = 384
NBLK = CAP // 128
ROWS = SEQ + 128


class Buf:
    __slots__ = ("name", "w", "r")

    def __init__(self, name):
        self.name = name
        self.w = {}
        self.r = {}


class Ctx:
    ENG = ("pe", "dve", "act", "pool", "sp")
    KROT = 4
    NDMA = 12

    def __init__(self, nc, es):
        self.nc = nc
        self.q = {e: [] for e in self.ENG}
        self.cnt = {e: 0 for e in self.ENG}
        self.seen = {e: {} for e in self.ENG}
        self.esem = {e: [es.enter_context(nc.semaphore(f"s_{e}{i}")) for i in range(self.KROT)]
                     for e in self.ENG}
        self.dsem = {e: [es.enter_context(nc.semaphore(f"d_{e}{i}")) for i in range(self.NDMA)]
                     for e in ("sp", "act", "pool")}
        self.dcnt = {e: [0] * self.NDMA for e in self.dsem}
        self.dnext = {e: 0 for e in self.dsem}
        self.nwait = 0
        self.waited = {e: set() for e in self.ENG}

    def _wait(self, eng, tok):
        key, val = tok
        if key[0] == 'e' and key[1] == eng and eng == "pe":
            return
        if self.seen[eng].get(key, -1) >= val:
            return
        self.seen[eng][key] = val
        if key[0] == 'e':
            self.waited[key[1]].add(val)
        self.q[eng].append(("w", key, val))
        self.nwait += 1

    def op(self, eng, fn, reads=(), writes=(), full=(), dma=False):
        toks = []
        for b in reads:
            toks.extend(b.w.items())
        for b in tuple(writes) + tuple(full):
            toks.extend(b.w.items())
            toks.extend(b.r.items())
        for t in toks:
            self._wait(eng, t)
        if dma:
            i = self.dnext[eng]
            self.dnext[eng] = (i + 1) % self.NDMA
            key = ('d', eng, i)
            if self.dcnt[eng][i] > 0:
                self._wait(eng, (key, self.dcnt[eng][i]))
            self.dcnt[eng][i] += 16
            val = self.dcnt[eng][i]
            self.q[eng].append(("d", fn, self.dsem[eng][i]))
        else:
            key = ('e', eng)
            val = self.cnt[eng]
            self.cnt[eng] += 1
            self.q[eng].append(("i", fn, val))
        for b in reads:
            b.r[key] = val
        for b in full:
            b.w = {key: val}
            b.r = {}
        for b in writes:
            b.w[key] = val
        return (key, val)

    def barrier(self, skip_pool_dma=False):
        toks = []
        for e in self.ENG:
            if skip_pool_dma and e == "pool":
                continue
            if self.cnt[e] > 0:
                toks.append((('e', e), self.cnt[e] - 1))
        for e in self.dsem:
            if skip_pool_dma and e == "pool":
                continue
            for i in range(self.NDMA):
                if self.dcnt[e][i] > 0:
                    toks.append((('d', e, i), self.dcnt[e][i]))
        for e in self.ENG:
            for t in toks:
                self._wait(e, t)

    def emit(self, block):
        nc = self.nc

        rank = {e: {v: i for i, v in enumerate(sorted(self.waited[e]))} for e in self.ENG}
        K_ = self.KROT

        def run(engname, engine):
            for item in self.q[engname]:
                if item[0] == "w":
                    key, val = item[1], item[2]
                    if key[0] == 'e':
                        r = rank[key[1]][val]
                        engine.wait_ge(self.esem[key[1]][r % K_], r // K_ + 1)
                    else:
                        engine.wait_ge(self.dsem[key[1]][key[2]], val)
                elif item[0] == "d":
                    item[1](engine).then_inc(item[2], 16)
                else:
                    ins = item[1](engine)
                    r = rank[engname].get(item[2])
                    if r is not None:
                        ins.then_inc(self.esem[engname][r % K_], 1)

        @block.tensor
        def _(e):
            run("pe", e)

        @block.vector
        def _(e):
            run("dve", e)

        @block.scalar
        def _(e):
            run("act", e)

        @block.gpsimd
        def _(e):
            run("pool", e)

        @block.sync
        def _(e):
            run("sp", e)


class TT:
    def __init__(self, t, name):
        self.t = t
        self.b = Buf(name)

    def __getitem__(self, k):
        return self.t[k]


class Alloc:
    cnt = [0]

    def __init__(self, nc, es=None):
        self.nc = nc
        self.es = es if es is not None else ExitStack()

    @property
    def n(self):
        return Alloc.cnt[0]

    @n.setter
    def n(self, v):
        Alloc.cnt[0] = v

    def close(self):
        self.es.close()

    def sb(self, shape, dt, name=None):
        self.n += 1
        name = name or f"sb{self.n}"
        t = self.es.enter_context(self.nc.sbuf_tensor(f"{name}_{self.n}", list(shape), dt))
        return TT(t, name)

    def ps(self, shape, dt, name=None):
        self.n += 1
        name = name or f"ps{self.n}"
        t = self.es.enter_context(self.nc.psum_tensor(f"{name}_{self.n}", list(shape), dt))
        return TT(t, name)


def build(stop_after="E", dbg=False):
    nc = bass.Bass("TRN2", target_bir_lowering=False)
    dram = {}

    def din(name, shape, dt=F32):
        dram[name] = nc.dram_tensor(name, list(shape), dt, kind="ExternalInput").ap()
        return dram[name]

    def dscr(name, shape, dt, kind="Internal"):
        dram[name] = nc.dram_tensor(name, list(shape), dt, kind=kind).ap()
        return dram[name]

    x_d = din("x", [SEQ, D])
    gmix_d = din("g_mix", [1, D])
    w_in_d = din("w_in", [D, 5120])
    ident_bf_d = din("ident_bf", [128, 128], BF16)
    ident_f_d = din("ident_f", [128, 128], F32)
    lamre_d = din("lamre_l", [128, 16])
    lamim_d = din("lamim_l", [128, 16])
    logdt_d = din("logdt_l", [128, 16])
    bre_d = din("bre_l", [128, 16, 16])
    bim_d = din("bim_l", [128, 16, 16])
    cre_d = din("cre_l", [128, 16, 16])
    cim_d = din("cim_l", [128, 16, 16])
    dl_d = din("d_l", [128, 32])
    psel_d = din("psel", [128, 8, 240], BF16)
    cmask_d = din("cmask", [128, 128])
    mem_d = din("mem", [256, D])
    gmem_d = din("g_mem", [1, D])
    gffn_d = din("g_ffn", [1, D])
    gfin_d = din("g_final", [1, D])
    wkv_d = din("w_mem_kv", [D, 1024])
    wmo_d = din("w_mem_out", [512, D])
    wco_d = din("w_conv_out", [512, D])
    wgl_d = din("w_ssm_glu", [512, 2048])
    wo_d = din("w_out", [D, D])
    wr_d = din("w_router", [D, 36])
    rbias_d = din("b_router", [1, 36])
    cdw_d = din("cdw_l", [128, 4, 31])
    cb_d = din("cb_l", [128, 4])
    lng_d = din("lng_l", [128, 4])
    lnb_d = din("lnb_l", [128, 4])
    tri_d = din("tri", [128, 128])
    ecap_d = din("ecap", [128, 32])
    tokid_d = din("tokid", [128, NT])
    lst_init_d = din("lst_init", [NEXP * CAP + 128, 4])
    trashp_d = din("trashp", [128, 1])
    weg_d = din("w_exp_gate", [NEXP, 128, 8, 256])
    weu_d = din("w_exp_up", [NEXP, 128, 8, 256])
    wed_d = din("w_exp_down", [NEXP, 128, 2, D])
    dk = "ExternalOutput" if dbg else "Internal"
    wbf_scr = dscr("wbf_scr", [NEXP, 3, 128, 2048], BF16)
    lst_d = dscr("lst", [NEXP * CAP + 128, 4], F32, kind=dk)
    h2_scr = dscr("h2_scr", [ROWS, D], BF16, kind=dk)
    moe_scr = dscr("moe_scr", [2 * ROWS, D], BF16, kind=dk)
    x2_scr = dscr("x2_scr", [SEQ, D], F32, kind=dk)
    ys_scr = dscr("ys_scr", [4, 128, SEQ], BF16, kind="ExternalOutput" if dbg else "Internal")
    out_d = dscr("out", [SEQ, D], F32, kind="ExternalOutput")
    hT_scr = dscr("hT_scr", [8, 128, SEQ], BF16, kind="ExternalOutput" if dbg else "Internal")
    u_dbg = dscr("u_dbg", [4, 128, SEQ], BF16, kind="ExternalOutput") if dbg else None

    with ExitStack() as es:
        cx = Ctx(nc, es)
        al = Alloc(nc, es)
        block = es.enter_context(nc.Block())

        ident_bf = al.sb([128, 128], BF16, "ident_bf")
        cx.op("sp", lambda e: e.dma_start(out=ident_bf[:], in_=ident_bf_d), full=[ident_bf.b], dma=True)
        ident_f = al.sb([128, 128], F32, "ident_f")
        cx.op("sp", lambda e: e.dma_start(out=ident_f[:], in_=ident_f_d), full=[ident_f.b], dma=True)

        psum = [al.ps([128, 512], F32, f"bank{i}") for i in range(6)]
        psb = [al.ps([128, 1024], BF16, f"bankb{i}") for i in range(2)]
        pctr = [0]

        def getps():
            p = psum[pctr[0] % len(psum)]
            pctr[0] += 1
            return p

        alAB = Alloc(nc)
        u_all = alAB.sb([128, 4, SEQ], BF16, "u_all")
        M_all = alAB.sb([128, 32, 128], BF16, "M_all")
        W2r = alAB.sb([128, 16, 2, 128], BF16, "W2r"); W2i = alAB.sb([128, 16, 2, 128], BF16, "W2i")
        C1r = alAB.sb([128, 16, 128], BF16, "C1r"); nC1i = alAB.sb([128, 16, 128], BF16, "nC1i")
        KAr = alAB.sb([128, 9, 16], F32, "KAr"); KAi = alAB.sb([128, 9, 16], F32, "KAi")
        KnAi = alAB.sb([128, 9, 16], F32, "KnAi")
        psel = alAB.sb([128, 8, 240], BF16, "psel")
        zt = alAB.sb([128, 1024], F32, "zt")
        NPB = 3
        pst = [alAB.sb([128, 2048], F32, f"pst{i}") for i in range(NPB)]
        pbf = [alAB.sb([128, 2048], BF16, f"pbf{i}") for i in range(NPB)]
        wbfB = Buf("wbf")
        pc_next = [0]

        def precast(n, mode):
            for _ in range(n):
                ci = pc_next[0]
                if ci >= NEXP * 3:
                    return
                pc_next[0] += 1
                e_, m_ = ci // 3, ci % 3
                srcw = (weg_d, weu_d, wed_d)[m_][e_].rearrange("p c n -> p (c n)")
                s_ = pst[ci % NPB]; b_ = pbf[ci % NPB]
                dst = wbf_scr[e_, m_]
                if mode == "pool":
                    cx.op("pool", lambda e, s_=s_, srcw=srcw: e.dma_start(out=s_[:], in_=srcw), full=[s_.b], dma=True)
                    cx.op("pool", lambda e, s_=s_, b_=b_: e.tensor_copy(out=b_[:], in_=s_[:]), reads=[s_.b], full=[b_.b])
                    cx.op("pool", lambda e, b_=b_, dst=dst: e.dma_start(out=dst, in_=b_[:]), reads=[b_.b], dma=True)
                else:
                    cx.op("sp", lambda e, s_=s_, srcw=srcw: e.dma_start(out=s_[:], in_=srcw), full=[s_.b], dma=True)
                    cx.op("act", lambda e, s_=s_, b_=b_: e.copy(out=b_[:], in_=s_[:]), reads=[s_.b], full=[b_.b])
                    cx.op("act", lambda e, b_=b_, dst=dst: e.dma_start(out=dst, in_=b_[:]), reads=[b_.b], dma=True)
        cx.op("sp", lambda e: e.dma_start(out=psel[:], in_=psel_d), full=[psel.b], dma=True)
        al_outer = al
        al = Alloc(nc)
        gmix = al.sb([128, D], F32, "gmix")
        cx.op("sp", lambda e: e.dma_start(out=gmix[:], in_=gmix_d.partition_broadcast(128)),
              full=[gmix.b], dma=True)

        stg = [al.sb([128, 8, 256], F32, f"stg{i}") for i in range(2)]
        sctr = [0]

        def load_cast(dst, dst_col0, src_d, c0, c1, kch):
            for cc in range(c0, c1, 256):
                w = min(256, c1 - cc)
                s = stg[sctr[0] % 2]
                sctr[0] += 1
                src = src_d[:, cc:cc + w].rearrange("(c p) n -> p c n", p=128)
                cx.op("sp", lambda e, s=s, src=src, w=w: e.dma_start(out=s[:, 0:kch, 0:w], in_=src),
                      full=[s.b], dma=True)
                o = dst_col0 + (cc - c0)
                cx.op("pool", lambda e, s=s, o=o, w=w: e.tensor_copy(out=dst[:, 0:kch, o:o + w],
                                                                     in_=s[:, 0:kch, 0:w]),
                      reads=[s.b], writes=[dst.b])

        w_ssm_in = al.sb([128, 8, 512], BF16, "w_ssm_in")
        load_cast(w_ssm_in, 0, w_in_d, 1024, 1536, 8)
        NA_ = 4
        xt = [al.sb([128, D], F32, f"xt{i}") for i in range(NA_)]
        junk = al.sb([128, D], BF16, "junk")
        ss = [al.sb([128, 1], F32, f"ss{i}") for i in range(NA_)]
        rt = [al.sb([128, 1], F32, f"rt{i}") for i in range(NA_)]
        rstd = [al.sb([128, 1], F32, f"rstd{i}") for i in range(NA_)]
        hbf = [al.sb([128, D], BF16, f"hbf{i}") for i in range(NA_)]
        hTb = [al.sb([128, 8, T], BF16, f"hTb{i}") for i in range(2)]
        cx.op("pool", lambda e: e.memset(zt[:], 0.0), full=[zt.b])
        lstB = Buf("lst"); h2B = Buf("h2scr"); moeB = Buf("moescr"); x2B = Buf("x2scr")
        cx.op("pool", lambda e: e.dma_start(out=lst_d, in_=lst_init_d), full=[lstB], dma=True)
        cx.op("pool", lambda e: e.dma_start(out=h2_scr[SEQ:ROWS, :], in_=zt[:, 0:512].bitcast(BF16)),
              reads=[zt.b], writes=[h2B], dma=True)
        moe_flat = moe_scr.rearrange("(n p) d -> n p d", p=128)
        for n in range(0, 2 * ROWS // 128):
            tok = cx.op("pool", lambda e, n=n: e.dma_start(out=moe_flat[n], in_=zt[:, 0:512].bitcast(BF16)),
                        reads=[zt.b], dma=True)
            moeB.w[tok[0]] = tok[1]

        def a_front(i):
            p = i % NA_
            cx.op("sp", lambda e, p=p, i=i: e.dma_start(out=xt[p][:], in_=x_d[i * 128:(i + 1) * 128, :]),
                  full=[xt[p].b], dma=True)
            cx.op("act", lambda e, p=p: e.activation(out=junk[:], in_=xt[p][:], func=AF.Square,
                                                     accum_out=ss[p][:]),
                  reads=[xt[p].b], writes=[junk.b], full=[ss[p].b])
            cx.op("act", lambda e, p=p: e.activation(out=rt[p][:], in_=ss[p][:], func=AF.Sqrt,
                                                     scale=1.0 / D, bias=EPS),
                  reads=[ss[p].b], full=[rt[p].b])
            cx.op("dve", lambda e, p=p: e.reciprocal(out=rstd[p][:], in_=rt[p][:]),
                  reads=[rt[p].b], full=[rstd[p].b])
            cx.op("dve", lambda e, p=p: e.scalar_tensor_tensor(out=hbf[p][:], in0=xt[p][:],
                                                               scalar=rstd[p][:, 0:1], in1=gmix[:],
                                                               op0=ALU.mult, op1=ALU.mult),
                  reads=[xt[p].b, rstd[p].b, gmix.b], full=[hbf[p].b])

        def a_back(i):
            p = i % NA_
            blk = i // 4
            hb = hTb[blk % 2]
            pb = psb[i % 2]
            for c in range(8):
                cx.op("pe", lambda e, pb=pb, p=p, c=c: e.transpose(out=pb[:, c * 128:(c + 1) * 128],
                                                                   in_=hbf[p][:, c * 128:(c + 1) * 128],
                                                                   identity=ident_bf[:]),
                      reads=[hbf[p].b, ident_bf.b], writes=[pb.b])
            tt = i % 4
            cx.op("act", lambda e, pb=pb, hb=hb, tt=tt: e.copy(
                out=hb[:, :, tt * 128:(tt + 1) * 128],
                in_=pb[:].rearrange("p (c t) -> p c t", c=8)),
                reads=[pb.b], writes=[hb.b])
            if tt == 3:
                for f in range(4):
                    ps = getps()
                    for c in range(8):
                        cx.op("pe", lambda e, ps=ps, hb=hb, f=f, c=c: e.matmul(
                            ps[:], lhsT=w_ssm_in[:, c, f * 128:(f + 1) * 128], rhs=hb[:, c, :],
                            start=(c == 0), stop=(c == 7)),
                            reads=[w_ssm_in.b, hb.b], writes=[ps.b])
                    cx.op("dve", lambda e, ps=ps, f=f, blk=blk: e.tensor_copy(
                        out=u_all[:, f, blk * T:(blk + 1) * T], in_=ps[:]),
                        reads=[ps.b], writes=[u_all.b])
                cx.op("act", lambda e, hb=hb, blk=blk: e.dma_start(
                    out=hT_scr[:, :, blk * T:(blk + 1) * T].rearrange("c p t -> p c t"), in_=hb[:]),
                    reads=[hb.b], dma=True)

        a_front(0); a_front(1)
        for i in range(NT):
            if i + 2 < NT:
                a_front(i + 2)
            a_back(i)
            if i % 3 == 2:
                precast(1, "pool")

        if dbg:
            cx.op("sp", lambda e: e.dma_start(out=u_dbg.rearrange("f p t -> p f t"), in_=u_all[:]),
                  reads=[u_all.b], dma=True)


        cx.barrier(skip_pool_dma=True)
        al.close()
        precast(8, "pool")
        al = Alloc(nc)
        TWO_PI = 2.0 * np.pi
        cmask = al.sb([128, 128], F32, "cmask")
        cx.op("sp", lambda e: e.dma_start(out=cmask[:], in_=cmask_d), full=[cmask.b], dma=True)
        dl = al.sb([128, 32], F32, "dl")
        cx.op("sp", lambda e: e.dma_start(out=dl[:], in_=dl_d), full=[dl.b], dma=True)
        SU = Buf("ssm_setup")

        def sload(shape, src, name):
            t = al.sb(shape, F32, name)
            cx.op("sp", lambda e: e.dma_start(out=t[:], in_=src), full=[t.b], dma=True)
            return t

        lamre = sload([128, 16], lamre_d, "lamre")
        lamim = sload([128, 16], lamim_d, "lamim")
        logdt = sload([128, 16], logdt_d, "logdt")
        Bre = sload([128, 16, 16], bre_d, "Bre")
        Bim = sload([128, 16, 16], bim_d, "Bim")
        Cre = sload([128, 16, 16], cre_d, "Cre")
        Cim = sload([128, 16, 16], cim_d, "Cim")
        ins_b = [lamre.b, lamim.b, logdt.b, Bre.b, Bim.b, Cre.b, Cim.b]

        def S(shape, name):
            return al.sb(shape, F32, name)

        def dv(fn):
            cx.op("dve", fn, reads=ins_b, writes=[SU])

        def ac(fn):
            cx.op("act", fn, reads=ins_b, writes=[SU])

        def tt_(out, a, b, op):
            dv(lambda e: e.tensor_tensor(out=out, in0=a, in1=b, op=op))

        sh16 = [128, 16]
        dt_ = S(sh16, "dt"); lrd = S(sh16, "lrd"); th = S(sh16, "th")
        ac(lambda e: e.activation(out=dt_[:], in_=logdt[:], func=AF.Exp))
        tt_(lrd[:], lamre[:], dt_[:], ALU.mult)
        tt_(th[:], lamim[:], dt_[:], ALU.mult)
        mag = S(sh16, "mag"); imag2 = S(sh16, "imag2")
        ac(lambda e: e.activation(out=mag[:], in_=lrd[:], func=AF.Exp))
        ac(lambda e: e.activation(out=imag2[:], in_=lrd[:], func=AF.Exp, scale=-2.0))
        kq_i = al.sb(sh16, I32, "kq_i"); kq = S(sh16, "kq"); red = S(sh16, "red"); msk = S(sh16, "msk")
        sinv = S(sh16, "sinv"); cosv = S(sh16, "cosv"); tmpa = S(sh16, "tmpa")

        def sin_of(outt, shift):
            dv(lambda e: e.tensor_scalar(out=tmpa[:], in0=th[:], scalar1=float(shift), scalar2=None,
                                         op0=ALU.add))
            dv(lambda e: e.tensor_scalar(out=kq[:], in0=tmpa[:], scalar1=float(1.0 / TWO_PI),
                                         scalar2=None, op0=ALU.mult))
            dv(lambda e: e.tensor_copy(out=kq_i[:], in_=kq[:]))
            dv(lambda e: e.tensor_copy(out=kq[:], in_=kq_i[:]))
            dv(lambda e: e.scalar_tensor_tensor(out=red[:], in0=kq[:], scalar=float(-TWO_PI),
                                                in1=tmpa[:], op0=ALU.mult, op1=ALU.add))
            dv(lambda e: e.tensor_single_scalar(out=msk[:], in_=red[:], scalar=float(np.pi), op=ALU.is_gt))
            dv(lambda e: e.scalar_tensor_tensor(out=red[:], in0=msk[:], scalar=float(-TWO_PI),
                                                in1=red[:], op0=ALU.mult, op1=ALU.add))
            dv(lambda e: e.tensor_single_scalar(out=msk[:], in_=red[:], scalar=float(-np.pi), op=ALU.is_lt))
            dv(lambda e: e.scalar_tensor_tensor(out=red[:], in0=msk[:], scalar=float(TWO_PI),
                                                in1=red[:], op0=ALU.mult, op1=ALU.add))
            ac(lambda e: e.activation(out=outt[:], in_=red[:], func=AF.Sin))

        sin_of(sinv, 0.0)
        sin_of(cosv, np.pi / 2)
        PWr = S([128, 9, 16], "PWr"); PWi = S([128, 9, 16], "PWi")
        IPr = S([128, 8, 16], "IPr"); IPi = S([128, 8, 16], "IPi")
        t1 = S([128, 16, 8, 16], "t1"); t2 = S([128, 16, 8, 16], "t2")

        def cmul(outr, outi, ar, ai, br, bi, shp, neg_i=False):
            a1 = t1[:].rearrange("p a b c -> p (a b c)")[:, 0:int(np.prod(shp[1:]))]
            a2 = t2[:].rearrange("p a b c -> p (a b c)")[:, 0:int(np.prod(shp[1:]))]
            if len(shp) == 3:
                a1 = a1.rearrange("p (a b) -> p a b", a=shp[1])
                a2 = a2.rearrange("p (a b) -> p a b", a=shp[1])
            if len(shp) == 4:
                a1 = t1[:, :, 0:shp[2], :]
                a2 = t2[:, :, 0:shp[2], :]
            tt_(a1, ar, br, ALU.mult)
            tt_(a2, ai, bi, ALU.mult)
            tt_(outr, a1, a2, ALU.subtract)
            tt_(a1, ar, bi, ALU.mult)
            tt_(a2, ai, br, ALU.mult)
            if neg_i:
                dv(lambda e: e.scalar_tensor_tensor(out=outi, in0=a1, scalar=-1.0, in1=a2,
                                                    op0=ALU.mult, op1=ALU.subtract))
            else:
                tt_(outi, a1, a2, ALU.add)

        dv(lambda e: e.memset(PWr[:, 0, :], 1.0))
        dv(lambda e: e.memset(PWi[:, 0, :], 0.0))
        dv(lambda e: e.memset(IPr[:, 0, :], 1.0))
        dv(lambda e: e.memset(IPi[:, 0, :], 0.0))
        tt_(PWr[:, 1, :], mag[:], cosv[:], ALU.mult)
        tt_(PWi[:, 1, :], mag[:], sinv[:], ALU.mult)
        tt_(IPr[:, 1, :], PWr[:, 1, :], imag2[:], ALU.mult)
        dv(lambda e: e.scalar_tensor_tensor(out=IPi[:, 1, :], in0=PWi[:, 1, :], scalar=-1.0, in1=imag2[:],
                                            op0=ALU.mult, op1=ALU.mult))
        for n in range(2, 9):
            cmul(PWr[:, n, :], PWi[:, n, :], PWr[:, n - 1, :], PWi[:, n - 1, :], PWr[:, 1, :], PWi[:, 1, :], sh16)
        for n in range(2, 8):
            cmul(IPr[:, n, :], IPi[:, n, :], IPr[:, n - 1, :], IPi[:, n - 1, :], IPr[:, 1, :], IPi[:, 1, :], sh16)
        dv(lambda e: e.tensor_copy(out=KAr[:, 0, :], in_=PWr[:, 8, :]))
        dv(lambda e: e.tensor_copy(out=KAi[:, 0, :], in_=PWi[:, 8, :]))
        for d_ in range(1, 9):
            cmul(KAr[:, d_, :], KAi[:, d_, :], KAr[:, d_ - 1, :], KAi[:, d_ - 1, :],
                 KAr[:, d_ - 1, :], KAi[:, d_ - 1, :], sh16)
        dv(lambda e: e.tensor_scalar(out=KnAi[:], in0=KAi[:], scalar1=-1.0, scalar2=None, op0=ALU.mult))
        am1 = S(sh16, "am1"); l2 = S(sh16, "l2"); il2 = S(sh16, "il2"); kr = S(sh16, "kr"); ki = S(sh16, "ki")
        dv(lambda e: e.tensor_scalar(out=am1[:], in0=PWr[:, 1, :], scalar1=-1.0, scalar2=None, op0=ALU.add))
        tt_(l2[:], lamre[:], lamre[:], ALU.mult)
        tt_(tmpa[:], lamim[:], lamim[:], ALU.mult)
        tt_(l2[:], l2[:], tmpa[:], ALU.add)
        dv(lambda e: e.reciprocal(out=il2[:], in_=l2[:]))
        tt_(kr[:], am1[:], lamre[:], ALU.mult)
        tt_(tmpa[:], PWi[:, 1, :], lamim[:], ALU.mult)
        tt_(kr[:], kr[:], tmpa[:], ALU.add)
        tt_(kr[:], kr[:], il2[:], ALU.mult)
        tt_(ki[:], PWi[:, 1, :], lamre[:], ALU.mult)
        tt_(tmpa[:], am1[:], lamim[:], ALU.mult)
        tt_(ki[:], ki[:], tmpa[:], ALU.subtract)
        tt_(ki[:], ki[:], il2[:], ALU.mult)
        sh3 = [128, 16, 16]

        def bc(a):
            return a.unsqueeze(2).to_broadcast(sh3)

        Bbr = S(sh3, "Bbr"); Bbi = S(sh3, "Bbi")
        cmul(Bbr[:], Bbi[:], bc(kr[:]), bc(ki[:]), Bre[:], Bim[:], sh3)
        Bhr = S([128, 16, 8, 16], "Bhr"); nBhi = S([128, 16, 8, 16], "nBhi"); Bhi = S([128, 16, 8, 16], "Bhi")
        Btr = S([128, 16, 8, 16], "Btr"); Bti = S([128, 16, 8, 16], "Bti")
        Chr = S([128, 16, 9, 16], "Chr"); Chi = S([128, 16, 9, 16], "Chi"); nChi = S([128, 16, 9, 16], "nChi")
        sh4 = [128, 16, 8, 16]

        def bk(a):
            return a.rearrange("p k r -> p r k").unsqueeze(3).to_broadcast(sh4)

        def bmid(a):
            return a.unsqueeze(2).to_broadcast(sh4)

        def b2(a):
            return a.unsqueeze(2).unsqueeze(3).to_broadcast(sh4)

        cmul(Bhr[:], Bhi[:], bk(IPr[:]), bk(IPi[:]), bmid(Bbr[:]), bmid(Bbi[:]), sh4)
        cmul(Btr[:], Bti[:], b2(PWr[:, 7, :]), b2(PWi[:, 7, :]), Bhr[:], Bhi[:], sh4)
        dv(lambda e: e.tensor_scalar(out=nBhi[:], in0=Bhi[:], scalar1=-1.0, scalar2=None, op0=ALU.mult))
        cmul(Chr[:, :, 0:8, :], Chi[:, :, 0:8, :], bk(PWr[:, 0:8, :]), bk(PWi[:, 0:8, :]), bmid(Cre[:]), bmid(Cim[:]), sh4)
        cmul(Chr[:, :, 8, :], Chi[:, :, 8, :], bc(PWr[:, 8, :]), bc(PWi[:, 8, :]), Cre[:], Cim[:], sh3)
        dv(lambda e: e.tensor_scalar(out=nChi[:], in0=Chi[:], scalar1=-1.0, scalar2=None, op0=ALU.mult))
        dv(lambda e: e.tensor_copy(out=C1r[:].rearrange("p r (j c) -> p r j c", j=8), in_=Chr[:, :, 1:9, :]))
        dv(lambda e: e.tensor_copy(out=nC1i[:].rearrange("p r (j c) -> p r j c", j=8), in_=nChi[:, :, 1:9, :]))
        mtmps = [S([128, 128], "mtmp0"), S([128, 128], "mtmp1")]
        cx.op("dve", lambda e: e.memset(W2r[:], 0.0), reads=ins_b, writes=[SU])
        cx.op("dve", lambda e: e.memset(W2i[:], 0.0), reads=ins_b, writes=[SU])
        for r in range(16):
            for two in range(2):
                g = 2 * r + two
                rng = slice(two * 64, (two + 1) * 64)
                ps = getps()
                cx.op("pe", lambda e, ps=ps, r=r, rng=rng: e.matmul(
                    ps[:, 0:128], lhsT=Bhr[rng, r, :, :].rearrange("p k c -> p (k c)"),
                    rhs=Chr[rng, r, 0:8, :].rearrange("p j c -> p (j c)"), start=True, stop=False),
                    reads=[SU], writes=[ps.b])
                cx.op("pe", lambda e, ps=ps, r=r, rng=rng: e.matmul(
                    ps[:, 0:128], lhsT=nBhi[rng, r, :, :].rearrange("p k c -> p (k c)"),
                    rhs=Chi[rng, r, 0:8, :].rearrange("p j c -> p (j c)"), start=False, stop=True),
                    reads=[SU], writes=[ps.b])
                mt_ = mtmps[g % 2]
                cx.op("dve", lambda e, ps=ps, mt_=mt_: e.tensor_tensor(out=mt_[:], in0=ps[:, 0:128], in1=cmask[:],
                                                                       op=ALU.mult),
                      reads=[ps.b, cmask.b], full=[mt_.b])
                cx.op("dve", lambda e, g=g, mt_=mt_: e.scalar_tensor_tensor(
                    out=M_all[:, g, :], in0=ident_f[:], scalar=dl[:, g:g + 1], in1=mt_[:],
                    op0=ALU.mult, op1=ALU.add),
                    reads=[ident_f.b, dl.b, mt_.b], writes=[M_all.b])
            for (Bt, W2) in ((Btr, W2r), (Bti, W2i)):
                ps = getps()
                cx.op("pe", lambda e, ps=ps, r=r, Bt=Bt: e.transpose(
                    out=ps[:, 0:128], in_=Bt[:, r, :, :].rearrange("p k c -> p (k c)"), identity=ident_f[:]),
                    reads=[SU, ident_f.b], writes=[ps.b])
                cx.op("act", lambda e, ps=ps, r=r, W2=W2: e.copy(out=W2[:, r, 0, 0:64], in_=ps[:, 0:64]),
                      reads=[ps.b], writes=[W2.b])
                cx.op("act", lambda e, ps=ps, r=r, W2=W2: e.copy(out=W2[:, r, 1, 64:128], in_=ps[:, 64:128]),
                      reads=[ps.b], writes=[W2.b])

        cx.barrier(skip_pool_dma=True)
        al.close()
        al = Alloc(nc)
        NCH = SEQ // 8
        Vg = [al.sb([128, NCH], BF16, f"Vg{i}") for i in range(4)]
        Sre = [[al.sb([128, NCH], F32, f"Sre{s}{i}") for i in range(2)] for s in range(2)]
        Sim = [[al.sb([128, NCH], F32, f"Sim{s}{i}") for i in range(2)] for s in range(2)]
        Sbr = [al.sb([128, NCH], BF16, f"Sbr{s}") for s in range(2)]
        Sbi = [al.sb([128, NCH], BF16, f"Sbi{s}") for s in range(2)]
        Gg = [al.sb([128, NCH], BF16, f"Gg{i}") for i in range(16)]
        ysf = [al.sb([128, SEQ], BF16, f"ysf{i}") for i in range(2)]
        for s in range(2):
            cx.op("pool", lambda e, s=s: e.memset(Sbr[s][:, 0:1], 0.0), writes=[Sbr[s].b])
            cx.op("pool", lambda e, s=s: e.memset(Sbi[s][:, 0:1], 0.0), writes=[Sbi[s].b])

        def b_front(r):
            f = r // 4
            st = r % 2
            vg = [Vg[(2 * r) % 4], Vg[(2 * r + 1) % 4]]
            for two in range(2):
                g = 2 * r + two
                gl = g % 8
                ps = getps()
                for k in range(8):
                    cx.op("pe", lambda e, ps=ps, gl=gl, k=k, f=f: e.matmul(
                        ps[:], lhsT=psel[:, gl, (7 - k) * 16:(7 - k) * 16 + 128],
                        rhs=u_all[:, f, k:SEQ:8], start=(k == 0), stop=(k == 7)),
                        reads=[psel.b, u_all.b], writes=[ps.b])
                cx.op("act", lambda e, ps=ps, v=vg[two]: e.copy(out=v[:], in_=ps[:]),
                      reads=[ps.b], full=[vg[two].b])
            psr = getps(); psi = getps()
            for (pp, W2) in ((psr, W2r), (psi, W2i)):
                for two in range(2):
                    cx.op("pe", lambda e, pp=pp, W2=W2, two=two, r=r, v=vg[two]: e.matmul(
                        pp[:], lhsT=W2[:, r, two, :], rhs=v[:], start=(two == 0), stop=(two == 1)),
                        reads=[W2.b, vg[two].b], writes=[pp.b])
            cx.op("act", lambda e, psr=psr, st=st: e.copy(out=Sre[st][0][:], in_=psr[:]),
                  reads=[psr.b], full=[Sre[st][0].b])
            cx.op("act", lambda e, psi=psi, st=st: e.copy(out=Sim[st][0][:], in_=psi[:]),
                  reads=[psi.b], full=[Sim[st][0].b])

        def b_mid(r):
            st = r % 2
            cur = 0
            for d_ in range(9):
                sh = 1 << d_
                s_r, s_i, d_r, d_i = Sre[st][cur], Sim[st][cur], Sre[st][1 - cur], Sim[st][1 - cur]
                n = NCH - sh
                cx.op("dve", lambda e, s_r=s_r, d_r=d_r, sh=sh, n=n, d_=d_, r=r: e.scalar_tensor_tensor(
                    out=d_r[:, sh:NCH], in0=s_r[:, 0:n], scalar=KAr[:, d_, r:r + 1], in1=s_r[:, sh:NCH],
                    op0=ALU.mult, op1=ALU.add), reads=[s_r.b, SU], writes=[d_r.b])
                cx.op("dve", lambda e, s_i=s_i, d_r=d_r, sh=sh, n=n, d_=d_, r=r: e.scalar_tensor_tensor(
                    out=d_r[:, sh:NCH], in0=s_i[:, 0:n], scalar=KnAi[:, d_, r:r + 1], in1=d_r[:, sh:NCH],
                    op0=ALU.mult, op1=ALU.add), reads=[s_i.b, SU], writes=[d_r.b])
                cx.op("dve", lambda e, s_i=s_i, d_i=d_i, sh=sh, n=n, d_=d_, r=r: e.scalar_tensor_tensor(
                    out=d_i[:, sh:NCH], in0=s_i[:, 0:n], scalar=KAr[:, d_, r:r + 1], in1=s_i[:, sh:NCH],
                    op0=ALU.mult, op1=ALU.add), reads=[s_i.b, SU], writes=[d_i.b])
                cx.op("dve", lambda e, s_r=s_r, d_i=d_i, sh=sh, n=n, d_=d_, r=r: e.scalar_tensor_tensor(
                    out=d_i[:, sh:NCH], in0=s_r[:, 0:n], scalar=KAi[:, d_, r:r + 1], in1=d_i[:, sh:NCH],
                    op0=ALU.mult, op1=ALU.add), reads=[s_r.b, SU], writes=[d_i.b])
                cx.op("pool", lambda e, s_r=s_r, d_r=d_r, sh=sh: e.tensor_copy(out=d_r[:, 0:sh], in_=s_r[:, 0:sh]),
                      reads=[s_r.b], writes=[d_r.b])
                cx.op("pool", lambda e, s_i=s_i, d_i=d_i, sh=sh: e.tensor_copy(out=d_i[:, 0:sh], in_=s_i[:, 0:sh]),
                      reads=[s_i.b], writes=[d_i.b])
                cur = 1 - cur
            fr, fi = Sre[st][cur], Sim[st][cur]
            cx.op("pool", lambda e, fr=fr, st=st: e.tensor_copy(out=Sbr[st][:, 1:NCH], in_=fr[:, 0:NCH - 1]),
                  reads=[fr.b], writes=[Sbr[st].b])
            cx.op("pool", lambda e, fi=fi, st=st: e.tensor_copy(out=Sbi[st][:, 1:NCH], in_=fi[:, 0:NCH - 1]),
                  reads=[fi.b], writes=[Sbi[st].b])

        def b_back(r):
            f = r // 4
            st = r % 2
            vg = [Vg[(2 * r) % 4], Vg[(2 * r + 1) % 4]]
            for two in range(2):
                g = 2 * r + two
                rng = slice(two * 64, (two + 1) * 64)
                ps = getps()
                cx.op("pe", lambda e, ps=ps, g=g, v=vg[two]: e.matmul(
                    ps[:], lhsT=M_all[:, g, :], rhs=v[:], start=True, stop=False),
                    reads=[M_all.b, vg[two].b], writes=[ps.b])
                cx.op("pe", lambda e, ps=ps, r=r, rng=rng, st=st: e.matmul(
                    ps[:], lhsT=C1r[rng, r, :], rhs=Sbr[st][rng, :], start=False, stop=False),
                    reads=[SU, Sbr[st].b], writes=[ps.b])
                cx.op("pe", lambda e, ps=ps, r=r, rng=rng, st=st: e.matmul(
                    ps[:], lhsT=nC1i[rng, r, :], rhs=Sbi[st][rng, :], start=False, stop=True),
                    reads=[SU, Sbi[st].b], writes=[ps.b])
                gg = Gg[g % 16]
                cx.op("act", lambda e, ps=ps, gg=gg: e.activation(out=gg[:], in_=ps[:], func=GELU),
                      reads=[ps.b], full=[gg.b])
            if r % 4 == 3:
                yb = ysf[f % 2]
                for j in range(8):
                    ps = getps()
                    for gl in range(8):
                        gg = Gg[(8 * f + gl) % 16]
                        cx.op("pe", lambda e, ps=ps, j=j, gl=gl, gg=gg: e.matmul(
                            ps[:], lhsT=psel[:, j, (7 - gl) * 16:(7 - gl) * 16 + 128], rhs=gg[:],
                            start=(gl == 0), stop=(gl == 7)),
                            reads=[psel.b, gg.b], writes=[ps.b])
                    cx.op("act", lambda e, ps=ps, yb=yb, j=j: e.copy(out=yb[:, j:SEQ:8], in_=ps[:]),
                          reads=[ps.b], writes=[yb.b])
                cx.op("sp", lambda e, yb=yb, f=f: e.dma_start(out=ys_scr[f], in_=yb[:]),
                      reads=[yb.b], dma=True)

        b_front(0)
        for r in range(16):
            precast(2, "act")
            b_mid(r)
            precast(2, "act")
            if r + 1 < 16:
                b_front(r + 1)
            precast(1, "act")
            b_back(r)
        precast(NEXP * 3, "act")

        cx.barrier()
        al.close()
        alAB.close()
        al = al_outer
        if stop_after in ("A", "B"):
            pass
        else:
            TC = 256
            NBC = SEQ // TC
            alC = Alloc(nc)
            wA = alC.sb([128, 8, 1536], BF16, "wA")
            wG = alC.sb([128, 8, 3072], BF16, "wG")
            wco = alC.sb([128, 4, 1024], BF16, "wco")
            wgl = alC.sb([128, 4, 2048], BF16, "wgl")
            wmo = alC.sb([128, 4, 1024], BF16, "wmo")
            wo = alC.sb([128, 8, 1024], BF16, "wo")
            Dg2 = [alC.sb([128, 31, 128], BF16, f"Dg{i}") for i in range(2)]
            kT = alC.sb([128, 4, 256], BF16, "kT")
            vtok = alC.sb([128, 2, 512], BF16, "vtok")
            gffn = alC.sb([128, D], F32, "gffn")
            wr = alC.sb([128, 8, 36], F32, "wr")
            rbias = alC.sb([128, 36], F32, "rbias")
            cdw = alC.sb([128, 4, 31], F32, "cdw")
            cb = alC.sb([128, 4], F32, "cb"); lng = alC.sb([128, 4], F32, "lng"); lnb = alC.sb([128, 4], F32, "lnb")
            onesm = alC.sb([128, 128], F32, "onesm")
            ones_bf = alC.sb([128, 128], BF16, "ones_bf")
            ecap = alC.sb([128, 32], F32, "ecap")
            tokid = alC.sb([128, NT], F32, "tokid")
            cum = alC.sb([128, 32], F32, "cum")
            lg_all = alC.sb([128, NT, 36], F32, "lg_all")
            trashp = alC.sb([128, 1], F32, "trashp")

            def ld(t, src):
                cx.op("sp", lambda e: e.dma_start(out=t[:], in_=src), full=[t.b], dma=True)

            ld(gffn, gffn_d.partition_broadcast(128))
            ld(wr, wr_d.rearrange("(c p) n -> p c n", p=128))
            ld(rbias, rbias_d.partition_broadcast(128))
            ld(cdw, cdw_d); ld(cb, cb_d); ld(lng, lng_d); ld(lnb, lnb_d)
            ld(ecap, ecap_d); ld(tokid, tokid_d); ld(trashp, trashp_d)
            cx.op("pool", lambda e: e.memset(onesm[:], 1.0 / 512.0), full=[onesm.b])
            cx.op("pool", lambda e: e.memset(ones_bf[:], 1.0), full=[ones_bf.b])
            cx.op("pool", lambda e: e.memset(cum[:], 0.0), full=[cum.b])
            alS = Alloc(nc)
            stg2 = [alS.sb([128, 8, 256], F32, f"stgc{i}") for i in range(2)]
            s2 = [0]

            def load_cast2(dst, dst_col0, src_d, c0, c1, kch, engs=("pool", "act")):
                for cc in range(c0, c1, 256):
                    w = min(256, c1 - cc)
                    s = stg2[s2[0] % 2]
                    eng = engs[s2[0] % len(engs)]
                    s2[0] += 1
                    src = src_d[:, cc:cc + w].rearrange("(c p) n -> p c n", p=128)
                    cx.op("sp", lambda e, s=s, src=src, w=w: e.dma_start(out=s[:, 0:kch, 0:w], in_=src),
                          full=[s.b], dma=True)
                    o = dst_col0 + (cc - c0)
                    if eng == "act":
                        cx.op("act", lambda e, s=s, o=o, w=w: e.copy(out=dst[:, 0:kch, o:o + w], in_=s[:, 0:kch, 0:w]),
                              reads=[s.b], writes=[dst.b])
                    else:
                        cx.op(eng, lambda e, s=s, o=o, w=w: e.tensor_copy(out=dst[:, 0:kch, o:o + w],
                                                                          in_=s[:, 0:kch, 0:w]),
                              reads=[s.b], writes=[dst.b])

            load_cast2(wA, 0, w_in_d, 0, 1024, 8)
            load_cast2(wA, 1024, w_in_d, 1536, 2048, 8)
            load_cast2(wG, 0, w_in_d, 2048, 5120, 8)
            load_cast2(wco, 0, wco_d, 0, 1024, 4)
            load_cast2(wgl, 0, wgl_d, 0, 2048, 4)
            load_cast2(wmo, 0, wmo_d, 0, 1024, 4)
            load_cast2(wo, 0, wo_d, 0, 1024, 8)
            wkv = alS.sb([128, 8, 1024], BF16, "wkv")
            load_cast2(wkv, 0, wkv_d, 0, 1024, 8)
            gmem = alS.sb([128, D], F32, "gmem")
            ld(gmem, gmem_d.partition_broadcast(128))
            memT = alS.sb([128, 8, 256], BF16, "memT")
            mx = alS.sb([128, D], F32, "mx"); mjunk = alS.sb([128, D], BF16, "mjunk")
            mss = alS.sb([128, 1], F32, "mss"); mrt = alS.sb([128, 1], F32, "mrt"); mrs = alS.sb([128, 1], F32, "mrs")
            mh = alS.sb([128, D], BF16, "mh")
            for mt in range(2):
                cx.op("sp", lambda e, mt=mt: e.dma_start(out=mx[:], in_=mem_d[mt * 128:(mt + 1) * 128, :]),
                      full=[mx.b], dma=True)
                cx.op("act", lambda e: e.activation(out=mjunk[:], in_=mx[:], func=AF.Square, accum_out=mss[:]),
                      reads=[mx.b], full=[mjunk.b, mss.b])
                cx.op("act", lambda e: e.activation(out=mrt[:], in_=mss[:], func=AF.Sqrt, scale=1.0 / D, bias=EPS),
                      reads=[mss.b], full=[mrt.b])
                cx.op("dve", lambda e: e.reciprocal(out=mrs[:], in_=mrt[:]), reads=[mrt.b], full=[mrs.b])
                cx.op("dve", lambda e: e.scalar_tensor_tensor(out=mh[:], in0=mx[:], scalar=mrs[:, 0:1], in1=gmem[:],
                                                              op0=ALU.mult, op1=ALU.mult),
                      reads=[mx.b, mrs.b, gmem.b], full=[mh.b])
                pb = psb[mt % 2]
                for c in range(8):
                    cx.op("pe", lambda e, pb=pb, c=c: e.transpose(out=pb[:, c * 128:(c + 1) * 128],
                                                                  in_=mh[:, c * 128:(c + 1) * 128],
                                                                  identity=ident_bf[:]),
                          reads=[mh.b, ident_bf.b], writes=[pb.b])
                cx.op("act", lambda e, pb=pb, mt=mt: e.copy(out=memT[:, :, mt * 128:(mt + 1) * 128],
                                                            in_=pb[:].rearrange("p (c t) -> p c t", c=8)),
                      reads=[pb.b], writes=[memT.b])
            for hd in range(4):
                ps = getps()
                for c in range(8):
                    cx.op("pe", lambda e, ps=ps, c=c, hd=hd: e.matmul(
                        ps[:, 0:256], lhsT=wkv[:, c, hd * 128:(hd + 1) * 128], rhs=memT[:, c, :],
                        start=(c == 0), stop=(c == 7)), reads=[wkv.b, memT.b], writes=[ps.b])
                cx.op("dve", lambda e, ps=ps, hd=hd: e.tensor_copy(out=kT[:, hd, :], in_=ps[:, 0:256]),
                      reads=[ps.b], writes=[kT.b])
            for mc in range(2):
                ps = getps()
                for c in range(8):
                    cx.op("pe", lambda e, ps=ps, c=c, mc=mc: e.matmul(
                        ps[:], lhsT=memT[:, c, mc * 128:(mc + 1) * 128], rhs=wkv[:, c, 512:1024],
                        start=(c == 0), stop=(c == 7)), reads=[wkv.b, memT.b], writes=[ps.b])
                cx.op("dve", lambda e, ps=ps, mc=mc: e.tensor_copy(out=vtok[:, mc, :], in_=ps[:]),
                      reads=[ps.b], writes=[vtok.b])
            cx.barrier()
            alS.close()

            alW = Alloc(nc)
            hT = [alW.sb([128, 8, TC], BF16, f"hTc{i}") for i in range(2)]
            ysb = [alW.sb([128, 4, TC], BF16, "ysb0")] * 2
            vbuf = alW.sb([128, 4, 30 + TC], BF16, "vbuf")
            sgt = [alW.sb([128, TC], F32, f"sgt{i}") for i in range(3)]
            cv = alW.sb([128, 4, TC], F32, "cv")
            sq = [sgt[1], sgt[2]]
            mean = alW.sb([128, TC], F32, "mean")
            var = alW.sb([128, TC], F32, "var"); lrs = alW.sb([128, TC], F32, "lrs")
            m2 = var; lnv = lrs
            cn = alW.sb([128, 4, TC], BF16, "cn")
            qb = alW.sb([128, 4, TC], BF16, "qb")
            Eb = [alW.sb([128, 2, TC], BF16, f"Eb{i}") for i in range(2)]
            ob = alW.sb([128, 4, TC], BF16, "ob")
            macc = alW.sb([128, TC], F32, "macc"); mt1 = alW.sb([128, TC], F32, "mt1"); mt2 = alW.sb([128, TC], F32, "mt2")
            rden = macc
            xc = [mt1, mt2]
            sqf = [sgt[1], sgt[2], mt1, mt2]
            merged = alW.sb([128, 8, TC], BF16, "merged")
            xt2 = [alW.sb([128, D], F32, f"xtc{i}") for i in range(2)]
            x2t = xt2
            h2f = alW.sb([128, D], F32, "h2f"); h2b = [alW.sb([128, D], BF16, "h2b0")] * 2
            junk2 = h2b[0]
            h2T = alW.sb([128, 8, 128], F32, "h2T")
            ss2 = alW.sb([128, 1], F32, "ss2"); rt2 = alW.sb([128, 1], F32, "rt2"); rs2 = alW.sb([128, 1], F32, "rs2")
            cx.op("pool", lambda e: e.memset(vbuf[:], 0.0), full=[vbuf.b])

            breg = {}

            def mmgrp(ps_ap, ps_b, pairs, reads):
                n = len(pairs)
                for idx, (l, r_) in enumerate(pairs):
                    cx.op("pe", lambda e, l=l, r_=r_, idx=idx: e.matmul(ps_ap, lhsT=l, rhs=r_, start=(idx == 0),
                                                                         stop=(idx == n - 1)),
                          reads=reads, writes=[ps_b])

            KCUT = int(os.environ.get("KCUT", "9"))
            KNB = int(os.environ.get("KNB", str(NBC)))
            def c_load_h(bi):
                t0 = bi * TC
                h = hT[bi % 2]
                cx.op("sp", lambda e, h=h, t0=t0: e.dma_start(
                    out=h[:], in_=hT_scr[:, :, t0:t0 + TC].rearrange("c p t -> p c t")), full=[h.b], dma=True)

            def c_load_y(bi):
                t0 = bi * TC
                yb = ysb[bi % 2]
                cx.op("sp", lambda e, yb=yb, t0=t0: e.dma_start(
                    out=yb[:], in_=ys_scr[:, :, t0:t0 + TC].rearrange("f p t -> p f t")), full=[yb.b], dma=True)

            def c_s2(bi):
                t0 = bi * TC
                h = hT[bi % 2]; yb = ysb[bi % 2]
                for f in range(4):
                    pa = getps(); pg = getps()
                    mmgrp(pa[:, 0:TC], pa.b, [(wA[:, c, f * 128:(f + 1) * 128], h[:, c, :]) for c in range(8)],
                          [wA.b, h.b])
                    mmgrp(pg[:, 0:TC], pg.b, [(wA[:, c, 512 + f * 128:512 + (f + 1) * 128], h[:, c, :]) for c in range(8)],
                          [wA.b, h.b])
                    s = sgt[f % 3]
                    cx.op("act", lambda e, pg=pg, s=s: e.activation(out=s[:], in_=pg[:, 0:TC], func=AF.Sigmoid),
                          reads=[pg.b], full=[s.b])
                    cx.op("dve", lambda e, pa=pa, s=s, f=f: e.tensor_tensor(out=vbuf[:, f, 30:30 + TC], in0=pa[:, 0:TC],
                                                                            in1=s[:], op=ALU.mult),
                          reads=[pa.b, s.b], writes=[vbuf.b])

            def c_taps(bi, fs):
                for f in fs:
                    pc = getps()
                    Dg = Dg2[f % 2]
                    cx.op("pool", lambda e, Dg=Dg, f=f: e.tensor_tensor(
                        out=Dg[:], in0=ident_f[:].unsqueeze(1).to_broadcast([128, 31, 128]),
                        in1=cdw[:, f, :].unsqueeze(2).to_broadcast([128, 31, 128]), op=ALU.mult),
                        reads=[ident_f.b, cdw.b], full=[Dg.b])
                    mmgrp(pc[:, 0:TC], pc.b, [(Dg[:, k, :], vbuf[:, f, k:k + TC]) for k in range(31)],
                          [Dg.b, vbuf.b])
                    cx.op("act", lambda e, pc=pc, f=f: e.activation(out=cv[:, f, :], in_=pc[:, 0:TC], func=AF.Identity,
                                                                    bias=cb[:, f:f + 1], scale=1.0),
                          reads=[pc.b, cb.b], writes=[cv.b])
                if 3 in fs:
                    cx.op("pool", lambda e: e.tensor_copy(out=vbuf[:, :, 0:30], in_=vbuf[:, :, TC:TC + 30]),
                          reads=[vbuf.b], writes=[vbuf.b])

            def c_rest(bi):
                t0 = bi * TC
                h = hT[bi % 2]; yb = ysb[bi % 2]
                for hd in range(4):
                    pq_ = getps()
                    mmgrp(pq_[:, 0:TC], pq_.b, [(wA[:, c, 1024 + hd * 128:1024 + (hd + 1) * 128], h[:, c, :])
                                                for c in range(8)], [wA.b, h.b])
                    cx.op("dve", lambda e, pq_=pq_, hd=hd: e.tensor_copy(out=qb[:, hd, :], in_=pq_[:, 0:TC]),
                          reads=[pq_.b], writes=[qb.b])
                for f in range(4):
                    cx.op("act", lambda e, f=f: e.activation(out=sq[f % 2][:] if False else sqf[f][:], in_=cv[:, f, :],
                                                             func=AF.Square),
                          reads=[cv.b], full=[sqf[f].b])

                def att_scores(hd):
                    E = Eb[hd % 2]
                    for mc in range(2):
                        psc = getps()
                        mmgrp(psc[:, 0:TC], psc.b, [(kT[:, hd, mc * 128:(mc + 1) * 128], qb[:, hd, :])], [kT.b, qb.b])
                        cx.op("act", lambda e, psc=psc, E=E, mc=mc: e.activation(
                            out=E[:, mc, :], in_=psc[:, 0:TC], func=AF.Exp, scale=float(128 ** -0.5)),
                            reads=[psc.b], writes=[E.b])

                def att_out(hd):
                    E = Eb[hd % 2]
                    po = getps(); pd = getps()
                    mmgrp(po[:, 0:TC], po.b, [(vtok[:, mc, hd * 128:(hd + 1) * 128], E[:, mc, :]) for mc in range(2)],
                          [vtok.b, E.b])
                    mmgrp(pd[:, 0:TC], pd.b, [(ones_bf[:], E[:, mc, :]) for mc in range(2)], [ones_bf.b, E.b])
                    cx.op("dve", lambda e, pd=pd: e.reciprocal(out=rden[:], in_=pd[:, 0:TC]), reads=[pd.b], full=[rden.b])
                    cx.op("dve", lambda e, po=po, hd=hd: e.tensor_tensor(out=ob[:, hd, :], in0=po[:, 0:TC], in1=rden[:],
                                                                         op=ALU.mult),
                          reads=[po.b, rden.b], writes=[ob.b])

                att_scores(0)
                att_scores(1)
                pm = getps(); pq = getps()
                mmgrp(pm[:, 0:TC], pm.b, [(onesm[:], cv[:, f, :]) for f in range(4)], [onesm.b, cv.b])
                mmgrp(pq[:, 0:TC], pq.b, [(onesm[:], sqf[f][:]) for f in range(4)], [onesm.b] + [sqf[f].b for f in range(4)])
                cx.op("act", lambda e, pm=pm: e.copy(out=mean[:], in_=pm[:, 0:TC]), reads=[pm.b], full=[mean.b])
                cx.op("dve", lambda e: e.tensor_tensor(out=var[:], in0=mean[:], in1=mean[:], op=ALU.mult),
                      reads=[mean.b], full=[var.b])
                cx.op("dve", lambda e, pq=pq: e.tensor_tensor(out=var[:], in0=pq[:, 0:TC], in1=var[:], op=ALU.subtract),
                      reads=[pq.b], writes=[var.b])
                cx.op("dve", lambda e: e.tensor_scalar(out=var[:], in0=var[:], scalar1=float(EPS), scalar2=None,
                                                       op0=ALU.add), reads=[var.b], writes=[var.b])
                cx.op("act", lambda e: e.activation(out=lrs[:], in_=var[:], func=AF.Ln), reads=[var.b], full=[lrs.b])
                cx.op("act", lambda e: e.activation(out=lrs[:], in_=lrs[:], func=AF.Exp, scale=-0.5),
                      reads=[], writes=[lrs.b])
                cx.op("dve", lambda e: e.tensor_tensor(out=cv[:], in0=cv[:],
                                                       in1=mean[:].unsqueeze(1).to_broadcast([128, 4, TC]),
                                                       op=ALU.subtract), reads=[mean.b], writes=[cv.b])
                cx.op("dve", lambda e: e.tensor_tensor(out=cv[:], in0=cv[:],
                                                       in1=lrs[:].unsqueeze(1).to_broadcast([128, 4, TC]),
                                                       op=ALU.mult), reads=[lrs.b], writes=[cv.b])
                att_out(0)
                att_scores(2)
                att_out(1)
                att_scores(3)
                att_out(2)
                att_out(3)
                for f in range(4):
                    cx.op("act", lambda e, f=f: e.activation(out=cn[:, f, :], in_=cv[:, f, :], func=AF.Silu,
                                                             bias=lnb[:, f:f + 1], scale=lng[:, f:f + 1]),
                          reads=[cv.b, lnb.b, lng.b], writes=[cn.b])
                for j in range(8):
                    js = slice(j * 128, (j + 1) * 128)
                    pga = getps(); pyc = getps()
                    mmgrp(pga[:, 0:TC], pga.b, [(wG[:, c, j * 128:(j + 1) * 128], h[:, c, :]) for c in range(8)], [wG.b, h.b])
                    mmgrp(pyc[:, 0:TC], pyc.b, [(wco[:, f, js], cn[:, f, :]) for f in range(4)], [wco.b, cn.b])
                    s = sgt[0]
                    cx.op("act", lambda e, pga=pga, s=s: e.activation(out=s[:], in_=pga[:, 0:TC], func=AF.Sigmoid),
                          reads=[pga.b], full=[s.b])
                    cx.op("dve", lambda e, pyc=pyc, s=s: e.tensor_tensor(out=macc[:], in0=pyc[:, 0:TC], in1=s[:], op=ALU.mult),
                          reads=[pyc.b, s.b], full=[macc.b])
                    pgb = getps(); pza = getps(); pzb = getps()
                    mmgrp(pgb[:, 0:TC], pgb.b, [(wG[:, c, 1024 + j * 128:1024 + (j + 1) * 128], h[:, c, :]) for c in range(8)],
                          [wG.b, h.b])
                    mmgrp(pza[:, 0:TC], pza.b, [(wgl[:, f, js], yb[:, f, :]) for f in range(4)], [wgl.b, yb.b])
                    mmgrp(pzb[:, 0:TC], pzb.b, [(wgl[:, f, 1024 + j * 128:1024 + (j + 1) * 128], yb[:, f, :]) for f in range(4)],
                          [wgl.b, yb.b])
                    sb_ = sgt[1]; sz = sgt[2]
                    cx.op("act", lambda e, pgb=pgb, sb_=sb_: e.activation(out=sb_[:], in_=pgb[:, 0:TC], func=AF.Sigmoid),
                          reads=[pgb.b], full=[sb_.b])
                    cx.op("act", lambda e, pzb=pzb, sz=sz: e.activation(out=sz[:], in_=pzb[:, 0:TC], func=AF.Sigmoid),
                          reads=[pzb.b], full=[sz.b])
                    cx.op("dve", lambda e, pza=pza, sz=sz: e.tensor_tensor(out=mt1[:], in0=pza[:, 0:TC], in1=sz[:], op=ALU.mult),
                          reads=[pza.b, sz.b], full=[mt1.b])
                    cx.op("dve", lambda e, sb_=sb_: e.tensor_tensor(out=mt1[:], in0=mt1[:], in1=sb_[:], op=ALU.mult),
                          reads=[sb_.b], writes=[mt1.b])
                    cx.op("dve", lambda e: e.tensor_tensor(out=macc[:], in0=macc[:], in1=mt1[:], op=ALU.add),
                          reads=[mt1.b], writes=[macc.b])
                    pgc = getps(); pym = getps()
                    mmgrp(pgc[:, 0:TC], pgc.b, [(wG[:, c, 2048 + j * 128:2048 + (j + 1) * 128], h[:, c, :]) for c in range(8)],
                          [wG.b, h.b])
                    mmgrp(pym[:, 0:TC], pym.b, [(wmo[:, hd, js], ob[:, hd, :]) for hd in range(4)], [wmo.b, ob.b])
                    s = sgt[0]
                    cx.op("act", lambda e, pgc=pgc, s=s: e.activation(out=s[:], in_=pgc[:, 0:TC], func=AF.Sigmoid),
                          reads=[pgc.b], full=[s.b])
                    cx.op("dve", lambda e, pym=pym, s=s: e.tensor_tensor(out=mt2[:], in0=pym[:, 0:TC], in1=s[:], op=ALU.mult),
                          reads=[pym.b, s.b], full=[mt2.b])
                    cx.op("dve", lambda e, j=j: e.tensor_tensor(out=merged[:, j, :], in0=macc[:], in1=mt2[:], op=ALU.add),
                          reads=[macc.b, mt2.b], writes=[merged.b])

            def c_tail_a(bi):
                ntt = TC // 128
                tis = [bi * ntt + tt for tt in range(ntt)]
                for tt, ti in enumerate(tis):
                    xt_ = xt2[ti % 2]
                    cx.op("sp", lambda e, xt_=xt_, ti=ti: e.dma_start(out=xt_[:], in_=x_d[ti * 128:(ti + 1) * 128, :]),
                          full=[xt_.b], dma=True)
                for tt, ti in enumerate(tis):
                    xt_ = xt2[ti % 2]; x2 = xt_
                    for half in range(2):
                        po_ = getps()
                        mmgrp(po_[:], po_.b, [(merged[:, j, tt * 128:(tt + 1) * 128], wo[:, j, half * 512:(half + 1) * 512])
                                              for j in range(8)], [merged.b, wo.b])
                        cx.op("dve", lambda e, po_=po_, x2=x2, xt_=xt_, half=half: e.tensor_tensor(
                            out=x2[:, half * 512:(half + 1) * 512], in0=po_[:], in1=xt_[:, half * 512:(half + 1) * 512],
                            op=ALU.add), reads=[po_.b, xt_.b], writes=[x2.b])
                    cx.op("sp", lambda e, x2=x2, ti=ti: e.dma_start(out=x2_scr[ti * 128:(ti + 1) * 128, :], in_=x2[:]),
                          reads=[x2.b], dma=True)

            def c_tail_norm(ti):
                if True:
                    x2 = xt2[ti % 2]; hb2 = h2b[0]
                    cx.op("act", lambda e, x2=x2: e.activation(out=junk2[:], in_=x2[:], func=AF.Square, accum_out=ss2[:]),
                          reads=[x2.b], full=[junk2.b, ss2.b])
                    cx.op("act", lambda e: e.activation(out=rt2[:], in_=ss2[:], func=AF.Sqrt, scale=1.0 / D, bias=EPS),
                          reads=[ss2.b], full=[rt2.b])
                    cx.op("dve", lambda e: e.reciprocal(out=rs2[:], in_=rt2[:]), reads=[rt2.b], full=[rs2.b])
                    cx.op("dve", lambda e, x2=x2: e.scalar_tensor_tensor(out=h2f[:], in0=x2[:], scalar=rs2[:, 0:1],
                                                                         in1=gffn[:], op0=ALU.mult, op1=ALU.mult),
                          reads=[x2.b, rs2.b, gffn.b], full=[h2f.b])
                    cx.op("act", lambda e, hb2=hb2: e.copy(out=hb2[:], in_=h2f[:]), reads=[h2f.b], full=[hb2.b])
                    cx.op("sp", lambda e, hb2=hb2, ti=ti: e.dma_start(out=h2_scr[ti * 128:(ti + 1) * 128, :], in_=hb2[:]),
                          reads=[hb2.b], dma=True)

            def c_tail_pe(ti):
                if True:
                    pra = getps(); prb = getps()
                    for c in range(8):
                        pr = pra if c < 4 else prb
                        cx.op("pe", lambda e, pr=pr, c=c: e.transpose(out=pr[:, (c % 4) * 128:(c % 4 + 1) * 128],
                                                                      in_=h2f[:, c * 128:(c + 1) * 128], identity=ident_f[:]),
                              reads=[h2f.b, ident_f.b], writes=[pr.b])
                    cx.op("act", lambda e, pra=pra: e.copy(out=h2T[:, 0:4, :], in_=pra[:].rearrange("p (c t) -> p c t", c=4)),
                          reads=[pra.b], writes=[h2T.b])
                    cx.op("dve", lambda e, prb=prb: e.tensor_copy(out=h2T[:, 4:8, :],
                                                                  in_=prb[:].rearrange("p (c t) -> p c t", c=4)),
                          reads=[prb.b], writes=[h2T.b])
                    plg = getps()
                    mmgrp(plg[:, 0:36], plg.b, [(h2T[:, c, :], wr[:, c, :]) for c in range(8)], [h2T.b, wr.b])
                    cx.op("dve", lambda e, plg=plg, ti=ti: e.tensor_tensor(out=lg_all[:, ti, :], in0=plg[:, 0:36],
                                                                        in1=rbias[:], op=ALU.add),
                          reads=[plg.b, rbias.b], writes=[lg_all.b])

            NBR = min(NBC, KNB) if KCUT >= 2 else 0
            if NBR > 0:
                c_load_h(0); c_load_y(0); c_s2(0); c_taps(0, [0, 1, 2, 3])
            for bi in range(NBR):
                nxt = bi + 1 < NBR
                if nxt:
                    c_load_h(bi + 1)
                c_rest(bi)
                if nxt:
                    c_load_y(bi + 1)
                    c_s2(bi + 1)
                c_tail_a(bi)
                t_a, t_b = 2 * bi, 2 * bi + 1
                c_tail_norm(t_a)
                if nxt:
                    c_taps(bi + 1, [0, 1])
                c_tail_pe(t_a)
                c_tail_norm(t_b)
                if nxt:
                    c_taps(bi + 1, [2, 3])
                c_tail_pe(t_b)
            cx.barrier()
            alW.close()
            alR = Alloc(nc)
            RS = Buf("route")
            tri = alR.sb([128, 128], F32, "tri")
            ones_f = alR.sb([128, 128], F32, "ones_f")
            cx.op("sp", lambda e: e.dma_start(out=tri[:], in_=tri_d), full=[tri.b], dma=True)
            cx.op("pool", lambda e: e.memset(ones_f[:], 1.0), full=[ones_f.b])

            def rd(fn, extra_reads=(), extra_writes=()):
                cx.op("dve", fn, reads=[RS, lg_all.b] + list(extra_reads), writes=[RS] + list(extra_writes))

            def R(shape, name, dt=F32):
                return alR.sb(shape, dt, name)

            NTT = NT
            NEB_ = NEXP * NBLK
            gmax = R([128, NTT], "gmax"); ohg = R([128, NTT, 4], "ohg"); eg = R([128, NTT, 4], "eg")
            sumg = R([128, NTT], "sumg"); ptop = R([128, NTT], "ptop")
            selm = R([128, NTT, 4, 8], "selm"); sel = R([128, NTT, 8], "sel"); sel2 = R([128, NTT, 8], "sel2")
            m1_ = R([128, NTT], "m1_"); m2_ = R([128, NTT], "m2_"); oh1 = R([128, NTT, 8], "oh1"); oh2 = R([128, NTT, 8], "oh2")
            dm = R([128, NTT], "dm"); w1 = R([128, NTT], "w1"); w2 = R([128, NTT], "w2")
            M1 = R([128, NTT, 4, 8], "M1"); M2 = R([128, NTT, 4, 8], "M2"); Mc = R([128, NTT, 32], "Mc")
            Cex = R([128, NTT, 32], "Cex"); pos = R([128, NTT, 32], "pos"); bk = R([128, NTT, 32], "bk")
            sf = R([128, NTT, 32], "sf"); ov = R([128, NTT, 32], "ov"); tq = R([128, NTT, 32], "tq")
            sk = [R([128, NTT], f"sk{k}") for k in range(2)]; okk = R([128, NTT], "okk"); dd = R([128, NTT], "dd")
            si = [R([128, NTT], f"si{k}", I32) for k in range(2)]
            ent = [R([128, NTT, 4], f"ent{k}") for k in range(2)]
            le4 = lg_all[:, :, 4:36].rearrange("p t (g j) -> p t g j", g=4)

            def bc3(a, n):
                return a.unsqueeze(2).to_broadcast([128, NTT, n])

            rd(lambda e: e.tensor_reduce(out=gmax[:], in_=lg_all[:, :, 0:4], axis=AX.X, op=ALU.max))
            rd(lambda e: e.tensor_tensor(out=ohg[:], in0=lg_all[:, :, 0:4], in1=bc3(gmax[:], 4), op=ALU.is_equal))
            rd(lambda e: e.tensor_tensor(out=eg[:], in0=lg_all[:, :, 0:4], in1=bc3(gmax[:], 4), op=ALU.subtract))
            cx.op("act", lambda e: e.activation(out=eg[:], in_=eg[:], func=AF.Exp), reads=[RS], writes=[RS])
            rd(lambda e: e.tensor_reduce(out=sumg[:], in_=eg[:], axis=AX.X, op=ALU.add))
            rd(lambda e: e.reciprocal(out=ptop[:], in_=sumg[:]))
            rd(lambda e: e.tensor_tensor(out=selm[:], in0=le4,
                                         in1=ohg[:].unsqueeze(3).to_broadcast([128, NTT, 4, 8]), op=ALU.mult))
            rd(lambda e: e.tensor_reduce(out=sel[:], in_=selm[:].rearrange("p t g j -> p t j g"), axis=AX.X, op=ALU.add))
            rd(lambda e: e.tensor_reduce(out=m1_[:], in_=sel[:], axis=AX.X, op=ALU.max))
            rd(lambda e: e.tensor_tensor(out=oh1[:], in0=sel[:], in1=bc3(m1_[:], 8), op=ALU.is_equal))
            rd(lambda e: e.scalar_tensor_tensor(out=sel2[:], in0=oh1[:], scalar=-1e30, in1=sel[:], op0=ALU.mult, op1=ALU.add))
            rd(lambda e: e.tensor_reduce(out=m2_[:], in_=sel2[:], axis=AX.X, op=ALU.max))
            rd(lambda e: e.tensor_tensor(out=oh2[:], in0=sel2[:], in1=bc3(m2_[:], 8), op=ALU.is_equal))
            rd(lambda e: e.tensor_tensor(out=dm[:], in0=m1_[:], in1=m2_[:], op=ALU.subtract))
            cx.op("act", lambda e: e.activation(out=w1[:], in_=dm[:], func=AF.Sigmoid), reads=[RS], writes=[RS])
            rd(lambda e: e.tensor_tensor(out=w1[:], in0=w1[:], in1=ptop[:], op=ALU.mult))
            rd(lambda e: e.tensor_tensor(out=w2[:], in0=ptop[:], in1=w1[:], op=ALU.subtract))
            rd(lambda e: e.tensor_tensor(out=M1[:], in0=ohg[:].unsqueeze(3).to_broadcast([128, NTT, 4, 8]),
                                         in1=oh1[:].unsqueeze(2).to_broadcast([128, NTT, 4, 8]), op=ALU.mult))
            rd(lambda e: e.tensor_tensor(out=M2[:], in0=ohg[:].unsqueeze(3).to_broadcast([128, NTT, 4, 8]),
                                         in1=oh2[:].unsqueeze(2).to_broadcast([128, NTT, 4, 8]), op=ALU.mult))
            rd(lambda e: e.tensor_tensor(out=Mc[:], in0=M1[:].rearrange("p t g j -> p t (g j)"),
                                         in1=M2[:].rearrange("p t g j -> p t (g j)"), op=ALU.add))
            rd(lambda e: e.memset(Cex[:, 0, :], 0.0))
            for i in range(1, NTT):
                rd(lambda e, i=i: e.tensor_tensor(out=Cex[:, i, :], in0=Cex[:, i - 1, :], in1=Mc[:, i - 1, :], op=ALU.add))
            pp = [getps(), getps()]
            for i in range(NTT):
                pb_ = pp[i // 16]
                o_ = pb_[:, (i % 16) * 32:(i % 16 + 1) * 32]
                cx.op("pe", lambda e, o_=o_, i=i: e.matmul(o_, lhsT=tri[:], rhs=Mc[:, i, :], start=True, stop=False),
                      reads=[tri.b, RS], writes=[pb_.b])
                cx.op("pe", lambda e, o_=o_, i=i: e.matmul(o_, lhsT=ones_f[:], rhs=Cex[:, i, :], start=False, stop=True),
                      reads=[ones_f.b, RS], writes=[pb_.b])
            for hh in range(2):
                rd(lambda e, hh=hh: e.tensor_copy(out=pos[:, hh * 16:(hh + 1) * 16, :],
                                                  in_=pp[hh][:].rearrange("p (t x) -> p t x", t=16)), [pp[hh].b])
            rd(lambda e: e.tensor_single_scalar(out=bk[:], in_=pos[:], scalar=127.5, op=ALU.is_gt))
            for thr in range(2, NBLK):
                rd(lambda e, thr=thr: e.tensor_single_scalar(out=tq[:], in_=pos[:], scalar=128.0 * thr - 0.5, op=ALU.is_gt))
                rd(lambda e: e.tensor_tensor(out=bk[:], in0=bk[:], in1=tq[:], op=ALU.add))
            rd(lambda e: e.scalar_tensor_tensor(out=bk[:], in0=bk[:], scalar=float(1 - 128 * NEB_),
                                                in1=ecap[:].unsqueeze(1).to_broadcast([128, NTT, 32]),
                                                op0=ALU.mult, op1=ALU.add), [ecap.b])
            rd(lambda e: e.scalar_tensor_tensor(out=sf[:], in0=pos[:], scalar=float(NEB_), in1=bk[:],
                                                op0=ALU.mult, op1=ALU.add))
            rd(lambda e: e.tensor_single_scalar(out=ov[:], in_=pos[:], scalar=float(CAP) - 0.5, op=ALU.is_gt))
            for k, (Mk, wk) in enumerate(((M1, w1), (M2, w2))):
                Mk32 = Mk[:].rearrange("p t g j -> p t (g j)")
                rd(lambda e, Mk32=Mk32: e.tensor_tensor(out=tq[:], in0=Mk32, in1=sf[:], op=ALU.mult))
                rd(lambda e, k=k: e.tensor_reduce(out=sk[k][:], in_=tq[:], axis=AX.X, op=ALU.add))
                rd(lambda e, Mk32=Mk32: e.tensor_tensor(out=tq[:], in0=Mk32, in1=ov[:], op=ALU.mult))
                rd(lambda e: e.tensor_reduce(out=okk[:], in_=tq[:], axis=AX.X, op=ALU.add))
                rd(lambda e, k=k: e.tensor_scalar(out=dd[:], in0=sk[k][:], scalar1=trashp[:, 0:1], scalar2=None,
                                                  op0=ALU.subtract), [trashp.b])
                rd(lambda e: e.tensor_tensor(out=dd[:], in0=dd[:], in1=okk[:], op=ALU.mult))
                rd(lambda e, k=k: e.tensor_tensor(out=sk[k][:], in0=sk[k][:], in1=dd[:], op=ALU.subtract))
                rd(lambda e, k=k: e.tensor_copy(out=si[k][:], in_=sk[k][:]), (), [si[k].b])
                rd(lambda e, k=k: e.memset(ent[k][:], 0.0), (), [ent[k].b])
                rd(lambda e, k=k: e.tensor_copy(out=ent[k][:, :, 0], in_=tokid[:]), [tokid.b], [ent[k].b])
                rd(lambda e, k=k: e.tensor_scalar(out=ent[k][:, :, 1], in0=tokid[:], scalar1=float(k * ROWS), scalar2=None,
                                                  op0=ALU.add), [tokid.b], [ent[k].b])
                rd(lambda e, k=k, wk=wk: e.tensor_copy(out=ent[k][:, :, 2], in_=wk[:]), (), [ent[k].b])
            for i in range(NTT):
                for k in range(2):
                    cx.op("pool", lambda e, i=i, k=k: e.indirect_dma_start(
                        out=lst_d, out_offset=bass.IndirectOffsetOnAxis(ap=si[k][:, i:i + 1], axis=0),
                        in_=ent[k][:, i, :], in_offset=None),
                        reads=[si[k].b, ent[k].b, lstB], dma=True)
            cx.barrier()
            alR.close()
            alC.close()
        if stop_after in ("A", "B", "C"):
            pass
        else:
            alD = Alloc(nc)
            NEB = NEXP * NBLK
            lst_sb = alD.sb([128, NEB, 4], F32, "lst_sb")
            idx_i = alD.sb([128, NEB], I32, "idx_i")
            dst_i = alD.sb([128, NEB], I32, "dst_i")
            cx.op("sp", lambda e: e.dma_start(out=lst_sb[:], in_=lst_d[0:NEXP * CAP, :].rearrange("(s eb) w -> s eb w", s=128)),
                  reads=[lstB], full=[lst_sb.b], dma=True)
            cx.op("dve", lambda e: e.tensor_copy(out=idx_i[:], in_=lst_sb[:, :, 0]), reads=[lst_sb.b], full=[idx_i.b])
            cx.op("dve", lambda e: e.tensor_copy(out=dst_i[:], in_=lst_sb[:, :, 1]), reads=[lst_sb.b], full=[dst_i.b])
            NWB = 3
            Wg = [alD.sb([128, 8, 256], BF16, f"Wg{i}") for i in range(NWB)]
            Wu = [alD.sb([128, 8, 256], BF16, f"Wu{i}") for i in range(NWB)]
            Wd = [alD.sb([128, 2, 1024], BF16, f"Wd{i}") for i in range(NWB)]
            Gt = [alD.sb([128, D], BF16, f"Gt{i}") for i in range(3)]
            Xe = [alD.sb([128, 8, CAP], BF16, f"Xe{i}") for i in range(2)]
            sgl = [alD.sb([128, CAP], F32, f"sgl{i}") for i in range(2)]
            ae = [alD.sb([128, 2, CAP], BF16, f"ae{i}") for i in range(2)]
            Yt = [alD.sb([128, D], BF16, f"Yt{i}") for i in range(3)]

            Gt6 = Gt + [alD.sb([128, D], BF16, f"Gtx{i}") for i in range(3)]

            def load_w_dma(e_):
                p = e_ % NWB
                cx.op("sp", lambda e: e.dma_start(out=Wg[p][:].rearrange("p c n -> p (c n)"), in_=wbf_scr[e_, 0]),
                      full=[Wg[p].b], dma=True)
                cx.op("sp", lambda e: e.dma_start(out=Wu[p][:].rearrange("p c n -> p (c n)"), in_=wbf_scr[e_, 1]),
                      full=[Wu[p].b], dma=True)
                cx.op("sp", lambda e: e.dma_start(out=Wd[p][:].rearrange("p c n -> p (c n)"), in_=wbf_scr[e_, 2]),
                      full=[Wd[p].b], dma=True)

            def load_w_cast(e_):
                pass

            def gathers(e_):
                for blk in range(NBLK):
                    eb = e_ * NBLK + blk
                    G = Gt6[(e_ % 2) * 3 + blk]
                    cx.op("pool", lambda e, G=G, eb=eb: e.indirect_dma_start(
                        out=G[:], out_offset=None, in_=h2_scr,
                        in_offset=bass.IndirectOffsetOnAxis(ap=idx_i[:, eb:eb + 1], axis=0)),
                        reads=[idx_i.b, h2B], full=[G.b], dma=True)

            gi = [0]
            load_w_dma(0)
            load_w_dma(1)
            gathers(0)
            KNE = int(os.environ.get("KNE", str(NEXP)))
            for e_ in range(KNE):
                p = e_ % NWB
                if e_ + 2 < NEXP:
                    load_w_dma(e_ + 2)
                if e_ + 1 < NEXP:
                    gathers(e_ + 1)
                X = Xe[e_ % 2]
                for blk in range(NBLK):
                    G = Gt6[(e_ % 2) * 3 + blk]
                    pbk = psb[gi[0] % 2]
                    gi[0] += 1
                    for c in range(8):
                        cx.op("pe", lambda e, pbk=pbk, G=G, c=c: e.transpose(
                            out=pbk[:, c * 128:(c + 1) * 128], in_=G[:, c * 128:(c + 1) * 128], identity=ident_bf[:]),
                            reads=[G.b, ident_bf.b], writes=[pbk.b])
                    if blk % 2 == 0:
                        cx.op("act", lambda e, pbk=pbk, X=X, blk=blk: e.copy(
                            out=X[:, :, blk * 128:(blk + 1) * 128], in_=pbk[:].rearrange("p (c t) -> p c t", c=8)),
                            reads=[pbk.b], writes=[X.b])
                    else:
                        cx.op("dve", lambda e, pbk=pbk, X=X, blk=blk: e.tensor_copy(
                            out=X[:, :, blk * 128:(blk + 1) * 128], in_=pbk[:].rearrange("p (c t) -> p c t", c=8)),
                            reads=[pbk.b], writes=[X.b])
                a_ = ae[e_ % 2]
                for ft in range(2):
                    pg = getps(); pu = getps()
                    for c in range(8):
                        cx.op("pe", lambda e, pg=pg, c=c, ft=ft, X=X, p=p: e.matmul(
                            pg[:, 0:CAP], lhsT=Wg[p][:, c, ft * 128:(ft + 1) * 128], rhs=X[:, c, :],
                            start=(c == 0), stop=(c == 7)), reads=[Wg[p].b, X.b], writes=[pg.b])
                    for c in range(8):
                        cx.op("pe", lambda e, pu=pu, c=c, ft=ft, X=X, p=p: e.matmul(
                            pu[:, 0:CAP], lhsT=Wu[p][:, c, ft * 128:(ft + 1) * 128], rhs=X[:, c, :],
                            start=(c == 0), stop=(c == 7)), reads=[Wu[p].b, X.b], writes=[pu.b])
                    s = sgl[ft]
                    cx.op("act", lambda e, pg=pg, s=s: e.activation(out=s[:], in_=pg[:, 0:CAP], func=AF.Silu),
                          reads=[pg.b], full=[s.b])
                    cx.op("dve", lambda e, pu=pu, s=s, a_=a_, ft=ft: e.tensor_tensor(
                        out=a_[:, ft, :], in0=pu[:, 0:CAP], in1=s[:], op=ALU.mult),
                        reads=[pu.b, s.b], writes=[a_.b])
                for blk in range(NBLK):
                    eb = e_ * NBLK + blk
                    Y = Yt[eb % 3]
                    for half in range(2):
                        py = getps()
                        for ft in range(2):
                            cx.op("pe", lambda e, py=py, ft=ft, blk=blk, half=half, a_=a_, p=p: e.matmul(
                                py[:], lhsT=a_[:, ft, blk * 128:(blk + 1) * 128],
                                rhs=Wd[p][:, ft, half * 512:(half + 1) * 512], start=(ft == 0), stop=(ft == 1)),
                                reads=[a_.b, Wd[p].b], writes=[py.b])
                        if half == 0:
                            cx.op("dve", lambda e, py=py, Y=Y, eb=eb: e.tensor_scalar(
                                out=Y[:, 0:512], in0=py[:], scalar1=lst_sb[:, eb, 2:3], scalar2=None, op0=ALU.mult),
                                reads=[py.b, lst_sb.b], writes=[Y.b])
                        else:
                            cx.op("act", lambda e, py=py, Y=Y, eb=eb: e.activation(
                                out=Y[:, 512:1024], in_=py[:], func=AF.Copy, scale=lst_sb[:, eb, 2:3]),
                                reads=[py.b, lst_sb.b], writes=[Y.b])
                    cx.op("pool", lambda e, Y=Y, eb=eb: e.indirect_dma_start(
                        out=moe_scr, out_offset=bass.IndirectOffsetOnAxis(ap=dst_i[:, eb:eb + 1], axis=0),
                        in_=Y[:], in_offset=None), reads=[Y.b, dst_i.b, moeB], dma=True)
                if e_ + 1 < NEXP:
                    load_w_cast(e_ + 1)
            cx.barrier()
            alD.close()

            alE = Alloc(nc)
            gfin = alE.sb([128, D], F32, "gfin")
            cx.op("sp", lambda e: e.dma_start(out=gfin[:], in_=gfin_d.partition_broadcast(128)), full=[gfin.b], dma=True)
            NE_ = 4
            xa = [alE.sb([128, D], F32, f"xa{i}") for i in range(NE_)]
            m0 = [alE.sb([128, D], BF16, f"m0{i}") for i in range(NE_)]
            m1 = [alE.sb([128, D], BF16, f"m1{i}") for i in range(NE_)]
            ot = [alE.sb([128, D], F32, f"ot{i}") for i in range(NE_)]
            junk3 = alE.sb([128, D], BF16, "junk3")
            sse = [alE.sb([128, 1], F32, f"sse{i}") for i in range(NE_)]
            rte = [alE.sb([128, 1], F32, f"rte{i}") for i in range(NE_)]
            rse = [alE.sb([128, 1], F32, f"rse{i}") for i in range(NE_)]
            outB = Buf("out")

            def e_load(ti):
                p = ti % NE_
                rows = slice(ti * 128, (ti + 1) * 128)
                cx.op("sp", lambda e, p=p, rows=rows: e.dma_start(out=xa[p][:], in_=x2_scr[rows, :]),
                      full=[xa[p].b], dma=True)
                cx.op("sp", lambda e, p=p, rows=rows: e.dma_start(out=m0[p][:], in_=moe_scr[rows, :]),
                      full=[m0[p].b], dma=True)
                cx.op("sp", lambda e, p=p, ti=ti: e.dma_start(
                    out=m1[p][:], in_=moe_scr[ROWS + ti * 128:ROWS + (ti + 1) * 128, :]),
                    full=[m1[p].b], dma=True)

            for ti in range(min(NE_ - 1, NT)):
                e_load(ti)
            for ti in range(NT):
                p = ti % NE_
                rows = slice(ti * 128, (ti + 1) * 128)
                if ti + NE_ - 1 < NT:
                    e_load(ti + NE_ - 1)
                cx.op("pool", lambda e, p=p: e.tensor_tensor(out=xa[p][:], in0=xa[p][:], in1=m0[p][:], op=ALU.add),
                      reads=[m0[p].b], writes=[xa[p].b])
                cx.op("dve", lambda e, p=p: e.tensor_tensor(out=xa[p][:], in0=xa[p][:], in1=m1[p][:], op=ALU.add),
                      reads=[m1[p].b], writes=[xa[p].b])
                cx.op("act", lambda e, p=p: e.activation(out=junk3[:], in_=xa[p][:], func=AF.Square, accum_out=sse[p][:]),
                      reads=[xa[p].b], full=[junk3.b, sse[p].b])
                cx.op("act", lambda e, p=p: e.activation(out=rte[p][:], in_=sse[p][:], func=AF.Sqrt, scale=1.0 / D, bias=EPS),
                      reads=[sse[p].b], full=[rte[p].b])
                cx.op("dve", lambda e, p=p: e.reciprocal(out=rse[p][:], in_=rte[p][:]), reads=[rte[p].b], full=[rse[p].b])
                cx.op("dve", lambda e, p=p: e.scalar_tensor_tensor(out=ot[p][:], in0=xa[p][:], scalar=rse[p][:, 0:1],
                                                                   in1=gfin[:], op0=ALU.mult, op1=ALU.mult),
                      reads=[xa[p].b, rse[p].b, gfin.b], full=[ot[p].b])
                cx.op("sp", lambda e, p=p, rows=rows: e.dma_start(out=out_d[rows, :], in_=ot[p][:]),
                      reads=[ot[p].b], writes=[outB], dma=True)
            cx.barrier()
            alE.close()
        cx.barrier()
        cx.emit(block)
        print("waits", cx.nwait, "instrs", {e: cx.cnt[e] for e in cx.ENG}, "signals", {e: len(cx.waited[e]) for e in cx.ENG})
    return nc


def host_consts():
    c = {}
    c["ident_bf"] = np.eye(128, dtype=np.float32).astype(ml_dtypes.bfloat16)
    c["ident_f"] = np.eye(128, dtype=np.float32)
    psel = np.zeros((128, 8, 240), np.float32)
    for a in range(8):
        for i in range(16):
            psel[a * 16 + i, a, 7 * 16 + i] = 1.0
    c["psel"] = psel.astype(ml_dtypes.bfloat16)
    kk = np.arange(128) // 16
    c["cmask"] = (kk[None, :] >= kk[:, None]).astype(np.float32)
    c["tri"] = (np.arange(128)[:, None] < np.arange(128)[None, :]).astype(np.float32)
    c["ecap"] = np.ascontiguousarray(np.broadcast_to((np.arange(32) * NBLK).astype(np.float32)[None, :], (128, 32)))
    c["tokid"] = (np.arange(NT)[None, :] * 128 + np.arange(128)[:, None]).astype(np.float32)
    li = np.zeros((NEXP * CAP + 128, 4), np.float32)
    li[:, 0] = SEQ + ((np.arange(NEXP * CAP + 128) // (NEXP * NBLK)) % 128)
    li[:, 1] = li[:, 0]
    c["trashp"] = (NEXP * CAP + np.arange(128)).astype(np.float32).reshape(128, 1)
    c["lst_init"] = li
    return c


def relayout_pc(w):
    E, K, N = w.shape
    return np.ascontiguousarray(w.reshape(E, K // 128, 128, N).transpose(0, 2, 1, 3))


def pair_layout(a):
    rest = a.shape[2:]
    a = a.reshape((16, 2, 64) + rest)
    a = np.moveaxis(a, 0, 2)
    return np.ascontiguousarray(a.reshape((128, 16) + rest))


def make_inmap(inputs, b, consts=None):
    f = lambda a: np.ascontiguousarray(a, dtype=np.float32)
    m = {"x": f(inputs["x"][b]),
         "g_mix": f(inputs["g_mix"]),
         "w_in": f(inputs["w_in"][0])}
    m["lamre_l"] = pair_layout(f(inputs["ssm_lambda_re"][0]))
    m["lamim_l"] = pair_layout(f(inputs["ssm_lambda_im"][0]))
    m["logdt_l"] = pair_layout(np.broadcast_to(f(inputs["ssm_log_dt"][0])[:, None], (32, 64)))
    m["bre_l"] = pair_layout(f(inputs["ssm_b_re"][0]))
    m["bim_l"] = pair_layout(f(inputs["ssm_b_im"][0]))
    m["cre_l"] = pair_layout(f(inputs["ssm_c_re"][0]).transpose(0, 2, 1))
    m["cim_l"] = pair_layout(f(inputs["ssm_c_im"][0]).transpose(0, 2, 1))
    m["d_l"] = np.ascontiguousarray(np.tile(f(inputs["ssm_d"][0]).reshape(32, 16).T, (8, 1)))
    m["mem"] = f(inputs["mem"][b])
    for k_, n_ in (("g_mem", "g_mem"), ("g_ffn", "g_ffn")):
        m[n_] = f(inputs[k_])
    m["g_final"] = f(inputs["g_final"]).reshape(1, D)
    m["w_mem_kv"] = f(inputs["w_mem_kv"][0]); m["w_mem_out"] = f(inputs["w_mem_out"][0])
    m["w_conv_out"] = f(inputs["w_conv_out"][0]); m["w_ssm_glu"] = f(inputs["w_ssm_glu"][0])
    m["w_out"] = f(inputs["w_out"][0])
    m["w_router"] = np.ascontiguousarray(np.concatenate([f(inputs["w_router_group"][0]),
                                                         f(inputs["w_router_expert"][0])], axis=1))
    m["b_router"] = np.ascontiguousarray(np.concatenate([f(inputs["b_router_group"][0]),
                                                         f(inputs["b_router_expert"][0])])[None, :])
    m["cdw_l"] = np.ascontiguousarray(f(inputs["conv_dw"][0]).T.reshape(4, 128, 31).transpose(1, 0, 2))
    m["cb_l"] = np.ascontiguousarray(f(inputs["conv_dw_bias"][0]).reshape(4, 128).T)
    m["lng_l"] = np.ascontiguousarray(f(inputs["conv_ln_g"][0]).reshape(4, 128).T)
    m["lnb_l"] = np.ascontiguousarray(f(inputs["conv_ln_b"][0]).reshape(4, 128).T)
    if consts is not None and "w_exp_gate" in consts:
        for k_ in ("w_exp_gate", "w_exp_up", "w_exp_down"):
            m[k_] = consts[k_]
    else:
        m["w_exp_gate"] = relayout_pc(f(inputs["w_exp_gate"][0]))
        m["w_exp_up"] = relayout_pc(f(inputs["w_exp_up"][0]))
        m["w_exp_down"] = relayout_pc(f(inputs["w_exp_down"][0]))
    m.update(consts if consts is not None else host_consts())
    return m


def kernel(**inputs):
    nc = build()
    consts = host_consts()
    f32 = lambda a: np.ascontiguousarray(a, dtype=np.float32)
    for k_ in ("w_exp_gate", "w_exp_up", "w_exp_down"):
        consts[k_] = relayout_pc(f32(inputs[k_][0]))
    in_maps = [make_inmap(inputs, b, consts) for b in range(NCORES)]
    res = run_bass_kernel_spmd(nc, in_maps, core_ids=list(range(NCORES)))
    return np.stack([r["out"] for r in res.results], axis=0)
```

```python
import os
import numpy as np
import ml_dtypes
from contextlib import ExitStack
import concourse.bass as bass
import concourse.mybir as mybir
from concourse.bass_utils import run_bass_kernel_spmd

F32 = mybir.dt.float32
BF16 = mybir.dt.bfloat16
I32 = mybir.dt.int32
U32 = mybir.dt.uint32
AF = mybir.ActivationFunctionType
ALU = mybir.AluOpType
AX = mybir.AxisListType
GELU = AF.Gelu_apprx_tanh

D = 1024
SEQ = 4096
NCORES = 8
T = 512
NB = SEQ // T
NT = SEQ // 128
EPS = 1e-6
NEXP = 32
CAP = 384
NBLK = CAP // 128
ROWS = SEQ + 128


class Buf:
    __slots__ = ("name", "w", "r")

    def __init__(self, name):
        self.name = name
        self.w = {}
        self.r = {}


class Ctx:
    ENG = ("pe", "dve", "act", "pool", "sp")
    KROT = 4
    NDMA = 12

    def __init__(self, nc, es):
        self.nc = nc
        self.q = {e: [] for e in self.ENG}
        self.cnt = {e: 0 for e in self.ENG}
        self.seen = {e: {} for e in self.ENG}
        self.esem = {e: [es.enter_context(nc.semaphore(f"s_{e}{i}")) for i in range(self.KROT)]
                     for e in self.ENG}
        self.dsem = {e: [es.enter_context(nc.semaphore(f"d_{e}{i}")) for i in range(self.NDMA)]
                     for e in ("sp", "act", "pool")}
        self.dcnt = {e: [0] * self.NDMA for e in self.dsem}
        self.dnext = {e: 0 for e in self.dsem}
        self.nwait = 0
        self.waited = {e: set() for e in self.ENG}

    def _wait(self, eng, tok):
        key, val = tok
        if key[0] == 'e' and key[1] == eng and eng == "pe":
            return
        if self.seen[eng].get(key, -1) >= val:
            return
        self.seen[eng][key] = val
        if key[0] == 'e':
            self.waited[key[1]].add(val)
        self.q[eng].append(("w", key, val))
        self.nwait += 1

    def op(self, eng, fn, reads=(), writes=(), full=(), dma=False):
        toks = []
        for b in reads:
            toks.extend(b.w.items())
        for b in tuple(writes) + tuple(full):
            toks.extend(b.w.items())
            toks.extend(b.r.items())
        for t in toks:
            self._wait(eng, t)
        if dma:
            i = self.dnext[eng]
            self.dnext[eng] = (i + 1) % self.NDMA
            key = ('d', eng, i)
            if self.dcnt[eng][i] > 0:
                self._wait(eng, (key, self.dcnt[eng][i]))
            self.dcnt[eng][i] += 16
            val = self.dcnt[eng][i]
            self.q[eng].append(("d", fn, self.dsem[eng][i]))
        else:
            key = ('e', eng)
            val = self.cnt[eng]
            self.cnt[eng] += 1
            self.q[eng].append(("i", fn, val))
        for b in reads:
            b.r[key] = val
        for b in full:
            b.w = {key: val}
            b.r = {}
        for b in writes:
            b.w[key] = val
        return (key, val)

    def barrier(self, skip_pool_dma=False):
        toks = []
        for e in self.ENG:
            if skip_pool_dma and e == "pool":
                continue
            if self.cnt[e] > 0:
                toks.append((('e', e), self.cnt[e] - 1))
        for e in self.dsem:
            if skip_pool_dma and e == "pool":
                continue
            for i in range(self.NDMA):
                if self.dcnt[e][i] > 0:
                    toks.append((('d', e, i), self.dcnt[e][i]))
        for e in self.ENG:
            for t in toks:
                self._wait(e, t)

    def emit(self, block):
        nc = self.nc

        rank = {e: {v: i for i, v in enumerate(sorted(self.waited[e]))} for e in self.ENG}
        K_ = self.KROT

        def run(engname, engine):
            for item in self.q[engname]:
                if item[0] == "w":
                    key, val = item[1], item[2]
                    if key[0] == 'e':
                        r = rank[key[1]][val]
                        engine.wait_ge(self.esem[key[1]][r % K_], r // K_ + 1)
                    else:
                        engine.wait_ge(self.dsem[key[1]][key[2]], val)
                elif item[0] == "d":
                    item[1](engine).then_inc(item[2], 16)
                else:
                    ins = item[1](engine)
                    r = rank[engname].get(item[2])
                    if r is not None:
                        ins.then_inc(self.esem[engname][r % K_], 1)

        @block.tensor
        def _(e):
            run("pe", e)

        @block.vector
        def _(e):
            run("dve", e)

        @block.scalar
        def _(e):
            run("act", e)

        @block.gpsimd
        def _(e):
            run("pool", e)

        @block.sync
        def _(e):
            run("sp", e)


class TT:
    def __init__(self, t, name):
        self.t = t
        self.b = Buf(name)

    def __getitem__(self, k):
        return self.t[k]


class Alloc:
    cnt = [0]

    def __init__(self, nc, es=None):
        self.nc = nc
        self.es = es if es is not None else ExitStack()

    @property
    def n(self):
        return Alloc.cnt[0]

    @n.setter
    def n(self, v):
        Alloc.cnt[0] = v

    def close(self):
        self.es.close()

    def sb(self, shape, dt, name=None):
        self.n += 1
        name = name or f"sb{self.n}"
        t = self.es.enter_context(self.nc.sbuf_tensor(f"{name}_{self.n}", list(shape), dt))
        return TT(t, name)

    def ps(self, shape, dt, name=None):
        self.n += 1
        name = name or f"ps{self.n}"
        t = self.es.enter_context(self.nc.psum_tensor(f"{name}_{self.n}", list(shape), dt))
        return TT(t, name)


def build(stop_after="E", dbg=False):
    nc = bass.Bass("TRN2", target_bir_lowering=False)
    dram = {}

    def din(name, shape, dt=F32):
        dram[name] = nc.dram_tensor(name, list(shape), dt, kind="ExternalInput").ap()
        return dram[name]

    def dscr(name, shape, dt, kind="Internal"):
        dram[name] = nc.dram_tensor(name, list(shape), dt, kind=kind).ap()
        return dram[name]

    x_d = din("x", [SEQ, D])
    gmix_d = din("g_mix", [1, D])
    w_in_d = din("w_in", [128, 8, 5120])
    ident_bf_d = din("ident_bf", [128, 128], BF16)
    ident_f_d = din("ident_f", [128, 128], F32)
    lamre_d = din("lamre_l", [128, 16])
    lamim_d = din("lamim_l", [128, 16])
    logdt_d = din("logdt_l", [128, 16])
    bre_d = din("bre_l", [128, 16, 16])
    bim_d = din("bim_l", [128, 16, 16])
    cre_d = din("cre_l", [128, 16, 16])
    cim_d = din("cim_l", [128, 16, 16])
    dl_d = din("d_l", [128, 32])
    psel_d = din("psel", [128, 8, 240], BF16)
    cmask_d = din("cmask", [128, 128])
    mem_d = din("mem", [256, D])
    gmem_d = din("g_mem", [1, D])
    gffn_d = din("g_ffn", [1, D])
    gfin_d = din("g_final", [1, D])
    wkv_d = din("w_mem_kv", [128, 8, 1024])
    wmo_d = din("w_mem_out", [128, 4, D])
    wco_d = din("w_conv_out", [128, 4, D])
    wgl_d = din("w_ssm_glu", [128, 4, 2048])
    wo_d = din("w_out", [128, 8, D])
    wr_d = din("w_router", [D, 36])
    rbias_d = din("b_router", [1, 36])
    cdw_d = din("cdw_l", [128, 4, 31])
    cb_d = din("cb_l", [128, 4])
    lng_d = din("lng_l", [128, 4])
    lnb_d = din("lnb_l", [128, 4])
    tri_d = din("tri", [128, 128])
    ecap_d = din("ecap", [128, 32])
    tokid_d = din("tokid", [128, NT])
    lst_init_d = din("lst_init", [NEXP * CAP + 128, 4])
    trashp_d = din("trashp", [128, 1])
    weg_d = din("w_exp_gate", [NEXP, 128, 8, 256])
    weu_d = din("w_exp_up", [NEXP, 128, 8, 256])
    wed_d = din("w_exp_down", [NEXP, 128, 2, D])
    dk = "ExternalOutput" if dbg else "Internal"
    wbf_scr = dscr("wbf_scr", [NEXP, 3, 128, 2048], BF16)
    lst_d = dscr("lst", [NEXP * CAP + 128, 4], F32, kind=dk)
    h2_scr = dscr("h2_scr", [ROWS, D], BF16, kind=dk)
    moe_scr = dscr("moe_scr", [2 * ROWS, D], BF16, kind=dk)
    x2_scr = dscr("x2_scr", [SEQ, D], F32, kind=dk)
    ys_scr = dscr("ys_scr", [4, 128, SEQ], BF16, kind="ExternalOutput" if dbg else "Internal")
    out_d = dscr("out", [SEQ, D], F32, kind="ExternalOutput")
    hT_scr = dscr("hT_scr", [8, 128, SEQ], BF16, kind="ExternalOutput" if dbg else "Internal")
    u_dbg = dscr("u_dbg", [4, 128, SEQ], BF16, kind="ExternalOutput") if dbg else None

    with ExitStack() as es:
        cx = Ctx(nc, es)
        al = Alloc(nc, es)
        block = es.enter_context(nc.Block())

        ident_bf = al.sb([128, 128], BF16, "ident_bf")
        cx.op("sp", lambda e: e.dma_start(out=ident_bf[:], in_=ident_bf_d), full=[ident_bf.b], dma=True)
        ident_f = al.sb([128, 128], F32, "ident_f")
        cx.op("sp", lambda e: e.dma_start(out=ident_f[:], in_=ident_f_d), full=[ident_f.b], dma=True)

        psum = [al.ps([128, 512], F32, f"bank{i}") for i in range(6)]
        psb = [al.ps([128, 1024], BF16, f"bankb{i}") for i in range(2)]
        pctr = [0]

        def getps():
            p = psum[pctr[0] % len(psum)]
            pctr[0] += 1
            return p

        alAB = Alloc(nc)
        u_all = alAB.sb([128, 4, SEQ], BF16, "u_all")
        M_all = alAB.sb([128, 32, 128], BF16, "M_all")
        W2r = alAB.sb([128, 16, 2, 128], BF16, "W2r"); W2i = alAB.sb([128, 16, 2, 128], BF16, "W2i")
        C1r = alAB.sb([128, 16, 128], BF16, "C1r"); nC1i = alAB.sb([128, 16, 128], BF16, "nC1i")
        KAr = alAB.sb([128, 9, 16], F32, "KAr"); KAi = alAB.sb([128, 9, 16], F32, "KAi")
        KnAi = alAB.sb([128, 9, 16], F32, "KnAi")
        psel = alAB.sb([128, 8, 240], BF16, "psel")
        zt = alAB.sb([128, 1024], F32, "zt")
        NPB = 3
        pst = [alAB.sb([128, 2048], F32, f"pst{i}") for i in range(NPB)]
        pbf = [alAB.sb([128, 2048], BF16, f"pbf{i}") for i in range(NPB)]
        wbfB = Buf("wbf")
        pc_next = [0]

        def precast(n, mode):
            for _ in range(n):
                ci = pc_next[0]
                if ci >= NEXP * 3:
                    return
                pc_next[0] += 1
                e_, m_ = ci // 3, ci % 3
                srcw = (weg_d, weu_d, wed_d)[m_][e_].rearrange("p c n -> p (c n)")
                s_ = pst[ci % NPB]; b_ = pbf[ci % NPB]
                dst = wbf_scr[e_, m_]
                if mode == "pool":
                    cx.op("pool", lambda e, s_=s_, srcw=srcw: e.dma_start(out=s_[:], in_=srcw), full=[s_.b], dma=True)
                    cx.op("pool", lambda e, s_=s_, b_=b_: e.tensor_copy(out=b_[:], in_=s_[:]), reads=[s_.b], full=[b_.b])
                    cx.op("pool", lambda e, b_=b_, dst=dst: e.dma_start(out=dst, in_=b_[:]), reads=[b_.b], dma=True)
                else:
                    cx.op("sp", lambda e, s_=s_, srcw=srcw: e.dma_start(out=s_[:], in_=srcw), full=[s_.b], dma=True)
                    cx.op("act", lambda e, s_=s_, b_=b_: e.copy(out=b_[:], in_=s_[:]), reads=[s_.b], full=[b_.b])
                    cx.op("act", lambda e, b_=b_, dst=dst: e.dma_start(out=dst, in_=b_[:]), reads=[b_.b], dma=True)
        cx.op("sp", lambda e: e.dma_start(out=psel[:], in_=psel_d), full=[psel.b], dma=True)
        al_outer = al
        al = Alloc(nc)
        gmix = al.sb([128, D], F32, "gmix")
        cx.op("sp", lambda e: e.dma_start(out=gmix[:], in_=gmix_d.partition_broadcast(128)),
              full=[gmix.b], dma=True)

        stg = [al.sb([128, 8, 256], F32, f"stg{i}") for i in range(2)]
        sctr = [0]

        def load_cast(dst, dst_col0, src_d, c0, c1, kch):
            for c in range(kch):
                for n0 in range(c0, c1, 2048):
                    w = min(2048, c1 - n0)
                    s = stg[sctr[0] % 2]
                    sctr[0] += 1
                    sf_ = s[:].rearrange("p c n -> p (c n)")
                    cx.op("sp", lambda e, sf_=sf_, c=c, n0=n0, w=w: e.dma_start(out=sf_[:, 0:w], in_=src_d[:, c, n0:n0 + w]),
                          full=[s.b], dma=True)
                    o = dst_col0 + (n0 - c0)
                    cx.op("pool", lambda e, sf_=sf_, c=c, o=o, w=w: e.tensor_copy(out=dst[:, c, o:o + w], in_=sf_[:, 0:w]),
                          reads=[s.b], writes=[dst.b])

        w_ssm_in = al.sb([128, 8, 512], BF16, "w_ssm_in")
        load_cast(w_ssm_in, 0, w_in_d, 1024, 1536, 8)
        NA_ = 4
        xt = [al.sb([128, D], F32, f"xt{i}") for i in range(NA_)]
        junk = al.sb([128, D], BF16, "junk")
        ss = [al.sb([128, 1], F32, f"ss{i}") for i in range(NA_)]
        rt = [al.sb([128, 1], F32, f"rt{i}") for i in range(NA_)]
        rstd = [al.sb([128, 1], F32, f"rstd{i}") for i in range(NA_)]
        hbf = [al.sb([128, D], BF16, f"hbf{i}") for i in range(NA_)]
        hTb = [al.sb([128, 8, T], BF16, f"hTb{i}") for i in range(2)]
        cx.op("pool", lambda e: e.memset(zt[:], 0.0), full=[zt.b])
        lstB = Buf("lst"); h2B = Buf("h2scr"); moeB = Buf("moescr"); x2B = Buf("x2scr")
        cx.op("pool", lambda e: e.dma_start(out=lst_d, in_=lst_init_d), full=[lstB], dma=True)
        cx.op("pool", lambda e: e.dma_start(out=h2_scr[SEQ:ROWS, :], in_=zt[:, 0:512].bitcast(BF16)),
              reads=[zt.b], writes=[h2B], dma=True)
        moe_flat = moe_scr.rearrange("(n p) d -> n p d", p=128)
        for n in range(0, 2 * ROWS // 128):
            tok = cx.op("pool", lambda e, n=n: e.dma_start(out=moe_flat[n], in_=zt[:, 0:512].bitcast(BF16)),
                        reads=[zt.b], dma=True)
            moeB.w[tok[0]] = tok[1]

        def a_front(i):
            p = i % NA_
            cx.op("sp", lambda e, p=p, i=i: e.dma_start(out=xt[p][:], in_=x_d[i * 128:(i + 1) * 128, :]),
                  full=[xt[p].b], dma=True)
            cx.op("act", lambda e, p=p: e.activation(out=junk[:], in_=xt[p][:], func=AF.Square,
                                                     accum_out=ss[p][:]),
                  reads=[xt[p].b], writes=[junk.b], full=[ss[p].b])
            cx.op("act", lambda e, p=p: e.activation(out=rt[p][:], in_=ss[p][:], func=AF.Sqrt,
                                                     scale=1.0 / D, bias=EPS),
                  reads=[ss[p].b], full=[rt[p].b])
            cx.op("dve", lambda e, p=p: e.reciprocal(out=rstd[p][:], in_=rt[p][:]),
                  reads=[rt[p].b], full=[rstd[p].b])
            cx.op("dve", lambda e, p=p: e.scalar_tensor_tensor(out=hbf[p][:], in0=xt[p][:],
                                                               scalar=rstd[p][:, 0:1], in1=gmix[:],
                                                               op0=ALU.mult, op1=ALU.mult),
                  reads=[xt[p].b, rstd[p].b, gmix.b], full=[hbf[p].b])

        def a_back(i):
            p = i % NA_
            blk = i // 4
            hb = hTb[blk % 2]
            pb = psb[i % 2]
            for c in range(8):
                cx.op("pe", lambda e, pb=pb, p=p, c=c: e.transpose(out=pb[:, c * 128:(c + 1) * 128],
                                                                   in_=hbf[p][:, c * 128:(c + 1) * 128],
                                                                   identity=ident_bf[:]),
                      reads=[hbf[p].b, ident_bf.b], writes=[pb.b])
            tt = i % 4
            cx.op("act", lambda e, pb=pb, hb=hb, tt=tt: e.copy(
                out=hb[:, :, tt * 128:(tt + 1) * 128],
                in_=pb[:].rearrange("p (c t) -> p c t", c=8)),
                reads=[pb.b], writes=[hb.b])
            if tt == 3:
                for f in range(4):
                    ps = getps()
                    for c in range(8):
                        cx.op("pe", lambda e, ps=ps, hb=hb, f=f, c=c: e.matmul(
                            ps[:], lhsT=w_ssm_in[:, c, f * 128:(f + 1) * 128], rhs=hb[:, c, :],
                            start=(c == 0), stop=(c == 7)),
                            reads=[w_ssm_in.b, hb.b], writes=[ps.b])
                    cx.op("dve", lambda e, ps=ps, f=f, blk=blk: e.tensor_copy(
                        out=u_all[:, f, blk * T:(blk + 1) * T], in_=ps[:]),
                        reads=[ps.b], writes=[u_all.b])
                cx.op("act", lambda e, hb=hb, blk=blk: e.dma_start(
                    out=hT_scr[:, :, blk * T:(blk + 1) * T].rearrange("c p t -> p c t"), in_=hb[:]),
                    reads=[hb.b], dma=True)

        a_front(0); a_front(1)
        for i in range(NT):
            if i + 2 < NT:
                a_front(i + 2)
            a_back(i)
            if i % 3 == 2:
                precast(1, "pool")

        if dbg:
            cx.op("sp", lambda e: e.dma_start(out=u_dbg.rearrange("f p t -> p f t"), in_=u_all[:]),
                  reads=[u_all.b], dma=True)


        cx.barrier(skip_pool_dma=True)
        al.close()
        precast(8, "pool")
        al = Alloc(nc)
        TWO_PI = 2.0 * np.pi
        cmask = al.sb([128, 128], F32, "cmask")
        cx.op("sp", lambda e: e.dma_start(out=cmask[:], in_=cmask_d), full=[cmask.b], dma=True)
        dl = al.sb([128, 32], F32, "dl")
        cx.op("sp", lambda e: e.dma_start(out=dl[:], in_=dl_d), full=[dl.b], dma=True)
        SU = Buf("ssm_setup")

        def sload(shape, src, name):
            t = al.sb(shape, F32, name)
            cx.op("sp", lambda e: e.dma_start(out=t[:], in_=src), full=[t.b], dma=True)
            return t

        lamre = sload([128, 16], lamre_d, "lamre")
        lamim = sload([128, 16], lamim_d, "lamim")
        logdt = sload([128, 16], logdt_d, "logdt")
        Bre = sload([128, 16, 16], bre_d, "Bre")
        Bim = sload([128, 16, 16], bim_d, "Bim")
        Cre = sload([128, 16, 16], cre_d, "Cre")
        Cim = sload([128, 16, 16], cim_d, "Cim")
        ins_b = [lamre.b, lamim.b, logdt.b, Bre.b, Bim.b, Cre.b, Cim.b]

        def S(shape, name):
            return al.sb(shape, F32, name)

        def dv(fn):
            cx.op("dve", fn, reads=ins_b, writes=[SU])

        def ac(fn):
            cx.op("act", fn, reads=ins_b, writes=[SU])

        def tt_(out, a, b, op):
            dv(lambda e: e.tensor_tensor(out=out, in0=a, in1=b, op=op))

        sh16 = [128, 16]
        dt_ = S(sh16, "dt"); lrd = S(sh16, "lrd"); th = S(sh16, "th")
        ac(lambda e: e.activation(out=dt_[:], in_=logdt[:], func=AF.Exp))
        tt_(lrd[:], lamre[:], dt_[:], ALU.mult)
        tt_(th[:], lamim[:], dt_[:], ALU.mult)
        mag = S(sh16, "mag"); imag2 = S(sh16, "imag2")
        ac(lambda e: e.activation(out=mag[:], in_=lrd[:], func=AF.Exp))
        ac(lambda e: e.activation(out=imag2[:], in_=lrd[:], func=AF.Exp, scale=-2.0))
        kq_i = al.sb(sh16, I32, "kq_i"); kq = S(sh16, "kq"); red = S(sh16, "red"); msk = S(sh16, "msk")
        sinv = S(sh16, "sinv"); cosv = S(sh16, "cosv"); tmpa = S(sh16, "tmpa")

        def sin_of(outt, shift):
            dv(lambda e: e.tensor_scalar(out=tmpa[:], in0=th[:], scalar1=float(shift), scalar2=None,
                                         op0=ALU.add))
            dv(lambda e: e.tensor_scalar(out=kq[:], in0=tmpa[:], scalar1=float(1.0 / TWO_PI),
                                         scalar2=None, op0=ALU.mult))
            dv(lambda e: e.tensor_copy(out=kq_i[:], in_=kq[:]))
            dv(lambda e: e.tensor_copy(out=kq[:], in_=kq_i[:]))
            dv(lambda e: e.scalar_tensor_tensor(out=red[:], in0=kq[:], scalar=float(-TWO_PI),
                                                in1=tmpa[:], op0=ALU.mult, op1=ALU.add))
            dv(lambda e: e.tensor_single_scalar(out=msk[:], in_=red[:], scalar=float(np.pi), op=ALU.is_gt))
            dv(lambda e: e.scalar_tensor_tensor(out=red[:], in0=msk[:], scalar=float(-TWO_PI),
                                                in1=red[:], op0=ALU.mult, op1=ALU.add))
            dv(lambda e: e.tensor_single_scalar(out=msk[:], in_=red[:], scalar=float(-np.pi), op=ALU.is_lt))
            dv(lambda e: e.scalar_tensor_tensor(out=red[:], in0=msk[:], scalar=float(TWO_PI),
                                                in1=red[:], op0=ALU.mult, op1=ALU.add))
            ac(lambda e: e.activation(out=outt[:], in_=red[:], func=AF.Sin))

        sin_of(sinv, 0.0)
        sin_of(cosv, np.pi / 2)
        PWr = S([128, 9, 16], "PWr"); PWi = S([128, 9, 16], "PWi")
        IPr = S([128, 8, 16], "IPr"); IPi = S([128, 8, 16], "IPi")
        t1 = S([128, 16, 8, 16], "t1"); t2 = S([128, 16, 8, 16], "t2")

        def cmul(outr, outi, ar, ai, br, bi, shp, neg_i=False):
            a1 = t1[:].rearrange("p a b c -> p (a b c)")[:, 0:int(np.prod(shp[1:]))]
            a2 = t2[:].rearrange("p a b c -> p (a b c)")[:, 0:int(np.prod(shp[1:]))]
            if len(shp) == 3:
                a1 = a1.rearrange("p (a b) -> p a b", a=shp[1])
                a2 = a2.rearrange("p (a b) -> p a b", a=shp[1])
            if len(shp) == 4:
                a1 = t1[:, :, 0:shp[2], :]
                a2 = t2[:, :, 0:shp[2], :]
            tt_(a1, ar, br, ALU.mult)
            tt_(a2, ai, bi, ALU.mult)
            tt_(outr, a1, a2, ALU.subtract)
            tt_(a1, ar, bi, ALU.mult)
            tt_(a2, ai, br, ALU.mult)
            if neg_i:
                dv(lambda e: e.scalar_tensor_tensor(out=outi, in0=a1, scalar=-1.0, in1=a2,
                                                    op0=ALU.mult, op1=ALU.subtract))
            else:
                tt_(outi, a1, a2, ALU.add)

        dv(lambda e: e.memset(PWr[:, 0, :], 1.0))
        dv(lambda e: e.memset(PWi[:, 0, :], 0.0))
        dv(lambda e: e.memset(IPr[:, 0, :], 1.0))
        dv(lambda e: e.memset(IPi[:, 0, :], 0.0))
        tt_(PWr[:, 1, :], mag[:], cosv[:], ALU.mult)
        tt_(PWi[:, 1, :], mag[:], sinv[:], ALU.mult)
        tt_(IPr[:, 1, :], PWr[:, 1, :], imag2[:], ALU.mult)
        dv(lambda e: e.scalar_tensor_tensor(out=IPi[:, 1, :], in0=PWi[:, 1, :], scalar=-1.0, in1=imag2[:],
                                            op0=ALU.mult, op1=ALU.mult))
        for n in range(2, 9):
            cmul(PWr[:, n, :], PWi[:, n, :], PWr[:, n - 1, :], PWi[:, n - 1, :], PWr[:, 1, :], PWi[:, 1, :], sh16)
        for n in range(2, 8):
            cmul(IPr[:, n, :], IPi[:, n, :], IPr[:, n - 1, :], IPi[:, n - 1, :], IPr[:, 1, :], IPi[:, 1, :], sh16)
        dv(lambda e: e.tensor_copy(out=KAr[:, 0, :], in_=PWr[:, 8, :]))
        dv(lambda e: e.tensor_copy(out=KAi[:, 0, :], in_=PWi[:, 8, :]))
        for d_ in range(1, 9):
            cmul(KAr[:, d_, :], KAi[:, d_, :], KAr[:, d_ - 1, :], KAi[:, d_ - 1, :],
                 KAr[:, d_ - 1, :], KAi[:, d_ - 1, :], sh16)
        dv(lambda e: e.tensor_scalar(out=KnAi[:], in0=KAi[:], scalar1=-1.0, scalar2=None, op0=ALU.mult))
        am1 = S(sh16, "am1"); l2 = S(sh16, "l2"); il2 = S(sh16, "il2"); kr = S(sh16, "kr"); ki = S(sh16, "ki")
        dv(lambda e: e.tensor_scalar(out=am1[:], in0=PWr[:, 1, :], scalar1=-1.0, scalar2=None, op0=ALU.add))
        tt_(l2[:], lamre[:], lamre[:], ALU.mult)
        tt_(tmpa[:], lamim[:], lamim[:], ALU.mult)
        tt_(l2[:], l2[:], tmpa[:], ALU.add)
        dv(lambda e: e.reciprocal(out=il2[:], in_=l2[:]))
        tt_(kr[:], am1[:], lamre[:], ALU.mult)
        tt_(tmpa[:], PWi[:, 1, :], lamim[:], ALU.mult)
        tt_(kr[:], kr[:], tmpa[:], ALU.add)
        tt_(kr[:], kr[:], il2[:], ALU.mult)
        tt_(ki[:], PWi[:, 1, :], lamre[:], ALU.mult)
        tt_(tmpa[:], am1[:], lamim[:], ALU.mult)
        tt_(ki[:], ki[:], tmpa[:], ALU.subtract)
        tt_(ki[:], ki[:], il2[:], ALU.mult)
        sh3 = [128, 16, 16]

        def bc(a):
            return a.unsqueeze(2).to_broadcast(sh3)

        Bbr = S(sh3, "Bbr"); Bbi = S(sh3, "Bbi")
        cmul(Bbr[:], Bbi[:], bc(kr[:]), bc(ki[:]), Bre[:], Bim[:], sh3)
        Bhr = S([128, 16, 8, 16], "Bhr"); nBhi = S([128, 16, 8, 16], "nBhi"); Bhi = S([128, 16, 8, 16], "Bhi")
        Btr = S([128, 16, 8, 16], "Btr"); Bti = S([128, 16, 8, 16], "Bti")
        Chr = S([128, 16, 9, 16], "Chr"); Chi = S([128, 16, 9, 16], "Chi"); nChi = S([128, 16, 9, 16], "nChi")
        sh4 = [128, 16, 8, 16]

        def bk(a):
            return a.rearrange("p k r -> p r k").unsqueeze(3).to_broadcast(sh4)

        def bmid(a):
            return a.unsqueeze(2).to_broadcast(sh4)

        def b2(a):
            return a.unsqueeze(2).unsqueeze(3).to_broadcast(sh4)

        cmul(Bhr[:], Bhi[:], bk(IPr[:]), bk(IPi[:]), bmid(Bbr[:]), bmid(Bbi[:]), sh4)
        cmul(Btr[:], Bti[:], b2(PWr[:, 7, :]), b2(PWi[:, 7, :]), Bhr[:], Bhi[:], sh4)
        dv(lambda e: e.tensor_scalar(out=nBhi[:], in0=Bhi[:], scalar1=-1.0, scalar2=None, op0=ALU.mult))
        cmul(Chr[:, :, 0:8, :], Chi[:, :, 0:8, :], bk(PWr[:, 0:8, :]), bk(PWi[:, 0:8, :]), bmid(Cre[:]), bmid(Cim[:]), sh4)
        cmul(Chr[:, :, 8, :], Chi[:, :, 8, :], bc(PWr[:, 8, :]), bc(PWi[:, 8, :]), Cre[:], Cim[:], sh3)
        dv(lambda e: e.tensor_scalar(out=nChi[:], in0=Chi[:], scalar1=-1.0, scalar2=None, op0=ALU.mult))
        dv(lambda e: e.tensor_copy(out=C1r[:].rearrange("p r (j c) -> p r j c", j=8), in_=Chr[:, :, 1:9, :]))
        dv(lambda e: e.tensor_copy(out=nC1i[:].rearrange("p r (j c) -> p r j c", j=8), in_=nChi[:, :, 1:9, :]))
        mtmps = [S([128, 128], "mtmp0"), S([128, 128], "mtmp1")]
        cx.op("dve", lambda e: e.memset(W2r[:], 0.0), reads=ins_b, writes=[SU])
        cx.op("dve", lambda e: e.memset(W2i[:], 0.0), reads=ins_b, writes=[SU])
        for r in range(16):
            for two in range(2):
                g = 2 * r + two
                rng = slice(two * 64, (two + 1) * 64)
                ps = getps()
                cx.op("pe", lambda e, ps=ps, r=r, rng=rng: e.matmul(
                    ps[:, 0:128], lhsT=Bhr[rng, r, :, :].rearrange("p k c -> p (k c)"),
                    rhs=Chr[rng, r, 0:8, :].rearrange("p j c -> p (j c)"), start=True, stop=False),
                    reads=[SU], writes=[ps.b])
                cx.op("pe", lambda e, ps=ps, r=r, rng=rng: e.matmul(
                    ps[:, 0:128], lhsT=nBhi[rng, r, :, :].rearrange("p k c -> p (k c)"),
                    rhs=Chi[rng, r, 0:8, :].rearrange("p j c -> p (j c)"), start=False, stop=True),
                    reads=[SU], writes=[ps.b])
                mt_ = mtmps[g % 2]
                cx.op("dve", lambda e, ps=ps, mt_=mt_: e.tensor_tensor(out=mt_[:], in0=ps[:, 0:128], in1=cmask[:],
                                                                       op=ALU.mult),
                      reads=[ps.b, cmask.b], full=[mt_.b])
                cx.op("dve", lambda e, g=g, mt_=mt_: e.scalar_tensor_tensor(
                    out=M_all[:, g, :], in0=ident_f[:], scalar=dl[:, g:g + 1], in1=mt_[:],
                    op0=ALU.mult, op1=ALU.add),
                    reads=[ident_f.b, dl.b, mt_.b], writes=[M_all.b])
            for (Bt, W2) in ((Btr, W2r), (Bti, W2i)):
                ps = getps()
                cx.op("pe", lambda e, ps=ps, r=r, Bt=Bt: e.transpose(
                    out=ps[:, 0:128], in_=Bt[:, r, :, :].rearrange("p k c -> p (k c)"), identity=ident_f[:]),
                    reads=[SU, ident_f.b], writes=[ps.b])
                cx.op("act", lambda e, ps=ps, r=r, W2=W2: e.copy(out=W2[:, r, 0, 0:64], in_=ps[:, 0:64]),
                      reads=[ps.b], writes=[W2.b])
                cx.op("act", lambda e, ps=ps, r=r, W2=W2: e.copy(out=W2[:, r, 1, 64:128], in_=ps[:, 64:128]),
                      reads=[ps.b], writes=[W2.b])

        cx.barrier(skip_pool_dma=True)
        al.close()
        al = Alloc(nc)
        NCH = SEQ // 8
        Vg = [al.sb([128, NCH], BF16, f"Vg{i}") for i in range(4)]
        Sre = [[al.sb([128, NCH], F32, f"Sre{s}{i}") for i in range(2)] for s in range(2)]
        Sim = [[al.sb([128, NCH], F32, f"Sim{s}{i}") for i in range(2)] for s in range(2)]
        Sbr = [al.sb([128, NCH], BF16, f"Sbr{s}") for s in range(2)]
        Sbi = [al.sb([128, NCH], BF16, f"Sbi{s}") for s in range(2)]
        Gg = [al.sb([128, NCH], BF16, f"Gg{i}") for i in range(16)]
        ysf = [al.sb([128, SEQ], BF16, f"ysf{i}") for i in range(2)]
        for s in range(2):
            cx.op("pool", lambda e, s=s: e.memset(Sbr[s][:, 0:1], 0.0), writes=[Sbr[s].b])
            cx.op("pool", lambda e, s=s: e.memset(Sbi[s][:, 0:1], 0.0), writes=[Sbi[s].b])

        def b_front(r):
            f = r // 4
            st = r % 2
            vg = [Vg[(2 * r) % 4], Vg[(2 * r + 1) % 4]]
            for two in range(2):
                g = 2 * r + two
                gl = g % 8
                ps = getps()
                for k in range(8):
                    cx.op("pe", lambda e, ps=ps, gl=gl, k=k, f=f: e.matmul(
                        ps[:], lhsT=psel[:, gl, (7 - k) * 16:(7 - k) * 16 + 128],
                        rhs=u_all[:, f, k:SEQ:8], start=(k == 0), stop=(k == 7)),
                        reads=[psel.b, u_all.b], writes=[ps.b])
                cx.op("act", lambda e, ps=ps, v=vg[two]: e.copy(out=v[:], in_=ps[:]),
                      reads=[ps.b], full=[vg[two].b])
            psr = getps(); psi = getps()
            for (pp, W2) in ((psr, W2r), (psi, W2i)):
                for two in range(2):
                    cx.op("pe", lambda e, pp=pp, W2=W2, two=two, r=r, v=vg[two]: e.matmul(
                        pp[:], lhsT=W2[:, r, two, :], rhs=v[:], start=(two == 0), stop=(two == 1)),
                        reads=[W2.b, vg[two].b], writes=[pp.b])
            cx.op("act", lambda e, psr=psr, st=st: e.copy(out=Sre[st][0][:], in_=psr[:]),
                  reads=[psr.b], full=[Sre[st][0].b])
            cx.op("act", lambda e, psi=psi, st=st: e.copy(out=Sim[st][0][:], in_=psi[:]),
                  reads=[psi.b], full=[Sim[st][0].b])

        def b_mid(r):
            st = r % 2
            cur = 0
            for d_ in range(9):
                sh = 1 << d_
                s_r, s_i, d_r, d_i = Sre[st][cur], Sim[st][cur], Sre[st][1 - cur], Sim[st][1 - cur]
                n = NCH - sh
                cx.op("dve", lambda e, s_r=s_r, d_r=d_r, sh=sh, n=n, d_=d_, r=r: e.scalar_tensor_tensor(
                    out=d_r[:, sh:NCH], in0=s_r[:, 0:n], scalar=KAr[:, d_, r:r + 1], in1=s_r[:, sh:NCH],
                    op0=ALU.mult, op1=ALU.add), reads=[s_r.b, SU], writes=[d_r.b])
                cx.op("dve", lambda e, s_i=s_i, d_r=d_r, sh=sh, n=n, d_=d_, r=r: e.scalar_tensor_tensor(
                    out=d_r[:, sh:NCH], in0=s_i[:, 0:n], scalar=KnAi[:, d_, r:r + 1], in1=d_r[:, sh:NCH],
                    op0=ALU.mult, op1=ALU.add), reads=[s_i.b, SU], writes=[d_r.b])
                cx.op("dve", lambda e, s_i=s_i, d_i=d_i, sh=sh, n=n, d_=d_, r=r: e.scalar_tensor_tensor(
                    out=d_i[:, sh:NCH], in0=s_i[:, 0:n], scalar=KAr[:, d_, r:r + 1], in1=s_i[:, sh:NCH],
                    op0=ALU.mult, op1=ALU.add), reads=[s_i.b, SU], writes=[d_i.b])
                cx.op("dve", lambda e, s_r=s_r, d_i=d_i, sh=sh, n=n, d_=d_, r=r: e.scalar_tensor_tensor(
                    out=d_i[:, sh:NCH], in0=s_r[:, 0:n], scalar=KAi[:, d_, r:r + 1], in1=d_i[:, sh:NCH],
                    op0=ALU.mult, op1=ALU.add), reads=[s_r.b, SU], writes=[d_i.b])
                cx.op("pool", lambda e, s_r=s_r, d_r=d_r, sh=sh: e.tensor_copy(out=d_r[:, 0:sh], in_=s_r[:, 0:sh]),
                      reads=[s_r.b], writes=[d_r.b])
                cx.op("pool", lambda e, s_i=s_i, d_i=d_i, sh=sh: e.tensor_copy(out=d_i[:, 0:sh], in_=s_i[:, 0:sh]),
                      reads=[s_i.b], writes=[d_i.b])
                cur = 1 - cur
            fr, fi = Sre[st][cur], Sim[st][cur]
            cx.op("pool", lambda e, fr=fr, st=st: e.tensor_copy(out=Sbr[st][:, 1:NCH], in_=fr[:, 0:NCH - 1]),
                  reads=[fr.b], writes=[Sbr[st].b])
            cx.op("pool", lambda e, fi=fi, st=st: e.tensor_copy(out=Sbi[st][:, 1:NCH], in_=fi[:, 0:NCH - 1]),
                  reads=[fi.b], writes=[Sbi[st].b])

        def b_back(r):
            f = r // 4
            st = r % 2
            vg = [Vg[(2 * r) % 4], Vg[(2 * r + 1) % 4]]
            for two in range(2):
                g = 2 * r + two
                rng = slice(two * 64, (two + 1) * 64)
                ps = getps()
                cx.op("pe", lambda e, ps=ps, g=g, v=vg[two]: e.matmul(
                    ps[:], lhsT=M_all[:, g, :], rhs=v[:], start=True, stop=False),
                    reads=[M_all.b, vg[two].b], writes=[ps.b])
                cx.op("pe", lambda e, ps=ps, r=r, rng=rng, st=st: e.matmul(
                    ps[:], lhsT=C1r[rng, r, :], rhs=Sbr[st][rng, :], start=False, stop=False),
                    reads=[SU, Sbr[st].b], writes=[ps.b])
                cx.op("pe", lambda e, ps=ps, r=r, rng=rng, st=st: e.matmul(
                    ps[:], lhsT=nC1i[rng, r, :], rhs=Sbi[st][rng, :], start=False, stop=True),
                    reads=[SU, Sbi[st].b], writes=[ps.b])
                gg = Gg[g % 16]
                cx.op("act", lambda e, ps=ps, gg=gg: e.activation(out=gg[:], in_=ps[:], func=GELU),
                      reads=[ps.b], full=[gg.b])
            if r % 4 == 3:
                yb = ysf[f % 2]
                for j in range(8):
                    ps = getps()
                    for gl in range(8):
                        gg = Gg[(8 * f + gl) % 16]
                        cx.op("pe", lambda e, ps=ps, j=j, gl=gl, gg=gg: e.matmul(
                            ps[:], lhsT=psel[:, j, (7 - gl) * 16:(7 - gl) * 16 + 128], rhs=gg[:],
                            start=(gl == 0), stop=(gl == 7)),
                            reads=[psel.b, gg.b], writes=[ps.b])
                    cx.op("act", lambda e, ps=ps, yb=yb, j=j: e.copy(out=yb[:, j:SEQ:8], in_=ps[:]),
                          reads=[ps.b], writes=[yb.b])
                cx.op("sp", lambda e, yb=yb, f=f: e.dma_start(out=ys_scr[f], in_=yb[:]),
                      reads=[yb.b], dma=True)

        b_front(0)
        for r in range(16):
            precast(2, "act")
            b_mid(r)
            precast(2, "act")
            if r + 1 < 16:
                b_front(r + 1)
            precast(1, "act")
            b_back(r)
        precast(NEXP * 3, "act")

        cx.barrier()
        al.close()
        alAB.close()
        al = al_outer
        if stop_after in ("A", "B"):
            pass
        else:
            TC = 256
            NBC = SEQ // TC
            alC = Alloc(nc)
            wA = alC.sb([128, 8, 1536], BF16, "wA")
            wG = alC.sb([128, 8, 3072], BF16, "wG")
            wco = alC.sb([128, 4, 1024], BF16, "wco")
            wgl = alC.sb([128, 4, 2048], BF16, "wgl")
            wmo = alC.sb([128, 4, 1024], BF16, "wmo")
            wo = alC.sb([128, 8, 1024], BF16, "wo")
            Dg2 = [alC.sb([128, 31, 128], BF16, f"Dg{i}") for i in range(2)]
            kT = alC.sb([128, 4, 256], BF16, "kT")
            vtok = alC.sb([128, 2, 512], BF16, "vtok")
            gffn = alC.sb([128, D], F32, "gffn")
            wr = alC.sb([128, 8, 36], F32, "wr")
            rbias = alC.sb([128, 36], F32, "rbias")
            cdw = alC.sb([128, 4, 31], F32, "cdw")
            cb = alC.sb([128, 4], F32, "cb"); lng = alC.sb([128, 4], F32, "lng"); lnb = alC.sb([128, 4], F32, "lnb")
            onesm = alC.sb([128, 128], F32, "onesm")
            ones_bf = alC.sb([128, 128], BF16, "ones_bf")
            ecap = alC.sb([128, 32], F32, "ecap")
            tokid = alC.sb([128, NT], F32, "tokid")
            cum = alC.sb([128, 32], F32, "cum")
            lg_all = alC.sb([128, NT, 36], F32, "lg_all")
            trashp = alC.sb([128, 1], F32, "trashp")

            def ld(t, src):
                cx.op("sp", lambda e: e.dma_start(out=t[:], in_=src), full=[t.b], dma=True)

            ld(gffn, gffn_d.partition_broadcast(128))
            ld(wr, wr_d.rearrange("(c p) n -> p c n", p=128))
            ld(rbias, rbias_d.partition_broadcast(128))
            ld(cdw, cdw_d); ld(cb, cb_d); ld(lng, lng_d); ld(lnb, lnb_d)
            ld(ecap, ecap_d); ld(tokid, tokid_d); ld(trashp, trashp_d)
            cx.op("pool", lambda e: e.memset(onesm[:], 1.0 / 512.0), full=[onesm.b])
            cx.op("pool", lambda e: e.memset(ones_bf[:], 1.0), full=[ones_bf.b])
            cx.op("pool", lambda e: e.memset(cum[:], 0.0), full=[cum.b])
            alS = Alloc(nc)
            stg2 = [alS.sb([128, 8, 256], F32, f"stgc{i}") for i in range(3)]
            s2 = [0]

            def load_cast2(dst, dst_col0, src_d, c0, c1, kch, engs=("pool", "act", "dve")):
                for c in range(kch):
                    for n0 in range(c0, c1, 2048):
                        w = min(2048, c1 - n0)
                        s = stg2[s2[0] % len(stg2)]
                        eng = engs[s2[0] % len(engs)]
                        s2[0] += 1
                        sf_ = s[:].rearrange("p c n -> p (c n)")
                        cx.op("sp", lambda e, sf_=sf_, c=c, n0=n0, w=w: e.dma_start(out=sf_[:, 0:w], in_=src_d[:, c, n0:n0 + w]),
                              full=[s.b], dma=True)
                        o = dst_col0 + (n0 - c0)
                        if eng == "act":
                            cx.op("act", lambda e, sf_=sf_, c=c, o=o, w=w: e.copy(out=dst[:, c, o:o + w], in_=sf_[:, 0:w]),
                                  reads=[s.b], writes=[dst.b])
                        else:
                            cx.op(eng, lambda e, sf_=sf_, c=c, o=o, w=w: e.tensor_copy(out=dst[:, c, o:o + w], in_=sf_[:, 0:w]),
                                  reads=[s.b], writes=[dst.b])

            load_cast2(wA, 0, w_in_d, 0, 1024, 8)
            load_cast2(wA, 1024, w_in_d, 1536, 2048, 8)
            load_cast2(wG, 0, w_in_d, 2048, 5120, 8)
            load_cast2(wco, 0, wco_d, 0, 1024, 4)
            load_cast2(wgl, 0, wgl_d, 0, 2048, 4)
            load_cast2(wmo, 0, wmo_d, 0, 1024, 4)
            load_cast2(wo, 0, wo_d, 0, 1024, 8)
            wkv = alS.sb([128, 8, 1024], BF16, "wkv")
            load_cast2(wkv, 0, wkv_d, 0, 1024, 8)
            gmem = alS.sb([128, D], F32, "gmem")
            ld(gmem, gmem_d.partition_broadcast(128))
            memT = alS.sb([128, 8, 256], BF16, "memT")
            mx = alS.sb([128, D], F32, "mx")
            mss = alS.sb([128, 1], F32, "mss"); mrt = alS.sb([128, 1], F32, "mrt"); mrs = alS.sb([128, 1], F32, "mrs")
            mh = alS.sb([128, D], BF16, "mh")
            mjunk = mh
            for mt in range(2):
                cx.op("sp", lambda e, mt=mt: e.dma_start(out=mx[:], in_=mem_d[mt * 128:(mt + 1) * 128, :]),
                      full=[mx.b], dma=True)
                cx.op("act", lambda e: e.activation(out=mjunk[:], in_=mx[:], func=AF.Square, accum_out=mss[:]),
                      reads=[mx.b], writes=[mjunk.b], full=[mss.b])
                cx.op("act", lambda e: e.activation(out=mrt[:], in_=mss[:], func=AF.Sqrt, scale=1.0 / D, bias=EPS),
                      reads=[mss.b], full=[mrt.b])
                cx.op("dve", lambda e: e.reciprocal(out=mrs[:], in_=mrt[:]), reads=[mrt.b], full=[mrs.b])
                cx.op("dve", lambda e: e.scalar_tensor_tensor(out=mh[:], in0=mx[:], scalar=mrs[:, 0:1], in1=gmem[:],
                                                              op0=ALU.mult, op1=ALU.mult),
                      reads=[mx.b, mrs.b, gmem.b], full=[mh.b])
                pb = psb[mt % 2]
                for c in range(8):
                    cx.op("pe", lambda e, pb=pb, c=c: e.transpose(out=pb[:, c * 128:(c + 1) * 128],
                                                                  in_=mh[:, c * 128:(c + 1) * 128],
                                                                  identity=ident_bf[:]),
                          reads=[mh.b, ident_bf.b], writes=[pb.b])
                cx.op("act", lambda e, pb=pb, mt=mt: e.copy(out=memT[:, :, mt * 128:(mt + 1) * 128],
                                                            in_=pb[:].rearrange("p (c t) -> p c t", c=8)),
                      reads=[pb.b], writes=[memT.b])
            for hd in range(4):
                ps = getps()
                for c in range(8):
                    cx.op("pe", lambda e, ps=ps, c=c, hd=hd: e.matmul(
                        ps[:, 0:256], lhsT=wkv[:, c, hd * 128:(hd + 1) * 128], rhs=memT[:, c, :],
                        start=(c == 0), stop=(c == 7)), reads=[wkv.b, memT.b], writes=[ps.b])
                cx.op("dve", lambda e, ps=ps, hd=hd: e.tensor_copy(out=kT[:, hd, :], in_=ps[:, 0:256]),
                      reads=[ps.b], writes=[kT.b])
            for mc in range(2):
                ps = getps()
                for c in range(8):
                    cx.op("pe", lambda e, ps=ps, c=c, mc=mc: e.matmul(
                        ps[:], lhsT=memT[:, c, mc * 128:(mc + 1) * 128], rhs=wkv[:, c, 512:1024],
                        start=(c == 0), stop=(c == 7)), reads=[wkv.b, memT.b], writes=[ps.b])
                cx.op("dve", lambda e, ps=ps, mc=mc: e.tensor_copy(out=vtok[:, mc, :], in_=ps[:]),
                      reads=[ps.b], writes=[vtok.b])
            cx.barrier()
            alS.close()

            alW = Alloc(nc)
            hT = [alW.sb([128, 8, TC], BF16, f"hTc{i}") for i in range(2)]
            ysb = [alW.sb([128, 4, TC], BF16, "ysb0")] * 2
            vbuf = alW.sb([128, 4, 30 + TC], BF16, "vbuf")
            sgt = [alW.sb([128, TC], F32, f"sgt{i}") for i in range(3)]
            cv = alW.sb([128, 4, TC], F32, "cv")
            sq = [sgt[1], sgt[2]]
            mean = alW.sb([128, TC], F32, "mean")
            var = alW.sb([128, TC], F32, "var"); lrs = alW.sb([128, TC], F32, "lrs")
            m2 = var; lnv = lrs
            cn = alW.sb([128, 4, TC], BF16, "cn")
            qb = alW.sb([128, 4, TC], BF16, "qb")
            Eb = [alW.sb([128, 2, TC], BF16, f"Eb{i}") for i in range(2)]
            ob = alW.sb([128, 4, TC], BF16, "ob")
            macc = alW.sb([128, TC], F32, "macc"); mt1 = alW.sb([128, TC], F32, "mt1"); mt2 = alW.sb([128, TC], F32, "mt2")
            rden = macc
            xc = [mt1, mt2]
            sqf = [sgt[1], sgt[2], mt1, mt2]
            merged = alW.sb([128, 8, TC], BF16, "merged")
            xt2 = [alW.sb([128, D], F32, f"xtc{i}") for i in range(2)]
            x2t = xt2
            h2f = alW.sb([128, D], F32, "h2f"); h2b = [alW.sb([128, D], BF16, "h2b0")] * 2
            junk2 = h2b[0]
            h2T = alW.sb([128, 8, 128], F32, "h2T")
            ss2 = alW.sb([128, 1], F32, "ss2"); rt2 = alW.sb([128, 1], F32, "rt2"); rs2 = alW.sb([128, 1], F32, "rs2")
            cx.op("pool", lambda e: e.memset(vbuf[:], 0.0), full=[vbuf.b])

            breg = {}

            def mmgrp(ps_ap, ps_b, pairs, reads):
                n = len(pairs)
                for idx, (l, r_) in enumerate(pairs):
                    cx.op("pe", lambda e, l=l, r_=r_, idx=idx: e.matmul(ps_ap, lhsT=l, rhs=r_, start=(idx == 0),
                                                                         stop=(idx == n - 1)),
                          reads=reads, writes=[ps_b])

            KCUT = int(os.environ.get("KCUT", "9"))
            KNB = int(os.environ.get("KNB", str(NBC)))
            def c_load_h(bi):
                t0 = bi * TC
                h = hT[bi % 2]
                cx.op("sp", lambda e, h=h, t0=t0: e.dma_start(
                    out=h[:], in_=hT_scr[:, :, t0:t0 + TC].rearrange("c p t -> p c t")), full=[h.b], dma=True)

            def c_load_y(bi):
                t0 = bi * TC
                yb = ysb[bi % 2]
                cx.op("sp", lambda e, yb=yb, t0=t0: e.dma_start(
                    out=yb[:], in_=ys_scr[:, :, t0:t0 + TC].rearrange("f p t -> p f t")), full=[yb.b], dma=True)

            def c_s2(bi):
                t0 = bi * TC
                h = hT[bi % 2]; yb = ysb[bi % 2]
                for f in range(4):
                    pa = getps(); pg = getps()
                    mmgrp(pa[:, 0:TC], pa.b, [(wA[:, c, f * 128:(f + 1) * 128], h[:, c, :]) for c in range(8)],
                          [wA.b, h.b])
                    mmgrp(pg[:, 0:TC], pg.b, [(wA[:, c, 512 + f * 128:512 + (f + 1) * 128], h[:, c, :]) for c in range(8)],
                          [wA.b, h.b])
                    s = sgt[f % 3]
                    cx.op("act", lambda e, pg=pg, s=s: e.activation(out=s[:], in_=pg[:, 0:TC], func=AF.Sigmoid),
                          reads=[pg.b], full=[s.b])
                    cx.op("dve", lambda e, pa=pa, s=s, f=f: e.tensor_tensor(out=vbuf[:, f, 30:30 + TC], in0=pa[:, 0:TC],
                                                                            in1=s[:], op=ALU.mult),
                          reads=[pa.b, s.b], writes=[vbuf.b])

            def c_taps(bi, fs):
                for f in fs:
                    pc = getps()
                    Dg = Dg2[f % 2]
                    cx.op("pool", lambda e, Dg=Dg, f=f: e.tensor_tensor(
                        out=Dg[:], in0=ident_f[:].unsqueeze(1).to_broadcast([128, 31, 128]),
                        in1=cdw[:, f, :].unsqueeze(2).to_broadcast([128, 31, 128]), op=ALU.mult),
                        reads=[ident_f.b, cdw.b], full=[Dg.b])
                    mmgrp(pc[:, 0:TC], pc.b, [(Dg[:, k, :], vbuf[:, f, k:k + TC]) for k in range(31)],
                          [Dg.b, vbuf.b])
                    cx.op("act", lambda e, pc=pc, f=f: e.activation(out=cv[:, f, :], in_=pc[:, 0:TC], func=AF.Identity,
                                                                    bias=cb[:, f:f + 1], scale=1.0),
                          reads=[pc.b, cb.b], writes=[cv.b])
                if 3 in fs:
                    cx.op("pool", lambda e: e.tensor_copy(out=vbuf[:, :, 0:30], in_=vbuf[:, :, TC:TC + 30]),
                          reads=[vbuf.b], writes=[vbuf.b])

            def c_rest(bi):
                t0 = bi * TC
                h = hT[bi % 2]; yb = ysb[bi % 2]
                for hd in range(4):
                    pq_ = getps()
                    mmgrp(pq_[:, 0:TC], pq_.b, [(wA[:, c, 1024 + hd * 128:1024 + (hd + 1) * 128], h[:, c, :])
                                                for c in range(8)], [wA.b, h.b])
                    cx.op("dve", lambda e, pq_=pq_, hd=hd: e.tensor_copy(out=qb[:, hd, :], in_=pq_[:, 0:TC]),
                          reads=[pq_.b], writes=[qb.b])
                for f in range(4):
                    cx.op("act", lambda e, f=f: e.activation(out=sq[f % 2][:] if False else sqf[f][:], in_=cv[:, f, :],
                                                             func=AF.Square),
                          reads=[cv.b], full=[sqf[f].b])

                def att_scores(hd):
                    E = Eb[hd % 2]
                    for mc in range(2):
                        psc = getps()
                        mmgrp(psc[:, 0:TC], psc.b, [(kT[:, hd, mc * 128:(mc + 1) * 128], qb[:, hd, :])], [kT.b, qb.b])
                        cx.op("act", lambda e, psc=psc, E=E, mc=mc: e.activation(
                            out=E[:, mc, :], in_=psc[:, 0:TC], func=AF.Exp, scale=float(128 ** -0.5)),
                            reads=[psc.b], writes=[E.b])

                def att_out(hd):
                    E = Eb[hd % 2]
                    po = getps(); pd = getps()
                    mmgrp(po[:, 0:TC], po.b, [(vtok[:, mc, hd * 128:(hd + 1) * 128], E[:, mc, :]) for mc in range(2)],
                          [vtok.b, E.b])
                    mmgrp(pd[:, 0:TC], pd.b, [(ones_bf[:], E[:, mc, :]) for mc in range(2)], [ones_bf.b, E.b])
                    cx.op("dve", lambda e, pd=pd: e.reciprocal(out=rden[:], in_=pd[:, 0:TC]), reads=[pd.b], full=[rden.b])
                    cx.op("dve", lambda e, po=po, hd=hd: e.tensor_tensor(out=ob[:, hd, :], in0=po[:, 0:TC], in1=rden[:],
                                                                         op=ALU.mult),
                          reads=[po.b, rden.b], writes=[ob.b])

                att_scores(0)
                att_scores(1)
                pm = getps(); pq = getps()
                mmgrp(pm[:, 0:TC], pm.b, [(onesm[:], cv[:, f, :]) for f in range(4)], [onesm.b, cv.b])
                mmgrp(pq[:, 0:TC], pq.b, [(onesm[:], sqf[f][:]) for f in range(4)], [onesm.b] + [sqf[f].b for f in range(4)])
                cx.op("act", lambda e, pm=pm: e.copy(out=mean[:], in_=pm[:, 0:TC]), reads=[pm.b], full=[mean.b])
                cx.op("dve", lambda e: e.tensor_tensor(out=var[:], in0=mean[:], in1=mean[:], op=ALU.mult),
                      reads=[mean.b], full=[var.b])
                cx.op("dve", lambda e, pq=pq: e.tensor_tensor(out=var[:], in0=pq[:, 0:TC], in1=var[:], op=ALU.subtract),
                      reads=[pq.b], writes=[var.b])
                cx.op("dve", lambda e: e.tensor_scalar(out=var[:], in0=var[:], scalar1=float(EPS), scalar2=None,
                                                       op0=ALU.add), reads=[var.b], writes=[var.b])
                cx.op("act", lambda e: e.activation(out=lrs[:], in_=var[:], func=AF.Ln), reads=[var.b], full=[lrs.b])
                cx.op("act", lambda e: e.activation(out=lrs[:], in_=lrs[:], func=AF.Exp, scale=-0.5),
                      reads=[], writes=[lrs.b])
                cx.op("dve", lambda e: e.tensor_tensor(out=cv[:], in0=cv[:],
                                                       in1=mean[:].unsqueeze(1).to_broadcast([128, 4, TC]),
                                                       op=ALU.subtract), reads=[mean.b], writes=[cv.b])
                cx.op("dve", lambda e: e.tensor_tensor(out=cv[:], in0=cv[:],
                                                       in1=lrs[:].unsqueeze(1).to_broadcast([128, 4, TC]),
                                                       op=ALU.mult), reads=[lrs.b], writes=[cv.b])
                att_out(0)
                att_scores(2)
                att_out(1)
                att_scores(3)
                att_out(2)
                att_out(3)
                for f in range(4):
                    cx.op("act", lambda e, f=f: e.activation(out=cn[:, f, :], in_=cv[:, f, :], func=AF.Silu,
                                                             bias=lnb[:, f:f + 1], scale=lng[:, f:f + 1]),
                          reads=[cv.b, lnb.b, lng.b], writes=[cn.b])
                for j in range(8):
                    js = slice(j * 128, (j + 1) * 128)
                    pga = getps(); pyc = getps()
                    mmgrp(pga[:, 0:TC], pga.b, [(wG[:, c, j * 128:(j + 1) * 128], h[:, c, :]) for c in range(8)], [wG.b, h.b])
                    mmgrp(pyc[:, 0:TC], pyc.b, [(wco[:, f, js], cn[:, f, :]) for f in range(4)], [wco.b, cn.b])
                    s = sgt[0]
                    cx.op("act", lambda e, pga=pga, s=s: e.activation(out=s[:], in_=pga[:, 0:TC], func=AF.Sigmoid),
                          reads=[pga.b], full=[s.b])
                    cx.op("dve", lambda e, pyc=pyc, s=s: e.tensor_tensor(out=macc[:], in0=pyc[:, 0:TC], in1=s[:], op=ALU.mult),
                          reads=[pyc.b, s.b], full=[macc.b])
                    pgb = getps(); pza = getps(); pzb = getps()
                    mmgrp(pgb[:, 0:TC], pgb.b, [(wG[:, c, 1024 + j * 128:1024 + (j + 1) * 128], h[:, c, :]) for c in range(8)],
                          [wG.b, h.b])
                    mmgrp(pza[:, 0:TC], pza.b, [(wgl[:, f, js], yb[:, f, :]) for f in range(4)], [wgl.b, yb.b])
                    mmgrp(pzb[:, 0:TC], pzb.b, [(wgl[:, f, 1024 + j * 128:1024 + (j + 1) * 128], yb[:, f, :]) for f in range(4)],
                          [wgl.b, yb.b])
                    sb_ = sgt[1]; sz = sgt[2]
                    cx.op("act", lambda e, pgb=pgb, sb_=sb_: e.activation(out=sb_[:], in_=pgb[:, 0:TC], func=AF.Sigmoid),
                          reads=[pgb.b], full=[sb_.b])
                    cx.op("act", lambda e, pzb=pzb, sz=sz: e.activation(out=sz[:], in_=pzb[:, 0:TC], func=AF.Sigmoid),
                          reads=[pzb.b], full=[sz.b])
                    cx.op("dve", lambda e, pza=pza, sz=sz: e.tensor_tensor(out=mt1[:], in0=pza[:, 0:TC], in1=sz[:], op=ALU.mult),
                          reads=[pza.b, sz.b], full=[mt1.b])
                    cx.op("dve", lambda e, sb_=sb_: e.tensor_tensor(out=mt1[:], in0=mt1[:], in1=sb_[:], op=ALU.mult),
                          reads=[sb_.b], writes=[mt1.b])
                    cx.op("dve", lambda e: e.tensor_tensor(out=macc[:], in0=macc[:], in1=mt1[:], op=ALU.add),
                          reads=[mt1.b], writes=[macc.b])
                    pgc = getps(); pym = getps()
                    mmgrp(pgc[:, 0:TC], pgc.b, [(wG[:, c, 2048 + j * 128:2048 + (j + 1) * 128], h[:, c, :]) for c in range(8)],
                          [wG.b, h.b])
                    mmgrp(pym[:, 0:TC], pym.b, [(wmo[:, hd, js], ob[:, hd, :]) for hd in range(4)], [wmo.b, ob.b])
                    s = sgt[0]
                    cx.op("act", lambda e, pgc=pgc, s=s: e.activation(out=s[:], in_=pgc[:, 0:TC], func=AF.Sigmoid),
                          reads=[pgc.b], full=[s.b])
                    cx.op("dve", lambda e, pym=pym, s=s: e.tensor_tensor(out=mt2[:], in0=pym[:, 0:TC], in1=s[:], op=ALU.mult),
                          reads=[pym.b, s.b], full=[mt2.b])
                    cx.op("dve", lambda e, j=j: e.tensor_tensor(out=merged[:, j, :], in0=macc[:], in1=mt2[:], op=ALU.add),
                          reads=[macc.b, mt2.b], writes=[merged.b])

            def c_tail_a(bi):
                ntt = TC // 128
                tis = [bi * ntt + tt for tt in range(ntt)]
                for tt, ti in enumerate(tis):
                    xt_ = xt2[ti % 2]
                    cx.op("sp", lambda e, xt_=xt_, ti=ti: e.dma_start(out=xt_[:], in_=x_d[ti * 128:(ti + 1) * 128, :]),
                          full=[xt_.b], dma=True)
                for tt, ti in enumerate(tis):
                    xt_ = xt2[ti % 2]; x2 = xt_
                    for half in range(2):
                        po_ = getps()
                        mmgrp(po_[:], po_.b, [(merged[:, j, tt * 128:(tt + 1) * 128], wo[:, j, half * 512:(half + 1) * 512])
                                              for j in range(8)], [merged.b, wo.b])
                        cx.op("dve", lambda e, po_=po_, x2=x2, xt_=xt_, half=half: e.tensor_tensor(
                            out=x2[:, half * 512:(half + 1) * 512], in0=po_[:], in1=xt_[:, half * 512:(half + 1) * 512],
                            op=ALU.add), reads=[po_.b, xt_.b], writes=[x2.b])
                    cx.op("sp", lambda e, x2=x2, ti=ti: e.dma_start(out=x2_scr[ti * 128:(ti + 1) * 128, :], in_=x2[:]),
                          reads=[x2.b], dma=True)

            def c_tail_norm(ti):
                if True:
                    x2 = xt2[ti % 2]; hb2 = h2b[0]
                    cx.op("act", lambda e, x2=x2: e.activation(out=junk2[:], in_=x2[:], func=AF.Square, accum_out=ss2[:]),
                          reads=[x2.b], full=[junk2.b, ss2.b])
                    cx.op("act", lambda e: e.activation(out=rt2[:], in_=ss2[:], func=AF.Sqrt, scale=1.0 / D, bias=EPS),
                          reads=[ss2.b], full=[rt2.b])
                    cx.op("dve", lambda e: e.reciprocal(out=rs2[:], in_=rt2[:]), reads=[rt2.b], full=[rs2.b])
                    cx.op("dve", lambda e, x2=x2: e.scalar_tensor_tensor(out=h2f[:], in0=x2[:], scalar=rs2[:, 0:1],
                                                                         in1=gffn[:], op0=ALU.mult, op1=ALU.mult),
                          reads=[x2.b, rs2.b, gffn.b], full=[h2f.b])
                    cx.op("act", lambda e, hb2=hb2: e.copy(out=hb2[:], in_=h2f[:]), reads=[h2f.b], full=[hb2.b])
                    cx.op("sp", lambda e, hb2=hb2, ti=ti: e.dma_start(out=h2_scr[ti * 128:(ti + 1) * 128, :], in_=hb2[:]),
                          reads=[hb2.b], dma=True)

            def c_tail_pe(ti):
                if True:
                    pra = getps(); prb = getps()
                    for c in range(8):
                        pr = pra if c < 4 else prb
                        cx.op("pe", lambda e, pr=pr, c=c: e.transpose(out=pr[:, (c % 4) * 128:(c % 4 + 1) * 128],
                                                                      in_=h2f[:, c * 128:(c + 1) * 128], identity=ident_f[:]),
                              reads=[h2f.b, ident_f.b], writes=[pr.b])
                    cx.op("act", lambda e, pra=pra: e.copy(out=h2T[:, 0:4, :], in_=pra[:].rearrange("p (c t) -> p c t", c=4)),
                          reads=[pra.b], writes=[h2T.b])
                    cx.op("dve", lambda e, prb=prb: e.tensor_copy(out=h2T[:, 4:8, :],
                                                                  in_=prb[:].rearrange("p (c t) -> p c t", c=4)),
                          reads=[prb.b], writes=[h2T.b])
                    plg = getps()
                    mmgrp(plg[:, 0:36], plg.b, [(h2T[:, c, :], wr[:, c, :]) for c in range(8)], [h2T.b, wr.b])
                    cx.op("dve", lambda e, plg=plg, ti=ti: e.tensor_tensor(out=lg_all[:, ti, :], in0=plg[:, 0:36],
                                                                        in1=rbias[:], op=ALU.add),
                          reads=[plg.b, rbias.b], writes=[lg_all.b])

            NBR = min(NBC, KNB) if KCUT >= 2 else 0
            if NBR > 0:
                c_load_h(0); c_load_y(0); c_s2(0); c_taps(0, [0, 1, 2, 3])
            for bi in range(NBR):
                nxt = bi + 1 < NBR
                if nxt:
                    c_load_h(bi + 1)
                c_rest(bi)
                if nxt:
                    c_load_y(bi + 1)
                    c_s2(bi + 1)
                c_tail_a(bi)
                t_a, t_b = 2 * bi, 2 * bi + 1
                c_tail_norm(t_a)
                if nxt:
                    c_taps(bi + 1, [0, 1])
                c_tail_pe(t_a)
                c_tail_norm(t_b)
                if nxt:
                    c_taps(bi + 1, [2, 3])
                c_tail_pe(t_b)
            cx.barrier()
            alW.close()
            alR = Alloc(nc)
            RS = Buf("route")
            tri = alR.sb([128, 128], F32, "tri")
            ones_f = alR.sb([128, 128], F32, "ones_f")
            cx.op("sp", lambda e: e.dma_start(out=tri[:], in_=tri_d), full=[tri.b], dma=True)
            cx.op("pool", lambda e: e.memset(ones_f[:], 1.0), full=[ones_f.b])

            def rd(fn, extra_reads=(), extra_writes=()):
                cx.op("dve", fn, reads=[RS, lg_all.b] + list(extra_reads), writes=[RS] + list(extra_writes))

            def R(shape, name, dt=F32):
                return alR.sb(shape, dt, name)

            NTT = NT
            NEB_ = NEXP * NBLK
            gmax = R([128, NTT], "gmax"); ohg = R([128, NTT, 4], "ohg"); eg = R([128, NTT, 4], "eg")
            sumg = R([128, NTT], "sumg"); ptop = R([128, NTT], "ptop")
            selm = R([128, NTT, 4, 8], "selm"); sel = R([128, NTT, 8], "sel"); sel2 = R([128, NTT, 8], "sel2")
            m1_ = R([128, NTT], "m1_"); m2_ = R([128, NTT], "m2_"); oh1 = R([128, NTT, 8], "oh1"); oh2 = R([128, NTT, 8], "oh2")
            dm = R([128, NTT], "dm"); w1 = R([128, NTT], "w1"); w2 = R([128, NTT], "w2")
            M1 = R([128, NTT, 4, 8], "M1"); M2 = R([128, NTT, 4, 8], "M2"); Mc = R([128, NTT, 32], "Mc")
            Cex = R([128, NTT, 32], "Cex"); pos = R([128, NTT, 32], "pos"); bk = R([128, NTT, 32], "bk")
            sf = R([128, NTT, 32], "sf"); ov = R([128, NTT, 32], "ov"); tq = R([128, NTT, 32], "tq")
            sk = [R([128, NTT], f"sk{k}") for k in range(2)]; okk = R([128, NTT], "okk"); dd = R([128, NTT], "dd")
            si = [R([128, NTT], f"si{k}", I32) for k in range(2)]
            ent = [R([128, NTT, 4], f"ent{k}") for k in range(2)]
            le4 = lg_all[:, :, 4:36].rearrange("p t (g j) -> p t g j", g=4)

            def bc3(a, n):
                return a.unsqueeze(2).to_broadcast([128, NTT, n])

            rd(lambda e: e.tensor_reduce(out=gmax[:], in_=lg_all[:, :, 0:4], axis=AX.X, op=ALU.max))
            rd(lambda e: e.tensor_tensor(out=ohg[:], in0=lg_all[:, :, 0:4], in1=bc3(gmax[:], 4), op=ALU.is_equal))
            rd(lambda e: e.tensor_tensor(out=eg[:], in0=lg_all[:, :, 0:4], in1=bc3(gmax[:], 4), op=ALU.subtract))
            cx.op("act", lambda e: e.activation(out=eg[:], in_=eg[:], func=AF.Exp), reads=[RS], writes=[RS])
            rd(lambda e: e.tensor_reduce(out=sumg[:], in_=eg[:], axis=AX.X, op=ALU.add))
            rd(lambda e: e.reciprocal(out=ptop[:], in_=sumg[:]))
            rd(lambda e: e.tensor_tensor(out=selm[:], in0=le4,
                                         in1=ohg[:].unsqueeze(3).to_broadcast([128, NTT, 4, 8]), op=ALU.mult))
            rd(lambda e: e.tensor_reduce(out=sel[:], in_=selm[:].rearrange("p t g j -> p t j g"), axis=AX.X, op=ALU.add))
            rd(lambda e: e.tensor_reduce(out=m1_[:], in_=sel[:], axis=AX.X, op=ALU.max))
            rd(lambda e: e.tensor_tensor(out=oh1[:], in0=sel[:], in1=bc3(m1_[:], 8), op=ALU.is_equal))
            rd(lambda e: e.scalar_tensor_tensor(out=sel2[:], in0=oh1[:], scalar=-1e30, in1=sel[:], op0=ALU.mult, op1=ALU.add))
            rd(lambda e: e.tensor_reduce(out=m2_[:], in_=sel2[:], axis=AX.X, op=ALU.max))
            rd(lambda e: e.tensor_tensor(out=oh2[:], in0=sel2[:], in1=bc3(m2_[:], 8), op=ALU.is_equal))
            rd(lambda e: e.tensor_tensor(out=dm[:], in0=m1_[:], in1=m2_[:], op=ALU.subtract))
            cx.op("act", lambda e: e.activation(out=w1[:], in_=dm[:], func=AF.Sigmoid), reads=[RS], writes=[RS])
            rd(lambda e: e.tensor_tensor(out=w1[:], in0=w1[:], in1=ptop[:], op=ALU.mult))
            rd(lambda e: e.tensor_tensor(out=w2[:], in0=ptop[:], in1=w1[:], op=ALU.subtract))
            rd(lambda e: e.tensor_tensor(out=M1[:], in0=ohg[:].unsqueeze(3).to_broadcast([128, NTT, 4, 8]),
                                         in1=oh1[:].unsqueeze(2).to_broadcast([128, NTT, 4, 8]), op=ALU.mult))
            rd(lambda e: e.tensor_tensor(out=M2[:], in0=ohg[:].unsqueeze(3).to_broadcast([128, NTT, 4, 8]),
                                         in1=oh2[:].unsqueeze(2).to_broadcast([128, NTT, 4, 8]), op=ALU.mult))
            rd(lambda e: e.tensor_tensor(out=Mc[:], in0=M1[:].rearrange("p t g j -> p t (g j)"),
                                         in1=M2[:].rearrange("p t g j -> p t (g j)"), op=ALU.add))
            rd(lambda e: e.memset(Cex[:, 0, :], 0.0))
            for i in range(1, NTT):
                rd(lambda e, i=i: e.tensor_tensor(out=Cex[:, i, :], in0=Cex[:, i - 1, :], in1=Mc[:, i - 1, :], op=ALU.add))
            pp = [getps(), getps()]
            for i in range(NTT):
                pb_ = pp[i // 16]
                o_ = pb_[:, (i % 16) * 32:(i % 16 + 1) * 32]
                cx.op("pe", lambda e, o_=o_, i=i: e.matmul(o_, lhsT=tri[:], rhs=Mc[:, i, :], start=True, stop=False),
                      reads=[tri.b, RS], writes=[pb_.b])
                cx.op("pe", lambda e, o_=o_, i=i: e.matmul(o_, lhsT=ones_f[:], rhs=Cex[:, i, :], start=False, stop=True),
                      reads=[ones_f.b, RS], writes=[pb_.b])
            for hh in range(2):
                rd(lambda e, hh=hh: e.tensor_copy(out=pos[:, hh * 16:(hh + 1) * 16, :],
                                                  in_=pp[hh][:].rearrange("p (t x) -> p t x", t=16)), [pp[hh].b])
            rd(lambda e: e.tensor_single_scalar(out=bk[:], in_=pos[:], scalar=127.5, op=ALU.is_gt))
            for thr in range(2, NBLK):
                rd(lambda e, thr=thr: e.tensor_single_scalar(out=tq[:], in_=pos[:], scalar=128.0 * thr - 0.5, op=ALU.is_gt))
                rd(lambda e: e.tensor_tensor(out=bk[:], in0=bk[:], in1=tq[:], op=ALU.add))
            rd(lambda e: e.scalar_tensor_tensor(out=bk[:], in0=bk[:], scalar=float(1 - 128 * NEB_),
                                                in1=ecap[:].unsqueeze(1).to_broadcast([128, NTT, 32]),
                                                op0=ALU.mult, op1=ALU.add), [ecap.b])
            rd(lambda e: e.scalar_tensor_tensor(out=sf[:], in0=pos[:], scalar=float(NEB_), in1=bk[:],
                                                op0=ALU.mult, op1=ALU.add))
            rd(lambda e: e.tensor_single_scalar(out=ov[:], in_=pos[:], scalar=float(CAP) - 0.5, op=ALU.is_gt))
            for k, (Mk, wk) in enumerate(((M1, w1), (M2, w2))):
                Mk32 = Mk[:].rearrange("p t g j -> p t (g j)")
                rd(lambda e, Mk32=Mk32: e.tensor_tensor(out=tq[:], in0=Mk32, in1=sf[:], op=ALU.mult))
                rd(lambda e, k=k: e.tensor_reduce(out=sk[k][:], in_=tq[:], axis=AX.X, op=ALU.add))
                rd(lambda e, Mk32=Mk32: e.tensor_tensor(out=tq[:], in0=Mk32, in1=ov[:], op=ALU.mult))
                rd(lambda e: e.tensor_reduce(out=okk[:], in_=tq[:], axis=AX.X, op=ALU.add))
                rd(lambda e, k=k: e.tensor_scalar(out=dd[:], in0=sk[k][:], scalar1=trashp[:, 0:1], scalar2=None,
                                                  op0=ALU.subtract), [trashp.b])
                rd(lambda e: e.tensor_tensor(out=dd[:], in0=dd[:], in1=okk[:], op=ALU.mult))
                rd(lambda e, k=k: e.tensor_tensor(out=sk[k][:], in0=sk[k][:], in1=dd[:], op=ALU.subtract))
                rd(lambda e, k=k: e.tensor_copy(out=si[k][:], in_=sk[k][:]), (), [si[k].b])
                rd(lambda e, k=k: e.memset(ent[k][:], 0.0), (), [ent[k].b])
                rd(lambda e, k=k: e.tensor_copy(out=ent[k][:, :, 0], in_=tokid[:]), [tokid.b], [ent[k].b])
                rd(lambda e, k=k: e.tensor_scalar(out=ent[k][:, :, 1], in0=tokid[:], scalar1=float(k * ROWS), scalar2=None,
                                                  op0=ALU.add), [tokid.b], [ent[k].b])
                rd(lambda e, k=k, wk=wk: e.tensor_copy(out=ent[k][:, :, 2], in_=wk[:]), (), [ent[k].b])
            for i in range(NTT):
                for k in range(2):
                    cx.op("pool", lambda e, i=i, k=k: e.indirect_dma_start(
                        out=lst_d, out_offset=bass.IndirectOffsetOnAxis(ap=si[k][:, i:i + 1], axis=0),
                        in_=ent[k][:, i, :], in_offset=None),
                        reads=[si[k].b, ent[k].b, lstB], dma=True)
            cx.barrier()
            alR.close()
            alC.close()
        if stop_after in ("A", "B", "C"):
            pass
        else:
            alD = Alloc(nc)
            NEB = NEXP * NBLK
            lst_sb = alD.sb([128, NEB, 4], F32, "lst_sb")
            idx_i = alD.sb([128, NEB], I32, "idx_i")
            dst_i = alD.sb([128, NEB], I32, "dst_i")
            cx.op("sp", lambda e: e.dma_start(out=lst_sb[:], in_=lst_d[0:NEXP * CAP, :].rearrange("(s eb) w -> s eb w", s=128)),
                  reads=[lstB], full=[lst_sb.b], dma=True)
            cx.op("dve", lambda e: e.tensor_copy(out=idx_i[:], in_=lst_sb[:, :, 0]), reads=[lst_sb.b], full=[idx_i.b])
            cx.op("dve", lambda e: e.tensor_copy(out=dst_i[:], in_=lst_sb[:, :, 1]), reads=[lst_sb.b], full=[dst_i.b])
            NWB = 3
            Wg = [alD.sb([128, 8, 256], BF16, f"Wg{i}") for i in range(NWB)]
            Wu = [alD.sb([128, 8, 256], BF16, f"Wu{i}") for i in range(NWB)]
            Wd = [alD.sb([128, 2, 1024], BF16, f"Wd{i}") for i in range(NWB)]
            Gt = [alD.sb([128, D], BF16, f"Gt{i}") for i in range(3)]
            Xe = [alD.sb([128, 8, CAP], BF16, f"Xe{i}") for i in range(2)]
            sgl = [alD.sb([128, CAP], F32, f"sgl{i}") for i in range(2)]
            ae = [alD.sb([128, 2, CAP], BF16, f"ae{i}") for i in range(2)]
            Yt = [alD.sb([128, D], BF16, f"Yt{i}") for i in range(3)]

            Gt6 = Gt + [alD.sb([128, D], BF16, f"Gtx{i}") for i in range(3)]

            def load_w_dma(e_):
                p = e_ % NWB
                cx.op("sp", lambda e: e.dma_start(out=Wg[p][:].rearrange("p c n -> p (c n)"), in_=wbf_scr[e_, 0]),
                      full=[Wg[p].b], dma=True)
                cx.op("sp", lambda e: e.dma_start(out=Wu[p][:].rearrange("p c n -> p (c n)"), in_=wbf_scr[e_, 1]),
                      full=[Wu[p].b], dma=True)
                cx.op("sp", lambda e: e.dma_start(out=Wd[p][:].rearrange("p c n -> p (c n)"), in_=wbf_scr[e_, 2]),
                      full=[Wd[p].b], dma=True)

            def load_w_cast(e_):
                pass

            def gathers(e_):
                for blk in range(NBLK):
                    eb = e_ * NBLK + blk
                    G = Gt6[(e_ % 2) * 3 + blk]
                    cx.op("pool", lambda e, G=G, eb=eb: e.indirect_dma_start(
                        out=G[:], out_offset=None, in_=h2_scr,
                        in_offset=bass.IndirectOffsetOnAxis(ap=idx_i[:, eb:eb + 1], axis=0)),
                        reads=[idx_i.b, h2B], full=[G.b], dma=True)

            gi = [0]
            load_w_dma(0)
            load_w_dma(1)
            gathers(0)
            KNE = int(os.environ.get("KNE", str(NEXP)))
            for e_ in range(KNE):
                p = e_ % NWB
                if e_ + 2 < NEXP:
                    load_w_dma(e_ + 2)
                if e_ + 1 < NEXP:
                    gathers(e_ + 1)
                X = Xe[e_ % 2]
                for blk in range(NBLK):
                    G = Gt6[(e_ % 2) * 3 + blk]
                    pbk = psb[gi[0] % 2]
                    gi[0] += 1
                    for c in range(8):
                        cx.op("pe", lambda e, pbk=pbk, G=G, c=c: e.transpose(
                            out=pbk[:, c * 128:(c + 1) * 128], in_=G[:, c * 128:(c + 1) * 128], identity=ident_bf[:]),
                            reads=[G.b, ident_bf.b], writes=[pbk.b])
                    if blk % 2 == 0:
                        cx.op("act", lambda e, pbk=pbk, X=X, blk=blk: e.copy(
                            out=X[:, :, blk * 128:(blk + 1) * 128], in_=pbk[:].rearrange("p (c t) -> p c t", c=8)),
                            reads=[pbk.b], writes=[X.b])
                    else:
                        cx.op("dve", lambda e, pbk=pbk, X=X, blk=blk: e.tensor_copy(
                            out=X[:, :, blk * 128:(blk + 1) * 128], in_=pbk[:].rearrange("p (c t) -> p c t", c=8)),
                            reads=[pbk.b], writes=[X.b])
                a_ = ae[e_ % 2]
                for ft in range(2):
                    pg = getps(); pu = getps()
                    for c in range(8):
                        cx.op("pe", lambda e, pg=pg, c=c, ft=ft, X=X, p=p: e.matmul(
                            pg[:, 0:CAP], lhsT=Wg[p][:, c, ft * 128:(ft + 1) * 128], rhs=X[:, c, :],
                            start=(c == 0), stop=(c == 7)), reads=[Wg[p].b, X.b], writes=[pg.b])
                    for c in range(8):
                        cx.op("pe", lambda e, pu=pu, c=c, ft=ft, X=X, p=p: e.matmul(
                            pu[:, 0:CAP], lhsT=Wu[p][:, c, ft * 128:(ft + 1) * 128], rhs=X[:, c, :],
                            start=(c == 0), stop=(c == 7)), reads=[Wu[p].b, X.b], writes=[pu.b])
                    s = sgl[ft]
                    cx.op("act", lambda e, pg=pg, s=s: e.activation(out=s[:], in_=pg[:, 0:CAP], func=AF.Silu),
                          reads=[pg.b], full=[s.b])
                    cx.op("dve", lambda e, pu=pu, s=s, a_=a_, ft=ft: e.tensor_tensor(
                        out=a_[:, ft, :], in0=pu[:, 0:CAP], in1=s[:], op=ALU.mult),
                        reads=[pu.b, s.b], writes=[a_.b])
                for blk in range(NBLK):
                    eb = e_ * NBLK + blk
                    Y = Yt[eb % 3]
                    for half in range(2):
                        py = getps()
                        for ft in range(2):
                            cx.op("pe", lambda e, py=py, ft=ft, blk=blk, half=half, a_=a_, p=p: e.matmul(
                                py[:], lhsT=a_[:, ft, blk * 128:(blk + 1) * 128],
                                rhs=Wd[p][:, ft, half * 512:(half + 1) * 512], start=(ft == 0), stop=(ft == 1)),
                                reads=[a_.b, Wd[p].b], writes=[py.b])
                        if half == 0:
                            cx.op("dve", lambda e, py=py, Y=Y, eb=eb: e.tensor_scalar(
                                out=Y[:, 0:512], in0=py[:], scalar1=lst_sb[:, eb, 2:3], scalar2=None, op0=ALU.mult),
                                reads=[py.b, lst_sb.b], writes=[Y.b])
                        else:
                            cx.op("act", lambda e, py=py, Y=Y, eb=eb: e.activation(
                                out=Y[:, 512:1024], in_=py[:], func=AF.Copy, scale=lst_sb[:, eb, 2:3]),
                                reads=[py.b, lst_sb.b], writes=[Y.b])
                    cx.op("pool", lambda e, Y=Y, eb=eb: e.indirect_dma_start(
                        out=moe_scr, out_offset=bass.IndirectOffsetOnAxis(ap=dst_i[:, eb:eb + 1], axis=0),
                        in_=Y[:], in_offset=None), reads=[Y.b, dst_i.b, moeB], dma=True)
                if e_ + 1 < NEXP:
                    load_w_cast(e_ + 1)
            cx.barrier()
            alD.close()

            alE = Alloc(nc)
            gfin = alE.sb([128, D], F32, "gfin")
            cx.op("sp", lambda e: e.dma_start(out=gfin[:], in_=gfin_d.partition_broadcast(128)), full=[gfin.b], dma=True)
            NE_ = 4
            xa = [alE.sb([128, D], F32, f"xa{i}") for i in range(NE_)]
            m0 = [alE.sb([128, D], BF16, f"m0{i}") for i in range(NE_)]
            m1 = [alE.sb([128, D], BF16, f"m1{i}") for i in range(NE_)]
            ot = [alE.sb([128, D], F32, f"ot{i}") for i in range(NE_)]
            junk3 = alE.sb([128, D], BF16, "junk3")
            sse = [alE.sb([128, 1], F32, f"sse{i}") for i in range(NE_)]
            rte = [alE.sb([128, 1], F32, f"rte{i}") for i in range(NE_)]
            rse = [alE.sb([128, 1], F32, f"rse{i}") for i in range(NE_)]
            outB = Buf("out")

            def e_load(ti):
                p = ti % NE_
                rows = slice(ti * 128, (ti + 1) * 128)
                cx.op("sp", lambda e, p=p, rows=rows: e.dma_start(out=xa[p][:], in_=x2_scr[rows, :]),
                      full=[xa[p].b], dma=True)
                cx.op("sp", lambda e, p=p, rows=rows: e.dma_start(out=m0[p][:], in_=moe_scr[rows, :]),
                      full=[m0[p].b], dma=True)
                cx.op("sp", lambda e, p=p, ti=ti: e.dma_start(
                    out=m1[p][:], in_=moe_scr[ROWS + ti * 128:ROWS + (ti + 1) * 128, :]),
                    full=[m1[p].b], dma=True)

            for ti in range(min(NE_ - 1, NT)):
                e_load(ti)
            for ti in range(NT):
                p = ti % NE_
                rows = slice(ti * 128, (ti + 1) * 128)
                if ti + NE_ - 1 < NT:
                    e_load(ti + NE_ - 1)
                cx.op("pool", lambda e, p=p: e.tensor_tensor(out=xa[p][:], in0=xa[p][:], in1=m0[p][:], op=ALU.add),
                      reads=[m0[p].b], writes=[xa[p].b])
                cx.op("dve", lambda e, p=p: e.tensor_tensor(out=xa[p][:], in0=xa[p][:], in1=m1[p][:], op=ALU.add),
                      reads=[m1[p].b], writes=[xa[p].b])
                cx.op("act", lambda e, p=p: e.activation(out=junk3[:], in_=xa[p][:], func=AF.Square, accum_out=sse[p][:]),
                      reads=[xa[p].b], full=[junk3.b, sse[p].b])
                cx.op("act", lambda e, p=p: e.activation(out=rte[p][:], in_=sse[p][:], func=AF.Sqrt, scale=1.0 / D, bias=EPS),
                      reads=[sse[p].b], full=[rte[p].b])
                cx.op("dve", lambda e, p=p: e.reciprocal(out=rse[p][:], in_=rte[p][:]), reads=[rte[p].b], full=[rse[p].b])
                cx.op("dve", lambda e, p=p: e.scalar_tensor_tensor(out=ot[p][:], in0=xa[p][:], scalar=rse[p][:, 0:1],
                                                                   in1=gfin[:], op0=ALU.mult, op1=ALU.mult),
                      reads=[xa[p].b, rse[p].b, gfin.b], full=[ot[p].b])
                cx.op("sp", lambda e, p=p, rows=rows: e.dma_start(out=out_d[rows, :], in_=ot[p][:]),
                      reads=[ot[p].b], writes=[outB], dma=True)
            cx.barrier()
            alE.close()
        cx.barrier()
        cx.emit(block)
        print("waits", cx.nwait, "instrs", {e: cx.cnt[e] for e in cx.ENG}, "signals", {e: len(cx.waited[e]) for e in cx.ENG})
    return nc


def host_consts():
    c = {}
    c["ident_bf"] = np.eye(128, dtype=np.float32).astype(ml_dtypes.bfloat16)
    c["ident_f"] = np.eye(128, dtype=np.float32)
    psel = np.zeros((128, 8, 240), np.float32)
    for a in range(8):
        for i in range(16):
            psel[a * 16 + i, a, 7 * 16 + i] = 1.0
    c["psel"] = psel.astype(ml_dtypes.bfloat16)
    kk = np.arange(128) // 16
    c["cmask"] = (kk[None, :] >= kk[:, None]).astype(np.float32)
    c["tri"] = (np.arange(128)[:, None] < np.arange(128)[None, :]).astype(np.float32)
    c["ecap"] = np.ascontiguousarray(np.broadcast_to((np.arange(32) * NBLK).astype(np.float32)[None, :], (128, 32)))
    c["tokid"] = (np.arange(NT)[None, :] * 128 + np.arange(128)[:, None]).astype(np.float32)
    li = np.zeros((NEXP * CAP + 128, 4), np.float32)
    li[:, 0] = SEQ + ((np.arange(NEXP * CAP + 128) // (NEXP * NBLK)) % 128)
    li[:, 1] = li[:, 0]
    c["trashp"] = (NEXP * CAP + np.arange(128)).astype(np.float32).reshape(128, 1)
    c["lst_init"] = li
    return c


def relayout_kn(w):
    K, N = w.shape
    return np.ascontiguousarray(w.reshape(K // 128, 128, N).transpose(1, 0, 2))


def relayout_pc(w):
    E, K, N = w.shape
    return np.ascontiguousarray(w.reshape(E, K // 128, 128, N).transpose(0, 2, 1, 3))


def pair_layout(a):
    rest = a.shape[2:]
    a = a.reshape((16, 2, 64) + rest)
    a = np.moveaxis(a, 0, 2)
    return np.ascontiguousarray(a.reshape((128, 16) + rest))


def make_inmap(inputs, b, consts=None):
    f = lambda a: np.ascontiguousarray(a, dtype=np.float32)
    m = {"x": f(inputs["x"][b]),
         "g_mix": f(inputs["g_mix"]),
         "w_in": relayout_kn(f(inputs["w_in"][0]))}
    m["lamre_l"] = pair_layout(f(inputs["ssm_lambda_re"][0]))
    m["lamim_l"] = pair_layout(f(inputs["ssm_lambda_im"][0]))
    m["logdt_l"] = pair_layout(np.broadcast_to(f(inputs["ssm_log_dt"][0])[:, None], (32, 64)))
    m["bre_l"] = pair_layout(f(inputs["ssm_b_re"][0]))
    m["bim_l"] = pair_layout(f(inputs["ssm_b_im"][0]))
    m["cre_l"] = pair_layout(f(inputs["ssm_c_re"][0]).transpose(0, 2, 1))
    m["cim_l"] = pair_layout(f(inputs["ssm_c_im"][0]).transpose(0, 2, 1))
    m["d_l"] = np.ascontiguousarray(np.tile(f(inputs["ssm_d"][0]).reshape(32, 16).T, (8, 1)))
    m["mem"] = f(inputs["mem"][b])
    for k_, n_ in (("g_mem", "g_mem"), ("g_ffn", "g_ffn")):
        m[n_] = f(inputs[k_])
    m["g_final"] = f(inputs["g_final"]).reshape(1, D)
    m["w_mem_kv"] = relayout_kn(f(inputs["w_mem_kv"][0])); m["w_mem_out"] = relayout_kn(f(inputs["w_mem_out"][0]))
    m["w_conv_out"] = relayout_kn(f(inputs["w_conv_out"][0])); m["w_ssm_glu"] = relayout_kn(f(inputs["w_ssm_glu"][0]))
    m["w_out"] = relayout_kn(f(inputs["w_out"][0]))
    m["w_router"] = np.ascontiguousarray(np.concatenate([f(inputs["w_router_group"][0]),
                                                         f(inputs["w_router_expert"][0])], axis=1))
    m["b_router"] = np.ascontiguousarray(np.concatenate([f(inputs["b_router_group"][0]),
                                                         f(inputs["b_router_expert"][0])])[None, :])
    m["cdw_l"] = np.ascontiguousarray(f(inputs["conv_dw"][0]).T.reshape(4, 128, 31).transpose(1, 0, 2))
    m["cb_l"] = np.ascontiguousarray(f(inputs["conv_dw_bias"][0]).reshape(4, 128).T)
    m["lng_l"] = np.ascontiguousarray(f(inputs["conv_ln_g"][0]).reshape(4, 128).T)
    m["lnb_l"] = np.ascontiguousarray(f(inputs["conv_ln_b"][0]).reshape(4, 128).T)
    if consts is not None and "w_exp_gate" in consts:
        for k_ in ("w_exp_gate", "w_exp_up", "w_exp_down"):
            m[k_] = consts[k_]
    else:
        m["w_exp_gate"] = relayout_pc(f(inputs["w_exp_gate"][0]))
        m["w_exp_up"] = relayout_pc(f(inputs["w_exp_up"][0]))
        m["w_exp_down"] = relayout_pc(f(inputs["w_exp_down"][0]))
    m.update(consts if consts is not None else host_consts())
    return m


def kernel(**inputs):
    nc = build()
    consts = host_consts()
    f32 = lambda a: np.ascontiguousarray(a, dtype=np.float32)
    for k_ in ("w_exp_gate", "w_exp_up", "w_exp_down"):
        consts[k_] = relayout_pc(f32(inputs[k_][0]))
    in_maps = [make_inmap(inputs, b, consts) for b in range(NCORES)]
    res = run_bass_kernel_spmd(nc, in_maps, core_ids=list(range(NCORES)))
    return np.stack([r["out"] for r in res.results], axis=0)
```

```python
import os
import numpy as np
import ml_dtypes
from contextlib import ExitStack
import concourse.bass as bass
import concourse.mybir as mybir
from concourse.bass_utils import run_bass_kernel_spmd

F32 = mybir.dt.float32
BF16 = mybir.dt.bfloat16
I32 = mybir.dt.int32
U32 = mybir.dt.uint32
AF = mybir.ActivationFunctionType
ALU = mybir.AluOpType
AX = mybir.AxisListType
GELU = AF.Gelu_apprx_tanh

D = 1024
SEQ = 4096
NCORES = 8
T = 512
NB = SEQ // T
NT = SEQ // 128
EPS = 1e-6
NEXP = 32
CAP = 384
NBLK = CAP // 128
ROWS = SEQ + 128


class Buf:
    __slots__ = ("name", "w", "r")

    def __init__(self, name):
        self.name = name
        self.w = {}
        self.r = {}


class Ctx:
    ENG = ("pe", "dve", "act", "pool", "sp")
    KROT = 4
    NDMA = 12

    def __init__(self, nc, es):
        self.nc = nc
        self.q = {e: [] for e in self.ENG}
        self.cnt = {e: 0 for e in self.ENG}
        self.seen = {e: {} for e in self.ENG}
        self.esem = {e: [es.enter_context(nc.semaphore(f"s_{e}{i}")) for i in range(self.KROT)]
                     for e in self.ENG}
        self.dsem = {e: [es.enter_context(nc.semaphore(f"d_{e}{i}")) for i in range(self.NDMA)]
                     for e in ("sp", "act", "pool")}
        self.dcnt = {e: [0] * self.NDMA for e in self.dsem}
        self.dnext = {e: 0 for e in self.dsem}
        self.nwait = 0
        self.waited = {e: set() for e in self.ENG}

    def _wait(self, eng, tok):
        key, val = tok
        if key[0] == 'e' and key[1] == eng and eng == "pe":
            return
        if self.seen[eng].get(key, -1) >= val:
            return
        self.seen[eng][key] = val
        if key[0] == 'e':
            self.waited[key[1]].add(val)
        self.q[eng].append(("w", key, val))
        self.nwait += 1

    def op(self, eng, fn, reads=(), writes=(), full=(), dma=False):
        toks = []
        for b in reads:
            toks.extend(b.w.items())
        for b in tuple(writes) + tuple(full):
            toks.extend(b.w.items())
            toks.extend(b.r.items())
        for t in toks:
            self._wait(eng, t)
        if dma:
            i = self.dnext[eng]
            self.dnext[eng] = (i + 1) % self.NDMA
            key = ('d', eng, i)
            if self.dcnt[eng][i] > 0:
                self._wait(eng, (key, self.dcnt[eng][i]))
            self.dcnt[eng][i] += 16
            val = self.dcnt[eng][i]
            self.q[eng].append(("d", fn, self.dsem[eng][i]))
        else:
            key = ('e', eng)
            val = self.cnt[eng]
            self.cnt[eng] += 1
            self.q[eng].append(("i", fn, val))
        for b in reads:
            b.r[key] = val
        for b in full:
            b.w = {key: val}
            b.r = {}
        for b in writes:
            b.w[key] = val
        return (key, val)

    def barrier(self, skip_pool_dma=False):
        toks = []
        for e in self.ENG:
            if skip_pool_dma and e == "pool":
                continue
            if self.cnt[e] > 0:
                toks.append((('e', e), self.cnt[e] - 1))
        for e in self.dsem:
            if skip_pool_dma and e == "pool":
                continue
            for i in range(self.NDMA):
                if self.dcnt[e][i] > 0:
                    toks.append((('d', e, i), self.dcnt[e][i]))
        for e in self.ENG:
            for t in toks:
                self._wait(e, t)

    def emit(self, block):
        nc = self.nc

        rank = {e: {v: i for i, v in enumerate(sorted(self.waited[e]))} for e in self.ENG}
        K_ = self.KROT

        def run(engname, engine):
            for item in self.q[engname]:
                if item[0] == "w":
                    key, val = item[1], item[2]
                    if key[0] == 'e':
                        r = rank[key[1]][val]
                        engine.wait_ge(self.esem[key[1]][r % K_], r // K_ + 1)
                    else:
                        engine.wait_ge(self.dsem[key[1]][key[2]], val)
                elif item[0] == "d":
                    item[1](engine).then_inc(item[2], 16)
                else:
                    ins = item[1](engine)
                    r = rank[engname].get(item[2])
                    if r is not None:
                        ins.then_inc(self.esem[engname][r % K_], 1)

        @block.tensor
        def _(e):
            run("pe", e)

        @block.vector
        def _(e):
            run("dve", e)

        @block.scalar
        def _(e):
            run("act", e)

        @block.gpsimd
        def _(e):
            run("pool", e)

        @block.sync
        def _(e):
            run("sp", e)


class TT:
    def __init__(self, t, name):
        self.t = t
        self.b = Buf(name)

    def __getitem__(self, k):
        return self.t[k]


class Alloc:
    cnt = [0]

    def __init__(self, nc, es=None):
        self.nc = nc
        self.es = es if es is not None else ExitStack()

    @property
    def n(self):
        return Alloc.cnt[0]

    @n.setter
    def n(self, v):
        Alloc.cnt[0] = v

    def close(self):
        self.es.close()

    def sb(self, shape, dt, name=None):
        self.n += 1
        name = name or f"sb{self.n}"
        t = self.es.enter_context(self.nc.sbuf_tensor(f"{name}_{self.n}", list(shape), dt))
        return TT(t, name)

    def ps(self, shape, dt, name=None):
        self.n += 1
        name = name or f"ps{self.n}"
        t = self.es.enter_context(self.nc.psum_tensor(f"{name}_{self.n}", list(shape), dt))
        return TT(t, name)


def build(stop_after="E", dbg=False):
    nc = bass.Bass("TRN2", target_bir_lowering=False)
    dram = {}

    def din(name, shape, dt=F32):
        dram[name] = nc.dram_tensor(name, list(shape), dt, kind="ExternalInput").ap()
        return dram[name]

    def dscr(name, shape, dt, kind="Internal"):
        dram[name] = nc.dram_tensor(name, list(shape), dt, kind=kind).ap()
        return dram[name]

    x_d = din("x", [SEQ, D])
    gmix_d = din("g_mix", [1, D])
    w_in_d = din("w_in", [128, 8, 5120])
    ident_bf_d = din("ident_bf", [128, 128], BF16)
    ident_f_d = din("ident_f", [128, 128], F32)
    lamre_d = din("lamre_l", [128, 16])
    lamim_d = din("lamim_l", [128, 16])
    logdt_d = din("logdt_l", [128, 16])
    bre_d = din("bre_l", [128, 16, 16])
    bim_d = din("bim_l", [128, 16, 16])
    cre_d = din("cre_l", [128, 16, 16])
    cim_d = din("cim_l", [128, 16, 16])
    dl_d = din("d_l", [128, 32])
    psel_d = din("psel", [128, 8, 240], BF16)
    cmask_d = din("cmask", [128, 128])
    mem_d = din("mem", [256, D])
    gmem_d = din("g_mem", [1, D])
    gffn_d = din("g_ffn", [1, D])
    gfin_d = din("g_final", [1, D])
    wkv_d = din("w_mem_kv", [128, 8, 1024])
    wmo_d = din("w_mem_out", [128, 4, D])
    wco_d = din("w_conv_out", [128, 4, D])
    wgl_d = din("w_ssm_glu", [128, 4, 2048])
    wo_d = din("w_out", [128, 8, D])
    wr_d = din("w_router", [D, 36])
    rbias_d = din("b_router", [1, 36])
    cdw_d = din("cdw_l", [128, 4, 31])
    cb_d = din("cb_l", [128, 4])
    lng_d = din("lng_l", [128, 4])
    lnb_d = din("lnb_l", [128, 4])
    tri_d = din("tri", [128, 128])
    ecap_d = din("ecap", [128, 32])
    tokid_d = din("tokid", [128, NT])
    lst_init_d = din("lst_init", [NEXP * CAP + 128, 4])
    trashp_d = din("trashp", [128, 1])
    weg_d = din("w_exp_gate", [NEXP, 128, 8, 256])
    weu_d = din("w_exp_up", [NEXP, 128, 8, 256])
    wed_d = din("w_exp_down", [NEXP, 128, 2, D])
    dk = "ExternalOutput" if dbg else "Internal"
    wbf_scr = dscr("wbf_scr", [NEXP, 3, 128, 2048], BF16)
    lst_d = dscr("lst", [NEXP * CAP + 128, 4], F32, kind=dk)
    h2_scr = dscr("h2_scr", [ROWS, D], BF16, kind=dk)
    moe_scr = dscr("moe_scr", [2 * ROWS, D], BF16, kind=dk)
    x2_scr = dscr("x2_scr", [SEQ, D], F32, kind=dk)
    ys_scr = dscr("ys_scr", [4, 128, SEQ], BF16, kind="ExternalOutput" if dbg else "Internal")
    out_d = dscr("out", [SEQ, D], F32, kind="ExternalOutput")
    hT_scr = dscr("hT_scr", [8, 128, SEQ], BF16, kind="ExternalOutput" if dbg else "Internal")
    u_dbg = dscr("u_dbg", [4, 128, SEQ], BF16, kind="ExternalOutput") if dbg else None

    with ExitStack() as es:
        cx = Ctx(nc, es)
        al = Alloc(nc, es)
        block = es.enter_context(nc.Block())

        ident_bf = al.sb([128, 128], BF16, "ident_bf")
        cx.op("sp", lambda e: e.dma_start(out=ident_bf[:], in_=ident_bf_d), full=[ident_bf.b], dma=True)
        ident_f = al.sb([128, 128], F32, "ident_f")
        cx.op("sp", lambda e: e.dma_start(out=ident_f[:], in_=ident_f_d), full=[ident_f.b], dma=True)

        psum = [al.ps([128, 512], F32, f"bank{i}") for i in range(6)]
        psb = [al.ps([128, 1024], BF16, f"bankb{i}") for i in range(2)]
        pctr = [0]

        def getps():
            p = psum[pctr[0] % len(psum)]
            pctr[0] += 1
            return p

        alAB = Alloc(nc)
        u_all = alAB.sb([128, 4, 8, SEQ // 8], BF16, "u_all")
        M_all = alAB.sb([128, 32, 128], BF16, "M_all")
        W2r = alAB.sb([128, 16, 2, 128], BF16, "W2r"); W2i = alAB.sb([128, 16, 2, 128], BF16, "W2i")
        C1r = alAB.sb([128, 16, 128], BF16, "C1r"); nC1i = alAB.sb([128, 16, 128], BF16, "nC1i")
        KAr = alAB.sb([128, 9, 16], F32, "KAr"); KAi = alAB.sb([128, 9, 16], F32, "KAi")
        KnAi = alAB.sb([128, 9, 16], F32, "KnAi")
        psel = alAB.sb([128, 8, 240], BF16, "psel")
        zt = alAB.sb([128, 1024], F32, "zt")
        NPB = 3
        pst = [alAB.sb([128, 2048], F32, f"pst{i}") for i in range(NPB)]
        pbf = [alAB.sb([128, 2048], BF16, f"pbf{i}") for i in range(NPB)]
        wbfB = Buf("wbf")
        pc_next = [0]

        def precast(n, mode):
            for _ in range(n):
                ci = pc_next[0]
                if ci >= NEXP * 3:
                    return
                pc_next[0] += 1
                e_, m_ = ci // 3, ci % 3
                srcw = (weg_d, weu_d, wed_d)[m_][e_].rearrange("p c n -> p (c n)")
                s_ = pst[ci % NPB]; b_ = pbf[ci % NPB]
                dst = wbf_scr[e_, m_]
                if mode == "pool":
                    cx.op("pool", lambda e, s_=s_, srcw=srcw: e.dma_start(out=s_[:], in_=srcw), full=[s_.b], dma=True)
                    cx.op("pool", lambda e, s_=s_, b_=b_: e.tensor_copy(out=b_[:], in_=s_[:]), reads=[s_.b], full=[b_.b])
                    cx.op("pool", lambda e, b_=b_, dst=dst: e.dma_start(out=dst, in_=b_[:]), reads=[b_.b], dma=True)
                else:
                    cx.op("sp", lambda e, s_=s_, srcw=srcw: e.dma_start(out=s_[:], in_=srcw), full=[s_.b], dma=True)
                    cx.op("act", lambda e, s_=s_, b_=b_: e.copy(out=b_[:], in_=s_[:]), reads=[s_.b], full=[b_.b])
                    cx.op("act", lambda e, b_=b_, dst=dst: e.dma_start(out=dst, in_=b_[:]), reads=[b_.b], dma=True)
        cx.op("sp", lambda e: e.dma_start(out=psel[:], in_=psel_d), full=[psel.b], dma=True)
        al_outer = al
        al = Alloc(nc)
        gmix = al.sb([128, D], F32, "gmix")
        cx.op("sp", lambda e: e.dma_start(out=gmix[:], in_=gmix_d.partition_broadcast(128)),
              full=[gmix.b], dma=True)

        stg = [al.sb([128, 8, 256], F32, f"stg{i}") for i in range(2)]
        sctr = [0]

        def load_cast(dst, dst_col0, src_d, c0, c1, kch):
            for c in range(kch):
                for n0 in range(c0, c1, 2048):
                    w = min(2048, c1 - n0)
                    s = stg[sctr[0] % 2]
                    sctr[0] += 1
                    sf_ = s[:].rearrange("p c n -> p (c n)")
                    cx.op("sp", lambda e, sf_=sf_, c=c, n0=n0, w=w: e.dma_start(out=sf_[:, 0:w], in_=src_d[:, c, n0:n0 + w]),
                          full=[s.b], dma=True)
                    o = dst_col0 + (n0 - c0)
                    cx.op("pool", lambda e, sf_=sf_, c=c, o=o, w=w: e.tensor_copy(out=dst[:, c, o:o + w], in_=sf_[:, 0:w]),
                          reads=[s.b], writes=[dst.b])

        w_ssm_in = al.sb([128, 8, 512], BF16, "w_ssm_in")
        load_cast(w_ssm_in, 0, w_in_d, 1024, 1536, 8)
        NA_ = 4
        xt = [al.sb([128, D], F32, f"xt{i}") for i in range(NA_)]
        junk = al.sb([128, D], BF16, "junk")
        ss = [al.sb([128, 1], F32, f"ss{i}") for i in range(NA_)]
        rt = [al.sb([128, 1], F32, f"rt{i}") for i in range(NA_)]
        rstd = [al.sb([128, 1], F32, f"rstd{i}") for i in range(NA_)]
        hbf = [al.sb([128, D], BF16, f"hbf{i}") for i in range(NA_)]
        hTb = [al.sb([128, 8, T], BF16, f"hTb{i}") for i in range(2)]
        cx.op("pool", lambda e: e.memset(zt[:], 0.0), full=[zt.b])
        lstB = Buf("lst"); h2B = Buf("h2scr"); moeB = Buf("moescr"); x2B = Buf("x2scr")
        cx.op("pool", lambda e: e.dma_start(out=lst_d, in_=lst_init_d), full=[lstB], dma=True)
        cx.op("pool", lambda e: e.dma_start(out=h2_scr[SEQ:ROWS, :], in_=zt[:, 0:512].bitcast(BF16)),
              reads=[zt.b], writes=[h2B], dma=True)
        moe_flat = moe_scr.rearrange("(n p) d -> n p d", p=128)
        for n in range(0, 2 * ROWS // 128):
            tok = cx.op("pool", lambda e, n=n: e.dma_start(out=moe_flat[n], in_=zt[:, 0:512].bitcast(BF16)),
                        reads=[zt.b], dma=True)
            moeB.w[tok[0]] = tok[1]

        def a_front(i):
            p = i % NA_
            cx.op("sp", lambda e, p=p, i=i: e.dma_start(out=xt[p][:], in_=x_d[i * 128:(i + 1) * 128, :]),
                  full=[xt[p].b], dma=True)
            cx.op("act", lambda e, p=p: e.activation(out=junk[:], in_=xt[p][:], func=AF.Square,
                                                     accum_out=ss[p][:]),
                  reads=[xt[p].b], writes=[junk.b], full=[ss[p].b])
            cx.op("act", lambda e, p=p: e.activation(out=rt[p][:], in_=ss[p][:], func=AF.Sqrt,
                                                     scale=1.0 / D, bias=EPS),
                  reads=[ss[p].b], full=[rt[p].b])
            cx.op("dve", lambda e, p=p: e.reciprocal(out=rstd[p][:], in_=rt[p][:]),
                  reads=[rt[p].b], full=[rstd[p].b])
            cx.op("dve", lambda e, p=p: e.scalar_tensor_tensor(out=hbf[p][:], in0=xt[p][:],
                                                               scalar=rstd[p][:, 0:1], in1=gmix[:],
                                                               op0=ALU.mult, op1=ALU.mult),
                  reads=[xt[p].b, rstd[p].b, gmix.b], full=[hbf[p].b])

        def a_back(i):
            p = i % NA_
            blk = i // 4
            hb = hTb[blk % 2]
            pb = psb[i % 2]
            for c in range(8):
                cx.op("pe", lambda e, pb=pb, p=p, c=c: e.transpose(out=pb[:, c * 128:(c + 1) * 128],
                                                                   in_=hbf[p][:, c * 128:(c + 1) * 128],
                                                                   identity=ident_bf[:]),
                      reads=[hbf[p].b, ident_bf.b], writes=[pb.b])
            tt = i % 4
            cx.op("act", lambda e, pb=pb, hb=hb, tt=tt: e.copy(
                out=hb[:, :, tt * 128:(tt + 1) * 128],
                in_=pb[:].rearrange("p (c t) -> p c t", c=8)),
                reads=[pb.b], writes=[hb.b])
            if tt == 3:
                for f in range(4):
                    ps = getps()
                    for c in range(8):
                        cx.op("pe", lambda e, ps=ps, hb=hb, f=f, c=c: e.matmul(
                            ps[:], lhsT=w_ssm_in[:, c, f * 128:(f + 1) * 128], rhs=hb[:, c, :],
                            start=(c == 0), stop=(c == 7)),
                            reads=[w_ssm_in.b, hb.b], writes=[ps.b])
                    cx.op("dve", lambda e, ps=ps, f=f, blk=blk: e.tensor_copy(
                        out=u_all[:, f, :, blk * (T // 8):(blk + 1) * (T // 8)],
                        in_=ps[:].rearrange("p (c k) -> p k c", k=8)),
                        reads=[ps.b], writes=[u_all.b])
                cx.op("act", lambda e, hb=hb, blk=blk: e.dma_start(
                    out=hT_scr[:, :, blk * T:(blk + 1) * T].rearrange("c p t -> p c t"), in_=hb[:]),
                    reads=[hb.b], dma=True)

        a_front(0); a_front(1)
        for i in range(NT):
            if i + 2 < NT:
                a_front(i + 2)
            a_back(i)
            if i % 3 == 2:
                precast(1, "pool")

        if dbg:
            cx.op("sp", lambda e: e.dma_start(out=u_dbg.rearrange("f p t -> p f t"), in_=u_all[:].rearrange("p f k c -> p f (k c)")),
                  reads=[u_all.b], dma=True)


        cx.barrier(skip_pool_dma=True)
        al.close()
        precast(8, "pool")
        al = Alloc(nc)
        TWO_PI = 2.0 * np.pi
        cmask = al.sb([128, 128], F32, "cmask")
        cx.op("sp", lambda e: e.dma_start(out=cmask[:], in_=cmask_d), full=[cmask.b], dma=True)
        dl = al.sb([128, 32], F32, "dl")
        cx.op("sp", lambda e: e.dma_start(out=dl[:], in_=dl_d), full=[dl.b], dma=True)
        SU = Buf("ssm_setup")

        def sload(shape, src, name):
            t = al.sb(shape, F32, name)
            cx.op("sp", lambda e: e.dma_start(out=t[:], in_=src), full=[t.b], dma=True)
            return t

        lamre = sload([128, 16], lamre_d, "lamre")
        lamim = sload([128, 16], lamim_d, "lamim")
        logdt = sload([128, 16], logdt_d, "logdt")
        Bre = sload([128, 16, 16], bre_d, "Bre")
        Bim = sload([128, 16, 16], bim_d, "Bim")
        Cre = sload([128, 16, 16], cre_d, "Cre")
        Cim = sload([128, 16, 16], cim_d, "Cim")
        ins_b = [lamre.b, lamim.b, logdt.b, Bre.b, Bim.b, Cre.b, Cim.b]

        def S(shape, name):
            return al.sb(shape, F32, name)

        def dv(fn):
            cx.op("dve", fn, reads=ins_b, writes=[SU])

        def ac(fn):
            cx.op("act", fn, reads=ins_b, writes=[SU])

        def tt_(out, a, b, op):
            dv(lambda e: e.tensor_tensor(out=out, in0=a, in1=b, op=op))

        sh16 = [128, 16]
        dt_ = S(sh16, "dt"); lrd = S(sh16, "lrd"); th = S(sh16, "th")
        ac(lambda e: e.activation(out=dt_[:], in_=logdt[:], func=AF.Exp))
        tt_(lrd[:], lamre[:], dt_[:], ALU.mult)
        tt_(th[:], lamim[:], dt_[:], ALU.mult)
        mag = S(sh16, "mag"); imag2 = S(sh16, "imag2")
        ac(lambda e: e.activation(out=mag[:], in_=lrd[:], func=AF.Exp))
        ac(lambda e: e.activation(out=imag2[:], in_=lrd[:], func=AF.Exp, scale=-2.0))
        kq_i = al.sb(sh16, I32, "kq_i"); kq = S(sh16, "kq"); red = S(sh16, "red"); msk = S(sh16, "msk")
        sinv = S(sh16, "sinv"); cosv = S(sh16, "cosv"); tmpa = S(sh16, "tmpa")

        def sin_of(outt, shift):
            dv(lambda e: e.tensor_scalar(out=tmpa[:], in0=th[:], scalar1=float(shift), scalar2=None,
                                         op0=ALU.add))
            dv(lambda e: e.tensor_scalar(out=kq[:], in0=tmpa[:], scalar1=float(1.0 / TWO_PI),
                                         scalar2=None, op0=ALU.mult))
            dv(lambda e: e.tensor_copy(out=kq_i[:], in_=kq[:]))
            dv(lambda e: e.tensor_copy(out=kq[:], in_=kq_i[:]))
            dv(lambda e: e.scalar_tensor_tensor(out=red[:], in0=kq[:], scalar=float(-TWO_PI),
                                                in1=tmpa[:], op0=ALU.mult, op1=ALU.add))
            dv(lambda e: e.tensor_single_scalar(out=msk[:], in_=red[:], scalar=float(np.pi), op=ALU.is_gt))
            dv(lambda e: e.scalar_tensor_tensor(out=red[:], in0=msk[:], scalar=float(-TWO_PI),
                                                in1=red[:], op0=ALU.mult, op1=ALU.add))
            dv(lambda e: e.tensor_single_scalar(out=msk[:], in_=red[:], scalar=float(-np.pi), op=ALU.is_lt))
            dv(lambda e: e.scalar_tensor_tensor(out=red[:], in0=msk[:], scalar=float(TWO_PI),
                                                in1=red[:], op0=ALU.mult, op1=ALU.add))
            ac(lambda e: e.activation(out=outt[:], in_=red[:], func=AF.Sin))

        sin_of(sinv, 0.0)
        sin_of(cosv, np.pi / 2)
        PWr = S([128, 9, 16], "PWr"); PWi = S([128, 9, 16], "PWi")
        IPr = S([128, 8, 16], "IPr"); IPi = S([128, 8, 16], "IPi")
        t1 = S([128, 16, 8, 16], "t1"); t2 = S([128, 16, 8, 16], "t2")

        def cmul(outr, outi, ar, ai, br, bi, shp, neg_i=False):
            a1 = t1[:].rearrange("p a b c -> p (a b c)")[:, 0:int(np.prod(shp[1:]))]
            a2 = t2[:].rearrange("p a b c -> p (a b c)")[:, 0:int(np.prod(shp[1:]))]
            if len(shp) == 3:
                a1 = a1.rearrange("p (a b) -> p a b", a=shp[1])
                a2 = a2.rearrange("p (a b) -> p a b", a=shp[1])
            if len(shp) == 4:
                a1 = t1[:, :, 0:shp[2], :]
                a2 = t2[:, :, 0:shp[2], :]
            tt_(a1, ar, br, ALU.mult)
            tt_(a2, ai, bi, ALU.mult)
            tt_(outr, a1, a2, ALU.subtract)
            tt_(a1, ar, bi, ALU.mult)
            tt_(a2, ai, br, ALU.mult)
            if neg_i:
                dv(lambda e: e.scalar_tensor_tensor(out=outi, in0=a1, scalar=-1.0, in1=a2,
                                                    op0=ALU.mult, op1=ALU.subtract))
            else:
                tt_(outi, a1, a2, ALU.add)

        dv(lambda e: e.memset(PWr[:, 0, :], 1.0))
        dv(lambda e: e.memset(PWi[:, 0, :], 0.0))
        dv(lambda e: e.memset(IPr[:, 0, :], 1.0))
        dv(lambda e: e.memset(IPi[:, 0, :], 0.0))
        tt_(PWr[:, 1, :], mag[:], cosv[:], ALU.mult)
        tt_(PWi[:, 1, :], mag[:], sinv[:], ALU.mult)
        tt_(IPr[:, 1, :], PWr[:, 1, :], imag2[:], ALU.mult)
        dv(lambda e: e.scalar_tensor_tensor(out=IPi[:, 1, :], in0=PWi[:, 1, :], scalar=-1.0, in1=imag2[:],
                                            op0=ALU.mult, op1=ALU.mult))
        for n in range(2, 9):
            cmul(PWr[:, n, :], PWi[:, n, :], PWr[:, n - 1, :], PWi[:, n - 1, :], PWr[:, 1, :], PWi[:, 1, :], sh16)
        for n in range(2, 8):
            cmul(IPr[:, n, :], IPi[:, n, :], IPr[:, n - 1, :], IPi[:, n - 1, :], IPr[:, 1, :], IPi[:, 1, :], sh16)
        dv(lambda e: e.tensor_copy(out=KAr[:, 0, :], in_=PWr[:, 8, :]))
        dv(lambda e: e.tensor_copy(out=KAi[:, 0, :], in_=PWi[:, 8, :]))
        for d_ in range(1, 9):
            cmul(KAr[:, d_, :], KAi[:, d_, :], KAr[:, d_ - 1, :], KAi[:, d_ - 1, :],
                 KAr[:, d_ - 1, :], KAi[:, d_ - 1, :], sh16)
        dv(lambda e: e.tensor_scalar(out=KnAi[:], in0=KAi[:], scalar1=-1.0, scalar2=None, op0=ALU.mult))
        am1 = S(sh16, "am1"); l2 = S(sh16, "l2"); il2 = S(sh16, "il2"); kr = S(sh16, "kr"); ki = S(sh16, "ki")
        dv(lambda e: e.tensor_scalar(out=am1[:], in0=PWr[:, 1, :], scalar1=-1.0, scalar2=None, op0=ALU.add))
        tt_(l2[:], lamre[:], lamre[:], ALU.mult)
        tt_(tmpa[:], lamim[:], lamim[:], ALU.mult)
        tt_(l2[:], l2[:], tmpa[:], ALU.add)
        dv(lambda e: e.reciprocal(out=il2[:], in_=l2[:]))
        tt_(kr[:], am1[:], lamre[:], ALU.mult)
        tt_(tmpa[:], PWi[:, 1, :], lamim[:], ALU.mult)
        tt_(kr[:], kr[:], tmpa[:], ALU.add)
        tt_(kr[:], kr[:], il2[:], ALU.mult)
        tt_(ki[:], PWi[:, 1, :], lamre[:], ALU.mult)
        tt_(tmpa[:], am1[:], lamim[:], ALU.mult)
        tt_(ki[:], ki[:], tmpa[:], ALU.subtract)
        tt_(ki[:], ki[:], il2[:], ALU.mult)
        sh3 = [128, 16, 16]

        def bc(a):
            return a.unsqueeze(2).to_broadcast(sh3)

        Bbr = S(sh3, "Bbr"); Bbi = S(sh3, "Bbi")
        cmul(Bbr[:], Bbi[:], bc(kr[:]), bc(ki[:]), Bre[:], Bim[:], sh3)
        Bhr = S([128, 16, 8, 16], "Bhr"); nBhi = S([128, 16, 8, 16], "nBhi"); Bhi = S([128, 16, 8, 16], "Bhi")
        Btr = S([128, 16, 8, 16], "Btr"); Bti = S([128, 16, 8, 16], "Bti")
        Chr = S([128, 16, 9, 16], "Chr"); Chi = S([128, 16, 9, 16], "Chi"); nChi = S([128, 16, 9, 16], "nChi")
        sh4 = [128, 16, 8, 16]

        def bk(a):
            return a.rearrange("p k r -> p r k").unsqueeze(3).to_broadcast(sh4)

        def bmid(a):
            return a.unsqueeze(2).to_broadcast(sh4)

        def b2(a):
            return a.unsqueeze(2).unsqueeze(3).to_broadcast(sh4)

        cmul(Bhr[:], Bhi[:], bk(IPr[:]), bk(IPi[:]), bmid(Bbr[:]), bmid(Bbi[:]), sh4)
        cmul(Btr[:], Bti[:], b2(PWr[:, 7, :]), b2(PWi[:, 7, :]), Bhr[:], Bhi[:], sh4)
        dv(lambda e: e.tensor_scalar(out=nBhi[:], in0=Bhi[:], scalar1=-1.0, scalar2=None, op0=ALU.mult))
        cmul(Chr[:, :, 0:8, :], Chi[:, :, 0:8, :], bk(PWr[:, 0:8, :]), bk(PWi[:, 0:8, :]), bmid(Cre[:]), bmid(Cim[:]), sh4)
        cmul(Chr[:, :, 8, :], Chi[:, :, 8, :], bc(PWr[:, 8, :]), bc(PWi[:, 8, :]), Cre[:], Cim[:], sh3)
        dv(lambda e: e.tensor_scalar(out=nChi[:], in0=Chi[:], scalar1=-1.0, scalar2=None, op0=ALU.mult))
        dv(lambda e: e.tensor_copy(out=C1r[:].rearrange("p r (j c) -> p r j c", j=8), in_=Chr[:, :, 1:9, :]))
        dv(lambda e: e.tensor_copy(out=nC1i[:].rearrange("p r (j c) -> p r j c", j=8), in_=nChi[:, :, 1:9, :]))
        mtmps = [S([128, 128], "mtmp0"), S([128, 128], "mtmp1")]
        cx.op("dve", lambda e: e.memset(W2r[:], 0.0), reads=ins_b, writes=[SU])
        cx.op("dve", lambda e: e.memset(W2i[:], 0.0), reads=ins_b, writes=[SU])
        for r in range(16):
            for two in range(2):
                g = 2 * r + two
                rng = slice(two * 64, (two + 1) * 64)
                ps = getps()
                cx.op("pe", lambda e, ps=ps, r=r, rng=rng: e.matmul(
                    ps[:, 0:128], lhsT=Bhr[rng, r, :, :].rearrange("p k c -> p (k c)"),
                    rhs=Chr[rng, r, 0:8, :].rearrange("p j c -> p (j c)"), start=True, stop=False),
                    reads=[SU], writes=[ps.b])
                cx.op("pe", lambda e, ps=ps, r=r, rng=rng: e.matmul(
                    ps[:, 0:128], lhsT=nBhi[rng, r, :, :].rearrange("p k c -> p (k c)"),
                    rhs=Chi[rng, r, 0:8, :].rearrange("p j c -> p (j c)"), start=False, stop=True),
                    reads=[SU], writes=[ps.b])
                mt_ = mtmps[g % 2]
                cx.op("dve", lambda e, ps=ps, mt_=mt_: e.tensor_tensor(out=mt_[:], in0=ps[:, 0:128], in1=cmask[:],
                                                                       op=ALU.mult),
                      reads=[ps.b, cmask.b], full=[mt_.b])
                cx.op("dve", lambda e, g=g, mt_=mt_: e.scalar_tensor_tensor(
                    out=M_all[:, g, :], in0=ident_f[:], scalar=dl[:, g:g + 1], in1=mt_[:],
                    op0=ALU.mult, op1=ALU.add),
                    reads=[ident_f.b, dl.b, mt_.b], writes=[M_all.b])
            for (Bt, W2) in ((Btr, W2r), (Bti, W2i)):
                ps = getps()
                cx.op("pe", lambda e, ps=ps, r=r, Bt=Bt: e.transpose(
                    out=ps[:, 0:128], in_=Bt[:, r, :, :].rearrange("p k c -> p (k c)"), identity=ident_f[:]),
                    reads=[SU, ident_f.b], writes=[ps.b])
                cx.op("act", lambda e, ps=ps, r=r, W2=W2: e.copy(out=W2[:, r, 0, 0:64], in_=ps[:, 0:64]),
                      reads=[ps.b], writes=[W2.b])
                cx.op("act", lambda e, ps=ps, r=r, W2=W2: e.copy(out=W2[:, r, 1, 64:128], in_=ps[:, 64:128]),
                      reads=[ps.b], writes=[W2.b])

        cx.barrier(skip_pool_dma=True)
        al.close()
        al = Alloc(nc)
        NCH = SEQ // 8
        Vg = [al.sb([128, NCH], BF16, f"Vg{i}") for i in range(8)]
        Sre = [[al.sb([128, NCH], F32, f"Sre{s}{i}") for i in range(2)] for s in range(2)]
        Sim = [[al.sb([128, NCH], F32, f"Sim{s}{i}") for i in range(2)] for s in range(2)]
        Sbr = [al.sb([128, NCH], BF16, f"Sbr{s}") for s in range(4)]
        Sbi = [al.sb([128, NCH], BF16, f"Sbi{s}") for s in range(4)]
        SreF = [Sre[0][0], Sre[0][1], Sre[1][0], Sre[1][1]]
        SimF = [Sim[0][0], Sim[0][1], Sim[1][0], Sim[1][1]]
        Gg = [al.sb([128, NCH], BF16, f"Gg{i}") for i in range(16)]
        ysf = [al.sb([128, SEQ], BF16, "ysf0")] * 2
        for s in range(4):
            cx.op("pool", lambda e, s=s: e.memset(Sbr[s][:, 0:1], 0.0), writes=[Sbr[s].b])
            cx.op("pool", lambda e, s=s: e.memset(Sbi[s][:, 0:1], 0.0), writes=[Sbi[s].b])

        def b_front(r):
            f = r // 4
            st = r % 4
            vg = [Vg[(2 * r) % 8], Vg[(2 * r + 1) % 8]]
            for two in range(2):
                g = 2 * r + two
                gl = g % 8
                ps = getps()
                for k in range(8):
                    cx.op("pe", lambda e, ps=ps, gl=gl, k=k, f=f: e.matmul(
                        ps[:], lhsT=psel[:, gl, (7 - k) * 16:(7 - k) * 16 + 128],
                        rhs=u_all[:, f, k, :], start=(k == 0), stop=(k == 7)),
                        reads=[psel.b, u_all.b], writes=[ps.b])
                cx.op("act", lambda e, ps=ps, v=vg[two]: e.copy(out=v[:], in_=ps[:]),
                      reads=[ps.b], full=[vg[two].b])
            psr = getps(); psi = getps()
            for (pp, W2) in ((psr, W2r), (psi, W2i)):
                for two in range(2):
                    cx.op("pe", lambda e, pp=pp, W2=W2, two=two, r=r, v=vg[two]: e.matmul(
                        pp[:], lhsT=W2[:, r, two, :], rhs=v[:], start=(two == 0), stop=(two == 1)),
                        reads=[W2.b, vg[two].b], writes=[pp.b])
            cx.op("act", lambda e, psr=psr, st=st: e.copy(out=SreF[st][:], in_=psr[:]),
                  reads=[psr.b], full=[SreF[st].b])
            cx.op("act", lambda e, psi=psi, st=st: e.copy(out=SimF[st][:], in_=psi[:]),
                  reads=[psi.b], full=[SimF[st].b])

        def b_mid2(rs):
            def level(r, t0_, s0_, step, cnt, d_):
                xr, xi = SreF[r % 4], SimF[r % 4]
                tr = xr[:, t0_:t0_ + (cnt - 1) * step + 1:step]
                ti_ = xi[:, t0_:t0_ + (cnt - 1) * step + 1:step]
                sr = xr[:, s0_:s0_ + (cnt - 1) * step + 1:step]
                si_ = xi[:, s0_:s0_ + (cnt - 1) * step + 1:step]
                return [
                    (lambda e: e.scalar_tensor_tensor(out=tr, in0=sr, scalar=KAr[:, d_, r:r + 1], in1=tr,
                                                      op0=ALU.mult, op1=ALU.add), [xr.b, SU], [xr.b]),
                    (lambda e: e.scalar_tensor_tensor(out=ti_, in0=si_, scalar=KAr[:, d_, r:r + 1], in1=ti_,
                                                      op0=ALU.mult, op1=ALU.add), [xi.b, SU], [xi.b]),
                    (lambda e: e.scalar_tensor_tensor(out=tr, in0=si_, scalar=KnAi[:, d_, r:r + 1], in1=tr,
                                                      op0=ALU.mult, op1=ALU.add), [xi.b, SU], [xr.b]),
                    (lambda e: e.scalar_tensor_tensor(out=ti_, in0=sr, scalar=KAi[:, d_, r:r + 1], in1=ti_,
                                                      op0=ALU.mult, op1=ALU.add), [xr.b, SU], [xi.b]),
                ]

            plan = []
            for d_ in range(9):
                half, step = 1 << d_, 2 << d_
                plan.append((step - 1, half - 1, step, NCH // step, d_))
            for d_ in range(7, -1, -1):
                half, step = 1 << d_, 2 << d_
                plan.append((step + half - 1, step - 1, step, NCH // step - 1, d_))
            for (t0_, s0_, step, cnt, d_) in plan:
                ops = [level(r, t0_, s0_, step, cnt, d_) for r in rs]
                for q in range(4):
                    for o in ops:
                        fn, rd_, wr_ = o[q]
                        cx.op("dve", fn, reads=rd_, writes=wr_)
            for r in rs:
                st = r % 4
                fr, fi = SreF[st], SimF[st]
                cx.op("pool", lambda e, fr=fr, st=st: e.tensor_copy(out=Sbr[st][:, 1:NCH], in_=fr[:, 0:NCH - 1]),
                      reads=[fr.b], writes=[Sbr[st].b])
                cx.op("pool", lambda e, fi=fi, st=st: e.tensor_copy(out=Sbi[st][:, 1:NCH], in_=fi[:, 0:NCH - 1]),
                      reads=[fi.b], writes=[Sbi[st].b])

        def b_back(r):
            f = r // 4
            st = r % 4
            vg = [Vg[(2 * r) % 8], Vg[(2 * r + 1) % 8]]
            for two in range(2):
                g = 2 * r + two
                rng = slice(two * 64, (two + 1) * 64)
                ps = getps()
                cx.op("pe", lambda e, ps=ps, g=g, v=vg[two]: e.matmul(
                    ps[:], lhsT=M_all[:, g, :], rhs=v[:], start=True, stop=False),
                    reads=[M_all.b, vg[two].b], writes=[ps.b])
                cx.op("pe", lambda e, ps=ps, r=r, rng=rng, st=st: e.matmul(
                    ps[:], lhsT=C1r[rng, r, :], rhs=Sbr[st][rng, :], start=False, stop=False),
                    reads=[SU, Sbr[st].b], writes=[ps.b])
                cx.op("pe", lambda e, ps=ps, r=r, rng=rng, st=st: e.matmul(
                    ps[:], lhsT=nC1i[rng, r, :], rhs=Sbi[st][rng, :], start=False, stop=True),
                    reads=[SU, Sbi[st].b], writes=[ps.b])
                gg = Gg[g % 16]
                cx.op("act", lambda e, ps=ps, gg=gg: e.activation(out=gg[:], in_=ps[:], func=GELU),
                      reads=[ps.b], full=[gg.b])
            if r % 4 == 3:
                yb = ysf[f % 2]
                for j in range(8):
                    ps = getps()
                    for gl in range(8):
                        gg = Gg[(8 * f + gl) % 16]
                        cx.op("pe", lambda e, ps=ps, j=j, gl=gl, gg=gg: e.matmul(
                            ps[:], lhsT=psel[:, j, (7 - gl) * 16:(7 - gl) * 16 + 128], rhs=gg[:],
                            start=(gl == 0), stop=(gl == 7)),
                            reads=[psel.b, gg.b], writes=[ps.b])
                    cx.op("act", lambda e, ps=ps, yb=yb, j=j: e.copy(out=yb[:, j:SEQ:8], in_=ps[:]),
                          reads=[ps.b], writes=[yb.b])
                cx.op("sp", lambda e, yb=yb, f=f: e.dma_start(out=ys_scr[f], in_=yb[:]),
                      reads=[yb.b], dma=True)

        b_front(0); b_front(1)
        for g2 in range(8):
            precast(4, "act")
            b_mid2([2 * g2, 2 * g2 + 1])
            precast(3, "act")
            if g2 + 1 < 8:
                b_front(2 * g2 + 2)
                b_front(2 * g2 + 3)
            precast(3, "act")
            b_back(2 * g2)
            b_back(2 * g2 + 1)
        precast(NEXP * 3, "act")

        cx.barrier()
        al.close()
        alAB.close()
        al = al_outer
        if stop_after in ("A", "B"):
            pass
        else:
            TC = 256
            NBC = SEQ // TC
            alC = Alloc(nc)
            wA = alC.sb([128, 8, 1536], BF16, "wA")
            wG = alC.sb([128, 8, 3072], BF16, "wG")
            wco = alC.sb([128, 4, 1024], BF16, "wco")
            wgl = alC.sb([128, 4, 2048], BF16, "wgl")
            wmo = alC.sb([128, 4, 1024], BF16, "wmo")
            wo = alC.sb([128, 8, 1024], BF16, "wo")
            Dg2 = [alC.sb([128, 31, 128], BF16, f"Dg{i}") for i in range(2)]
            kT = alC.sb([128, 4, 256], BF16, "kT")
            vtok = alC.sb([128, 2, 512], BF16, "vtok")
            gffn = alC.sb([128, D], F32, "gffn")
            wr = alC.sb([128, 8, 36], F32, "wr")
            rbias = alC.sb([128, 36], F32, "rbias")
            cdw = alC.sb([128, 4, 31], F32, "cdw")
            cb = alC.sb([128, 4], F32, "cb"); lng = alC.sb([128, 4], F32, "lng"); lnb = alC.sb([128, 4], F32, "lnb")
            onesm = alC.sb([128, 128], F32, "onesm")
            ones_bf = alC.sb([128, 128], BF16, "ones_bf")
            ecap = alC.sb([128, 32], F32, "ecap")
            tokid = alC.sb([128, NT], F32, "tokid")
            cum = alC.sb([128, 32], F32, "cum")
            lg_all = alC.sb([128, NT, 36], F32, "lg_all")
            trashp = alC.sb([128, 1], F32, "trashp")

            def ld(t, src):
                cx.op("sp", lambda e: e.dma_start(out=t[:], in_=src), full=[t.b], dma=True)

            ld(gffn, gffn_d.partition_broadcast(128))
            ld(wr, wr_d.rearrange("(c p) n -> p c n", p=128))
            ld(rbias, rbias_d.partition_broadcast(128))
            ld(cdw, cdw_d); ld(cb, cb_d); ld(lng, lng_d); ld(lnb, lnb_d)
            ld(ecap, ecap_d); ld(tokid, tokid_d); ld(trashp, trashp_d)
            cx.op("pool", lambda e: e.memset(onesm[:], 1.0 / 512.0), full=[onesm.b])
            cx.op("pool", lambda e: e.memset(ones_bf[:], 1.0), full=[ones_bf.b])
            cx.op("pool", lambda e: e.memset(cum[:], 0.0), full=[cum.b])
            alS = Alloc(nc)
            stg2 = [alS.sb([128, 8, 256], F32, f"stgc{i}") for i in range(3)]
            s2 = [0]

            def load_cast2(dst, dst_col0, src_d, c0, c1, kch, engs=("pool", "act", "dve")):
                for c in range(kch):
                    for n0 in range(c0, c1, 2048):
                        w = min(2048, c1 - n0)
                        s = stg2[s2[0] % len(stg2)]
                        eng = engs[s2[0] % len(engs)]
                        s2[0] += 1
                        sf_ = s[:].rearrange("p c n -> p (c n)")
                        cx.op("sp", lambda e, sf_=sf_, c=c, n0=n0, w=w: e.dma_start(out=sf_[:, 0:w], in_=src_d[:, c, n0:n0 + w]),
                              full=[s.b], dma=True)
                        o = dst_col0 + (n0 - c0)
                        if eng == "act":
                            cx.op("act", lambda e, sf_=sf_, c=c, o=o, w=w: e.copy(out=dst[:, c, o:o + w], in_=sf_[:, 0:w]),
                                  reads=[s.b], writes=[dst.b])
                        else:
                            cx.op(eng, lambda e, sf_=sf_, c=c, o=o, w=w: e.tensor_copy(out=dst[:, c, o:o + w], in_=sf_[:, 0:w]),
                                  reads=[s.b], writes=[dst.b])

            load_cast2(wA, 0, w_in_d, 0, 1024, 8)
            load_cast2(wA, 1024, w_in_d, 1536, 2048, 8)
            load_cast2(wG, 0, w_in_d, 2048, 5120, 8)
            load_cast2(wco, 0, wco_d, 0, 1024, 4)
            load_cast2(wgl, 0, wgl_d, 0, 2048, 4)
            load_cast2(wmo, 0, wmo_d, 0, 1024, 4)
            load_cast2(wo, 0, wo_d, 0, 1024, 8)
            wkv = alS.sb([128, 8, 1024], BF16, "wkv")
            load_cast2(wkv, 0, wkv_d, 0, 1024, 8)
            gmem = alS.sb([128, D], F32, "gmem")
            ld(gmem, gmem_d.partition_broadcast(128))
            memT = alS.sb([128, 8, 256], BF16, "memT")
            mx = alS.sb([128, D], F32, "mx")
            mss = alS.sb([128, 1], F32, "mss"); mrt = alS.sb([128, 1], F32, "mrt"); mrs = alS.sb([128, 1], F32, "mrs")
            mh = alS.sb([128, D], BF16, "mh")
            mjunk = mh
            for mt in range(2):
                cx.op("sp", lambda e, mt=mt: e.dma_start(out=mx[:], in_=mem_d[mt * 128:(mt + 1) * 128, :]),
                      full=[mx.b], dma=True)
                cx.op("act", lambda e: e.activation(out=mjunk[:], in_=mx[:], func=AF.Square, accum_out=mss[:]),
                      reads=[mx.b], writes=[mjunk.b], full=[mss.b])
                cx.op("act", lambda e: e.activation(out=mrt[:], in_=mss[:], func=AF.Sqrt, scale=1.0 / D, bias=EPS),
                      reads=[mss.b], full=[mrt.b])
                cx.op("dve", lambda e: e.reciprocal(out=mrs[:], in_=mrt[:]), reads=[mrt.b], full=[mrs.b])
                cx.op("dve", lambda e: e.scalar_tensor_tensor(out=mh[:], in0=mx[:], scalar=mrs[:, 0:1], in1=gmem[:],
                                                              op0=ALU.mult, op1=ALU.mult),
                      reads=[mx.b, mrs.b, gmem.b], full=[mh.b])
                pb = psb[mt % 2]
                for c in range(8):
                    cx.op("pe", lambda e, pb=pb, c=c: e.transpose(out=pb[:, c * 128:(c + 1) * 128],
                                                                  in_=mh[:, c * 128:(c + 1) * 128],
                                                                  identity=ident_bf[:]),
                          reads=[mh.b, ident_bf.b], writes=[pb.b])
                cx.op("act", lambda e, pb=pb, mt=mt: e.copy(out=memT[:, :, mt * 128:(mt + 1) * 128],
                                                            in_=pb[:].rearrange("p (c t) -> p c t", c=8)),
                      reads=[pb.b], writes=[memT.b])
            for hd in range(4):
                ps = getps()
                for c in range(8):
                    cx.op("pe", lambda e, ps=ps, c=c, hd=hd: e.matmul(
                        ps[:, 0:256], lhsT=wkv[:, c, hd * 128:(hd + 1) * 128], rhs=memT[:, c, :],
                        start=(c == 0), stop=(c == 7)), reads=[wkv.b, memT.b], writes=[ps.b])
                cx.op("dve", lambda e, ps=ps, hd=hd: e.tensor_copy(out=kT[:, hd, :], in_=ps[:, 0:256]),
                      reads=[ps.b], writes=[kT.b])
            for mc in range(2):
                ps = getps()
                for c in range(8):
                    cx.op("pe", lambda e, ps=ps, c=c, mc=mc: e.matmul(
                        ps[:], lhsT=memT[:, c, mc * 128:(mc + 1) * 128], rhs=wkv[:, c, 512:1024],
                        start=(c == 0), stop=(c == 7)), reads=[wkv.b, memT.b], writes=[ps.b])
                cx.op("dve", lambda e, ps=ps, mc=mc: e.tensor_copy(out=vtok[:, mc, :], in_=ps[:]),
                      reads=[ps.b], writes=[vtok.b])
            cx.barrier()
            alS.close()

            alW = Alloc(nc)
            hT = [alW.sb([128, 8, TC], BF16, f"hTc{i}") for i in range(2)]
            ysb = [alW.sb([128, 4, TC], BF16, "ysb0")] * 2
            vbuf = alW.sb([128, 4, 30 + TC], BF16, "vbuf")
            sgt = [alW.sb([128, TC], F32, f"sgt{i}") for i in range(3)]
            cv = alW.sb([128, 4, TC], F32, "cv")
            sq = [sgt[1], sgt[2]]
            mean = alW.sb([128, TC], F32, "mean")
            var = alW.sb([128, TC], F32, "var"); lrs = alW.sb([128, TC], F32, "lrs")
            m2 = var; lnv = lrs
            cn = alW.sb([128, 4, TC], BF16, "cn")
            qb = alW.sb([128, 4, TC], BF16, "qb")
            Eb = [alW.sb([128, 2, TC], BF16, f"Eb{i}") for i in range(2)]
            ob = alW.sb([128, 4, TC], BF16, "ob")
            macc = alW.sb([128, TC], F32, "macc"); mt1 = alW.sb([128, TC], F32, "mt1"); mt2 = alW.sb([128, TC], F32, "mt2")
            rden = macc
            xc = [mt1, mt2]
            sqf = [sgt[1], sgt[2], mt1, mt2]
            merged = alW.sb([128, 8, TC], BF16, "merged")
            xt2 = [alW.sb([128, D], F32, f"xtc{i}") for i in range(2)]
            x2t = xt2
            h2f = alW.sb([128, D], F32, "h2f"); h2b = [alW.sb([128, D], BF16, "h2b0")] * 2
            junk2 = h2b[0]
            h2T = alW.sb([128, 8, 128], F32, "h2T")
            ss2 = alW.sb([128, 1], F32, "ss2"); rt2 = alW.sb([128, 1], F32, "rt2"); rs2 = alW.sb([128, 1], F32, "rs2")
            cx.op("pool", lambda e: e.memset(vbuf[:], 0.0), full=[vbuf.b])

            breg = {}

            def mmgrp(ps_ap, ps_b, pairs, reads):
                n = len(pairs)
                for idx, (l, r_) in enumerate(pairs):
                    cx.op("pe", lambda e, l=l, r_=r_, idx=idx: e.matmul(ps_ap, lhsT=l, rhs=r_, start=(idx == 0),
                                                                         stop=(idx == n - 1)),
                          reads=reads, writes=[ps_b])

            KCUT = int(os.environ.get("KCUT", "9"))
            KNB = int(os.environ.get("KNB", str(NBC)))
            def c_load_h(bi):
                t0 = bi * TC
                h = hT[bi % 2]
                cx.op("sp", lambda e, h=h, t0=t0: e.dma_start(
                    out=h[:], in_=hT_scr[:, :, t0:t0 + TC].rearrange("c p t -> p c t")), full=[h.b], dma=True)

            def c_load_y(bi):
                t0 = bi * TC
                yb = ysb[bi % 2]
                cx.op("sp", lambda e, yb=yb, t0=t0: e.dma_start(
                    out=yb[:], in_=ys_scr[:, :, t0:t0 + TC].rearrange("f p t -> p f t")), full=[yb.b], dma=True)

            def c_s2(bi):
                t0 = bi * TC
                h = hT[bi % 2]; yb = ysb[bi % 2]
                for f in range(4):
                    pa = getps(); pg = getps()
                    mmgrp(pa[:, 0:TC], pa.b, [(wA[:, c, f * 128:(f + 1) * 128], h[:, c, :]) for c in range(8)],
                          [wA.b, h.b])
                    mmgrp(pg[:, 0:TC], pg.b, [(wA[:, c, 512 + f * 128:512 + (f + 1) * 128], h[:, c, :]) for c in range(8)],
                          [wA.b, h.b])
                    s = sgt[f % 3]
                    cx.op("act", lambda e, pg=pg, s=s: e.activation(out=s[:], in_=pg[:, 0:TC], func=AF.Sigmoid),
                          reads=[pg.b], full=[s.b])
                    cx.op("dve", lambda e, pa=pa, s=s, f=f: e.tensor_tensor(out=vbuf[:, f, 30:30 + TC], in0=pa[:, 0:TC],
                                                                            in1=s[:], op=ALU.mult),
                          reads=[pa.b, s.b], writes=[vbuf.b])

            def c_taps(bi, fs):
                for f in fs:
                    pc = getps()
                    Dg = Dg2[f % 2]
                    cx.op("pool", lambda e, Dg=Dg, f=f: e.tensor_tensor(
                        out=Dg[:], in0=ident_f[:].unsqueeze(1).to_broadcast([128, 31, 128]),
                        in1=cdw[:, f, :].unsqueeze(2).to_broadcast([128, 31, 128]), op=ALU.mult),
                        reads=[ident_f.b, cdw.b], full=[Dg.b])
                    mmgrp(pc[:, 0:TC], pc.b, [(Dg[:, k, :], vbuf[:, f, k:k + TC]) for k in range(31)],
                          [Dg.b, vbuf.b])
                    cx.op("act", lambda e, pc=pc, f=f: e.activation(out=cv[:, f, :], in_=pc[:, 0:TC], func=AF.Identity,
                                                                    bias=cb[:, f:f + 1], scale=1.0),
                          reads=[pc.b, cb.b], writes=[cv.b])
                if 3 in fs:
                    cx.op("pool", lambda e: e.tensor_copy(out=vbuf[:, :, 0:30], in_=vbuf[:, :, TC:TC + 30]),
                          reads=[vbuf.b], writes=[vbuf.b])

            def c_rest(bi):
                t0 = bi * TC
                h = hT[bi % 2]; yb = ysb[bi % 2]
                for hd in range(4):
                    pq_ = getps()
                    mmgrp(pq_[:, 0:TC], pq_.b, [(wA[:, c, 1024 + hd * 128:1024 + (hd + 1) * 128], h[:, c, :])
                                                for c in range(8)], [wA.b, h.b])
                    cx.op("dve", lambda e, pq_=pq_, hd=hd: e.tensor_copy(out=qb[:, hd, :], in_=pq_[:, 0:TC]),
                          reads=[pq_.b], writes=[qb.b])
                for f in range(4):
                    cx.op("act", lambda e, f=f: e.activation(out=sq[f % 2][:] if False else sqf[f][:], in_=cv[:, f, :],
                                                             func=AF.Square),
                          reads=[cv.b], full=[sqf[f].b])

                def att_scores(hd):
                    E = Eb[hd % 2]
                    for mc in range(2):
                        psc = getps()
                        mmgrp(psc[:, 0:TC], psc.b, [(kT[:, hd, mc * 128:(mc + 1) * 128], qb[:, hd, :])], [kT.b, qb.b])
                        cx.op("act", lambda e, psc=psc, E=E, mc=mc: e.activation(
                            out=E[:, mc, :], in_=psc[:, 0:TC], func=AF.Exp, scale=float(128 ** -0.5)),
                            reads=[psc.b], writes=[E.b])

                def att_out(hd):
                    E = Eb[hd % 2]
                    po = getps(); pd = getps()
                    mmgrp(po[:, 0:TC], po.b, [(vtok[:, mc, hd * 128:(hd + 1) * 128], E[:, mc, :]) for mc in range(2)],
                          [vtok.b, E.b])
                    mmgrp(pd[:, 0:TC], pd.b, [(ones_bf[:], E[:, mc, :]) for mc in range(2)], [ones_bf.b, E.b])
                    cx.op("dve", lambda e, pd=pd: e.reciprocal(out=rden[:], in_=pd[:, 0:TC]), reads=[pd.b], full=[rden.b])
                    cx.op("dve", lambda e, po=po, hd=hd: e.tensor_tensor(out=ob[:, hd, :], in0=po[:, 0:TC], in1=rden[:],
                                                                         op=ALU.mult),
                          reads=[po.b, rden.b], writes=[ob.b])

                att_scores(0)
                att_scores(1)
                pm = getps(); pq = getps()
                mmgrp(pm[:, 0:TC], pm.b, [(onesm[:], cv[:, f, :]) for f in range(4)], [onesm.b, cv.b])
                mmgrp(pq[:, 0:TC], pq.b, [(onesm[:], sqf[f][:]) for f in range(4)], [onesm.b] + [sqf[f].b for f in range(4)])
                cx.op("act", lambda e, pm=pm: e.copy(out=mean[:], in_=pm[:, 0:TC]), reads=[pm.b], full=[mean.b])
                cx.op("dve", lambda e: e.tensor_tensor(out=var[:], in0=mean[:], in1=mean[:], op=ALU.mult),
                      reads=[mean.b], full=[var.b])
                cx.op("dve", lambda e, pq=pq: e.tensor_tensor(out=var[:], in0=pq[:, 0:TC], in1=var[:], op=ALU.subtract),
                      reads=[pq.b], writes=[var.b])
                cx.op("dve", lambda e: e.tensor_scalar(out=var[:], in0=var[:], scalar1=float(EPS), scalar2=None,
                                                       op0=ALU.add), reads=[var.b], writes=[var.b])
                cx.op("act", lambda e: e.activation(out=lrs[:], in_=var[:], func=AF.Ln), reads=[var.b], full=[lrs.b])
                cx.op("act", lambda e: e.activation(out=lrs[:], in_=lrs[:], func=AF.Exp, scale=-0.5),
                      reads=[], writes=[lrs.b])
                cx.op("dve", lambda e: e.tensor_tensor(out=cv[:], in0=cv[:],
                                                       in1=mean[:].unsqueeze(1).to_broadcast([128, 4, TC]),
                                                       op=ALU.subtract), reads=[mean.b], writes=[cv.b])
                cx.op("dve", lambda e: e.tensor_tensor(out=cv[:], in0=cv[:],
                                                       in1=lrs[:].unsqueeze(1).to_broadcast([128, 4, TC]),
                                                       op=ALU.mult), reads=[lrs.b], writes=[cv.b])
                att_out(0)
                att_scores(2)
                att_out(1)
                att_scores(3)
                att_out(2)
                att_out(3)
                for f in range(4):
                    cx.op("act", lambda e, f=f: e.activation(out=cn[:, f, :], in_=cv[:, f, :], func=AF.Silu,
                                                             bias=lnb[:, f:f + 1], scale=lng[:, f:f + 1]),
                          reads=[cv.b, lnb.b, lng.b], writes=[cn.b])
                for j in range(8):
                    js = slice(j * 128, (j + 1) * 128)
                    pga = getps(); pyc = getps()
                    mmgrp(pga[:, 0:TC], pga.b, [(wG[:, c, j * 128:(j + 1) * 128], h[:, c, :]) for c in range(8)], [wG.b, h.b])
                    mmgrp(pyc[:, 0:TC], pyc.b, [(wco[:, f, js], cn[:, f, :]) for f in range(4)], [wco.b, cn.b])
                    s = sgt[0]
                    cx.op("act", lambda e, pga=pga, s=s: e.activation(out=s[:], in_=pga[:, 0:TC], func=AF.Sigmoid),
                          reads=[pga.b], full=[s.b])
                    cx.op("dve", lambda e, pyc=pyc, s=s: e.tensor_tensor(out=macc[:], in0=pyc[:, 0:TC], in1=s[:], op=ALU.mult),
                          reads=[pyc.b, s.b], full=[macc.b])
                    pgb = getps(); pza = getps(); pzb = getps()
                    mmgrp(pgb[:, 0:TC], pgb.b, [(wG[:, c, 1024 + j * 128:1024 + (j + 1) * 128], h[:, c, :]) for c in range(8)],
                          [wG.b, h.b])
                    mmgrp(pza[:, 0:TC], pza.b, [(wgl[:, f, js], yb[:, f, :]) for f in range(4)], [wgl.b, yb.b])
                    mmgrp(pzb[:, 0:TC], pzb.b, [(wgl[:, f, 1024 + j * 128:1024 + (j + 1) * 128], yb[:, f, :]) for f in range(4)],
                          [wgl.b, yb.b])
                    sb_ = sgt[1]; sz = sgt[2]
                    cx.op("act", lambda e, pgb=pgb, sb_=sb_: e.activation(out=sb_[:], in_=pgb[:, 0:TC], func=AF.Sigmoid),
                          reads=[pgb.b], full=[sb_.b])
                    cx.op("act", lambda e, pzb=pzb, sz=sz: e.activation(out=sz[:], in_=pzb[:, 0:TC], func=AF.Sigmoid),
                          reads=[pzb.b], full=[sz.b])
                    cx.op("dve", lambda e, pza=pza, sz=sz: e.tensor_tensor(out=mt1[:], in0=pza[:, 0:TC], in1=sz[:], op=ALU.mult),
                          reads=[pza.b, sz.b], full=[mt1.b])
                    cx.op("dve", lambda e, sb_=sb_: e.tensor_tensor(out=mt1[:], in0=mt1[:], in1=sb_[:], op=ALU.mult),
                          reads=[sb_.b], writes=[mt1.b])
                    cx.op("dve", lambda e: e.tensor_tensor(out=macc[:], in0=macc[:], in1=mt1[:], op=ALU.add),
                          reads=[mt1.b], writes=[macc.b])
                    pgc = getps(); pym = getps()
                    mmgrp(pgc[:, 0:TC], pgc.b, [(wG[:, c, 2048 + j * 128:2048 + (j + 1) * 128], h[:, c, :]) for c in range(8)],
                          [wG.b, h.b])
                    mmgrp(pym[:, 0:TC], pym.b, [(wmo[:, hd, js], ob[:, hd, :]) for hd in range(4)], [wmo.b, ob.b])
                    s = sgt[0]
                    cx.op("act", lambda e, pgc=pgc, s=s: e.activation(out=s[:], in_=pgc[:, 0:TC], func=AF.Sigmoid),
                          reads=[pgc.b], full=[s.b])
                    cx.op("dve", lambda e, pym=pym, s=s: e.tensor_tensor(out=mt2[:], in0=pym[:, 0:TC], in1=s[:], op=ALU.mult),
                          reads=[pym.b, s.b], full=[mt2.b])
                    cx.op("dve", lambda e, j=j: e.tensor_tensor(out=merged[:, j, :], in0=macc[:], in1=mt2[:], op=ALU.add),
                          reads=[macc.b, mt2.b], writes=[merged.b])

            def c_tail_a(bi):
                ntt = TC // 128
                tis = [bi * ntt + tt for tt in range(ntt)]
                for tt, ti in enumerate(tis):
                    xt_ = xt2[ti % 2]
                    cx.op("sp", lambda e, xt_=xt_, ti=ti: e.dma_start(out=xt_[:], in_=x_d[ti * 128:(ti + 1) * 128, :]),
                          full=[xt_.b], dma=True)
                for tt, ti in enumerate(tis):
                    xt_ = xt2[ti % 2]; x2 = xt_
                    for half in range(2):
                        po_ = getps()
                        mmgrp(po_[:], po_.b, [(merged[:, j, tt * 128:(tt + 1) * 128], wo[:, j, half * 512:(half + 1) * 512])
                                              for j in range(8)], [merged.b, wo.b])
                        cx.op("dve", lambda e, po_=po_, x2=x2, xt_=xt_, half=half: e.tensor_tensor(
                            out=x2[:, half * 512:(half + 1) * 512], in0=po_[:], in1=xt_[:, half * 512:(half + 1) * 512],
                            op=ALU.add), reads=[po_.b, xt_.b], writes=[x2.b])
                    cx.op("sp", lambda e, x2=x2, ti=ti: e.dma_start(out=x2_scr[ti * 128:(ti + 1) * 128, :], in_=x2[:]),
                          reads=[x2.b], dma=True)

            def c_tail_norm(ti):
                if True:
                    x2 = xt2[ti % 2]; hb2 = h2b[0]
                    cx.op("act", lambda e, x2=x2: e.activation(out=junk2[:], in_=x2[:], func=AF.Square, accum_out=ss2[:]),
                          reads=[x2.b], full=[junk2.b, ss2.b])
                    cx.op("act", lambda e: e.activation(out=rt2[:], in_=ss2[:], func=AF.Sqrt, scale=1.0 / D, bias=EPS),
                          reads=[ss2.b], full=[rt2.b])
                    cx.op("dve", lambda e: e.reciprocal(out=rs2[:], in_=rt2[:]), reads=[rt2.b], full=[rs2.b])
                    cx.op("dve", lambda e, x2=x2: e.scalar_tensor_tensor(out=h2f[:], in0=x2[:], scalar=rs2[:, 0:1],
                                                                         in1=gffn[:], op0=ALU.mult, op1=ALU.mult),
                          reads=[x2.b, rs2.b, gffn.b], full=[h2f.b])
                    cx.op("act", lambda e, hb2=hb2: e.copy(out=hb2[:], in_=h2f[:]), reads=[h2f.b], full=[hb2.b])
                    cx.op("sp", lambda e, hb2=hb2, ti=ti: e.dma_start(out=h2_scr[ti * 128:(ti + 1) * 128, :], in_=hb2[:]),
                          reads=[hb2.b], dma=True)

            def c_tail_pe(ti):
                if True:
                    pra = getps(); prb = getps()
                    for c in range(8):
                        pr = pra if c < 4 else prb
                        cx.op("pe", lambda e, pr=pr, c=c: e.transpose(out=pr[:, (c % 4) * 128:(c % 4 + 1) * 128],
                                                                      in_=h2f[:, c * 128:(c + 1) * 128], identity=ident_f[:]),
                              reads=[h2f.b, ident_f.b], writes=[pr.b])
                    cx.op("act", lambda e, pra=pra: e.copy(out=h2T[:, 0:4, :], in_=pra[:].rearrange("p (c t) -> p c t", c=4)),
                          reads=[pra.b], writes=[h2T.b])
                    cx.op("dve", lambda e, prb=prb: e.tensor_copy(out=h2T[:, 4:8, :],
                                                                  in_=prb[:].rearrange("p (c t) -> p c t", c=4)),
                          reads=[prb.b], writes=[h2T.b])
                    plg = getps()
                    mmgrp(plg[:, 0:36], plg.b, [(h2T[:, c, :], wr[:, c, :]) for c in range(8)], [h2T.b, wr.b])
                    cx.op("dve", lambda e, plg=plg, ti=ti: e.tensor_tensor(out=lg_all[:, ti, :], in0=plg[:, 0:36],
                                                                        in1=rbias[:], op=ALU.add),
                          reads=[plg.b, rbias.b], writes=[lg_all.b])

            NBR = min(NBC, KNB) if KCUT >= 2 else 0
            if NBR > 0:
                c_load_h(0); c_load_y(0); c_s2(0); c_taps(0, [0, 1, 2, 3])
            for bi in range(NBR):
                nxt = bi + 1 < NBR
                if nxt:
                    c_load_h(bi + 1)
                c_rest(bi)
                if nxt:
                    c_load_y(bi + 1)
                    c_s2(bi + 1)
                c_tail_a(bi)
                t_a, t_b = 2 * bi, 2 * bi + 1
                c_tail_norm(t_a)
                if nxt:
                    c_taps(bi + 1, [0, 1])
                c_tail_pe(t_a)
                c_tail_norm(t_b)
                if nxt:
                    c_taps(bi + 1, [2, 3])
                c_tail_pe(t_b)
            cx.barrier()
            alW.close()
            alR = Alloc(nc)
            RS = Buf("route")
            tri = alR.sb([128, 128], F32, "tri")
            ones_f = alR.sb([128, 128], F32, "ones_f")
            cx.op("sp", lambda e: e.dma_start(out=tri[:], in_=tri_d), full=[tri.b], dma=True)
            cx.op("pool", lambda e: e.memset(ones_f[:], 1.0), full=[ones_f.b])

            def rd(fn, extra_reads=(), extra_writes=()):
                cx.op("dve", fn, reads=[RS, lg_all.b] + list(extra_reads), writes=[RS] + list(extra_writes))

            def R(shape, name, dt=F32):
                return alR.sb(shape, dt, name)

            NTT = NT
            NEB_ = NEXP * NBLK
            gmax = R([128, NTT], "gmax"); ohg = R([128, NTT, 4], "ohg"); eg = R([128, NTT, 4], "eg")
            sumg = R([128, NTT], "sumg"); ptop = R([128, NTT], "ptop")
            selm = R([128, NTT, 4, 8], "selm"); sel = R([128, NTT, 8], "sel"); sel2 = R([128, NTT, 8], "sel2")
            m1_ = R([128, NTT], "m1_"); m2_ = R([128, NTT], "m2_"); oh1 = R([128, NTT, 8], "oh1"); oh2 = R([128, NTT, 8], "oh2")
            dm = R([128, NTT], "dm"); w1 = R([128, NTT], "w1"); w2 = R([128, NTT], "w2")
            M1 = R([128, NTT, 4, 8], "M1"); M2 = R([128, NTT, 4, 8], "M2"); Mc = R([128, NTT, 32], "Mc")
            Cex = R([128, NTT, 32], "Cex"); pos = R([128, NTT, 32], "pos"); bk = R([128, NTT, 32], "bk")
            sf = R([128, NTT, 32], "sf"); ov = R([128, NTT, 32], "ov"); tq = R([128, NTT, 32], "tq")
            sk = [R([128, NTT], f"sk{k}") for k in range(2)]; okk = R([128, NTT], "okk"); dd = R([128, NTT], "dd")
            si = [R([128, NTT], f"si{k}", I32) for k in range(2)]
            ent = [R([128, NTT, 4], f"ent{k}") for k in range(2)]
            le4 = lg_all[:, :, 4:36].rearrange("p t (g j) -> p t g j", g=4)

            def bc3(a, n):
                return a.unsqueeze(2).to_broadcast([128, NTT, n])

            rd(lambda e: e.tensor_reduce(out=gmax[:], in_=lg_all[:, :, 0:4], axis=AX.X, op=ALU.max))
            rd(lambda e: e.tensor_tensor(out=ohg[:], in0=lg_all[:, :, 0:4], in1=bc3(gmax[:], 4), op=ALU.is_equal))
            rd(lambda e: e.tensor_tensor(out=eg[:], in0=lg_all[:, :, 0:4], in1=bc3(gmax[:], 4), op=ALU.subtract))
            cx.op("act", lambda e: e.activation(out=eg[:], in_=eg[:], func=AF.Exp), reads=[RS], writes=[RS])
            rd(lambda e: e.tensor_reduce(out=sumg[:], in_=eg[:], axis=AX.X, op=ALU.add))
            rd(lambda e: e.reciprocal(out=ptop[:], in_=sumg[:]))
            rd(lambda e: e.tensor_tensor(out=selm[:], in0=le4,
                                         in1=ohg[:].unsqueeze(3).to_broadcast([128, NTT, 4, 8]), op=ALU.mult))
            rd(lambda e: e.tensor_reduce(out=sel[:], in_=selm[:].rearrange("p t g j -> p t j g"), axis=AX.X, op=ALU.add))
            rd(lambda e: e.tensor_reduce(out=m1_[:], in_=sel[:], axis=AX.X, op=ALU.max))
            rd(lambda e: e.tensor_tensor(out=oh1[:], in0=sel[:], in1=bc3(m1_[:], 8), op=ALU.is_equal))
            rd(lambda e: e.scalar_tensor_tensor(out=sel2[:], in0=oh1[:], scalar=-1e30, in1=sel[:], op0=ALU.mult, op1=ALU.add))
            rd(lambda e: e.tensor_reduce(out=m2_[:], in_=sel2[:], axis=AX.X, op=ALU.max))
            rd(lambda e: e.tensor_tensor(out=oh2[:], in0=sel2[:], in1=bc3(m2_[:], 8), op=ALU.is_equal))
            rd(lambda e: e.tensor_tensor(out=dm[:], in0=m1_[:], in1=m2_[:], op=ALU.subtract))
            cx.op("act", lambda e: e.activation(out=w1[:], in_=dm[:], func=AF.Sigmoid), reads=[RS], writes=[RS])
            rd(lambda e: e.tensor_tensor(out=w1[:], in0=w1[:], in1=ptop[:], op=ALU.mult))
            rd(lambda e: e.tensor_tensor(out=w2[:], in0=ptop[:], in1=w1[:], op=ALU.subtract))
            rd(lambda e: e.tensor_tensor(out=M1[:], in0=ohg[:].unsqueeze(3).to_broadcast([128, NTT, 4, 8]),
                                         in1=oh1[:].unsqueeze(2).to_broadcast([128, NTT, 4, 8]), op=ALU.mult))
            rd(lambda e: e.tensor_tensor(out=M2[:], in0=ohg[:].unsqueeze(3).to_broadcast([128, NTT, 4, 8]),
                                         in1=oh2[:].unsqueeze(2).to_broadcast([128, NTT, 4, 8]), op=ALU.mult))
            rd(lambda e: e.tensor_tensor(out=Mc[:], in0=M1[:].rearrange("p t g j -> p t (g j)"),
                                         in1=M2[:].rearrange("p t g j -> p t (g j)"), op=ALU.add))
            rd(lambda e: e.memset(Cex[:, 0, :], 0.0))
            for i in range(1, NTT):
                rd(lambda e, i=i: e.tensor_tensor(out=Cex[:, i, :], in0=Cex[:, i - 1, :], in1=Mc[:, i - 1, :], op=ALU.add))
            pp = [getps(), getps()]
            for i in range(NTT):
                pb_ = pp[i // 16]
                o_ = pb_[:, (i % 16) * 32:(i % 16 + 1) * 32]
                cx.op("pe", lambda e, o_=o_, i=i: e.matmul(o_, lhsT=tri[:], rhs=Mc[:, i, :], start=True, stop=False),
                      reads=[tri.b, RS], writes=[pb_.b])
                cx.op("pe", lambda e, o_=o_, i=i: e.matmul(o_, lhsT=ones_f[:], rhs=Cex[:, i, :], start=False, stop=True),
                      reads=[ones_f.b, RS], writes=[pb_.b])
            for hh in range(2):
                rd(lambda e, hh=hh: e.tensor_copy(out=pos[:, hh * 16:(hh + 1) * 16, :],
                                                  in_=pp[hh][:].rearrange("p (t x) -> p t x", t=16)), [pp[hh].b])
            rd(lambda e: e.tensor_single_scalar(out=bk[:], in_=pos[:], scalar=127.5, op=ALU.is_gt))
            for thr in range(2, NBLK):
                rd(lambda e, thr=thr: e.tensor_single_scalar(out=tq[:], in_=pos[:], scalar=128.0 * thr - 0.5, op=ALU.is_gt))
                rd(lambda e: e.tensor_tensor(out=bk[:], in0=bk[:], in1=tq[:], op=ALU.add))
            rd(lambda e: e.scalar_tensor_tensor(out=bk[:], in0=bk[:], scalar=float(1 - 128 * NEB_),
                                                in1=ecap[:].unsqueeze(1).to_broadcast([128, NTT, 32]),
                                                op0=ALU.mult, op1=ALU.add), [ecap.b])
            rd(lambda e: e.scalar_tensor_tensor(out=sf[:], in0=pos[:], scalar=float(NEB_), in1=bk[:],
                                                op0=ALU.mult, op1=ALU.add))
            rd(lambda e: e.tensor_single_scalar(out=ov[:], in_=pos[:], scalar=float(CAP) - 0.5, op=ALU.is_gt))
            for k, (Mk, wk) in enumerate(((M1, w1), (M2, w2))):
                Mk32 = Mk[:].rearrange("p t g j -> p t (g j)")
                rd(lambda e, Mk32=Mk32: e.tensor_tensor(out=tq[:], in0=Mk32, in1=sf[:], op=ALU.mult))
                rd(lambda e, k=k: e.tensor_reduce(out=sk[k][:], in_=tq[:], axis=AX.X, op=ALU.add))
                rd(lambda e, Mk32=Mk32: e.tensor_tensor(out=tq[:], in0=Mk32, in1=ov[:], op=ALU.mult))
                rd(lambda e: e.tensor_reduce(out=okk[:], in_=tq[:], axis=AX.X, op=ALU.add))
                rd(lambda e, k=k: e.tensor_scalar(out=dd[:], in0=sk[k][:], scalar1=trashp[:, 0:1], scalar2=None,
                                                  op0=ALU.subtract), [trashp.b])
                rd(lambda e: e.tensor_tensor(out=dd[:], in0=dd[:], in1=okk[:], op=ALU.mult))
                rd(lambda e, k=k: e.tensor_tensor(out=sk[k][:], in0=sk[k][:], in1=dd[:], op=ALU.subtract))
                rd(lambda e, k=k: e.tensor_copy(out=si[k][:], in_=sk[k][:]), (), [si[k].b])
                rd(lambda e, k=k: e.memset(ent[k][:], 0.0), (), [ent[k].b])
                rd(lambda e, k=k: e.tensor_copy(out=ent[k][:, :, 0], in_=tokid[:]), [tokid.b], [ent[k].b])
                rd(lambda e, k=k: e.tensor_scalar(out=ent[k][:, :, 1], in0=tokid[:], scalar1=float(k * ROWS), scalar2=None,
                                                  op0=ALU.add), [tokid.b], [ent[k].b])
                rd(lambda e, k=k, wk=wk: e.tensor_copy(out=ent[k][:, :, 2], in_=wk[:]), (), [ent[k].b])
            for i in range(NTT):
                for k in range(2):
                    cx.op("pool", lambda e, i=i, k=k: e.indirect_dma_start(
                        out=lst_d, out_offset=bass.IndirectOffsetOnAxis(ap=si[k][:, i:i + 1], axis=0),
                        in_=ent[k][:, i, :], in_offset=None),
                        reads=[si[k].b, ent[k].b, lstB], dma=True)
            cx.barrier()
            alR.close()
            alC.close()
        if stop_after in ("A", "B", "C"):
            pass
        else:
            alD = Alloc(nc)
            NEB = NEXP * NBLK
            lst_sb = alD.sb([128, NEB, 4], F32, "lst_sb")
            idx_i = alD.sb([128, NEB], I32, "idx_i")
            dst_i = alD.sb([128, NEB], I32, "dst_i")
            cx.op("sp", lambda e: e.dma_start(out=lst_sb[:], in_=lst_d[0:NEXP * CAP, :].rearrange("(s eb) w -> s eb w", s=128)),
                  reads=[lstB], full=[lst_sb.b], dma=True)
            cx.op("dve", lambda e: e.tensor_copy(out=idx_i[:], in_=lst_sb[:, :, 0]), reads=[lst_sb.b], full=[idx_i.b])
            cx.op("dve", lambda e: e.tensor_copy(out=dst_i[:], in_=lst_sb[:, :, 1]), reads=[lst_sb.b], full=[dst_i.b])
            NWB = 3
            Wg = [alD.sb([128, 8, 256], BF16, f"Wg{i}") for i in range(NWB)]
            Wu = [alD.sb([128, 8, 256], BF16, f"Wu{i}") for i in range(NWB)]
            Wd = [alD.sb([128, 2, 1024], BF16, f"Wd{i}") for i in range(NWB)]
            Gt = [alD.sb([128, D], BF16, f"Gt{i}") for i in range(3)]
            Xe = [alD.sb([128, 8, CAP], BF16, f"Xe{i}") for i in range(2)]
            sgl = [alD.sb([128, CAP], F32, f"sgl{i}") for i in range(2)]
            ae = [alD.sb([128, 2, CAP], BF16, f"ae{i}") for i in range(2)]
            Yt = [alD.sb([128, D], BF16, f"Yt{i}") for i in range(3)]

            Gt6 = Gt + [alD.sb([128, D], BF16, f"Gtx{i}") for i in range(3)]

            def load_w_dma(e_):
                p = e_ % NWB
                cx.op("sp", lambda e: e.dma_start(out=Wg[p][:].rearrange("p c n -> p (c n)"), in_=wbf_scr[e_, 0]),
                      full=[Wg[p].b], dma=True)
                cx.op("sp", lambda e: e.dma_start(out=Wu[p][:].rearrange("p c n -> p (c n)"), in_=wbf_scr[e_, 1]),
                      full=[Wu[p].b], dma=True)
                cx.op("sp", lambda e: e.dma_start(out=Wd[p][:].rearrange("p c n -> p (c n)"), in_=wbf_scr[e_, 2]),
                      full=[Wd[p].b], dma=True)

            def load_w_cast(e_):
                pass

            def gathers(e_):
                for blk in range(NBLK):
                    eb = e_ * NBLK + blk
                    G = Gt6[(e_ % 2) * 3 + blk]
                    cx.op("pool", lambda e, G=G, eb=eb: e.indirect_dma_start(
                        out=G[:], out_offset=None, in_=h2_scr,
                        in_offset=bass.IndirectOffsetOnAxis(ap=idx_i[:, eb:eb + 1], axis=0)),
                        reads=[idx_i.b, h2B], full=[G.b], dma=True)

            gi = [0]
            load_w_dma(0)
            load_w_dma(1)
            gathers(0)
            KNE = int(os.environ.get("KNE", str(NEXP)))
            for e_ in range(KNE):
                p = e_ % NWB
                if e_ + 2 < NEXP:
                    load_w_dma(e_ + 2)
                if e_ + 1 < NEXP:
                    gathers(e_ + 1)
                X = Xe[e_ % 2]
                for blk in range(NBLK):
                    G = Gt6[(e_ % 2) * 3 + blk]
                    pbk = psb[gi[0] % 2]
                    gi[0] += 1
                    for c in range(8):
                        cx.op("pe", lambda e, pbk=pbk, G=G, c=c: e.transpose(
                            out=pbk[:, c * 128:(c + 1) * 128], in_=G[:, c * 128:(c + 1) * 128], identity=ident_bf[:]),
                            reads=[G.b, ident_bf.b], writes=[pbk.b])
                    if blk % 2 == 0:
                        cx.op("act", lambda e, pbk=pbk, X=X, blk=blk: e.copy(
                            out=X[:, :, blk * 128:(blk + 1) * 128], in_=pbk[:].rearrange("p (c t) -> p c t", c=8)),
                            reads=[pbk.b], writes=[X.b])
                    else:
                        cx.op("dve", lambda e, pbk=pbk, X=X, blk=blk: e.tensor_copy(
                            out=X[:, :, blk * 128:(blk + 1) * 128], in_=pbk[:].rearrange("p (c t) -> p c t", c=8)),
                            reads=[pbk.b], writes=[X.b])
                a_ = ae[e_ % 2]
                for ft in range(2):
                    pg = getps(); pu = getps()
                    for c in range(8):
                        cx.op("pe", lambda e, pg=pg, c=c, ft=ft, X=X, p=p: e.matmul(
                            pg[:, 0:CAP], lhsT=Wg[p][:, c, ft * 128:(ft + 1) * 128], rhs=X[:, c, :],
                            start=(c == 0), stop=(c == 7)), reads=[Wg[p].b, X.b], writes=[pg.b])
                    for c in range(8):
                        cx.op("pe", lambda e, pu=pu, c=c, ft=ft, X=X, p=p: e.matmul(
                            pu[:, 0:CAP], lhsT=Wu[p][:, c, ft * 128:(ft + 1) * 128], rhs=X[:, c, :],
                            start=(c == 0), stop=(c == 7)), reads=[Wu[p].b, X.b], writes=[pu.b])
                    s = sgl[ft]
                    cx.op("act", lambda e, pg=pg, s=s: e.activation(out=s[:], in_=pg[:, 0:CAP], func=AF.Silu),
                          reads=[pg.b], full=[s.b])
                    cx.op("dve", lambda e, pu=pu, s=s, a_=a_, ft=ft: e.tensor_tensor(
                        out=a_[:, ft, :], in0=pu[:, 0:CAP], in1=s[:], op=ALU.mult),
                        reads=[pu.b, s.b], writes=[a_.b])
                for blk in range(NBLK):
                    eb = e_ * NBLK + blk
                    Y = Yt[eb % 3]
                    for half in range(2):
                        py = getps()
                        for ft in range(2):
                            cx.op("pe", lambda e, py=py, ft=ft, blk=blk, half=half, a_=a_, p=p: e.matmul(
                                py[:], lhsT=a_[:, ft, blk * 128:(blk + 1) * 128],
                                rhs=Wd[p][:, ft, half * 512:(half + 1) * 512], start=(ft == 0), stop=(ft == 1)),
                                reads=[a_.b, Wd[p].b], writes=[py.b])
                        if half == 0:
                            cx.op("dve", lambda e, py=py, Y=Y, eb=eb: e.tensor_scalar(
                                out=Y[:, 0:512], in0=py[:], scalar1=lst_sb[:, eb, 2:3], scalar2=None, op0=ALU.mult),
                                reads=[py.b, lst_sb.b], writes=[Y.b])
                        else:
                            cx.op("act", lambda e, py=py, Y=Y, eb=eb: e.activation(
                                out=Y[:, 512:1024], in_=py[:], func=AF.Copy, scale=lst_sb[:, eb, 2:3]),
                                reads=[py.b, lst_sb.b], writes=[Y.b])
                    cx.op("pool", lambda e, Y=Y, eb=eb: e.indirect_dma_start(
                        out=moe_scr, out_offset=bass.IndirectOffsetOnAxis(ap=dst_i[:, eb:eb + 1], axis=0),
                        in_=Y[:], in_offset=None), reads=[Y.b, dst_i.b, moeB], dma=True)
                if e_ + 1 < NEXP:
                    load_w_cast(e_ + 1)
            cx.barrier()
            alD.close()

            alE = Alloc(nc)
            gfin = alE.sb([128, D], F32, "gfin")
            cx.op("sp", lambda e: e.dma_start(out=gfin[:], in_=gfin_d.partition_broadcast(128)), full=[gfin.b], dma=True)
            NE_ = 4
            xa = [alE.sb([128, D], F32, f"xa{i}") for i in range(NE_)]
            m0 = [alE.sb([128, D], BF16, f"m0{i}") for i in range(NE_)]
            m1 = [alE.sb([128, D], BF16, f"m1{i}") for i in range(NE_)]
            ot = [alE.sb([128, D], F32, f"ot{i}") for i in range(NE_)]
            junk3 = alE.sb([128, D], BF16, "junk3")
            sse = [alE.sb([128, 1], F32, f"sse{i}") for i in range(NE_)]
            rte = [alE.sb([128, 1], F32, f"rte{i}") for i in range(NE_)]
            rse = [alE.sb([128, 1], F32, f"rse{i}") for i in range(NE_)]
            outB = Buf("out")

            def e_load(ti):
                p = ti % NE_
                rows = slice(ti * 128, (ti + 1) * 128)
                cx.op("sp", lambda e, p=p, rows=rows: e.dma_start(out=xa[p][:], in_=x2_scr[rows, :]),
                      full=[xa[p].b], dma=True)
                cx.op("sp", lambda e, p=p, rows=rows: e.dma_start(out=m0[p][:], in_=moe_scr[rows, :]),
                      full=[m0[p].b], dma=True)
                cx.op("sp", lambda e, p=p, ti=ti: e.dma_start(
                    out=m1[p][:], in_=moe_scr[ROWS + ti * 128:ROWS + (ti + 1) * 128, :]),
                    full=[m1[p].b], dma=True)

            for ti in range(min(NE_ - 1, NT)):
                e_load(ti)
            for ti in range(NT):
                p = ti % NE_
                rows = slice(ti * 128, (ti + 1) * 128)
                if ti + NE_ - 1 < NT:
                    e_load(ti + NE_ - 1)
                cx.op("pool", lambda e, p=p: e.tensor_tensor(out=xa[p][:], in0=xa[p][:], in1=m0[p][:], op=ALU.add),
                      reads=[m0[p].b], writes=[xa[p].b])
                cx.op("dve", lambda e, p=p: e.tensor_tensor(out=xa[p][:], in0=xa[p][:], in1=m1[p][:], op=ALU.add),
                      reads=[m1[p].b], writes=[xa[p].b])
                cx.op("act", lambda e, p=p: e.activation(out=junk3[:], in_=xa[p][:], func=AF.Square, accum_out=sse[p][:]),
                      reads=[xa[p].b], full=[junk3.b, sse[p].b])
                cx.op("act", lambda e, p=p: e.activation(out=rte[p][:], in_=sse[p][:], func=AF.Sqrt, scale=1.0 / D, bias=EPS),
                      reads=[sse[p].b], full=[rte[p].b])
                cx.op("dve", lambda e, p=p: e.reciprocal(out=rse[p][:], in_=rte[p][:]), reads=[rte[p].b], full=[rse[p].b])
                cx.op("dve", lambda e, p=p: e.scalar_tensor_tensor(out=ot[p][:], in0=xa[p][:], scalar=rse[p][:, 0:1],
                                                                   in1=gfin[:], op0=ALU.mult, op1=ALU.mult),
                      reads=[xa[p].b, rse[p].b, gfin.b], full=[ot[p].b])
                cx.op("sp", lambda e, p=p, rows=rows: e.dma_start(out=out_d[rows, :], in_=ot[p][:]),
                      reads=[ot[p].b], writes=[outB], dma=True)
            cx.barrier()
            alE.close()
        cx.barrier()
        cx.emit(block)
        print("waits", cx.nwait, "instrs", {e: cx.cnt[e] for e in cx.ENG}, "signals", {e: len(cx.waited[e]) for e in cx.ENG})
    return nc


def host_consts():
    c = {}
    c["ident_bf"] = np.eye(128, dtype=np.float32).astype(ml_dtypes.bfloat16)
    c["ident_f"] = np.eye(128, dtype=np.float32)
    psel = np.zeros((128, 8, 240), np.float32)
    for a in range(8):
        for i in range(16):
            psel[a * 16 + i, a, 7 * 16 + i] = 1.0
    c["psel"] = psel.astype(ml_dtypes.bfloat16)
    kk = np.arange(128) // 16
    c["cmask"] = (kk[None, :] >= kk[:, None]).astype(np.float32)
    c["tri"] = (np.arange(128)[:, None] < np.arange(128)[None, :]).astype(np.float32)
    c["ecap"] = np.ascontiguousarray(np.broadcast_to((np.arange(32) * NBLK).astype(np.float32)[None, :], (128, 32)))
    c["tokid"] = (np.arange(NT)[None, :] * 128 + np.arange(128)[:, None]).astype(np.float32)
    li = np.zeros((NEXP * CAP + 128, 4), np.float32)
    li[:, 0] = SEQ + ((np.arange(NEXP * CAP + 128) // (NEXP * NBLK)) % 128)
    li[:, 1] = li[:, 0]
    c["trashp"] = (NEXP * CAP + np.arange(128)).astype(np.float32).reshape(128, 1)
    c["lst_init"] = li
    return c


def relayout_kn(w):
    K, N = w.shape
    return np.ascontiguousarray(w.reshape(K // 128, 128, N).transpose(1, 0, 2))


def relayout_pc(w):
    E, K, N = w.shape
    return np.ascontiguousarray(w.reshape(E, K // 128, 128, N).transpose(0, 2, 1, 3))


def pair_layout(a):
    rest = a.shape[2:]
    a = a.reshape((16, 2, 64) + rest)
    a = np.moveaxis(a, 0, 2)
    return np.ascontiguousarray(a.reshape((128, 16) + rest))


def make_inmap(inputs, b, consts=None):
    f = lambda a: np.ascontiguousarray(a, dtype=np.float32)
    m = {"x": f(inputs["x"][b]),
         "g_mix": f(inputs["g_mix"]),
         "w_in": relayout_kn(f(inputs["w_in"][0]))}
    m["lamre_l"] = pair_layout(f(inputs["ssm_lambda_re"][0]))
    m["lamim_l"] = pair_layout(f(inputs["ssm_lambda_im"][0]))
    m["logdt_l"] = pair_layout(np.broadcast_to(f(inputs["ssm_log_dt"][0])[:, None], (32, 64)))
    m["bre_l"] = pair_layout(f(inputs["ssm_b_re"][0]))
    m["bim_l"] = pair_layout(f(inputs["ssm_b_im"][0]))
    m["cre_l"] = pair_layout(f(inputs["ssm_c_re"][0]).transpose(0, 2, 1))
    m["cim_l"] = pair_layout(f(inputs["ssm_c_im"][0]).transpose(0, 2, 1))
    m["d_l"] = np.ascontiguousarray(np.tile(f(inputs["ssm_d"][0]).reshape(32, 16).T, (8, 1)))
    m["mem"] = f(inputs["mem"][b])
    for k_, n_ in (("g_mem", "g_mem"), ("g_ffn", "g_ffn")):
        m[n_] = f(inputs[k_])
    m["g_final"] = f(inputs["g_final"]).reshape(1, D)
    m["w_mem_kv"] = relayout_kn(f(inputs["w_mem_kv"][0])); m["w_mem_out"] = relayout_kn(f(inputs["w_mem_out"][0]))
    m["w_conv_out"] = relayout_kn(f(inputs["w_conv_out"][0])); m["w_ssm_glu"] = relayout_kn(f(inputs["w_ssm_glu"][0]))
    m["w_out"] = relayout_kn(f(inputs["w_out"][0]))
    m["w_router"] = np.ascontiguousarray(np.concatenate([f(inputs["w_router_group"][0]),
                                                         f(inputs["w_router_expert"][0])], axis=1))
    m["b_router"] = np.ascontiguousarray(np.concatenate([f(inputs["b_router_group"][0]),
                                                         f(inputs["b_router_expert"][0])])[None, :])
    m["cdw_l"] = np.ascontiguousarray(f(inputs["conv_dw"][0]).T.reshape(4, 128, 31).transpose(1, 0, 2))
    m["cb_l"] = np.ascontiguousarray(f(inputs["conv_dw_bias"][0]).reshape(4, 128).T)
    m["lng_l"] = np.ascontiguousarray(f(inputs["conv_ln_g"][0]).reshape(4, 128).T)
    m["lnb_l"] = np.ascontiguousarray(f(inputs["conv_ln_b"][0]).reshape(4, 128).T)
    if consts is not None and "w_exp_gate" in consts:
        for k_ in ("w_exp_gate", "w_exp_up", "w_exp_down"):
            m[k_] = consts[k_]
    else:
        m["w_exp_gate"] = relayout_pc(f(inputs["w_exp_gate"][0]))
        m["w_exp_up"] = relayout_pc(f(inputs["w_exp_up"][0]))
        m["w_exp_down"] = relayout_pc(f(inputs["w_exp_down"][0]))
    m.update(consts if consts is not None else host_consts())
    return m


def kernel(**inputs):
    nc = build()
    consts = host_consts()
    f32 = lambda a: np.ascontiguousarray(a, dtype=np.float32)
    for k_ in ("w_exp_gate", "w_exp_up", "w_exp_down"):
        consts[k_] = relayout_pc(f32(inputs[k_][0]))
    in_maps = [make_inmap(inputs, b, consts) for b in range(NCORES)]
    res = run_bass_kernel_spmd(nc, in_maps, core_ids=list(range(NCORES)))
    return np.stack([r["out"] for r in res.results], axis=0)
```

```python
import os
import numpy as np
import ml_dtypes
from contextlib import ExitStack
import concourse.bass as bass
import concourse.mybir as mybir
from concourse.bass_utils import run_bass_kernel_spmd

F32 = mybir.dt.float32
BF16 = mybir.dt.bfloat16
I32 = mybir.dt.int32
U32 = mybir.dt.uint32
AF = mybir.ActivationFunctionType
ALU = mybir.AluOpType
AX = mybir.AxisListType
GELU = AF.Gelu_apprx_tanh

D = 1024
SEQ = 4096
NCORES = 8
T = 512
NB = SEQ // T
NT = SEQ // 128
EPS = 1e-6
NEXP = 32
CAP = 384
NBLK = CAP // 128
ROWS = SEQ + 128


class Buf:
    __slots__ = ("name", "w", "r")

    def __init__(self, name):
        self.name = name
        self.w = {}
        self.r = {}


class Ctx:
    ENG = ("pe", "dve", "act", "pool", "sp")
    KROT = 4
    NDMA = 12

    def __init__(self, nc, es):
        self.nc = nc
        self.q = {e: [] for e in self.ENG}
        self.cnt = {e: 0 for e in self.ENG}
        self.seen = {e: {} for e in self.ENG}
        self.esem = {e: [es.enter_context(nc.semaphore(f"s_{e}{i}")) for i in range(self.KROT)]
                     for e in self.ENG}
        self.dsem = {e: [es.enter_context(nc.semaphore(f"d_{e}{i}")) for i in range(self.NDMA)]
                     for e in ("sp", "act", "pool")}
        self.dcnt = {e: [0] * self.NDMA for e in self.dsem}
        self.dnext = {e: 0 for e in self.dsem}
        self.nwait = 0
        self.waited = {e: set() for e in self.ENG}

    def _wait(self, eng, tok):
        key, val = tok
        if key[0] == 'e' and key[1] == eng and eng == "pe":
            return
        if self.seen[eng].get(key, -1) >= val:
            return
        self.seen[eng][key] = val
        if key[0] == 'e':
            self.waited[key[1]].add(val)
        self.q[eng].append(("w", key, val))
        self.nwait += 1

    def op(self, eng, fn, reads=(), writes=(), full=(), dma=False):
        toks = []
        for b in reads:
            toks.extend(b.w.items())
        for b in tuple(writes) + tuple(full):
            toks.extend(b.w.items())
            toks.extend(b.r.items())
        for t in toks:
            self._wait(eng, t)
        if dma:
            i = self.dnext[eng]
            self.dnext[eng] = (i + 1) % self.NDMA
            key = ('d', eng, i)
            if self.dcnt[eng][i] > 0:
                self._wait(eng, (key, self.dcnt[eng][i]))
            self.dcnt[eng][i] += 16
            val = self.dcnt[eng][i]
            self.q[eng].append(("d", fn, self.dsem[eng][i]))
        else:
            key = ('e', eng)
            val = self.cnt[eng]
            self.cnt[eng] += 1
            self.q[eng].append(("i", fn, val))
        for b in reads:
            b.r[key] = val
        for b in full:
            b.w = {key: val}
            b.r = {}
        for b in writes:
            b.w[key] = val
        return (key, val)

    def barrier(self, skip_pool_dma=False):
        toks = []
        for e in self.ENG:
            if skip_pool_dma and e == "pool":
                continue
            if self.cnt[e] > 0:
                toks.append((('e', e), self.cnt[e] - 1))
        for e in self.dsem:
            if skip_pool_dma and e == "pool":
                continue
            for i in range(self.NDMA):
                if self.dcnt[e][i] > 0:
                    toks.append((('d', e, i), self.dcnt[e][i]))
        for e in self.ENG:
            for t in toks:
                self._wait(e, t)

    def emit(self, block):
        nc = self.nc

        rank = {e: {v: i for i, v in enumerate(sorted(self.waited[e]))} for e in self.ENG}
        K_ = self.KROT

        def run(engname, engine):
            for item in self.q[engname]:
                if item[0] == "w":
                    key, val = item[1], item[2]
                    if key[0] == 'e':
                        r = rank[key[1]][val]
                        engine.wait_ge(self.esem[key[1]][r % K_], r // K_ + 1)
                    else:
                        engine.wait_ge(self.dsem[key[1]][key[2]], val)
                elif item[0] == "d":
                    item[1](engine).then_inc(item[2], 16)
                else:
                    ins = item[1](engine)
                    r = rank[engname].get(item[2])
                    if r is not None:
                        ins.then_inc(self.esem[engname][r % K_], 1)

        @block.tensor
        def _(e):
            run("pe", e)

        @block.vector
        def _(e):
            run("dve", e)

        @block.scalar
        def _(e):
            run("act", e)

        @block.gpsimd
        def _(e):
            run("pool", e)

        @block.sync
        def _(e):
            run("sp", e)


class TT:
    def __init__(self, t, name):
        self.t = t
        self.b = Buf(name)

    def __getitem__(self, k):
        return self.t[k]


class Alloc:
    cnt = [0]

    def __init__(self, nc, es=None):
        self.nc = nc
        self.es = es if es is not None else ExitStack()

    @property
    def n(self):
        return Alloc.cnt[0]

    @n.setter
    def n(self, v):
        Alloc.cnt[0] = v

    def close(self):
        self.es.close()

    def sb(self, shape, dt, name=None):
        self.n += 1
        name = name or f"sb{self.n}"
        t = self.es.enter_context(self.nc.sbuf_tensor(f"{name}_{self.n}", list(shape), dt))
        return TT(t, name)

    def ps(self, shape, dt, name=None):
        self.n += 1
        name = name or f"ps{self.n}"
        t = self.es.enter_context(self.nc.psum_tensor(f"{name}_{self.n}", list(shape), dt))
        return TT(t, name)


def build(stop_after="E", dbg=False):
    nc = bass.Bass("TRN2", target_bir_lowering=False)
    dram = {}

    def din(name, shape, dt=F32):
        dram[name] = nc.dram_tensor(name, list(shape), dt, kind="ExternalInput").ap()
        return dram[name]

    def dscr(name, shape, dt, kind="Internal"):
        dram[name] = nc.dram_tensor(name, list(shape), dt, kind=kind).ap()
        return dram[name]

    x_d = din("x", [SEQ, D])
    gmix_d = din("g_mix", [1, D])
    w_in_d = din("w_in", [128, 8, 5120])
    ident_bf_d = din("ident_bf", [128, 128], BF16)
    ident_f_d = din("ident_f", [128, 128], F32)
    lamre_d = din("lamre_l", [128, 16])
    lamim_d = din("lamim_l", [128, 16])
    logdt_d = din("logdt_l", [128, 16])
    bre_d = din("bre_l", [128, 16, 16])
    bim_d = din("bim_l", [128, 16, 16])
    cre_d = din("cre_l", [128, 16, 16])
    cim_d = din("cim_l", [128, 16, 16])
    dl_d = din("d_l", [128, 32])
    psel_d = din("psel", [128, 8, 240], BF16)
    cmask_d = din("cmask", [128, 128])
    mem_d = din("mem", [256, D])
    gmem_d = din("g_mem", [1, D])
    gffn_d = din("g_ffn", [1, D])
    gfin_d = din("g_final", [1, D])
    wkv_d = din("w_mem_kv", [128, 8, 1024])
    wmo_d = din("w_mem_out", [128, 4, D])
    wco_d = din("w_conv_out", [128, 4, D])
    wgl_d = din("w_ssm_glu", [128, 4, 2048])
    wo_d = din("w_out", [128, 8, D])
    wr_d = din("w_router", [D, 36])
    rbias_d = din("b_router", [1, 36])
    cdw_d = din("cdw_l", [128, 4, 31])
    cb_d = din("cb_l", [128, 4])
    lng_d = din("lng_l", [128, 4])
    lnb_d = din("lnb_l", [128, 4])
    tri_d = din("tri", [128, 128])
    ecap_d = din("ecap", [128, 32])
    tokid_d = din("tokid", [128, NT])
    lst_init_d = din("lst_init", [NEXP * CAP + 128, 4])
    trashp_d = din("trashp", [128, 1])
    weg_d = din("w_exp_gate", [NEXP, 128, 8, 256])
    weu_d = din("w_exp_up", [NEXP, 128, 8, 256])
    wed_d = din("w_exp_down", [NEXP, 128, 2, D])
    dk = "ExternalOutput" if dbg else "Internal"
    wbf_scr = dscr("wbf_scr", [NEXP, 3, 128, 2048], BF16)
    lst_d = dscr("lst", [NEXP * CAP + 128, 4], F32, kind=dk)
    h2_scr = dscr("h2_scr", [ROWS, D], BF16, kind=dk)
    moe_scr = dscr("moe_scr", [2 * ROWS, D], BF16, kind=dk)
    x2_scr = dscr("x2_scr", [SEQ, D], F32, kind=dk)
    ys_scr = dscr("ys_scr", [4, 128, SEQ], BF16, kind="ExternalOutput" if dbg else "Internal")
    out_d = dscr("out", [SEQ, D], F32, kind="ExternalOutput")
    hT_scr = dscr("hT_scr", [8, 128, SEQ], BF16, kind="ExternalOutput" if dbg else "Internal")
    u_dbg = dscr("u_dbg", [4, 128, SEQ], BF16, kind="ExternalOutput") if dbg else None

    with ExitStack() as es:
        cx = Ctx(nc, es)
        al = Alloc(nc, es)
        block = es.enter_context(nc.Block())

        ident_bf = al.sb([128, 128], BF16, "ident_bf")
        cx.op("sp", lambda e: e.dma_start(out=ident_bf[:], in_=ident_bf_d), full=[ident_bf.b], dma=True)
        ident_f = al.sb([128, 128], F32, "ident_f")
        cx.op("sp", lambda e: e.dma_start(out=ident_f[:], in_=ident_f_d), full=[ident_f.b], dma=True)

        psum = [al.ps([128, 512], F32, f"bank{i}") for i in range(6)]
        psb = [al.ps([128, 1024], BF16, f"bankb{i}") for i in range(2)]
        pctr = [0]

        def getps():
            p = psum[pctr[0] % len(psum)]
            pctr[0] += 1
            return p

        alAB = Alloc(nc)
        u_all = alAB.sb([128, 4, 8, SEQ // 8], BF16, "u_all")
        M_all = alAB.sb([128, 32, 128], BF16, "M_all")
        W2r = alAB.sb([128, 16, 2, 128], BF16, "W2r"); W2i = alAB.sb([128, 16, 2, 128], BF16, "W2i")
        C1r = alAB.sb([128, 16, 128], BF16, "C1r"); nC1i = alAB.sb([128, 16, 128], BF16, "nC1i")
        KAr = alAB.sb([128, 9, 16], F32, "KAr"); KAi = alAB.sb([128, 9, 16], F32, "KAi")
        KnAi = alAB.sb([128, 9, 16], F32, "KnAi")
        psel = alAB.sb([128, 8, 240], BF16, "psel")
        zt = alAB.sb([128, 1024], F32, "zt")
        NPB = 3
        pst = [alAB.sb([128, 2048], F32, f"pst{i}") for i in range(NPB)]
        pbf = [alAB.sb([128, 2048], BF16, f"pbf{i}") for i in range(NPB)]
        wbfB = Buf("wbf")
        pc_next = [0]

        def precast(n, mode):
            for _ in range(n):
                ci = pc_next[0]
                if ci >= NEXP * 3:
                    return
                pc_next[0] += 1
                e_, m_ = ci // 3, ci % 3
                srcw = (weg_d, weu_d, wed_d)[m_][e_].rearrange("p c n -> p (c n)")
                s_ = pst[ci % NPB]; b_ = pbf[ci % NPB]
                dst = wbf_scr[e_, m_]
                if mode == "pool":
                    cx.op("pool", lambda e, s_=s_, srcw=srcw: e.dma_start(out=s_[:], in_=srcw), full=[s_.b], dma=True)
                    cx.op("pool", lambda e, s_=s_, b_=b_: e.tensor_copy(out=b_[:], in_=s_[:]), reads=[s_.b], full=[b_.b])
                    cx.op("pool", lambda e, b_=b_, dst=dst: e.dma_start(out=dst, in_=b_[:]), reads=[b_.b], dma=True)
                else:
                    cx.op("sp", lambda e, s_=s_, srcw=srcw: e.dma_start(out=s_[:], in_=srcw), full=[s_.b], dma=True)
                    cx.op("act", lambda e, s_=s_, b_=b_: e.copy(out=b_[:], in_=s_[:]), reads=[s_.b], full=[b_.b])
                    cx.op("act", lambda e, b_=b_, dst=dst: e.dma_start(out=dst, in_=b_[:]), reads=[b_.b], dma=True)
        cx.op("sp", lambda e: e.dma_start(out=psel[:], in_=psel_d), full=[psel.b], dma=True)
        al_outer = al
        al = Alloc(nc)
        gmix = al.sb([128, D], F32, "gmix")
        cx.op("sp", lambda e: e.dma_start(out=gmix[:], in_=gmix_d.partition_broadcast(128)),
              full=[gmix.b], dma=True)

        stg = [al.sb([128, 8, 256], F32, f"stg{i}") for i in range(2)]
        sctr = [0]

        def load_cast(dst, dst_col0, src_d, c0, c1, kch):
            for c in range(kch):
                for n0 in range(c0, c1, 2048):
                    w = min(2048, c1 - n0)
                    s = stg[sctr[0] % 2]
                    sctr[0] += 1
                    sf_ = s[:].rearrange("p c n -> p (c n)")
                    cx.op("sp", lambda e, sf_=sf_, c=c, n0=n0, w=w: e.dma_start(out=sf_[:, 0:w], in_=src_d[:, c, n0:n0 + w]),
                          full=[s.b], dma=True)
                    o = dst_col0 + (n0 - c0)
                    cx.op("pool", lambda e, sf_=sf_, c=c, o=o, w=w: e.tensor_copy(out=dst[:, c, o:o + w], in_=sf_[:, 0:w]),
                          reads=[s.b], writes=[dst.b])

        w_ssm_in = al.sb([128, 8, 512], BF16, "w_ssm_in")
        load_cast(w_ssm_in, 0, w_in_d, 1024, 1536, 8)
        NA_ = 4
        xt = [al.sb([128, D], F32, f"xt{i}") for i in range(NA_)]
        junk = al.sb([128, D], BF16, "junk")
        ss = [al.sb([128, 1], F32, f"ss{i}") for i in range(NA_)]
        rt = [al.sb([128, 1], F32, f"rt{i}") for i in range(NA_)]
        rstd = [al.sb([128, 1], F32, f"rstd{i}") for i in range(NA_)]
        hbf = [al.sb([128, D], BF16, f"hbf{i}") for i in range(NA_)]
        hTb = [al.sb([128, 8, T], BF16, f"hTb{i}") for i in range(2)]
        cx.op("pool", lambda e: e.memset(zt[:], 0.0), full=[zt.b])
        lstB = Buf("lst"); h2B = Buf("h2scr"); moeB = Buf("moescr"); x2B = Buf("x2scr")
        cx.op("pool", lambda e: e.dma_start(out=lst_d, in_=lst_init_d), full=[lstB], dma=True)
        cx.op("pool", lambda e: e.dma_start(out=h2_scr[SEQ:ROWS, :], in_=zt[:, 0:512].bitcast(BF16)),
              reads=[zt.b], writes=[h2B], dma=True)
        moe_flat = moe_scr.rearrange("(n p) d -> n p d", p=128)
        for n in range(0, 2 * ROWS // 128):
            tok = cx.op("pool", lambda e, n=n: e.dma_start(out=moe_flat[n], in_=zt[:, 0:512].bitcast(BF16)),
                        reads=[zt.b], dma=True)
            moeB.w[tok[0]] = tok[1]

        def a_front(i):
            p = i % NA_
            cx.op("sp", lambda e, p=p, i=i: e.dma_start(out=xt[p][:], in_=x_d[i * 128:(i + 1) * 128, :]),
                  full=[xt[p].b], dma=True)
            cx.op("act", lambda e, p=p: e.activation(out=junk[:], in_=xt[p][:], func=AF.Square,
                                                     accum_out=ss[p][:]),
                  reads=[xt[p].b], writes=[junk.b], full=[ss[p].b])
            cx.op("act", lambda e, p=p: e.activation(out=rt[p][:], in_=ss[p][:], func=AF.Sqrt,
                                                     scale=1.0 / D, bias=EPS),
                  reads=[ss[p].b], full=[rt[p].b])
            cx.op("dve", lambda e, p=p: e.reciprocal(out=rstd[p][:], in_=rt[p][:]),
                  reads=[rt[p].b], full=[rstd[p].b])
            cx.op("dve", lambda e, p=p: e.scalar_tensor_tensor(out=hbf[p][:], in0=xt[p][:],
                                                               scalar=rstd[p][:, 0:1], in1=gmix[:],
                                                               op0=ALU.mult, op1=ALU.mult),
                  reads=[xt[p].b, rstd[p].b, gmix.b], full=[hbf[p].b])

        def a_back(i):
            p = i % NA_
            blk = i // 4
            hb = hTb[blk % 2]
            pb = psb[i % 2]
            for c in range(8):
                cx.op("pe", lambda e, pb=pb, p=p, c=c: e.transpose(out=pb[:, c * 128:(c + 1) * 128],
                                                                   in_=hbf[p][:, c * 128:(c + 1) * 128],
                                                                   identity=ident_bf[:]),
                      reads=[hbf[p].b, ident_bf.b], writes=[pb.b])
            tt = i % 4
            cx.op("act", lambda e, pb=pb, hb=hb, tt=tt: e.copy(
                out=hb[:, :, tt * 128:(tt + 1) * 128],
                in_=pb[:].rearrange("p (c t) -> p c t", c=8)),
                reads=[pb.b], writes=[hb.b])
            if tt == 3:
                for f in range(4):
                    ps = getps()
                    for c in range(8):
                        cx.op("pe", lambda e, ps=ps, hb=hb, f=f, c=c: e.matmul(
                            ps[:], lhsT=w_ssm_in[:, c, f * 128:(f + 1) * 128], rhs=hb[:, c, :],
                            start=(c == 0), stop=(c == 7)),
                            reads=[w_ssm_in.b, hb.b], writes=[ps.b])
                    cx.op("dve", lambda e, ps=ps, f=f, blk=blk: e.tensor_copy(
                        out=u_all[:, f, :, blk * (T // 8):(blk + 1) * (T // 8)],
                        in_=ps[:].rearrange("p (c k) -> p k c", k=8)),
                        reads=[ps.b], writes=[u_all.b])
                cx.op("act", lambda e, hb=hb, blk=blk: e.dma_start(
                    out=hT_scr[:, :, blk * T:(blk + 1) * T].rearrange("c p t -> p c t"), in_=hb[:]),
                    reads=[hb.b], dma=True)

        a_front(0); a_front(1)
        for i in range(NT):
            if i + 2 < NT:
                a_front(i + 2)
            a_back(i)
            if i % 3 == 2:
                precast(1, "pool")

        if dbg:
            cx.op("sp", lambda e: e.dma_start(out=u_dbg.rearrange("f p t -> p f t"), in_=u_all[:].rearrange("p f k c -> p f (k c)")),
                  reads=[u_all.b], dma=True)


        cx.barrier(skip_pool_dma=True)
        al.close()
        precast(8, "pool")
        al = Alloc(nc)
        TWO_PI = 2.0 * np.pi
        cmask = al.sb([128, 128], F32, "cmask")
        cx.op("sp", lambda e: e.dma_start(out=cmask[:], in_=cmask_d), full=[cmask.b], dma=True)
        dl = al.sb([128, 32], F32, "dl")
        cx.op("sp", lambda e: e.dma_start(out=dl[:], in_=dl_d), full=[dl.b], dma=True)
        SU = Buf("ssm_setup")

        def sload(shape, src, name):
            t = al.sb(shape, F32, name)
            cx.op("sp", lambda e: e.dma_start(out=t[:], in_=src), full=[t.b], dma=True)
            return t

        lamre = sload([128, 16], lamre_d, "lamre")
        lamim = sload([128, 16], lamim_d, "lamim")
        logdt = sload([128, 16], logdt_d, "logdt")
        Bre = sload([128, 16, 16], bre_d, "Bre")
        Bim = sload([128, 16, 16], bim_d, "Bim")
        Cre = sload([128, 16, 16], cre_d, "Cre")
        Cim = sload([128, 16, 16], cim_d, "Cim")
        ins_b = [lamre.b, lamim.b, logdt.b, Bre.b, Bim.b, Cre.b, Cim.b]

        def S(shape, name):
            return al.sb(shape, F32, name)

        def dv(fn):
            cx.op("dve", fn, reads=ins_b, writes=[SU])

        def ac(fn):
            cx.op("act", fn, reads=ins_b, writes=[SU])

        def tt_(out, a, b, op):
            dv(lambda e: e.tensor_tensor(out=out, in0=a, in1=b, op=op))

        sh16 = [128, 16]
        dt_ = S(sh16, "dt"); lrd = S(sh16, "lrd"); th = S(sh16, "th")
        ac(lambda e: e.activation(out=dt_[:], in_=logdt[:], func=AF.Exp))
        tt_(lrd[:], lamre[:], dt_[:], ALU.mult)
        tt_(th[:], lamim[:], dt_[:], ALU.mult)
        mag = S(sh16, "mag"); imag2 = S(sh16, "imag2")
        ac(lambda e: e.activation(out=mag[:], in_=lrd[:], func=AF.Exp))
        ac(lambda e: e.activation(out=imag2[:], in_=lrd[:], func=AF.Exp, scale=-2.0))
        kq_i = al.sb(sh16, I32, "kq_i"); kq = S(sh16, "kq"); red = S(sh16, "red"); msk = S(sh16, "msk")
        sinv = S(sh16, "sinv"); cosv = S(sh16, "cosv"); tmpa = S(sh16, "tmpa")

        def sin_of(outt, shift):
            dv(lambda e: e.tensor_scalar(out=tmpa[:], in0=th[:], scalar1=float(shift), scalar2=None,
                                         op0=ALU.add))
            dv(lambda e: e.tensor_scalar(out=kq[:], in0=tmpa[:], scalar1=float(1.0 / TWO_PI),
                                         scalar2=None, op0=ALU.mult))
            dv(lambda e: e.tensor_copy(out=kq_i[:], in_=kq[:]))
            dv(lambda e: e.tensor_copy(out=kq[:], in_=kq_i[:]))
            dv(lambda e: e.scalar_tensor_tensor(out=red[:], in0=kq[:], scalar=float(-TWO_PI),
                                                in1=tmpa[:], op0=ALU.mult, op1=ALU.add))
            dv(lambda e: e.tensor_single_scalar(out=msk[:], in_=red[:], scalar=float(np.pi), op=ALU.is_gt))
            dv(lambda e: e.scalar_tensor_tensor(out=red[:], in0=msk[:], scalar=float(-TWO_PI),
                                                in1=red[:], op0=ALU.mult, op1=ALU.add))
            dv(lambda e: e.tensor_single_scalar(out=msk[:], in_=red[:], scalar=float(-np.pi), op=ALU.is_lt))
            dv(lambda e: e.scalar_tensor_tensor(out=red[:], in0=msk[:], scalar=float(TWO_PI),
                                                in1=red[:], op0=ALU.mult, op1=ALU.add))
            ac(lambda e: e.activation(out=outt[:], in_=red[:], func=AF.Sin))

        sin_of(sinv, 0.0)
        sin_of(cosv, np.pi / 2)
        PWr = S([128, 9, 16], "PWr"); PWi = S([128, 9, 16], "PWi")
        IPr = S([128, 8, 16], "IPr"); IPi = S([128, 8, 16], "IPi")
        t1 = S([128, 16, 8, 16], "t1"); t2 = S([128, 16, 8, 16], "t2")

        def cmul(outr, outi, ar, ai, br, bi, shp, neg_i=False):
            a1 = t1[:].rearrange("p a b c -> p (a b c)")[:, 0:int(np.prod(shp[1:]))]
            a2 = t2[:].rearrange("p a b c -> p (a b c)")[:, 0:int(np.prod(shp[1:]))]
            if len(shp) == 3:
                a1 = a1.rearrange("p (a b) -> p a b", a=shp[1])
                a2 = a2.rearrange("p (a b) -> p a b", a=shp[1])
            if len(shp) == 4:
                a1 = t1[:, :, 0:shp[2], :]
                a2 = t2[:, :, 0:shp[2], :]
            tt_(a1, ar, br, ALU.mult)
            tt_(a2, ai, bi, ALU.mult)
            tt_(outr, a1, a2, ALU.subtract)
            tt_(a1, ar, bi, ALU.mult)
            tt_(a2, ai, br, ALU.mult)
            if neg_i:
                dv(lambda e: e.scalar_tensor_tensor(out=outi, in0=a1, scalar=-1.0, in1=a2,
                                                    op0=ALU.mult, op1=ALU.subtract))
            else:
                tt_(outi, a1, a2, ALU.add)

        dv(lambda e: e.memset(PWr[:, 0, :], 1.0))
        dv(lambda e: e.memset(PWi[:, 0, :], 0.0))
        dv(lambda e: e.memset(IPr[:, 0, :], 1.0))
        dv(lambda e: e.memset(IPi[:, 0, :], 0.0))
        tt_(PWr[:, 1, :], mag[:], cosv[:], ALU.mult)
        tt_(PWi[:, 1, :], mag[:], sinv[:], ALU.mult)
        tt_(IPr[:, 1, :], PWr[:, 1, :], imag2[:], ALU.mult)
        dv(lambda e: e.scalar_tensor_tensor(out=IPi[:, 1, :], in0=PWi[:, 1, :], scalar=-1.0, in1=imag2[:],
                                            op0=ALU.mult, op1=ALU.mult))
        def bn(a, n):
            return a.unsqueeze(1).to_broadcast([128, n, 16])

        for (Pr, Pi, top) in ((PWr, PWi, 9), (IPr, IPi, 8)):
            cmul(Pr[:, 2, :], Pi[:, 2, :], Pr[:, 1, :], Pi[:, 1, :], Pr[:, 1, :], Pi[:, 1, :], sh16)
            cmul(Pr[:, 3:5, :], Pi[:, 3:5, :], Pr[:, 1:3, :], Pi[:, 1:3, :], bn(Pr[:, 2, :], 2), bn(Pi[:, 2, :], 2),
                 [128, 2, 16])
            hi_ = top - 5
            cmul(Pr[:, 5:top, :], Pi[:, 5:top, :], Pr[:, 1:1 + hi_, :], Pi[:, 1:1 + hi_, :],
                 bn(Pr[:, 4, :], hi_), bn(Pi[:, 4, :], hi_), [128, hi_, 16])
        KAB = Buf("ka_chain")
        tk1 = S(sh16, "tk1"); tk2 = S(sh16, "tk2")
        ka_pending = []

        def ka(fn, rd=(), first=False):
            ka_pending.append((fn, [KAB] + list(rd)))

        ka(lambda e: e.tensor_copy(out=KAr[:, 0, :], in_=PWr[:, 8, :]), [SU])
        ka(lambda e: e.tensor_copy(out=KAi[:, 0, :], in_=PWi[:, 8, :]), [SU])
        for d_ in range(1, 9):
            pr, pi_ = KAr[:, d_ - 1, :], KAi[:, d_ - 1, :]
            nr, ni = KAr[:, d_, :], KAi[:, d_, :]
            ka(lambda e, pr=pr: e.tensor_tensor(out=tk1[:], in0=pr, in1=pr, op=ALU.mult))
            ka(lambda e, pi_=pi_: e.tensor_tensor(out=tk2[:], in0=pi_, in1=pi_, op=ALU.mult))
            ka(lambda e, nr=nr: e.tensor_tensor(out=nr, in0=tk1[:], in1=tk2[:], op=ALU.subtract))
            ka(lambda e, pr=pr, pi_=pi_: e.tensor_tensor(out=tk1[:], in0=pr, in1=pi_, op=ALU.mult))
            ka(lambda e, ni=ni: e.tensor_scalar(out=ni, in0=tk1[:], scalar1=2.0, scalar2=None, op0=ALU.mult))

        def ka_drain(n):
            for _ in range(n):
                if ka_pending:
                    fn, rd = ka_pending.pop(0)
                    cx.op("dve", fn, reads=rd, writes=[KAB])

        _dv_plain = dv

        def dv(fn):
            _dv_plain(fn)
            ka_drain(1)

        def tt_(out, a, b, op):
            dv(lambda e: e.tensor_tensor(out=out, in0=a, in1=b, op=op))

        am1 = S(sh16, "am1"); l2 = S(sh16, "l2"); il2 = S(sh16, "il2"); kr = S(sh16, "kr"); ki = S(sh16, "ki")
        dv(lambda e: e.tensor_scalar(out=am1[:], in0=PWr[:, 1, :], scalar1=-1.0, scalar2=None, op0=ALU.add))
        tt_(l2[:], lamre[:], lamre[:], ALU.mult)
        tt_(tmpa[:], lamim[:], lamim[:], ALU.mult)
        tt_(l2[:], l2[:], tmpa[:], ALU.add)
        dv(lambda e: e.reciprocal(out=il2[:], in_=l2[:]))
        tt_(kr[:], am1[:], lamre[:], ALU.mult)
        tt_(tmpa[:], PWi[:, 1, :], lamim[:], ALU.mult)
        tt_(kr[:], kr[:], tmpa[:], ALU.add)
        tt_(kr[:], kr[:], il2[:], ALU.mult)
        tt_(ki[:], PWi[:, 1, :], lamre[:], ALU.mult)
        tt_(tmpa[:], am1[:], lamim[:], ALU.mult)
        tt_(ki[:], ki[:], tmpa[:], ALU.subtract)
        tt_(ki[:], ki[:], il2[:], ALU.mult)
        sh3 = [128, 16, 16]

        def bc(a):
            return a.unsqueeze(2).to_broadcast(sh3)

        Bbr = S(sh3, "Bbr"); Bbi = S(sh3, "Bbi")
        cmul(Bbr[:], Bbi[:], bc(kr[:]), bc(ki[:]), Bre[:], Bim[:], sh3)
        Bhr = S([128, 16, 8, 16], "Bhr"); nBhi = S([128, 16, 8, 16], "nBhi"); Bhi = S([128, 16, 8, 16], "Bhi")
        Btr = S([128, 16, 8, 16], "Btr"); Bti = S([128, 16, 8, 16], "Bti")
        Chr = S([128, 16, 9, 16], "Chr"); Chi = S([128, 16, 9, 16], "Chi"); nChi = S([128, 16, 9, 16], "nChi")
        sh4 = [128, 16, 8, 16]

        def bk(a):
            return a.rearrange("p k r -> p r k").unsqueeze(3).to_broadcast(sh4)

        def bmid(a):
            return a.unsqueeze(2).to_broadcast(sh4)

        def b2(a):
            return a.unsqueeze(2).unsqueeze(3).to_broadcast(sh4)

        cmul(Bhr[:], Bhi[:], bk(IPr[:]), bk(IPi[:]), bmid(Bbr[:]), bmid(Bbi[:]), sh4)
        cmul(Btr[:], Bti[:], b2(PWr[:, 7, :]), b2(PWi[:, 7, :]), Bhr[:], Bhi[:], sh4)
        dv(lambda e: e.tensor_scalar(out=nBhi[:], in0=Bhi[:], scalar1=-1.0, scalar2=None, op0=ALU.mult))
        cmul(Chr[:, :, 0:8, :], Chi[:, :, 0:8, :], bk(PWr[:, 0:8, :]), bk(PWi[:, 0:8, :]), bmid(Cre[:]), bmid(Cim[:]), sh4)
        cmul(Chr[:, :, 8, :], Chi[:, :, 8, :], bc(PWr[:, 8, :]), bc(PWi[:, 8, :]), Cre[:], Cim[:], sh3)
        dv(lambda e: e.tensor_scalar(out=nChi[:], in0=Chi[:], scalar1=-1.0, scalar2=None, op0=ALU.mult))
        dv(lambda e: e.tensor_copy(out=C1r[:].rearrange("p r (j c) -> p r j c", j=8), in_=Chr[:, :, 1:9, :]))
        dv(lambda e: e.tensor_copy(out=nC1i[:].rearrange("p r (j c) -> p r j c", j=8), in_=nChi[:, :, 1:9, :]))
        ka_drain(10 ** 6)
        cx.op("dve", lambda e: e.tensor_scalar(out=KnAi[:], in0=KAi[:], scalar1=-1.0, scalar2=None, op0=ALU.mult),
              reads=[KAB], writes=[SU, KAB])
        mtmps = [S([128, 128], "mtmp0"), S([128, 128], "mtmp1")]
        cx.op("dve", lambda e: e.memset(W2r[:], 0.0), reads=ins_b, writes=[SU])
        cx.op("dve", lambda e: e.memset(W2i[:], 0.0), reads=ins_b, writes=[SU])
        for r in range(16):
            for two in range(2):
                g = 2 * r + two
                rng = slice(two * 64, (two + 1) * 64)
                ps = getps()
                cx.op("pe", lambda e, ps=ps, r=r, rng=rng: e.matmul(
                    ps[:, 0:128], lhsT=Bhr[rng, r, :, :].rearrange("p k c -> p (k c)"),
                    rhs=Chr[rng, r, 0:8, :].rearrange("p j c -> p (j c)"), start=True, stop=False),
                    reads=[SU], writes=[ps.b])
                cx.op("pe", lambda e, ps=ps, r=r, rng=rng: e.matmul(
                    ps[:, 0:128], lhsT=nBhi[rng, r, :, :].rearrange("p k c -> p (k c)"),
                    rhs=Chi[rng, r, 0:8, :].rearrange("p j c -> p (j c)"), start=False, stop=True),
                    reads=[SU], writes=[ps.b])
                mt_ = mtmps[g % 2]
                cx.op("dve", lambda e, ps=ps, mt_=mt_: e.tensor_tensor(out=mt_[:], in0=ps[:, 0:128], in1=cmask[:],
                                                                       op=ALU.mult),
                      reads=[ps.b, cmask.b], full=[mt_.b])
                cx.op("dve", lambda e, g=g, mt_=mt_: e.scalar_tensor_tensor(
                    out=M_all[:, g, :], in0=ident_f[:], scalar=dl[:, g:g + 1], in1=mt_[:],
                    op0=ALU.mult, op1=ALU.add),
                    reads=[ident_f.b, dl.b, mt_.b], writes=[M_all.b])
            for (Bt, W2) in ((Btr, W2r), (Bti, W2i)):
                ps = getps()
                cx.op("pe", lambda e, ps=ps, r=r, Bt=Bt: e.transpose(
                    out=ps[:, 0:128], in_=Bt[:, r, :, :].rearrange("p k c -> p (k c)"), identity=ident_f[:]),
                    reads=[SU, ident_f.b], writes=[ps.b])
                cx.op("act", lambda e, ps=ps, r=r, W2=W2: e.copy(out=W2[:, r, 0, 0:64], in_=ps[:, 0:64]),
                      reads=[ps.b], writes=[W2.b])
                cx.op("act", lambda e, ps=ps, r=r, W2=W2: e.copy(out=W2[:, r, 1, 64:128], in_=ps[:, 64:128]),
                      reads=[ps.b], writes=[W2.b])

        cx.barrier(skip_pool_dma=True)
        al.close()
        al = Alloc(nc)
        NCH = SEQ // 8
        Vg = [al.sb([128, NCH], BF16, f"Vg{i}") for i in range(8)]
        Sre = [[al.sb([128, NCH], F32, f"Sre{s}{i}") for i in range(2)] for s in range(2)]
        Sim = [[al.sb([128, NCH], F32, f"Sim{s}{i}") for i in range(2)] for s in range(2)]
        Sbr = [al.sb([128, NCH], BF16, f"Sbr{s}") for s in range(4)]
        Sbi = [al.sb([128, NCH], BF16, f"Sbi{s}") for s in range(4)]
        SreF = [Sre[0][0], Sre[0][1], Sre[1][0], Sre[1][1]]
        SimF = [Sim[0][0], Sim[0][1], Sim[1][0], Sim[1][1]]
        Gg = [al.sb([128, NCH], BF16, f"Gg{i}") for i in range(16)]
        ysf = [al.sb([128, SEQ], BF16, "ysf0")] * 2
        for s in range(4):
            cx.op("pool", lambda e, s=s: e.memset(Sbr[s][:, 0:1], 0.0), writes=[Sbr[s].b])
            cx.op("pool", lambda e, s=s: e.memset(Sbi[s][:, 0:1], 0.0), writes=[Sbi[s].b])

        def b_front(r):
            f = r // 4
            st = r % 4
            vg = [Vg[(2 * r) % 8], Vg[(2 * r + 1) % 8]]
            for two in range(2):
                g = 2 * r + two
                gl = g % 8
                ps = getps()
                for k in range(8):
                    cx.op("pe", lambda e, ps=ps, gl=gl, k=k, f=f: e.matmul(
                        ps[:], lhsT=psel[:, gl, (7 - k) * 16:(7 - k) * 16 + 128],
                        rhs=u_all[:, f, k, :], start=(k == 0), stop=(k == 7)),
                        reads=[psel.b, u_all.b], writes=[ps.b])
                cx.op("act", lambda e, ps=ps, v=vg[two]: e.copy(out=v[:], in_=ps[:]),
                      reads=[ps.b], full=[vg[two].b])
            psr = getps(); psi = getps()
            for (pp, W2) in ((psr, W2r), (psi, W2i)):
                for two in range(2):
                    cx.op("pe", lambda e, pp=pp, W2=W2, two=two, r=r, v=vg[two]: e.matmul(
                        pp[:], lhsT=W2[:, r, two, :], rhs=v[:], start=(two == 0), stop=(two == 1)),
                        reads=[W2.b, vg[two].b], writes=[pp.b])
            cx.op("act", lambda e, psr=psr, st=st: e.copy(out=SreF[st][:], in_=psr[:]),
                  reads=[psr.b], full=[SreF[st].b])
            cx.op("act", lambda e, psi=psi, st=st: e.copy(out=SimF[st][:], in_=psi[:]),
                  reads=[psi.b], full=[SimF[st].b])

        def b_mid2(rs):
            def level(r, t0_, s0_, step, cnt, d_):
                xr, xi = SreF[r % 4], SimF[r % 4]
                tr = xr[:, t0_:t0_ + (cnt - 1) * step + 1:step]
                ti_ = xi[:, t0_:t0_ + (cnt - 1) * step + 1:step]
                sr = xr[:, s0_:s0_ + (cnt - 1) * step + 1:step]
                si_ = xi[:, s0_:s0_ + (cnt - 1) * step + 1:step]
                return [
                    (lambda e: e.scalar_tensor_tensor(out=tr, in0=sr, scalar=KAr[:, d_, r:r + 1], in1=tr,
                                                      op0=ALU.mult, op1=ALU.add), [xr.b, SU], [xr.b]),
                    (lambda e: e.scalar_tensor_tensor(out=ti_, in0=si_, scalar=KAr[:, d_, r:r + 1], in1=ti_,
                                                      op0=ALU.mult, op1=ALU.add), [xi.b, SU], [xi.b]),
                    (lambda e: e.scalar_tensor_tensor(out=tr, in0=si_, scalar=KnAi[:, d_, r:r + 1], in1=tr,
                                                      op0=ALU.mult, op1=ALU.add), [xi.b, SU], [xr.b]),
                    (lambda e: e.scalar_tensor_tensor(out=ti_, in0=sr, scalar=KAi[:, d_, r:r + 1], in1=ti_,
                                                      op0=ALU.mult, op1=ALU.add), [xr.b, SU], [xi.b]),
                ]

            plan = []
            for d_ in range(9):
                half, step = 1 << d_, 2 << d_
                plan.append((step - 1, half - 1, step, NCH // step, d_))
            for d_ in range(7, -1, -1):
                half, step = 1 << d_, 2 << d_
                plan.append((step + half - 1, step - 1, step, NCH // step - 1, d_))
            for (t0_, s0_, step, cnt, d_) in plan:
                ops = [level(r, t0_, s0_, step, cnt, d_) for r in rs]
                for q in range(4):
                    for o in ops:
                        fn, rd_, wr_ = o[q]
                        cx.op("dve", fn, reads=rd_, writes=wr_)
            for r in rs:
                st = r % 4
                fr, fi = SreF[st], SimF[st]
                cx.op("pool", lambda e, fr=fr, st=st: e.tensor_copy(out=Sbr[st][:, 1:NCH], in_=fr[:, 0:NCH - 1]),
                      reads=[fr.b], writes=[Sbr[st].b])
                cx.op("pool", lambda e, fi=fi, st=st: e.tensor_copy(out=Sbi[st][:, 1:NCH], in_=fi[:, 0:NCH - 1]),
                      reads=[fi.b], writes=[Sbi[st].b])

        def b_back(r):
            f = r // 4
            st = r % 4
            vg = [Vg[(2 * r) % 8], Vg[(2 * r + 1) % 8]]
            for two in range(2):
                g = 2 * r + two
                rng = slice(two * 64, (two + 1) * 64)
                ps = getps()
                cx.op("pe", lambda e, ps=ps, g=g, v=vg[two]: e.matmul(
                    ps[:], lhsT=M_all[:, g, :], rhs=v[:], start=True, stop=False),
                    reads=[M_all.b, vg[two].b], writes=[ps.b])
                cx.op("pe", lambda e, ps=ps, r=r, rng=rng, st=st: e.matmul(
                    ps[:], lhsT=C1r[rng, r, :], rhs=Sbr[st][rng, :], start=False, stop=False),
                    reads=[SU, Sbr[st].b], writes=[ps.b])
                cx.op("pe", lambda e, ps=ps, r=r, rng=rng, st=st: e.matmul(
                    ps[:], lhsT=nC1i[rng, r, :], rhs=Sbi[st][rng, :], start=False, stop=True),
                    reads=[SU, Sbi[st].b], writes=[ps.b])
                gg = Gg[g % 16]
                cx.op("act", lambda e, ps=ps, gg=gg: e.activation(out=gg[:], in_=ps[:], func=GELU),
                      reads=[ps.b], full=[gg.b])
            if r % 4 == 3:
                yb = ysf[f % 2]
                for j in range(8):
                    ps = getps()
                    for gl in range(8):
                        gg = Gg[(8 * f + gl) % 16]
                        cx.op("pe", lambda e, ps=ps, j=j, gl=gl, gg=gg: e.matmul(
                            ps[:], lhsT=psel[:, j, (7 - gl) * 16:(7 - gl) * 16 + 128], rhs=gg[:],
                            start=(gl == 0), stop=(gl == 7)),
                            reads=[psel.b, gg.b], writes=[ps.b])
                    cx.op("act", lambda e, ps=ps, yb=yb, j=j: e.copy(out=yb[:, j:SEQ:8], in_=ps[:]),
                          reads=[ps.b], writes=[yb.b])
                cx.op("sp", lambda e, yb=yb, f=f: e.dma_start(out=ys_scr[f], in_=yb[:]),
                      reads=[yb.b], dma=True)

        b_front(0); b_front(1)
        for g2 in range(8):
            precast(4, "act")
            b_mid2([2 * g2, 2 * g2 + 1])
            precast(3, "act")
            if g2 + 1 < 8:
                b_front(2 * g2 + 2)
                b_front(2 * g2 + 3)
            precast(3, "act")
            b_back(2 * g2)
            b_back(2 * g2 + 1)
        precast(NEXP * 3, "act")

        cx.barrier()
        al.close()
        alAB.close()
        al = al_outer
        if stop_after in ("A", "B"):
            pass
        else:
            TC = 256
            NBC = SEQ // TC
            alC = Alloc(nc)
            wA = alC.sb([128, 8, 1536], BF16, "wA")
            wG = alC.sb([128, 8, 3072], BF16, "wG")
            wco = alC.sb([128, 4, 1024], BF16, "wco")
            wgl = alC.sb([128, 4, 2048], BF16, "wgl")
            wmo = alC.sb([128, 4, 1024], BF16, "wmo")
            wo = alC.sb([128, 8, 1024], BF16, "wo")
            Dg2 = [alC.sb([128, 31, 128], BF16, f"Dg{i}") for i in range(2)]
            kT = alC.sb([128, 4, 256], BF16, "kT")
            vtok = alC.sb([128, 2, 512], BF16, "vtok")
            gffn = alC.sb([128, D], F32, "gffn")
            wr = alC.sb([128, 8, 36], F32, "wr")
            rbias = alC.sb([128, 36], F32, "rbias")
            cdw = alC.sb([128, 4, 31], F32, "cdw")
            cb = alC.sb([128, 4], F32, "cb"); lng = alC.sb([128, 4], F32, "lng"); lnb = alC.sb([128, 4], F32, "lnb")
            onesm = alC.sb([128, 128], F32, "onesm")
            ones_bf = alC.sb([128, 128], BF16, "ones_bf")
            ecap = alC.sb([128, 32], F32, "ecap")
            tokid = alC.sb([128, NT], F32, "tokid")
            cum = alC.sb([128, 32], F32, "cum")
            lg_all = alC.sb([128, NT, 36], F32, "lg_all")
            trashp = alC.sb([128, 1], F32, "trashp")

            def ld(t, src):
                cx.op("sp", lambda e: e.dma_start(out=t[:], in_=src), full=[t.b], dma=True)

            ld(gffn, gffn_d.partition_broadcast(128))
            ld(wr, wr_d.rearrange("(c p) n -> p c n", p=128))
            ld(rbias, rbias_d.partition_broadcast(128))
            ld(cdw, cdw_d); ld(cb, cb_d); ld(lng, lng_d); ld(lnb, lnb_d)
            ld(ecap, ecap_d); ld(tokid, tokid_d); ld(trashp, trashp_d)
            cx.op("pool", lambda e: e.memset(onesm[:], 1.0 / 512.0), full=[onesm.b])
            cx.op("pool", lambda e: e.memset(ones_bf[:], 1.0), full=[ones_bf.b])
            cx.op("pool", lambda e: e.memset(cum[:], 0.0), full=[cum.b])
            alS = Alloc(nc)
            stg2 = [alS.sb([128, 8, 256], F32, f"stgc{i}") for i in range(3)]
            s2 = [0]

            def load_cast2(dst, dst_col0, src_d, c0, c1, kch, engs=("pool", "act", "dve")):
                for c in range(kch):
                    for n0 in range(c0, c1, 2048):
                        w = min(2048, c1 - n0)
                        s = stg2[s2[0] % len(stg2)]
                        eng = engs[s2[0] % len(engs)]
                        s2[0] += 1
                        sf_ = s[:].rearrange("p c n -> p (c n)")
                        cx.op("sp", lambda e, sf_=sf_, c=c, n0=n0, w=w: e.dma_start(out=sf_[:, 0:w], in_=src_d[:, c, n0:n0 + w]),
                              full=[s.b], dma=True)
                        o = dst_col0 + (n0 - c0)
                        if eng == "act":
                            cx.op("act", lambda e, sf_=sf_, c=c, o=o, w=w: e.copy(out=dst[:, c, o:o + w], in_=sf_[:, 0:w]),
                                  reads=[s.b], writes=[dst.b])
                        else:
                            cx.op(eng, lambda e, sf_=sf_, c=c, o=o, w=w: e.tensor_copy(out=dst[:, c, o:o + w], in_=sf_[:, 0:w]),
                                  reads=[s.b], writes=[dst.b])

            load_cast2(wA, 0, w_in_d, 0, 1024, 8)
            load_cast2(wA, 1024, w_in_d, 1536, 2048, 8)
            load_cast2(wG, 0, w_in_d, 2048, 5120, 8)
            load_cast2(wco, 0, wco_d, 0, 1024, 4)
            load_cast2(wgl, 0, wgl_d, 0, 2048, 4)
            load_cast2(wmo, 0, wmo_d, 0, 1024, 4)
            load_cast2(wo, 0, wo_d, 0, 1024, 8)
            wkv = alS.sb([128, 8, 1024], BF16, "wkv")
            load_cast2(wkv, 0, wkv_d, 0, 1024, 8)
            gmem = alS.sb([128, D], F32, "gmem")
            ld(gmem, gmem_d.partition_broadcast(128))
            memT = alS.sb([128, 8, 256], BF16, "memT")
            mx = alS.sb([128, D], F32, "mx")
            mss = alS.sb([128, 1], F32, "mss"); mrt = alS.sb([128, 1], F32, "mrt"); mrs = alS.sb([128, 1], F32, "mrs")
            mh = alS.sb([128, D], BF16, "mh")
            mjunk = mh
            for mt in range(2):
                cx.op("sp", lambda e, mt=mt: e.dma_start(out=mx[:], in_=mem_d[mt * 128:(mt + 1) * 128, :]),
                      full=[mx.b], dma=True)
                cx.op("act", lambda e: e.activation(out=mjunk[:], in_=mx[:], func=AF.Square, accum_out=mss[:]),
                      reads=[mx.b], writes=[mjunk.b], full=[mss.b])
                cx.op("act", lambda e: e.activation(out=mrt[:], in_=mss[:], func=AF.Sqrt, scale=1.0 / D, bias=EPS),
                      reads=[mss.b], full=[mrt.b])
                cx.op("dve", lambda e: e.reciprocal(out=mrs[:], in_=mrt[:]), reads=[mrt.b], full=[mrs.b])
                cx.op("dve", lambda e: e.scalar_tensor_tensor(out=mh[:], in0=mx[:], scalar=mrs[:, 0:1], in1=gmem[:],
                                                              op0=ALU.mult, op1=ALU.mult),
                      reads=[mx.b, mrs.b, gmem.b], full=[mh.b])
                pb = psb[mt % 2]
                for c in range(8):
                    cx.op("pe", lambda e, pb=pb, c=c: e.transpose(out=pb[:, c * 128:(c + 1) * 128],
                                                                  in_=mh[:, c * 128:(c + 1) * 128],
                                                                  identity=ident_bf[:]),
                          reads=[mh.b, ident_bf.b], writes=[pb.b])
                cx.op("act", lambda e, pb=pb, mt=mt: e.copy(out=memT[:, :, mt * 128:(mt + 1) * 128],
                                                            in_=pb[:].rearrange("p (c t) -> p c t", c=8)),
                      reads=[pb.b], writes=[memT.b])
            for hd in range(4):
                ps = getps()
                for c in range(8):
                    cx.op("pe", lambda e, ps=ps, c=c, hd=hd: e.matmul(
                        ps[:, 0:256], lhsT=wkv[:, c, hd * 128:(hd + 1) * 128], rhs=memT[:, c, :],
                        start=(c == 0), stop=(c == 7)), reads=[wkv.b, memT.b], writes=[ps.b])
                cx.op("dve", lambda e, ps=ps, hd=hd: e.tensor_copy(out=kT[:, hd, :], in_=ps[:, 0:256]),
                      reads=[ps.b], writes=[kT.b])
            for mc in range(2):
                ps = getps()
                for c in range(8):
                    cx.op("pe", lambda e, ps=ps, c=c, mc=mc: e.matmul(
                        ps[:], lhsT=memT[:, c, mc * 128:(mc + 1) * 128], rhs=wkv[:, c, 512:1024],
                        start=(c == 0), stop=(c == 7)), reads=[wkv.b, memT.b], writes=[ps.b])
                cx.op("dve", lambda e, ps=ps, mc=mc: e.tensor_copy(out=vtok[:, mc, :], in_=ps[:]),
                      reads=[ps.b], writes=[vtok.b])
            cx.barrier()
            alS.close()

            alW = Alloc(nc)
            hT = [alW.sb([128, 8, TC], BF16, f"hTc{i}") for i in range(2)]
            ysb = [alW.sb([128, 4, TC], BF16, "ysb0")] * 2
            vbuf = alW.sb([128, 4, 30 + TC], BF16, "vbuf")
            sgt = [alW.sb([128, TC], F32, f"sgt{i}") for i in range(3)]
            cv = alW.sb([128, 4, TC], F32, "cv")
            sq = [sgt[1], sgt[2]]
            mean = alW.sb([128, TC], F32, "mean")
            var = alW.sb([128, TC], F32, "var"); lrs = alW.sb([128, TC], F32, "lrs")
            m2 = var; lnv = lrs
            cn = alW.sb([128, 4, TC], BF16, "cn")
            qb = alW.sb([128, 4, TC], BF16, "qb")
            Eb = [alW.sb([128, 2, TC], BF16, f"Eb{i}") for i in range(2)]
            ob = alW.sb([128, 4, TC], BF16, "ob")
            macc = alW.sb([128, TC], F32, "macc"); mt1 = alW.sb([128, TC], F32, "mt1"); mt2 = alW.sb([128, TC], F32, "mt2")
            rden = macc
            xc = [mt1, mt2]
            sqf = [sgt[1], sgt[2], mt1, mt2]
            merged = alW.sb([128, 8, TC], BF16, "merged")
            xt2 = [alW.sb([128, D], F32, f"xtc{i}") for i in range(2)]
            x2t = xt2
            h2f = alW.sb([128, D], F32, "h2f"); h2b = [alW.sb([128, D], BF16, "h2b0")] * 2
            junk2 = h2b[0]
            h2T = alW.sb([128, 8, 128], F32, "h2T")
            ss2 = alW.sb([128, 1], F32, "ss2"); rt2 = alW.sb([128, 1], F32, "rt2"); rs2 = alW.sb([128, 1], F32, "rs2")
            cx.op("pool", lambda e: e.memset(vbuf[:], 0.0), full=[vbuf.b])

            breg = {}

            def mmgrp(ps_ap, ps_b, pairs, reads):
                n = len(pairs)
                for idx, (l, r_) in enumerate(pairs):
                    cx.op("pe", lambda e, l=l, r_=r_, idx=idx: e.matmul(ps_ap, lhsT=l, rhs=r_, start=(idx == 0),
                                                                         stop=(idx == n - 1)),
                          reads=reads, writes=[ps_b])

            KCUT = int(os.environ.get("KCUT", "9"))
            KNB = int(os.environ.get("KNB", str(NBC)))
            def c_load_h(bi):
                t0 = bi * TC
                h = hT[bi % 2]
                cx.op("sp", lambda e, h=h, t0=t0: e.dma_start(
                    out=h[:], in_=hT_scr[:, :, t0:t0 + TC].rearrange("c p t -> p c t")), full=[h.b], dma=True)

            def c_load_y(bi):
                t0 = bi * TC
                yb = ysb[bi % 2]
                cx.op("sp", lambda e, yb=yb, t0=t0: e.dma_start(
                    out=yb[:], in_=ys_scr[:, :, t0:t0 + TC].rearrange("f p t -> p f t")), full=[yb.b], dma=True)

            def c_s2(bi):
                t0 = bi * TC
                h = hT[bi % 2]; yb = ysb[bi % 2]
                for f in range(4):
                    pa = getps(); pg = getps()
                    mmgrp(pa[:, 0:TC], pa.b, [(wA[:, c, f * 128:(f + 1) * 128], h[:, c, :]) for c in range(8)],
                          [wA.b, h.b])
                    mmgrp(pg[:, 0:TC], pg.b, [(wA[:, c, 512 + f * 128:512 + (f + 1) * 128], h[:, c, :]) for c in range(8)],
                          [wA.b, h.b])
                    s = sgt[f % 3]
                    cx.op("act", lambda e, pg=pg, s=s: e.activation(out=s[:], in_=pg[:, 0:TC], func=AF.Sigmoid),
                          reads=[pg.b], full=[s.b])
                    cx.op("dve", lambda e, pa=pa, s=s, f=f: e.tensor_tensor(out=vbuf[:, f, 30:30 + TC], in0=pa[:, 0:TC],
                                                                            in1=s[:], op=ALU.mult),
                          reads=[pa.b, s.b], writes=[vbuf.b])

            def c_taps(bi, fs):
                for f in fs:
                    pc = getps()
                    Dg = Dg2[f % 2]
                    cx.op("pool", lambda e, Dg=Dg, f=f: e.tensor_tensor(
                        out=Dg[:], in0=ident_f[:].unsqueeze(1).to_broadcast([128, 31, 128]),
                        in1=cdw[:, f, :].unsqueeze(2).to_broadcast([128, 31, 128]), op=ALU.mult),
                        reads=[ident_f.b, cdw.b], full=[Dg.b])
                    mmgrp(pc[:, 0:TC], pc.b, [(Dg[:, k, :], vbuf[:, f, k:k + TC]) for k in range(31)],
                          [Dg.b, vbuf.b])
                    cx.op("act", lambda e, pc=pc, f=f: e.activation(out=cv[:, f, :], in_=pc[:, 0:TC], func=AF.Identity,
                                                                    bias=cb[:, f:f + 1], scale=1.0),
                          reads=[pc.b, cb.b], writes=[cv.b])
                if 3 in fs:
                    cx.op("pool", lambda e: e.tensor_copy(out=vbuf[:, :, 0:30], in_=vbuf[:, :, TC:TC + 30]),
                          reads=[vbuf.b], writes=[vbuf.b])

            def c_rest(bi):
                t0 = bi * TC
                h = hT[bi % 2]; yb = ysb[bi % 2]
                for hd in range(4):
                    pq_ = getps()
                    mmgrp(pq_[:, 0:TC], pq_.b, [(wA[:, c, 1024 + hd * 128:1024 + (hd + 1) * 128], h[:, c, :])
                                                for c in range(8)], [wA.b, h.b])
                    cx.op("dve", lambda e, pq_=pq_, hd=hd: e.tensor_copy(out=qb[:, hd, :], in_=pq_[:, 0:TC]),
                          reads=[pq_.b], writes=[qb.b])
                for f in range(4):
                    cx.op("act", lambda e, f=f: e.activation(out=sq[f % 2][:] if False else sqf[f][:], in_=cv[:, f, :],
                                                             func=AF.Square),
                          reads=[cv.b], full=[sqf[f].b])

                def att_scores(hd):
                    E = Eb[hd % 2]
                    for mc in range(2):
                        psc = getps()
                        mmgrp(psc[:, 0:TC], psc.b, [(kT[:, hd, mc * 128:(mc + 1) * 128], qb[:, hd, :])], [kT.b, qb.b])
                        cx.op("act", lambda e, psc=psc, E=E, mc=mc: e.activation(
                            out=E[:, mc, :], in_=psc[:, 0:TC], func=AF.Exp, scale=float(128 ** -0.5)),
                            reads=[psc.b], writes=[E.b])

                def att_out(hd):
                    E = Eb[hd % 2]
                    po = getps(); pd = getps()
                    mmgrp(po[:, 0:TC], po.b, [(vtok[:, mc, hd * 128:(hd + 1) * 128], E[:, mc, :]) for mc in range(2)],
                          [vtok.b, E.b])
                    mmgrp(pd[:, 0:TC], pd.b, [(ones_bf[:], E[:, mc, :]) for mc in range(2)], [ones_bf.b, E.b])
                    cx.op("dve", lambda e, pd=pd: e.reciprocal(out=rden[:], in_=pd[:, 0:TC]), reads=[pd.b], full=[rden.b])
                    cx.op("dve", lambda e, po=po, hd=hd: e.tensor_tensor(out=ob[:, hd, :], in0=po[:, 0:TC], in1=rden[:],
                                                                         op=ALU.mult),
                          reads=[po.b, rden.b], writes=[ob.b])

                att_scores(0)
                att_scores(1)
                pm = getps(); pq = getps()
                mmgrp(pm[:, 0:TC], pm.b, [(onesm[:], cv[:, f, :]) for f in range(4)], [onesm.b, cv.b])
                mmgrp(pq[:, 0:TC], pq.b, [(onesm[:], sqf[f][:]) for f in range(4)], [onesm.b] + [sqf[f].b for f in range(4)])
                cx.op("act", lambda e, pm=pm: e.copy(out=mean[:], in_=pm[:, 0:TC]), reads=[pm.b], full=[mean.b])
                cx.op("dve", lambda e: e.tensor_tensor(out=var[:], in0=mean[:], in1=mean[:], op=ALU.mult),
                      reads=[mean.b], full=[var.b])
                cx.op("dve", lambda e, pq=pq: e.tensor_tensor(out=var[:], in0=pq[:, 0:TC], in1=var[:], op=ALU.subtract),
                      reads=[pq.b], writes=[var.b])
                cx.op("dve", lambda e: e.tensor_scalar(out=var[:], in0=var[:], scalar1=float(EPS), scalar2=None,
                                                       op0=ALU.add), reads=[var.b], writes=[var.b])
                cx.op("act", lambda e: e.activation(out=lrs[:], in_=var[:], func=AF.Ln), reads=[var.b], full=[lrs.b])
                cx.op("act", lambda e: e.activation(out=lrs[:], in_=lrs[:], func=AF.Exp, scale=-0.5),
                      reads=[], writes=[lrs.b])
                cx.op("dve", lambda e: e.tensor_tensor(out=cv[:], in0=cv[:],
                                                       in1=mean[:].unsqueeze(1).to_broadcast([128, 4, TC]),
                                                       op=ALU.subtract), reads=[mean.b], writes=[cv.b])
                cx.op("dve", lambda e: e.tensor_tensor(out=cv[:], in0=cv[:],
                                                       in1=lrs[:].unsqueeze(1).to_broadcast([128, 4, TC]),
                                                       op=ALU.mult), reads=[lrs.b], writes=[cv.b])
                att_out(0)
                att_scores(2)
                att_out(1)
                att_scores(3)
                att_out(2)
                att_out(3)
                for f in range(4):
                    cx.op("act", lambda e, f=f: e.activation(out=cn[:, f, :], in_=cv[:, f, :], func=AF.Silu,
                                                             bias=lnb[:, f:f + 1], scale=lng[:, f:f + 1]),
                          reads=[cv.b, lnb.b, lng.b], writes=[cn.b])
                for j in range(8):
                    js = slice(j * 128, (j + 1) * 128)
                    pga = getps(); pyc = getps()
                    mmgrp(pga[:, 0:TC], pga.b, [(wG[:, c, j * 128:(j + 1) * 128], h[:, c, :]) for c in range(8)], [wG.b, h.b])
                    mmgrp(pyc[:, 0:TC], pyc.b, [(wco[:, f, js], cn[:, f, :]) for f in range(4)], [wco.b, cn.b])
                    s = sgt[0]
                    cx.op("act", lambda e, pga=pga, s=s: e.activation(out=s[:], in_=pga[:, 0:TC], func=AF.Sigmoid),
                          reads=[pga.b], full=[s.b])
                    cx.op("dve", lambda e, pyc=pyc, s=s: e.tensor_tensor(out=macc[:], in0=pyc[:, 0:TC], in1=s[:], op=ALU.mult),
                          reads=[pyc.b, s.b], full=[macc.b])
                    pgb = getps(); pza = getps(); pzb = getps()
                    mmgrp(pgb[:, 0:TC], pgb.b, [(wG[:, c, 1024 + j * 128:1024 + (j + 1) * 128], h[:, c, :]) for c in range(8)],
                          [wG.b, h.b])
                    mmgrp(pza[:, 0:TC], pza.b, [(wgl[:, f, js], yb[:, f, :]) for f in range(4)], [wgl.b, yb.b])
                    mmgrp(pzb[:, 0:TC], pzb.b, [(wgl[:, f, 1024 + j * 128:1024 + (j + 1) * 128], yb[:, f, :]) for f in range(4)],
                          [wgl.b, yb.b])
                    sb_ = sgt[1]; sz = sgt[2]
                    cx.op("act", lambda e, pgb=pgb, sb_=sb_: e.activation(out=sb_[:], in_=pgb[:, 0:TC], func=AF.Sigmoid),
                          reads=[pgb.b], full=[sb_.b])
                    cx.op("act", lambda e, pzb=pzb, sz=sz: e.activation(out=sz[:], in_=pzb[:, 0:TC], func=AF.Sigmoid),
                          reads=[pzb.b], full=[sz.b])
                    cx.op("dve", lambda e, pza=pza, sz=sz: e.tensor_tensor(out=mt1[:], in0=pza[:, 0:TC], in1=sz[:], op=ALU.mult),
                          reads=[pza.b, sz.b], full=[mt1.b])
                    cx.op("dve", lambda e, sb_=sb_: e.tensor_tensor(out=mt1[:], in0=mt1[:], in1=sb_[:], op=ALU.mult),
                          reads=[sb_.b], writes=[mt1.b])
                    cx.op("dve", lambda e: e.tensor_tensor(out=macc[:], in0=macc[:], in1=mt1[:], op=ALU.add),
                          reads=[mt1.b], writes=[macc.b])
                    pgc = getps(); pym = getps()
                    mmgrp(pgc[:, 0:TC], pgc.b, [(wG[:, c, 2048 + j * 128:2048 + (j + 1) * 128], h[:, c, :]) for c in range(8)],
                          [wG.b, h.b])
                    mmgrp(pym[:, 0:TC], pym.b, [(wmo[:, hd, js], ob[:, hd, :]) for hd in range(4)], [wmo.b, ob.b])
                    s = sgt[0]
                    cx.op("act", lambda e, pgc=pgc, s=s: e.activation(out=s[:], in_=pgc[:, 0:TC], func=AF.Sigmoid),
                          reads=[pgc.b], full=[s.b])
                    cx.op("dve", lambda e, pym=pym, s=s: e.tensor_tensor(out=mt2[:], in0=pym[:, 0:TC], in1=s[:], op=ALU.mult),
                          reads=[pym.b, s.b], full=[mt2.b])
                    cx.op("dve", lambda e, j=j: e.tensor_tensor(out=merged[:, j, :], in0=macc[:], in1=mt2[:], op=ALU.add),
                          reads=[macc.b, mt2.b], writes=[merged.b])

            def c_tail_a(bi):
                ntt = TC // 128
                tis = [bi * ntt + tt for tt in range(ntt)]
                for tt, ti in enumerate(tis):
                    xt_ = xt2[ti % 2]
                    cx.op("sp", lambda e, xt_=xt_, ti=ti: e.dma_start(out=xt_[:], in_=x_d[ti * 128:(ti + 1) * 128, :]),
                          full=[xt_.b], dma=True)
                for tt, ti in enumerate(tis):
                    xt_ = xt2[ti % 2]; x2 = xt_
                    for half in range(2):
                        po_ = getps()
                        mmgrp(po_[:], po_.b, [(merged[:, j, tt * 128:(tt + 1) * 128], wo[:, j, half * 512:(half + 1) * 512])
                                              for j in range(8)], [merged.b, wo.b])
                        cx.op("dve", lambda e, po_=po_, x2=x2, xt_=xt_, half=half: e.tensor_tensor(
                            out=x2[:, half * 512:(half + 1) * 512], in0=po_[:], in1=xt_[:, half * 512:(half + 1) * 512],
                            op=ALU.add), reads=[po_.b, xt_.b], writes=[x2.b])
                    cx.op("sp", lambda e, x2=x2, ti=ti: e.dma_start(out=x2_scr[ti * 128:(ti + 1) * 128, :], in_=x2[:]),
                          reads=[x2.b], dma=True)

            def c_tail_norm(ti):
                if True:
                    x2 = xt2[ti % 2]; hb2 = h2b[0]
                    cx.op("act", lambda e, x2=x2: e.activation(out=junk2[:], in_=x2[:], func=AF.Square, accum_out=ss2[:]),
                          reads=[x2.b], full=[junk2.b, ss2.b])
                    cx.op("act", lambda e: e.activation(out=rt2[:], in_=ss2[:], func=AF.Sqrt, scale=1.0 / D, bias=EPS),
                          reads=[ss2.b], full=[rt2.b])
                    cx.op("dve", lambda e: e.reciprocal(out=rs2[:], in_=rt2[:]), reads=[rt2.b], full=[rs2.b])
                    cx.op("dve", lambda e, x2=x2: e.scalar_tensor_tensor(out=h2f[:], in0=x2[:], scalar=rs2[:, 0:1],
                                                                         in1=gffn[:], op0=ALU.mult, op1=ALU.mult),
                          reads=[x2.b, rs2.b, gffn.b], full=[h2f.b])
                    cx.op("act", lambda e, hb2=hb2: e.copy(out=hb2[:], in_=h2f[:]), reads=[h2f.b], full=[hb2.b])
                    cx.op("sp", lambda e, hb2=hb2, ti=ti: e.dma_start(out=h2_scr[ti * 128:(ti + 1) * 128, :], in_=hb2[:]),
                          reads=[hb2.b], dma=True)

            def c_tail_pe(ti):
                if True:
                    pra = getps(); prb = getps()
                    for c in range(8):
                        pr = pra if c < 4 else prb
                        cx.op("pe", lambda e, pr=pr, c=c: e.transpose(out=pr[:, (c % 4) * 128:(c % 4 + 1) * 128],
                                                                      in_=h2f[:, c * 128:(c + 1) * 128], identity=ident_f[:]),
                              reads=[h2f.b, ident_f.b], writes=[pr.b])
                    cx.op("act", lambda e, pra=pra: e.copy(out=h2T[:, 0:4, :], in_=pra[:].rearrange("p (c t) -> p c t", c=4)),
                          reads=[pra.b], writes=[h2T.b])
                    cx.op("dve", lambda e, prb=prb: e.tensor_copy(out=h2T[:, 4:8, :],
                                                                  in_=prb[:].rearrange("p (c t) -> p c t", c=4)),
                          reads=[prb.b], writes=[h2T.b])
                    plg = getps()
                    mmgrp(plg[:, 0:36], plg.b, [(h2T[:, c, :], wr[:, c, :]) for c in range(8)], [h2T.b, wr.b])
                    cx.op("dve", lambda e, plg=plg, ti=ti: e.tensor_tensor(out=lg_all[:, ti, :], in0=plg[:, 0:36],
                                                                        in1=rbias[:], op=ALU.add),
                          reads=[plg.b, rbias.b], writes=[lg_all.b])

            NBR = min(NBC, KNB) if KCUT >= 2 else 0
            if NBR > 0:
                c_load_h(0); c_load_y(0); c_s2(0); c_taps(0, [0, 1, 2, 3])
            for bi in range(NBR):
                nxt = bi + 1 < NBR
                if nxt:
                    c_load_h(bi + 1)
                c_rest(bi)
                if nxt:
                    c_load_y(bi + 1)
                    c_s2(bi + 1)
                c_tail_a(bi)
                t_a, t_b = 2 * bi, 2 * bi + 1
                c_tail_norm(t_a)
                if nxt:
                    c_taps(bi + 1, [0, 1])
                c_tail_pe(t_a)
                c_tail_norm(t_b)
                if nxt:
                    c_taps(bi + 1, [2, 3])
                c_tail_pe(t_b)
            cx.barrier()
            alW.close()
            alR = Alloc(nc)
            RS = Buf("route")
            tri = alR.sb([128, 128], F32, "tri")
            ones_f = alR.sb([128, 128], F32, "ones_f")
            cx.op("sp", lambda e: e.dma_start(out=tri[:], in_=tri_d), full=[tri.b], dma=True)
            cx.op("pool", lambda e: e.memset(ones_f[:], 1.0), full=[ones_f.b])

            def rd(fn, extra_reads=(), extra_writes=()):
                cx.op("dve", fn, reads=[RS, lg_all.b] + list(extra_reads), writes=[RS] + list(extra_writes))

            def R(shape, name, dt=F32):
                return alR.sb(shape, dt, name)

            NTT = NT
            NEB_ = NEXP * NBLK
            gmax = R([128, NTT], "gmax"); ohg = R([128, NTT, 4], "ohg"); eg = R([128, NTT, 4], "eg")
            sumg = R([128, NTT], "sumg"); ptop = R([128, NTT], "ptop")
            selm = R([128, NTT, 4, 8], "selm"); sel = R([128, NTT, 8], "sel"); sel2 = R([128, NTT, 8], "sel2")
            m1_ = R([128, NTT], "m1_"); m2_ = R([128, NTT], "m2_"); oh1 = R([128, NTT, 8], "oh1"); oh2 = R([128, NTT, 8], "oh2")
            dm = R([128, NTT], "dm"); w1 = R([128, NTT], "w1"); w2 = R([128, NTT], "w2")
            M1 = R([128, NTT, 4, 8], "M1"); M2 = R([128, NTT, 4, 8], "M2"); Mc = R([128, NTT, 32], "Mc")
            Cex = R([128, NTT, 32], "Cex"); pos = R([128, NTT, 32], "pos"); bk = R([128, NTT, 32], "bk")
            sf = R([128, NTT, 32], "sf"); ov = R([128, NTT, 32], "ov"); tq = R([128, NTT, 32], "tq")
            sk = [R([128, NTT], f"sk{k}") for k in range(2)]; okk = R([128, NTT], "okk"); dd = R([128, NTT], "dd")
            si = [R([128, NTT], f"si{k}", I32) for k in range(2)]
            ent = [R([128, NTT, 4], f"ent{k}") for k in range(2)]
            le4 = lg_all[:, :, 4:36].rearrange("p t (g j) -> p t g j", g=4)

            def bc3(a, n):
                return a.unsqueeze(2).to_broadcast([128, NTT, n])

            rd(lambda e: e.tensor_reduce(out=gmax[:], in_=lg_all[:, :, 0:4], axis=AX.X, op=ALU.max))
            rd(lambda e: e.tensor_tensor(out=ohg[:], in0=lg_all[:, :, 0:4], in1=bc3(gmax[:], 4), op=ALU.is_equal))
            rd(lambda e: e.tensor_tensor(out=eg[:], in0=lg_all[:, :, 0:4], in1=bc3(gmax[:], 4), op=ALU.subtract))
            cx.op("act", lambda e: e.activation(out=eg[:], in_=eg[:], func=AF.Exp), reads=[RS], writes=[RS])
            rd(lambda e: e.tensor_reduce(out=sumg[:], in_=eg[:], axis=AX.X, op=ALU.add))
            rd(lambda e: e.reciprocal(out=ptop[:], in_=sumg[:]))
            rd(lambda e: e.tensor_tensor(out=selm[:], in0=le4,
                                         in1=ohg[:].unsqueeze(3).to_broadcast([128, NTT, 4, 8]), op=ALU.mult))
            rd(lambda e: e.tensor_reduce(out=sel[:], in_=selm[:].rearrange("p t g j -> p t j g"), axis=AX.X, op=ALU.add))
            rd(lambda e: e.tensor_reduce(out=m1_[:], in_=sel[:], axis=AX.X, op=ALU.max))
            rd(lambda e: e.tensor_tensor(out=oh1[:], in0=sel[:], in1=bc3(m1_[:], 8), op=ALU.is_equal))
            rd(lambda e: e.scalar_tensor_tensor(out=sel2[:], in0=oh1[:], scalar=-1e30, in1=sel[:], op0=ALU.mult, op1=ALU.add))
            rd(lambda e: e.tensor_reduce(out=m2_[:], in_=sel2[:], axis=AX.X, op=ALU.max))
            rd(lambda e: e.tensor_tensor(out=oh2[:], in0=sel2[:], in1=bc3(m2_[:], 8), op=ALU.is_equal))
            rd(lambda e: e.tensor_tensor(out=dm[:], in0=m1_[:], in1=m2_[:], op=ALU.subtract))
            cx.op("act", lambda e: e.activation(out=w1[:], in_=dm[:], func=AF.Sigmoid), reads=[RS], writes=[RS])
            rd(lambda e: e.tensor_tensor(out=w1[:], in0=w1[:], in1=ptop[:], op=ALU.mult))
            rd(lambda e: e.tensor_tensor(out=w2[:], in0=ptop[:], in1=w1[:], op=ALU.subtract))
            rd(lambda e: e.tensor_tensor(out=M1[:], in0=ohg[:].unsqueeze(3).to_broadcast([128, NTT, 4, 8]),
                                         in1=oh1[:].unsqueeze(2).to_broadcast([128, NTT, 4, 8]), op=ALU.mult))
            rd(lambda e: e.tensor_tensor(out=M2[:], in0=ohg[:].unsqueeze(3).to_broadcast([128, NTT, 4, 8]),
                                         in1=oh2[:].unsqueeze(2).to_broadcast([128, NTT, 4, 8]), op=ALU.mult))
            rd(lambda e: e.tensor_tensor(out=Mc[:], in0=M1[:].rearrange("p t g j -> p t (g j)"),
                                         in1=M2[:].rearrange("p t g j -> p t (g j)"), op=ALU.add))
            rd(lambda e: e.memset(Cex[:, 0, :], 0.0))
            for i in range(1, NTT):
                rd(lambda e, i=i: e.tensor_tensor(out=Cex[:, i, :], in0=Cex[:, i - 1, :], in1=Mc[:, i - 1, :], op=ALU.add))
            pp = [getps(), getps()]
            for i in range(NTT):
                pb_ = pp[i // 16]
                o_ = pb_[:, (i % 16) * 32:(i % 16 + 1) * 32]
                cx.op("pe", lambda e, o_=o_, i=i: e.matmul(o_, lhsT=tri[:], rhs=Mc[:, i, :], start=True, stop=False),
                      reads=[tri.b, RS], writes=[pb_.b])
                cx.op("pe", lambda e, o_=o_, i=i: e.matmul(o_, lhsT=ones_f[:], rhs=Cex[:, i, :], start=False, stop=True),
                      reads=[ones_f.b, RS], writes=[pb_.b])
            for hh in range(2):
                rd(lambda e, hh=hh: e.tensor_copy(out=pos[:, hh * 16:(hh + 1) * 16, :],
                                                  in_=pp[hh][:].rearrange("p (t x) -> p t x", t=16)), [pp[hh].b])
            rd(lambda e: e.tensor_single_scalar(out=bk[:], in_=pos[:], scalar=127.5, op=ALU.is_gt))
            for thr in range(2, NBLK):
                rd(lambda e, thr=thr: e.tensor_single_scalar(out=tq[:], in_=pos[:], scalar=128.0 * thr - 0.5, op=ALU.is_gt))
                rd(lambda e: e.tensor_tensor(out=bk[:], in0=bk[:], in1=tq[:], op=ALU.add))
            rd(lambda e: e.scalar_tensor_tensor(out=bk[:], in0=bk[:], scalar=float(1 - 128 * NEB_),
                                                in1=ecap[:].unsqueeze(1).to_broadcast([128, NTT, 32]),
                                                op0=ALU.mult, op1=ALU.add), [ecap.b])
            rd(lambda e: e.scalar_tensor_tensor(out=sf[:], in0=pos[:], scalar=float(NEB_), in1=bk[:],
                                                op0=ALU.mult, op1=ALU.add))
            rd(lambda e: e.tensor_single_scalar(out=ov[:], in_=pos[:], scalar=float(CAP) - 0.5, op=ALU.is_gt))
            for k, (Mk, wk) in enumerate(((M1, w1), (M2, w2))):
                Mk32 = Mk[:].rearrange("p t g j -> p t (g j)")
                rd(lambda e, Mk32=Mk32: e.tensor_tensor(out=tq[:], in0=Mk32, in1=sf[:], op=ALU.mult))
                rd(lambda e, k=k: e.tensor_reduce(out=sk[k][:], in_=tq[:], axis=AX.X, op=ALU.add))
                rd(lambda e, Mk32=Mk32: e.tensor_tensor(out=tq[:], in0=Mk32, in1=ov[:], op=ALU.mult))
                rd(lambda e: e.tensor_reduce(out=okk[:], in_=tq[:], axis=AX.X, op=ALU.add))
                rd(lambda e, k=k: e.tensor_scalar(out=dd[:], in0=sk[k][:], scalar1=trashp[:, 0:1], scalar2=None,
                                                  op0=ALU.subtract), [trashp.b])
                rd(lambda e: e.tensor_tensor(out=dd[:], in0=dd[:], in1=okk[:], op=ALU.mult))
                rd(lambda e, k=k: e.tensor_tensor(out=sk[k][:], in0=sk[k][:], in1=dd[:], op=ALU.subtract))
                rd(lambda e, k=k: e.tensor_copy(out=si[k][:], in_=sk[k][:]), (), [si[k].b])
                rd(lambda e, k=k: e.memset(ent[k][:], 0.0), (), [ent[k].b])
                rd(lambda e, k=k: e.tensor_copy(out=ent[k][:, :, 0], in_=tokid[:]), [tokid.b], [ent[k].b])
                rd(lambda e, k=k: e.tensor_scalar(out=ent[k][:, :, 1], in0=tokid[:], scalar1=float(k * ROWS), scalar2=None,
                                                  op0=ALU.add), [tokid.b], [ent[k].b])
                rd(lambda e, k=k, wk=wk: e.tensor_copy(out=ent[k][:, :, 2], in_=wk[:]), (), [ent[k].b])
            for i in range(NTT):
                for k in range(2):
                    cx.op("pool", lambda e, i=i, k=k: e.indirect_dma_start(
                        out=lst_d, out_offset=bass.IndirectOffsetOnAxis(ap=si[k][:, i:i + 1], axis=0),
                        in_=ent[k][:, i, :], in_offset=None),
                        reads=[si[k].b, ent[k].b, lstB], dma=True)
            cx.barrier()
            alR.close()
            alC.close()
        if stop_after in ("A", "B", "C"):
            pass
        else:
            alD = Alloc(nc)
            NEB = NEXP * NBLK
            lst_sb = alD.sb([128, NEB, 4], F32, "lst_sb")
            idx_i = alD.sb([128, NEB], I32, "idx_i")
            dst_i = alD.sb([128, NEB], I32, "dst_i")
            cx.op("sp", lambda e: e.dma_start(out=lst_sb[:], in_=lst_d[0:NEXP * CAP, :].rearrange("(s eb) w -> s eb w", s=128)),
                  reads=[lstB], full=[lst_sb.b], dma=True)
            cx.op("dve", lambda e: e.tensor_copy(out=idx_i[:], in_=lst_sb[:, :, 0]), reads=[lst_sb.b], full=[idx_i.b])
            cx.op("dve", lambda e: e.tensor_copy(out=dst_i[:], in_=lst_sb[:, :, 1]), reads=[lst_sb.b], full=[dst_i.b])
            NWB = 3
            Wg = [alD.sb([128, 8, 256], BF16, f"Wg{i}") for i in range(NWB)]
            Wu = [alD.sb([128, 8, 256], BF16, f"Wu{i}") for i in range(NWB)]
            Wd = [alD.sb([128, 2, 1024], BF16, f"Wd{i}") for i in range(NWB)]
            Gt = [alD.sb([128, D], BF16, f"Gt{i}") for i in range(3)]
            Xe = [alD.sb([128, 8, CAP], BF16, f"Xe{i}") for i in range(2)]
            sgl = [alD.sb([128, CAP], F32, f"sgl{i}") for i in range(2)]
            ae = [alD.sb([128, 2, CAP], BF16, f"ae{i}") for i in range(2)]
            Yt = [alD.sb([128, D], BF16, f"Yt{i}") for i in range(3)]

            Gt6 = Gt + [alD.sb([128, D], BF16, f"Gtx{i}") for i in range(3)]

            def load_w_dma(e_):
                p = e_ % NWB
                cx.op("sp", lambda e: e.dma_start(out=Wg[p][:].rearrange("p c n -> p (c n)"), in_=wbf_scr[e_, 0]),
                      full=[Wg[p].b], dma=True)
                cx.op("sp", lambda e: e.dma_start(out=Wu[p][:].rearrange("p c n -> p (c n)"), in_=wbf_scr[e_, 1]),
                      full=[Wu[p].b], dma=True)
                cx.op("sp", lambda e: e.dma_start(out=Wd[p][:].rearrange("p c n -> p (c n)"), in_=wbf_scr[e_, 2]),
                      full=[Wd[p].b], dma=True)

            def load_w_cast(e_):
                pass

            def gathers(e_):
                for blk in range(NBLK):
                    eb = e_ * NBLK + blk
                    G = Gt6[(e_ % 2) * 3 + blk]
                    cx.op("pool", lambda e, G=G, eb=eb: e.indirect_dma_start(
                        out=G[:], out_offset=None, in_=h2_scr,
                        in_offset=bass.IndirectOffsetOnAxis(ap=idx_i[:, eb:eb + 1], axis=0)),
                        reads=[idx_i.b, h2B], full=[G.b], dma=True)

            gi = [0]
            load_w_dma(0)
            load_w_dma(1)
            gathers(0)
            KNE = int(os.environ.get("KNE", str(NEXP)))
            for e_ in range(KNE):
                p = e_ % NWB
                if e_ + 2 < NEXP:
                    load_w_dma(e_ + 2)
                if e_ + 1 < NEXP:
                    gathers(e_ + 1)
                X = Xe[e_ % 2]
                for blk in range(NBLK):
                    G = Gt6[(e_ % 2) * 3 + blk]
                    pbk = psb[gi[0] % 2]
                    gi[0] += 1
                    for c in range(8):
                        cx.op("pe", lambda e, pbk=pbk, G=G, c=c: e.transpose(
                            out=pbk[:, c * 128:(c + 1) * 128], in_=G[:, c * 128:(c + 1) * 128], identity=ident_bf[:]),
                            reads=[G.b, ident_bf.b], writes=[pbk.b])
                    if blk % 2 == 0:
                        cx.op("act", lambda e, pbk=pbk, X=X, blk=blk: e.copy(
                            out=X[:, :, blk * 128:(blk + 1) * 128], in_=pbk[:].rearrange("p (c t) -> p c t", c=8)),
                            reads=[pbk.b], writes=[X.b])
                    else:
                        cx.op("dve", lambda e, pbk=pbk, X=X, blk=blk: e.tensor_copy(
                            out=X[:, :, blk * 128:(blk + 1) * 128], in_=pbk[:].rearrange("p (c t) -> p c t", c=8)),
                            reads=[pbk.b], writes=[X.b])
                a_ = ae[e_ % 2]
                for ft in range(2):
                    pg = getps(); pu = getps()
                    for c in range(8):
                        cx.op("pe", lambda e, pg=pg, c=c, ft=ft, X=X, p=p: e.matmul(
                            pg[:, 0:CAP], lhsT=Wg[p][:, c, ft * 128:(ft + 1) * 128], rhs=X[:, c, :],
                            start=(c == 0), stop=(c == 7)), reads=[Wg[p].b, X.b], writes=[pg.b])
                    for c in range(8):
                        cx.op("pe", lambda e, pu=pu, c=c, ft=ft, X=X, p=p: e.matmul(
                            pu[:, 0:CAP], lhsT=Wu[p][:, c, ft * 128:(ft + 1) * 128], rhs=X[:, c, :],
                            start=(c == 0), stop=(c == 7)), reads=[Wu[p].b, X.b], writes=[pu.b])
                    s = sgl[ft]
                    cx.op("act", lambda e, pg=pg, s=s: e.activation(out=s[:], in_=pg[:, 0:CAP], func=AF.Silu),
                          reads=[pg.b], full=[s.b])
                    cx.op("dve", lambda e, pu=pu, s=s, a_=a_, ft=ft: e.tensor_tensor(
                        out=a_[:, ft, :], in0=pu[:, 0:CAP], in1=s[:], op=ALU.mult),
                        reads=[pu.b, s.b], writes=[a_.b])
                for blk in range(NBLK):
                    eb = e_ * NBLK + blk
                    Y = Yt[eb % 3]
                    for half in range(2):
                        py = getps()
                        for ft in range(2):
                            cx.op("pe", lambda e, py=py, ft=ft, blk=blk, half=half, a_=a_, p=p: e.matmul(
                                py[:], lhsT=a_[:, ft, blk * 128:(blk + 1) * 128],
                                rhs=Wd[p][:, ft, half * 512:(half + 1) * 512], start=(ft == 0), stop=(ft == 1)),
                                reads=[a_.b, Wd[p].b], writes=[py.b])
                        if half == 0:
                            cx.op("dve", lambda e, py=py, Y=Y, eb=eb: e.tensor_scalar(
                                out=Y[:, 0:512], in0=py[:], scalar1=lst_sb[:, eb, 2:3], scalar2=None, op0=ALU.mult),
                                reads=[py.b, lst_sb.b], writes=[Y.b])
                        else:
                            cx.op("act", lambda e, py=py, Y=Y, eb=eb: e.activation(
                                out=Y[:, 512:1024], in_=py[:], func=AF.Copy, scale=lst_sb[:, eb, 2:3]),
                                reads=[py.b, lst_sb.b], writes=[Y.b])
                    cx.op("pool", lambda e, Y=Y, eb=eb: e.indirect_dma_start(
                        out=moe_scr, out_offset=bass.IndirectOffsetOnAxis(ap=dst_i[:, eb:eb + 1], axis=0),
                        in_=Y[:], in_offset=None), reads=[Y.b, dst_i.b, moeB], dma=True)
                if e_ + 1 < NEXP:
                    load_w_cast(e_ + 1)
            cx.barrier()
            alD.close()

            alE = Alloc(nc)
            gfin = alE.sb([128, D], F32, "gfin")
            cx.op("sp", lambda e: e.dma_start(out=gfin[:], in_=gfin_d.partition_broadcast(128)), full=[gfin.b], dma=True)
            NE_ = 4
            xa = [alE.sb([128, D], F32, f"xa{i}") for i in range(NE_)]
            m0 = [alE.sb([128, D], BF16, f"m0{i}") for i in range(NE_)]
            m1 = [alE.sb([128, D], BF16, f"m1{i}") for i in range(NE_)]
            ot = [alE.sb([128, D], F32, f"ot{i}") for i in range(NE_)]
            junk3 = alE.sb([128, D], BF16, "junk3")
            sse = [alE.sb([128, 1], F32, f"sse{i}") for i in range(NE_)]
            rte = [alE.sb([128, 1], F32, f"rte{i}") for i in range(NE_)]
            rse = [alE.sb([128, 1], F32, f"rse{i}") for i in range(NE_)]
            outB = Buf("out")

            def e_load(ti):
                p = ti % NE_
                rows = slice(ti * 128, (ti + 1) * 128)
                cx.op("sp", lambda e, p=p, rows=rows: e.dma_start(out=xa[p][:], in_=x2_scr[rows, :]),
                      full=[xa[p].b], dma=True)
                cx.op("sp", lambda e, p=p, rows=rows: e.dma_start(out=m0[p][:], in_=moe_scr[rows, :]),
                      full=[m0[p].b], dma=True)
                cx.op("sp", lambda e, p=p, ti=ti: e.dma_start(
                    out=m1[p][:], in_=moe_scr[ROWS + ti * 128:ROWS + (ti + 1) * 128, :]),
                    full=[m1[p].b], dma=True)

            for ti in range(min(NE_ - 1, NT)):
                e_load(ti)
            for ti in range(NT):
                p = ti % NE_
                rows = slice(ti * 128, (ti + 1) * 128)
                if ti + NE_ - 1 < NT:
                    e_load(ti + NE_ - 1)
                cx.op("pool", lambda e, p=p: e.tensor_tensor(out=xa[p][:], in0=xa[p][:], in1=m0[p][:], op=ALU.add),
                      reads=[m0[p].b], writes=[xa[p].b])
                cx.op("dve", lambda e, p=p: e.tensor_tensor(out=xa[p][:], in0=xa[p][:], in1=m1[p][:], op=ALU.add),
                      reads=[m1[p].b], writes=[xa[p].b])
                cx.op("act", lambda e, p=p: e.activation(out=junk3[:], in_=xa[p][:], func=AF.Square, accum_out=sse[p][:]),
                      reads=[xa[p].b], full=[junk3.b, sse[p].b])
                cx.op("act", lambda e, p=p: e.activation(out=rte[p][:], in_=sse[p][:], func=AF.Sqrt, scale=1.0 / D, bias=EPS),
                      reads=[sse[p].b], full=[rte[p].b])
                cx.op("dve", lambda e, p=p: e.reciprocal(out=rse[p][:], in_=rte[p][:]), reads=[rte[p].b], full=[rse[p].b])
                cx.op("dve", lambda e, p=p: e.scalar_tensor_tensor(out=ot[p][:], in0=xa[p][:], scalar=rse[p][:, 0:1],
                                                                   in1=gfin[:], op0=ALU.mult, op1=ALU.mult),
                      reads=[xa[p].b, rse[p].b, gfin.b], full=[ot[p].b])
                cx.op("sp", lambda e, p=p, rows=rows: e.dma_start(out=out_d[rows, :], in_=ot[p][:]),
                      reads=[ot[p].b], writes=[outB], dma=True)
            cx.barrier()
            alE.close()
        cx.barrier()
        cx.emit(block)
        print("waits", cx.nwait, "instrs", {e: cx.cnt[e] for e in cx.ENG}, "signals", {e: len(cx.waited[e]) for e in cx.ENG})
    return nc


def host_consts():
    c = {}
    c["ident_bf"] = np.eye(128, dtype=np.float32).astype(ml_dtypes.bfloat16)
    c["ident_f"] = np.eye(128, dtype=np.float32)
    psel = np.zeros((128, 8, 240), np.float32)
    for a in range(8):
        for i in range(16):
            psel[a * 16 + i, a, 7 * 16 + i] = 1.0
    c["psel"] = psel.astype(ml_dtypes.bfloat16)
    kk = np.arange(128) // 16
    c["cmask"] = (kk[None, :] >= kk[:, None]).astype(np.float32)
    c["tri"] = (np.arange(128)[:, None] < np.arange(128)[None, :]).astype(np.float32)
    c["ecap"] = np.ascontiguousarray(np.broadcast_to((np.arange(32) * NBLK).astype(np.float32)[None, :], (128, 32)))
    c["tokid"] = (np.arange(NT)[None, :] * 128 + np.arange(128)[:, None]).astype(np.float32)
    li = np.zeros((NEXP * CAP + 128, 4), np.float32)
    li[:, 0] = SEQ + ((np.arange(NEXP * CAP + 128) // (NEXP * NBLK)) % 128)
    li[:, 1] = li[:, 0]
    c["trashp"] = (NEXP * CAP + np.arange(128)).astype(np.float32).reshape(128, 1)
    c["lst_init"] = li
    return c


def relayout_kn(w):
    K, N = w.shape
    return np.ascontiguousarray(w.reshape(K // 128, 128, N).transpose(1, 0, 2))


def relayout_pc(w):
    E, K, N = w.shape
    return np.ascontiguousarray(w.reshape(E, K // 128, 128, N).transpose(0, 2, 1, 3))


def pair_layout(a):
    rest = a.shape[2:]
    a = a.reshape((16, 2, 64) + rest)
    a = np.moveaxis(a, 0, 2)
    return np.ascontiguousarray(a.reshape((128, 16) + rest))


def make_inmap(inputs, b, consts=None):
    f = lambda a: np.ascontiguousarray(a, dtype=np.float32)
    m = {"x": f(inputs["x"][b]),
         "g_mix": f(inputs["g_mix"]),
         "w_in": relayout_kn(f(inputs["w_in"][0]))}
    m["lamre_l"] = pair_layout(f(inputs["ssm_lambda_re"][0]))
    m["lamim_l"] = pair_layout(f(inputs["ssm_lambda_im"][0]))
    m["logdt_l"] = pair_layout(np.broadcast_to(f(inputs["ssm_log_dt"][0])[:, None], (32, 64)))
    m["bre_l"] = pair_layout(f(inputs["ssm_b_re"][0]))
    m["bim_l"] = pair_layout(f(inputs["ssm_b_im"][0]))
    m["cre_l"] = pair_layout(f(inputs["ssm_c_re"][0]).transpose(0, 2, 1))
    m["cim_l"] = pair_layout(f(inputs["ssm_c_im"][0]).transpose(0, 2, 1))
    m["d_l"] = np.ascontiguousarray(np.tile(f(inputs["ssm_d"][0]).reshape(32, 16).T, (8, 1)))
    m["mem"] = f(inputs["mem"][b])
    for k_, n_ in (("g_mem", "g_mem"), ("g_ffn", "g_ffn")):
        m[n_] = f(inputs[k_])
    m["g_final"] = f(inputs["g_final"]).reshape(1, D)
    m["w_mem_kv"] = relayout_kn(f(inputs["w_mem_kv"][0])); m["w_mem_out"] = relayout_kn(f(inputs["w_mem_out"][0]))
    m["w_conv_out"] = relayout_kn(f(inputs["w_conv_out"][0])); m["w_ssm_glu"] = relayout_kn(f(inputs["w_ssm_glu"][0]))
    m["w_out"] = relayout_kn(f(inputs["w_out"][0]))
    m["w_router"] = np.ascontiguousarray(np.concatenate([f(inputs["w_router_group"][0]),
                                                         f(inputs["w_router_expert"][0])], axis=1))
    m["b_router"] = np.ascontiguousarray(np.concatenate([f(inputs["b_router_group"][0]),
                                                         f(inputs["b_router_expert"][0])])[None, :])
    m["cdw_l"] = np.ascontiguousarray(f(inputs["conv_dw"][0]).T.reshape(4, 128, 31).transpose(1, 0, 2))
    m["cb_l"] = np.ascontiguousarray(f(inputs["conv_dw_bias"][0]).reshape(4, 128).T)
    m["lng_l"] = np.ascontiguousarray(f(inputs["conv_ln_g"][0]).reshape(4, 128).T)
    m["lnb_l"] = np.ascontiguousarray(f(inputs["conv_ln_b"][0]).reshape(4, 128).T)
    if consts is not None and "w_exp_gate" in consts:
        for k_ in ("w_exp_gate", "w_exp_up", "w_exp_down"):
            m[k_] = consts[k_]
    else:
        m["w_exp_gate"] = relayout_pc(f(inputs["w_exp_gate"][0]))
        m["w_exp_up"] = relayout_pc(f(inputs["w_exp_up"][0]))
        m["w_exp_down"] = relayout_pc(f(inputs["w_exp_down"][0]))
    m.update(consts if consts is not None else host_consts())
    return m


def kernel(**inputs):
    nc = build()
    consts = host_consts()
    f32 = lambda a: np.ascontiguousarray(a, dtype=np.float32)
    for k_ in ("w_exp_gate", "w_exp_up", "w_exp_down"):
        consts[k_] = relayout_pc(f32(inputs[k_][0]))
    in_maps = [make_inmap(inputs, b, consts) for b in range(NCORES)]
    res = run_bass_kernel_spmd(nc, in_maps, core_ids=list(range(NCORES)))
    return np.stack([r["out"] for r in res.results], axis=0)
```
